# Optimizing a Trainium2 kernel written in Bass

```python
import math
import jax, jax.numpy as jnp
from jax import lax
import numpy as np

D_MODEL = 2048
BATCH = 8
SEQ = 2048
DEPTH = 1
DEC_BATCH = 32
DEC_SEQ = 1
PAST_LEN = 16384
PAGE_SIZE = 128

D_MIX = D_MODEL
HEAD_DIM = 128
N_HEADS_A = D_MIX // (2 * HEAD_DIM)
DK_A = HEAD_DIM
DV_A = HEAD_DIM
KEY_DIM_A = N_HEADS_A * DK_A
VAL_DIM_A = N_HEADS_A * DV_A
CONV_DIM = 2 * KEY_DIM_A + VAL_DIM_A
CONV_W = 4
CHUNK = 64
N_HEADS_B = D_MIX // (2 * HEAD_DIM)
N_KV_B = 2
GQA_GROUP = N_HEADS_B // N_KV_B
N_IDX_HEADS = 16
D_IDX = 128
TOPK_MAX = 256
Q_BLOCK = 128
N_BUCKETS = 32
MAX_DISTANCE = 128
D_FF = 4 * D_MODEL
EPS = 1e-6
F32 = jnp.float32

SPLIT_SIZES = (CONV_DIM, N_HEADS_A, N_HEADS_A, VAL_DIM_A,
               N_HEADS_B * HEAD_DIM, N_KV_B * HEAD_DIM, N_KV_B * HEAD_DIM,
               N_IDX_HEADS * D_IDX, N_IDX_HEADS, D_IDX)
SPLIT_OFFSETS = tuple(int(o) for o in np.cumsum(SPLIT_SIZES)[:-1])
N_PROJ = int(sum(SPLIT_SIZES))

kernel_name = "hymba_gdn_dsa_sandwich_adaln_decode_step"


def rmsnorm(x, g):
    xf = x.astype(F32)
    y = xf * lax.rsqrt(jnp.mean(xf * xf, axis=-1, keepdims=True) + EPS)
    return (y * g.astype(F32)).astype(x.dtype)


def layernorm(x, g, b):
    xf = x.astype(F32)
    mu = jnp.mean(xf, axis=-1, keepdims=True)
    var = jnp.mean(jnp.square(xf - mu), axis=-1, keepdims=True)
    return ((xf - mu) * lax.rsqrt(var + EPS) * g.astype(F32) + b.astype(F32)).astype(x.dtype)


def l2norm(x):
    xf = x.astype(F32)
    return xf * lax.rsqrt(jnp.sum(xf * xf, axis=-1, keepdims=True) + EPS)


def adaln(c, w_ada, b_ada):
    m = jax.nn.silu(c) @ w_ada + b_ada
    return jnp.split(m[:, None, :], 6, axis=-1)


def causal_dwconv(x_pad, w):
    y = lax.conv_general_dilated(x_pad, w.astype(x_pad.dtype)[:, None, :], window_strides=(1,),
                                 padding="VALID", dimension_numbers=("NWC", "WIO", "NWC"),
                                 feature_group_count=x_pad.shape[-1])
    return jax.nn.silu(y)


def gdn_chunked(q, k, v, g, beta, s0):
    B, T, H, _ = q.shape
    n = T // CHUNK

    def to_chunks(t):
        t = jnp.moveaxis(t, 2, 1)
        return t.reshape((B, H, n, CHUNK) + t.shape[3:])

    q, k, v, g, beta = (to_chunks(t) for t in (q, k, v, g, beta))
    g = jnp.cumsum(g, axis=-1)
    k_beta = k * beta[..., None]
    v_beta = v * beta[..., None]
    causal = jnp.tril(jnp.ones((CHUNK, CHUNK), dtype=bool))
    strict = jnp.tril(jnp.ones((CHUNK, CHUNK), dtype=bool), k=-1)
    diff = g[..., :, None] - g[..., None, :]
    decay = jnp.where(causal, jnp.exp(jnp.where(causal, diff, 0.0)), 0.0)
    lower = jnp.where(strict, jnp.einsum("bhncd,bhnsd->bhncs", k_beta, k) * decay, 0.0)
    eye = jnp.eye(CHUNK, dtype=F32)
    t_inv = lax.linalg.triangular_solve(eye + lower, jnp.broadcast_to(eye, lower.shape),
                                        left_side=True, lower=True, unit_diagonal=True)
    u = jnp.einsum("bhncs,bhnsv->bhncv", t_inv, v_beta)
    w = jnp.einsum("bhncs,bhnsd->bhncd", t_inv, k_beta * jnp.exp(g)[..., None])
    intra = jnp.where(causal, jnp.einsum("bhncd,bhnsd->bhncs", q, k) * decay, 0.0)

    def step(s, inp):
        q_c, k_c, u_c, w_c, g_c, a_c = inp
        v_new = u_c - jnp.einsum("bhcd,bhdv->bhcv", w_c, s)
        o_c = (jnp.einsum("bhcd,bhdv->bhcv", q_c * jnp.exp(g_c)[..., None], s)
               + jnp.einsum("bhcs,bhsv->bhcv", a_c, v_new))
        g_last = g_c[..., -1]
        k_dec = k_c * jnp.exp(g_last[..., None] - g_c)[..., None]
        s = s * jnp.exp(g_last)[..., None, None] + jnp.einsum("bhcd,bhcv->bhdv", k_dec, v_new)
        return s, o_c

    xs = tuple(jnp.moveaxis(t, 2, 0) for t in (q, k, u, w, g, intra))
    s_new, o = lax.scan(step, s0, xs)
    o = jnp.moveaxis(o, 0, 2).reshape(B, H, T, -1)
    return jnp.moveaxis(o, 1, 2), s_new


def gdn_recurrent(q, k, v, g, beta, s0):
    def step(s, inp):
        q_t, k_t, v_t, g_t, b_t = inp
        s = s * jnp.exp(g_t)[..., None, None]
        kv = jnp.einsum("bhd,bhdv->bhv", k_t, s)
        delta = (v_t - kv) * b_t[..., None]
        s = s + k_t[..., :, None] * delta[..., None, :]
        return s, jnp.einsum("bhd,bhdv->bhv", q_t, s)

    xs = tuple(jnp.moveaxis(t, 1, 0) for t in (q, k, v, g, beta))
    s_new, o = lax.scan(step, s0, xs)
    return jnp.moveaxis(o, 0, 1), s_new


def gated_deltanet(conv_in, a_raw, b_raw, z, conv_prev, ssm_prev, conv_w, a_log, dt_bias, norm_g, chunked):
    B, T, _ = conv_in.shape
    x_pad = jnp.concatenate([conv_prev.astype(conv_in.dtype), conv_in], axis=1)
    conv_new = x_pad[:, x_pad.shape[1] - (CONV_W - 1):]
    y = causal_dwconv(x_pad, conv_w)
    q, k, v = jnp.split(y, (KEY_DIM_A, 2 * KEY_DIM_A), axis=-1)
    q = l2norm(q.reshape(B, T, N_HEADS_A, DK_A)) * (DK_A ** -0.5)
    k = l2norm(k.reshape(B, T, N_HEADS_A, DK_A))
    v = v.reshape(B, T, N_HEADS_A, DV_A).astype(F32)
    g = -jnp.exp(a_log.astype(F32)) * jax.nn.softplus(a_raw.astype(F32) + dt_bias.astype(F32))
    beta = jax.nn.sigmoid(b_raw.astype(F32))
    s0 = ssm_prev.astype(F32)
    if chunked:
        o, s_new = gdn_chunked(q, k, v, g, beta, s0)
    else:
        o, s_new = gdn_recurrent(q, k, v, g, beta, s0)
    zg = z.reshape(B, T, N_HEADS_A, DV_A).astype(F32)
    o = rmsnorm(o, norm_g) * jax.nn.silu(zg)
    return o.reshape(B, T, VAL_DIM_A).astype(conv_in.dtype), s_new, conv_new


def t5_bucket(dist):
    n = jnp.maximum(dist, 0)
    max_exact = N_BUCKETS // 2
    nf = jnp.maximum(n, 1).astype(F32)
    large = max_exact + (jnp.log(nf / max_exact) / math.log(MAX_DISTANCE / max_exact)
                         * (N_BUCKETS - max_exact)).astype(jnp.int32)
    large = jnp.minimum(large, N_BUCKETS - 1)
    return jnp.where(n < max_exact, n, large)


def indexer_scores(q_i, w_i, k_i):
    s = jax.nn.relu(jnp.einsum("bqhd,bld->bqhl", q_i, k_i).astype(F32))
    return jnp.einsum("bqhl,bqh->bql", s, w_i.astype(F32)) * (D_IDX ** -0.5)


def select_topk(score, q_pos, key_pos, n_sel):
    score = jnp.where(key_pos[None, None, :] <= q_pos[None, :, None], score, -jnp.inf)
    top, idx = lax.top_k(score, n_sel)
    return idx, jnp.isfinite(top)


def attend_selected(q, k_sel, v_sel, dist, valid, rel_bias):
    B, Tq = q.shape[:2]
    n_sel = k_sel.shape[2]
    qg = q.reshape(B, Tq, N_KV_B, GQA_GROUP, HEAD_DIM)
    logits = jnp.einsum("bqngd,bqknd->bqngk", qg, k_sel).astype(F32) * (HEAD_DIM ** -0.5)
    bias = rel_bias.astype(F32)[t5_bucket(dist)]
    bias = bias.reshape(B, Tq, n_sel, N_KV_B, GQA_GROUP).transpose(0, 1, 3, 4, 2)
    logits = jnp.where(valid[:, :, None, None, :], logits + bias, -jnp.inf)
    p = jax.nn.softmax(logits, axis=-1).astype(v_sel.dtype)
    o = jnp.einsum("bqngk,bqknd->bqngd", p, v_sel)
    return o.reshape(B, Tq, N_HEADS_B * HEAD_DIM)


def sparse_attention_prompt(q, k, v, q_i, w_i, k_i, rel_bias):
    B, T = q.shape[:2]
    n_sel = min(TOPK_MAX, T // 4)
    n_blk = T // Q_BLOCK
    key_pos = jnp.arange(T, dtype=jnp.int32)
    b_idx = jnp.arange(B)[:, None, None]

    def blocks(t):
        return jnp.moveaxis(t.reshape((B, n_blk, Q_BLOCK) + t.shape[2:]), 1, 0)

    def one_block(args):
        q_blk, qi_blk, wi_blk, q_pos = args
        score = indexer_scores(qi_blk, wi_blk, k_i)
        idx, valid = select_topk(score, q_pos, key_pos, n_sel)
        return attend_selected(q_blk, k[b_idx, idx], v[b_idx, idx],
                               q_pos[None, :, None] - idx, valid, rel_bias)

    q_pos = key_pos.reshape(n_blk, Q_BLOCK)
    out = lax.map(one_block, (blocks(q), blocks(q_i), blocks(w_i), q_pos))
    return jnp.moveaxis(out, 0, 1).reshape(B, T, N_HEADS_B * HEAD_DIM)


def sparse_attention_sample(q, k_new, v_new, q_i, w_i, k_i_new, cache_k, cache_v, cache_kidx,
                            page_table, rel_bias):
    B, T = q.shape[:2]
    n_pages = page_table.shape[1]
    past = n_pages * PAGE_SIZE
    n_keys = past + T
    n_sel = min(TOPK_MAX, n_keys // 4)
    ki_past = cache_kidx[page_table].reshape(B, past, D_IDX)
    ki_all = jnp.concatenate([ki_past.astype(k_i_new.dtype), k_i_new], axis=1)
    q_pos = past + jnp.arange(T, dtype=jnp.int32)
    key_pos = jnp.arange(n_keys, dtype=jnp.int32)
    score = indexer_scores(q_i, w_i, ki_all)
    idx, valid = select_topk(score, q_pos, key_pos, n_sel)
    b_idx = jnp.arange(B)[:, None, None]
    p_idx = jnp.minimum(idx, past - 1)
    phys = page_table[b_idx, p_idx // PAGE_SIZE]
    slot = p_idx % PAGE_SIZE
    n_idx = jnp.clip(idx - past, 0, T - 1)
    in_past = (idx < past)[..., None, None]
    k_sel = jnp.where(in_past, cache_k[phys, slot].astype(k_new.dtype), k_new[b_idx, n_idx])
    v_sel = jnp.where(in_past, cache_v[phys, slot].astype(v_new.dtype), v_new[b_idx, n_idx])
    return attend_selected(q, k_sel, v_sel, q_pos[None, :, None] - idx, valid, rel_bias)


def layer_step(x, c, conv_prev, ssm_prev, past, page_table, w_ada, b_ada, pre1_g, post1_g,
               pre2_g, post2_g, w_in, w_out, conv_w, a_log, dt_bias, gdn_norm_g,
               idx_knorm_g, idx_knorm_b, rel_bias, w_ff1, w_ff2):
    B, T, _ = x.shape
    sh1, sc1, g1, sh2, sc2, g2 = adaln(c, w_ada, b_ada)
    h = rmsnorm(x, pre1_g) * (1 + sc1) + sh1
    (conv_in, a_raw, b_raw, z, q_b, k_b, v_b, q_i, w_i, k_i) = jnp.split(h @ w_in, SPLIT_OFFSETS, axis=-1)
    o_a, ssm_new, conv_new = gated_deltanet(conv_in, a_raw, b_raw, z, conv_prev, ssm_prev, conv_w,
                                            a_log, dt_bias, gdn_norm_g, past is None)
    q_b = q_b.reshape(B, T, N_HEADS_B, HEAD_DIM)
    k_b = k_b.reshape(B, T, N_KV_B, HEAD_DIM)
    v_b = v_b.reshape(B, T, N_KV_B, HEAD_DIM)
    q_i = q_i.reshape(B, T, N_IDX_HEADS, D_IDX)
    w_i = w_i * (N_IDX_HEADS ** -0.5)
    k_i = layernorm(k_i, idx_knorm_g, idx_knorm_b)
    if past is None:
        o_b = sparse_attention_prompt(q_b, k_b, v_b, q_i, w_i, k_i, rel_bias)
    else:
        o_b = sparse_attention_sample(q_b, k_b, v_b, q_i, w_i, k_i, past[0], past[1], past[2],
                                      page_table, rel_bias)
    mix = jnp.concatenate([o_a, o_b.astype(o_a.dtype)], axis=-1) @ w_out
    x = x + g1 * rmsnorm(mix, post1_g)
    h = rmsnorm(x, pre2_g) * (1 + sc2) + sh2
    f = jnp.square(jax.nn.relu(h @ w_ff1)) @ w_ff2
    x = x + g2 * rmsnorm(f, post2_g)
    return x, k_b, v_b, k_i, ssm_new, conv_new


def _normal(k, shape, scale=1.0):
    return scale * jax.random.normal(k, shape, F32)


def setup_inputs(seed: int = 0) -> dict:
    key = jax.random.key(seed)
    ks = jax.random.split(key, 32)
    n_pages = PAST_LEN // PAGE_SIZE
    n_used = DEC_BATCH * n_pages
    n_pool = n_used + max(1, n_used // 4)
    page_table = jax.random.permutation(ks[9], n_pool)[:n_used].reshape(DEC_BATCH, n_pages).astype(jnp.int32)
    dt = jnp.exp(jax.random.uniform(ks[16], (DEPTH, N_HEADS_A), F32, math.log(1e-3), math.log(1e-1)))
    dt_bias = dt + jnp.log(-jnp.expm1(-dt))
    a_log = jnp.log(jax.random.uniform(ks[15], (DEPTH, N_HEADS_A), F32, 1.0, 16.0))
    return {
        "x_prompt": _normal(ks[0], (BATCH, SEQ, D_MODEL)),
        "x_sample": _normal(ks[1], (DEC_BATCH, DEC_SEQ, D_MODEL)),
        "c_prompt": _normal(ks[2], (BATCH, D_MODEL)),
        "c_sample": _normal(ks[3], (DEC_BATCH, D_MODEL)),
        "cache_k": _normal(ks[4], (DEPTH, n_pool, PAGE_SIZE, N_KV_B, HEAD_DIM)),
        "cache_v": _normal(ks[5], (DEPTH, n_pool, PAGE_SIZE, N_KV_B, HEAD_DIM)),
        "cache_kidx": _normal(ks[6], (DEPTH, n_pool, PAGE_SIZE, D_IDX)),
        "state_ssm": _normal(ks[7], (DEPTH, DEC_BATCH, N_HEADS_A, DK_A, DV_A), 0.1),
        "state_conv": _normal(ks[8], (DEPTH, DEC_BATCH, CONV_W - 1, CONV_DIM)),
        "page_table": page_table,
        "w_ada": _normal(ks[10], (DEPTH, D_MODEL, 6 * D_MODEL), 0.5 * D_MODEL ** -0.5),
        "b_ada": _normal(ks[11], (DEPTH, 6 * D_MODEL), 0.01),
        "pre1_g": 1.0 + _normal(ks[12], (DEPTH, D_MODEL), 0.05),
        "post1_g": 1.0 + _normal(ks[13], (DEPTH, D_MODEL), 0.05),
        "pre2_g": 1.0 + _normal(ks[14], (DEPTH, D_MODEL), 0.05),
        "post2_g": 1.0 + _normal(ks[17], (DEPTH, D_MODEL), 0.05),
        "w_in": _normal(ks[18], (DEPTH, D_MODEL, N_PROJ), D_MODEL ** -0.5),
        "w_out": _normal(ks[19], (DEPTH, D_MIX, D_MODEL), D_MIX ** -0.5),
        "conv_w": _normal(ks[20], (DEPTH, CONV_W, CONV_DIM), CONV_W ** -0.5),
        "a_log": a_log,
        "dt_bias": dt_bias,
        "gdn_norm_g": 1.0 + _normal(ks[21], (DEPTH, DV_A), 0.05),
        "idx_knorm_g": 1.0 + _normal(ks[22], (DEPTH, D_IDX), 0.05),
        "idx_knorm_b": _normal(ks[23], (DEPTH, D_IDX), 0.02),
        "rel_bias": _normal(ks[24], (N_BUCKETS, N_HEADS_B), 0.5),
        "w_ff1": _normal(ks[25], (DEPTH, D_MODEL, D_FF), D_MODEL ** -0.5),
        "w_ff2": _normal(ks[26], (DEPTH, D_FF, D_MODEL), D_FF ** -0.5),
    }


def reference(x_prompt, x_sample, c_prompt, c_sample, cache_k, cache_v, cache_kidx, state_ssm,
              state_conv, page_table, w_ada, b_ada, pre1_g, post1_g, pre2_g, post2_g, w_in, w_out,
              conv_w, a_log, dt_bias, gdn_norm_g, idx_knorm_g, idx_knorm_b, rel_bias, w_ff1, w_ff2):
    h_p, h_s = x_prompt, x_sample
    new_p, new_s = [], []
    for l in range(DEPTH):
        conv0 = jnp.zeros((x_prompt.shape[0], CONV_W - 1, CONV_DIM), x_prompt.dtype)
        ssm0 = jnp.zeros((x_prompt.shape[0], N_HEADS_A, DK_A, DV_A), F32)
        h_p, *st_p = layer_step(h_p, c_prompt, conv0, ssm0, None, None, w_ada[l], b_ada[l],
                                pre1_g[l], post1_g[l], pre2_g[l], post2_g[l], w_in[l], w_out[l],
                                conv_w[l], a_log[l], dt_bias[l], gdn_norm_g[l], idx_knorm_g[l],
                                idx_knorm_b[l], rel_bias, w_ff1[l], w_ff2[l])
        new_p.append(st_p)
        h_s, *st_s = layer_step(h_s, c_sample, state_conv[l], state_ssm[l],
                                (cache_k[l], cache_v[l], cache_kidx[l]), page_table, w_ada[l], b_ada[l],
                                pre1_g[l], post1_g[l], pre2_g[l], post2_g[l], w_in[l], w_out[l],
                                conv_w[l], a_log[l], dt_bias[l], gdn_norm_g[l], idx_knorm_g[l],
                                idx_knorm_b[l], rel_bias, w_ff1[l], w_ff2[l])
        new_s.append(st_s)
    p_k, p_v, p_kidx, p_ssm, p_conv = [jnp.stack([st[i] for st in new_p]) for i in range(5)]
    s_k, s_v, s_kidx, s_ssm, s_conv = [jnp.stack([st[i] for st in new_s]) for i in range(5)]
    return (h_p, h_s, p_k, p_v, p_kidx, p_ssm, p_conv, s_k, s_v, s_kidx, s_ssm, s_conv)
```

```python
import math
import numpy as np
import concourse.bass as bass
import concourse.mybir as mybir
from concourse.bass_utils import run_bass_kernel_spmd

F32 = mybir.dt.float32
BF16 = mybir.dt.bfloat16
I32 = mybir.dt.int32
ALU = mybir.AluOpType
AF = mybir.ActivationFunctionType
AX = mybir.AxisListType

ENGS = ("pe", "dve", "act", "pool", "sp")

D = 2048
T = 2048
TS = 4
TT = T + TS
NT = 16
NPROJ = 7840
DFF = 8192
EPS = 1e-6
NPAGES = 128
PAGE = 128
O_CONV, O_A, O_B, O_Z, O_QB, O_KB, O_VB, O_QI, O_WI, O_KI = 0, 3072, 3080, 3088, 4112, 5136, 5392, 5648, 7696, 7712


def _dsize(dt):
    return {F32: 4, BF16: 2, I32: 4}[dt]


class Buf:
    def __init__(self, name, ap):
        self.name = name
        self.ap = ap

    def __getitem__(self, key):
        return self.ap[key]


class KB:
    def __init__(self, n_dma_sems=(24, 8, 24)):
        self.nc = bass.Bass("TRN2", target_bir_lowering=False)
        nc = self.nc
        self.ops = {e: [] for e in ENGS}
        self._ctx = []
        self.psem = {}
        self.cnt = {e: 0 for e in ENGS}
        for e in ENGS:
            self.psem[e] = self._enter(nc.semaphore("p_" + e))
        self.dsem = {}
        self.dcnt = {}
        self.drr = {}
        for q, n in zip(("sp", "act", "pool"), n_dma_sems):
            self.dsem[q] = [self._enter(nc.semaphore(f"d_{q}{i}")) for i in range(n)]
            self.dcnt[q] = [0] * n
            self.drr[q] = 0
        self.known = {e: {} for e in ENGS}
        self.state = {}
        self.semobj = {}
        for e in ENGS:
            self.semobj[("p", e)] = self.psem[e]
        for q in self.dsem:
            for i, s in enumerate(self.dsem[q]):
                self.semobj[("d", q, i)] = s
        self.n_ops = 0
        self.arena = None
        self.aoff = 0
        self.awords = 0
        self.nbank = 0
        self.reserved = set()

    def _enter(self, cm):
        v = cm.__enter__()
        self._ctx.append(cm)
        return v

    def init_arena(self, words):
        self.arena = self._enter(self.nc.sbuf_tensor("arena", [128, words], F32))
        self.awords = words
        self.aoff = 0

    def alloc(self, name, shape, dt=F32):
        p = shape[0]
        n = int(np.prod(shape[1:]))
        words = (n * _dsize(dt) + 3) // 4
        words = (words + 7) // 8 * 8
        assert self.aoff + words <= self.awords, f"arena overflow at {name}: {self.aoff + words} > {self.awords}"
        ap = self.arena[0:p, self.aoff:self.aoff + words]
        self.aoff += words
        if dt != F32:
            ap = ap.bitcast(dt)
        ap = ap[:, 0:n]
        if len(shape) == 3:
            ap = ap.rearrange("p (a b) -> p a b", a=shape[1])
        elif len(shape) == 4:
            ap = ap.rearrange("p (a b c) -> p a b c", a=shape[1], b=shape[2])
        return Buf(name, ap)

    def mark(self):
        return self.aoff

    def release(self, m):
        self.aoff = m

    def psum_init(self):
        self.pbanks = []
        for i in range(4):
            t = self._enter(self.nc.psum_tensor(f"pp{i}", [128, 1024], F32))
            self.pbanks.append(Buf(f"bank{2 * i}", t[:, 0:512]))
            self.pbanks.append(Buf(f"bank{2 * i + 1}", t[:, 512:1024]))
        self.pdbl = [self._dbl(i) for i in range(4)]

    def _dbl(self, i):
        return None

    def bank(self):
        while True:
            b = self.pbanks[self.nbank % 8]
            self.nbank += 1
            if b.name not in self.reserved:
                return b

    @staticmethod
    def _key(x):
        if isinstance(x, tuple):
            return (KB._key(x[0]),) + tuple(x[1:])
        if isinstance(x, str):
            return x
        return x.name

    def _collect(self, eng, r, w):
        need = {}
        own = ("p", eng)

        def add(tok):
            if tok is None:
                return
            sk, v = tok
            if sk == own and eng == "pe":
                return
            if need.get(sk, 0) < v:
                need[sk] = v

        for x in r:
            st = self.state.get(self._key(x))
            if st:
                add(st[0])
        for x in w:
            st = self.state.get(self._key(x))
            if st:
                add(st[0])
                for t in st[1]:
                    add(t)
        waits = []
        kn = self.known[eng]
        for sk, v in need.items():
            if kn.get(sk, 0) >= v:
                continue
            kn[sk] = v
            waits.append((sk, v))
        return waits

    def _update(self, tok, r, w):
        for x in w:
            self.state[self._key(x)] = [tok, []]
        for x in r:
            kk = self._key(x)
            st = self.state.get(kk)
            if st is None:
                st = self.state[kk] = [None, []]
            st[1].append(tok)
            if len(st[1]) > 24:
                best = {}
                for sk, v in st[1]:
                    if best.get(sk, 0) < v:
                        best[sk] = v
                st[1] = list(best.items())

    def op(self, eng, fn, r=(), w=()):
        waits = self._collect(eng, r, w)
        self.cnt[eng] += 1
        tok = (("p", eng), self.cnt[eng])
        self.ops[eng].append((waits, fn, (("p", eng), 1)))
        self._update(tok, r, w)
        self.n_ops += 1
        return tok

    def dma(self, q, out, in_, r=(), w=(), slow=False):
        if slow:
            fn = lambda e: e.dma_start(out=out, in_=in_, allow_slow_non_contiguous=True)
        else:
            fn = lambda e: e.dma_start(out=out, in_=in_)
        i = self.drr[q]
        self.drr[q] = (i + 1) % len(self.dsem[q])
        sk = ("d", q, i)
        waits = self._collect(q, r, w)
        prev = self.dcnt[q][i]
        kn = self.known[q]
        if prev > 0 and kn.get(sk, 0) < prev:
            kn[sk] = prev
            waits.append((sk, prev))
        self.dcnt[q][i] = prev + 16
        tok = (sk, prev + 16)
        self.ops[q].append((waits, fn, (sk, 16)))
        self._update(tok, r, w)
        self.n_ops += 1
        return tok

    def barrier(self):
        toks = [(("p", e), self.cnt[e]) for e in ENGS if self.cnt[e] > 0]
        for q in self.dsem:
            for i, c in enumerate(self.dcnt[q]):
                if c > 0:
                    toks.append((("d", q, i), c))
        for e in ENGS:
            waits = []
            kn = self.known[e]
            for sk, v in toks:
                if sk == ("p", e):
                    continue
                if kn.get(sk, 0) < v:
                    kn[sk] = v
                    waits.append((sk, v))
            if waits:
                self.ops[e].append((waits, None, None))

    def check_deadlock(self):
        sem = {}
        ptr = {e: 0 for e in ENGS}
        progress = True
        while progress:
            progress = False
            for e in ENGS:
                lst = self.ops[e]
                while ptr[e] < len(lst):
                    waits, fn, inc = lst[ptr[e]]
                    if all(sem.get(sk, 0) >= v for sk, v in waits):
                        if inc is not None:
                            sem[inc[0]] = sem.get(inc[0], 0) + inc[1]
                        ptr[e] += 1
                        progress = True
                    else:
                        break
        stuck = {e: (ptr[e], len(self.ops[e])) for e in ENGS if ptr[e] < len(self.ops[e])}
        if stuck:
            for e in stuck:
                waits, fn, inc = self.ops[e][ptr[e]]
                print("STUCK", e, ptr[e], [(sk, v, sem.get(sk, 0)) for sk, v in waits])
            raise RuntimeError(f"deadlock in sync graph: {stuck}")

    def finish(self):
        self.barrier()
        self.check_deadlock()
        nc = self.nc
        ops = self.ops
        semobj = self.semobj

        def run(e, lst):
            for waits, fn, inc in lst:
                for sk, v in waits:
                    e.wait_ge(semobj[sk], v)
                if fn is not None:
                    ins = fn(e)
                    ins.then_inc(semobj[inc[0]], inc[1])

        with nc.Block() as block:
            @block.tensor
            def _(e):
                run(e, ops["pe"])

            @block.vector
            def _(e):
                run(e, ops["dve"])

            @block.scalar
            def _(e):
                run(e, ops["act"])

            @block.gpsimd
            def _(e):
                run(e, ops["pool"])

            @block.sync
            def _(e):
                run(e, ops["sp"])

        for cm in reversed(self._ctx):
            cm.__exit__(None, None, None)
        self._ctx = []
        return nc


def _t5_bucket_table():
    n = np.arange(0, 256, dtype=np.int32)
    max_exact = 16
    nf = np.maximum(n, 1).astype(np.float32)
    large = max_exact + (np.log(nf / np.float32(max_exact)) / np.float32(math.log(128 / max_exact))
                         * np.float32(32 - max_exact)).astype(np.int32)
    large = np.minimum(large, 31)
    return np.where(n < max_exact, n, large)


def _boh():
    tab = _t5_bucket_table()
    oh = np.zeros((32, 384), np.float32)
    for j in range(384):
        dist = min(max(j - 127, 0), 255)
        oh[tab[dist], j] = 1.0
    return oh


def _bthr():
    tab = _t5_bucket_table()
    thr = np.zeros((1, 31), np.float32)
    for kk in range(1, 32):
        nz = np.nonzero(tab >= kk)[0]
        thr[0, kk - 1] = float(nz[0]) if len(nz) else 1e9
    return thr


def build(stop_after=None, n_pool=5120):
    k = KB()
    nc = k.nc

    def din(name, shape, dt=F32):
        return nc.dram_tensor(name, list(shape), dt, kind="ExternalInput")

    def dout(name, shape, dt=F32):
        return nc.dram_tensor(name, list(shape), dt, kind="ExternalOutput")

    xp = din("xp", [T, D])
    xs = din("xs", [TS, D])
    c5 = din("c5", [5, D])
    w_ada = din("w_ada", [D, 6 * D])
    b_ada = din("b_ada", [1, 6 * D])
    gvec = din("gvec", [4, D])
    w_in = din("w_in", [D, NPROJ])
    w_out = din("w_out", [D, D])
    w_ff1 = din("w_ff1", [D, DFF])
    w_ff2 = din("w_ff2", [DFF, D])
    conv_w = din("conv_w", [4, 3072])
    st_conv = din("st_conv", [TS * 3, 3072])
    hv = din("hv", [1, 16])
    ln_gb = din("ln_gb", [2, 128])
    gdn_g = din("gdn_g", [1, 128])
    st_ssm = din("st_ssm", [TS, 8, 128, 128])
    rel_bias = din("rel_bias", [32, 8])
    boh = din("boh", [32, 384])
    bthr = din("bthr", [1, 31])
    page_table = din("page_table", [TS, NPAGES], I32)
    cache_kidx = din("cache_kidx", [n_pool, PAGE * 128])
    cache_k = din("cache_k", [n_pool * PAGE, 256])
    cache_v = din("cache_v", [n_pool * PAGE, 256])

    y_p = dout("y_p", [T, D])
    y_s = dout("y_s", [TS, D])
    k_p = dout("k_p", [T, 256])
    v_p = dout("v_p", [T, 256])
    ki_p = dout("ki_p", [T, 128])
    ssm_p = dout("ssm_p", [8, 128, 128])
    conv_p = dout("conv_p", [3, 3072])
    k_s = dout("k_s", [TS, 256])
    v_s = dout("v_s", [TS, 256])
    ki_s = dout("ki_s", [TS, 128])
    ssm_s = dout("ssm_s", [TS, 8, 128, 128])
    conv_s = dout("conv_s", [TS, 3, 3072])

    modd = nc.dram_tensor("modd", [5, 6 * D], F32)
    gq = nc.dram_tensor("gq", [8, 128, TT], BF16)
    gk = nc.dram_tensor("gk", [8, 128, TT], BF16)
    gv = nc.dram_tensor("gv", [8, 128, TT], BF16)
    gz = nc.dram_tensor("gz", [TT, 1024], BF16)
    gab = nc.dram_tensor("gab", [TT, 16], F32)
    aq = nc.dram_tensor("aq", [8, 128, TT], BF16)
    akT = nc.dram_tensor("akT", [2, 128, TT], BF16)
    av = nc.dram_tensor("av", [TT, 256], BF16)
    iq = nc.dram_tensor("iq", [16, 128, TT], BF16)
    iw = nc.dram_tensor("iw", [TT, 16], F32)
    ikTd = nc.dram_tensor("ikTd", [128, TT], BF16)
    biasd = nc.dram_tensor("biasd", [8, 384], F32)
    rbTd = nc.dram_tensor("rbTd", [8, 32], F32)
    x1d = nc.dram_tensor("x1d", [TT, D], F32)
    h2Td = nc.dram_tensor("h2Td", [128, 16, TT], BF16)

    k.init_arena(47 * 1024)
    k.psum_init()

    ident_f = k.alloc("ident_f", [128, 128], F32)
    ident_b = k.alloc("ident_b", [128, 128], BF16)
    ones_b = k.alloc("ones_b", [128, 128], BF16)
    k.op("pool", lambda e: e.memset(ident_f[:], 1.0), w=[ident_f])
    k.op("pool", lambda e: e.affine_select(out=ident_f[:], in_=ident_f[:], pattern=[[-1, 128]],
                                           compare_op=ALU.is_equal, fill=0.0, base=0, channel_multiplier=1),
         r=[ident_f], w=[ident_f])
    k.op("pool", lambda e: e.tensor_copy(out=ident_b[:], in_=ident_f[:]), r=[ident_f], w=[ident_b])
    k.op("pool", lambda e: e.memset(ones_b[:], 1.0), w=[ones_b])

    wb = []
    wcnt = [0]

    def alloc_wb():
        wb.clear()
        wb.extend(k.alloc(f"wb{i}", [128, 16, 512], BF16) for i in range(2))

    def load_w(src_dram, c0, ncols):
        b = wb[wcnt[0] % 2]
        wcnt[0] += 1
        k.dma("pool", b[:, :, 0:ncols], src_dram[:, c0:c0 + ncols].rearrange("(k p) n -> p k n", p=128), w=[b])
        return b

    m0 = k.mark()
    alloc_wb()
    c80 = k.alloc("c80", [80, 128], F32)
    cT = k.alloc("cT", [128, 16, 5], BF16)
    mod = k.alloc("mod", [5, 6 * D], F32)
    gv5 = k.alloc("gv5", [5, 4, D], F32)
    k.dma("sp", c80[:], c5.ap().rearrange("r (k p) -> (r k) p", p=128), w=[c80])
    k.dma("sp", mod[:], b_ada.ap().to_broadcast([5, 6 * D]), w=[mod])
    k.dma("sp", gv5[:].rearrange("p a b -> p (a b)"), gvec.ap().rearrange("a b -> (a b)").unsqueeze(0).to_broadcast([5, 4 * D]), w=[gv5])
    bk = k.bank()
    k.op("pe", lambda e: e.transpose(out=bk[:, 0:80], in_=c80[:], identity=ident_f[0:80, 0:80]), r=[c80, ident_f], w=[bk])
    k.op("act", lambda e: e.activation(out=cT[:], in_=bk[:, 0:80].rearrange("p (r k) -> p k r", r=5), func=AF.Silu), r=[bk], w=[cT])
    for n in range(24):
        wt = load_w(w_ada, n * 512, 512)
        bk = k.bank()
        for kk in range(16):
            k.op("pe", lambda e, kk=kk, wt=wt, bk=bk: e.matmul(bk[0:5, :], lhsT=cT[:, kk, :], rhs=wt[:, kk, :], start=(kk == 0), stop=(kk == 15)),
                 r=[cT, wt], w=[bk])
        k.op("dve", lambda e, n=n, bk=bk: e.tensor_tensor(out=mod[:, n * 512:(n + 1) * 512], in0=bk[0:5, :], in1=mod[:, n * 512:(n + 1) * 512], op=ALU.add),
             r=[bk, mod], w=[mod])
    for (sc, gi) in ((1, 0), (4, 2)):
        k.op("dve", lambda e, sc=sc, gi=gi: e.scalar_tensor_tensor(out=mod[:, sc * D:(sc + 1) * D], in0=mod[:, sc * D:(sc + 1) * D], scalar=1.0, op0=ALU.add,
                                                                    in1=gv5[:, gi, :], op1=ALU.mult), r=[mod, gv5], w=[mod])
    for (g, gi) in ((2, 1), (5, 3)):
        k.op("dve", lambda e, g=g, gi=gi: e.tensor_tensor(out=mod[:, g * D:(g + 1) * D], in0=mod[:, g * D:(g + 1) * D], in1=gv5[:, gi, :], op=ALU.mult),
             r=[mod, gv5], w=[mod])
    k.dma("sp", modd.ap(), mod[:], r=[mod], w=["modd"])
    k.barrier()
    k.release(m0)
    if stop_after == "P0":
        return k.finish()

    def load_mod_bcast(buf, idx):
        k.dma("sp", buf[:], modd[0:1, idx * D:(idx + 1) * D].to_broadcast([128, D]), r=["modd"], w=[buf])

    def load_mod_rows(buf, idx):
        k.dma("sp", buf[:], modd[1:5, idx * D:(idx + 1) * D], r=["modd"], w=[buf])

    mP = k.mark()
    hT = k.alloc("hT", [128, 16, TT], BF16)
    ikT = k.alloc("ikT", [128, TT], BF16)
    m1 = k.mark()
    A1 = k.alloc("A1", [128, D], F32)
    SH1 = k.alloc("SH1", [128, D], F32)
    A1s = k.alloc("A1s", [TS, D], F32)
    SH1s = k.alloc("SH1s", [TS, D], F32)
    load_mod_bcast(SH1, 0)
    load_mod_bcast(A1, 1)
    load_mod_rows(SH1s, 0)
    load_mod_rows(A1s, 1)
    xt = [k.alloc(f"xt{i}", [128, D], F32) for i in range(2)]
    hb = [k.alloc(f"hb{i}", [128, D], BF16) for i in range(2)]
    junk = k.alloc("junk", [128, D], BF16)
    st1 = [k.alloc(f"st1_{i}", [128, 4], F32) for i in range(2)]

    def norm_mod(i, np_, src_ap, A, SH, col0, ncol):
        x_ = xt[i % 2]
        h_ = hb[i % 2]
        s_ = st1[i % 2]
        k.dma("sp", x_[0:np_, :], src_ap, w=[x_])
        k.op("act", lambda e: e.activation(out=junk[0:np_, :], in_=x_[0:np_, :], func=AF.Square, accum_out=s_[0:np_, 0:1]), r=[x_], w=[junk, s_])
        k.op("act", lambda e: e.activation(out=s_[0:np_, 1:2], in_=s_[0:np_, 0:1], func=AF.Sqrt, scale=1.0 / D, bias=EPS), r=[s_], w=[s_])
        k.op("dve", lambda e: e.reciprocal(out=s_[0:np_, 2:3], in_=s_[0:np_, 1:2]), r=[s_], w=[s_])
        k.op("dve", lambda e: e.scalar_tensor_tensor(out=x_[0:np_, :], in0=x_[0:np_, :], scalar=s_[0:np_, 2:3], op0=ALU.mult, in1=A[0:np_, :], op1=ALU.mult),
             r=[x_, s_, A], w=[x_])
        k.op("pool", lambda e: e.tensor_tensor(out=h_[0:np_, :], in0=x_[0:np_, :], in1=SH[0:np_, :], op=ALU.add), r=[x_, SH], w=[h_])
        b0 = k.bank()
        b1 = k.bank()
        for kk in range(16):
            bb = b0 if kk < 8 else b1
            k.op("pe", lambda e, kk=kk, bb=bb: e.transpose(out=bb[:].bitcast(BF16)[:, (kk % 8) * 128:(kk % 8) * 128 + np_],
                                                           in_=h_[0:np_, kk * 128:(kk + 1) * 128], identity=ident_b[0:np_, 0:np_]),
                 r=[h_, ident_b], w=[bb])
        for half, bb in ((0, b0), (1, b1)):
            k.op("act", lambda e, half=half, bb=bb: e.copy(out=hT[:, half * 8:(half + 1) * 8, col0:col0 + ncol],
                                                          in_=bb[:].bitcast(BF16).rearrange("p (a b) -> p a b", a=8)[:, :, 0:ncol]),
                 r=[bb], w=[(hT, col0)])

    for i in range(NT):
        norm_mod(i, 128, xp[i * 128:(i + 1) * 128, :], A1, SH1, i * 128, 128)
    norm_mod(NT, TS, xs.ap(), A1s, SH1s, T, TS)
    k.barrier()
    k.release(m1)
    if stop_after == "P1":
        dbg = dout("dbg_hT", [128, 16 * TT], BF16)
        k.dma("sp", dbg.ap(), hT[:].rearrange("p a b -> p (a b)"), r=[hT])
        return k.finish()

    hT_keys = [(hT, i * 128) for i in range(NT)] + [(hT, T)]

    m2 = k.mark()
    alloc_wb()
    cw = k.alloc("cw", [128, 4, 24], F32)
    stc = k.alloc("stc", [128, TS * 3, 24], F32)
    cwl = k.alloc("cwl", [96, 128], F32)
    stl = k.alloc("stl", [96, 3, 128], F32)
    k.dma("sp", cwl[:], conv_w.ap().rearrange("j (ch c) -> (j ch) c", c=128), w=[cwl])
    k.dma("sp", stl[:], st_conv.ap().rearrange("r (ch c) -> (r ch) c", c=128).rearrange("(g p) c -> p g c", p=96), w=[stl])
    bk = k.bank()
    k.op("pe", lambda e, bk=bk: e.transpose(out=bk[:, 0:96], in_=cwl[:], identity=ident_f[0:96, 0:96]), r=[cwl, ident_f], w=[bk])
    k.op("act", lambda e, bk=bk: e.copy(out=cw[:].rearrange("p a b -> p (a b)"), in_=bk[:, 0:96]), r=[bk], w=[cw])
    bk = k.bank()
    for g in range(3):
        k.op("pe", lambda e, g=g, bk=bk: e.transpose(out=bk[:, g * 96:(g + 1) * 96], in_=stl[:, g, :], identity=ident_f[0:96, 0:96]),
             r=[stl, ident_f], w=[bk])
    k.op("act", lambda e, bk=bk: e.copy(out=stc[:].rearrange("p a b -> p (a b)"), in_=bk[:, 0:288]), r=[bk], w=[stc])
    k.dma("sp", conv_s.ap()[:, 0:2, :], st_conv.ap().rearrange("(s j) c -> s j c", j=3)[:, 1:3, :])

    cin = [k.alloc(f"cin{i}", [128, 3 + T], F32) for i in range(2)]
    for cb in cin:
        k.op("pool", lambda e, cb=cb: e.memset(cb[:, 0:3], 0.0), w=[cb])
    acc = [k.alloc(f"acc{i}", [128, TT], F32) for i in range(2)]
    cins = [k.alloc(f"cins{i}", [128, TS, 4], F32) for i in range(2)]
    tmp4 = [k.alloc(f"tmp4{i}", [128, TS, 4], F32) for i in range(2)]
    sqb = [k.alloc(f"sqb{i}", [128, TT], BF16) for i in range(2)]
    rsb = [k.alloc(f"rsb{i}", [128, TT], F32) for i in range(2)]
    ob = [k.alloc(f"ob{i}", [128, TT], BF16) for i in range(2)]
    obc = [0]

    def fm_matmuls(wt, wc0, tg, bk):
        c0, n = (tg * 512, 512) if tg < 4 else (T, TS)
        keys = hT_keys[tg * 4:(tg + 1) * 4] if tg < 4 else [hT_keys[16]]
        for kk in range(16):
            k.op("pe", lambda e, kk=kk: e.matmul(bk[:, 0:n], lhsT=wt[:, kk, wc0:wc0 + 128], rhs=hT[:, kk, c0:c0 + n], start=(kk == 0), stop=(kk == 15)),
                 r=[wt] + keys, w=[bk])

    def next_ob():
        b = ob[obc[0] % 2]
        obc[0] += 1
        return b

    chunk_i = [0]

    def conv_chunk(wt, wc0, ch):
        ci = chunk_i[0]
        chunk_i[0] += 1
        cb = cin[ci % 2]
        ac = acc[ci % 2]
        cs = cins[ci % 2]
        t4 = tmp4[ci % 2]
        for tg in range(4):
            bk = k.bank()
            fm_matmuls(wt, wc0, tg, bk)
            k.op("act", lambda e, bk=bk, tg=tg: e.copy(out=cb[:, 3 + tg * 512:3 + (tg + 1) * 512], in_=bk[:, 0:512]), r=[bk], w=[cb])
        bk = k.bank()
        fm_matmuls(wt, wc0, 4, bk)
        k.op("dve", lambda e: e.tensor_copy(out=cs[:, :, 0:3], in_=stc[:, :, ch].rearrange("p (s j) -> p s j", j=3)), r=[stc], w=[cs])
        k.op("dve", lambda e, bk=bk: e.tensor_copy(out=cs[:, :, 3:4], in_=bk[:, 0:TS].unsqueeze(2)), r=[bk, cs], w=[cs])
        k.dma("sp", conv_p.ap()[:, ch * 128:(ch + 1) * 128].rearrange("r c -> c r"), cb[:, T:T + 3], r=[cb], slow=True)
        k.dma("sp", conv_s.ap()[:, 2, ch * 128:(ch + 1) * 128].rearrange("s c -> c s"), cs[:, :, 3], r=[cs], slow=True)
        k.op("dve", lambda e: e.tensor_scalar(out=ac[:, 0:T], in0=cb[:, 0:T], scalar1=cw[:, 0, ch:ch + 1], scalar2=None, op0=ALU.mult), r=[cb, cw], w=[ac])
        for j in range(1, 4):
            k.op("dve", lambda e, j=j: e.scalar_tensor_tensor(out=ac[:, 0:T], in0=cb[:, j:j + T], scalar=cw[:, j, ch:ch + 1], op0=ALU.mult, in1=ac[:, 0:T], op1=ALU.add),
                 r=[cb, cw, ac], w=[ac])
        k.op("dve", lambda e: e.tensor_tensor(out=t4[:], in0=cs[:], in1=cw[:, :, ch].unsqueeze(1).to_broadcast([128, TS, 4]), op=ALU.mult), r=[cs, cw], w=[t4])
        k.op("dve", lambda e: e.tensor_reduce(out=ac[:, T:TT], in_=t4[:], axis=AX.X, op=ALU.add), r=[t4, ac], w=[ac])
        o = next_ob()
        if ch >= 16:
            k.op("act", lambda e: e.activation(out=o[:], in_=ac[:], func=AF.Silu), r=[ac], w=[o])
            k.dma("sp", gv[ch - 16], o[:], r=[o], w=["gv"])
            return
        sq = sqb[ci % 2]
        rs = rsb[ci % 2]
        k.op("act", lambda e: e.activation(out=ac[:], in_=ac[:], func=AF.Silu), r=[ac], w=[ac])
        k.op("act", lambda e: e.activation(out=sq[:], in_=ac[:], func=AF.Square), r=[ac], w=[sq])
        for tg in range(5):
            c0, n = (tg * 512, 512) if tg < 4 else (T, TS)
            bk = k.bank()
            k.op("pe", lambda e, bk=bk, c0=c0, n=n: e.matmul(bk[:, 0:n], lhsT=ones_b[:], rhs=sq[:, c0:c0 + n], start=True, stop=True), r=[sq, ones_b], w=[bk])
            k.op("act", lambda e, bk=bk, c0=c0, n=n: e.activation(out=rs[:, c0:c0 + n], in_=bk[:, 0:n], func=AF.Sqrt, bias=EPS, scale=1.0), r=[bk], w=[rs])
        k.op("dve", lambda e: e.reciprocal(out=rs[:], in_=rs[:]), r=[rs], w=[rs])
        scl = 128.0 ** -0.5 if ch < 8 else 1.0
        k.op("dve", lambda e: e.scalar_tensor_tensor(out=o[:], in0=ac[:], scalar=scl, op0=ALU.mult, in1=rs[:], op1=ALU.mult), r=[ac, rs], w=[o])
        dst = gq[ch] if ch < 8 else gk[ch - 8]
        k.dma("sp", dst, o[:], r=[o], w=["gqk"])

    def plain_chunk(wt, wc0, dst, scale):
        o = next_ob()
        for tg in range(5):
            c0, n = (tg * 512, 512) if tg < 4 else (T, TS)
            bk = k.bank()
            fm_matmuls(wt, wc0, tg, bk)
            k.op("act", lambda e, bk=bk, c0=c0, n=n: e.activation(out=o[:, c0:c0 + n], in_=bk[:, 0:n], func=AF.Copy, scale=scale), r=[bk], w=[o])
        k.dma("sp", dst, o[:], r=[o], w=["plain"])

    for g in range(6):
        wt = load_w(w_in, O_CONV + g * 512, 512)
        for j in range(4):
            conv_chunk(wt, j * 128, g * 4 + j)
    if stop_after == "P2a":
        k.barrier()
        return k.finish()
    for g in range(2):
        wt = load_w(w_in, O_QB + g * 512, 512)
        for j in range(4):
            plain_chunk(wt, j * 128, aq[g * 4 + j], 128.0 ** -0.5)
    for g in range(4):
        wt = load_w(w_in, O_QI + g * 512, 512)
        for j in range(4):
            plain_chunk(wt, j * 128, iq[g * 4 + j], 1.0)
    wt_kv = load_w(w_in, O_KB, 512)
    for j in range(2):
        plain_chunk(wt_kv, j * 128, akT[j], 1.0)

    if stop_after == "P2b":
        k.barrier()
        return k.finish()
    stg_f = [k.alloc(f"stgf{i}", [128, 512], F32) for i in range(2)]
    stg_b = [k.alloc(f"stgb{i}", [128, 512], BF16) for i in range(2)]
    lnw = [k.alloc(f"lnw{i}", [128, 8], F32) for i in range(2)]
    kib = [k.alloc(f"kib{i}", [128, 128], BF16) for i in range(2)]
    lng = k.alloc("lng", [128, 128], F32)
    lnb = k.alloc("lnb", [128, 128], F32)
    k.dma("sp", lng[:], ln_gb[0:1, :].to_broadcast([128, 128]), w=[lng])
    k.dma("sp", lnb[:], ln_gb[1:2, :].to_broadcast([128, 128]), w=[lnb])
    tcnt = [0]

    def tm_tile(wt, ncols, ti, epilogue):
        c0, n = (ti * 128, 128) if ti < NT else (T, TS)
        bk = k.bank()
        for kk in range(16):
            k.op("pe", lambda e, kk=kk: e.matmul(bk[0:n, 0:ncols], lhsT=hT[:, kk, c0:c0 + n], rhs=wt[:, kk, 0:ncols], start=(kk == 0), stop=(kk == 15)),
                 r=[wt, hT_keys[ti]], w=[bk])
        i = tcnt[0]
        tcnt[0] += 1
        epilogue(bk, c0, n, i)

    def ep_kv(bk, c0, n, i):
        sf = stg_f[i % 2]
        sb_ = stg_b[i % 2]
        import os
        dbg = int(os.environ.get("DBG", "0"))
        k.op("act", lambda e: e.copy(out=sf[0:n, :], in_=bk[0:n, :]), r=[bk], w=[sf])
        if dbg != 3:
            k.op("pool", lambda e: e.tensor_copy(out=sb_[0:n, 0:256], in_=sf[0:n, 256:512]), r=[sf], w=[sb_])
        if dbg == 1:
            pass
        elif c0 < T:
            k.dma("sp", k_p[c0:c0 + n, :], sf[0:n, 0:256], r=[sf])
            k.dma("sp", v_p[c0:c0 + n, :], sf[0:n, 256:512], r=[sf])
        else:
            k.dma("sp", k_s.ap(), sf[0:n, 0:256], r=[sf])
            k.dma("sp", v_s.ap(), sf[0:n, 256:512], r=[sf])
        if dbg not in (2, 3):
            k.dma("sp", av[c0:c0 + n, :], sb_[0:n, 0:256], r=[sb_], w=["av"])

    import os
    _d = int(os.environ.get("DBG", "0"))
    for ti in range(0 if _d == 4 else (NT if _d == 5 else NT + 1)):
        tm_tile(wt_kv, 512, ti, ep_kv)

    if stop_after == "P2c":
        k.barrier()
        return k.finish()

    def ep_z(half):
        def ep(bk, c0, n, i):
            sb_ = stg_b[i % 2]
            k.op("act", lambda e: e.copy(out=sb_[0:n, :], in_=bk[0:n, :]), r=[bk], w=[sb_])
            k.dma("sp", gz[c0:c0 + n, half * 512:(half + 1) * 512], sb_[0:n, :], r=[sb_], w=["gz"])
        return ep

    for half in range(2):
        wt = load_w(w_in, O_Z + half * 512, 512)
        for ti in range(NT + 1):
            tm_tile(wt, 512, ti, ep_z(half))

    if stop_after == "P2d":
        k.barrier()
        return k.finish()

    def ep_ab(bk, c0, n, i):
        sf = stg_f[i % 2]
        k.op("act", lambda e: e.copy(out=sf[0:n, 0:16], in_=bk[0:n, 0:16]), r=[bk], w=[sf])
        k.dma("sp", gab[c0:c0 + n, :], sf[0:n, 0:16], r=[sf], w=["gab"])

    wt = load_w(w_in, O_A, 16)
    for ti in range(NT + 1):
        tm_tile(wt, 16, ti, ep_ab)

    if stop_after == "P2e":
        k.barrier()
        return k.finish()

    def ep_wk(bk, c0, n, i):
        sf = stg_f[i % 2]
        s_ = lnw[i % 2]
        kb_ = kib[i % 2]
        k.op("act", lambda e: e.activation(out=sf[0:n, 0:16], in_=bk[0:n, 0:16], func=AF.Copy, scale=0.25), r=[bk], w=[sf])
        k.dma("sp", iw[c0:c0 + n, :], sf[0:n, 0:16], r=[sf], w=["iw"])
        kf = sf[0:n, 128:256]
        k.op("act", lambda e: e.activation(out=kf, in_=bk[0:n, 16:144], func=AF.Copy, accum_out=s_[0:n, 0:1]), r=[bk, sf], w=[sf, s_])
        k.op("dve", lambda e: e.tensor_scalar(out=s_[0:n, 1:2], in0=s_[0:n, 0:1], scalar1=-1.0 / 128, scalar2=None, op0=ALU.mult), r=[s_], w=[s_])
        k.op("dve", lambda e: e.tensor_scalar(out=kf, in0=kf, scalar1=s_[0:n, 1:2], scalar2=None, op0=ALU.add), r=[sf, s_], w=[sf])
        k.op("act", lambda e: e.activation(out=sf[0:n, 256:384], in_=kf, func=AF.Square, accum_out=s_[0:n, 2:3]), r=[sf, s_], w=[sf, s_])
        k.op("act", lambda e: e.activation(out=s_[0:n, 3:4], in_=s_[0:n, 2:3], func=AF.Sqrt, scale=1.0 / 128, bias=EPS), r=[s_], w=[s_])
        k.op("dve", lambda e: e.reciprocal(out=s_[0:n, 4:5], in_=s_[0:n, 3:4]), r=[s_], w=[s_])
        k.op("dve", lambda e: e.scalar_tensor_tensor(out=kf, in0=kf, scalar=s_[0:n, 4:5], op0=ALU.mult, in1=lng[0:n, :], op1=ALU.mult), r=[sf, s_, lng], w=[sf])
        k.op("dve", lambda e: e.tensor_tensor(out=kf, in0=kf, in1=lnb[0:n, :], op=ALU.add), r=[sf, lnb], w=[sf])
        k.op("dve", lambda e: e.tensor_copy(out=kb_[0:n, :], in_=kf), r=[sf], w=[kb_])
        if c0 < T:
            k.dma("sp", ki_p[c0:c0 + n, :], kf, r=[sf])
        else:
            k.dma("sp", ki_s.ap(), kf, r=[sf])
        b2 = k.bank()
        k.op("pe", lambda e: e.transpose(out=b2[:].bitcast(BF16)[:, 0:n], in_=kb_[0:n, :], identity=ident_b[0:n, 0:n]), r=[kb_, ident_b], w=[b2])
        k.op("act", lambda e: e.copy(out=ikT[:, c0:c0 + n], in_=b2[:].bitcast(BF16)[:, 0:n]), r=[b2], w=[(ikT, c0)])

    wt = load_w(w_in, O_WI, 144)
    for ti in range(NT + 1):
        tm_tile(wt, 144, ti, ep_wk)
    k.dma("sp", ikTd.ap(), ikT[:], r=[(ikT, c) for c in range(0, TT, 128)], w=["ikTd"])
    k.barrier()
    k.release(mP)
    if stop_after == "P2":
        return k.finish()
    mixT = k.alloc("mixT", [128, 16, TT], BF16)
    m3 = k.mark()
    HG = 4
    HW = HG * 128
    ones_f = k.alloc("ones_f", [128, 128], F32)
    TRI = k.alloc("TRI", [128, 128], F32)
    POSM = k.alloc("POSM", [128, HG, 128], F32)
    OFFD = k.alloc("OFFD", [128, HG, 128], F32)
    hvb = k.alloc("hvb", [128, 16], F32)
    gnb = k.alloc("gnb", [128, 128], F32)
    k.op("pool", lambda e: e.memset(ones_f[:], 1.0), w=[ones_f])
    k.op("pool", lambda e: e.memset(TRI[:], 1.0), w=[TRI])
    k.op("pool", lambda e: e.affine_select(out=TRI[:], in_=TRI[:], pattern=[[1, 128]], compare_op=ALU.is_ge, fill=0.0, base=0, channel_multiplier=-1),
         r=[TRI], w=[TRI])
    k.op("pool", lambda e: e.memset(POSM[:], 0.0), w=[POSM])
    k.op("pool", lambda e: e.affine_select(out=POSM[:], in_=POSM[:], pattern=[[0, HG], [-1, 128]], compare_op=ALU.is_ge, fill=30000.0, base=0, channel_multiplier=1),
         r=[POSM], w=[POSM])
    k.op("pool", lambda e: e.memset(OFFD[:], 1.0), w=[OFFD])
    k.op("pool", lambda e: e.affine_select(out=OFFD[:], in_=OFFD[:], pattern=[[0, HG], [-1, 128]], compare_op=ALU.not_equal, fill=0.0, base=0, channel_multiplier=1),
         r=[OFFD], w=[OFFD])
    k.dma("sp", hvb[:], hv.ap().to_broadcast([128, 16]), w=[hvb])
    k.dma("sp", gnb[:], gdn_g.ap().to_broadcast([128, 128]), w=[gnb])

    gabt = k.alloc("gabt", [128, NT, 16], F32)
    k.dma("sp", gabt[:], gab[0:T, :].rearrange("(t p) c -> p t c", p=128), r=["gab"], w=[gabt], slow=True)
    nA = k.alloc("nA", [128, 8], F32)
    G_ = k.alloc("G_", [128, NT, 8], F32)
    Bt = k.alloc("Bt", [128, NT, 8], F32)
    NB = k.alloc("NB", [128, NT, 8], F32)
    GC = k.alloc("GC", [128, NT, 8], F32)
    GL = k.alloc("GL", [128, NT, 8], F32)
    EG = k.alloc("EG", [128, NT, 8], F32)
    EGL = k.alloc("EGL", [128, NT, 8], F32)
    EKD = k.alloc("EKD", [128, NT, 8], F32)
    BEG = k.alloc("BEG", [128, NT, 8], F32)
    k.op("act", lambda e: e.activation(out=nA[:], in_=hvb[:, 0:8], func=AF.Exp), r=[hvb], w=[nA])
    k.op("dve", lambda e: e.tensor_scalar(out=nA[:], in0=nA[:], scalar1=-1.0, scalar2=None, op0=ALU.mult), r=[nA], w=[nA])
    k.op("dve", lambda e: e.tensor_tensor(out=G_[:], in0=gabt[:, :, 0:8], in1=hvb[:, 8:16].unsqueeze(1).to_broadcast([128, NT, 8]), op=ALU.add), r=[gabt, hvb], w=[G_])
    k.op("act", lambda e: e.activation(out=G_[:], in_=G_[:], func=AF.Exp), r=[G_], w=[G_])
    k.op("act", lambda e: e.activation(out=G_[:], in_=G_[:], func=AF.Ln, bias=1.0, scale=1.0), r=[G_], w=[G_])
    k.op("dve", lambda e: e.tensor_tensor(out=G_[:], in0=G_[:], in1=nA[:].unsqueeze(1).to_broadcast([128, NT, 8]), op=ALU.mult), r=[G_, nA], w=[G_])
    k.op("act", lambda e: e.activation(out=Bt[:], in_=gabt[:, :, 8:16], func=AF.Sigmoid), r=[gabt], w=[Bt])
    k.op("dve", lambda e: e.tensor_scalar(out=NB[:], in0=Bt[:], scalar1=-1.0, scalar2=None, op0=ALU.mult), r=[Bt], w=[NB])
    bA = k.bank()
    bB = k.bank()
    for t in range(NT):
        k.op("pe", lambda e, t=t: e.matmul(bA[:, t * 8:(t + 1) * 8], lhsT=TRI[:], rhs=G_[:, t, :], start=True, stop=True), r=[TRI, G_], w=[bA])
        k.op("pe", lambda e, t=t: e.matmul(bB[:, t * 8:(t + 1) * 8], lhsT=ones_f[:], rhs=G_[:, t, :], start=True, stop=True), r=[ones_f, G_], w=[bB])
    k.op("act", lambda e: e.copy(out=GC[:].rearrange("p a b -> p (a b)"), in_=bA[:, 0:NT * 8]), r=[bA], w=[GC])
    k.op("act", lambda e: e.copy(out=GL[:].rearrange("p a b -> p (a b)"), in_=bB[:, 0:NT * 8]), r=[bB], w=[GL])
    k.op("act", lambda e: e.activation(out=EG[:], in_=GC[:], func=AF.Exp), r=[GC], w=[EG])
    k.op("act", lambda e: e.activation(out=EGL[:], in_=GL[:], func=AF.Exp), r=[GL], w=[EGL])
    k.op("dve", lambda e: e.tensor_tensor(out=EKD[:], in0=GL[:], in1=GC[:], op=ALU.subtract), r=[GL, GC], w=[EKD])
    k.op("act", lambda e: e.activation(out=EKD[:], in_=EKD[:], func=AF.Exp), r=[EKD], w=[EKD])
    k.op("dve", lambda e: e.tensor_tensor(out=BEG[:], in0=Bt[:], in1=EG[:], op=ALU.mult), r=[Bt, EG], w=[BEG])

    if stop_after == "P3a":
        k.barrier()
        return k.finish()
    m3g = k.mark()
    qT = k.alloc("qT", [128, HG, TT], BF16)
    kT = k.alloc("kT", [128, HG, TT], BF16)
    vT = k.alloc("vT", [128, HG, TT], BF16)
    NSLOT = 2

    def mk_slot(j):
        d = {}
        for nm, dt in (("Dg", F32), ("egT", BF16), ("decay", F32), ("M0", F32), ("M1", F32), ("MT0", F32), ("MT1", F32),
                       ("PT", F32), ("PTb", BF16), ("vbeta", BF16), ("kbg", BF16), ("kdec", BF16), ("u", F32), ("wT", BF16),
                       ("intra", BF16), ("intraT", BF16), ("qg", BF16)):
            d[nm] = k.alloc(f"{nm}_{j}", [128, HG, 128], dt)
        return d

    slots = [mk_slot(j) for j in range(NSLOT)]
    S_ = k.alloc("S_", [128, HG, 128], F32)
    Sb = k.alloc("Sb", [128, HG, 128], BF16)
    vnew = k.alloc("vnew", [128, HG, 128], BF16)
    o_ = k.alloc("o_", [128, HG, 128], F32)
    sq_ = k.alloc("sq_", [128, HG, 128], F32)
    zt = [k.alloc(f"zt{i}", [128, HG, 128], BF16) for i in range(2)]
    zs = k.alloc("zs", [128, HG, 128], F32)
    oa = k.alloc("oa", [128, HG, 128], BF16)
    sst = k.alloc("sst", [128, 3 * HG], F32)

    def fl(b):
        return b[:].rearrange("p a b -> p (a b)")

    def bc_tok(src_ap):
        return src_ap.unsqueeze(2).to_broadcast([128, HG, 128])

    def stageA(g, i, sl):
        d = slots[sl]
        h0 = g * HG
        tsl = slice(i * 128, (i + 1) * 128)
        Dg, egT, decay, PT, PTb = d["Dg"], d["egT"], d["decay"], d["PT"], d["PTb"]
        Ms = [d["M0"], d["M1"]]
        MTs = [d["MT0"], d["MT1"]]
        k.op("dve", lambda e: e.tensor_tensor(out=Dg[:], in0=ident_f[:].unsqueeze(1).to_broadcast([128, HG, 128]), in1=bc_tok(GC[:, i, h0:h0 + HG]), op=ALU.mult),
             r=[ident_f, GC], w=[Dg])
        b1 = k.bank()
        k.op("pe", lambda e: e.matmul(b1[:, 0:HW], lhsT=ones_f[:], rhs=fl(Dg), start=True, stop=True), r=[ones_f, Dg], w=[b1])
        k.op("act", lambda e: e.activation(out=fl(egT), in_=b1[:, 0:HW], func=AF.Exp), r=[b1], w=[egT])
        b2 = k.bank()
        k.op("pe", lambda e: e.matmul(b2[:, 0:HW], lhsT=ones_f[:], rhs=fl(Dg), start=True, stop=False), r=[ones_f, Dg], w=[b2])
        k.op("pe", lambda e: e.matmul(b2[:, 0:HW], lhsT=ident_f[:], rhs=fl(POSM), start=False, stop=True), r=[ident_f, POSM], w=[b2])
        for h in range(HG):
            k.op("act", lambda e, h=h: e.activation(out=decay[:, h, :], in_=b2[:, h * 128:(h + 1) * 128], func=AF.Exp, scale=-1.0, bias=GC[:, i, h0 + h:h0 + h + 1]),
                 r=[b2, GC], w=[decay])
        k.op("pool", lambda e: e.tensor_tensor(out=d["qg"][:], in0=qT[:, :, tsl], in1=egT[:], op=ALU.mult), r=[qT, egT], w=[d["qg"]])
        b3 = k.bank()
        b4 = k.bank()
        for h in range(HG):
            k.op("pe", lambda e, h=h: e.matmul(b3[:, h * 128:(h + 1) * 128], lhsT=kT[:, h, tsl], rhs=kT[:, h, tsl], start=True, stop=True), r=[kT], w=[b3])
        for h in range(HG):
            k.op("pe", lambda e, h=h: e.matmul(b4[:, h * 128:(h + 1) * 128], lhsT=qT[:, h, tsl], rhs=kT[:, h, tsl], start=True, stop=True), r=[qT, kT], w=[b4])
        k.op("dve", lambda e: e.tensor_tensor(out=fl(d["intra"]), in0=b4[:, 0:HW], in1=fl(decay), op=ALU.mult), r=[b4, decay], w=[d["intra"]])
        k.op("pool", lambda e: e.tensor_tensor(out=Dg[:], in0=decay[:], in1=OFFD[:], op=ALU.mult), r=[decay, OFFD], w=[Dg])
        for h in range(HG):
            k.op("dve", lambda e, h=h: e.scalar_tensor_tensor(out=Ms[0][:, h, :], in0=b3[:, h * 128:(h + 1) * 128], scalar=NB[:, i, h0 + h:h0 + h + 1], op0=ALU.mult,
                                                                in1=Dg[:, h, :], op1=ALU.mult), r=[b3, NB, Dg], w=[Ms[0]])
        yield
        b5 = k.bank()
        b5i = k.bank()
        b5b = b5i[:].bitcast(BF16)
        for h in range(HG):
            k.op("pe", lambda e, h=h: e.transpose(out=b5[:, h * 128:(h + 1) * 128], in_=Ms[0][:, h, :], identity=ident_f[:]), r=[Ms[0], ident_f], w=[b5])
        for h in range(HG):
            k.op("pe", lambda e, h=h: e.transpose(out=b5b[:, h * 128:(h + 1) * 128], in_=d["intra"][:, h, :], identity=ident_b[:]), r=[d["intra"], ident_b], w=[b5i])
        k.op("act", lambda e: e.copy(out=fl(MTs[0]), in_=b5[:, 0:HW]), r=[b5], w=[MTs[0]])
        k.op("act", lambda e: e.copy(out=fl(d["intraT"]), in_=b5b[:, 0:HW]), r=[b5i], w=[d["intraT"]])
        k.op("dve", lambda e: e.tensor_tensor(out=PT[:], in0=MTs[0][:], in1=ident_f[:].unsqueeze(1).to_broadcast([128, HG, 128]), op=ALU.add), r=[MTs[0], ident_f], w=[PT])
        yield
        cur = 0
        for lvl in range(1, 7):
            nx = 1 - cur
            last = (lvl == 6)
            b6 = k.bank()
            for h in range(HG):
                k.op("pe", lambda e, h=h, cur=cur, b6=b6: e.matmul(b6[:, h * 128:(h + 1) * 128], lhsT=MTs[cur][:, h, :], rhs=Ms[cur][:, h, :], start=True, stop=True),
                     r=[MTs[cur], Ms[cur]], w=[b6])
            if not last:
                b7 = k.bank()
                for h in range(HG):
                    k.op("pe", lambda e, h=h, cur=cur, b7=b7: e.matmul(b7[:, h * 128:(h + 1) * 128], lhsT=Ms[cur][:, h, :], rhs=MTs[cur][:, h, :], start=True, stop=True),
                         r=[MTs[cur], Ms[cur]], w=[b7])
            k.op("act", lambda e, nx=nx, b6=b6: e.copy(out=fl(Ms[nx]), in_=b6[:, 0:HW]), r=[b6], w=[Ms[nx]])
            if not last:
                k.op("act", lambda e, nx=nx, b7=b7: e.copy(out=fl(MTs[nx]), in_=b7[:, 0:HW]), r=[b7], w=[MTs[nx]])
            b8 = k.bank()
            for h in range(HG):
                k.op("pe", lambda e, h=h, nx=nx, b8=b8: e.matmul(b8[:, h * 128:(h + 1) * 128], lhsT=Ms[nx][:, h, :], rhs=PT[:, h, :], start=True, stop=True),
                     r=[Ms[nx], PT], w=[b8])
            k.op("dve", lambda e, b8=b8: e.tensor_tensor(out=fl(PT), in0=b8[:, 0:HW], in1=fl(PT), op=ALU.add), r=[b8, PT], w=[PT])
            if last:
                k.op("pool", lambda e: e.tensor_copy(out=PTb[:], in_=PT[:]), r=[PT], w=[PTb])
            cur = nx
            yield
        b9 = k.bank()
        b9b = b9[:].bitcast(BF16)
        for h in range(HG):
            k.op("pe", lambda e, h=h: e.transpose(out=b9b[:, h * 128:(h + 1) * 128], in_=vT[:, h, tsl], identity=ident_b[:]), r=[vT, ident_b], w=[b9])
        for h in range(HG):
            k.op("pe", lambda e, h=h: e.transpose(out=b9b[:, HW + h * 128:HW + (h + 1) * 128], in_=kT[:, h, tsl], identity=ident_b[:]), r=[kT, ident_b], w=[b9])
        vps = b9b[:, 0:HW].rearrange("p (a b) -> p a b", a=HG)
        kps = b9b[:, HW:2 * HW].rearrange("p (a b) -> p a b", a=HG)
        k.op("dve", lambda e: e.tensor_tensor(out=d["vbeta"][:], in0=vps, in1=bc_tok(Bt[:, i, h0:h0 + HG]), op=ALU.mult), r=[b9, Bt], w=[d["vbeta"]])
        k.op("dve", lambda e: e.tensor_tensor(out=d["kbg"][:], in0=kps, in1=bc_tok(BEG[:, i, h0:h0 + HG]), op=ALU.mult), r=[b9, BEG], w=[d["kbg"]])
        k.op("dve", lambda e: e.tensor_tensor(out=d["kdec"][:], in0=kps, in1=bc_tok(EKD[:, i, h0:h0 + HG]), op=ALU.mult), r=[b9, EKD], w=[d["kdec"]])
        b10 = k.bank()
        b11 = k.bank()
        for h in range(HG):
            k.op("pe", lambda e, h=h: e.matmul(b10[:, h * 128:(h + 1) * 128], lhsT=PTb[:, h, :], rhs=d["vbeta"][:, h, :], start=True, stop=True), r=[PTb, d["vbeta"]], w=[b10])
        for h in range(HG):
            k.op("pe", lambda e, h=h: e.matmul(b11[:, h * 128:(h + 1) * 128], lhsT=d["kbg"][:, h, :], rhs=PTb[:, h, :], start=True, stop=True), r=[PTb, d["kbg"]], w=[b11])
        k.op("act", lambda e: e.copy(out=fl(d["u"]), in_=b10[:, 0:HW]), r=[b10], w=[d["u"]])
        k.op("act", lambda e: e.copy(out=fl(d["wT"]), in_=b11[:, 0:HW]), r=[b11], w=[d["wT"]])
        yield

    def scan_step(g, i, sl):
        d = slots[sl]
        h0 = g * HG
        tsl = slice(i * 128, (i + 1) * 128)
        z_ = zt[i % 2]
        k.dma("sp", fl(z_), gz[i * 128:(i + 1) * 128, h0 * 128:(h0 + HG) * 128], r=["gz"], w=[z_])
        bx = k.bank()
        for h in range(HG):
            k.op("pe", lambda e, h=h: e.matmul(bx[:, h * 128:(h + 1) * 128], lhsT=d["wT"][:, h, :], rhs=Sb[:, h, :], start=True, stop=True), r=[d["wT"], Sb], w=[bx])
        k.op("dve", lambda e: e.tensor_tensor(out=fl(vnew), in0=fl(d["u"]), in1=bx[:, 0:HW], op=ALU.subtract), r=[d["u"], bx], w=[vnew])
        bo = k.bank()
        for h in range(HG):
            k.op("pe", lambda e, h=h: e.matmul(bo[:, h * 128:(h + 1) * 128], lhsT=d["qg"][:, h, :], rhs=Sb[:, h, :], start=True, stop=False), r=[d["qg"], Sb], w=[bo])
            k.op("pe", lambda e, h=h: e.matmul(bo[:, h * 128:(h + 1) * 128], lhsT=d["intraT"][:, h, :], rhs=vnew[:, h, :], start=False, stop=True), r=[d["intraT"], vnew], w=[bo])
        bz = k.bank()
        for h in range(HG):
            k.op("pe", lambda e, h=h: e.matmul(bz[:, h * 128:(h + 1) * 128], lhsT=d["kdec"][:, h, :], rhs=vnew[:, h, :], start=True, stop=True), r=[d["kdec"], vnew], w=[bz])
        k.op("dve", lambda e: e.tensor_tensor(out=S_[:], in0=S_[:], in1=bc_tok(EGL[:, i, h0:h0 + HG]), op=ALU.mult), r=[S_, EGL], w=[S_])
        k.op("dve", lambda e: e.tensor_tensor(out=fl(S_), in0=fl(S_), in1=bz[:, 0:HW], op=ALU.add), r=[S_, bz], w=[S_])
        k.op("act", lambda e: e.copy(out=Sb[:], in_=S_[:]), r=[S_], w=[Sb])
        k.op("act", lambda e: e.copy(out=fl(o_), in_=bo[:, 0:HW]), r=[bo], w=[o_])
        k.op("pool", lambda e: e.tensor_tensor(out=sq_[:], in0=o_[:], in1=o_[:], op=ALU.mult), r=[o_], w=[sq_])
        k.op("dve", lambda e: e.tensor_reduce(out=sst[:, 0:HG], in_=sq_[:], axis=AX.X, op=ALU.add), r=[sq_], w=[sst])
        k.op("act", lambda e: e.activation(out=sst[:, HG:2 * HG], in_=sst[:, 0:HG], func=AF.Sqrt, scale=1.0 / 128, bias=EPS), r=[sst], w=[sst])
        k.op("dve", lambda e: e.reciprocal(out=sst[:, 2 * HG:3 * HG], in_=sst[:, HG:2 * HG]), r=[sst], w=[sst])
        k.op("act", lambda e: e.activation(out=zs[:], in_=z_[:], func=AF.Silu), r=[z_], w=[zs])
        k.op("dve", lambda e: e.tensor_tensor(out=o_[:], in0=o_[:], in1=bc_tok(sst[:, 2 * HG:3 * HG]), op=ALU.mult), r=[o_, sst], w=[o_])
        k.op("pool", lambda e: e.tensor_tensor(out=o_[:], in0=o_[:], in1=gnb[:].unsqueeze(1).to_broadcast([128, HG, 128]), op=ALU.mult), r=[o_, gnb], w=[o_])
        k.op("pool", lambda e: e.tensor_tensor(out=oa[:], in0=o_[:], in1=zs[:], op=ALU.mult), r=[o_, zs], w=[oa])
        bt = k.bank()
        btb = bt[:].bitcast(BF16)
        for h in range(HG):
            k.op("pe", lambda e, h=h: e.transpose(out=btb[:, h * 128:(h + 1) * 128], in_=oa[:, h, :], identity=ident_b[:]), r=[oa, ident_b], w=[bt])
        k.op("act", lambda e: e.copy(out=mixT[:, h0:h0 + HG, tsl], in_=btb[:, 0:HW].rearrange("p (a b) -> p a b", a=HG)), r=[bt], w=[(mixT, i)])

    for g in range(8 // HG):
        h0 = g * HG
        for nm, src, buf in (("q", gq, qT), ("k", gk, kT), ("v", gv, vT)):
            k.dma("sp", buf[:], src[h0:h0 + HG].rearrange("h d t -> d h t"), r=["gqk", "gv"], w=[buf])
        k.op("pool", lambda e: e.memset(S_[:], 0.0), w=[S_])
        k.op("pool", lambda e: e.memset(Sb[:], 0.0), w=[Sb])
        if stop_after == "P3d":
            for _ in stageA(0, 0, 0):
                pass
            for _ in stageA(0, 1, 1):
                pass
            scan_step(0, 0, 0)
            scan_step(0, 1, 1)
            d = slots[0]
            names = ["decay", "M0", "PT", "u", "wT", "intraT", "qg", "kdec", "vbeta", "kbg", "egT"]
            tmpfs = [sq_, zs]
            for ii, nm in enumerate(names):
                dd = dout("dbg_" + nm, [128, HW], F32)
                tmpf = tmpfs[ii % 2]
                k.op("dve", lambda e, nm=nm, tmpf=tmpf: e.tensor_copy(out=fl(tmpf), in_=fl(d[nm])), r=[d[nm]], w=[tmpf])
                k.dma("sp", dd.ap(), fl(tmpf), r=[tmpf])
            for nm, b in (("S", S_), ("o", o_), ("GC", GC), ("G", G_), ("Bt", Bt), ("EKD", EKD), ("EGL", EGL)):
                dd = dout("dbg_" + nm, [128, int(np.prod(b.ap.shape[1:]))], F32)
                k.dma("sp", dd.ap(), b[:].rearrange("p a b -> p (a b)"), r=[b])
            k.barrier()
            return k.finish()
        import os
        _lim = int(os.environ.get("YLIM", "100"))
        for i0 in range(0, NT, NSLOT):
            gens = [stageA(g, i0 + j, j) for j in range(NSLOT)]
            if stop_after == "P3b":
                for _ in range(_lim):
                    for gen in gens:
                        next(gen, None)
                k.barrier()
                return k.finish()
            alive = True
            while alive:
                alive = False
                for gen in gens:
                    try:
                        next(gen)
                        alive = True
                    except StopIteration:
                        pass
            for j in range(NSLOT):
                scan_step(g, i0 + j, j)
        k.dma("sp", ssm_p.ap()[h0:h0 + HG].rearrange("h a b -> a h b"), S_[:], r=[S_])
    if stop_after == "P3":
        k.barrier()
        return k.finish()
    k.barrier()
    k.release(m3g)
    S0 = k.alloc("S0", [128, 8, 128], F32)
    qc = k.alloc("qc", [128, 8], BF16)
    kc = k.alloc("kc", [128, 8], BF16)
    vc = k.alloc("vc", [128, 8], BF16)
    qcf = k.alloc("qcf", [128, 8], F32)
    kcf = k.alloc("kcf", [128, 8], F32)
    gabr = k.alloc("gabr", [1, 16], F32)
    zr = k.alloc("zr", [1, 1024], BF16)
    zrs = k.alloc("zrs", [1, 8, 128], F32)
    rw = k.alloc("rw", [1, 64], F32)
    t1 = k.alloc("t1", [1, 8, 128], F32)
    orow = k.alloc("orow", [1, 8, 128], F32)
    sqr = k.alloc("sqr", [1, 8, 128], F32)
    oar = k.alloc("oar", [1, 1024], BF16)
    abs_ = k.alloc("abs_", [128, 8], F32)

    def bc_row(ap8):
        return ap8.unsqueeze(2).to_broadcast([1, 8, 128])

    def sample_gdn(s_i):
        col = T + s_i
        k.dma("sp", S0[:], st_ssm.ap()[s_i].rearrange("h a b -> a h b"), w=[S0])
        k.dma("sp", qc[:], gq.ap()[:, :, col].rearrange("h d -> d h"), r=["gqk"], w=[qc], slow=True)
        k.dma("sp", kc[:], gk.ap()[:, :, col].rearrange("h d -> d h"), r=["gqk"], w=[kc], slow=True)
        k.dma("sp", vc[:], gv.ap()[:, :, col].rearrange("h d -> d h"), r=["gv"], w=[vc], slow=True)
        k.dma("sp", gabr[:], gab[col:col + 1, :], r=["gab"], w=[gabr])
        k.dma("sp", zr[:], gz[col:col + 1, :], r=["gz"], w=[zr])
        k.op("dve", lambda e: e.tensor_copy(out=qcf[:], in_=qc[:]), r=[qc], w=[qcf])
        k.op("dve", lambda e: e.tensor_copy(out=kcf[:], in_=kc[:]), r=[kc], w=[kcf])
        k.op("dve", lambda e: e.tensor_tensor(out=rw[:, 0:8], in0=gabr[:, 0:8], in1=hvb[0:1, 8:16], op=ALU.add), r=[gabr, hvb], w=[rw])
        k.op("act", lambda e: e.activation(out=rw[:, 0:8], in_=rw[:, 0:8], func=AF.Exp), r=[rw], w=[rw])
        k.op("act", lambda e: e.activation(out=rw[:, 0:8], in_=rw[:, 0:8], func=AF.Ln, bias=1.0, scale=1.0), r=[rw], w=[rw])
        k.op("dve", lambda e: e.tensor_tensor(out=rw[:, 0:8], in0=rw[:, 0:8], in1=nA[0:1, :], op=ALU.mult), r=[rw, nA], w=[rw])
        k.op("act", lambda e: e.activation(out=rw[:, 8:16], in_=rw[:, 0:8], func=AF.Exp), r=[rw], w=[rw])
        k.op("act", lambda e: e.activation(out=rw[:, 16:24], in_=gabr[:, 8:16], func=AF.Sigmoid), r=[gabr, rw], w=[rw])
        ba = k.bank()
        bb_ = k.bank()
        for h in range(8):
            bk_ = ba if h < 4 else bb_
            k.op("pe", lambda e, h=h, bk_=bk_: e.matmul(bk_[0:1, (h % 4) * 128:(h % 4 + 1) * 128], lhsT=kcf[:, h:h + 1], rhs=S0[:, h, :], start=True, stop=True),
                 r=[kcf, S0], w=[bk_])
        bv = k.bank()
        bvb = bv[:].bitcast(BF16)
        for h in range(8):
            k.op("pe", lambda e, h=h: e.transpose(out=bvb[0:1, h * 128:(h + 1) * 128], in_=vc[:, h:h + 1], identity=ident_b[:]), r=[vc, ident_b], w=[bv])
        k.op("dve", lambda e: e.tensor_tensor(out=t1[:, 0:4, :], in0=ba[0:1, :].rearrange("p (a b) -> p a b", a=4), in1=bc_row(rw[:, 8:16])[:, 0:4, :], op=ALU.mult),
             r=[ba, rw], w=[t1])
        k.op("dve", lambda e: e.tensor_tensor(out=t1[:, 4:8, :], in0=bb_[0:1, :].rearrange("p (a b) -> p a b", a=4), in1=bc_row(rw[:, 8:16])[:, 4:8, :], op=ALU.mult),
             r=[bb_, rw, t1], w=[t1])
        k.op("dve", lambda e: e.tensor_tensor(out=t1[:], in0=bvb[0:1, 0:1024].rearrange("p (a b) -> p a b", a=8), in1=t1[:], op=ALU.subtract), r=[bv, t1], w=[t1])
        k.op("dve", lambda e: e.tensor_tensor(out=t1[:], in0=t1[:], in1=bc_row(rw[:, 16:24]), op=ALU.mult), r=[t1, rw], w=[t1])
        bd0 = k.bank()
        bd1 = k.bank()
        k.op("pe", lambda e: e.matmul(bd0[:, :], lhsT=ones_f[0:1, :], rhs=t1[:].rearrange("p a b -> p (a b)")[:, 0:512], start=True, stop=True), r=[ones_f, t1], w=[bd0])
        k.op("pe", lambda e: e.matmul(bd1[:, :], lhsT=ones_f[0:1, :], rhs=t1[:].rearrange("p a b -> p (a b)")[:, 512:1024], start=True, stop=True), r=[ones_f, t1], w=[bd1])
        bab = k.bank()
        k.op("pe", lambda e: e.matmul(bab[:, 0:8], lhsT=ones_f[0:1, :], rhs=rw[:, 8:16], start=True, stop=True), r=[ones_f, rw], w=[bab])
        k.op("act", lambda e: e.copy(out=abs_[:], in_=bab[:, 0:8]), r=[bab], w=[abs_])
        k.op("dve", lambda e: e.tensor_tensor(out=S0[:], in0=S0[:], in1=abs_[:].unsqueeze(2).to_broadcast([128, 8, 128]), op=ALU.mult), r=[S0, abs_], w=[S0])
        for h in range(8):
            bd = bd0 if h < 4 else bd1
            k.op("dve", lambda e, h=h, bd=bd: e.scalar_tensor_tensor(out=S0[:, h, :], in0=bd[:, (h % 4) * 128:(h % 4 + 1) * 128], scalar=kcf[:, h:h + 1], op0=ALU.mult,
                                                                      in1=S0[:, h, :], op1=ALU.add), r=[bd, kcf, S0], w=[S0])
        k.dma("sp", ssm_s.ap()[s_i].rearrange("h a b -> a h b"), S0[:], r=[S0])
        bo0 = k.bank()
        bo1 = k.bank()
        for h in range(8):
            bk_ = bo0 if h < 4 else bo1
            k.op("pe", lambda e, h=h, bk_=bk_: e.matmul(bk_[0:1, (h % 4) * 128:(h % 4 + 1) * 128], lhsT=qcf[:, h:h + 1], rhs=S0[:, h, :], start=True, stop=True),
                 r=[qcf, S0], w=[bk_])
        k.op("act", lambda e: e.copy(out=orow[:, 0:4, :], in_=bo0[0:1, :].rearrange("p (a b) -> p a b", a=4)), r=[bo0], w=[orow])
        k.op("act", lambda e: e.copy(out=orow[:, 4:8, :], in_=bo1[0:1, :].rearrange("p (a b) -> p a b", a=4)), r=[bo1, orow], w=[orow])
        k.op("dve", lambda e: e.tensor_tensor(out=sqr[:], in0=orow[:], in1=orow[:], op=ALU.mult), r=[orow], w=[sqr])
        k.op("dve", lambda e: e.tensor_reduce(out=rw[:, 24:32], in_=sqr[:], axis=AX.X, op=ALU.add), r=[sqr, rw], w=[rw])
        k.op("act", lambda e: e.activation(out=rw[:, 32:40], in_=rw[:, 24:32], func=AF.Sqrt, scale=1.0 / 128, bias=EPS), r=[rw], w=[rw])
        k.op("dve", lambda e: e.reciprocal(out=rw[:, 40:48], in_=rw[:, 32:40]), r=[rw], w=[rw])
        k.op("act", lambda e: e.activation(out=zrs[:].rearrange("p a b -> p (a b)"), in_=zr[:], func=AF.Silu), r=[zr], w=[zrs])
        k.op("dve", lambda e: e.tensor_tensor(out=orow[:], in0=orow[:], in1=bc_row(rw[:, 40:48]), op=ALU.mult), r=[orow, rw], w=[orow])
        k.op("dve", lambda e: e.tensor_tensor(out=orow[:], in0=orow[:], in1=gnb[0:1, :].unsqueeze(1).to_broadcast([1, 8, 128]), op=ALU.mult), r=[orow, gnb], w=[orow])
        k.op("dve", lambda e: e.tensor_tensor(out=oar[:].rearrange("p (a b) -> p a b", a=8), in0=orow[:], in1=zrs[:], op=ALU.mult), r=[orow, zrs], w=[oar])
        bt_ = k.bank()
        for h in range(8):
            k.op("pe", lambda e, h=h: e.matmul(bt_[:, h:h + 1], lhsT=oar[0:1, h * 128:(h + 1) * 128], rhs=ones_b[0:1, 0:1], start=True, stop=True), r=[oar, ones_b], w=[bt_])
        k.op("act", lambda e: e.copy(out=mixT[:, 0:8, col], in_=bt_[:, 0:8]), r=[bt_], w=[(mixT, "s%d" % s_i)])
    for s_i in range(TS):
        sample_gdn(s_i)
    k.barrier()
    k.release(m3)
    if stop_after == "P3S":
        return k.finish()
    m4 = k.mark()
    NEG = -30000.0
    NIT = 26
    kTb = k.alloc("kTb", [128, 2, TT], BF16)
    vtok = k.alloc("vtok", [128, NT, 256], BF16)
    ikT4 = k.alloc("ikT4", [128, TT], BF16)
    k.dma("sp", kTb[:], akT.ap().rearrange("h d t -> d h t"), r=["plain"], w=[kTb])
    k.dma("sp", vtok[:], av[0:T, :].rearrange("(t p) c -> p t c", p=128), r=["av"], w=[vtok])
    k.dma("sp", ikT4[:], ikTd.ap(), r=["ikTd"], w=[ikT4])
    ones_f4 = k.alloc("ones_f4", [128, 128], F32)
    zeros_b = k.alloc("zeros_b", [128, 128], BF16)
    Jm = k.alloc("Jm", [128, 128], F32)
    CMT = k.alloc("CMT", [128, 128], F32)
    CM = k.alloc("CM", [128, 128], F32)
    k.op("pool", lambda e: e.memset(ones_f4[:], 1.0), w=[ones_f4])
    k.op("pool", lambda e: e.memset(zeros_b[:], 0.0), w=[zeros_b])
    k.op("pool", lambda e: e.memset(Jm[:], 1.0), w=[Jm])
    k.op("pool", lambda e: e.affine_select(out=Jm[:], in_=Jm[:], pattern=[[1, 128]], compare_op=ALU.is_equal, fill=0.0, base=-127, channel_multiplier=1), r=[Jm], w=[Jm])
    k.op("pool", lambda e: e.memset(CMT[:], 0.0), w=[CMT])
    k.op("pool", lambda e: e.affine_select(out=CMT[:], in_=CMT[:], pattern=[[1, 128]], compare_op=ALU.is_ge, fill=NEG, base=0, channel_multiplier=-1), r=[CMT], w=[CMT])
    k.op("pool", lambda e: e.memset(CM[:], 0.0), w=[CM])
    k.op("pool", lambda e: e.affine_select(out=CM[:], in_=CM[:], pattern=[[-1, 128]], compare_op=ALU.is_ge, fill=NEG, base=0, channel_multiplier=1), r=[CM], w=[CM])
    rb = k.alloc("rb", [32, 8], F32)
    rb31 = k.alloc("rb31", [32, 8], F32)
    bohs = k.alloc("bohs", [32, 384], F32)
    bvec = k.alloc("bvec", [8, 384], F32)
    Tp = k.alloc("Tp", [128, 8, 128], F32)
    Bt4 = [k.alloc(f"Bt4_{i}", [128, 8, 128], F32) for i in range(2)]
    k.dma("sp", rb[:], rel_bias.ap(), w=[rb])
    k.dma("sp", rb31[:], rel_bias[31:32, :].to_broadcast([32, 8]), w=[rb31])
    k.dma("sp", bohs[:], boh.ap(), w=[bohs])
    k.op("dve", lambda e: e.tensor_tensor(out=rb[:], in0=rb[:], in1=rb31[:], op=ALU.subtract), r=[rb, rb31], w=[rb])
    bkb = k.bank()
    k.op("pe", lambda e: e.matmul(bkb[0:8, 0:384], lhsT=rb[:], rhs=bohs[:], start=True, stop=True), r=[rb, bohs], w=[bkb])
    k.op("act", lambda e: e.copy(out=bvec[:], in_=bkb[0:8, 0:384]), r=[bkb], w=[bvec])
    k.dma("sp", biasd.ap(), bvec[:], r=[bvec], w=["biasd"])

    def mk_bias(dl):
        k.dma("sp", Tp[:], bass.AP(tensor=biasd, offset=128 * dl, ap=[[1, 128], [384, 8], [1, 128]]), r=["biasd"], w=[Tp])
        for half in range(2):
            bj = k.bank()
            k.op("pe", lambda e, bj=bj, half=half: e.matmul(bj[:, :], lhsT=Jm[:], rhs=Tp[:].rearrange("p a b -> p (a b)")[:, half * 512:(half + 1) * 512], start=True, stop=True), r=[Jm, Tp], w=[bj])
            k.op("act", lambda e, bj=bj, half=half: e.copy(out=Bt4[dl][:].rearrange("p a b -> p (a b)")[:, half * 512:(half + 1) * 512], in_=bj[:, :]), r=[bj], w=[Bt4[dl]])

    mk_bias(0)
    mk_bias(1)

    qbT = [k.alloc(f"qbT{i}", [128, 8, 128], BF16) for i in range(2)]
    qiT = [k.alloc(f"qiT{i}", [128, 16, 128], BF16) for i in range(2)]
    iwt = [k.alloc(f"iwt{i}", [128, 48], F32) for i in range(2)]
    sc = k.alloc("sc", [128, T], F32)
    jnk = k.alloc("jnk", [128, T], BF16)
    rl = [k.alloc(f"rl{i}", [128, 512], F32) for i in range(2)]
    bs = k.alloc("bs", [128, 8], F32)
    negT = k.alloc("negT", [128, NT, 128], F32)
    Eb = [k.alloc(f"Eb{i}", [128, 4, 128], F32) for i in range(2)]
    PTs = [k.alloc(f"PTs{i}", [128, 4, 128], BF16) for i in range(3)]
    rden = k.alloc("rden", [128, 8], F32)
    oab = k.alloc("oab", [128, 8, 128], BF16)
    ecnt = [0]

    def attn_tile(qb):
        L = 128 * (qb + 1)
        tsl = slice(qb * 128, (qb + 1) * 128)
        qb_ = qbT[qb % 2]
        qi_ = qiT[qb % 2]
        iw_ = iwt[qb % 2]
        k.dma("sp", qb_[:], aq.ap()[:, :, tsl].rearrange("h d t -> d h t"), r=["plain"], w=[qb_])
        k.dma("sp", qi_[:], iq.ap()[:, :, tsl].rearrange("h d t -> d h t"), r=["plain"], w=[qi_])
        k.dma("sp", iw_[:, 0:16], iw[qb * 128:(qb + 1) * 128, :], r=["iw"], w=[iw_])
        if qb >= 2:
            k.op("act", lambda e: e.activation(out=iw_[:, 16:32], in_=iw_[:, 0:16], func=AF.Abs), r=[iw_], w=[iw_])
            k.op("act", lambda e: e.activation(out=iw_[:, 32:48], in_=iw_[:, 0:16], func=AF.Sign), r=[iw_], w=[iw_])
            def idx_kg(kg):
                c0 = kg * 512
                n = min(512, L - c0)
                for h in range(16):
                    bk_ = k.bank()
                    r_ = rl[h % 2]
                    k.op("pe", lambda e, h=h, bk_=bk_: e.matmul(bk_[:, 0:n], lhsT=qi_[:, h, :], rhs=ikT4[:, c0:c0 + n], start=True, stop=True), r=[qi_, ikT4], w=[bk_])
                    k.op("act", lambda e, h=h, bk_=bk_, r_=r_: e.activation(out=r_[:, 0:n], in_=bk_[:, 0:n], func=AF.Relu, scale=iw_[:, 16 + h:17 + h]), r=[bk_, iw_], w=[r_])
                    if h == 0:
                        k.op("dve", lambda e, r_=r_: e.tensor_scalar(out=sc[:, c0:c0 + n], in0=r_[:, 0:n], scalar1=iw_[:, 32:33], scalar2=None, op0=ALU.mult), r=[r_, iw_], w=[sc])
                    else:
                        k.op("dve", lambda e, h=h, r_=r_: e.scalar_tensor_tensor(out=sc[:, c0:c0 + n], in0=r_[:, 0:n], scalar=iw_[:, 32 + h:33 + h], op0=ALU.mult,
                                                                                in1=sc[:, c0:c0 + n], op1=ALU.add), r=[r_, iw_, sc], w=[sc])
            for kg in range((L + 511) // 512):
                idx_kg(kg)
            k.op("dve", lambda e: e.tensor_reduce(out=bs[:, 0:1], in_=sc[:, 0:L], axis=AX.X, op=ALU.max), r=[sc], w=[bs])
            k.op("dve", lambda e: e.tensor_reduce(out=bs[:, 1:2], in_=sc[:, 0:L], axis=AX.X, op=ALU.min), r=[sc, bs], w=[bs])
            k.op("dve", lambda e: e.tensor_scalar(out=bs[:, 1:2], in0=bs[:, 1:2], scalar1=-1.0, scalar2=None, op0=ALU.add), r=[bs], w=[bs])
            k.op("dve", lambda e: e.tensor_tensor(out=bs[:, 2:3], in0=bs[:, 0:1], in1=bs[:, 1:2], op=ALU.subtract), r=[bs], w=[bs])
            k.op("dve", lambda e: e.tensor_tensor(out=sc[:, L - 128:L], in0=sc[:, L - 128:L], in1=CM[:], op=ALU.add), r=[sc, CM], w=[sc])
            for it in range(NIT):
                f = 2.0 ** -(it + 1)
                k.op("dve", lambda e, f=f: e.scalar_tensor_tensor(out=bs[:, 3:4], in0=bs[:, 2:3], scalar=f, op0=ALU.mult, in1=bs[:, 1:2], op1=ALU.add), r=[bs], w=[bs])
                k.op("dve", lambda e: e.tensor_scalar(out=jnk[:, 0:L], in0=sc[:, 0:L], scalar1=bs[:, 3:4], scalar2=None, op0=ALU.is_gt, op1=ALU.add, accum_out=bs[:, 4:5]),
                     r=[sc, bs], w=[jnk, bs])
                k.op("dve", lambda e, f=f: e.tensor_scalar(out=bs[:, 5:6], in0=bs[:, 4:5], scalar1=255.5, scalar2=f, op0=ALU.is_gt, op1=ALU.mult), r=[bs], w=[bs])
                k.op("dve", lambda e: e.scalar_tensor_tensor(out=bs[:, 1:2], in0=bs[:, 5:6], scalar=bs[:, 2:3], op0=ALU.mult, in1=bs[:, 1:2], op1=ALU.add), r=[bs], w=[bs])
            k.op("dve", lambda e: e.tensor_scalar(out=sc[:, 0:L], in0=sc[:, 0:L], scalar1=bs[:, 1:2], scalar2=None, op0=ALU.subtract), r=[sc, bs], w=[sc])
            for k4 in range((qb + 1 + 3) // 4):
                nb = min(4, qb + 1 - k4 * 4)
                bk_ = k.bank()
                for j in range(nb):
                    kb = k4 * 4 + j
                    k.op("pe", lambda e, j=j, kb=kb, bk_=bk_: e.transpose(out=bk_[:, j * 128:(j + 1) * 128], in_=sc[:, kb * 128:(kb + 1) * 128], identity=ident_f[:]),
                         r=[sc, ident_f], w=[bk_])
                k.op("dve", lambda e, k4=k4, nb=nb, bk_=bk_: e.tensor_scalar(out=negT[:, k4 * 4:k4 * 4 + nb, :].rearrange("p a b -> p (a b)"), in0=bk_[:, 0:nb * 128],
                                                                             scalar1=0.0, scalar2=NEG, op0=ALU.is_le, op1=ALU.mult), r=[bk_], w=[negT])
            k.op("dve", lambda e: e.tensor_tensor(out=negT[:, qb, :], in0=negT[:, qb, :], in1=CMT[:], op=ALU.add), r=[negT, CMT], w=[negT])
        else:
            if qb == 1:
                k.op("pool", lambda e: e.memset(negT[:, 0, :], 0.0), w=[negT])
            k.op("pool", lambda e: e.tensor_copy(out=negT[:, qb, :], in_=CMT[:]), r=[CMT, negT], w=[negT])
        bo0 = k.bank()
        bo1 = k.bank()
        bdn = k.bank()
        k.reserved = {bo0.name, bo1.name, bdn.name}
        for bz_ in (bo0, bo1, bdn):
            k.op("pe", lambda e, bz_=bz_: e.matmul(bz_[:, :], lhsT=zeros_b[:], rhs=qi_[:].rearrange("p a b -> p (a b)")[:, 0:512], start=True, stop=False), r=[zeros_b, qi_], w=[bz_])
        def kv_pair(kb, kvh):
            bl = k.bank()
            k.op("pe", lambda e, bl=bl: e.matmul(bl[:, :], lhsT=kTb[:, kvh, kb * 128:(kb + 1) * 128], rhs=qb_[:, kvh * 4:(kvh + 1) * 4, :].rearrange("p a b -> p (a b)"),
                                                start=True, stop=True), r=[kTb, qb_], w=[bl])
            E_ = Eb[ecnt[0] % 2]
            P_ = PTs[ecnt[0] % 3]
            ecnt[0] += 1
            k.op("dve", lambda e, bl=bl, E_=E_: e.tensor_tensor(out=E_[:], in0=bl[:, :].rearrange("p (a b) -> p a b", a=4), in1=negT[:, kb, :].unsqueeze(1).to_broadcast([128, 4, 128]),
                                                             op=ALU.add), r=[bl, negT], w=[E_])
            if qb - kb <= 1:
                k.op("pool", lambda e, E_=E_: e.tensor_tensor(out=E_[:], in0=E_[:], in1=Bt4[qb - kb][:, kvh * 4:(kvh + 1) * 4, :], op=ALU.add), r=[E_, Bt4[qb - kb]], w=[E_])
            k.op("act", lambda e, E_=E_, P_=P_: e.activation(out=P_[:], in_=E_[:], func=AF.Exp), r=[E_], w=[P_])
            last = (kb == qb)
            for g_ in range(4):
                h = kvh * 4 + g_
                bo = bo0 if h < 4 else bo1
                k.op("pe", lambda e, g_=g_, h=h, bo=bo, P_=P_: e.matmul(bo[:, (h % 4) * 128:(h % 4 + 1) * 128], lhsT=P_[:, g_, :], rhs=vtok[:, kb, kvh * 128:(kvh + 1) * 128],
                                                                    start=False, stop=last), r=[P_, vtok], w=[bo])
                k.op("pe", lambda e, g_=g_, h=h, P_=P_: e.matmul(bdn[:, h:h + 1], lhsT=P_[:, g_, :], rhs=ones_b[:, 0:1], start=False, stop=last), r=[P_, ones_b], w=[bdn])
        for kb in range(qb + 1):
            for kvh in range(2):
                kv_pair(kb, kvh)
        k.reserved = set()
        k.op("dve", lambda e: e.reciprocal(out=rden[:], in_=bdn[:, 0:8]), r=[bdn], w=[rden])
        for half, bo in ((0, bo0), (1, bo1)):
            k.op("dve", lambda e, half=half, bo=bo: e.tensor_tensor(out=oab[:, half * 4:(half + 1) * 4, :], in0=bo[:, :].rearrange("p (a b) -> p a b", a=4),
                                                                    in1=rden[:, half * 4:(half + 1) * 4].unsqueeze(2).to_broadcast([128, 4, 128]), op=ALU.mult),
                 r=[bo, rden], w=[oab])
        bt_ = k.bank()
        btb = bt_[:].bitcast(BF16)
        for h in range(8):
            k.op("pe", lambda e, h=h: e.transpose(out=btb[:, h * 128:(h + 1) * 128], in_=oab[:, h, :], identity=ident_b[:]), r=[oab, ident_b], w=[bt_])
        k.op("act", lambda e: e.copy(out=mixT[:, 8:16, tsl], in_=btb[:, 0:1024].rearrange("p (a b) -> p a b", a=8)), r=[bt_], w=[(mixT, "b%d" % qb)])

    import os
    for qb in range(int(os.environ.get("QBMAX", str(NT)))):
        attn_tile(qb)
    if stop_after == "P4" and os.environ.get("P4DBG"):
        for nm, b, n in (("sc", sc, T), ("bs", bs, 8), ("negT", negT, NT * 128)):
            dd = dout("dbg_" + nm, [128, n], F32)
            src = b[:] if nm != "negT" else b[:].rearrange("p a b -> p (a b)")
            k.dma("sp", dd.ap(), src, r=[b])
    if stop_after == "P4":
        dbg = dout("dbg_mixT", [128, 16 * TT], BF16)
        k.barrier()
        k.dma("sp", dbg.ap(), mixT[:].rearrange("p a b -> p (a b)"))
        return k.finish()
    k.barrier()
    k.release(m4)
    NPG = NPAGES
    ones4 = k.alloc("ones4", [128, 128], F32)
    zeros4 = k.alloc("zeros4", [128, 128], F32)
    Ltri = k.alloc("Ltri", [128, 128], BF16)
    siota = k.alloc("siota", [128, 128], F32)
    piota = k.alloc("piota", [128, 1], F32)
    jrow = k.alloc("jrow", [128, 256], F32)
    jcol = k.alloc("jcol", [128, 2], F32)
    posc = k.alloc("posc", [128, 128], F32)
    k.op("pool", lambda e: e.memset(ones4[:], 1.0), w=[ones4])
    k.op("pool", lambda e: e.memset(zeros4[:], 0.0), w=[zeros4])
    k.op("pool", lambda e: e.memset(Ltri[:], 1.0), w=[Ltri])
    k.op("pool", lambda e: e.affine_select(out=Ltri[:], in_=Ltri[:], pattern=[[1, 128]], compare_op=ALU.is_ge, fill=0.0, base=-1, channel_multiplier=-1), r=[Ltri], w=[Ltri])
    k.op("pool", lambda e: e.iota(siota[:], pattern=[[1, 128]], base=0, channel_multiplier=0, allow_small_or_imprecise_dtypes=True), w=[siota])
    k.op("pool", lambda e: e.iota(piota[:], pattern=[[0, 1]], base=0, channel_multiplier=1, allow_small_or_imprecise_dtypes=True), w=[piota])
    k.op("pool", lambda e: e.iota(jrow[:], pattern=[[1, 256]], base=0, channel_multiplier=0, allow_small_or_imprecise_dtypes=True), w=[jrow])
    k.op("pool", lambda e: e.iota(jcol[:], pattern=[[128, 2]], base=0, channel_multiplier=1, allow_small_or_imprecise_dtypes=True), w=[jcol])
    k.op("pool", lambda e: e.iota(posc[:], pattern=[[1, 128]], base=0, channel_multiplier=128, allow_small_or_imprecise_dtypes=True), w=[posc])
    thrb = k.alloc("thrb", [128, 31], F32)
    k.dma("sp", thrb[:], bthr.ap().to_broadcast([128, 31]), w=[thrb])
    rbs = k.alloc("rbs", [32, 8], F32)
    rbT = k.alloc("rbT", [8, 32], F32)
    drbT = k.alloc("drbT", [128, 8, 32], F32)
    k.dma("sp", rbs[:], rel_bias.ap(), w=[rbs])
    bq = k.bank()
    k.op("pe", lambda e: e.transpose(out=bq[0:8, 0:32], in_=rbs[:], identity=ident_f[0:32, 0:32]), r=[rbs, ident_f], w=[bq])
    k.op("act", lambda e: e.copy(out=rbT[:], in_=bq[0:8, 0:32]), r=[bq], w=[rbT])
    k.dma("sp", rbTd.ap(), rbT[:], r=[rbT], w=["rbTd"])
    k.dma("sp", drbT[:].rearrange("p a b -> p (a b)"), rbTd.ap().rearrange("a b -> (a b)").unsqueeze(0).to_broadcast([128, 256]), r=["rbTd"], w=[drbT])
    rb0b = k.alloc("rb0b", [128, 8], F32)
    k.op("dve", lambda e: e.tensor_copy(out=rb0b[:], in_=drbT[:, :, 0]), r=[drbT], w=[rb0b])
    dtmp = k.alloc("dtmp", [128, 8, 31], F32)
    k.op("dve", lambda e: e.tensor_tensor(out=dtmp[:], in0=drbT[:, :, 1:32], in1=drbT[:, :, 0:31], op=ALU.subtract), r=[drbT], w=[dtmp])
    pt_i = k.alloc("pt_i", [128, TS], I32)
    pt_f = k.alloc("pt_f", [128, TS], F32)
    k.dma("sp", pt_i[:], page_table.ap().rearrange("s p -> p s"), w=[pt_i], slow=True)
    k.op("dve", lambda e: e.tensor_copy(out=pt_f[:], in_=pt_i[:]), r=[pt_i], w=[pt_f])
    k.op("dve", lambda e: e.tensor_scalar(out=pt_f[:], in0=pt_f[:], scalar1=128.0, scalar2=None, op0=ALU.mult), r=[pt_f], w=[pt_f])
    qiS = k.alloc("qiS", [128, TS, 16], BF16)
    wS = k.alloc("wS", [128, TS, 16], F32)
    kiS = k.alloc("kiS", [128, TS], BF16)
    qbS = k.alloc("qbS", [128, 8, TS], BF16)
    knS = k.alloc("knS", [128, 2, TS], BF16)
    for s_i in range(TS):
        k.dma("sp", qiS[:, s_i, :], iq.ap()[:, :, T + s_i].rearrange("h d -> d h"), r=["plain"], w=[qiS], slow=True)
    k.dma("sp", wS[:].rearrange("p a b -> p (a b)"), iw[T:TT, :].rearrange("a b -> (a b)").unsqueeze(0).to_broadcast([128, TS * 16]), r=["iw"], w=[wS])
    k.dma("sp", kiS[:], ikTd[:, T:TT], r=["ikTd"], w=[kiS])
    k.dma("sp", qbS[:], aq.ap()[:, :, T:TT].rearrange("h d s -> d h s"), r=["plain"], w=[qbS])
    k.dma("sp", knS[:], akT.ap()[:, :, T:TT].rearrange("h d s -> d h s"), r=["plain"], w=[knS])
    scS = k.alloc("scS", [128, TS, 128], F32)
    snew = k.alloc("snew", [128, TS], F32)
    Gp = k.alloc("Gp", [128, NPG * 128], F32)
    kTs = [k.alloc(f"kTs{i}", [128, 4, 128], BF16) for i in range(2)]
    rr = k.alloc("rr", [128, 32, 16], F32)
    knb = k.alloc("knb", [128, 128], BF16)
    ckx2 = cache_kidx.ap()

    def dma_raw(q, fn, r=(), w=()):
        i = k.drr[q]
        k.drr[q] = (i + 1) % len(k.dsem[q])
        sk = ("d", q, i)
        waits = k._collect(q, r, w)
        prev = k.dcnt[q][i]
        kn = k.known[q]
        if prev > 0 and kn.get(sk, 0) < prev:
            kn[sk] = prev
            waits.append((sk, prev))
        k.dcnt[q][i] = prev + 16
        tok = (sk, prev + 16)
        k.ops[q].append((waits, fn, (sk, 16)))
        k._update(tok, r, w)
        return tok

    def scores_seq(s_i):
        dma_raw("pool", lambda e: e.indirect_dma_start(out=Gp[:], out_offset=None, in_=ckx2, in_offset=bass.IndirectOffsetOnAxis(ap=pt_i[:, s_i:s_i + 1], axis=0)),
                r=[pt_i], w=[Gp])
        scb = [k.bank() for _ in range(4)]
        k.reserved = {b_.name for b_ in scb}
        for sb in range(32):
            bt_ = k.bank()
            kt_ = kTs[sb % 2]
            for j in range(4):
                sl_ = 4 * sb + j
                k.op("pe", lambda e, j=j, sl_=sl_, bt_=bt_: e.transpose(out=bt_[:, j * 128:(j + 1) * 128], in_=Gp[:, sl_ * 128:(sl_ + 1) * 128], identity=ident_f[:]),
                     r=[Gp, ident_f], w=[bt_])
            k.op("act", lambda e, bt_=bt_, kt_=kt_: e.copy(out=kt_[:].rearrange("p a b -> p (a b)"), in_=bt_[:, :]), r=[bt_], w=[kt_])
            for j in range(4):
                sl_ = 4 * sb + j
                sbk = scb[sl_ // 32]
                k.op("pe", lambda e, j=j, sl_=sl_, sbk=sbk, kt_=kt_: e.matmul(sbk[:, (sl_ % 32) * 16:(sl_ % 32 + 1) * 16], lhsT=kt_[:, j, :], rhs=qiS[:, s_i, :], start=True, stop=True),
                     r=[kt_, qiS], w=[sbk])
        k.reserved = set()
        for b_i in range(4):
            sbk = scb[b_i]
            k.op("dve", lambda e, sbk=sbk: e.tensor_scalar(out=rr[:].rearrange("p a b -> p (a b)"), in0=sbk[:, :], scalar1=0.0, scalar2=None, op0=ALU.max), r=[sbk], w=[rr])
            k.op("dve", lambda e: e.tensor_tensor(out=rr[:], in0=rr[:], in1=wS[:, s_i, :].unsqueeze(1).to_broadcast([128, 32, 16]), op=ALU.mult), r=[rr, wS], w=[rr])
            k.op("dve", lambda e, b_i=b_i: e.tensor_reduce(out=scS[:, s_i, b_i * 32:(b_i + 1) * 32], in_=rr[:], axis=AX.X, op=ALU.add), r=[rr], w=[scS])
        k.op("dve", lambda e: e.tensor_copy(out=knb[:], in_=kiS[:, s_i:s_i + 1].to_broadcast([128, 128])), r=[kiS], w=[knb])
        bn = k.bank()
        k.op("pe", lambda e: e.matmul(bn[:, 0:16], lhsT=knb[:], rhs=qiS[:, s_i, :], start=True, stop=True), r=[knb, qiS], w=[bn])
        k.op("dve", lambda e: e.tensor_scalar(out=rr[:, 0, :], in0=bn[:, 0:16], scalar1=0.0, scalar2=None, op0=ALU.max), r=[bn], w=[rr])
        k.op("dve", lambda e: e.tensor_tensor(out=rr[:, 0, :], in0=rr[:, 0, :], in1=wS[:, s_i, :], op=ALU.mult), r=[rr, wS], w=[rr])
        k.op("dve", lambda e: e.tensor_reduce(out=snew[:, s_i:s_i + 1], in_=rr[:, 0, :], axis=AX.X, op=ALU.add), r=[rr], w=[snew])

    for s_i in range(TS):
        scores_seq(s_i)

    pm = k.alloc("pm", [128, 2 * TS], F32)
    gmm = k.alloc("gmm", [TS, 4], F32)
    dgm = k.alloc("dgm", [TS, 2 * TS], F32)
    lo_ = k.alloc("lo_", [128, TS], F32)
    w0_ = k.alloc("w0_", [128, TS], F32)
    bsS = k.alloc("bsS", [128, 6 * TS], F32)
    cmpb = k.alloc("cmpb", [128, TS, 128], F32)
    k.op("dve", lambda e: e.tensor_reduce(out=pm[:, 0:TS], in_=scS[:], axis=AX.X, op=ALU.max), r=[scS], w=[pm])
    k.op("dve", lambda e: e.tensor_reduce(out=pm[:, TS:2 * TS], in_=scS[:], axis=AX.X, op=ALU.min), r=[scS, pm], w=[pm])
    k.op("dve", lambda e: e.tensor_tensor(out=pm[:, 0:TS], in0=pm[:, 0:TS], in1=snew[:], op=ALU.max), r=[pm, snew], w=[pm])
    k.op("dve", lambda e: e.tensor_tensor(out=pm[:, TS:2 * TS], in0=pm[:, TS:2 * TS], in1=snew[:], op=ALU.min), r=[pm, snew], w=[pm])
    bmx = k.bank()
    bmn = k.bank()
    k.op("pe", lambda e: e.transpose(out=bmx[0:TS, 0:128], in_=pm[:, 0:TS], identity=ident_f[:]), r=[pm, ident_f], w=[bmx])
    k.op("pe", lambda e: e.transpose(out=bmn[0:TS, 0:128], in_=pm[:, TS:2 * TS], identity=ident_f[:]), r=[pm, ident_f], w=[bmn])
    k.op("dve", lambda e: e.tensor_reduce(out=gmm[:, 0:1], in_=bmx[0:TS, 0:128], axis=AX.X, op=ALU.max), r=[bmx], w=[gmm])
    k.op("dve", lambda e: e.tensor_reduce(out=gmm[:, 1:2], in_=bmn[0:TS, 0:128], axis=AX.X, op=ALU.min), r=[bmn, gmm], w=[gmm])
    k.op("dve", lambda e: e.tensor_scalar(out=gmm[:, 1:2], in0=gmm[:, 1:2], scalar1=-1.0, scalar2=None, op0=ALU.add), r=[gmm], w=[gmm])
    k.op("dve", lambda e: e.tensor_tensor(out=gmm[:, 2:3], in0=gmm[:, 0:1], in1=gmm[:, 1:2], op=ALU.subtract), r=[gmm], w=[gmm])
    k.op("dve", lambda e: e.tensor_scalar(out=dgm[:, 0:TS], in0=ident_f[0:TS, 0:TS], scalar1=gmm[:, 1:2], scalar2=None, op0=ALU.mult), r=[gmm, ident_f], w=[dgm])
    k.op("dve", lambda e: e.tensor_scalar(out=dgm[:, TS:2 * TS], in0=ident_f[0:TS, 0:TS], scalar1=gmm[:, 2:3], scalar2=None, op0=ALU.mult), r=[gmm, ident_f, dgm], w=[dgm])
    bbc = k.bank()
    k.op("pe", lambda e: e.matmul(bbc[:, 0:2 * TS], lhsT=ones4[0:TS, :], rhs=dgm[:], start=True, stop=True), r=[ones4, dgm], w=[bbc])
    k.op("act", lambda e: e.copy(out=lo_[:], in_=bbc[:, 0:TS]), r=[bbc], w=[lo_])
    k.op("act", lambda e: e.copy(out=w0_[:], in_=bbc[:, TS:2 * TS]), r=[bbc], w=[w0_])
    mid_ = bsS[:, 0:TS]
    cnt_ = bsS[:, TS:2 * TS]
    gn_ = bsS[:, 2 * TS:3 * TS]
    tot_ = bsS[:, 3 * TS:4 * TS]
    ge_ = bsS[:, 4 * TS:5 * TS]

    def bis_iter(it):
        f = 2.0 ** -(it + 1)
        k.op("dve", lambda e: e.scalar_tensor_tensor(out=mid_, in0=w0_[:], scalar=f, op0=ALU.mult, in1=lo_[:], op1=ALU.add), r=[w0_, lo_], w=[bsS])
        k.op("dve", lambda e: e.tensor_tensor(out=cmpb[:], in0=scS[:], in1=mid_.unsqueeze(2).to_broadcast([128, TS, 128]), op=ALU.is_gt), r=[scS, bsS], w=[cmpb])
        k.op("dve", lambda e: e.tensor_reduce(out=cnt_, in_=cmpb[:], axis=AX.X, op=ALU.add), r=[cmpb, bsS], w=[bsS])
        bc_ = k.bank()
        k.op("pe", lambda e: e.matmul(bc_[:, 0:TS], lhsT=ones4[:], rhs=cnt_, start=True, stop=True), r=[ones4, bsS], w=[bc_])
        k.op("dve", lambda e: e.tensor_tensor(out=gn_, in0=snew[:], in1=mid_, op=ALU.is_gt), r=[snew, bsS], w=[bsS])
        k.op("dve", lambda e: e.tensor_tensor(out=tot_, in0=bc_[:, 0:TS], in1=gn_, op=ALU.add), r=[bc_, bsS], w=[bsS])
        k.op("dve", lambda e: e.tensor_scalar(out=ge_, in0=tot_, scalar1=255.5, scalar2=f, op0=ALU.is_gt, op1=ALU.mult), r=[bsS], w=[bsS])
        k.op("dve", lambda e: e.tensor_tensor(out=ge_, in0=ge_, in1=w0_[:], op=ALU.mult), r=[bsS, w0_], w=[bsS])
        k.op("dve", lambda e: e.tensor_tensor(out=lo_[:], in0=lo_[:], in1=ge_, op=ALU.add), r=[lo_, bsS], w=[lo_])

    for it in range(26):
        bis_iter(it)

    Msel = k.alloc("Msel", [128, 128], F32)
    Mb = k.alloc("Mb", [128, 128], BF16)
    Bs = k.alloc("Bs", [128, 128], F32)
    cum = k.alloc("cum", [128, 128], F32)
    rank = k.alloc("rank", [128, 128], F32)
    payl = k.alloc("payl", [128, 128, 2], F32)
    Soh = [k.alloc(f"Soh{i}", [128, 256], F32) for i in range(2)]
    idxf = k.alloc("idxf", [128, 4], F32)
    idx_i = k.alloc("idx_i", [128, 2], I32)
    Ksel = k.alloc("Ksel", [128, 2, 256], F32)
    Vsel = k.alloc("Vsel", [128, 2, 256], F32)
    KTs = k.alloc("KTs", [128, 4, 128], F32)
    qf = k.alloc("qf", [128, 8], F32)
    knf = k.alloc("knf", [128, 2], F32)
    sm = k.alloc("sm", [128, 64], F32)
    ind = k.alloc("ind", [128, 2, 31], F32)
    prod = k.alloc("prod", [128, 2, 8, 31], F32)
    Eg = k.alloc("Eg", [128, 2, 8], F32)
    rowp = k.alloc("rowp", [1, 64], F32)
    vnr = k.alloc("vnr", [1, 256], BF16)
    vnf = k.alloc("vnf", [1, 256], F32)
    osb = k.alloc("osb", [4, 2, 130], F32)
    ck2 = cache_k.ap()
    cv2 = cache_v.ap()
    k.op("dve", lambda e: e.tensor_copy(out=payl[:, :, 1], in_=posc[:]), r=[posc], w=[payl])

    def attend_seq(s_i):
        col = T + s_i
        k.op("dve", lambda e: e.tensor_scalar(out=Msel[:], in0=scS[:, s_i, :], scalar1=lo_[:, s_i:s_i + 1], scalar2=None, op0=ALU.is_gt), r=[scS, lo_], w=[Msel])
        k.op("dve", lambda e: e.tensor_copy(out=Mb[:], in_=Msel[:]), r=[Msel], w=[Mb])
        k.op("dve", lambda e: e.tensor_tensor(out=sm[:, 0:1], in0=snew[:, s_i:s_i + 1], in1=lo_[:, s_i:s_i + 1], op=ALU.is_gt), r=[snew, lo_], w=[sm])
        bA_ = k.bank()
        bB_ = k.bank()
        k.op("pe", lambda e: e.matmul(bA_[:, 0:128], lhsT=Ltri[:], rhs=Mb[:], start=True, stop=True), r=[Ltri, Mb], w=[bA_])
        k.op("pe", lambda e: e.matmul(bB_[:, 0:128], lhsT=ones_b[:], rhs=Mb[:], start=True, stop=True), r=[ones_b, Mb], w=[bB_])
        k.op("act", lambda e: e.copy(out=Bs[:], in_=bB_[:, 0:128]), r=[bB_], w=[Bs])
        k.op("dve", lambda e: e.tensor_tensor_scan(out=cum[:], data0=Bs[:], data1=zeros4[:], initial=0.0, op0=ALU.add, op1=ALU.add), r=[Bs, zeros4], w=[cum])
        k.op("dve", lambda e: e.tensor_tensor(out=rank[:], in0=cum[:], in1=Bs[:], op=ALU.subtract), r=[cum, Bs], w=[rank])
        k.op("dve", lambda e: e.tensor_tensor(out=rank[:], in0=rank[:], in1=bA_[:, 0:128], op=ALU.add), r=[rank, bA_], w=[rank])
        k.op("dve", lambda e: e.scalar_tensor_tensor(out=rank[:], in0=rank[:], scalar=1.0, op0=ALU.add, in1=Msel[:], op1=ALU.mult), r=[rank, Msel], w=[rank])
        k.op("dve", lambda e: e.tensor_scalar(out=rank[:], in0=rank[:], scalar1=-1.0, scalar2=None, op0=ALU.add), r=[rank], w=[rank])
        k.op("dve", lambda e: e.tensor_scalar(out=payl[:, :, 0], in0=siota[:], scalar1=pt_f[:, s_i:s_i + 1], scalar2=None, op0=ALU.add), r=[siota, pt_f], w=[payl])
        bacc = k.bank()
        k.reserved = {bacc.name}
        k.op("pe", lambda e: e.matmul(bacc[:, 0:4], lhsT=zeros4[:], rhs=ones4[:, 0:4], start=True, stop=False), r=[zeros4, ones4], w=[bacc])
        for sl_ in range(128):
            so_ = Soh[sl_ % 2]
            k.op("dve", lambda e, sl_=sl_, so_=so_: e.tensor_scalar(out=so_[:], in0=jrow[:], scalar1=rank[:, sl_:sl_ + 1], scalar2=None, op0=ALU.is_equal), r=[jrow, rank], w=[so_])
            for half in range(2):
                k.op("pe", lambda e, sl_=sl_, so_=so_, half=half: e.matmul(bacc[:, half * 2:(half + 1) * 2], lhsT=so_[:, half * 128:(half + 1) * 128], rhs=payl[:, sl_, :],
                                                                      start=False, stop=(sl_ == 127)), r=[so_, payl], w=[bacc])
        k.reserved = set()
        k.op("act", lambda e: e.copy(out=idxf[:], in_=bacc[:, 0:4]), r=[bacc], w=[idxf])
        k.op("dve", lambda e: e.tensor_copy(out=idx_i[:], in_=idxf[:].rearrange("p (a b) -> p a b", b=2)[:, :, 0]), r=[idxf], w=[idx_i])
        for half in range(2):
            dma_raw("pool", lambda e, half=half: e.indirect_dma_start(out=Ksel[:, half, :], out_offset=None, in_=ck2, in_offset=bass.IndirectOffsetOnAxis(ap=idx_i[:, half:half + 1], axis=0)),
                    r=[idx_i], w=[Ksel])
            dma_raw("pool", lambda e, half=half: e.indirect_dma_start(out=Vsel[:, half, :], out_offset=None, in_=cv2, in_offset=bass.IndirectOffsetOnAxis(ap=idx_i[:, half:half + 1], axis=0)),
                    r=[idx_i], w=[Vsel])
        bkt = k.bank()
        for half in range(2):
            for kvh in range(2):
                jj = half * 2 + kvh
                k.op("pe", lambda e, half=half, kvh=kvh, jj=jj: e.transpose(out=bkt[:, jj * 128:(jj + 1) * 128], in_=Ksel[:, half, kvh * 128:(kvh + 1) * 128], identity=ident_f[:]),
                     r=[Ksel, ident_f], w=[bkt])
        k.op("act", lambda e: e.copy(out=KTs[:].rearrange("p a b -> p (a b)"), in_=bkt[:, :]), r=[bkt], w=[KTs])
        k.op("dve", lambda e: e.tensor_copy(out=qf[:], in_=qbS[:, :, s_i]), r=[qbS], w=[qf])
        k.op("dve", lambda e: e.tensor_copy(out=knf[:], in_=knS[:, :, s_i]), r=[knS], w=[knf])
        blg = k.bank()
        for half in range(2):
            for kvh in range(2):
                jj = half * 2 + kvh
                k.op("pe", lambda e, half=half, kvh=kvh, jj=jj: e.matmul(blg[:, half * 8 + kvh * 4:half * 8 + kvh * 4 + 4], lhsT=KTs[:, jj, :], rhs=qf[:, kvh * 4:(kvh + 1) * 4], start=True, stop=True),
                     r=[KTs, qf], w=[blg])
        bln = k.bank()
        for kvh in range(2):
            k.op("pe", lambda e, kvh=kvh: e.matmul(bln[0:1, kvh * 4:(kvh + 1) * 4], lhsT=knf[:, kvh:kvh + 1], rhs=qf[:, kvh * 4:(kvh + 1) * 4], start=True, stop=True), r=[knf, qf], w=[bln])
        k.op("dve", lambda e: e.tensor_scalar(out=sm[:, 2:4], in0=idxf[:].rearrange("p (a b) -> p a b", b=2)[:, :, 1], scalar1=-1.0, scalar2=float(NPG * 128), op0=ALU.mult, op1=ALU.add), r=[idxf], w=[sm])
        k.op("dve", lambda e: e.tensor_tensor(out=ind[:], in0=sm[:, 2:4].unsqueeze(2).to_broadcast([128, 2, 31]), in1=thrb[:].unsqueeze(1).to_broadcast([128, 2, 31]), op=ALU.is_ge), r=[sm, thrb], w=[ind])
        k.op("dve", lambda e: e.tensor_tensor(out=prod[:], in0=ind[:].unsqueeze(2).to_broadcast([128, 2, 8, 31]), in1=dtmp[:].unsqueeze(1).to_broadcast([128, 2, 8, 31]), op=ALU.mult), r=[ind, dtmp], w=[prod])
        k.op("dve", lambda e: e.tensor_reduce(out=Eg[:], in_=prod[:], axis=AX.X, op=ALU.add), r=[prod], w=[Eg])
        k.op("dve", lambda e: e.tensor_tensor(out=Eg[:], in0=Eg[:], in1=rb0b[:].unsqueeze(1).to_broadcast([128, 2, 8]), op=ALU.add), r=[Eg, rb0b], w=[Eg])
        k.op("dve", lambda e: e.tensor_scalar(out=sm[:, 4:6], in0=jcol[:], scalar1=cum[:, 127:128], scalar2=None, op0=ALU.is_lt), r=[jcol, cum, sm], w=[sm])
        k.op("dve", lambda e: e.tensor_scalar(out=sm[:, 4:6], in0=sm[:, 4:6], scalar1=-1.0, scalar2=30000.0, op0=ALU.add, op1=ALU.mult), r=[sm], w=[sm])
        k.op("dve", lambda e: e.tensor_tensor(out=Eg[:], in0=Eg[:], in1=sm[:, 4:6].unsqueeze(2).to_broadcast([128, 2, 8]), op=ALU.add), r=[Eg, sm], w=[Eg])
        k.op("dve", lambda e: e.tensor_tensor(out=Eg[:].rearrange("p a b -> p (a b)"), in0=Eg[:].rearrange("p a b -> p (a b)"), in1=blg[:, 0:16], op=ALU.add), r=[Eg, blg], w=[Eg])
        k.op("act", lambda e: e.activation(out=Eg[:], in_=Eg[:], func=AF.Exp), r=[Eg], w=[Eg])
        k.op("dve", lambda e: e.tensor_scalar(out=rowp[:, 8:9], in0=sm[0:1, 0:1], scalar1=-1.0, scalar2=30000.0, op0=ALU.add, op1=ALU.mult), r=[sm], w=[rowp])
        k.op("dve", lambda e: e.tensor_tensor(out=rowp[:, 0:8], in0=bln[0:1, 0:8], in1=rb0b[0:1, :], op=ALU.add), r=[bln, rb0b, rowp], w=[rowp])
        k.op("dve", lambda e: e.tensor_scalar(out=rowp[:, 0:8], in0=rowp[:, 0:8], scalar1=rowp[:, 8:9], scalar2=None, op0=ALU.add), r=[rowp], w=[rowp])
        k.op("act", lambda e: e.activation(out=rowp[:, 0:8], in_=rowp[:, 0:8], func=AF.Exp), r=[rowp], w=[rowp])
        k.dma("sp", vnr[:], av[col:col + 1, :], r=["av"], w=[vnr])
        k.op("dve", lambda e: e.tensor_copy(out=vnf[:], in_=vnr[:]), r=[vnr], w=[vnf])
        for kvh in range(2):
            bon = k.bank()
            bod = k.bank()
            for half in range(2):
                k.op("pe", lambda e, kvh=kvh, half=half, bon=bon: e.matmul(bon[0:4, 0:128], lhsT=Eg[:, half, kvh * 4:(kvh + 1) * 4], rhs=Vsel[:, half, kvh * 128:(kvh + 1) * 128], start=(half == 0), stop=False),
                     r=[Eg, Vsel], w=[bon])
            k.op("pe", lambda e, kvh=kvh, bon=bon: e.matmul(bon[0:4, 0:128], lhsT=rowp[0:1, kvh * 4:(kvh + 1) * 4], rhs=vnf[0:1, kvh * 128:(kvh + 1) * 128], start=False, stop=True), r=[rowp, vnf], w=[bon])
            for half in range(2):
                k.op("pe", lambda e, kvh=kvh, half=half, bod=bod: e.matmul(bod[0:4, 0:1], lhsT=Eg[:, half, kvh * 4:(kvh + 1) * 4], rhs=ones4[:, 0:1], start=(half == 0), stop=False), r=[Eg, ones4], w=[bod])
            k.op("pe", lambda e, kvh=kvh, bod=bod: e.matmul(bod[0:4, 0:1], lhsT=rowp[0:1, kvh * 4:(kvh + 1) * 4], rhs=ones4[0:1, 0:1], start=False, stop=True), r=[rowp, ones4], w=[bod])
            k.op("dve", lambda e, kvh=kvh, bod=bod: e.reciprocal(out=osb[:, kvh, 128:129], in_=bod[0:4, 0:1]), r=[bod], w=[osb])
            k.op("dve", lambda e, kvh=kvh, bon=bon: e.tensor_scalar(out=osb[:, kvh, 0:128], in0=bon[0:4, 0:128], scalar1=osb[:, kvh, 128:129], scalar2=None, op0=ALU.mult), r=[bon, osb], w=[osb])
        bot = k.bank()
        for kvh in range(2):
            k.op("pe", lambda e, kvh=kvh: e.transpose(out=bot[:, kvh * 4:(kvh + 1) * 4], in_=osb[:, kvh, 0:128], identity=ident_f[0:4, 0:4]), r=[osb, ident_f], w=[bot])
        k.op("act", lambda e: e.copy(out=mixT[:, 8:16, col], in_=bot[:, 0:8]), r=[bot], w=[(mixT, "sb")])

    for s_i in range(TS):
        attend_seq(s_i)
    k.barrier()
    k.release(m4)
    m5 = k.mark()
    wout = k.alloc("wout", [128, 16, D], BF16)
    for g in range(4):
        k.dma("pool", wout[:, :, g * 512:(g + 1) * 512], w_out[:, g * 512:(g + 1) * 512].rearrange("(k p) n -> p k n", p=128), w=[(wout, g)])
    G1b = k.alloc("G1b", [128, D], F32)
    A2b = k.alloc("A2b", [128, D], F32)
    SH2b = k.alloc("SH2b", [128, D], F32)
    load_mod_bcast(G1b, 2)
    load_mod_bcast(SH2b, 3)
    load_mod_bcast(A2b, 4)
    xt5 = [k.alloc("xt5_0", [128, D], F32)] * 2
    mo = k.alloc("mo", [128, D], F32)
    h2b = k.alloc("h2b", [128, D], BF16)
    jnk5 = h2b
    st5 = [k.alloc(f"st5_{i}", [128, 8], F32) for i in range(2)]
    h2st = [k.alloc(f"h2st{i}", [128, 16, 128], BF16) for i in range(2)]

    def p5_tile(ti):
        c0, n = (ti * 128, 128) if ti < NT else (T, TS)
        x_ = xt5[ti % 2]
        s_ = st5[ti % 2]
        hs_ = h2st[ti % 2]
        G1_, A2_, SH2_ = (G1b, A2b, SH2b)
        if ti == NT:
            k.dma("sp", G1b[0:TS, :], modd[1:5, 2 * D:3 * D], r=["modd"], w=[G1b])
            k.dma("sp", SH2b[0:TS, :], modd[1:5, 3 * D:4 * D], r=["modd"], w=[SH2b])
            k.dma("sp", A2b[0:TS, :], modd[1:5, 4 * D:5 * D], r=["modd"], w=[A2b])
        k.dma("sp", x_[0:n, :], xp[c0:c0 + n, :] if ti < NT else xs.ap(), w=[x_])
        mkeys = [(mixT, ti), (mixT, "b%d" % ti)] if ti < NT else [(mixT, "s%d" % j) for j in range(TS)] + [(mixT, "sb")]
        for nq in range(4):
            bk_ = k.bank()
            for kk in range(16):
                k.op("pe", lambda e, kk=kk, bk_=bk_, nq=nq: e.matmul(bk_[0:n, :], lhsT=mixT[:, kk, c0:c0 + n], rhs=wout[:, kk, nq * 512:(nq + 1) * 512], start=(kk == 0), stop=(kk == 15)),
                     r=mkeys + [(wout, nq)], w=[bk_])
            k.op("act", lambda e, bk_=bk_, nq=nq: e.copy(out=mo[0:n, nq * 512:(nq + 1) * 512], in_=bk_[0:n, :]), r=[bk_], w=[mo])
        k.op("act", lambda e: e.activation(out=jnk5[0:n, :], in_=mo[0:n, :], func=AF.Square, accum_out=s_[0:n, 0:1]), r=[mo], w=[h2b, s_])
        k.op("act", lambda e: e.activation(out=s_[0:n, 1:2], in_=s_[0:n, 0:1], func=AF.Sqrt, scale=1.0 / D, bias=EPS), r=[s_], w=[s_])
        k.op("dve", lambda e: e.reciprocal(out=s_[0:n, 2:3], in_=s_[0:n, 1:2]), r=[s_], w=[s_])
        k.op("dve", lambda e: e.scalar_tensor_tensor(out=mo[0:n, :], in0=mo[0:n, :], scalar=s_[0:n, 2:3], op0=ALU.mult, in1=G1_[0:n, :], op1=ALU.mult), r=[mo, s_, G1_], w=[mo])
        k.op("pool", lambda e: e.tensor_tensor(out=x_[0:n, :], in0=x_[0:n, :], in1=mo[0:n, :], op=ALU.add), r=[x_, mo], w=[x_])
        k.dma("sp", x1d[c0:c0 + n, :], x_[0:n, :], r=[x_], w=["x1d"])
        k.op("act", lambda e: e.activation(out=jnk5[0:n, :], in_=x_[0:n, :], func=AF.Square, accum_out=s_[0:n, 3:4]), r=[x_, s_], w=[h2b, s_])
        k.op("act", lambda e: e.activation(out=s_[0:n, 4:5], in_=s_[0:n, 3:4], func=AF.Sqrt, scale=1.0 / D, bias=EPS), r=[s_], w=[s_])
        k.op("dve", lambda e: e.reciprocal(out=s_[0:n, 5:6], in_=s_[0:n, 4:5]), r=[s_], w=[s_])
        k.op("dve", lambda e: e.scalar_tensor_tensor(out=mo[0:n, :], in0=x_[0:n, :], scalar=s_[0:n, 5:6], op0=ALU.mult, in1=A2_[0:n, :], op1=ALU.mult), r=[x_, s_, A2_, mo], w=[mo])
        k.op("pool", lambda e: e.tensor_tensor(out=h2b[0:n, :], in0=mo[0:n, :], in1=SH2_[0:n, :], op=ALU.add), r=[mo, SH2_], w=[h2b])
        b0 = k.bank()
        b1 = k.bank()
        for kk in range(16):
            bb = b0 if kk < 8 else b1
            k.op("pe", lambda e, kk=kk, bb=bb: e.transpose(out=bb[:].bitcast(BF16)[:, (kk % 8) * 128:(kk % 8) * 128 + n], in_=h2b[0:n, kk * 128:(kk + 1) * 128], identity=ident_b[0:n, 0:n]),
                 r=[h2b, ident_b], w=[bb])
        for half, bb in ((0, b0), (1, b1)):
            k.op("act", lambda e, half=half, bb=bb: e.copy(out=hs_[:, half * 8:(half + 1) * 8, 0:n], in_=bb[:].bitcast(BF16).rearrange("p (a b) -> p a b", a=8)[:, :, 0:n]),
                 r=[bb], w=[hs_])
        k.dma("sp", h2Td[:, :, c0:c0 + n], hs_[:, :, 0:n], r=[hs_], w=["h2Td"])

    for ti in range(NT + 1):
        p5_tile(ti)
    k.barrier()
    k.release(mP)
    if stop_after == "P5":
        return k.finish()

    alloc_wb()
    TB = 512
    NBLK6 = T // TB
    uT = k.alloc("uT", [128, 64, TB], BF16)
    uTs = k.alloc("uTs", [128, 64, TS], BF16)
    h2Tb = k.alloc("h2Tb", [128, 16, TB], BF16)
    h2Ts = k.alloc("h2Ts", [128, 16, TS], BF16)
    w2b = [k.alloc(f"w2b{i}", [128, 4, 512], BF16) for i in range(2)]
    fbuf = [k.alloc(f"fbuf{i}", [128, D], F32) for i in range(4)]
    x1t = [k.alloc("x1t0", [128, D], F32)] * 2
    G2b = k.alloc("G2b", [128, D], F32)
    load_mod_bcast(G2b, 5)
    rl6 = [k.alloc(f"rl6_{i}", [128, TB], BF16) for i in range(2)]
    st6 = [k.alloc(f"st6_{i}", [128, 4], F32) for i in range(2)]
    w2cnt = [0]
    k.dma("sp", h2Ts[:], h2Td[:, :, T:TT], r=["h2Td"], w=[h2Ts])

    def ffn_block(b):
        with_s = (b == NBLK6 - 1)
        k.dma("sp", h2Tb[:], h2Td[:, :, b * TB:(b + 1) * TB], r=["h2Td"], w=[h2Tb])
        def phaseA(g):
            wt = load_w(w_ff1, g * 512, 512)
            for j in range(4):
                ch = g * 4 + j
                bk_ = k.bank()
                for kk in range(16):
                    k.op("pe", lambda e, kk=kk, bk_=bk_, j=j: e.matmul(bk_[:, :], lhsT=wt[:, kk, j * 128:(j + 1) * 128], rhs=h2Tb[:, kk, :], start=(kk == 0), stop=(kk == 15)),
                         r=[wt, h2Tb], w=[bk_])
                r_ = rl6[ch % 2]
                k.op("act", lambda e, bk_=bk_, r_=r_: e.activation(out=r_[:], in_=bk_[:, :], func=AF.Relu), r=[bk_], w=[r_])
                k.op("pool", lambda e, r_=r_, ch=ch: e.tensor_tensor(out=uT[:, ch, :], in0=r_[:], in1=r_[:], op=ALU.mult), r=[r_], w=[(uT, ch)])
                if with_s:
                    bs_ = k.bank()
                    for kk in range(16):
                        k.op("pe", lambda e, kk=kk, bs_=bs_, j=j: e.matmul(bs_[:, 0:TS], lhsT=wt[:, kk, j * 128:(j + 1) * 128], rhs=h2Ts[:, kk, :], start=(kk == 0), stop=(kk == 15)),
                             r=[wt, h2Ts], w=[bs_])
                    k.op("act", lambda e, bs_=bs_, ch=ch: e.activation(out=uTs[:, ch, :], in_=bs_[:, 0:TS], func=AF.Relu), r=[bs_], w=[(uTs, ch)])
                    k.op("dve", lambda e, ch=ch: e.tensor_tensor(out=uTs[:, ch, :], in0=uTs[:, ch, :], in1=uTs[:, ch, :], op=ALU.mult), r=[(uTs, ch)], w=[(uTs, ch)])
        for g in range(16):
            phaseA(g)
        def phaseB(qc):
            accs = [k.bank() for _ in range(4)]
            accS = k.bank() if with_s else None
            k.reserved = {a_.name for a_ in accs} | ({accS.name} if with_s else set())
            for fg in range(16):
                w2 = w2b[w2cnt[0] % 2]
                w2cnt[0] += 1
                k.dma("pool", w2[:], w_ff2[fg * 512:(fg + 1) * 512, qc * 512:(qc + 1) * 512].rearrange("(c p) n -> p c n", p=128), w=[w2])
                for c in range(4):
                    ch = fg * 4 + c
                    first = (fg == 0 and c == 0)
                    last = (fg == 15 and c == 3)
                    for tt in range(4):
                        k.op("pe", lambda e, tt=tt, c=c, ch=ch, first=first, last=last, w2=w2: e.matmul(accs[tt][:, :], lhsT=uT[:, ch, tt * 128:(tt + 1) * 128], rhs=w2[:, c, :], start=first, stop=last),
                             r=[(uT, ch), w2], w=[accs[tt]])
                    if with_s:
                        k.op("pe", lambda e, c=c, ch=ch, first=first, last=last, w2=w2: e.matmul(accS[0:TS, :], lhsT=uTs[:, ch, :], rhs=w2[:, c, :], start=first, stop=last),
                             r=[(uTs, ch), w2], w=[accS])
            for tt in range(4):
                k.op("act", lambda e, tt=tt: e.copy(out=fbuf[tt][:, qc * 512:(qc + 1) * 512], in_=accs[tt][:, :]), r=[accs[tt]], w=[(fbuf[tt], qc)])
            if with_s:
                k.op("act", lambda e: e.copy(out=fs[0:TS, qc * 512:(qc + 1) * 512], in_=accS[0:TS, :]), r=[accS], w=[(fs, qc)])
            k.reserved = set()
        for qc in range(4):
            phaseB(qc)
        def epi(fb, n, x1src, ydst, G2_, i):
            x_ = x1t[i % 2]
            s_ = st6[i % 2]
            k.dma("sp", x_[0:n, :], x1src, r=["x1d"], w=[x_])
            fk = [(fb, q) for q in range(4)]
            k.op("act", lambda e: e.activation(out=jnk6[0:n, :], in_=fb[0:n, :], func=AF.Square, accum_out=s_[0:n, 0:1]), r=fk, w=[jnk6, s_])
            k.op("act", lambda e: e.activation(out=s_[0:n, 1:2], in_=s_[0:n, 0:1], func=AF.Sqrt, scale=1.0 / D, bias=EPS), r=[s_], w=[s_])
            k.op("dve", lambda e: e.reciprocal(out=s_[0:n, 2:3], in_=s_[0:n, 1:2]), r=[s_], w=[s_])
            k.op("dve", lambda e: e.scalar_tensor_tensor(out=fb[0:n, :], in0=fb[0:n, :], scalar=s_[0:n, 2:3], op0=ALU.mult, in1=G2_[0:n, :], op1=ALU.mult), r=fk + [s_, G2_], w=fk)
            k.op("pool", lambda e: e.tensor_tensor(out=x_[0:n, :], in0=x_[0:n, :], in1=fb[0:n, :], op=ALU.add), r=[x_] + fk, w=[x_])
            k.dma("sp", ydst, x_[0:n, :], r=[x_])
        for tt in range(4):
            r0 = b * TB + tt * 128
            epi(fbuf[tt], 128, x1d[r0:r0 + 128, :], y_p[r0:r0 + 128, :], G2b, tt)
        if with_s:
            k.dma("sp", G2b[0:TS, :], modd[1:5, 5 * D:6 * D], r=["modd"], w=[G2b])
            epi(fs, TS, x1d[T:TT, :], y_s.ap(), G2b, 0)

    fs = k.alloc("fs", [TS, D], F32)
    jnk6 = k.alloc("jnk6", [128, D], BF16)
    import os
    for b in range(int(os.environ.get("NBLK", str(NBLK6)))):
        ffn_block(b if "NBLK" not in os.environ else NBLK6 - 1 - b)
    k.barrier()
    return k.finish()


_CACHE = {}


def _core_inputs(i, a):
    f = np.ascontiguousarray
    return {
        "xp": f(a["x_prompt"][i]),
        "xs": f(a["x_sample"][4 * i:4 * i + 4, 0, :]),
        "c5": f(np.concatenate([a["c_prompt"][i:i + 1], a["c_sample"][4 * i:4 * i + 4]], axis=0)),
        "w_ada": f(a["w_ada"][0]),
        "b_ada": f(a["b_ada"][0][None, :]),
        "gvec": f(np.stack([a["pre1_g"][0], a["post1_g"][0], a["pre2_g"][0], a["post2_g"][0]])),
        "w_in": f(a["w_in"][0]),
        "w_out": f(a["w_out"][0]),
        "w_ff1": f(a["w_ff1"][0]),
        "w_ff2": f(a["w_ff2"][0]),
        "conv_w": f(a["conv_w"][0]),
        "st_conv": f(a["state_conv"][0, 4 * i:4 * i + 4].reshape(12, 3072)),
        "hv": f(np.concatenate([a["a_log"][0], a["dt_bias"][0]])[None, :]),
        "ln_gb": f(np.stack([a["idx_knorm_g"][0], a["idx_knorm_b"][0]])),
        "gdn_g": f(a["gdn_norm_g"][0][None, :]),
        "st_ssm": f(a["state_ssm"][0, 4 * i:4 * i + 4]),
        "rel_bias": f(a["rel_bias"]),
        "boh": _boh(),
        "bthr": _bthr(),
        "page_table": f(a["page_table"][4 * i:4 * i + 4]),
        "cache_kidx": a["cache_kidx"][0].reshape(-1, PAGE * 128),
        "cache_k": a["cache_k"][0].reshape(-1, 256),
        "cache_v": a["cache_v"][0].reshape(-1, 256),
    }


def kernel(**inputs):
    n = 8
    nc = build(n_pool=int(inputs["cache_k"].shape[1]))
    in_maps = [_core_inputs(i, inputs) for i in range(n)]
    res = run_bass_kernel_spmd(nc, in_maps, core_ids=list(range(n)))
    R = res.results
    cat = lambda name: np.stack([r[name] for r in R])
    y_p = cat("y_p")
    y_s = np.concatenate([r["y_s"] for r in R])[:, None, :]
    k_p = cat("k_p").reshape(1, 8, T, 2, 128)
    v_p = cat("v_p").reshape(1, 8, T, 2, 128)
    ki_p = cat("ki_p")[None]
    ssm_p = cat("ssm_p")[None]
    conv_p = cat("conv_p")[None]
    k_s = np.concatenate([r["k_s"] for r in R]).reshape(1, 32, 1, 2, 128)
    v_s = np.concatenate([r["v_s"] for r in R]).reshape(1, 32, 1, 2, 128)
    ki_s = np.concatenate([r["ki_s"] for r in R]).reshape(1, 32, 1, 128)
    ssm_s = np.concatenate([r["ssm_s"] for r in R])[None]
    conv_s = np.concatenate([r["conv_s"] for r in R])[None]
    return (y_p, y_s, k_p, v_p, ki_p, ssm_p, conv_p, k_s, v_s, ki_s, ssm_s, conv_s)
```

```python
import math
import numpy as np
import concourse.bass as bass
import concourse.mybir as mybir
from concourse.bass_utils import run_bass_kernel_spmd

F32 = mybir.dt.float32
BF16 = mybir.dt.bfloat16
I32 = mybir.dt.int32
ALU = mybir.AluOpType
AF = mybir.ActivationFunctionType
AX = mybir.AxisListType

ENGS = ("pe", "dve", "act", "pool", "sp")

D = 2048
T = 2048
TS = 4
TT = T + TS
NT = 16
NPROJ = 7840
DFF = 8192
EPS = 1e-6
NPAGES = 128
PAGE = 128
O_CONV, O_A, O_B, O_Z, O_QB, O_KB, O_VB, O_QI, O_WI, O_KI = 0, 3072, 3080, 3088, 4112, 5136, 5392, 5648, 7696, 7712


def _dsize(dt):
    return {F32: 4, BF16: 2, I32: 4}[dt]


class Buf:
    def __init__(self, name, ap):
        self.name = name
        self.ap = ap

    def __getitem__(self, key):
        return self.ap[key]


class KB:
    def __init__(self, n_dma_sems=(24, 8, 52)):
        self.nc = bass.Bass("TRN2", target_bir_lowering=False)
        nc = self.nc
        self.ops = {e: [] for e in ENGS}
        self._ctx = []
        self.psem = {}
        self.cnt = {e: 0 for e in ENGS}
        for e in ENGS:
            self.psem[e] = self._enter(nc.semaphore("p_" + e))
        self.dsem = {}
        self.dcnt = {}
        self.drr = {}
        for q, n in zip(("sp", "act", "pool"), n_dma_sems):
            self.dsem[q] = [self._enter(nc.semaphore(f"d_{q}{i}")) for i in range(n)]
            self.dcnt[q] = [0] * n
            self.drr[q] = 0
        self.known = {e: {} for e in ENGS}
        self.state = {}
        self.semobj = {}
        for e in ENGS:
            self.semobj[("p", e)] = self.psem[e]
        for q in self.dsem:
            for i, s in enumerate(self.dsem[q]):
                self.semobj[("d", q, i)] = s
        self.n_ops = 0
        self.arena = None
        self.aoff = 0
        self.awords = 0
        self.nbank = 0
        self.reserved = set()

    def _enter(self, cm):
        v = cm.__enter__()
        self._ctx.append(cm)
        return v

    def init_arena(self, words):
        self.arena = self._enter(self.nc.sbuf_tensor("arena", [128, words], F32))
        self.awords = words
        self.aoff = 0

    def alloc(self, name, shape, dt=F32):
        p = shape[0]
        n = int(np.prod(shape[1:]))
        words = (n * _dsize(dt) + 3) // 4
        words = (words + 7) // 8 * 8
        assert self.aoff + words <= self.awords, f"arena overflow at {name}: {self.aoff + words} > {self.awords}"
        ap = self.arena[0:p, self.aoff:self.aoff + words]
        self.aoff += words
        if dt != F32:
            ap = ap.bitcast(dt)
        ap = ap[:, 0:n]
        if len(shape) == 3:
            ap = ap.rearrange("p (a b) -> p a b", a=shape[1])
        elif len(shape) == 4:
            ap = ap.rearrange("p (a b c) -> p a b c", a=shape[1], b=shape[2])
        return Buf(name, ap)

    def mark(self):
        return self.aoff

    def release(self, m):
        self.aoff = m

    def psum_init(self):
        self.pbanks = []
        for i in range(4):
            t = self._enter(self.nc.psum_tensor(f"pp{i}", [128, 1024], F32))
            self.pbanks.append(Buf(f"bank{2 * i}", t[:, 0:512]))
            self.pbanks.append(Buf(f"bank{2 * i + 1}", t[:, 512:1024]))
        self.pdbl = [self._dbl(i) for i in range(4)]

    def _dbl(self, i):
        return None

    def bank(self):
        while True:
            b = self.pbanks[self.nbank % 8]
            self.nbank += 1
            if b.name not in self.reserved:
                return b

    @staticmethod
    def _key(x):
        if isinstance(x, tuple):
            return (KB._key(x[0]),) + tuple(x[1:])
        if isinstance(x, str):
            return x
        return x.name

    def _collect(self, eng, r, w):
        need = {}
        own = ("p", eng)

        def add(tok):
            if tok is None:
                return
            sk, v = tok
            if sk == own and eng == "pe":
                return
            if need.get(sk, 0) < v:
                need[sk] = v

        for x in r:
            st = self.state.get(self._key(x))
            if st:
                add(st[0])
        for x in w:
            st = self.state.get(self._key(x))
            if st:
                add(st[0])
                for t in st[1]:
                    add(t)
        waits = []
        kn = self.known[eng]
        for sk, v in need.items():
            if kn.get(sk, 0) >= v:
                continue
            kn[sk] = v
            waits.append((sk, v))
        return waits

    def _update(self, tok, r, w):
        for x in w:
            self.state[self._key(x)] = [tok, []]
        for x in r:
            kk = self._key(x)
            st = self.state.get(kk)
            if st is None:
                st = self.state[kk] = [None, []]
            st[1].append(tok)
            if len(st[1]) > 24:
                best = {}
                for sk, v in st[1]:
                    if best.get(sk, 0) < v:
                        best[sk] = v
                st[1] = list(best.items())

    def op(self, eng, fn, r=(), w=()):
        waits = self._collect(eng, r, w)
        self.cnt[eng] += 1
        tok = (("p", eng), self.cnt[eng])
        self.ops[eng].append((waits, fn, (("p", eng), 1)))
        self._update(tok, r, w)
        self.n_ops += 1
        return tok

    def dma(self, q, out, in_, r=(), w=(), slow=False):
        if slow:
            fn = lambda e: e.dma_start(out=out, in_=in_, allow_slow_non_contiguous=True)
        else:
            fn = lambda e: e.dma_start(out=out, in_=in_)
        i = self.drr[q]
        self.drr[q] = (i + 1) % len(self.dsem[q])
        sk = ("d", q, i)
        waits = self._collect(q, r, w)
        prev = self.dcnt[q][i]
        kn = self.known[q]
        if prev > 0 and kn.get(sk, 0) < prev:
            kn[sk] = prev
            waits.append((sk, prev))
        self.dcnt[q][i] = prev + 16
        tok = (sk, prev + 16)
        self.ops[q].append((waits, fn, (sk, 16)))
        self._update(tok, r, w)
        self.n_ops += 1
        return tok

    def barrier(self):
        toks = [(("p", e), self.cnt[e]) for e in ENGS if self.cnt[e] > 0]
        for q in self.dsem:
            for i, c in enumerate(self.dcnt[q]):
                if c > 0:
                    toks.append((("d", q, i), c))
        for e in ENGS:
            waits = []
            kn = self.known[e]
            for sk, v in toks:
                if sk == ("p", e):
                    continue
                if kn.get(sk, 0) < v:
                    kn[sk] = v
                    waits.append((sk, v))
            if waits:
                self.ops[e].append((waits, None, None))

    def check_deadlock(self):
        sem = {}
        ptr = {e: 0 for e in ENGS}
        progress = True
        while progress:
            progress = False
            for e in ENGS:
                lst = self.ops[e]
                while ptr[e] < len(lst):
                    waits, fn, inc = lst[ptr[e]]
                    if all(sem.get(sk, 0) >= v for sk, v in waits):
                        if inc is not None:
                            sem[inc[0]] = sem.get(inc[0], 0) + inc[1]
                        ptr[e] += 1
                        progress = True
                    else:
                        break
        stuck = {e: (ptr[e], len(self.ops[e])) for e in ENGS if ptr[e] < len(self.ops[e])}
        if stuck:
            for e in stuck:
                waits, fn, inc = self.ops[e][ptr[e]]
                print("STUCK", e, ptr[e], [(sk, v, sem.get(sk, 0)) for sk, v in waits])
            raise RuntimeError(f"deadlock in sync graph: {stuck}")

    def finish(self):
        self.barrier()
        self.check_deadlock()
        nc = self.nc
        ops = self.ops
        semobj = self.semobj

        def run(e, lst):
            for waits, fn, inc in lst:
                for sk, v in waits:
                    e.wait_ge(semobj[sk], v)
                if fn is not None:
                    ins = fn(e)
                    ins.then_inc(semobj[inc[0]], inc[1])

        with nc.Block() as block:
            @block.tensor
            def _(e):
                run(e, ops["pe"])

            @block.vector
            def _(e):
                run(e, ops["dve"])

            @block.scalar
            def _(e):
                run(e, ops["act"])

            @block.gpsimd
            def _(e):
                run(e, ops["pool"])

            @block.sync
            def _(e):
                run(e, ops["sp"])

        for cm in reversed(self._ctx):
            cm.__exit__(None, None, None)
        self._ctx = []
        return nc


def _t5_bucket_table():
    n = np.arange(0, 256, dtype=np.int32)
    max_exact = 16
    nf = np.maximum(n, 1).astype(np.float32)
    large = max_exact + (np.log(nf / np.float32(max_exact)) / np.float32(math.log(128 / max_exact))
                         * np.float32(32 - max_exact)).astype(np.int32)
    large = np.minimum(large, 31)
    return np.where(n < max_exact, n, large)


def _boh():
    tab = _t5_bucket_table()
    oh = np.zeros((32, 384), np.float32)
    for j in range(384):
        dist = min(max(j - 127, 0), 255)
        oh[tab[dist], j] = 1.0
    return oh


def _bthr():
    tab = _t5_bucket_table()
    thr = np.zeros((1, 31), np.float32)
    for kk in range(1, 32):
        nz = np.nonzero(tab >= kk)[0]
        thr[0, kk - 1] = float(nz[0]) if len(nz) else 1e9
    return thr


def build(stop_after=None, n_pool=5120, skip_p4s=False):
    k = KB()
    nc = k.nc

    def din(name, shape, dt=F32):
        return nc.dram_tensor(name, list(shape), dt, kind="ExternalInput")

    def dout(name, shape, dt=F32):
        return nc.dram_tensor(name, list(shape), dt, kind="ExternalOutput")

    xp = din("xp", [T, D])
    xs = din("xs", [TS, D])
    c5 = din("c5", [5, D])
    w_ada = din("w_ada", [D, 6 * D])
    b_ada = din("b_ada", [1, 6 * D])
    gvec = din("gvec", [4, D])
    w_in = din("w_in", [D, NPROJ])
    w_out = din("w_out", [D, D])
    w_ff1 = din("w_ff1", [D, DFF])
    w_ff2 = din("w_ff2", [DFF, D])
    conv_w = din("conv_w", [4, 3072])
    st_conv = din("st_conv", [TS * 3, 3072])
    hv = din("hv", [1, 16])
    ln_gb = din("ln_gb", [2, 128])
    gdn_g = din("gdn_g", [1, 128])
    st_ssm = din("st_ssm", [TS, 8, 128, 128])
    rel_bias = din("rel_bias", [32, 8])
    boh = din("boh", [32, 384])
    bthr = din("bthr", [1, 31])
    page_table = din("page_table", [TS, NPAGES], I32)
    cache_kidx = din("cache_kidx", [n_pool, PAGE * 128])
    cache_k = din("cache_k", [n_pool * PAGE, 256])
    cache_v = din("cache_v", [n_pool * PAGE, 256])

    y_p = dout("y_p", [T, D])
    y_s = dout("y_s", [TS, D])
    k_p = dout("k_p", [T, 256])
    v_p = dout("v_p", [T, 256])
    ki_p = dout("ki_p", [T, 128])
    ssm_p = dout("ssm_p", [8, 128, 128])
    conv_p = dout("conv_p", [3, 3072])
    k_s = dout("k_s", [TS, 256])
    v_s = dout("v_s", [TS, 256])
    ki_s = dout("ki_s", [TS, 128])
    ssm_s = dout("ssm_s", [TS, 8, 128, 128])
    conv_s = dout("conv_s", [TS, 3, 3072])

    modd = nc.dram_tensor("modd", [5, 6 * D], F32)
    gq = nc.dram_tensor("gq", [8, 128, TT], BF16)
    gk = nc.dram_tensor("gk", [8, 128, TT], BF16)
    gv = nc.dram_tensor("gv", [8, 128, TT], BF16)
    gz = nc.dram_tensor("gz", [TT, 1024], BF16)
    gab = nc.dram_tensor("gab", [TT, 16], F32)
    aq = nc.dram_tensor("aq", [8, 128, TT], BF16)
    akT = nc.dram_tensor("akT", [2, 128, TT], BF16)
    av = nc.dram_tensor("av", [TT, 256], BF16)
    iq = nc.dram_tensor("iq", [16, 128, TT], BF16)
    iw = nc.dram_tensor("iw", [TT, 16], F32)
    ikTd = nc.dram_tensor("ikTd", [128, TT], BF16)
    biasd = nc.dram_tensor("biasd", [8, 384], F32)
    rbTd = nc.dram_tensor("rbTd", [8, 32], F32)
    w1s = nc.dram_tensor("w1s", [16, 128, 16, 512], BF16)
    w2s = nc.dram_tensor("w2s", [4, 8, 128, 8, 512], BF16)
    x1d = nc.dram_tensor("x1d", [TT, D], F32)
    h2Td = nc.dram_tensor("h2Td", [128, 16, TT], BF16)

    k.init_arena(47 * 1024)
    k.psum_init()

    ident_f = k.alloc("ident_f", [128, 128], F32)
    ident_b = k.alloc("ident_b", [128, 128], BF16)
    ones_b = k.alloc("ones_b", [128, 128], BF16)
    k.op("pool", lambda e: e.memset(ident_f[:], 1.0), w=[ident_f])
    k.op("pool", lambda e: e.affine_select(out=ident_f[:], in_=ident_f[:], pattern=[[-1, 128]],
                                           compare_op=ALU.is_equal, fill=0.0, base=0, channel_multiplier=1),
         r=[ident_f], w=[ident_f])
    k.op("pool", lambda e: e.tensor_copy(out=ident_b[:], in_=ident_f[:]), r=[ident_f], w=[ident_b])
    k.op("pool", lambda e: e.memset(ones_b[:], 1.0), w=[ones_b])

    wb = []
    wcnt = [0]

    def alloc_wb():
        wb.clear()
        wb.extend(k.alloc(f"wb{i}", [128, 16, 512], BF16) for i in range(2))

    def load_w(src_dram, c0, ncols):
        b = wb[wcnt[0] % 2]
        wcnt[0] += 1
        k.dma("pool", b[:, :, 0:ncols], src_dram[:, c0:c0 + ncols].rearrange("(k p) n -> p k n", p=128), w=[b])
        return b

    m0 = k.mark()
    alloc_wb()
    c80 = k.alloc("c80", [80, 128], F32)
    cT = k.alloc("cT", [128, 16, 5], BF16)
    mod = k.alloc("mod", [5, 6 * D], F32)
    gv5 = k.alloc("gv5", [5, 4, D], F32)
    k.dma("sp", c80[:], c5.ap().rearrange("r (k p) -> (r k) p", p=128), w=[c80])
    k.dma("sp", mod[:], b_ada.ap().to_broadcast([5, 6 * D]), w=[mod])
    k.dma("sp", gv5[:].rearrange("p a b -> p (a b)"), gvec.ap().rearrange("a b -> (a b)").unsqueeze(0).to_broadcast([5, 4 * D]), w=[gv5])
    bk = k.bank()
    k.op("pe", lambda e: e.transpose(out=bk[:, 0:80], in_=c80[:], identity=ident_f[0:80, 0:80]), r=[c80, ident_f], w=[bk])
    k.op("act", lambda e: e.activation(out=cT[:], in_=bk[:, 0:80].rearrange("p (r k) -> p k r", r=5), func=AF.Silu), r=[bk], w=[cT])
    for n in range(24):
        wt = load_w(w_ada, n * 512, 512)
        bk = k.bank()
        for kk in range(16):
            k.op("pe", lambda e, kk=kk, wt=wt, bk=bk: e.matmul(bk[0:5, :], lhsT=cT[:, kk, :], rhs=wt[:, kk, :], start=(kk == 0), stop=(kk == 15)),
                 r=[cT, wt], w=[bk])
        k.op("dve", lambda e, n=n, bk=bk: e.tensor_tensor(out=mod[:, n * 512:(n + 1) * 512], in0=bk[0:5, :], in1=mod[:, n * 512:(n + 1) * 512], op=ALU.add),
             r=[bk, mod], w=[mod])
    for (sc, gi) in ((1, 0), (4, 2)):
        k.op("dve", lambda e, sc=sc, gi=gi: e.scalar_tensor_tensor(out=mod[:, sc * D:(sc + 1) * D], in0=mod[:, sc * D:(sc + 1) * D], scalar=1.0, op0=ALU.add,
                                                                    in1=gv5[:, gi, :], op1=ALU.mult), r=[mod, gv5], w=[mod])
    for (g, gi) in ((2, 1), (5, 3)):
        k.op("dve", lambda e, g=g, gi=gi: e.tensor_tensor(out=mod[:, g * D:(g + 1) * D], in0=mod[:, g * D:(g + 1) * D], in1=gv5[:, gi, :], op=ALU.mult),
             r=[mod, gv5], w=[mod])
    k.dma("sp", modd.ap(), mod[:], r=[mod], w=["modd"])
    k.barrier()
    k.release(m0)
    if stop_after == "P0":
        return k.finish()

    def load_mod_bcast(buf, idx):
        k.dma("sp", buf[:], modd[0:1, idx * D:(idx + 1) * D].to_broadcast([128, D]), r=["modd"], w=[buf])

    def load_mod_rows(buf, idx):
        k.dma("sp", buf[:], modd[1:5, idx * D:(idx + 1) * D], r=["modd"], w=[buf])

    mP = k.mark()
    hT = k.alloc("hT", [128, 16, TT], BF16)
    ikT = k.alloc("ikT", [128, TT], BF16)
    m1 = k.mark()
    A1 = k.alloc("A1", [128, D], F32)
    SH1 = k.alloc("SH1", [128, D], F32)
    A1s = k.alloc("A1s", [TS, D], F32)
    SH1s = k.alloc("SH1s", [TS, D], F32)
    load_mod_bcast(SH1, 0)
    load_mod_bcast(A1, 1)
    load_mod_rows(SH1s, 0)
    load_mod_rows(A1s, 1)
    xt = [k.alloc(f"xt{i}", [128, D], F32) for i in range(2)]
    hb = [k.alloc(f"hb{i}", [128, D], BF16) for i in range(2)]
    junk = k.alloc("junk", [128, D], BF16)
    st1 = [k.alloc(f"st1_{i}", [128, 4], F32) for i in range(2)]

    def norm_mod(i, np_, src_ap, A, SH, col0, ncol):
        x_ = xt[i % 2]
        h_ = hb[i % 2]
        s_ = st1[i % 2]
        k.dma("sp", x_[0:np_, :], src_ap, w=[x_])
        k.op("act", lambda e: e.activation(out=junk[0:np_, :], in_=x_[0:np_, :], func=AF.Square, accum_out=s_[0:np_, 0:1]), r=[x_], w=[junk, s_])
        k.op("act", lambda e: e.activation(out=s_[0:np_, 1:2], in_=s_[0:np_, 0:1], func=AF.Sqrt, scale=1.0 / D, bias=EPS), r=[s_], w=[s_])
        k.op("dve", lambda e: e.reciprocal(out=s_[0:np_, 2:3], in_=s_[0:np_, 1:2]), r=[s_], w=[s_])
        k.op("dve", lambda e: e.scalar_tensor_tensor(out=x_[0:np_, :], in0=x_[0:np_, :], scalar=s_[0:np_, 2:3], op0=ALU.mult, in1=A[0:np_, :], op1=ALU.mult),
             r=[x_, s_, A], w=[x_])
        k.op("pool", lambda e: e.tensor_tensor(out=h_[0:np_, :], in0=x_[0:np_, :], in1=SH[0:np_, :], op=ALU.add), r=[x_, SH], w=[h_])
        b0 = k.bank()
        b1 = k.bank()
        for kk in range(16):
            bb = b0 if kk < 8 else b1
            k.op("pe", lambda e, kk=kk, bb=bb: e.transpose(out=bb[:].bitcast(BF16)[:, (kk % 8) * 128:(kk % 8) * 128 + np_],
                                                           in_=h_[0:np_, kk * 128:(kk + 1) * 128], identity=ident_b[0:np_, 0:np_]),
                 r=[h_, ident_b], w=[bb])
        for half, bb in ((0, b0), (1, b1)):
            k.op("act", lambda e, half=half, bb=bb: e.copy(out=hT[:, half * 8:(half + 1) * 8, col0:col0 + ncol],
                                                          in_=bb[:].bitcast(BF16).rearrange("p (a b) -> p a b", a=8)[:, :, 0:ncol]),
                 r=[bb], w=[(hT, col0)])

    for i in range(NT):
        norm_mod(i, 128, xp[i * 128:(i + 1) * 128, :], A1, SH1, i * 128, 128)
    norm_mod(NT, TS, xs.ap(), A1s, SH1s, T, TS)
    k.barrier()
    k.release(m1)
    if stop_after == "P1":
        dbg = dout("dbg_hT", [128, 16 * TT], BF16)
        k.dma("sp", dbg.ap(), hT[:].rearrange("p a b -> p (a b)"), r=[hT])
        return k.finish()

    hT_keys = [(hT, i * 128) for i in range(NT)] + [(hT, T)]

    m2 = k.mark()
    alloc_wb()
    cw = k.alloc("cw", [128, 4, 24], F32)
    stc = k.alloc("stc", [128, TS * 3, 24], F32)
    cwl = k.alloc("cwl", [96, 128], F32)
    stl = k.alloc("stl", [96, 3, 128], F32)
    k.dma("sp", cwl[:], conv_w.ap().rearrange("j (ch c) -> (j ch) c", c=128), w=[cwl])
    k.dma("sp", stl[:], st_conv.ap().rearrange("r (ch c) -> (r ch) c", c=128).rearrange("(g p) c -> p g c", p=96), w=[stl])
    bk = k.bank()
    k.op("pe", lambda e, bk=bk: e.transpose(out=bk[:, 0:96], in_=cwl[:], identity=ident_f[0:96, 0:96]), r=[cwl, ident_f], w=[bk])
    k.op("act", lambda e, bk=bk: e.copy(out=cw[:].rearrange("p a b -> p (a b)"), in_=bk[:, 0:96]), r=[bk], w=[cw])
    bk = k.bank()
    for g in range(3):
        k.op("pe", lambda e, g=g, bk=bk: e.transpose(out=bk[:, g * 96:(g + 1) * 96], in_=stl[:, g, :], identity=ident_f[0:96, 0:96]),
             r=[stl, ident_f], w=[bk])
    k.op("act", lambda e, bk=bk: e.copy(out=stc[:].rearrange("p a b -> p (a b)"), in_=bk[:, 0:288]), r=[bk], w=[stc])
    k.dma("sp", conv_s.ap()[:, 0:2, :], st_conv.ap().rearrange("(s j) c -> s j c", j=3)[:, 1:3, :])

    cin = [k.alloc(f"cin{i}", [128, 3 + T], F32) for i in range(2)]
    for cb in cin:
        k.op("pool", lambda e, cb=cb: e.memset(cb[:, 0:3], 0.0), w=[cb])
    acc = [k.alloc(f"acc{i}", [128, TT], F32) for i in range(2)]
    cins = [k.alloc(f"cins{i}", [128, TS, 4], F32) for i in range(2)]
    tmp4 = [k.alloc(f"tmp4{i}", [128, TS, 4], F32) for i in range(2)]
    sqb = [k.alloc(f"sqb{i}", [128, TT], BF16) for i in range(2)]
    rsb = [k.alloc(f"rsb{i}", [128, TT], F32) for i in range(2)]
    ob = [k.alloc(f"ob{i}", [128, TT], BF16) for i in range(2)]
    obc = [0]

    def fm_matmuls(wt, wc0, tg, bk):
        c0, n = (tg * 512, 512) if tg < 4 else (T, TS)
        keys = hT_keys[tg * 4:(tg + 1) * 4] if tg < 4 else [hT_keys[16]]
        for kk in range(16):
            k.op("pe", lambda e, kk=kk: e.matmul(bk[:, 0:n], lhsT=wt[:, kk, wc0:wc0 + 128], rhs=hT[:, kk, c0:c0 + n], start=(kk == 0), stop=(kk == 15)),
                 r=[wt] + keys, w=[bk])

    def next_ob():
        b = ob[obc[0] % 2]
        obc[0] += 1
        return b

    chunk_i = [0]

    def conv_chunk(wt, wc0, ch):
        ci = chunk_i[0]
        chunk_i[0] += 1
        cb = cin[ci % 2]
        ac = acc[ci % 2]
        cs = cins[ci % 2]
        t4 = tmp4[ci % 2]
        for tg in range(4):
            bk = k.bank()
            fm_matmuls(wt, wc0, tg, bk)
            k.op("act", lambda e, bk=bk, tg=tg: e.copy(out=cb[:, 3 + tg * 512:3 + (tg + 1) * 512], in_=bk[:, 0:512]), r=[bk], w=[cb])
        bk = k.bank()
        fm_matmuls(wt, wc0, 4, bk)
        k.op("dve", lambda e: e.tensor_copy(out=cs[:, :, 0:3], in_=stc[:, :, ch].rearrange("p (s j) -> p s j", j=3)), r=[stc], w=[cs])
        k.op("dve", lambda e, bk=bk: e.tensor_copy(out=cs[:, :, 3:4], in_=bk[:, 0:TS].unsqueeze(2)), r=[bk, cs], w=[cs])
        k.dma("sp", conv_p.ap()[:, ch * 128:(ch + 1) * 128].rearrange("r c -> c r"), cb[:, T:T + 3], r=[cb], slow=True)
        k.dma("sp", conv_s.ap()[:, 2, ch * 128:(ch + 1) * 128].rearrange("s c -> c s"), cs[:, :, 3], r=[cs], slow=True)
        k.op("dve", lambda e: e.tensor_scalar(out=ac[:, 0:T], in0=cb[:, 0:T], scalar1=cw[:, 0, ch:ch + 1], scalar2=None, op0=ALU.mult), r=[cb, cw], w=[ac])
        for j in range(1, 4):
            k.op("dve", lambda e, j=j: e.scalar_tensor_tensor(out=ac[:, 0:T], in0=cb[:, j:j + T], scalar=cw[:, j, ch:ch + 1], op0=ALU.mult, in1=ac[:, 0:T], op1=ALU.add),
                 r=[cb, cw, ac], w=[ac])
        k.op("dve", lambda e: e.tensor_tensor(out=t4[:], in0=cs[:], in1=cw[:, :, ch].unsqueeze(1).to_broadcast([128, TS, 4]), op=ALU.mult), r=[cs, cw], w=[t4])
        k.op("dve", lambda e: e.tensor_reduce(out=ac[:, T:TT], in_=t4[:], axis=AX.X, op=ALU.add), r=[t4, ac], w=[ac])
        o = next_ob()
        if ch >= 16:
            k.op("act", lambda e: e.activation(out=o[:], in_=ac[:], func=AF.Silu), r=[ac], w=[o])
            k.dma("sp", gv[ch - 16], o[:], r=[o], w=["gv"])
            return
        sq = sqb[ci % 2]
        rs = rsb[ci % 2]
        k.op("act", lambda e: e.activation(out=ac[:], in_=ac[:], func=AF.Silu), r=[ac], w=[ac])
        k.op("act", lambda e: e.activation(out=sq[:], in_=ac[:], func=AF.Square), r=[ac], w=[sq])
        for tg in range(5):
            c0, n = (tg * 512, 512) if tg < 4 else (T, TS)
            bk = k.bank()
            k.op("pe", lambda e, bk=bk, c0=c0, n=n: e.matmul(bk[:, 0:n], lhsT=ones_b[:], rhs=sq[:, c0:c0 + n], start=True, stop=True), r=[sq, ones_b], w=[bk])
            k.op("act", lambda e, bk=bk, c0=c0, n=n: e.activation(out=rs[:, c0:c0 + n], in_=bk[:, 0:n], func=AF.Sqrt, bias=EPS, scale=1.0), r=[bk], w=[rs])
        k.op("dve", lambda e: e.reciprocal(out=rs[:], in_=rs[:]), r=[rs], w=[rs])
        scl = 128.0 ** -0.5 if ch < 8 else 1.0
        k.op("dve", lambda e: e.scalar_tensor_tensor(out=o[:], in0=ac[:], scalar=scl, op0=ALU.mult, in1=rs[:], op1=ALU.mult), r=[ac, rs], w=[o])
        dst = gq[ch] if ch < 8 else gk[ch - 8]
        k.dma("sp", dst, o[:], r=[o], w=["gqk"])

    def plain_chunk(wt, wc0, dst, scale):
        o = next_ob()
        for tg in range(5):
            c0, n = (tg * 512, 512) if tg < 4 else (T, TS)
            bk = k.bank()
            fm_matmuls(wt, wc0, tg, bk)
            k.op("act", lambda e, bk=bk, c0=c0, n=n: e.activation(out=o[:, c0:c0 + n], in_=bk[:, 0:n], func=AF.Copy, scale=scale), r=[bk], w=[o])
        k.dma("sp", dst, o[:], r=[o], w=["plain"])

    for g in range(6):
        wt = load_w(w_in, O_CONV + g * 512, 512)
        for j in range(4):
            conv_chunk(wt, j * 128, g * 4 + j)
    if stop_after == "P2a":
        k.barrier()
        return k.finish()
    for g in range(2):
        wt = load_w(w_in, O_QB + g * 512, 512)
        for j in range(4):
            plain_chunk(wt, j * 128, aq[g * 4 + j], 128.0 ** -0.5)
    for g in range(4):
        wt = load_w(w_in, O_QI + g * 512, 512)
        for j in range(4):
            plain_chunk(wt, j * 128, iq[g * 4 + j], 1.0)
    wt_kv = load_w(w_in, O_KB, 512)
    for j in range(2):
        plain_chunk(wt_kv, j * 128, akT[j], 1.0)

    if stop_after == "P2b":
        k.barrier()
        return k.finish()
    stg_f = [k.alloc(f"stgf{i}", [128, 512], F32) for i in range(2)]
    stg_b = [k.alloc(f"stgb{i}", [128, 512], BF16) for i in range(2)]
    lnw = [k.alloc(f"lnw{i}", [128, 8], F32) for i in range(2)]
    kib = [k.alloc(f"kib{i}", [128, 128], BF16) for i in range(2)]
    lng = k.alloc("lng", [128, 128], F32)
    lnb = k.alloc("lnb", [128, 128], F32)
    k.dma("sp", lng[:], ln_gb[0:1, :].to_broadcast([128, 128]), w=[lng])
    k.dma("sp", lnb[:], ln_gb[1:2, :].to_broadcast([128, 128]), w=[lnb])
    tcnt = [0]

    def tm_tile(wt, ncols, ti, epilogue):
        c0, n = (ti * 128, 128) if ti < NT else (T, TS)
        bk = k.bank()
        for kk in range(16):
            k.op("pe", lambda e, kk=kk: e.matmul(bk[0:n, 0:ncols], lhsT=hT[:, kk, c0:c0 + n], rhs=wt[:, kk, 0:ncols], start=(kk == 0), stop=(kk == 15)),
                 r=[wt, hT_keys[ti]], w=[bk])
        i = tcnt[0]
        tcnt[0] += 1
        epilogue(bk, c0, n, i)

    def ep_kv(bk, c0, n, i):
        sf = stg_f[i % 2]
        sb_ = stg_b[i % 2]
        import os
        dbg = int(os.environ.get("DBG", "0"))
        k.op("act", lambda e: e.copy(out=sf[0:n, :], in_=bk[0:n, :]), r=[bk], w=[sf])
        if dbg != 3:
            k.op("pool", lambda e: e.tensor_copy(out=sb_[0:n, 0:256], in_=sf[0:n, 256:512]), r=[sf], w=[sb_])
        if dbg == 1:
            pass
        elif c0 < T:
            k.dma("sp", k_p[c0:c0 + n, :], sf[0:n, 0:256], r=[sf])
            k.dma("sp", v_p[c0:c0 + n, :], sf[0:n, 256:512], r=[sf])
        else:
            k.dma("sp", k_s.ap(), sf[0:n, 0:256], r=[sf])
            k.dma("sp", v_s.ap(), sf[0:n, 256:512], r=[sf])
        if dbg not in (2, 3):
            k.dma("sp", av[c0:c0 + n, :], sb_[0:n, 0:256], r=[sb_], w=["av"])

    import os
    _d = int(os.environ.get("DBG", "0"))
    for ti in range(0 if _d == 4 else (NT if _d == 5 else NT + 1)):
        tm_tile(wt_kv, 512, ti, ep_kv)

    if stop_after == "P2c":
        k.barrier()
        return k.finish()

    def ep_z(half):
        def ep(bk, c0, n, i):
            sb_ = stg_b[i % 2]
            k.op("act", lambda e: e.copy(out=sb_[0:n, :], in_=bk[0:n, :]), r=[bk], w=[sb_])
            k.dma("sp", gz[c0:c0 + n, half * 512:(half + 1) * 512], sb_[0:n, :], r=[sb_], w=["gz"])
        return ep

    for half in range(2):
        wt = load_w(w_in, O_Z + half * 512, 512)
        for ti in range(NT + 1):
            tm_tile(wt, 512, ti, ep_z(half))

    if stop_after == "P2d":
        k.barrier()
        return k.finish()

    def ep_ab(bk, c0, n, i):
        sf = stg_f[i % 2]
        k.op("act", lambda e: e.copy(out=sf[0:n, 0:16], in_=bk[0:n, 0:16]), r=[bk], w=[sf])
        k.dma("sp", gab[c0:c0 + n, :], sf[0:n, 0:16], r=[sf], w=["gab"])

    wt = load_w(w_in, O_A, 16)
    for ti in range(NT + 1):
        tm_tile(wt, 16, ti, ep_ab)

    if stop_after == "P2e":
        k.barrier()
        return k.finish()

    def ep_wk(bk, c0, n, i):
        sf = stg_f[i % 2]
        s_ = lnw[i % 2]
        kb_ = kib[i % 2]
        k.op("act", lambda e: e.activation(out=sf[0:n, 0:16], in_=bk[0:n, 0:16], func=AF.Copy, scale=0.25), r=[bk], w=[sf])
        k.dma("sp", iw[c0:c0 + n, :], sf[0:n, 0:16], r=[sf], w=["iw"])
        kf = sf[0:n, 128:256]
        k.op("act", lambda e: e.activation(out=kf, in_=bk[0:n, 16:144], func=AF.Copy, accum_out=s_[0:n, 0:1]), r=[bk, sf], w=[sf, s_])
        k.op("dve", lambda e: e.tensor_scalar(out=s_[0:n, 1:2], in0=s_[0:n, 0:1], scalar1=-1.0 / 128, scalar2=None, op0=ALU.mult), r=[s_], w=[s_])
        k.op("dve", lambda e: e.tensor_scalar(out=kf, in0=kf, scalar1=s_[0:n, 1:2], scalar2=None, op0=ALU.add), r=[sf, s_], w=[sf])
        k.op("act", lambda e: e.activation(out=sf[0:n, 256:384], in_=kf, func=AF.Square, accum_out=s_[0:n, 2:3]), r=[sf, s_], w=[sf, s_])
        k.op("act", lambda e: e.activation(out=s_[0:n, 3:4], in_=s_[0:n, 2:3], func=AF.Sqrt, scale=1.0 / 128, bias=EPS), r=[s_], w=[s_])
        k.op("dve", lambda e: e.reciprocal(out=s_[0:n, 4:5], in_=s_[0:n, 3:4]), r=[s_], w=[s_])
        k.op("dve", lambda e: e.scalar_tensor_tensor(out=kf, in0=kf, scalar=s_[0:n, 4:5], op0=ALU.mult, in1=lng[0:n, :], op1=ALU.mult), r=[sf, s_, lng], w=[sf])
        k.op("dve", lambda e: e.tensor_tensor(out=kf, in0=kf, in1=lnb[0:n, :], op=ALU.add), r=[sf, lnb], w=[sf])
        k.op("dve", lambda e: e.tensor_copy(out=kb_[0:n, :], in_=kf), r=[sf], w=[kb_])
        if c0 < T:
            k.dma("sp", ki_p[c0:c0 + n, :], kf, r=[sf])
        else:
            k.dma("sp", ki_s.ap(), kf, r=[sf])
        b2 = k.bank()
        k.op("pe", lambda e: e.transpose(out=b2[:].bitcast(BF16)[:, 0:n], in_=kb_[0:n, :], identity=ident_b[0:n, 0:n]), r=[kb_, ident_b], w=[b2])
        k.op("act", lambda e: e.copy(out=ikT[:, c0:c0 + n], in_=b2[:].bitcast(BF16)[:, 0:n]), r=[b2], w=[(ikT, c0)])

    wt = load_w(w_in, O_WI, 144)
    for ti in range(NT + 1):
        tm_tile(wt, 144, ti, ep_wk)
    k.dma("sp", ikTd.ap(), ikT[:], r=[(ikT, c) for c in range(0, TT, 128)], w=["ikTd"])
    k.barrier()
    k.release(mP)
    if stop_after == "P2":
        return k.finish()
    conv_jobs = []
    for g in range(16):
        conv_jobs.append((w1s[g], w_ff1[:, g * 512:(g + 1) * 512].rearrange("(k p) c -> p k c", p=128), ("w1s", g)))
    for qc in range(4):
        for fgg in range(8):
            conv_jobs.append((w2s[qc, fgg], w_ff2[fgg * 1024:(fgg + 1) * 1024, qc * 512:(qc + 1) * 512].rearrange("(c p) n -> p c n", p=128), ("w2s", qc, fgg)))

    def issue_conv(nj):
        for _ in range(nj):
            if conv_jobs:
                o_, i_, key_ = conv_jobs.pop(0)
                k.dma("pool", o_, i_, w=[key_])
    mixT = k.alloc("mixT", [128, 16, TT], BF16)
    m3 = k.mark()
    HG = 4
    HW = HG * 128
    ones_f = k.alloc("ones_f", [128, 128], F32)
    TRI = k.alloc("TRI", [128, 128], F32)
    POSM = k.alloc("POSM", [128, HG, 128], F32)
    OFFD = k.alloc("OFFD", [128, HG, 128], F32)
    hvb = k.alloc("hvb", [128, 16], F32)
    gnb = k.alloc("gnb", [128, 128], F32)
    k.op("pool", lambda e: e.memset(ones_f[:], 1.0), w=[ones_f])
    k.op("pool", lambda e: e.memset(TRI[:], 1.0), w=[TRI])
    k.op("pool", lambda e: e.affine_select(out=TRI[:], in_=TRI[:], pattern=[[1, 128]], compare_op=ALU.is_ge, fill=0.0, base=0, channel_multiplier=-1),
         r=[TRI], w=[TRI])
    k.op("pool", lambda e: e.memset(POSM[:], 0.0), w=[POSM])
    k.op("pool", lambda e: e.affine_select(out=POSM[:], in_=POSM[:], pattern=[[0, HG], [-1, 128]], compare_op=ALU.is_ge, fill=30000.0, base=0, channel_multiplier=1),
         r=[POSM], w=[POSM])
    k.op("pool", lambda e: e.memset(OFFD[:], 1.0), w=[OFFD])
    k.op("pool", lambda e: e.affine_select(out=OFFD[:], in_=OFFD[:], pattern=[[0, HG], [-1, 128]], compare_op=ALU.not_equal, fill=0.0, base=0, channel_multiplier=1),
         r=[OFFD], w=[OFFD])
    k.dma("sp", hvb[:], hv.ap().to_broadcast([128, 16]), w=[hvb])
    k.dma("sp", gnb[:], gdn_g.ap().to_broadcast([128, 128]), w=[gnb])

    gabt = k.alloc("gabt", [128, NT, 16], F32)
    k.dma("sp", gabt[:], gab[0:T, :].rearrange("(t p) c -> p t c", p=128), r=["gab"], w=[gabt], slow=True)
    nA = k.alloc("nA", [128, 8], F32)
    G_ = k.alloc("G_", [128, NT, 8], F32)
    Bt = k.alloc("Bt", [128, NT, 8], F32)
    NB = k.alloc("NB", [128, NT, 8], F32)
    GC = k.alloc("GC", [128, NT, 8], F32)
    GL = k.alloc("GL", [128, NT, 8], F32)
    EG = k.alloc("EG", [128, NT, 8], F32)
    EGL = k.alloc("EGL", [128, NT, 8], F32)
    EKD = k.alloc("EKD", [128, NT, 8], F32)
    BEG = k.alloc("BEG", [128, NT, 8], F32)
    k.op("act", lambda e: e.activation(out=nA[:], in_=hvb[:, 0:8], func=AF.Exp), r=[hvb], w=[nA])
    k.op("dve", lambda e: e.tensor_scalar(out=nA[:], in0=nA[:], scalar1=-1.0, scalar2=None, op0=ALU.mult), r=[nA], w=[nA])
    k.op("dve", lambda e: e.tensor_tensor(out=G_[:], in0=gabt[:, :, 0:8], in1=hvb[:, 8:16].unsqueeze(1).to_broadcast([128, NT, 8]), op=ALU.add), r=[gabt, hvb], w=[G_])
    k.op("act", lambda e: e.activation(out=G_[:], in_=G_[:], func=AF.Exp), r=[G_], w=[G_])
    k.op("act", lambda e: e.activation(out=G_[:], in_=G_[:], func=AF.Ln, bias=1.0, scale=1.0), r=[G_], w=[G_])
    k.op("dve", lambda e: e.tensor_tensor(out=G_[:], in0=G_[:], in1=nA[:].unsqueeze(1).to_broadcast([128, NT, 8]), op=ALU.mult), r=[G_, nA], w=[G_])
    k.op("act", lambda e: e.activation(out=Bt[:], in_=gabt[:, :, 8:16], func=AF.Sigmoid), r=[gabt], w=[Bt])
    k.op("dve", lambda e: e.tensor_scalar(out=NB[:], in0=Bt[:], scalar1=-1.0, scalar2=None, op0=ALU.mult), r=[Bt], w=[NB])
    bA = k.bank()
    bB = k.bank()
    for t in range(NT):
        k.op("pe", lambda e, t=t: e.matmul(bA[:, t * 8:(t + 1) * 8], lhsT=TRI[:], rhs=G_[:, t, :], start=True, stop=True), r=[TRI, G_], w=[bA])
        k.op("pe", lambda e, t=t: e.matmul(bB[:, t * 8:(t + 1) * 8], lhsT=ones_f[:], rhs=G_[:, t, :], start=True, stop=True), r=[ones_f, G_], w=[bB])
    k.op("act", lambda e: e.copy(out=GC[:].rearrange("p a b -> p (a b)"), in_=bA[:, 0:NT * 8]), r=[bA], w=[GC])
    k.op("act", lambda e: e.copy(out=GL[:].rearrange("p a b -> p (a b)"), in_=bB[:, 0:NT * 8]), r=[bB], w=[GL])
    k.op("act", lambda e: e.activation(out=EG[:], in_=GC[:], func=AF.Exp), r=[GC], w=[EG])
    k.op("act", lambda e: e.activation(out=EGL[:], in_=GL[:], func=AF.Exp), r=[GL], w=[EGL])
    k.op("dve", lambda e: e.tensor_tensor(out=EKD[:], in0=GL[:], in1=GC[:], op=ALU.subtract), r=[GL, GC], w=[EKD])
    k.op("act", lambda e: e.activation(out=EKD[:], in_=EKD[:], func=AF.Exp), r=[EKD], w=[EKD])
    k.op("dve", lambda e: e.tensor_tensor(out=BEG[:], in0=Bt[:], in1=EG[:], op=ALU.mult), r=[Bt, EG], w=[BEG])

    if stop_after == "P3a":
        k.barrier()
        return k.finish()
    m3g = k.mark()
    qT = k.alloc("qT", [128, HG, TT], BF16)
    kT = k.alloc("kT", [128, HG, TT], BF16)
    vT = k.alloc("vT", [128, HG, TT], BF16)
    NSLOT = 2

    def mk_slot(j):
        d = {}
        for nm, dt in (("Dg", F32), ("egT", BF16), ("decay", F32), ("M0", F32), ("M1", F32), ("MT0", F32), ("MT1", F32),
                       ("PT", F32), ("PTb", BF16), ("vbeta", BF16), ("kbg", BF16), ("kdec", BF16), ("u", F32), ("wT", BF16),
                       ("intra", BF16), ("intraT", BF16), ("qg", BF16)):
            d[nm] = k.alloc(f"{nm}_{j}", [128, HG, 128], dt)
        return d

    slots = [mk_slot(j) for j in range(NSLOT)]
    S_ = k.alloc("S_", [128, HG, 128], F32)
    Sb = k.alloc("Sb", [128, HG, 128], BF16)
    vnew = k.alloc("vnew", [128, HG, 128], BF16)
    o_ = k.alloc("o_", [128, HG, 128], F32)
    sq_ = k.alloc("sq_", [128, HG, 128], F32)
    zt = [k.alloc(f"zt{i}", [128, HG, 128], BF16) for i in range(2)]
    zs = k.alloc("zs", [128, HG, 128], F32)
    oa = k.alloc("oa", [128, HG, 128], BF16)
    sst = k.alloc("sst", [128, 3 * HG], F32)

    def fl(b):
        return b[:].rearrange("p a b -> p (a b)")

    def bc_tok(src_ap):
        return src_ap.unsqueeze(2).to_broadcast([128, HG, 128])

    def stageA(g, i, sl):
        d = slots[sl]
        h0 = g * HG
        tsl = slice(i * 128, (i + 1) * 128)
        Dg, egT, decay, PT, PTb = d["Dg"], d["egT"], d["decay"], d["PT"], d["PTb"]
        Ms = [d["M0"], d["M1"]]
        MTs = [d["MT0"], d["MT1"]]
        k.op("dve", lambda e: e.tensor_tensor(out=Dg[:], in0=ident_f[:].unsqueeze(1).to_broadcast([128, HG, 128]), in1=bc_tok(GC[:, i, h0:h0 + HG]), op=ALU.mult),
             r=[ident_f, GC], w=[Dg])
        b1 = k.bank()
        k.op("pe", lambda e: e.matmul(b1[:, 0:HW], lhsT=ones_f[:], rhs=fl(Dg), start=True, stop=True), r=[ones_f, Dg], w=[b1])
        k.op("act", lambda e: e.activation(out=fl(egT), in_=b1[:, 0:HW], func=AF.Exp), r=[b1], w=[egT])
        b2 = k.bank()
        k.op("pe", lambda e: e.matmul(b2[:, 0:HW], lhsT=ones_f[:], rhs=fl(Dg), start=True, stop=False), r=[ones_f, Dg], w=[b2])
        k.op("pe", lambda e: e.matmul(b2[:, 0:HW], lhsT=ident_f[:], rhs=fl(POSM), start=False, stop=True), r=[ident_f, POSM], w=[b2])
        for h in range(HG):
            k.op("act", lambda e, h=h: e.activation(out=decay[:, h, :], in_=b2[:, h * 128:(h + 1) * 128], func=AF.Exp, scale=-1.0, bias=GC[:, i, h0 + h:h0 + h + 1]),
                 r=[b2, GC], w=[decay])
        k.op("pool", lambda e: e.tensor_tensor(out=d["qg"][:], in0=qT[:, :, tsl], in1=egT[:], op=ALU.mult), r=[qT, egT], w=[d["qg"]])
        b3 = k.bank()
        b4 = k.bank()
        for h in range(HG):
            k.op("pe", lambda e, h=h: e.matmul(b3[:, h * 128:(h + 1) * 128], lhsT=kT[:, h, tsl], rhs=kT[:, h, tsl], start=True, stop=True), r=[kT], w=[b3])
        for h in range(HG):
            k.op("pe", lambda e, h=h: e.matmul(b4[:, h * 128:(h + 1) * 128], lhsT=qT[:, h, tsl], rhs=kT[:, h, tsl], start=True, stop=True), r=[qT, kT], w=[b4])
        k.op("dve", lambda e: e.tensor_tensor(out=fl(d["intra"]), in0=b4[:, 0:HW], in1=fl(decay), op=ALU.mult), r=[b4, decay], w=[d["intra"]])
        k.op("pool", lambda e: e.tensor_tensor(out=Dg[:], in0=decay[:], in1=OFFD[:], op=ALU.mult), r=[decay, OFFD], w=[Dg])
        for h in range(HG):
            k.op("dve", lambda e, h=h: e.scalar_tensor_tensor(out=Ms[0][:, h, :], in0=b3[:, h * 128:(h + 1) * 128], scalar=NB[:, i, h0 + h:h0 + h + 1], op0=ALU.mult,
                                                                in1=Dg[:, h, :], op1=ALU.mult), r=[b3, NB, Dg], w=[Ms[0]])
        yield
        b5 = k.bank()
        b5i = k.bank()
        b5b = b5i[:].bitcast(BF16)
        for h in range(HG):
            k.op("pe", lambda e, h=h: e.transpose(out=b5[:, h * 128:(h + 1) * 128], in_=Ms[0][:, h, :], identity=ident_f[:]), r=[Ms[0], ident_f], w=[b5])
        for h in range(HG):
            k.op("pe", lambda e, h=h: e.transpose(out=b5b[:, h * 128:(h + 1) * 128], in_=d["intra"][:, h, :], identity=ident_b[:]), r=[d["intra"], ident_b], w=[b5i])
        k.op("act", lambda e: e.copy(out=fl(MTs[0]), in_=b5[:, 0:HW]), r=[b5], w=[MTs[0]])
        k.op("act", lambda e: e.copy(out=fl(d["intraT"]), in_=b5b[:, 0:HW]), r=[b5i], w=[d["intraT"]])
        k.op("dve", lambda e: e.tensor_tensor(out=PT[:], in0=MTs[0][:], in1=ident_f[:].unsqueeze(1).to_broadcast([128, HG, 128]), op=ALU.add), r=[MTs[0], ident_f], w=[PT])
        yield
        cur = 0
        for lvl in range(1, 7):
            nx = 1 - cur
            last = (lvl == 6)
            b6 = k.bank()
            for h in range(HG):
                k.op("pe", lambda e, h=h, cur=cur, b6=b6: e.matmul(b6[:, h * 128:(h + 1) * 128], lhsT=MTs[cur][:, h, :], rhs=Ms[cur][:, h, :], start=True, stop=True),
                     r=[MTs[cur], Ms[cur]], w=[b6])
            if not last:
                b7 = k.bank()
                for h in range(HG):
                    k.op("pe", lambda e, h=h, cur=cur, b7=b7: e.matmul(b7[:, h * 128:(h + 1) * 128], lhsT=Ms[cur][:, h, :], rhs=MTs[cur][:, h, :], start=True, stop=True),
                         r=[MTs[cur], Ms[cur]], w=[b7])
            k.op("act", lambda e, nx=nx, b6=b6: e.copy(out=fl(Ms[nx]), in_=b6[:, 0:HW]), r=[b6], w=[Ms[nx]])
            if not last:
                k.op("act", lambda e, nx=nx, b7=b7: e.copy(out=fl(MTs[nx]), in_=b7[:, 0:HW]), r=[b7], w=[MTs[nx]])
            b8 = k.bank()
            for h in range(HG):
                k.op("pe", lambda e, h=h, nx=nx, b8=b8: e.matmul(b8[:, h * 128:(h + 1) * 128], lhsT=Ms[nx][:, h, :], rhs=PT[:, h, :], start=True, stop=True),
                     r=[Ms[nx], PT], w=[b8])
            k.op("dve", lambda e, b8=b8: e.tensor_tensor(out=fl(PT), in0=b8[:, 0:HW], in1=fl(PT), op=ALU.add), r=[b8, PT], w=[PT])
            if last:
                k.op("pool", lambda e: e.tensor_copy(out=PTb[:], in_=PT[:]), r=[PT], w=[PTb])
            cur = nx
            yield
        b9 = k.bank()
        b9b = b9[:].bitcast(BF16)
        for h in range(HG):
            k.op("pe", lambda e, h=h: e.transpose(out=b9b[:, h * 128:(h + 1) * 128], in_=vT[:, h, tsl], identity=ident_b[:]), r=[vT, ident_b], w=[b9])
        for h in range(HG):
            k.op("pe", lambda e, h=h: e.transpose(out=b9b[:, HW + h * 128:HW + (h + 1) * 128], in_=kT[:, h, tsl], identity=ident_b[:]), r=[kT, ident_b], w=[b9])
        vps = b9b[:, 0:HW].rearrange("p (a b) -> p a b", a=HG)
        kps = b9b[:, HW:2 * HW].rearrange("p (a b) -> p a b", a=HG)
        k.op("dve", lambda e: e.tensor_tensor(out=d["vbeta"][:], in0=vps, in1=bc_tok(Bt[:, i, h0:h0 + HG]), op=ALU.mult), r=[b9, Bt], w=[d["vbeta"]])
        k.op("dve", lambda e: e.tensor_tensor(out=d["kbg"][:], in0=kps, in1=bc_tok(BEG[:, i, h0:h0 + HG]), op=ALU.mult), r=[b9, BEG], w=[d["kbg"]])
        k.op("dve", lambda e: e.tensor_tensor(out=d["kdec"][:], in0=kps, in1=bc_tok(EKD[:, i, h0:h0 + HG]), op=ALU.mult), r=[b9, EKD], w=[d["kdec"]])
        b10 = k.bank()
        b11 = k.bank()
        for h in range(HG):
            k.op("pe", lambda e, h=h: e.matmul(b10[:, h * 128:(h + 1) * 128], lhsT=PTb[:, h, :], rhs=d["vbeta"][:, h, :], start=True, stop=True), r=[PTb, d["vbeta"]], w=[b10])
        for h in range(HG):
            k.op("pe", lambda e, h=h: e.matmul(b11[:, h * 128:(h + 1) * 128], lhsT=d["kbg"][:, h, :], rhs=PTb[:, h, :], start=True, stop=True), r=[PTb, d["kbg"]], w=[b11])
        k.op("act", lambda e: e.copy(out=fl(d["u"]), in_=b10[:, 0:HW]), r=[b10], w=[d["u"]])
        k.op("act", lambda e: e.copy(out=fl(d["wT"]), in_=b11[:, 0:HW]), r=[b11], w=[d["wT"]])
        yield

    def scan_step(g, i, sl):
        d = slots[sl]
        h0 = g * HG
        tsl = slice(i * 128, (i + 1) * 128)
        z_ = zt[i % 2]
        k.dma("sp", fl(z_), gz[i * 128:(i + 1) * 128, h0 * 128:(h0 + HG) * 128], r=["gz"], w=[z_])
        bx = k.bank()
        for h in range(HG):
            k.op("pe", lambda e, h=h: e.matmul(bx[:, h * 128:(h + 1) * 128], lhsT=d["wT"][:, h, :], rhs=Sb[:, h, :], start=True, stop=True), r=[d["wT"], Sb], w=[bx])
        k.op("dve", lambda e: e.tensor_tensor(out=fl(vnew), in0=fl(d["u"]), in1=bx[:, 0:HW], op=ALU.subtract), r=[d["u"], bx], w=[vnew])
        bo = k.bank()
        for h in range(HG):
            k.op("pe", lambda e, h=h: e.matmul(bo[:, h * 128:(h + 1) * 128], lhsT=d["qg"][:, h, :], rhs=Sb[:, h, :], start=True, stop=False), r=[d["qg"], Sb], w=[bo])
            k.op("pe", lambda e, h=h: e.matmul(bo[:, h * 128:(h + 1) * 128], lhsT=d["intraT"][:, h, :], rhs=vnew[:, h, :], start=False, stop=True), r=[d["intraT"], vnew], w=[bo])
        bz = k.bank()
        for h in range(HG):
            k.op("pe", lambda e, h=h: e.matmul(bz[:, h * 128:(h + 1) * 128], lhsT=d["kdec"][:, h, :], rhs=vnew[:, h, :], start=True, stop=True), r=[d["kdec"], vnew], w=[bz])
        k.op("dve", lambda e: e.tensor_tensor(out=S_[:], in0=S_[:], in1=bc_tok(EGL[:, i, h0:h0 + HG]), op=ALU.mult), r=[S_, EGL], w=[S_])
        k.op("dve", lambda e: e.tensor_tensor(out=fl(S_), in0=fl(S_), in1=bz[:, 0:HW], op=ALU.add), r=[S_, bz], w=[S_])
        k.op("act", lambda e: e.copy(out=Sb[:], in_=S_[:]), r=[S_], w=[Sb])
        k.op("act", lambda e: e.copy(out=fl(o_), in_=bo[:, 0:HW]), r=[bo], w=[o_])
        k.op("pool", lambda e: e.tensor_tensor(out=sq_[:], in0=o_[:], in1=o_[:], op=ALU.mult), r=[o_], w=[sq_])
        k.op("dve", lambda e: e.tensor_reduce(out=sst[:, 0:HG], in_=sq_[:], axis=AX.X, op=ALU.add), r=[sq_], w=[sst])
        k.op("act", lambda e: e.activation(out=sst[:, HG:2 * HG], in_=sst[:, 0:HG], func=AF.Sqrt, scale=1.0 / 128, bias=EPS), r=[sst], w=[sst])
        k.op("dve", lambda e: e.reciprocal(out=sst[:, 2 * HG:3 * HG], in_=sst[:, HG:2 * HG]), r=[sst], w=[sst])
        k.op("act", lambda e: e.activation(out=zs[:], in_=z_[:], func=AF.Silu), r=[z_], w=[zs])
        k.op("dve", lambda e: e.tensor_tensor(out=o_[:], in0=o_[:], in1=bc_tok(sst[:, 2 * HG:3 * HG]), op=ALU.mult), r=[o_, sst], w=[o_])
        k.op("pool", lambda e: e.tensor_tensor(out=o_[:], in0=o_[:], in1=gnb[:].unsqueeze(1).to_broadcast([128, HG, 128]), op=ALU.mult), r=[o_, gnb], w=[o_])
        k.op("pool", lambda e: e.tensor_tensor(out=oa[:], in0=o_[:], in1=zs[:], op=ALU.mult), r=[o_, zs], w=[oa])
        bt = k.bank()
        btb = bt[:].bitcast(BF16)
        for h in range(HG):
            k.op("pe", lambda e, h=h: e.transpose(out=btb[:, h * 128:(h + 1) * 128], in_=oa[:, h, :], identity=ident_b[:]), r=[oa, ident_b], w=[bt])
        k.op("act", lambda e: e.copy(out=mixT[:, h0:h0 + HG, tsl], in_=btb[:, 0:HW].rearrange("p (a b) -> p a b", a=HG)), r=[bt], w=[(mixT, i)])

    for g in range(8 // HG):
        h0 = g * HG
        for nm, src, buf in (("q", gq, qT), ("k", gk, kT), ("v", gv, vT)):
            k.dma("sp", buf[:], src[h0:h0 + HG].rearrange("h d t -> d h t"), r=["gqk", "gv"], w=[buf])
        k.op("pool", lambda e: e.memset(S_[:], 0.0), w=[S_])
        k.op("pool", lambda e: e.memset(Sb[:], 0.0), w=[Sb])
        if stop_after == "P3d":
            for _ in stageA(0, 0, 0):
                pass
            for _ in stageA(0, 1, 1):
                pass
            scan_step(0, 0, 0)
            scan_step(0, 1, 1)
            d = slots[0]
            names = ["decay", "M0", "PT", "u", "wT", "intraT", "qg", "kdec", "vbeta", "kbg", "egT"]
            tmpfs = [sq_, zs]
            for ii, nm in enumerate(names):
                dd = dout("dbg_" + nm, [128, HW], F32)
                tmpf = tmpfs[ii % 2]
                k.op("dve", lambda e, nm=nm, tmpf=tmpf: e.tensor_copy(out=fl(tmpf), in_=fl(d[nm])), r=[d[nm]], w=[tmpf])
                k.dma("sp", dd.ap(), fl(tmpf), r=[tmpf])
            for nm, b in (("S", S_), ("o", o_), ("GC", GC), ("G", G_), ("Bt", Bt), ("EKD", EKD), ("EGL", EGL)):
                dd = dout("dbg_" + nm, [128, int(np.prod(b.ap.shape[1:]))], F32)
                k.dma("sp", dd.ap(), b[:].rearrange("p a b -> p (a b)"), r=[b])
            k.barrier()
            return k.finish()
        import os
        _lim = int(os.environ.get("YLIM", "100"))
        for i0 in range(0, NT, NSLOT):
            gens = [stageA(g, i0 + j, j) for j in range(NSLOT)]
            if stop_after == "P3b":
                for _ in range(_lim):
                    for gen in gens:
                        next(gen, None)
                k.barrier()
                return k.finish()
            alive = True
            while alive:
                alive = False
                for gen in gens:
                    try:
                        next(gen)
                        alive = True
                    except StopIteration:
                        pass
            for j in range(NSLOT):
                scan_step(g, i0 + j, j)
            issue_conv(3)
        k.dma("sp", ssm_p.ap()[h0:h0 + HG].rearrange("h a b -> a h b"), S_[:], r=[S_])
    if stop_after == "P3":
        k.barrier()
        return k.finish()
    issue_conv(100)
    k.barrier()
    k.release(m3g)
    S0 = k.alloc("S0", [128, 8, 128], F32)
    qc = k.alloc("qc", [128, 8], BF16)
    kc = k.alloc("kc", [128, 8], BF16)
    vc = k.alloc("vc", [128, 8], BF16)
    qcf = k.alloc("qcf", [128, 8], F32)
    kcf = k.alloc("kcf", [128, 8], F32)
    gabr = k.alloc("gabr", [1, 16], F32)
    zr = k.alloc("zr", [1, 1024], BF16)
    zrs = k.alloc("zrs", [1, 8, 128], F32)
    rw = k.alloc("rw", [1, 64], F32)
    t1 = k.alloc("t1", [1, 8, 128], F32)
    orow = k.alloc("orow", [1, 8, 128], F32)
    sqr = k.alloc("sqr", [1, 8, 128], F32)
    oar = k.alloc("oar", [1, 1024], BF16)
    abs_ = k.alloc("abs_", [128, 8], F32)

    def bc_row(ap8):
        return ap8.unsqueeze(2).to_broadcast([1, 8, 128])

    def sample_gdn(s_i):
        col = T + s_i
        k.dma("sp", S0[:], st_ssm.ap()[s_i].rearrange("h a b -> a h b"), w=[S0])
        k.dma("sp", qc[:], gq.ap()[:, :, col].rearrange("h d -> d h"), r=["gqk"], w=[qc], slow=True)
        k.dma("sp", kc[:], gk.ap()[:, :, col].rearrange("h d -> d h"), r=["gqk"], w=[kc], slow=True)
        k.dma("sp", vc[:], gv.ap()[:, :, col].rearrange("h d -> d h"), r=["gv"], w=[vc], slow=True)
        k.dma("sp", gabr[:], gab[col:col + 1, :], r=["gab"], w=[gabr])
        k.dma("sp", zr[:], gz[col:col + 1, :], r=["gz"], w=[zr])
        k.op("dve", lambda e: e.tensor_copy(out=qcf[:], in_=qc[:]), r=[qc], w=[qcf])
        k.op("dve", lambda e: e.tensor_copy(out=kcf[:], in_=kc[:]), r=[kc], w=[kcf])
        k.op("dve", lambda e: e.tensor_tensor(out=rw[:, 0:8], in0=gabr[:, 0:8], in1=hvb[0:1, 8:16], op=ALU.add), r=[gabr, hvb], w=[rw])
        k.op("act", lambda e: e.activation(out=rw[:, 0:8], in_=rw[:, 0:8], func=AF.Exp), r=[rw], w=[rw])
        k.op("act", lambda e: e.activation(out=rw[:, 0:8], in_=rw[:, 0:8], func=AF.Ln, bias=1.0, scale=1.0), r=[rw], w=[rw])
        k.op("dve", lambda e: e.tensor_tensor(out=rw[:, 0:8], in0=rw[:, 0:8], in1=nA[0:1, :], op=ALU.mult), r=[rw, nA], w=[rw])
        k.op("act", lambda e: e.activation(out=rw[:, 8:16], in_=rw[:, 0:8], func=AF.Exp), r=[rw], w=[rw])
        k.op("act", lambda e: e.activation(out=rw[:, 16:24], in_=gabr[:, 8:16], func=AF.Sigmoid), r=[gabr, rw], w=[rw])
        ba = k.bank()
        bb_ = k.bank()
        for h in range(8):
            bk_ = ba if h < 4 else bb_
            k.op("pe", lambda e, h=h, bk_=bk_: e.matmul(bk_[0:1, (h % 4) * 128:(h % 4 + 1) * 128], lhsT=kcf[:, h:h + 1], rhs=S0[:, h, :], start=True, stop=True),
                 r=[kcf, S0], w=[bk_])
        bv = k.bank()
        bvb = bv[:].bitcast(BF16)
        for h in range(8):
            k.op("pe", lambda e, h=h: e.transpose(out=bvb[0:1, h * 128:(h + 1) * 128], in_=vc[:, h:h + 1], identity=ident_b[:]), r=[vc, ident_b], w=[bv])
        k.op("dve", lambda e: e.tensor_tensor(out=t1[:, 0:4, :], in0=ba[0:1, :].rearrange("p (a b) -> p a b", a=4), in1=bc_row(rw[:, 8:16])[:, 0:4, :], op=ALU.mult),
             r=[ba, rw], w=[t1])
        k.op("dve", lambda e: e.tensor_tensor(out=t1[:, 4:8, :], in0=bb_[0:1, :].rearrange("p (a b) -> p a b", a=4), in1=bc_row(rw[:, 8:16])[:, 4:8, :], op=ALU.mult),
             r=[bb_, rw, t1], w=[t1])
        k.op("dve", lambda e: e.tensor_tensor(out=t1[:], in0=bvb[0:1, 0:1024].rearrange("p (a b) -> p a b", a=8), in1=t1[:], op=ALU.subtract), r=[bv, t1], w=[t1])
        k.op("dve", lambda e: e.tensor_tensor(out=t1[:], in0=t1[:], in1=bc_row(rw[:, 16:24]), op=ALU.mult), r=[t1, rw], w=[t1])
        bd0 = k.bank()
        bd1 = k.bank()
        k.op("pe", lambda e: e.matmul(bd0[:, :], lhsT=ones_f[0:1, :], rhs=t1[:].rearrange("p a b -> p (a b)")[:, 0:512], start=True, stop=True), r=[ones_f, t1], w=[bd0])
        k.op("pe", lambda e: e.matmul(bd1[:, :], lhsT=ones_f[0:1, :], rhs=t1[:].rearrange("p a b -> p (a b)")[:, 512:1024], start=True, stop=True), r=[ones_f, t1], w=[bd1])
        bab = k.bank()
        k.op("pe", lambda e: e.matmul(bab[:, 0:8], lhsT=ones_f[0:1, :], rhs=rw[:, 8:16], start=True, stop=True), r=[ones_f, rw], w=[bab])
        k.op("act", lambda e: e.copy(out=abs_[:], in_=bab[:, 0:8]), r=[bab], w=[abs_])
        k.op("dve", lambda e: e.tensor_tensor(out=S0[:], in0=S0[:], in1=abs_[:].unsqueeze(2).to_broadcast([128, 8, 128]), op=ALU.mult), r=[S0, abs_], w=[S0])
        for h in range(8):
            bd = bd0 if h < 4 else bd1
            k.op("dve", lambda e, h=h, bd=bd: e.scalar_tensor_tensor(out=S0[:, h, :], in0=bd[:, (h % 4) * 128:(h % 4 + 1) * 128], scalar=kcf[:, h:h + 1], op0=ALU.mult,
                                                                      in1=S0[:, h, :], op1=ALU.add), r=[bd, kcf, S0], w=[S0])
        k.dma("sp", ssm_s.ap()[s_i].rearrange("h a b -> a h b"), S0[:], r=[S0])
        bo0 = k.bank()
        bo1 = k.bank()
        for h in range(8):
            bk_ = bo0 if h < 4 else bo1
            k.op("pe", lambda e, h=h, bk_=bk_: e.matmul(bk_[0:1, (h % 4) * 128:(h % 4 + 1) * 128], lhsT=qcf[:, h:h + 1], rhs=S0[:, h, :], start=True, stop=True),
                 r=[qcf, S0], w=[bk_])
        k.op("act", lambda e: e.copy(out=orow[:, 0:4, :], in_=bo0[0:1, :].rearrange("p (a b) -> p a b", a=4)), r=[bo0], w=[orow])
        k.op("act", lambda e: e.copy(out=orow[:, 4:8, :], in_=bo1[0:1, :].rearrange("p (a b) -> p a b", a=4)), r=[bo1, orow], w=[orow])
        k.op("dve", lambda e: e.tensor_tensor(out=sqr[:], in0=orow[:], in1=orow[:], op=ALU.mult), r=[orow], w=[sqr])
        k.op("dve", lambda e: e.tensor_reduce(out=rw[:, 24:32], in_=sqr[:], axis=AX.X, op=ALU.add), r=[sqr, rw], w=[rw])
        k.op("act", lambda e: e.activation(out=rw[:, 32:40], in_=rw[:, 24:32], func=AF.Sqrt, scale=1.0 / 128, bias=EPS), r=[rw], w=[rw])
        k.op("dve", lambda e: e.reciprocal(out=rw[:, 40:48], in_=rw[:, 32:40]), r=[rw], w=[rw])
        k.op("act", lambda e: e.activation(out=zrs[:].rearrange("p a b -> p (a b)"), in_=zr[:], func=AF.Silu), r=[zr], w=[zrs])
        k.op("dve", lambda e: e.tensor_tensor(out=orow[:], in0=orow[:], in1=bc_row(rw[:, 40:48]), op=ALU.mult), r=[orow, rw], w=[orow])
        k.op("dve", lambda e: e.tensor_tensor(out=orow[:], in0=orow[:], in1=gnb[0:1, :].unsqueeze(1).to_broadcast([1, 8, 128]), op=ALU.mult), r=[orow, gnb], w=[orow])
        k.op("dve", lambda e: e.tensor_tensor(out=oar[:].rearrange("p (a b) -> p a b", a=8), in0=orow[:], in1=zrs[:], op=ALU.mult), r=[orow, zrs], w=[oar])
        bt_ = k.bank()
        for h in range(8):
            k.op("pe", lambda e, h=h: e.matmul(bt_[:, h:h + 1], lhsT=oar[0:1, h * 128:(h + 1) * 128], rhs=ones_b[0:1, 0:1], start=True, stop=True), r=[oar, ones_b], w=[bt_])
        k.op("act", lambda e: e.copy(out=mixT[:, 0:8, col], in_=bt_[:, 0:8]), r=[bt_], w=[(mixT, "s%d" % s_i)])
    for s_i in range(TS):
        sample_gdn(s_i)
    k.barrier()
    k.release(m3)
    if stop_after == "P3S":
        return k.finish()
    m4 = k.mark()
    NEG = -30000.0
    NIT = 18
    kTb = k.alloc("kTb", [128, 2, TT], BF16)
    vtok = k.alloc("vtok", [128, NT, 256], BF16)
    ikT4 = k.alloc("ikT4", [128, TT], BF16)
    k.dma("sp", kTb[:], akT.ap().rearrange("h d t -> d h t"), r=["plain"], w=[kTb])
    k.dma("sp", vtok[:], av[0:T, :].rearrange("(t p) c -> p t c", p=128), r=["av"], w=[vtok])
    k.dma("sp", ikT4[:], ikTd.ap(), r=["ikTd"], w=[ikT4])
    ones_f4 = k.alloc("ones_f4", [128, 128], F32)
    zeros_b = k.alloc("zeros_b", [128, 128], BF16)
    Jm = k.alloc("Jm", [128, 128], F32)
    CMT = k.alloc("CMT", [128, 128], F32)
    CM = k.alloc("CM", [128, 128], F32)
    k.op("pool", lambda e: e.memset(ones_f4[:], 1.0), w=[ones_f4])
    k.op("pool", lambda e: e.memset(zeros_b[:], 0.0), w=[zeros_b])
    k.op("pool", lambda e: e.memset(Jm[:], 1.0), w=[Jm])
    k.op("pool", lambda e: e.affine_select(out=Jm[:], in_=Jm[:], pattern=[[1, 128]], compare_op=ALU.is_equal, fill=0.0, base=-127, channel_multiplier=1), r=[Jm], w=[Jm])
    k.op("pool", lambda e: e.memset(CMT[:], 0.0), w=[CMT])
    k.op("pool", lambda e: e.affine_select(out=CMT[:], in_=CMT[:], pattern=[[1, 128]], compare_op=ALU.is_ge, fill=NEG, base=0, channel_multiplier=-1), r=[CMT], w=[CMT])
    k.op("pool", lambda e: e.memset(CM[:], 0.0), w=[CM])
    k.op("pool", lambda e: e.affine_select(out=CM[:], in_=CM[:], pattern=[[-1, 128]], compare_op=ALU.is_ge, fill=NEG, base=0, channel_multiplier=1), r=[CM], w=[CM])
    rb = k.alloc("rb", [32, 8], F32)
    rb31 = k.alloc("rb31", [32, 8], F32)
    bohs = k.alloc("bohs", [32, 384], F32)
    bvec = k.alloc("bvec", [8, 384], F32)
    Tp = k.alloc("Tp", [128, 8, 128], F32)
    Bt4 = [k.alloc(f"Bt4_{i}", [128, 8, 128], F32) for i in range(2)]
    k.dma("sp", rb[:], rel_bias.ap(), w=[rb])
    k.dma("sp", rb31[:], rel_bias[31:32, :].to_broadcast([32, 8]), w=[rb31])
    k.dma("sp", bohs[:], boh.ap(), w=[bohs])
    k.op("dve", lambda e: e.tensor_tensor(out=rb[:], in0=rb[:], in1=rb31[:], op=ALU.subtract), r=[rb, rb31], w=[rb])
    bkb = k.bank()
    k.op("pe", lambda e: e.matmul(bkb[0:8, 0:384], lhsT=rb[:], rhs=bohs[:], start=True, stop=True), r=[rb, bohs], w=[bkb])
    k.op("act", lambda e: e.copy(out=bvec[:], in_=bkb[0:8, 0:384]), r=[bkb], w=[bvec])
    k.dma("sp", biasd.ap(), bvec[:], r=[bvec], w=["biasd"])

    def mk_bias(dl):
        k.dma("sp", Tp[:], bass.AP(tensor=biasd, offset=128 * dl, ap=[[1, 128], [384, 8], [1, 128]]), r=["biasd"], w=[Tp])
        for half in range(2):
            bj = k.bank()
            k.op("pe", lambda e, bj=bj, half=half: e.matmul(bj[:, :], lhsT=Jm[:], rhs=Tp[:].rearrange("p a b -> p (a b)")[:, half * 512:(half + 1) * 512], start=True, stop=True), r=[Jm, Tp], w=[bj])
            k.op("act", lambda e, bj=bj, half=half: e.copy(out=Bt4[dl][:].rearrange("p a b -> p (a b)")[:, half * 512:(half + 1) * 512], in_=bj[:, :]), r=[bj], w=[Bt4[dl]])

    mk_bias(0)
    mk_bias(1)

    qbT = [k.alloc(f"qbT{i}", [128, 8, 128], BF16) for i in range(2)]
    qiT = [k.alloc(f"qiT{i}", [128, 16, 128], BF16) for i in range(2)]
    iwt = [k.alloc(f"iwt{i}", [128, 48], F32) for i in range(2)]
    scs = [k.alloc(f"sc{i}", [128, T], F32) for i in range(2)]
    sc = scs[0]
    Dws = [k.alloc(f"Dw{i}", [128, 16, 128], BF16) for i in range(2)]
    jnk = k.alloc("jnk", [128, T], BF16)
    rl = [k.alloc(f"rl{i}", [128, 512], BF16) for i in range(4)]
    rcnt = [0]
    bs = k.alloc("bs", [128, 8], F32)
    negT = k.alloc("negT", [128, NT, 128], F32)
    Eb = [k.alloc(f"Eb{i}", [128, 4, 128], F32) for i in range(2)]
    PTs = [k.alloc(f"PTs{i}", [128, 4, 128], BF16) for i in range(3)]
    rden = k.alloc("rden", [128, 8], F32)
    oab = k.alloc("oab", [128, 8, 128], BF16)
    ecnt = [0]

    def stage_I(qb):
        L = 128 * (qb + 1)
        tsl = slice(qb * 128, (qb + 1) * 128)
        qb_ = qbT[qb % 2]
        qi_ = qiT[qb % 2]
        iw_ = iwt[qb % 2]
        sc = scs[qb % 2]
        Dw_ = Dws[qb % 2]
        k.dma("sp", qb_[:], aq.ap()[:, :, tsl].rearrange("h d t -> d h t"), r=["plain"], w=[qb_])
        k.dma("sp", qi_[:], iq.ap()[:, :, tsl].rearrange("h d t -> d h t"), r=["plain"], w=[qi_])
        k.dma("sp", iw_[:, 0:16], iw[qb * 128:(qb + 1) * 128, :], r=["iw"], w=[iw_])
        if qb < 2:
            return
        for h in range(16):
            k.op("pool", lambda e, h=h: e.tensor_scalar(out=Dw_[:, h, :], in0=ident_f[:], scalar1=iw_[:, h:h + 1], scalar2=None, op0=ALU.mult), r=[ident_f, iw_], w=[Dw_])

        def idx_kg(kg):
            c0 = kg * 512
            n = min(512, L - c0)
            bacc = k.bank()
            k.reserved = {bacc.name}
            pend = []

            def acc(h, r_):
                k.op("pe", lambda e: e.matmul(bacc[:, 0:n], lhsT=Dw_[:, h, :], rhs=r_[:, 0:n], start=(h == 0), stop=(h == 15)), r=[Dw_, r_], w=[bacc])

            for h in range(16):
                bk_ = k.bank()
                r_ = rl[rcnt[0] % 4]
                rcnt[0] += 1
                k.op("pe", lambda e, h=h, bk_=bk_: e.matmul(bk_[:, 0:n], lhsT=qi_[:, h, :], rhs=ikT4[:, c0:c0 + n], start=True, stop=True), r=[qi_, ikT4], w=[bk_])
                k.op("act", lambda e, bk_=bk_, r_=r_: e.activation(out=r_[:, 0:n], in_=bk_[:, 0:n], func=AF.Relu), r=[bk_], w=[r_])
                pend.append((h, r_))
                if len(pend) > 2:
                    acc(*pend.pop(0))
            while pend:
                acc(*pend.pop(0))
            k.reserved = set()
            k.op("act", lambda e: e.copy(out=sc[:, c0:c0 + n], in_=bacc[:, 0:n]), r=[bacc], w=[sc])

        for kg in range((L + 511) // 512):
            idx_kg(kg)

    def stage_B(qb):
        L = 128 * (qb + 1)
        sc = scs[qb % 2]
        if qb >= 2:
            k.op("dve", lambda e: e.tensor_reduce(out=bs[:, 0:1], in_=sc[:, 0:L], axis=AX.X, op=ALU.max), r=[sc], w=[bs])
            k.op("dve", lambda e: e.tensor_reduce(out=bs[:, 1:2], in_=sc[:, 0:L], axis=AX.X, op=ALU.min), r=[sc, bs], w=[bs])
            k.op("dve", lambda e: e.tensor_scalar(out=bs[:, 1:2], in0=bs[:, 1:2], scalar1=-1.0, scalar2=None, op0=ALU.add), r=[bs], w=[bs])
            k.op("dve", lambda e: e.tensor_tensor(out=bs[:, 2:3], in0=bs[:, 0:1], in1=bs[:, 1:2], op=ALU.subtract), r=[bs], w=[bs])
            k.op("dve", lambda e: e.tensor_tensor(out=sc[:, L - 128:L], in0=sc[:, L - 128:L], in1=CM[:], op=ALU.add), r=[sc, CM], w=[sc])
            for it in range(NIT):
                f = 2.0 ** -(it + 1)
                k.op("dve", lambda e, f=f: e.scalar_tensor_tensor(out=bs[:, 3:4], in0=bs[:, 2:3], scalar=f, op0=ALU.mult, in1=bs[:, 1:2], op1=ALU.add), r=[bs], w=[bs])
                k.op("dve", lambda e: e.tensor_scalar(out=jnk[:, 0:L], in0=sc[:, 0:L], scalar1=bs[:, 3:4], scalar2=None, op0=ALU.is_gt, op1=ALU.add, accum_out=bs[:, 4:5]),
                     r=[sc, bs], w=[jnk, bs])
                k.op("dve", lambda e, f=f: e.tensor_scalar(out=bs[:, 5:6], in0=bs[:, 4:5], scalar1=255.5, scalar2=f, op0=ALU.is_gt, op1=ALU.mult), r=[bs], w=[bs])
                k.op("dve", lambda e: e.scalar_tensor_tensor(out=bs[:, 1:2], in0=bs[:, 5:6], scalar=bs[:, 2:3], op0=ALU.mult, in1=bs[:, 1:2], op1=ALU.add), r=[bs], w=[bs])
            k.op("dve", lambda e: e.tensor_scalar(out=sc[:, 0:L], in0=sc[:, 0:L], scalar1=bs[:, 1:2], scalar2=None, op0=ALU.subtract), r=[sc, bs], w=[sc])
            for k4 in range((qb + 1 + 3) // 4):
                nb = min(4, qb + 1 - k4 * 4)
                bk_ = k.bank()
                for j in range(nb):
                    kb = k4 * 4 + j
                    k.op("pe", lambda e, j=j, kb=kb, bk_=bk_: e.transpose(out=bk_[:, j * 128:(j + 1) * 128], in_=sc[:, kb * 128:(kb + 1) * 128], identity=ident_f[:]),
                         r=[sc, ident_f], w=[bk_])
                k.op("dve", lambda e, k4=k4, nb=nb, bk_=bk_: e.tensor_scalar(out=negT[:, k4 * 4:k4 * 4 + nb, :].rearrange("p a b -> p (a b)"), in0=bk_[:, 0:nb * 128],
                                                                             scalar1=0.0, scalar2=NEG, op0=ALU.is_le, op1=ALU.mult), r=[bk_], w=[negT])
            k.op("dve", lambda e: e.tensor_tensor(out=negT[:, qb, :], in0=negT[:, qb, :], in1=CMT[:], op=ALU.add), r=[negT, CMT], w=[negT])
        else:
            if qb == 1:
                k.op("pool", lambda e: e.memset(negT[:, 0, :], 0.0), w=[negT])
            k.op("pool", lambda e: e.tensor_copy(out=negT[:, qb, :], in_=CMT[:]), r=[CMT, negT], w=[negT])

    def stage_A(qb):
        tsl = slice(qb * 128, (qb + 1) * 128)
        qb_ = qbT[qb % 2]
        qi_ = qiT[qb % 2]
        bo0 = k.bank()
        bo1 = k.bank()
        bdn = k.bank()
        k.reserved = {bo0.name, bo1.name, bdn.name}
        for bz_ in (bo0, bo1, bdn):
            k.op("pe", lambda e, bz_=bz_: e.matmul(bz_[:, :], lhsT=zeros_b[:], rhs=qi_[:].rearrange("p a b -> p (a b)")[:, 0:512], start=True, stop=False), r=[zeros_b, qi_], w=[bz_])
        def kv_pair(kb, kvh):
            bl = k.bank()
            k.op("pe", lambda e, bl=bl: e.matmul(bl[:, :], lhsT=kTb[:, kvh, kb * 128:(kb + 1) * 128], rhs=qb_[:, kvh * 4:(kvh + 1) * 4, :].rearrange("p a b -> p (a b)"),
                                                start=True, stop=True), r=[kTb, qb_], w=[bl])
            E_ = Eb[ecnt[0] % 2]
            P_ = PTs[ecnt[0] % 3]
            ecnt[0] += 1
            k.op("dve", lambda e, bl=bl, E_=E_: e.tensor_tensor(out=E_[:], in0=bl[:, :].rearrange("p (a b) -> p a b", a=4), in1=negT[:, kb, :].unsqueeze(1).to_broadcast([128, 4, 128]),
                                                             op=ALU.add), r=[bl, negT], w=[E_])
            if qb - kb <= 1:
                k.op("pool", lambda e, E_=E_: e.tensor_tensor(out=E_[:], in0=E_[:], in1=Bt4[qb - kb][:, kvh * 4:(kvh + 1) * 4, :], op=ALU.add), r=[E_, Bt4[qb - kb]], w=[E_])
            k.op("act", lambda e, E_=E_, P_=P_: e.activation(out=P_[:], in_=E_[:], func=AF.Exp), r=[E_], w=[P_])
            last = (kb == qb)
            for g_ in range(4):
                h = kvh * 4 + g_
                bo = bo0 if h < 4 else bo1
                k.op("pe", lambda e, g_=g_, h=h, bo=bo, P_=P_: e.matmul(bo[:, (h % 4) * 128:(h % 4 + 1) * 128], lhsT=P_[:, g_, :], rhs=vtok[:, kb, kvh * 128:(kvh + 1) * 128],
                                                                    start=False, stop=last), r=[P_, vtok], w=[bo])
                k.op("pe", lambda e, g_=g_, h=h, P_=P_: e.matmul(bdn[:, h:h + 1], lhsT=P_[:, g_, :], rhs=ones_b[:, 0:1], start=False, stop=last), r=[P_, ones_b], w=[bdn])
        for kb in range(qb + 1):
            for kvh in range(2):
                kv_pair(kb, kvh)
        k.reserved = set()
        k.op("dve", lambda e: e.reciprocal(out=rden[:], in_=bdn[:, 0:8]), r=[bdn], w=[rden])
        for half, bo in ((0, bo0), (1, bo1)):
            k.op("dve", lambda e, half=half, bo=bo: e.tensor_tensor(out=oab[:, half * 4:(half + 1) * 4, :], in0=bo[:, :].rearrange("p (a b) -> p a b", a=4),
                                                                    in1=rden[:, half * 4:(half + 1) * 4].unsqueeze(2).to_broadcast([128, 4, 128]), op=ALU.mult),
                 r=[bo, rden], w=[oab])
        bt_ = k.bank()
        btb = bt_[:].bitcast(BF16)
        for h in range(8):
            k.op("pe", lambda e, h=h: e.transpose(out=btb[:, h * 128:(h + 1) * 128], in_=oab[:, h, :], identity=ident_b[:]), r=[oab, ident_b], w=[bt_])
        k.op("act", lambda e: e.copy(out=mixT[:, 8:16, tsl], in_=btb[:, 0:1024].rearrange("p (a b) -> p a b", a=8)), r=[bt_], w=[(mixT, "b%d" % qb)])

    stage_I(0)
    for qb in range(NT):
        if qb + 1 < NT:
            stage_I(qb + 1)
        stage_B(qb)
        stage_A(qb)
    if stop_after == "P4" and os.environ.get("P4DBG"):
        for nm, b, n in (("sc", sc, T), ("bs", bs, 8), ("negT", negT, NT * 128)):
            dd = dout("dbg_" + nm, [128, n], F32)
            src = b[:] if nm != "negT" else b[:].rearrange("p a b -> p (a b)")
            k.dma("sp", dd.ap(), src, r=[b])
    if stop_after == "P4":
        dbg = dout("dbg_mixT", [128, 16 * TT], BF16)
        k.barrier()
        k.dma("sp", dbg.ap(), mixT[:].rearrange("p a b -> p (a b)"))
        return k.finish()
    k.barrier()
    k.release(m4)
    NPG = NPAGES
    ones4 = k.alloc("ones4", [128, 128], F32)
    zeros4 = k.alloc("zeros4", [128, 128], F32)
    Ltri = k.alloc("Ltri", [128, 128], BF16)
    siota = k.alloc("siota", [128, 128], F32)
    piota = k.alloc("piota", [128, 1], F32)
    jrow = k.alloc("jrow", [128, 256], F32)
    jcol = k.alloc("jcol", [128, 2], F32)
    posc = k.alloc("posc", [128, 128], F32)
    k.op("pool", lambda e: e.memset(ones4[:], 1.0), w=[ones4])
    k.op("pool", lambda e: e.memset(zeros4[:], 0.0), w=[zeros4])
    k.op("pool", lambda e: e.memset(Ltri[:], 1.0), w=[Ltri])
    k.op("pool", lambda e: e.affine_select(out=Ltri[:], in_=Ltri[:], pattern=[[1, 128]], compare_op=ALU.is_ge, fill=0.0, base=-1, channel_multiplier=-1), r=[Ltri], w=[Ltri])
    k.op("pool", lambda e: e.iota(siota[:], pattern=[[1, 128]], base=0, channel_multiplier=0, allow_small_or_imprecise_dtypes=True), w=[siota])
    k.op("pool", lambda e: e.iota(piota[:], pattern=[[0, 1]], base=0, channel_multiplier=1, allow_small_or_imprecise_dtypes=True), w=[piota])
    k.op("pool", lambda e: e.iota(jrow[:], pattern=[[1, 256]], base=0, channel_multiplier=0, allow_small_or_imprecise_dtypes=True), w=[jrow])
    k.op("pool", lambda e: e.iota(jcol[:], pattern=[[128, 2]], base=0, channel_multiplier=1, allow_small_or_imprecise_dtypes=True), w=[jcol])
    k.op("pool", lambda e: e.iota(posc[:], pattern=[[1, 128]], base=0, channel_multiplier=128, allow_small_or_imprecise_dtypes=True), w=[posc])
    thrb = k.alloc("thrb", [128, 31], F32)
    k.dma("sp", thrb[:], bthr.ap().to_broadcast([128, 31]), w=[thrb])
    rbs = k.alloc("rbs", [32, 8], F32)
    rbT = k.alloc("rbT", [8, 32], F32)
    drbT = k.alloc("drbT", [128, 8, 32], F32)
    k.dma("sp", rbs[:], rel_bias.ap(), w=[rbs])
    bq = k.bank()
    k.op("pe", lambda e: e.transpose(out=bq[0:8, 0:32], in_=rbs[:], identity=ident_f[0:32, 0:32]), r=[rbs, ident_f], w=[bq])
    k.op("act", lambda e: e.copy(out=rbT[:], in_=bq[0:8, 0:32]), r=[bq], w=[rbT])
    k.dma("sp", rbTd.ap(), rbT[:], r=[rbT], w=["rbTd"])
    k.dma("sp", drbT[:].rearrange("p a b -> p (a b)"), rbTd.ap().rearrange("a b -> (a b)").unsqueeze(0).to_broadcast([128, 256]), r=["rbTd"], w=[drbT])
    rb0b = k.alloc("rb0b", [128, 8], F32)
    k.op("dve", lambda e: e.tensor_copy(out=rb0b[:], in_=drbT[:, :, 0]), r=[drbT], w=[rb0b])
    dtmp = k.alloc("dtmp", [128, 8, 31], F32)
    k.op("dve", lambda e: e.tensor_tensor(out=dtmp[:], in0=drbT[:, :, 1:32], in1=drbT[:, :, 0:31], op=ALU.subtract), r=[drbT], w=[dtmp])
    pt_i = k.alloc("pt_i", [128, TS], I32)
    pt_f = k.alloc("pt_f", [128, TS], F32)
    k.dma("sp", pt_i[:], page_table.ap().rearrange("s p -> p s"), w=[pt_i], slow=True)
    k.op("dve", lambda e: e.tensor_copy(out=pt_f[:], in_=pt_i[:]), r=[pt_i], w=[pt_f])
    k.op("dve", lambda e: e.tensor_scalar(out=pt_f[:], in0=pt_f[:], scalar1=128.0, scalar2=None, op0=ALU.mult), r=[pt_f], w=[pt_f])
    qiS = k.alloc("qiS", [128, TS, 16], BF16)
    wS = k.alloc("wS", [128, TS, 16], F32)
    kiS = k.alloc("kiS", [128, TS], BF16)
    qbS = k.alloc("qbS", [128, 8, TS], BF16)
    knS = k.alloc("knS", [128, 2, TS], BF16)
    for s_i in range(TS):
        k.dma("sp", qiS[:, s_i, :], iq.ap()[:, :, T + s_i].rearrange("h d -> d h"), r=["plain"], w=[qiS], slow=True)
    k.dma("sp", wS[:].rearrange("p a b -> p (a b)"), iw[T:TT, :].rearrange("a b -> (a b)").unsqueeze(0).to_broadcast([128, TS * 16]), r=["iw"], w=[wS])
    k.dma("sp", kiS[:], ikTd[:, T:TT], r=["ikTd"], w=[kiS])
    k.dma("sp", qbS[:], aq.ap()[:, :, T:TT].rearrange("h d s -> d h s"), r=["plain"], w=[qbS])
    k.dma("sp", knS[:], akT.ap()[:, :, T:TT].rearrange("h d s -> d h s"), r=["plain"], w=[knS])
    scS = k.alloc("scS", [128, TS, 128], F32)
    snew = k.alloc("snew", [128, TS], F32)
    Gp = k.alloc("Gp", [128, NPG * 128], F32)
    kTs = [k.alloc(f"kTs{i}", [128, 4, 128], BF16) for i in range(2)]
    rr = k.alloc("rr", [128, 32, 16], F32)
    knb = k.alloc("knb", [128, 128], BF16)
    ckx2 = cache_kidx.ap()

    def dma_raw(q, fn, r=(), w=()):
        i = k.drr[q]
        k.drr[q] = (i + 1) % len(k.dsem[q])
        sk = ("d", q, i)
        waits = k._collect(q, r, w)
        prev = k.dcnt[q][i]
        kn = k.known[q]
        if prev > 0 and kn.get(sk, 0) < prev:
            kn[sk] = prev
            waits.append((sk, prev))
        k.dcnt[q][i] = prev + 16
        tok = (sk, prev + 16)
        k.ops[q].append((waits, fn, (sk, 16)))
        k._update(tok, r, w)
        return tok

    def scores_seq(s_i):
        dma_raw("pool", lambda e: e.indirect_dma_start(out=Gp[:], out_offset=None, in_=ckx2, in_offset=bass.IndirectOffsetOnAxis(ap=pt_i[:, s_i:s_i + 1], axis=0)),
                r=[pt_i], w=[Gp])
        scb = [k.bank() for _ in range(4)]
        k.reserved = {b_.name for b_ in scb}
        for sb in range(32):
            bt_ = k.bank()
            kt_ = kTs[sb % 2]
            for j in range(4):
                sl_ = 4 * sb + j
                k.op("pe", lambda e, j=j, sl_=sl_, bt_=bt_: e.transpose(out=bt_[:, j * 128:(j + 1) * 128], in_=Gp[:, sl_ * 128:(sl_ + 1) * 128], identity=ident_f[:]),
                     r=[Gp, ident_f], w=[bt_])
            k.op("act", lambda e, bt_=bt_, kt_=kt_: e.copy(out=kt_[:].rearrange("p a b -> p (a b)"), in_=bt_[:, :]), r=[bt_], w=[kt_])
            for j in range(4):
                sl_ = 4 * sb + j
                sbk = scb[sl_ // 32]
                k.op("pe", lambda e, j=j, sl_=sl_, sbk=sbk, kt_=kt_: e.matmul(sbk[:, (sl_ % 32) * 16:(sl_ % 32 + 1) * 16], lhsT=kt_[:, j, :], rhs=qiS[:, s_i, :], start=True, stop=True),
                     r=[kt_, qiS], w=[sbk])
        k.reserved = set()
        for b_i in range(4):
            sbk = scb[b_i]
            k.op("dve", lambda e, sbk=sbk: e.tensor_scalar(out=rr[:].rearrange("p a b -> p (a b)"), in0=sbk[:, :], scalar1=0.0, scalar2=None, op0=ALU.max), r=[sbk], w=[rr])
            k.op("dve", lambda e: e.tensor_tensor(out=rr[:], in0=rr[:], in1=wS[:, s_i, :].unsqueeze(1).to_broadcast([128, 32, 16]), op=ALU.mult), r=[rr, wS], w=[rr])
            k.op("dve", lambda e, b_i=b_i: e.tensor_reduce(out=scS[:, s_i, b_i * 32:(b_i + 1) * 32], in_=rr[:], axis=AX.X, op=ALU.add), r=[rr], w=[scS])
        k.op("dve", lambda e: e.tensor_copy(out=knb[:], in_=kiS[:, s_i:s_i + 1].to_broadcast([128, 128])), r=[kiS], w=[knb])
        bn = k.bank()
        k.op("pe", lambda e: e.matmul(bn[:, 0:16], lhsT=knb[:], rhs=qiS[:, s_i, :], start=True, stop=True), r=[knb, qiS], w=[bn])
        k.op("dve", lambda e: e.tensor_scalar(out=rr[:, 0, :], in0=bn[:, 0:16], scalar1=0.0, scalar2=None, op0=ALU.max), r=[bn], w=[rr])
        k.op("dve", lambda e: e.tensor_tensor(out=rr[:, 0, :], in0=rr[:, 0, :], in1=wS[:, s_i, :], op=ALU.mult), r=[rr, wS], w=[rr])
        k.op("dve", lambda e: e.tensor_reduce(out=snew[:, s_i:s_i + 1], in_=rr[:, 0, :], axis=AX.X, op=ALU.add), r=[rr], w=[snew])

    for s_i in range(0 if skip_p4s else TS):
        scores_seq(s_i)

    pm = k.alloc("pm", [128, 2 * TS], F32)
    gmm = k.alloc("gmm", [TS, 4], F32)
    dgm = k.alloc("dgm", [TS, 2 * TS], F32)
    lo_ = k.alloc("lo_", [128, TS], F32)
    w0_ = k.alloc("w0_", [128, TS], F32)
    bsS = k.alloc("bsS", [128, 6 * TS], F32)
    cmpb = k.alloc("cmpb", [128, TS, 128], F32)
    k.op("dve", lambda e: e.tensor_reduce(out=pm[:, 0:TS], in_=scS[:], axis=AX.X, op=ALU.max), r=[scS], w=[pm])
    k.op("dve", lambda e: e.tensor_reduce(out=pm[:, TS:2 * TS], in_=scS[:], axis=AX.X, op=ALU.min), r=[scS, pm], w=[pm])
    k.op("dve", lambda e: e.tensor_tensor(out=pm[:, 0:TS], in0=pm[:, 0:TS], in1=snew[:], op=ALU.max), r=[pm, snew], w=[pm])
    k.op("dve", lambda e: e.tensor_tensor(out=pm[:, TS:2 * TS], in0=pm[:, TS:2 * TS], in1=snew[:], op=ALU.min), r=[pm, snew], w=[pm])
    bmx = k.bank()
    bmn = k.bank()
    k.op("pe", lambda e: e.transpose(out=bmx[0:TS, 0:128], in_=pm[:, 0:TS], identity=ident_f[:]), r=[pm, ident_f], w=[bmx])
    k.op("pe", lambda e: e.transpose(out=bmn[0:TS, 0:128], in_=pm[:, TS:2 * TS], identity=ident_f[:]), r=[pm, ident_f], w=[bmn])
    k.op("dve", lambda e: e.tensor_reduce(out=gmm[:, 0:1], in_=bmx[0:TS, 0:128], axis=AX.X, op=ALU.max), r=[bmx], w=[gmm])
    k.op("dve", lambda e: e.tensor_reduce(out=gmm[:, 1:2], in_=bmn[0:TS, 0:128], axis=AX.X, op=ALU.min), r=[bmn, gmm], w=[gmm])
    k.op("dve", lambda e: e.tensor_scalar(out=gmm[:, 1:2], in0=gmm[:, 1:2], scalar1=-1.0, scalar2=None, op0=ALU.add), r=[gmm], w=[gmm])
    k.op("dve", lambda e: e.tensor_tensor(out=gmm[:, 2:3], in0=gmm[:, 0:1], in1=gmm[:, 1:2], op=ALU.subtract), r=[gmm], w=[gmm])
    k.op("dve", lambda e: e.tensor_scalar(out=dgm[:, 0:TS], in0=ident_f[0:TS, 0:TS], scalar1=gmm[:, 1:2], scalar2=None, op0=ALU.mult), r=[gmm, ident_f], w=[dgm])
    k.op("dve", lambda e: e.tensor_scalar(out=dgm[:, TS:2 * TS], in0=ident_f[0:TS, 0:TS], scalar1=gmm[:, 2:3], scalar2=None, op0=ALU.mult), r=[gmm, ident_f, dgm], w=[dgm])
    bbc = k.bank()
    k.op("pe", lambda e: e.matmul(bbc[:, 0:2 * TS], lhsT=ones4[0:TS, :], rhs=dgm[:], start=True, stop=True), r=[ones4, dgm], w=[bbc])
    k.op("act", lambda e: e.copy(out=lo_[:], in_=bbc[:, 0:TS]), r=[bbc], w=[lo_])
    k.op("act", lambda e: e.copy(out=w0_[:], in_=bbc[:, TS:2 * TS]), r=[bbc], w=[w0_])
    mid_ = bsS[:, 0:TS]
    cnt_ = bsS[:, TS:2 * TS]
    gn_ = bsS[:, 2 * TS:3 * TS]
    tot_ = bsS[:, 3 * TS:4 * TS]
    ge_ = bsS[:, 4 * TS:5 * TS]

    def bis_iter(it):
        f = 2.0 ** -(it + 1)
        k.op("dve", lambda e: e.scalar_tensor_tensor(out=mid_, in0=w0_[:], scalar=f, op0=ALU.mult, in1=lo_[:], op1=ALU.add), r=[w0_, lo_], w=[bsS])
        k.op("dve", lambda e: e.tensor_tensor(out=cmpb[:], in0=scS[:], in1=mid_.unsqueeze(2).to_broadcast([128, TS, 128]), op=ALU.is_gt), r=[scS, bsS], w=[cmpb])
        k.op("dve", lambda e: e.tensor_reduce(out=cnt_, in_=cmpb[:], axis=AX.X, op=ALU.add), r=[cmpb, bsS], w=[bsS])
        bc_ = k.bank()
        k.op("pe", lambda e: e.matmul(bc_[:, 0:TS], lhsT=ones4[:], rhs=cnt_, start=True, stop=True), r=[ones4, bsS], w=[bc_])
        k.op("dve", lambda e: e.tensor_tensor(out=gn_, in0=snew[:], in1=mid_, op=ALU.is_gt), r=[snew, bsS], w=[bsS])
        k.op("dve", lambda e: e.tensor_tensor(out=tot_, in0=bc_[:, 0:TS], in1=gn_, op=ALU.add), r=[bc_, bsS], w=[bsS])
        k.op("dve", lambda e: e.tensor_scalar(out=ge_, in0=tot_, scalar1=255.5, scalar2=f, op0=ALU.is_gt, op1=ALU.mult), r=[bsS], w=[bsS])
        k.op("dve", lambda e: e.tensor_tensor(out=ge_, in0=ge_, in1=w0_[:], op=ALU.mult), r=[bsS, w0_], w=[bsS])
        k.op("dve", lambda e: e.tensor_tensor(out=lo_[:], in0=lo_[:], in1=ge_, op=ALU.add), r=[lo_, bsS], w=[lo_])

    for it in range(0 if skip_p4s else 20):
        bis_iter(it)

    Msel = k.alloc("Msel", [128, 128], F32)
    Mb = k.alloc("Mb", [128, 128], BF16)
    Bs = k.alloc("Bs", [128, 128], F32)
    cum = k.alloc("cum", [128, 128], F32)
    rank = k.alloc("rank", [128, 128], F32)
    payl = k.alloc("payl", [128, 128, 2], F32)
    Soh = [k.alloc(f"Soh{i}", [128, 256], F32) for i in range(2)]
    idxf = k.alloc("idxf", [128, 4], F32)
    idx_i = k.alloc("idx_i", [128, 2], I32)
    Ksel = k.alloc("Ksel", [128, 2, 256], F32)
    Vsel = k.alloc("Vsel", [128, 2, 256], F32)
    KTs = k.alloc("KTs", [128, 4, 128], F32)
    qf = k.alloc("qf", [128, 8], F32)
    knf = k.alloc("knf", [128, 2], F32)
    sm = k.alloc("sm", [128, 64], F32)
    ind = k.alloc("ind", [128, 2, 31], F32)
    prod = k.alloc("prod", [128, 2, 8, 31], F32)
    Eg = k.alloc("Eg", [128, 2, 8], F32)
    rowp = k.alloc("rowp", [1, 64], F32)
    vnr = k.alloc("vnr", [1, 256], BF16)
    vnf = k.alloc("vnf", [1, 256], F32)
    osb = k.alloc("osb", [4, 2, 130], F32)
    ck2 = cache_k.ap()
    cv2 = cache_v.ap()
    k.op("dve", lambda e: e.tensor_copy(out=payl[:, :, 1], in_=posc[:]), r=[posc], w=[payl])

    def attend_seq(s_i):
        col = T + s_i
        k.op("dve", lambda e: e.tensor_scalar(out=Msel[:], in0=scS[:, s_i, :], scalar1=lo_[:, s_i:s_i + 1], scalar2=None, op0=ALU.is_gt), r=[scS, lo_], w=[Msel])
        k.op("dve", lambda e: e.tensor_copy(out=Mb[:], in_=Msel[:]), r=[Msel], w=[Mb])
        k.op("dve", lambda e: e.tensor_tensor(out=sm[:, 0:1], in0=snew[:, s_i:s_i + 1], in1=lo_[:, s_i:s_i + 1], op=ALU.is_gt), r=[snew, lo_], w=[sm])
        bA_ = k.bank()
        bB_ = k.bank()
        k.op("pe", lambda e: e.matmul(bA_[:, 0:128], lhsT=Ltri[:], rhs=Mb[:], start=True, stop=True), r=[Ltri, Mb], w=[bA_])
        k.op("pe", lambda e: e.matmul(bB_[:, 0:128], lhsT=ones_b[:], rhs=Mb[:], start=True, stop=True), r=[ones_b, Mb], w=[bB_])
        k.op("act", lambda e: e.copy(out=Bs[:], in_=bB_[:, 0:128]), r=[bB_], w=[Bs])
        k.op("dve", lambda e: e.tensor_tensor_scan(out=cum[:], data0=Bs[:], data1=zeros4[:], initial=0.0, op0=ALU.add, op1=ALU.add), r=[Bs, zeros4], w=[cum])
        k.op("dve", lambda e: e.tensor_tensor(out=rank[:], in0=cum[:], in1=Bs[:], op=ALU.subtract), r=[cum, Bs], w=[rank])
        k.op("dve", lambda e: e.tensor_tensor(out=rank[:], in0=rank[:], in1=bA_[:, 0:128], op=ALU.add), r=[rank, bA_], w=[rank])
        k.op("dve", lambda e: e.scalar_tensor_tensor(out=rank[:], in0=rank[:], scalar=1.0, op0=ALU.add, in1=Msel[:], op1=ALU.mult), r=[rank, Msel], w=[rank])
        k.op("dve", lambda e: e.tensor_scalar(out=rank[:], in0=rank[:], scalar1=-1.0, scalar2=None, op0=ALU.add), r=[rank], w=[rank])
        k.op("dve", lambda e: e.tensor_scalar(out=payl[:, :, 0], in0=siota[:], scalar1=pt_f[:, s_i:s_i + 1], scalar2=None, op0=ALU.add), r=[siota, pt_f], w=[payl])
        bacc = k.bank()
        k.reserved = {bacc.name}
        k.op("pe", lambda e: e.matmul(bacc[:, 0:4], lhsT=zeros4[:], rhs=ones4[:, 0:4], start=True, stop=False), r=[zeros4, ones4], w=[bacc])
        for sl_ in range(128):
            so_ = Soh[sl_ % 2]
            k.op("dve", lambda e, sl_=sl_, so_=so_: e.tensor_scalar(out=so_[:], in0=jrow[:], scalar1=rank[:, sl_:sl_ + 1], scalar2=None, op0=ALU.is_equal), r=[jrow, rank], w=[so_])
            for half in range(2):
                k.op("pe", lambda e, sl_=sl_, so_=so_, half=half: e.matmul(bacc[:, half * 2:(half + 1) * 2], lhsT=so_[:, half * 128:(half + 1) * 128], rhs=payl[:, sl_, :],
                                                                      start=False, stop=(sl_ == 127)), r=[so_, payl], w=[bacc])
        k.reserved = set()
        k.op("act", lambda e: e.copy(out=idxf[:], in_=bacc[:, 0:4]), r=[bacc], w=[idxf])
        k.op("dve", lambda e: e.tensor_copy(out=idx_i[:], in_=idxf[:].rearrange("p (a b) -> p a b", b=2)[:, :, 0]), r=[idxf], w=[idx_i])
        for half in range(2):
            dma_raw("pool", lambda e, half=half: e.indirect_dma_start(out=Ksel[:, half, :], out_offset=None, in_=ck2, in_offset=bass.IndirectOffsetOnAxis(ap=idx_i[:, half:half + 1], axis=0)),
                    r=[idx_i], w=[Ksel])
            dma_raw("pool", lambda e, half=half: e.indirect_dma_start(out=Vsel[:, half, :], out_offset=None, in_=cv2, in_offset=bass.IndirectOffsetOnAxis(ap=idx_i[:, half:half + 1], axis=0)),
                    r=[idx_i], w=[Vsel])
        bkt = k.bank()
        for half in range(2):
            for kvh in range(2):
                jj = half * 2 + kvh
                k.op("pe", lambda e, half=half, kvh=kvh, jj=jj: e.transpose(out=bkt[:, jj * 128:(jj + 1) * 128], in_=Ksel[:, half, kvh * 128:(kvh + 1) * 128], identity=ident_f[:]),
                     r=[Ksel, ident_f], w=[bkt])
        k.op("act", lambda e: e.copy(out=KTs[:].rearrange("p a b -> p (a b)"), in_=bkt[:, :]), r=[bkt], w=[KTs])
        k.op("dve", lambda e: e.tensor_copy(out=qf[:], in_=qbS[:, :, s_i]), r=[qbS], w=[qf])
        k.op("dve", lambda e: e.tensor_copy(out=knf[:], in_=knS[:, :, s_i]), r=[knS], w=[knf])
        blg = k.bank()
        for half in range(2):
            for kvh in range(2):
                jj = half * 2 + kvh
                k.op("pe", lambda e, half=half, kvh=kvh, jj=jj: e.matmul(blg[:, half * 8 + kvh * 4:half * 8 + kvh * 4 + 4], lhsT=KTs[:, jj, :], rhs=qf[:, kvh * 4:(kvh + 1) * 4], start=True, stop=True),
                     r=[KTs, qf], w=[blg])
        bln = k.bank()
        for kvh in range(2):
            k.op("pe", lambda e, kvh=kvh: e.matmul(bln[0:1, kvh * 4:(kvh + 1) * 4], lhsT=knf[:, kvh:kvh + 1], rhs=qf[:, kvh * 4:(kvh + 1) * 4], start=True, stop=True), r=[knf, qf], w=[bln])
        k.op("dve", lambda e: e.tensor_scalar(out=sm[:, 2:4], in0=idxf[:].rearrange("p (a b) -> p a b", b=2)[:, :, 1], scalar1=-1.0, scalar2=float(NPG * 128), op0=ALU.mult, op1=ALU.add), r=[idxf], w=[sm])
        k.op("dve", lambda e: e.tensor_tensor(out=ind[:], in0=sm[:, 2:4].unsqueeze(2).to_broadcast([128, 2, 31]), in1=thrb[:].unsqueeze(1).to_broadcast([128, 2, 31]), op=ALU.is_ge), r=[sm, thrb], w=[ind])
        k.op("dve", lambda e: e.tensor_tensor(out=prod[:], in0=ind[:].unsqueeze(2).to_broadcast([128, 2, 8, 31]), in1=dtmp[:].unsqueeze(1).to_broadcast([128, 2, 8, 31]), op=ALU.mult), r=[ind, dtmp], w=[prod])
        k.op("dve", lambda e: e.tensor_reduce(out=Eg[:], in_=prod[:], axis=AX.X, op=ALU.add), r=[prod], w=[Eg])
        k.op("dve", lambda e: e.tensor_tensor(out=Eg[:], in0=Eg[:], in1=rb0b[:].unsqueeze(1).to_broadcast([128, 2, 8]), op=ALU.add), r=[Eg, rb0b], w=[Eg])
        k.op("dve", lambda e: e.tensor_scalar(out=sm[:, 4:6], in0=jcol[:], scalar1=cum[:, 127:128], scalar2=None, op0=ALU.is_lt), r=[jcol, cum, sm], w=[sm])
        k.op("dve", lambda e: e.tensor_scalar(out=sm[:, 4:6], in0=sm[:, 4:6], scalar1=-1.0, scalar2=30000.0, op0=ALU.add, op1=ALU.mult), r=[sm], w=[sm])
        k.op("dve", lambda e: e.tensor_tensor(out=Eg[:], in0=Eg[:], in1=sm[:, 4:6].unsqueeze(2).to_broadcast([128, 2, 8]), op=ALU.add), r=[Eg, sm], w=[Eg])
        k.op("dve", lambda e: e.tensor_tensor(out=Eg[:].rearrange("p a b -> p (a b)"), in0=Eg[:].rearrange("p a b -> p (a b)"), in1=blg[:, 0:16], op=ALU.add), r=[Eg, blg], w=[Eg])
        k.op("act", lambda e: e.activation(out=Eg[:], in_=Eg[:], func=AF.Exp), r=[Eg], w=[Eg])
        k.op("dve", lambda e: e.tensor_scalar(out=rowp[:, 8:9], in0=sm[0:1, 0:1], scalar1=-1.0, scalar2=30000.0, op0=ALU.add, op1=ALU.mult), r=[sm], w=[rowp])
        k.op("dve", lambda e: e.tensor_tensor(out=rowp[:, 0:8], in0=bln[0:1, 0:8], in1=rb0b[0:1, :], op=ALU.add), r=[bln, rb0b, rowp], w=[rowp])
        k.op("dve", lambda e: e.tensor_scalar(out=rowp[:, 0:8], in0=rowp[:, 0:8], scalar1=rowp[:, 8:9], scalar2=None, op0=ALU.add), r=[rowp], w=[rowp])
        k.op("act", lambda e: e.activation(out=rowp[:, 0:8], in_=rowp[:, 0:8], func=AF.Exp), r=[rowp], w=[rowp])
        k.dma("sp", vnr[:], av[col:col + 1, :], r=["av"], w=[vnr])
        k.op("dve", lambda e: e.tensor_copy(out=vnf[:], in_=vnr[:]), r=[vnr], w=[vnf])
        for kvh in range(2):
            bon = k.bank()
            bod = k.bank()
            for half in range(2):
                k.op("pe", lambda e, kvh=kvh, half=half, bon=bon: e.matmul(bon[0:4, 0:128], lhsT=Eg[:, half, kvh * 4:(kvh + 1) * 4], rhs=Vsel[:, half, kvh * 128:(kvh + 1) * 128], start=(half == 0), stop=False),
                     r=[Eg, Vsel], w=[bon])
            k.op("pe", lambda e, kvh=kvh, bon=bon: e.matmul(bon[0:4, 0:128], lhsT=rowp[0:1, kvh * 4:(kvh + 1) * 4], rhs=vnf[0:1, kvh * 128:(kvh + 1) * 128], start=False, stop=True), r=[rowp, vnf], w=[bon])
            for half in range(2):
                k.op("pe", lambda e, kvh=kvh, half=half, bod=bod: e.matmul(bod[0:4, 0:1], lhsT=Eg[:, half, kvh * 4:(kvh + 1) * 4], rhs=ones4[:, 0:1], start=(half == 0), stop=False), r=[Eg, ones4], w=[bod])
            k.op("pe", lambda e, kvh=kvh, bod=bod: e.matmul(bod[0:4, 0:1], lhsT=rowp[0:1, kvh * 4:(kvh + 1) * 4], rhs=ones4[0:1, 0:1], start=False, stop=True), r=[rowp, ones4], w=[bod])
            k.op("dve", lambda e, kvh=kvh, bod=bod: e.reciprocal(out=osb[:, kvh, 128:129], in_=bod[0:4, 0:1]), r=[bod], w=[osb])
            k.op("dve", lambda e, kvh=kvh, bon=bon: e.tensor_scalar(out=osb[:, kvh, 0:128], in0=bon[0:4, 0:128], scalar1=osb[:, kvh, 128:129], scalar2=None, op0=ALU.mult), r=[bon, osb], w=[osb])
        bot = k.bank()
        for kvh in range(2):
            k.op("pe", lambda e, kvh=kvh: e.transpose(out=bot[:, kvh * 4:(kvh + 1) * 4], in_=osb[:, kvh, 0:128], identity=ident_f[0:4, 0:4]), r=[osb, ident_f], w=[bot])
        k.op("act", lambda e: e.copy(out=mixT[:, 8:16, col], in_=bot[:, 0:8]), r=[bot], w=[(mixT, "sb")])

    for s_i in range(0 if skip_p4s else TS):
        attend_seq(s_i)
    k.barrier()
    k.release(m4)
    m5 = k.mark()
    wout = k.alloc("wout", [128, 16, D], BF16)
    for g in range(4):
        k.dma("pool", wout[:, :, g * 512:(g + 1) * 512], w_out[:, g * 512:(g + 1) * 512].rearrange("(k p) n -> p k n", p=128), w=[(wout, g)])
    G1b = k.alloc("G1b", [128, D], BF16)
    A2b = k.alloc("A2b", [128, D], BF16)
    SH2b = k.alloc("SH2b", [128, D], BF16)
    for buf_, idx_ in ((G1b, 2), (SH2b, 3), (A2b, 4)):
        k.dma("pool", buf_[:], modd[0:1, idx_ * D:(idx_ + 1) * D].to_broadcast([128, D]), r=["modd"], w=[buf_])
    xt5 = [k.alloc(f"xt5_{i}", [128, D], F32) for i in range(2)]
    mos = [k.alloc(f"mo{i}", [128, D], F32) for i in range(2)]
    h2b = k.alloc("h2b", [128, D], BF16)
    jnk5 = h2b
    st5 = [k.alloc(f"st5_{i}", [128, 8], F32) for i in range(2)]
    h2st = [k.alloc(f"h2st{i}", [128, 16, 128], BF16) for i in range(2)]

    def p5_tile(ti):
        c0, n = (ti * 128, 128) if ti < NT else (T, TS)
        x_ = xt5[ti % 2]
        mo = mos[ti % 2]
        s_ = st5[ti % 2]
        hs_ = h2st[ti % 2]
        G1_, A2_, SH2_ = (G1b, A2b, SH2b)
        if ti == NT:
            k.dma("pool", G1b[0:TS, :], modd[1:5, 2 * D:3 * D], r=["modd"], w=[G1b])
            k.dma("pool", SH2b[0:TS, :], modd[1:5, 3 * D:4 * D], r=["modd"], w=[SH2b])
            k.dma("pool", A2b[0:TS, :], modd[1:5, 4 * D:5 * D], r=["modd"], w=[A2b])
        if ti == 0:
            k.dma("sp", x_[0:n, :], xp[c0:c0 + n, :], w=[x_])
        if ti + 1 <= NT:
            tn = ti + 1
            cn, nn = (tn * 128, 128) if tn < NT else (T, TS)
            xn_ = xt5[tn % 2]
            k.dma("sp", xn_[0:nn, :], xp[cn:cn + nn, :] if tn < NT else xs.ap(), w=[xn_])
        mkeys = [(mixT, ti), (mixT, "b%d" % ti)] if ti < NT else [(mixT, "s%d" % j) for j in range(TS)] + [(mixT, "sb")]
        for nq in range(4):
            bk_ = k.bank()
            for kk in range(16):
                k.op("pe", lambda e, kk=kk, bk_=bk_, nq=nq: e.matmul(bk_[0:n, :], lhsT=mixT[:, kk, c0:c0 + n], rhs=wout[:, kk, nq * 512:(nq + 1) * 512], start=(kk == 0), stop=(kk == 15)),
                     r=mkeys + [(wout, nq)], w=[bk_])
            k.op("act", lambda e, bk_=bk_, nq=nq: e.copy(out=mo[0:n, nq * 512:(nq + 1) * 512], in_=bk_[0:n, :]), r=[bk_], w=[mo])
        k.op("act", lambda e: e.activation(out=jnk5[0:n, :], in_=mo[0:n, :], func=AF.Square, accum_out=s_[0:n, 0:1]), r=[mo], w=[h2b, s_])
        k.op("act", lambda e: e.activation(out=s_[0:n, 1:2], in_=s_[0:n, 0:1], func=AF.Sqrt, scale=1.0 / D, bias=EPS), r=[s_], w=[s_])
        k.op("dve", lambda e: e.reciprocal(out=s_[0:n, 2:3], in_=s_[0:n, 1:2]), r=[s_], w=[s_])
        k.op("dve", lambda e: e.scalar_tensor_tensor(out=mo[0:n, :], in0=mo[0:n, :], scalar=s_[0:n, 2:3], op0=ALU.mult, in1=G1_[0:n, :], op1=ALU.mult), r=[mo, s_, G1_], w=[mo])
        k.op("pool", lambda e: e.tensor_tensor(out=x_[0:n, :], in0=x_[0:n, :], in1=mo[0:n, :], op=ALU.add), r=[x_, mo], w=[x_])
        k.dma("sp", x1d[c0:c0 + n, :], x_[0:n, :], r=[x_], w=["x1d"])
        k.op("act", lambda e: e.activation(out=jnk5[0:n, :], in_=x_[0:n, :], func=AF.Square, accum_out=s_[0:n, 3:4]), r=[x_, s_], w=[h2b, s_])
        k.op("act", lambda e: e.activation(out=s_[0:n, 4:5], in_=s_[0:n, 3:4], func=AF.Sqrt, scale=1.0 / D, bias=EPS), r=[s_], w=[s_])
        k.op("dve", lambda e: e.reciprocal(out=s_[0:n, 5:6], in_=s_[0:n, 4:5]), r=[s_], w=[s_])
        k.op("dve", lambda e: e.scalar_tensor_tensor(out=mo[0:n, :], in0=x_[0:n, :], scalar=s_[0:n, 5:6], op0=ALU.mult, in1=A2_[0:n, :], op1=ALU.mult), r=[x_, s_, A2_, mo], w=[mo])
        k.op("pool", lambda e: e.tensor_tensor(out=h2b[0:n, :], in0=mo[0:n, :], in1=SH2_[0:n, :], op=ALU.add), r=[mo, SH2_], w=[h2b])
        b0 = k.bank()
        b1 = k.bank()
        for kk in range(16):
            bb = b0 if kk < 8 else b1
            k.op("pe", lambda e, kk=kk, bb=bb: e.transpose(out=bb[:].bitcast(BF16)[:, (kk % 8) * 128:(kk % 8) * 128 + n], in_=h2b[0:n, kk * 128:(kk + 1) * 128], identity=ident_b[0:n, 0:n]),
                 r=[h2b, ident_b], w=[bb])
        for half, bb in ((0, b0), (1, b1)):
            k.op("act", lambda e, half=half, bb=bb: e.copy(out=hs_[:, half * 8:(half + 1) * 8, 0:n], in_=bb[:].bitcast(BF16).rearrange("p (a b) -> p a b", a=8)[:, :, 0:n]),
                 r=[bb], w=[hs_])
        k.dma("sp", h2Td[:, :, c0:c0 + n], hs_[:, :, 0:n], r=[hs_], w=["h2Td"])

    for ti in range(NT + 1):
        p5_tile(ti)
    k.barrier()
    k.release(mP)
    if stop_after == "P5":
        return k.finish()

    alloc_wb()
    TB = 512
    NBLK6 = T // TB
    uT = k.alloc("uT", [128, 64, TB], BF16)
    uTs = k.alloc("uTs", [128, 64, TS], BF16)
    h2Tb = k.alloc("h2Tb", [128, 16, TB], BF16)
    h2Ts = k.alloc("h2Ts", [128, 16, TS], BF16)
    w2b = [k.alloc(f"w2b{i}", [128, 8, 512], BF16) for i in range(2)]
    fbuf = [k.alloc(f"fbuf{i}", [128, D], F32) for i in range(4)]
    x1t = [k.alloc("x1t0", [128, D], F32)] * 2
    G2b = k.alloc("G2b", [128, D], F32)
    load_mod_bcast(G2b, 5)
    rl6 = [k.alloc(f"rl6_{i}", [128, TB], BF16) for i in range(2)]
    st6 = [k.alloc(f"st6_{i}", [128, 4], F32) for i in range(2)]
    w2cnt = [0]
    k.dma("sp", h2Ts[:], h2Td[:, :, T:TT], r=["h2Td"], w=[h2Ts])

    def ffn_block(b):
        with_s = (b == NBLK6 - 1)
        k.dma("sp", h2Tb[:], h2Td[:, :, b * TB:(b + 1) * TB], r=["h2Td"], w=[h2Tb])
        def phaseA(g):
            wt = wb[wcnt[0] % 2]
            wcnt[0] += 1
            k.dma("sp", wt[:], w1s[g], r=[("w1s", g)], w=[wt])
            for j in range(4):
                ch = g * 4 + j
                bk_ = k.bank()
                for kk in range(16):
                    k.op("pe", lambda e, kk=kk, bk_=bk_, j=j: e.matmul(bk_[:, :], lhsT=wt[:, kk, j * 128:(j + 1) * 128], rhs=h2Tb[:, kk, :], start=(kk == 0), stop=(kk == 15)),
                         r=[wt, h2Tb], w=[bk_])
                r_ = rl6[ch % 2]
                k.op("act", lambda e, bk_=bk_, r_=r_: e.activation(out=r_[:], in_=bk_[:, :], func=AF.Relu), r=[bk_], w=[r_])
                k.op("dve", lambda e, r_=r_, ch=ch: e.tensor_tensor(out=uT[:, ch, :], in0=r_[:], in1=r_[:], op=ALU.mult), r=[r_], w=[(uT, ch)])
                if with_s:
                    bs_ = k.bank()
                    for kk in range(16):
                        k.op("pe", lambda e, kk=kk, bs_=bs_, j=j: e.matmul(bs_[:, 0:TS], lhsT=wt[:, kk, j * 128:(j + 1) * 128], rhs=h2Ts[:, kk, :], start=(kk == 0), stop=(kk == 15)),
                             r=[wt, h2Ts], w=[bs_])
                    k.op("act", lambda e, bs_=bs_, ch=ch: e.activation(out=uTs[:, ch, :], in_=bs_[:, 0:TS], func=AF.Relu), r=[bs_], w=[(uTs, ch)])
                    k.op("dve", lambda e, ch=ch: e.tensor_tensor(out=uTs[:, ch, :], in0=uTs[:, ch, :], in1=uTs[:, ch, :], op=ALU.mult), r=[(uTs, ch)], w=[(uTs, ch)])
        for g in range(16):
            phaseA(g)
        def phaseB(qc):
            accs = [k.bank() for _ in range(4)]
            accS = k.bank() if with_s else None
            k.reserved = {a_.name for a_ in accs} | ({accS.name} if with_s else set())
            for fgg in range(8):
                w2 = w2b[w2cnt[0] % 2]
                w2cnt[0] += 1
                k.dma("pool", w2[:], w2s[qc, fgg], r=[("w2s", qc, fgg)], w=[w2])
                for c in range(8):
                    ch = fgg * 8 + c
                    first = (fgg == 0 and c == 0)
                    last = (fgg == 7 and c == 7)
                    for tt in range(4):
                        k.op("pe", lambda e, tt=tt, c=c, ch=ch, first=first, last=last, w2=w2: e.matmul(accs[tt][:, :], lhsT=uT[:, ch, tt * 128:(tt + 1) * 128], rhs=w2[:, c, :], start=first, stop=last),
                             r=[(uT, ch), w2], w=[accs[tt]])
                    if with_s:
                        k.op("pe", lambda e, c=c, ch=ch, first=first, last=last, w2=w2: e.matmul(accS[0:TS, :], lhsT=uTs[:, ch, :], rhs=w2[:, c, :], start=first, stop=last),
                             r=[(uTs, ch), w2], w=[accS])
            for tt in range(4):
                k.op("act", lambda e, tt=tt: e.copy(out=fbuf[tt][:, qc * 512:(qc + 1) * 512], in_=accs[tt][:, :]), r=[accs[tt]], w=[(fbuf[tt], qc)])
            if with_s:
                k.op("act", lambda e: e.copy(out=fs[0:TS, qc * 512:(qc + 1) * 512], in_=accS[0:TS, :]), r=[accS], w=[(fs, qc)])
            k.reserved = set()
        for qc in range(4):
            phaseB(qc)
        def epi(fb, n, x1src, ydst, G2_, i):
            x_ = x1t[i % 2]
            s_ = st6[i % 2]
            k.dma("sp", x_[0:n, :], x1src, r=["x1d"], w=[x_])
            fk = [(fb, q) for q in range(4)]
            k.op("act", lambda e: e.activation(out=h2Tb[:].rearrange("p a b -> p (a b)")[0:n, 0:D], in_=fb[0:n, :], func=AF.Square, accum_out=s_[0:n, 0:1]), r=fk, w=[h2Tb, s_])
            k.op("act", lambda e: e.activation(out=s_[0:n, 1:2], in_=s_[0:n, 0:1], func=AF.Sqrt, scale=1.0 / D, bias=EPS), r=[s_], w=[s_])
            k.op("dve", lambda e: e.reciprocal(out=s_[0:n, 2:3], in_=s_[0:n, 1:2]), r=[s_], w=[s_])
            k.op("dve", lambda e: e.scalar_tensor_tensor(out=fb[0:n, :], in0=fb[0:n, :], scalar=s_[0:n, 2:3], op0=ALU.mult, in1=G2_[0:n, :], op1=ALU.mult), r=fk + [s_, G2_], w=fk)
            k.op("pool", lambda e: e.tensor_tensor(out=x_[0:n, :], in0=x_[0:n, :], in1=fb[0:n, :], op=ALU.add), r=[x_] + fk, w=[x_])
            k.dma("sp", ydst, x_[0:n, :], r=[x_])
        for tt in range(4):
            r0 = b * TB + tt * 128
            epi(fbuf[tt], 128, x1d[r0:r0 + 128, :], y_p[r0:r0 + 128, :], G2b, tt)
        if with_s:
            k.dma("sp", G2b[0:TS, :], modd[1:5, 5 * D:6 * D], r=["modd"], w=[G2b])
            epi(fs, TS, x1d[T:TT, :], y_s.ap(), G2b, 0)

    fs = k.alloc("fs", [TS, D], F32)
    import os
    for b in range(int(os.environ.get("NBLK", str(NBLK6)))):
        ffn_block(b if "NBLK" not in os.environ else NBLK6 - 1 - b)
    k.barrier()
    return k.finish()


_CACHE = {}


def _core_inputs(i, a):
    f = np.ascontiguousarray
    return {
        "xp": f(a["x_prompt"][i]),
        "xs": f(a["x_sample"][4 * i:4 * i + 4, 0, :]),
        "c5": f(np.concatenate([a["c_prompt"][i:i + 1], a["c_sample"][4 * i:4 * i + 4]], axis=0)),
        "w_ada": f(a["w_ada"][0]),
        "b_ada": f(a["b_ada"][0][None, :]),
        "gvec": f(np.stack([a["pre1_g"][0], a["post1_g"][0], a["pre2_g"][0], a["post2_g"][0]])),
        "w_in": f(a["w_in"][0]),
        "w_out": f(a["w_out"][0]),
        "w_ff1": f(a["w_ff1"][0]),
        "w_ff2": f(a["w_ff2"][0]),
        "conv_w": f(a["conv_w"][0]),
        "st_conv": f(a["state_conv"][0, 4 * i:4 * i + 4].reshape(12, 3072)),
        "hv": f(np.concatenate([a["a_log"][0], a["dt_bias"][0]])[None, :]),
        "ln_gb": f(np.stack([a["idx_knorm_g"][0], a["idx_knorm_b"][0]])),
        "gdn_g": f(a["gdn_norm_g"][0][None, :]),
        "st_ssm": f(a["state_ssm"][0, 4 * i:4 * i + 4]),
        "rel_bias": f(a["rel_bias"]),
        "boh": _boh(),
        "bthr": _bthr(),
        "page_table": f(a["page_table"][4 * i:4 * i + 4]),
        "cache_kidx": a["cache_kidx"][0].reshape(-1, PAGE * 128),
        "cache_k": a["cache_k"][0].reshape(-1, 256),
        "cache_v": a["cache_v"][0].reshape(-1, 256),
    }


def kernel(**inputs):
    n = 8
    nc = build(n_pool=int(inputs["cache_k"].shape[1]))
    in_maps = [_core_inputs(i, inputs) for i in range(n)]
    res = run_bass_kernel_spmd(nc, in_maps, core_ids=list(range(n)))
    R = res.results
    cat = lambda name: np.stack([r[name] for r in R])
    y_p = cat("y_p")
    y_s = np.concatenate([r["y_s"] for r in R])[:, None, :]
    k_p = cat("k_p").reshape(1, 8, T, 2, 128)
    v_p = cat("v_p").reshape(1, 8, T, 2, 128)
    ki_p = cat("ki_p")[None]
    ssm_p = cat("ssm_p")[None]
    conv_p = cat("conv_p")[None]
    k_s = np.concatenate([r["k_s"] for r in R]).reshape(1, 32, 1, 2, 128)
    v_s = np.concatenate([r["v_s"] for r in R]).reshape(1, 32, 1, 2, 128)
    ki_s = np.concatenate([r["ki_s"] for r in R]).reshape(1, 32, 1, 128)
    ssm_s = np.concatenate([r["ssm_s"] for r in R])[None]
    conv_s = np.concatenate([r["conv_s"] for r in R])[None]
    return (y_p, y_s, k_p, v_p, ki_p, ssm_p, conv_p, k_s, v_s, ki_s, ssm_s, conv_s)
```

```python
import math
import numpy as np
import concourse.bass as bass
import concourse.mybir as mybir
from concourse.bass_utils import run_bass_kernel_spmd

F32 = mybir.dt.float32
BF16 = mybir.dt.bfloat16
I32 = mybir.dt.int32
ALU = mybir.AluOpType
AF = mybir.ActivationFunctionType
AX = mybir.AxisListType

ENGS = ("pe", "dve", "act", "pool", "sp")

D = 2048
T = 2048
TS = 4
TT = T + TS
NT = 16
NPROJ = 7840
DFF = 8192
EPS = 1e-6
NPAGES = 128
PAGE = 128
O_CONV, O_A, O_B, O_Z, O_QB, O_KB, O_VB, O_QI, O_WI, O_KI = 0, 3072, 3080, 3088, 4112, 5136, 5392, 5648, 7696, 7712


def _dsize(dt):
    return {F32: 4, BF16: 2, I32: 4}[dt]


class Buf:
    def __init__(self, name, ap):
        self.name = name
        self.ap = ap

    def __getitem__(self, key):
        return self.ap[key]


class KB:
    def __init__(self, n_dma_sems=(24, 8, 52)):
        self.nc = bass.Bass("TRN2", target_bir_lowering=False)
        nc = self.nc
        self.ops = {e: [] for e in ENGS}
        self._ctx = []
        self.psem = {}
        self.cnt = {e: 0 for e in ENGS}
        for e in ENGS:
            self.psem[e] = self._enter(nc.semaphore("p_" + e))
        self.dsem = {}
        self.dcnt = {}
        self.drr = {}
        for q, n in zip(("sp", "act", "pool"), n_dma_sems):
            self.dsem[q] = [self._enter(nc.semaphore(f"d_{q}{i}")) for i in range(n)]
            self.dcnt[q] = [0] * n
            self.drr[q] = 0
        self.known = {e: {} for e in ENGS}
        self.state = {}
        self.semobj = {}
        for e in ENGS:
            self.semobj[("p", e)] = self.psem[e]
        for q in self.dsem:
            for i, s in enumerate(self.dsem[q]):
                self.semobj[("d", q, i)] = s
        self.n_ops = 0
        self.arena = None
        self.aoff = 0
        self.awords = 0
        self.nbank = 0
        self.reserved = set()

    def _enter(self, cm):
        v = cm.__enter__()
        self._ctx.append(cm)
        return v

    def init_arena(self, words):
        self.arena = self._enter(self.nc.sbuf_tensor("arena", [128, words], F32))
        self.awords = words
        self.aoff = 0

    def alloc(self, name, shape, dt=F32):
        p = shape[0]
        n = int(np.prod(shape[1:]))
        words = (n * _dsize(dt) + 3) // 4
        words = (words + 7) // 8 * 8
        assert self.aoff + words <= self.awords, f"arena overflow at {name}: {self.aoff + words} > {self.awords}"
        ap = self.arena[0:p, self.aoff:self.aoff + words]
        self.aoff += words
        if dt != F32:
            ap = ap.bitcast(dt)
        ap = ap[:, 0:n]
        if len(shape) == 3:
            ap = ap.rearrange("p (a b) -> p a b", a=shape[1])
        elif len(shape) == 4:
            ap = ap.rearrange("p (a b c) -> p a b c", a=shape[1], b=shape[2])
        return Buf(name, ap)

    def mark(self):
        return self.aoff

    def release(self, m):
        self.aoff = m

    def psum_init(self):
        self.pbanks = []
        for i in range(4):
            t = self._enter(self.nc.psum_tensor(f"pp{i}", [128, 1024], F32))
            self.pbanks.append(Buf(f"bank{2 * i}", t[:, 0:512]))
            self.pbanks.append(Buf(f"bank{2 * i + 1}", t[:, 512:1024]))
        self.pdbl = [self._dbl(i) for i in range(4)]

    def _dbl(self, i):
        return None

    def bank(self):
        while True:
            b = self.pbanks[self.nbank % 8]
            self.nbank += 1
            if b.name not in self.reserved:
                return b

    @staticmethod
    def _key(x):
        if isinstance(x, tuple):
            return (KB._key(x[0]),) + tuple(x[1:])
        if isinstance(x, str):
            return x
        return x.name

    def _collect(self, eng, r, w):
        need = {}
        own = ("p", eng)

        def add(tok):
            if tok is None:
                return
            sk, v = tok
            if sk == own and eng == "pe":
                return
            if need.get(sk, 0) < v:
                need[sk] = v

        for x in r:
            st = self.state.get(self._key(x))
            if st:
                add(st[0])
        for x in w:
            st = self.state.get(self._key(x))
            if st:
                add(st[0])
                for t in st[1]:
                    add(t)
        waits = []
        kn = self.known[eng]
        for sk, v in need.items():
            if kn.get(sk, 0) >= v:
                continue
            kn[sk] = v
            waits.append((sk, v))
        return waits

    def _update(self, tok, r, w):
        for x in w:
            self.state[self._key(x)] = [tok, []]
        for x in r:
            kk = self._key(x)
            st = self.state.get(kk)
            if st is None:
                st = self.state[kk] = [None, []]
            st[1].append(tok)
            if len(st[1]) > 24:
                best = {}
                for sk, v in st[1]:
                    if best.get(sk, 0) < v:
                        best[sk] = v
                st[1] = list(best.items())

    def op(self, eng, fn, r=(), w=()):
        waits = self._collect(eng, r, w)
        self.cnt[eng] += 1
        tok = (("p", eng), self.cnt[eng])
        self.ops[eng].append((waits, fn, (("p", eng), 1)))
        self._update(tok, r, w)
        self.n_ops += 1
        return tok

    def dma(self, q, out, in_, r=(), w=(), slow=False):
        if slow:
            fn = lambda e: e.dma_start(out=out, in_=in_, allow_slow_non_contiguous=True)
        else:
            fn = lambda e: e.dma_start(out=out, in_=in_)
        i = self.drr[q]
        self.drr[q] = (i + 1) % len(self.dsem[q])
        sk = ("d", q, i)
        waits = self._collect(q, r, w)
        prev = self.dcnt[q][i]
        kn = self.known[q]
        if prev > 0 and kn.get(sk, 0) < prev:
            kn[sk] = prev
            waits.append((sk, prev))
        self.dcnt[q][i] = prev + 16
        tok = (sk, prev + 16)
        self.ops[q].append((waits, fn, (sk, 16)))
        self._update(tok, r, w)
        self.n_ops += 1
        return tok

    def barrier(self):
        toks = [(("p", e), self.cnt[e]) for e in ENGS if self.cnt[e] > 0]
        for q in self.dsem:
            for i, c in enumerate(self.dcnt[q]):
                if c > 0:
                    toks.append((("d", q, i), c))
        for e in ENGS:
            waits = []
            kn = self.known[e]
            for sk, v in toks:
                if sk == ("p", e):
                    continue
                if kn.get(sk, 0) < v:
                    kn[sk] = v
                    waits.append((sk, v))
            if waits:
                self.ops[e].append((waits, None, None))

    def check_deadlock(self):
        sem = {}
        ptr = {e: 0 for e in ENGS}
        progress = True
        while progress:
            progress = False
            for e in ENGS:
                lst = self.ops[e]
                while ptr[e] < len(lst):
                    waits, fn, inc = lst[ptr[e]]
                    if all(sem.get(sk, 0) >= v for sk, v in waits):
                        if inc is not None:
                            sem[inc[0]] = sem.get(inc[0], 0) + inc[1]
                        ptr[e] += 1
                        progress = True
                    else:
                        break
        stuck = {e: (ptr[e], len(self.ops[e])) for e in ENGS if ptr[e] < len(self.ops[e])}
        if stuck:
            for e in stuck:
                waits, fn, inc = self.ops[e][ptr[e]]
                print("STUCK", e, ptr[e], [(sk, v, sem.get(sk, 0)) for sk, v in waits])
            raise RuntimeError(f"deadlock in sync graph: {stuck}")

    def finish(self):
        self.barrier()
        self.check_deadlock()
        nc = self.nc
        ops = self.ops
        semobj = self.semobj

        def run(e, lst):
            for waits, fn, inc in lst:
                for sk, v in waits:
                    e.wait_ge(semobj[sk], v)
                if fn is not None:
                    ins = fn(e)
                    ins.then_inc(semobj[inc[0]], inc[1])

        with nc.Block() as block:
            @block.tensor
            def _(e):
                run(e, ops["pe"])

            @block.vector
            def _(e):
                run(e, ops["dve"])

            @block.scalar
            def _(e):
                run(e, ops["act"])

            @block.gpsimd
            def _(e):
                run(e, ops["pool"])

            @block.sync
            def _(e):
                run(e, ops["sp"])

        for cm in reversed(self._ctx):
            cm.__exit__(None, None, None)
        self._ctx = []
        return nc


def _t5_bucket_table():
    n = np.arange(0, 256, dtype=np.int32)
    max_exact = 16
    nf = np.maximum(n, 1).astype(np.float32)
    large = max_exact + (np.log(nf / np.float32(max_exact)) / np.float32(math.log(128 / max_exact))
                         * np.float32(32 - max_exact)).astype(np.int32)
    large = np.minimum(large, 31)
    return np.where(n < max_exact, n, large)


def _boh():
    tab = _t5_bucket_table()
    oh = np.zeros((32, 384), np.float32)
    for j in range(384):
        dist = min(max(j - 127, 0), 255)
        oh[tab[dist], j] = 1.0
    return oh


def _bthr():
    tab = _t5_bucket_table()
    thr = np.zeros((1, 31), np.float32)
    for kk in range(1, 32):
        nz = np.nonzero(tab >= kk)[0]
        thr[0, kk - 1] = float(nz[0]) if len(nz) else 1e9
    return thr


def build(stop_after=None, n_pool=5120, skip_p4s=False):
    k = KB()
    nc = k.nc

    def din(name, shape, dt=F32):
        return nc.dram_tensor(name, list(shape), dt, kind="ExternalInput")

    def dout(name, shape, dt=F32):
        return nc.dram_tensor(name, list(shape), dt, kind="ExternalOutput")

    xp = din("xp", [T, D])
    xs = din("xs", [TS, D])
    c5 = din("c5", [5, D])
    w_ada = din("w_ada", [D, 6 * D])
    b_ada = din("b_ada", [1, 6 * D])
    gvec = din("gvec", [4, D])
    w_in = din("w_in", [D, NPROJ])
    w_out = din("w_out", [D, D])
    w_ff1 = din("w_ff1", [D, DFF])
    w_ff2 = din("w_ff2", [DFF, D])
    conv_w = din("conv_w", [4, 3072])
    st_conv = din("st_conv", [TS * 3, 3072])
    hv = din("hv", [1, 16])
    ln_gb = din("ln_gb", [2, 128])
    gdn_g = din("gdn_g", [1, 128])
    st_ssm = din("st_ssm", [TS, 8, 128, 128])
    rel_bias = din("rel_bias", [32, 8])
    boh = din("boh", [32, 384])
    bthr = din("bthr", [1, 31])
    page_table = din("page_table", [TS, NPAGES], I32)
    cache_kidx = din("cache_kidx", [n_pool, PAGE * 128])
    cache_k = din("cache_k", [n_pool * PAGE, 256])
    cache_v = din("cache_v", [n_pool * PAGE, 256])

    y_p = dout("y_p", [T, D])
    y_s = dout("y_s", [TS, D])
    k_p = dout("k_p", [T, 256])
    v_p = dout("v_p", [T, 256])
    ki_p = dout("ki_p", [T, 128])
    ssm_p = dout("ssm_p", [8, 128, 128])
    conv_p = dout("conv_p", [3, 3072])
    k_s = dout("k_s", [TS, 256])
    v_s = dout("v_s", [TS, 256])
    ki_s = dout("ki_s", [TS, 128])
    ssm_s = dout("ssm_s", [TS, 8, 128, 128])
    conv_s = dout("conv_s", [TS, 3, 3072])

    modd = nc.dram_tensor("modd", [5, 6 * D], F32)
    gq = nc.dram_tensor("gq", [8, 128, TT], BF16)
    gk = nc.dram_tensor("gk", [8, 128, TT], BF16)
    gv = nc.dram_tensor("gv", [8, 128, TT], BF16)
    gz = nc.dram_tensor("gz", [TT, 1024], BF16)
    gab = nc.dram_tensor("gab", [TT, 16], F32)
    aq = nc.dram_tensor("aq", [8, 128, TT], BF16)
    akT = nc.dram_tensor("akT", [2, 128, TT], BF16)
    av = nc.dram_tensor("av", [TT, 256], BF16)
    iq = nc.dram_tensor("iq", [16, 128, TT], BF16)
    iw = nc.dram_tensor("iw", [TT, 16], F32)
    ikTd = nc.dram_tensor("ikTd", [128, TT], BF16)
    biasd = nc.dram_tensor("biasd", [8, 384], F32)
    rbTd = nc.dram_tensor("rbTd", [8, 32], F32)
    w1s = nc.dram_tensor("w1s", [16, 128, 16, 512], BF16)
    w2s = nc.dram_tensor("w2s", [4, 8, 128, 8, 512], BF16)
    x1d = nc.dram_tensor("x1d", [TT, D], F32)
    h2Td = nc.dram_tensor("h2Td", [128, 16, TT], BF16)

    k.init_arena(47 * 1024)
    k.psum_init()

    ident_f = k.alloc("ident_f", [128, 128], F32)
    ident_b = k.alloc("ident_b", [128, 128], BF16)
    ones_b = k.alloc("ones_b", [128, 128], BF16)
    k.op("pool", lambda e: e.memset(ident_f[:], 1.0), w=[ident_f])
    k.op("pool", lambda e: e.affine_select(out=ident_f[:], in_=ident_f[:], pattern=[[-1, 128]],
                                           compare_op=ALU.is_equal, fill=0.0, base=0, channel_multiplier=1),
         r=[ident_f], w=[ident_f])
    k.op("pool", lambda e: e.tensor_copy(out=ident_b[:], in_=ident_f[:]), r=[ident_f], w=[ident_b])
    k.op("pool", lambda e: e.memset(ones_b[:], 1.0), w=[ones_b])

    wb = []
    wcnt = [0]

    def alloc_wb():
        wb.clear()
        wb.extend(k.alloc(f"wb{i}", [128, 16, 512], BF16) for i in range(2))

    def load_w(src_dram, c0, ncols):
        b = wb[wcnt[0] % 2]
        wcnt[0] += 1
        k.dma("pool", b[:, :, 0:ncols], src_dram[:, c0:c0 + ncols].rearrange("(k p) n -> p k n", p=128), w=[b])
        return b

    m0 = k.mark()
    alloc_wb()
    c80 = k.alloc("c80", [80, 128], F32)
    cT = k.alloc("cT", [128, 16, 5], BF16)
    mod = k.alloc("mod", [5, 6 * D], F32)
    gv5 = k.alloc("gv5", [5, 4, D], F32)
    k.dma("sp", c80[:], c5.ap().rearrange("r (k p) -> (r k) p", p=128), w=[c80])
    k.dma("sp", mod[:], b_ada.ap().to_broadcast([5, 6 * D]), w=[mod])
    k.dma("sp", gv5[:].rearrange("p a b -> p (a b)"), gvec.ap().rearrange("a b -> (a b)").unsqueeze(0).to_broadcast([5, 4 * D]), w=[gv5])
    bk = k.bank()
    k.op("pe", lambda e: e.transpose(out=bk[:, 0:80], in_=c80[:], identity=ident_f[0:80, 0:80]), r=[c80, ident_f], w=[bk])
    k.op("act", lambda e: e.activation(out=cT[:], in_=bk[:, 0:80].rearrange("p (r k) -> p k r", r=5), func=AF.Silu), r=[bk], w=[cT])
    for n in range(24):
        wt = load_w(w_ada, n * 512, 512)
        bk = k.bank()
        for kk in range(16):
            k.op("pe", lambda e, kk=kk, wt=wt, bk=bk: e.matmul(bk[0:5, :], lhsT=cT[:, kk, :], rhs=wt[:, kk, :], start=(kk == 0), stop=(kk == 15)),
                 r=[cT, wt], w=[bk])
        k.op("dve", lambda e, n=n, bk=bk: e.tensor_tensor(out=mod[:, n * 512:(n + 1) * 512], in0=bk[0:5, :], in1=mod[:, n * 512:(n + 1) * 512], op=ALU.add),
             r=[bk, mod], w=[mod])
    for (sc, gi) in ((1, 0), (4, 2)):
        k.op("dve", lambda e, sc=sc, gi=gi: e.scalar_tensor_tensor(out=mod[:, sc * D:(sc + 1) * D], in0=mod[:, sc * D:(sc + 1) * D], scalar=1.0, op0=ALU.add,
                                                                    in1=gv5[:, gi, :], op1=ALU.mult), r=[mod, gv5], w=[mod])
    for (g, gi) in ((2, 1), (5, 3)):
        k.op("dve", lambda e, g=g, gi=gi: e.tensor_tensor(out=mod[:, g * D:(g + 1) * D], in0=mod[:, g * D:(g + 1) * D], in1=gv5[:, gi, :], op=ALU.mult),
             r=[mod, gv5], w=[mod])
    k.dma("sp", modd.ap(), mod[:], r=[mod], w=["modd"])
    k.barrier()
    k.release(m0)
    if stop_after == "P0":
        return k.finish()

    def load_mod_bcast(buf, idx):
        k.dma("sp", buf[:], modd[0:1, idx * D:(idx + 1) * D].to_broadcast([128, D]), r=["modd"], w=[buf])

    def load_mod_rows(buf, idx):
        k.dma("sp", buf[:], modd[1:5, idx * D:(idx + 1) * D], r=["modd"], w=[buf])

    mP = k.mark()
    hT = k.alloc("hT", [128, 16, TT], BF16)
    ikT = k.alloc("ikT", [128, TT], BF16)
    m1 = k.mark()
    A1 = k.alloc("A1", [128, D], F32)
    SH1 = k.alloc("SH1", [128, D], F32)
    A1s = k.alloc("A1s", [TS, D], F32)
    SH1s = k.alloc("SH1s", [TS, D], F32)
    load_mod_bcast(SH1, 0)
    load_mod_bcast(A1, 1)
    load_mod_rows(SH1s, 0)
    load_mod_rows(A1s, 1)
    xt = [k.alloc(f"xt{i}", [128, D], F32) for i in range(2)]
    hb = [k.alloc(f"hb{i}", [128, D], BF16) for i in range(2)]
    junk = k.alloc("junk", [128, D], BF16)
    st1 = [k.alloc(f"st1_{i}", [128, 4], F32) for i in range(2)]

    def norm_mod(i, np_, src_ap, A, SH, col0, ncol):
        x_ = xt[i % 2]
        h_ = hb[i % 2]
        s_ = st1[i % 2]
        k.dma("sp", x_[0:np_, :], src_ap, w=[x_])
        k.op("act", lambda e: e.activation(out=junk[0:np_, :], in_=x_[0:np_, :], func=AF.Square, accum_out=s_[0:np_, 0:1]), r=[x_], w=[junk, s_])
        k.op("act", lambda e: e.activation(out=s_[0:np_, 1:2], in_=s_[0:np_, 0:1], func=AF.Sqrt, scale=1.0 / D, bias=EPS), r=[s_], w=[s_])
        k.op("dve", lambda e: e.reciprocal(out=s_[0:np_, 2:3], in_=s_[0:np_, 1:2]), r=[s_], w=[s_])
        k.op("dve", lambda e: e.scalar_tensor_tensor(out=x_[0:np_, :], in0=x_[0:np_, :], scalar=s_[0:np_, 2:3], op0=ALU.mult, in1=A[0:np_, :], op1=ALU.mult),
             r=[x_, s_, A], w=[x_])
        k.op("pool", lambda e: e.tensor_tensor(out=h_[0:np_, :], in0=x_[0:np_, :], in1=SH[0:np_, :], op=ALU.add), r=[x_, SH], w=[h_])
        b0 = k.bank()
        b1 = k.bank()
        for kk in range(16):
            bb = b0 if kk < 8 else b1
            k.op("pe", lambda e, kk=kk, bb=bb: e.transpose(out=bb[:].bitcast(BF16)[:, (kk % 8) * 128:(kk % 8) * 128 + np_],
                                                           in_=h_[0:np_, kk * 128:(kk + 1) * 128], identity=ident_b[0:np_, 0:np_]),
                 r=[h_, ident_b], w=[bb])
        for half, bb in ((0, b0), (1, b1)):
            k.op("act", lambda e, half=half, bb=bb: e.copy(out=hT[:, half * 8:(half + 1) * 8, col0:col0 + ncol],
                                                          in_=bb[:].bitcast(BF16).rearrange("p (a b) -> p a b", a=8)[:, :, 0:ncol]),
                 r=[bb], w=[(hT, col0)])

    for i in range(NT):
        norm_mod(i, 128, xp[i * 128:(i + 1) * 128, :], A1, SH1, i * 128, 128)
    norm_mod(NT, TS, xs.ap(), A1s, SH1s, T, TS)
    k.barrier()
    k.release(m1)
    if stop_after == "P1":
        dbg = dout("dbg_hT", [128, 16 * TT], BF16)
        k.dma("sp", dbg.ap(), hT[:].rearrange("p a b -> p (a b)"), r=[hT])
        return k.finish()

    hT_keys = [(hT, i * 128) for i in range(NT)] + [(hT, T)]

    m2 = k.mark()
    alloc_wb()
    cw = k.alloc("cw", [128, 4, 24], F32)
    stc = k.alloc("stc", [128, TS * 3, 24], F32)
    cwl = k.alloc("cwl", [96, 128], F32)
    stl = k.alloc("stl", [96, 3, 128], F32)
    k.dma("sp", cwl[:], conv_w.ap().rearrange("j (ch c) -> (j ch) c", c=128), w=[cwl])
    k.dma("sp", stl[:], st_conv.ap().rearrange("r (ch c) -> (r ch) c", c=128).rearrange("(g p) c -> p g c", p=96), w=[stl])
    bk = k.bank()
    k.op("pe", lambda e, bk=bk: e.transpose(out=bk[:, 0:96], in_=cwl[:], identity=ident_f[0:96, 0:96]), r=[cwl, ident_f], w=[bk])
    k.op("act", lambda e, bk=bk: e.copy(out=cw[:].rearrange("p a b -> p (a b)"), in_=bk[:, 0:96]), r=[bk], w=[cw])
    bk = k.bank()
    for g in range(3):
        k.op("pe", lambda e, g=g, bk=bk: e.transpose(out=bk[:, g * 96:(g + 1) * 96], in_=stl[:, g, :], identity=ident_f[0:96, 0:96]),
             r=[stl, ident_f], w=[bk])
    k.op("act", lambda e, bk=bk: e.copy(out=stc[:].rearrange("p a b -> p (a b)"), in_=bk[:, 0:288]), r=[bk], w=[stc])
    k.dma("sp", conv_s.ap()[:, 0:2, :], st_conv.ap().rearrange("(s j) c -> s j c", j=3)[:, 1:3, :])

    cin = [k.alloc(f"cin{i}", [128, 3 + T], F32) for i in range(2)]
    for cb in cin:
        k.op("pool", lambda e, cb=cb: e.memset(cb[:, 0:3], 0.0), w=[cb])
    acc = [k.alloc(f"acc{i}", [128, TT], F32) for i in range(2)]
    cins = [k.alloc(f"cins{i}", [128, TS, 4], F32) for i in range(2)]
    tmp4 = [k.alloc(f"tmp4{i}", [128, TS, 4], F32) for i in range(2)]
    sqb = [k.alloc(f"sqb{i}", [128, TT], BF16) for i in range(2)]
    rsb = [k.alloc(f"rsb{i}", [128, TT], F32) for i in range(2)]
    ob = [k.alloc(f"ob{i}", [128, TT], BF16) for i in range(2)]
    obc = [0]

    def fm_matmuls(wt, wc0, tg, bk):
        c0, n = (tg * 512, 512) if tg < 4 else (T, TS)
        keys = hT_keys[tg * 4:(tg + 1) * 4] if tg < 4 else [hT_keys[16]]
        for kk in range(16):
            k.op("pe", lambda e, kk=kk: e.matmul(bk[:, 0:n], lhsT=wt[:, kk, wc0:wc0 + 128], rhs=hT[:, kk, c0:c0 + n], start=(kk == 0), stop=(kk == 15)),
                 r=[wt] + keys, w=[bk])

    def next_ob():
        b = ob[obc[0] % 2]
        obc[0] += 1
        return b

    chunk_i = [0]

    def conv_chunk(wt, wc0, ch):
        ci = chunk_i[0]
        chunk_i[0] += 1
        cb = cin[ci % 2]
        ac = acc[ci % 2]
        cs = cins[ci % 2]
        t4 = tmp4[ci % 2]
        for tg in range(4):
            bk = k.bank()
            fm_matmuls(wt, wc0, tg, bk)
            k.op("act", lambda e, bk=bk, tg=tg: e.copy(out=cb[:, 3 + tg * 512:3 + (tg + 1) * 512], in_=bk[:, 0:512]), r=[bk], w=[cb])
        bk = k.bank()
        fm_matmuls(wt, wc0, 4, bk)
        k.op("dve", lambda e: e.tensor_copy(out=cs[:, :, 0:3], in_=stc[:, :, ch].rearrange("p (s j) -> p s j", j=3)), r=[stc], w=[cs])
        k.op("dve", lambda e, bk=bk: e.tensor_copy(out=cs[:, :, 3:4], in_=bk[:, 0:TS].unsqueeze(2)), r=[bk, cs], w=[cs])
        k.dma("sp", conv_p.ap()[:, ch * 128:(ch + 1) * 128].rearrange("r c -> c r"), cb[:, T:T + 3], r=[cb], slow=True)
        k.dma("sp", conv_s.ap()[:, 2, ch * 128:(ch + 1) * 128].rearrange("s c -> c s"), cs[:, :, 3], r=[cs], slow=True)
        k.op("dve", lambda e: e.tensor_scalar(out=ac[:, 0:T], in0=cb[:, 0:T], scalar1=cw[:, 0, ch:ch + 1], scalar2=None, op0=ALU.mult), r=[cb, cw], w=[ac])
        for j in range(1, 4):
            k.op("dve", lambda e, j=j: e.scalar_tensor_tensor(out=ac[:, 0:T], in0=cb[:, j:j + T], scalar=cw[:, j, ch:ch + 1], op0=ALU.mult, in1=ac[:, 0:T], op1=ALU.add),
                 r=[cb, cw, ac], w=[ac])
        k.op("dve", lambda e: e.tensor_tensor(out=t4[:], in0=cs[:], in1=cw[:, :, ch].unsqueeze(1).to_broadcast([128, TS, 4]), op=ALU.mult), r=[cs, cw], w=[t4])
        k.op("dve", lambda e: e.tensor_reduce(out=ac[:, T:TT], in_=t4[:], axis=AX.X, op=ALU.add), r=[t4, ac], w=[ac])
        o = next_ob()
        if ch >= 16:
            k.op("act", lambda e: e.activation(out=o[:], in_=ac[:], func=AF.Silu), r=[ac], w=[o])
            k.dma("sp", gv[ch - 16], o[:], r=[o], w=["gv"])
            return
        sq = sqb[ci % 2]
        rs = rsb[ci % 2]
        k.op("act", lambda e: e.activation(out=ac[:], in_=ac[:], func=AF.Silu), r=[ac], w=[ac])
        k.op("act", lambda e: e.activation(out=sq[:], in_=ac[:], func=AF.Square), r=[ac], w=[sq])
        for tg in range(5):
            c0, n = (tg * 512, 512) if tg < 4 else (T, TS)
            bk = k.bank()
            k.op("pe", lambda e, bk=bk, c0=c0, n=n: e.matmul(bk[:, 0:n], lhsT=ones_b[:], rhs=sq[:, c0:c0 + n], start=True, stop=True), r=[sq, ones_b], w=[bk])
            k.op("act", lambda e, bk=bk, c0=c0, n=n: e.activation(out=rs[:, c0:c0 + n], in_=bk[:, 0:n], func=AF.Sqrt, bias=EPS, scale=1.0), r=[bk], w=[rs])
        k.op("dve", lambda e: e.reciprocal(out=rs[:], in_=rs[:]), r=[rs], w=[rs])
        scl = 128.0 ** -0.5 if ch < 8 else 1.0
        k.op("dve", lambda e: e.scalar_tensor_tensor(out=o[:], in0=ac[:], scalar=scl, op0=ALU.mult, in1=rs[:], op1=ALU.mult), r=[ac, rs], w=[o])
        dst = gq[ch] if ch < 8 else gk[ch - 8]
        k.dma("sp", dst, o[:], r=[o], w=["gqk"])

    def plain_chunk(wt, wc0, dst, scale):
        o = next_ob()
        for tg in range(5):
            c0, n = (tg * 512, 512) if tg < 4 else (T, TS)
            bk = k.bank()
            fm_matmuls(wt, wc0, tg, bk)
            k.op("act", lambda e, bk=bk, c0=c0, n=n: e.activation(out=o[:, c0:c0 + n], in_=bk[:, 0:n], func=AF.Copy, scale=scale), r=[bk], w=[o])
        k.dma("sp", dst, o[:], r=[o], w=["plain"])

    for g in range(6):
        wt = load_w(w_in, O_CONV + g * 512, 512)
        for j in range(4):
            conv_chunk(wt, j * 128, g * 4 + j)
    if stop_after == "P2a":
        k.barrier()
        return k.finish()
    for g in range(2):
        wt = load_w(w_in, O_QB + g * 512, 512)
        for j in range(4):
            plain_chunk(wt, j * 128, aq[g * 4 + j], 128.0 ** -0.5)
    for g in range(4):
        wt = load_w(w_in, O_QI + g * 512, 512)
        for j in range(4):
            plain_chunk(wt, j * 128, iq[g * 4 + j], 1.0)
    wt_kv = load_w(w_in, O_KB, 512)
    for j in range(2):
        plain_chunk(wt_kv, j * 128, akT[j], 1.0)

    if stop_after == "P2b":
        k.barrier()
        return k.finish()
    stg_f = [k.alloc(f"stgf{i}", [128, 512], F32) for i in range(2)]
    stg_b = [k.alloc(f"stgb{i}", [128, 512], BF16) for i in range(2)]
    lnw = [k.alloc(f"lnw{i}", [128, 8], F32) for i in range(2)]
    kib = [k.alloc(f"kib{i}", [128, 128], BF16) for i in range(2)]
    lng = k.alloc("lng", [128, 128], F32)
    lnb = k.alloc("lnb", [128, 128], F32)
    k.dma("sp", lng[:], ln_gb[0:1, :].to_broadcast([128, 128]), w=[lng])
    k.dma("sp", lnb[:], ln_gb[1:2, :].to_broadcast([128, 128]), w=[lnb])
    tcnt = [0]

    def tm_tile(wt, ncols, ti, epilogue):
        c0, n = (ti * 128, 128) if ti < NT else (T, TS)
        bk = k.bank()
        for kk in range(16):
            k.op("pe", lambda e, kk=kk: e.matmul(bk[0:n, 0:ncols], lhsT=hT[:, kk, c0:c0 + n], rhs=wt[:, kk, 0:ncols], start=(kk == 0), stop=(kk == 15)),
                 r=[wt, hT_keys[ti]], w=[bk])
        i = tcnt[0]
        tcnt[0] += 1
        epilogue(bk, c0, n, i)

    def ep_kv(bk, c0, n, i):
        sf = stg_f[i % 2]
        sb_ = stg_b[i % 2]
        import os
        dbg = int(os.environ.get("DBG", "0"))
        k.op("act", lambda e: e.copy(out=sf[0:n, :], in_=bk[0:n, :]), r=[bk], w=[sf])
        if dbg != 3:
            k.op("pool", lambda e: e.tensor_copy(out=sb_[0:n, 0:256], in_=sf[0:n, 256:512]), r=[sf], w=[sb_])
        if dbg == 1:
            pass
        elif c0 < T:
            k.dma("sp", k_p[c0:c0 + n, :], sf[0:n, 0:256], r=[sf])
            k.dma("sp", v_p[c0:c0 + n, :], sf[0:n, 256:512], r=[sf])
        else:
            k.dma("sp", k_s.ap(), sf[0:n, 0:256], r=[sf])
            k.dma("sp", v_s.ap(), sf[0:n, 256:512], r=[sf])
        if dbg not in (2, 3):
            k.dma("sp", av[c0:c0 + n, :], sb_[0:n, 0:256], r=[sb_], w=["av"])

    import os
    _d = int(os.environ.get("DBG", "0"))
    for ti in range(0 if _d == 4 else (NT if _d == 5 else NT + 1)):
        tm_tile(wt_kv, 512, ti, ep_kv)

    if stop_after == "P2c":
        k.barrier()
        return k.finish()

    def ep_z(half):
        def ep(bk, c0, n, i):
            sb_ = stg_b[i % 2]
            k.op("act", lambda e: e.copy(out=sb_[0:n, :], in_=bk[0:n, :]), r=[bk], w=[sb_])
            k.dma("sp", gz[c0:c0 + n, half * 512:(half + 1) * 512], sb_[0:n, :], r=[sb_], w=["gz"])
        return ep

    for half in range(2):
        wt = load_w(w_in, O_Z + half * 512, 512)
        for ti in range(NT + 1):
            tm_tile(wt, 512, ti, ep_z(half))

    if stop_after == "P2d":
        k.barrier()
        return k.finish()

    def ep_ab(bk, c0, n, i):
        sf = stg_f[i % 2]
        k.op("act", lambda e: e.copy(out=sf[0:n, 0:16], in_=bk[0:n, 0:16]), r=[bk], w=[sf])
        k.dma("sp", gab[c0:c0 + n, :], sf[0:n, 0:16], r=[sf], w=["gab"])

    wt = load_w(w_in, O_A, 16)
    for ti in range(NT + 1):
        tm_tile(wt, 16, ti, ep_ab)

    if stop_after == "P2e":
        k.barrier()
        return k.finish()

    def ep_wk(bk, c0, n, i):
        sf = stg_f[i % 2]
        s_ = lnw[i % 2]
        kb_ = kib[i % 2]
        k.op("act", lambda e: e.activation(out=sf[0:n, 0:16], in_=bk[0:n, 0:16], func=AF.Copy, scale=0.25), r=[bk], w=[sf])
        k.dma("sp", iw[c0:c0 + n, :], sf[0:n, 0:16], r=[sf], w=["iw"])
        kf = sf[0:n, 128:256]
        k.op("act", lambda e: e.activation(out=kf, in_=bk[0:n, 16:144], func=AF.Copy, accum_out=s_[0:n, 0:1]), r=[bk, sf], w=[sf, s_])
        k.op("dve", lambda e: e.tensor_scalar(out=s_[0:n, 1:2], in0=s_[0:n, 0:1], scalar1=-1.0 / 128, scalar2=None, op0=ALU.mult), r=[s_], w=[s_])
        k.op("dve", lambda e: e.tensor_scalar(out=kf, in0=kf, scalar1=s_[0:n, 1:2], scalar2=None, op0=ALU.add), r=[sf, s_], w=[sf])
        k.op("act", lambda e: e.activation(out=sf[0:n, 256:384], in_=kf, func=AF.Square, accum_out=s_[0:n, 2:3]), r=[sf, s_], w=[sf, s_])
        k.op("act", lambda e: e.activation(out=s_[0:n, 3:4], in_=s_[0:n, 2:3], func=AF.Sqrt, scale=1.0 / 128, bias=EPS), r=[s_], w=[s_])
        k.op("dve", lambda e: e.reciprocal(out=s_[0:n, 4:5], in_=s_[0:n, 3:4]), r=[s_], w=[s_])
        k.op("dve", lambda e: e.scalar_tensor_tensor(out=kf, in0=kf, scalar=s_[0:n, 4:5], op0=ALU.mult, in1=lng[0:n, :], op1=ALU.mult), r=[sf, s_, lng], w=[sf])
        k.op("dve", lambda e: e.tensor_tensor(out=kf, in0=kf, in1=lnb[0:n, :], op=ALU.add), r=[sf, lnb], w=[sf])
        k.op("dve", lambda e: e.tensor_copy(out=kb_[0:n, :], in_=kf), r=[sf], w=[kb_])
        if c0 < T:
            k.dma("sp", ki_p[c0:c0 + n, :], kf, r=[sf])
        else:
            k.dma("sp", ki_s.ap(), kf, r=[sf])
        b2 = k.bank()
        k.op("pe", lambda e: e.transpose(out=b2[:].bitcast(BF16)[:, 0:n], in_=kb_[0:n, :], identity=ident_b[0:n, 0:n]), r=[kb_, ident_b], w=[b2])
        k.op("act", lambda e: e.copy(out=ikT[:, c0:c0 + n], in_=b2[:].bitcast(BF16)[:, 0:n]), r=[b2], w=[(ikT, c0)])

    wt = load_w(w_in, O_WI, 144)
    for ti in range(NT + 1):
        tm_tile(wt, 144, ti, ep_wk)
    k.dma("sp", ikTd.ap(), ikT[:], r=[(ikT, c) for c in range(0, TT, 128)], w=["ikTd"])
    k.barrier()
    k.release(mP)
    if stop_after == "P2":
        return k.finish()
    conv_jobs = []
    for g in range(16):
        conv_jobs.append((w1s[g], w_ff1[:, g * 512:(g + 1) * 512].rearrange("(k p) c -> p k c", p=128), ("w1s", g)))
    for qc in range(4):
        for fgg in range(8):
            conv_jobs.append((w2s[qc, fgg], w_ff2[fgg * 1024:(fgg + 1) * 1024, qc * 512:(qc + 1) * 512].rearrange("(c p) n -> p c n", p=128), ("w2s", qc, fgg)))

    def issue_conv(nj):
        for _ in range(nj):
            if conv_jobs:
                o_, i_, key_ = conv_jobs.pop(0)
                k.dma("pool", o_, i_, w=[key_])
    mixT = k.alloc("mixT", [128, 16, TT], BF16)
    m3 = k.mark()
    HG = 4
    HW = HG * 128
    ones_f = k.alloc("ones_f", [128, 128], F32)
    TRI = k.alloc("TRI", [128, 128], F32)
    POSM = k.alloc("POSM", [128, HG, 128], F32)
    OFFD = k.alloc("OFFD", [128, HG, 128], F32)
    hvb = k.alloc("hvb", [128, 16], F32)
    gnb = k.alloc("gnb", [128, 128], F32)
    k.op("pool", lambda e: e.memset(ones_f[:], 1.0), w=[ones_f])
    k.op("pool", lambda e: e.memset(TRI[:], 1.0), w=[TRI])
    k.op("pool", lambda e: e.affine_select(out=TRI[:], in_=TRI[:], pattern=[[1, 128]], compare_op=ALU.is_ge, fill=0.0, base=0, channel_multiplier=-1),
         r=[TRI], w=[TRI])
    k.op("pool", lambda e: e.memset(POSM[:], 0.0), w=[POSM])
    k.op("pool", lambda e: e.affine_select(out=POSM[:], in_=POSM[:], pattern=[[0, HG], [-1, 128]], compare_op=ALU.is_ge, fill=30000.0, base=0, channel_multiplier=1),
         r=[POSM], w=[POSM])
    k.op("pool", lambda e: e.memset(OFFD[:], 1.0), w=[OFFD])
    k.op("pool", lambda e: e.affine_select(out=OFFD[:], in_=OFFD[:], pattern=[[0, HG], [-1, 128]], compare_op=ALU.not_equal, fill=0.0, base=0, channel_multiplier=1),
         r=[OFFD], w=[OFFD])
    k.dma("sp", hvb[:], hv.ap().to_broadcast([128, 16]), w=[hvb])
    k.dma("sp", gnb[:], gdn_g.ap().to_broadcast([128, 128]), w=[gnb])

    gabt = k.alloc("gabt", [128, NT, 16], F32)
    k.dma("sp", gabt[:], gab[0:T, :].rearrange("(t p) c -> p t c", p=128), r=["gab"], w=[gabt], slow=True)
    nA = k.alloc("nA", [128, 8], F32)
    G_ = k.alloc("G_", [128, NT, 8], F32)
    Bt = k.alloc("Bt", [128, NT, 8], F32)
    NB = k.alloc("NB", [128, NT, 8], F32)
    GC = k.alloc("GC", [128, NT, 8], F32)
    GL = k.alloc("GL", [128, NT, 8], F32)
    EG = k.alloc("EG", [128, NT, 8], F32)
    EGL = k.alloc("EGL", [128, NT, 8], F32)
    EKD = k.alloc("EKD", [128, NT, 8], F32)
    BEG = k.alloc("BEG", [128, NT, 8], F32)
    k.op("act", lambda e: e.activation(out=nA[:], in_=hvb[:, 0:8], func=AF.Exp), r=[hvb], w=[nA])
    k.op("dve", lambda e: e.tensor_scalar(out=nA[:], in0=nA[:], scalar1=-1.0, scalar2=None, op0=ALU.mult), r=[nA], w=[nA])
    k.op("dve", lambda e: e.tensor_tensor(out=G_[:], in0=gabt[:, :, 0:8], in1=hvb[:, 8:16].unsqueeze(1).to_broadcast([128, NT, 8]), op=ALU.add), r=[gabt, hvb], w=[G_])
    k.op("act", lambda e: e.activation(out=G_[:], in_=G_[:], func=AF.Exp), r=[G_], w=[G_])
    k.op("act", lambda e: e.activation(out=G_[:], in_=G_[:], func=AF.Ln, bias=1.0, scale=1.0), r=[G_], w=[G_])
    k.op("dve", lambda e: e.tensor_tensor(out=G_[:], in0=G_[:], in1=nA[:].unsqueeze(1).to_broadcast([128, NT, 8]), op=ALU.mult), r=[G_, nA], w=[G_])
    k.op("act", lambda e: e.activation(out=Bt[:], in_=gabt[:, :, 8:16], func=AF.Sigmoid), r=[gabt], w=[Bt])
    k.op("dve", lambda e: e.tensor_scalar(out=NB[:], in0=Bt[:], scalar1=-1.0, scalar2=None, op0=ALU.mult), r=[Bt], w=[NB])
    bA = k.bank()
    bB = k.bank()
    for t in range(NT):
        k.op("pe", lambda e, t=t: e.matmul(bA[:, t * 8:(t + 1) * 8], lhsT=TRI[:], rhs=G_[:, t, :], start=True, stop=True), r=[TRI, G_], w=[bA])
        k.op("pe", lambda e, t=t: e.matmul(bB[:, t * 8:(t + 1) * 8], lhsT=ones_f[:], rhs=G_[:, t, :], start=True, stop=True), r=[ones_f, G_], w=[bB])
    k.op("act", lambda e: e.copy(out=GC[:].rearrange("p a b -> p (a b)"), in_=bA[:, 0:NT * 8]), r=[bA], w=[GC])
    k.op("act", lambda e: e.copy(out=GL[:].rearrange("p a b -> p (a b)"), in_=bB[:, 0:NT * 8]), r=[bB], w=[GL])
    k.op("act", lambda e: e.activation(out=EG[:], in_=GC[:], func=AF.Exp), r=[GC], w=[EG])
    k.op("act", lambda e: e.activation(out=EGL[:], in_=GL[:], func=AF.Exp), r=[GL], w=[EGL])
    k.op("dve", lambda e: e.tensor_tensor(out=EKD[:], in0=GL[:], in1=GC[:], op=ALU.subtract), r=[GL, GC], w=[EKD])
    k.op("act", lambda e: e.activation(out=EKD[:], in_=EKD[:], func=AF.Exp), r=[EKD], w=[EKD])
    k.op("dve", lambda e: e.tensor_tensor(out=BEG[:], in0=Bt[:], in1=EG[:], op=ALU.mult), r=[Bt, EG], w=[BEG])

    if stop_after == "P3a":
        k.barrier()
        return k.finish()
    m3g = k.mark()
    qT = k.alloc("qT", [128, HG, TT], BF16)
    kT = k.alloc("kT", [128, HG, TT], BF16)
    vT = k.alloc("vT", [128, HG, TT], BF16)
    NSLOT = 2

    def mk_slot(j):
        d = {}
        for nm, dt in (("Dg", F32), ("egT", BF16), ("decay", F32), ("M0", F32), ("M1", F32), ("MT0", F32), ("MT1", F32),
                       ("PT", F32), ("PTb", BF16), ("vbeta", BF16), ("kbg", BF16), ("kdec", BF16), ("u", F32), ("wT", BF16),
                       ("intra", BF16), ("intraT", BF16), ("qg", BF16)):
            d[nm] = k.alloc(f"{nm}_{j}", [128, HG, 128], dt)
        return d

    slots = [mk_slot(j) for j in range(NSLOT)]
    S_ = k.alloc("S_", [128, HG, 128], F32)
    Sb = k.alloc("Sb", [128, HG, 128], BF16)
    vnew = k.alloc("vnew", [128, HG, 128], BF16)
    o_ = k.alloc("o_", [128, HG, 128], F32)
    sq_ = k.alloc("sq_", [128, HG, 128], F32)
    zt = [k.alloc(f"zt{i}", [128, HG, 128], BF16) for i in range(2)]
    zs = k.alloc("zs", [128, HG, 128], F32)
    oa = k.alloc("oa", [128, HG, 128], BF16)
    sst = k.alloc("sst", [128, 3 * HG], F32)

    def fl(b):
        return b[:].rearrange("p a b -> p (a b)")

    def bc_tok(src_ap):
        return src_ap.unsqueeze(2).to_broadcast([128, HG, 128])

    def stageA(g, i, sl):
        d = slots[sl]
        h0 = g * HG
        tsl = slice(i * 128, (i + 1) * 128)
        Dg, egT, decay, PT, PTb = d["Dg"], d["egT"], d["decay"], d["PT"], d["PTb"]
        Ms = [d["M0"], d["M1"]]
        MTs = [d["MT0"], d["MT1"]]
        k.op("dve", lambda e: e.tensor_tensor(out=Dg[:], in0=ident_f[:].unsqueeze(1).to_broadcast([128, HG, 128]), in1=bc_tok(GC[:, i, h0:h0 + HG]), op=ALU.mult),
             r=[ident_f, GC], w=[Dg])
        b1 = k.bank()
        k.op("pe", lambda e: e.matmul(b1[:, 0:HW], lhsT=ones_f[:], rhs=fl(Dg), start=True, stop=True), r=[ones_f, Dg], w=[b1])
        k.op("act", lambda e: e.activation(out=fl(egT), in_=b1[:, 0:HW], func=AF.Exp), r=[b1], w=[egT])
        b2 = k.bank()
        k.op("pe", lambda e: e.matmul(b2[:, 0:HW], lhsT=ones_f[:], rhs=fl(Dg), start=True, stop=False), r=[ones_f, Dg], w=[b2])
        k.op("pe", lambda e: e.matmul(b2[:, 0:HW], lhsT=ident_f[:], rhs=fl(POSM), start=False, stop=True), r=[ident_f, POSM], w=[b2])
        for h in range(HG):
            k.op("act", lambda e, h=h: e.activation(out=decay[:, h, :], in_=b2[:, h * 128:(h + 1) * 128], func=AF.Exp, scale=-1.0, bias=GC[:, i, h0 + h:h0 + h + 1]),
                 r=[b2, GC], w=[decay])
        k.op("pool", lambda e: e.tensor_tensor(out=d["qg"][:], in0=qT[:, :, tsl], in1=egT[:], op=ALU.mult), r=[qT, egT], w=[d["qg"]])
        b3 = k.bank()
        b4 = k.bank()
        for h in range(HG):
            k.op("pe", lambda e, h=h: e.matmul(b3[:, h * 128:(h + 1) * 128], lhsT=kT[:, h, tsl], rhs=kT[:, h, tsl], start=True, stop=True), r=[kT], w=[b3])
        for h in range(HG):
            k.op("pe", lambda e, h=h: e.matmul(b4[:, h * 128:(h + 1) * 128], lhsT=qT[:, h, tsl], rhs=kT[:, h, tsl], start=True, stop=True), r=[qT, kT], w=[b4])
        k.op("dve", lambda e: e.tensor_tensor(out=fl(d["intra"]), in0=b4[:, 0:HW], in1=fl(decay), op=ALU.mult), r=[b4, decay], w=[d["intra"]])
        k.op("pool", lambda e: e.tensor_tensor(out=Dg[:], in0=decay[:], in1=OFFD[:], op=ALU.mult), r=[decay, OFFD], w=[Dg])
        for h in range(HG):
            k.op("dve", lambda e, h=h: e.scalar_tensor_tensor(out=Ms[0][:, h, :], in0=b3[:, h * 128:(h + 1) * 128], scalar=NB[:, i, h0 + h:h0 + h + 1], op0=ALU.mult,
                                                                in1=Dg[:, h, :], op1=ALU.mult), r=[b3, NB, Dg], w=[Ms[0]])
        yield
        b5 = k.bank()
        b5i = k.bank()
        b5b = b5i[:].bitcast(BF16)
        for h in range(HG):
            k.op("pe", lambda e, h=h: e.transpose(out=b5[:, h * 128:(h + 1) * 128], in_=Ms[0][:, h, :], identity=ident_f[:]), r=[Ms[0], ident_f], w=[b5])
        for h in range(HG):
            k.op("pe", lambda e, h=h: e.transpose(out=b5b[:, h * 128:(h + 1) * 128], in_=d["intra"][:, h, :], identity=ident_b[:]), r=[d["intra"], ident_b], w=[b5i])
        k.op("act", lambda e: e.copy(out=fl(MTs[0]), in_=b5[:, 0:HW]), r=[b5], w=[MTs[0]])
        k.op("act", lambda e: e.copy(out=fl(d["intraT"]), in_=b5b[:, 0:HW]), r=[b5i], w=[d["intraT"]])
        k.op("dve", lambda e: e.tensor_tensor(out=PT[:], in0=MTs[0][:], in1=ident_f[:].unsqueeze(1).to_broadcast([128, HG, 128]), op=ALU.add), r=[MTs[0], ident_f], w=[PT])
        yield
        cur = 0
        for lvl in range(1, 7):
            nx = 1 - cur
            last = (lvl == 6)
            b6 = k.bank()
            for h in range(HG):
                k.op("pe", lambda e, h=h, cur=cur, b6=b6: e.matmul(b6[:, h * 128:(h + 1) * 128], lhsT=MTs[cur][:, h, :], rhs=Ms[cur][:, h, :], start=True, stop=True),
                     r=[MTs[cur], Ms[cur]], w=[b6])
            if not last:
                b7 = k.bank()
                for h in range(HG):
                    k.op("pe", lambda e, h=h, cur=cur, b7=b7: e.matmul(b7[:, h * 128:(h + 1) * 128], lhsT=Ms[cur][:, h, :], rhs=MTs[cur][:, h, :], start=True, stop=True),
                         r=[MTs[cur], Ms[cur]], w=[b7])
            k.op("act", lambda e, nx=nx, b6=b6: e.copy(out=fl(Ms[nx]), in_=b6[:, 0:HW]), r=[b6], w=[Ms[nx]])
            if not last:
                k.op("act", lambda e, nx=nx, b7=b7: e.copy(out=fl(MTs[nx]), in_=b7[:, 0:HW]), r=[b7], w=[MTs[nx]])
            b8 = k.bank()
            for h in range(HG):
                k.op("pe", lambda e, h=h, nx=nx, b8=b8: e.matmul(b8[:, h * 128:(h + 1) * 128], lhsT=Ms[nx][:, h, :], rhs=PT[:, h, :], start=True, stop=True),
                     r=[Ms[nx], PT], w=[b8])
            k.op("dve", lambda e, b8=b8: e.tensor_tensor(out=fl(PT), in0=b8[:, 0:HW], in1=fl(PT), op=ALU.add), r=[b8, PT], w=[PT])
            if last:
                k.op("pool", lambda e: e.tensor_copy(out=PTb[:], in_=PT[:]), r=[PT], w=[PTb])
            cur = nx
            yield
        b9 = k.bank()
        b9b = b9[:].bitcast(BF16)
        for h in range(HG):
            k.op("pe", lambda e, h=h: e.transpose(out=b9b[:, h * 128:(h + 1) * 128], in_=vT[:, h, tsl], identity=ident_b[:]), r=[vT, ident_b], w=[b9])
        for h in range(HG):
            k.op("pe", lambda e, h=h: e.transpose(out=b9b[:, HW + h * 128:HW + (h + 1) * 128], in_=kT[:, h, tsl], identity=ident_b[:]), r=[kT, ident_b], w=[b9])
        vps = b9b[:, 0:HW].rearrange("p (a b) -> p a b", a=HG)
        kps = b9b[:, HW:2 * HW].rearrange("p (a b) -> p a b", a=HG)
        k.op("dve", lambda e: e.tensor_tensor(out=d["vbeta"][:], in0=vps, in1=bc_tok(Bt[:, i, h0:h0 + HG]), op=ALU.mult), r=[b9, Bt], w=[d["vbeta"]])
        k.op("dve", lambda e: e.tensor_tensor(out=d["kbg"][:], in0=kps, in1=bc_tok(BEG[:, i, h0:h0 + HG]), op=ALU.mult), r=[b9, BEG], w=[d["kbg"]])
        k.op("dve", lambda e: e.tensor_tensor(out=d["kdec"][:], in0=kps, in1=bc_tok(EKD[:, i, h0:h0 + HG]), op=ALU.mult), r=[b9, EKD], w=[d["kdec"]])
        b10 = k.bank()
        b11 = k.bank()
        for h in range(HG):
            k.op("pe", lambda e, h=h: e.matmul(b10[:, h * 128:(h + 1) * 128], lhsT=PTb[:, h, :], rhs=d["vbeta"][:, h, :], start=True, stop=True), r=[PTb, d["vbeta"]], w=[b10])
        for h in range(HG):
            k.op("pe", lambda e, h=h: e.matmul(b11[:, h * 128:(h + 1) * 128], lhsT=d["kbg"][:, h, :], rhs=PTb[:, h, :], start=True, stop=True), r=[PTb, d["kbg"]], w=[b11])
        k.op("act", lambda e: e.copy(out=fl(d["u"]), in_=b10[:, 0:HW]), r=[b10], w=[d["u"]])
        k.op("act", lambda e: e.copy(out=fl(d["wT"]), in_=b11[:, 0:HW]), r=[b11], w=[d["wT"]])
        yield

    def scan_step(g, i, sl):
        d = slots[sl]
        h0 = g * HG
        tsl = slice(i * 128, (i + 1) * 128)
        z_ = zt[i % 2]
        k.dma("sp", fl(z_), gz[i * 128:(i + 1) * 128, h0 * 128:(h0 + HG) * 128], r=["gz"], w=[z_])
        bx = k.bank()
        for h in range(HG):
            k.op("pe", lambda e, h=h: e.matmul(bx[:, h * 128:(h + 1) * 128], lhsT=d["wT"][:, h, :], rhs=Sb[:, h, :], start=True, stop=True), r=[d["wT"], Sb], w=[bx])
        k.op("dve", lambda e: e.tensor_tensor(out=fl(vnew), in0=fl(d["u"]), in1=bx[:, 0:HW], op=ALU.subtract), r=[d["u"], bx], w=[vnew])
        bo = k.bank()
        for h in range(HG):
            k.op("pe", lambda e, h=h: e.matmul(bo[:, h * 128:(h + 1) * 128], lhsT=d["qg"][:, h, :], rhs=Sb[:, h, :], start=True, stop=False), r=[d["qg"], Sb], w=[bo])
            k.op("pe", lambda e, h=h: e.matmul(bo[:, h * 128:(h + 1) * 128], lhsT=d["intraT"][:, h, :], rhs=vnew[:, h, :], start=False, stop=True), r=[d["intraT"], vnew], w=[bo])
        bz = k.bank()
        for h in range(HG):
            k.op("pe", lambda e, h=h: e.matmul(bz[:, h * 128:(h + 1) * 128], lhsT=d["kdec"][:, h, :], rhs=vnew[:, h, :], start=True, stop=True), r=[d["kdec"], vnew], w=[bz])
        k.op("dve", lambda e: e.tensor_tensor(out=S_[:], in0=S_[:], in1=bc_tok(EGL[:, i, h0:h0 + HG]), op=ALU.mult), r=[S_, EGL], w=[S_])
        k.op("dve", lambda e: e.tensor_tensor(out=fl(S_), in0=fl(S_), in1=bz[:, 0:HW], op=ALU.add), r=[S_, bz], w=[S_])
        k.op("act", lambda e: e.copy(out=Sb[:], in_=S_[:]), r=[S_], w=[Sb])
        k.op("act", lambda e: e.copy(out=fl(o_), in_=bo[:, 0:HW]), r=[bo], w=[o_])
        k.op("pool", lambda e: e.tensor_tensor(out=sq_[:], in0=o_[:], in1=o_[:], op=ALU.mult), r=[o_], w=[sq_])
        k.op("dve", lambda e: e.tensor_reduce(out=sst[:, 0:HG], in_=sq_[:], axis=AX.X, op=ALU.add), r=[sq_], w=[sst])
        k.op("act", lambda e: e.activation(out=sst[:, HG:2 * HG], in_=sst[:, 0:HG], func=AF.Sqrt, scale=1.0 / 128, bias=EPS), r=[sst], w=[sst])
        k.op("dve", lambda e: e.reciprocal(out=sst[:, 2 * HG:3 * HG], in_=sst[:, HG:2 * HG]), r=[sst], w=[sst])
        k.op("act", lambda e: e.activation(out=zs[:], in_=z_[:], func=AF.Silu), r=[z_], w=[zs])
        k.op("dve", lambda e: e.tensor_tensor(out=o_[:], in0=o_[:], in1=bc_tok(sst[:, 2 * HG:3 * HG]), op=ALU.mult), r=[o_, sst], w=[o_])
        k.op("pool", lambda e: e.tensor_tensor(out=o_[:], in0=o_[:], in1=gnb[:].unsqueeze(1).to_broadcast([128, HG, 128]), op=ALU.mult), r=[o_, gnb], w=[o_])
        k.op("pool", lambda e: e.tensor_tensor(out=oa[:], in0=o_[:], in1=zs[:], op=ALU.mult), r=[o_, zs], w=[oa])
        bt = k.bank()
        btb = bt[:].bitcast(BF16)
        for h in range(HG):
            k.op("pe", lambda e, h=h: e.transpose(out=btb[:, h * 128:(h + 1) * 128], in_=oa[:, h, :], identity=ident_b[:]), r=[oa, ident_b], w=[bt])
        k.op("act", lambda e: e.copy(out=mixT[:, h0:h0 + HG, tsl], in_=btb[:, 0:HW].rearrange("p (a b) -> p a b", a=HG)), r=[bt], w=[(mixT, i)])

    for g in range(8 // HG):
        h0 = g * HG
        for nm, src, buf in (("q", gq, qT), ("k", gk, kT), ("v", gv, vT)):
            k.dma("sp", buf[:], src[h0:h0 + HG].rearrange("h d t -> d h t"), r=["gqk", "gv"], w=[buf])
        k.op("pool", lambda e: e.memset(S_[:], 0.0), w=[S_])
        k.op("pool", lambda e: e.memset(Sb[:], 0.0), w=[Sb])
        if stop_after == "P3d":
            for _ in stageA(0, 0, 0):
                pass
            for _ in stageA(0, 1, 1):
                pass
            scan_step(0, 0, 0)
            scan_step(0, 1, 1)
            d = slots[0]
            names = ["decay", "M0", "PT", "u", "wT", "intraT", "qg", "kdec", "vbeta", "kbg", "egT"]
            tmpfs = [sq_, zs]
            for ii, nm in enumerate(names):
                dd = dout("dbg_" + nm, [128, HW], F32)
                tmpf = tmpfs[ii % 2]
                k.op("dve", lambda e, nm=nm, tmpf=tmpf: e.tensor_copy(out=fl(tmpf), in_=fl(d[nm])), r=[d[nm]], w=[tmpf])
                k.dma("sp", dd.ap(), fl(tmpf), r=[tmpf])
            for nm, b in (("S", S_), ("o", o_), ("GC", GC), ("G", G_), ("Bt", Bt), ("EKD", EKD), ("EGL", EGL)):
                dd = dout("dbg_" + nm, [128, int(np.prod(b.ap.shape[1:]))], F32)
                k.dma("sp", dd.ap(), b[:].rearrange("p a b -> p (a b)"), r=[b])
            k.barrier()
            return k.finish()
        import os
        _lim = int(os.environ.get("YLIM", "100"))
        for i0 in range(0, NT, NSLOT):
            gens = [stageA(g, i0 + j, j) for j in range(NSLOT)]
            if stop_after == "P3b":
                for _ in range(_lim):
                    for gen in gens:
                        next(gen, None)
                k.barrier()
                return k.finish()
            alive = True
            while alive:
                alive = False
                for gen in gens:
                    try:
                        next(gen)
                        alive = True
                    except StopIteration:
                        pass
            for j in range(NSLOT):
                scan_step(g, i0 + j, j)
            issue_conv(3)
        k.dma("sp", ssm_p.ap()[h0:h0 + HG].rearrange("h a b -> a h b"), S_[:], r=[S_])
    if stop_after == "P3":
        k.barrier()
        return k.finish()
    issue_conv(100)
    k.barrier()
    k.release(m3g)
    S0 = k.alloc("S0", [128, 8, 128], F32)
    qc = k.alloc("qc", [128, 8], BF16)
    kc = k.alloc("kc", [128, 8], BF16)
    vc = k.alloc("vc", [128, 8], BF16)
    qcf = k.alloc("qcf", [128, 8], F32)
    kcf = k.alloc("kcf", [128, 8], F32)
    gabr = k.alloc("gabr", [1, 16], F32)
    zr = k.alloc("zr", [1, 1024], BF16)
    zrs = k.alloc("zrs", [1, 8, 128], F32)
    rw = k.alloc("rw", [1, 64], F32)
    t1 = k.alloc("t1", [1, 8, 128], F32)
    orow = k.alloc("orow", [1, 8, 128], F32)
    sqr = k.alloc("sqr", [1, 8, 128], F32)
    oar = k.alloc("oar", [1, 1024], BF16)
    abs_ = k.alloc("abs_", [128, 8], F32)

    def bc_row(ap8):
        return ap8.unsqueeze(2).to_broadcast([1, 8, 128])

    def sample_gdn(s_i):
        col = T + s_i
        k.dma("sp", S0[:], st_ssm.ap()[s_i].rearrange("h a b -> a h b"), w=[S0])
        k.dma("sp", qc[:], gq.ap()[:, :, col].rearrange("h d -> d h"), r=["gqk"], w=[qc], slow=True)
        k.dma("sp", kc[:], gk.ap()[:, :, col].rearrange("h d -> d h"), r=["gqk"], w=[kc], slow=True)
        k.dma("sp", vc[:], gv.ap()[:, :, col].rearrange("h d -> d h"), r=["gv"], w=[vc], slow=True)
        k.dma("sp", gabr[:], gab[col:col + 1, :], r=["gab"], w=[gabr])
        k.dma("sp", zr[:], gz[col:col + 1, :], r=["gz"], w=[zr])
        k.op("dve", lambda e: e.tensor_copy(out=qcf[:], in_=qc[:]), r=[qc], w=[qcf])
        k.op("dve", lambda e: e.tensor_copy(out=kcf[:], in_=kc[:]), r=[kc], w=[kcf])
        k.op("dve", lambda e: e.tensor_tensor(out=rw[:, 0:8], in0=gabr[:, 0:8], in1=hvb[0:1, 8:16], op=ALU.add), r=[gabr, hvb], w=[rw])
        k.op("act", lambda e: e.activation(out=rw[:, 0:8], in_=rw[:, 0:8], func=AF.Exp), r=[rw], w=[rw])
        k.op("act", lambda e: e.activation(out=rw[:, 0:8], in_=rw[:, 0:8], func=AF.Ln, bias=1.0, scale=1.0), r=[rw], w=[rw])
        k.op("dve", lambda e: e.tensor_tensor(out=rw[:, 0:8], in0=rw[:, 0:8], in1=nA[0:1, :], op=ALU.mult), r=[rw, nA], w=[rw])
        k.op("act", lambda e: e.activation(out=rw[:, 8:16], in_=rw[:, 0:8], func=AF.Exp), r=[rw], w=[rw])
        k.op("act", lambda e: e.activation(out=rw[:, 16:24], in_=gabr[:, 8:16], func=AF.Sigmoid), r=[gabr, rw], w=[rw])
        ba = k.bank()
        bb_ = k.bank()
        for h in range(8):
            bk_ = ba if h < 4 else bb_
            k.op("pe", lambda e, h=h, bk_=bk_: e.matmul(bk_[0:1, (h % 4) * 128:(h % 4 + 1) * 128], lhsT=kcf[:, h:h + 1], rhs=S0[:, h, :], start=True, stop=True),
                 r=[kcf, S0], w=[bk_])
        bv = k.bank()
        bvb = bv[:].bitcast(BF16)
        for h in range(8):
            k.op("pe", lambda e, h=h: e.transpose(out=bvb[0:1, h * 128:(h + 1) * 128], in_=vc[:, h:h + 1], identity=ident_b[:]), r=[vc, ident_b], w=[bv])
        k.op("dve", lambda e: e.tensor_tensor(out=t1[:, 0:4, :], in0=ba[0:1, :].rearrange("p (a b) -> p a b", a=4), in1=bc_row(rw[:, 8:16])[:, 0:4, :], op=ALU.mult),
             r=[ba, rw], w=[t1])
        k.op("dve", lambda e: e.tensor_tensor(out=t1[:, 4:8, :], in0=bb_[0:1, :].rearrange("p (a b) -> p a b", a=4), in1=bc_row(rw[:, 8:16])[:, 4:8, :], op=ALU.mult),
             r=[bb_, rw, t1], w=[t1])
        k.op("dve", lambda e: e.tensor_tensor(out=t1[:], in0=bvb[0:1, 0:1024].rearrange("p (a b) -> p a b", a=8), in1=t1[:], op=ALU.subtract), r=[bv, t1], w=[t1])
        k.op("dve", lambda e: e.tensor_tensor(out=t1[:], in0=t1[:], in1=bc_row(rw[:, 16:24]), op=ALU.mult), r=[t1, rw], w=[t1])
        bd0 = k.bank()
        bd1 = k.bank()
        k.op("pe", lambda e: e.matmul(bd0[:, :], lhsT=ones_f[0:1, :], rhs=t1[:].rearrange("p a b -> p (a b)")[:, 0:512], start=True, stop=True), r=[ones_f, t1], w=[bd0])
        k.op("pe", lambda e: e.matmul(bd1[:, :], lhsT=ones_f[0:1, :], rhs=t1[:].rearrange("p a b -> p (a b)")[:, 512:1024], start=True, stop=True), r=[ones_f, t1], w=[bd1])
        bab = k.bank()
        k.op("pe", lambda e: e.matmul(bab[:, 0:8], lhsT=ones_f[0:1, :], rhs=rw[:, 8:16], start=True, stop=True), r=[ones_f, rw], w=[bab])
        k.op("act", lambda e: e.copy(out=abs_[:], in_=bab[:, 0:8]), r=[bab], w=[abs_])
        k.op("dve", lambda e: e.tensor_tensor(out=S0[:], in0=S0[:], in1=abs_[:].unsqueeze(2).to_broadcast([128, 8, 128]), op=ALU.mult), r=[S0, abs_], w=[S0])
        for h in range(8):
            bd = bd0 if h < 4 else bd1
            k.op("dve", lambda e, h=h, bd=bd: e.scalar_tensor_tensor(out=S0[:, h, :], in0=bd[:, (h % 4) * 128:(h % 4 + 1) * 128], scalar=kcf[:, h:h + 1], op0=ALU.mult,
                                                                      in1=S0[:, h, :], op1=ALU.add), r=[bd, kcf, S0], w=[S0])
        k.dma("sp", ssm_s.ap()[s_i].rearrange("h a b -> a h b"), S0[:], r=[S0])
        bo0 = k.bank()
        bo1 = k.bank()
        for h in range(8):
            bk_ = bo0 if h < 4 else bo1
            k.op("pe", lambda e, h=h, bk_=bk_: e.matmul(bk_[0:1, (h % 4) * 128:(h % 4 + 1) * 128], lhsT=qcf[:, h:h + 1], rhs=S0[:, h, :], start=True, stop=True),
                 r=[qcf, S0], w=[bk_])
        k.op("act", lambda e: e.copy(out=orow[:, 0:4, :], in_=bo0[0:1, :].rearrange("p (a b) -> p a b", a=4)), r=[bo0], w=[orow])
        k.op("act", lambda e: e.copy(out=orow[:, 4:8, :], in_=bo1[0:1, :].rearrange("p (a b) -> p a b", a=4)), r=[bo1, orow], w=[orow])
        k.op("dve", lambda e: e.tensor_tensor(out=sqr[:], in0=orow[:], in1=orow[:], op=ALU.mult), r=[orow], w=[sqr])
        k.op("dve", lambda e: e.tensor_reduce(out=rw[:, 24:32], in_=sqr[:], axis=AX.X, op=ALU.add), r=[sqr, rw], w=[rw])
        k.op("act", lambda e: e.activation(out=rw[:, 32:40], in_=rw[:, 24:32], func=AF.Sqrt, scale=1.0 / 128, bias=EPS), r=[rw], w=[rw])
        k.op("dve", lambda e: e.reciprocal(out=rw[:, 40:48], in_=rw[:, 32:40]), r=[rw], w=[rw])
        k.op("act", lambda e: e.activation(out=zrs[:].rearrange("p a b -> p (a b)"), in_=zr[:], func=AF.Silu), r=[zr], w=[zrs])
        k.op("dve", lambda e: e.tensor_tensor(out=orow[:], in0=orow[:], in1=bc_row(rw[:, 40:48]), op=ALU.mult), r=[orow, rw], w=[orow])
        k.op("dve", lambda e: e.tensor_tensor(out=orow[:], in0=orow[:], in1=gnb[0:1, :].unsqueeze(1).to_broadcast([1, 8, 128]), op=ALU.mult), r=[orow, gnb], w=[orow])
        k.op("dve", lambda e: e.tensor_tensor(out=oar[:].rearrange("p (a b) -> p a b", a=8), in0=orow[:], in1=zrs[:], op=ALU.mult), r=[orow, zrs], w=[oar])
        bt_ = k.bank()
        for h in range(8):
            k.op("pe", lambda e, h=h: e.matmul(bt_[:, h:h + 1], lhsT=oar[0:1, h * 128:(h + 1) * 128], rhs=ones_b[0:1, 0:1], start=True, stop=True), r=[oar, ones_b], w=[bt_])
        k.op("act", lambda e: e.copy(out=mixT[:, 0:8, col], in_=bt_[:, 0:8]), r=[bt_], w=[(mixT, "s%d" % s_i)])
    for s_i in range(TS):
        sample_gdn(s_i)
    k.barrier()
    k.release(m3)
    if stop_after == "P3S":
        return k.finish()
    m4 = k.mark()
    NEG = -30000.0
    NIT = 17
    kTb = k.alloc("kTb", [128, 2, TT], BF16)
    vtok = k.alloc("vtok", [128, NT, 256], BF16)
    ikT4 = k.alloc("ikT4", [128, TT], BF16)
    k.dma("sp", kTb[:], akT.ap().rearrange("h d t -> d h t"), r=["plain"], w=[kTb])
    k.dma("sp", vtok[:], av[0:T, :].rearrange("(t p) c -> p t c", p=128), r=["av"], w=[vtok])
    k.dma("sp", ikT4[:], ikTd.ap(), r=["ikTd"], w=[ikT4])
    ones_f4 = k.alloc("ones_f4", [128, 128], F32)
    zeros_b = k.alloc("zeros_b", [128, 128], BF16)
    Jm = k.alloc("Jm", [128, 128], F32)
    CMT = k.alloc("CMT", [128, 128], F32)
    CM = k.alloc("CM", [128, 128], F32)
    k.op("pool", lambda e: e.memset(ones_f4[:], 1.0), w=[ones_f4])
    k.op("pool", lambda e: e.memset(zeros_b[:], 0.0), w=[zeros_b])
    k.op("pool", lambda e: e.memset(Jm[:], 1.0), w=[Jm])
    k.op("pool", lambda e: e.affine_select(out=Jm[:], in_=Jm[:], pattern=[[1, 128]], compare_op=ALU.is_equal, fill=0.0, base=-127, channel_multiplier=1), r=[Jm], w=[Jm])
    k.op("pool", lambda e: e.memset(CMT[:], 0.0), w=[CMT])
    k.op("pool", lambda e: e.affine_select(out=CMT[:], in_=CMT[:], pattern=[[1, 128]], compare_op=ALU.is_ge, fill=NEG, base=0, channel_multiplier=-1), r=[CMT], w=[CMT])
    k.op("pool", lambda e: e.memset(CM[:], 0.0), w=[CM])
    k.op("pool", lambda e: e.affine_select(out=CM[:], in_=CM[:], pattern=[[-1, 128]], compare_op=ALU.is_ge, fill=NEG, base=0, channel_multiplier=1), r=[CM], w=[CM])
    rb = k.alloc("rb", [32, 8], F32)
    rb31 = k.alloc("rb31", [32, 8], F32)
    bohs = k.alloc("bohs", [32, 384], F32)
    bvec = k.alloc("bvec", [8, 384], F32)
    Tp = k.alloc("Tp", [128, 8, 128], F32)
    Bt4 = [k.alloc(f"Bt4_{i}", [128, 8, 128], F32) for i in range(2)]
    k.dma("sp", rb[:], rel_bias.ap(), w=[rb])
    k.dma("sp", rb31[:], rel_bias[31:32, :].to_broadcast([32, 8]), w=[rb31])
    k.dma("sp", bohs[:], boh.ap(), w=[bohs])
    k.op("dve", lambda e: e.tensor_tensor(out=rb[:], in0=rb[:], in1=rb31[:], op=ALU.subtract), r=[rb, rb31], w=[rb])
    bkb = k.bank()
    k.op("pe", lambda e: e.matmul(bkb[0:8, 0:384], lhsT=rb[:], rhs=bohs[:], start=True, stop=True), r=[rb, bohs], w=[bkb])
    k.op("act", lambda e: e.copy(out=bvec[:], in_=bkb[0:8, 0:384]), r=[bkb], w=[bvec])
    k.dma("sp", biasd.ap(), bvec[:], r=[bvec], w=["biasd"])

    def mk_bias(dl):
        k.dma("sp", Tp[:], bass.AP(tensor=biasd, offset=128 * dl, ap=[[1, 128], [384, 8], [1, 128]]), r=["biasd"], w=[Tp])
        for half in range(2):
            bj = k.bank()
            k.op("pe", lambda e, bj=bj, half=half: e.matmul(bj[:, :], lhsT=Jm[:], rhs=Tp[:].rearrange("p a b -> p (a b)")[:, half * 512:(half + 1) * 512], start=True, stop=True), r=[Jm, Tp], w=[bj])
            k.op("act", lambda e, bj=bj, half=half: e.copy(out=Bt4[dl][:].rearrange("p a b -> p (a b)")[:, half * 512:(half + 1) * 512], in_=bj[:, :]), r=[bj], w=[Bt4[dl]])

    mk_bias(0)
    mk_bias(1)

    qbT = [k.alloc(f"qbT{i}", [128, 8, 128], BF16) for i in range(2)]
    qiT = [k.alloc(f"qiT{i}", [128, 16, 128], BF16) for i in range(2)]
    iwt = [k.alloc(f"iwt{i}", [128, 48], F32) for i in range(2)]
    scs = [k.alloc(f"sc{i}", [128, T], F32) for i in range(2)]
    sc = scs[0]
    Dws = [k.alloc(f"Dw{i}", [128, 16, 128], BF16) for i in range(2)]
    jnk = k.alloc("jnk", [128, T], BF16)
    rl = [k.alloc(f"rl{i}", [128, 512], BF16) for i in range(4)]
    rcnt = [0]
    bs = k.alloc("bs", [128, 8], F32)
    negT = k.alloc("negT", [128, NT, 128], F32)
    Eb = [k.alloc(f"Eb{i}", [128, 4, 128], F32) for i in range(2)]
    PTs = [k.alloc(f"PTs{i}", [128, 4, 128], BF16) for i in range(3)]
    rden = k.alloc("rden", [128, 8], F32)
    oab = k.alloc("oab", [128, 8, 128], BF16)
    ecnt = [0]

    def stage_I(qb):
        L = 128 * (qb + 1)
        tsl = slice(qb * 128, (qb + 1) * 128)
        qb_ = qbT[qb % 2]
        qi_ = qiT[qb % 2]
        iw_ = iwt[qb % 2]
        sc = scs[qb % 2]
        Dw_ = Dws[qb % 2]
        k.dma("sp", qb_[:], aq.ap()[:, :, tsl].rearrange("h d t -> d h t"), r=["plain"], w=[qb_])
        k.dma("sp", qi_[:], iq.ap()[:, :, tsl].rearrange("h d t -> d h t"), r=["plain"], w=[qi_])
        k.dma("sp", iw_[:, 0:16], iw[qb * 128:(qb + 1) * 128, :], r=["iw"], w=[iw_])
        if qb < 2:
            return
        for h in range(16):
            k.op("pool", lambda e, h=h: e.tensor_scalar(out=Dw_[:, h, :], in0=ident_f[:], scalar1=iw_[:, h:h + 1], scalar2=None, op0=ALU.mult), r=[ident_f, iw_], w=[Dw_])

        def idx_kg(kg):
            c0 = kg * 512
            n = min(512, L - c0)
            bacc = k.bank()
            k.reserved = {bacc.name}
            pend = []

            def acc(h, r_):
                k.op("pe", lambda e: e.matmul(bacc[:, 0:n], lhsT=Dw_[:, h, :], rhs=r_[:, 0:n], start=(h == 0), stop=(h == 15)), r=[Dw_, r_], w=[bacc])

            for h in range(16):
                bk_ = k.bank()
                r_ = rl[rcnt[0] % 4]
                rcnt[0] += 1
                k.op("pe", lambda e, h=h, bk_=bk_: e.matmul(bk_[:, 0:n], lhsT=qi_[:, h, :], rhs=ikT4[:, c0:c0 + n], start=True, stop=True), r=[qi_, ikT4], w=[bk_])
                k.op("act", lambda e, bk_=bk_, r_=r_: e.activation(out=r_[:, 0:n], in_=bk_[:, 0:n], func=AF.Relu), r=[bk_], w=[r_])
                pend.append((h, r_))
                if len(pend) > 2:
                    acc(*pend.pop(0))
            while pend:
                acc(*pend.pop(0))
            k.reserved = set()
            k.op("act", lambda e: e.copy(out=sc[:, c0:c0 + n], in_=bacc[:, 0:n]), r=[bacc], w=[sc])

        for kg in range((L + 511) // 512):
            idx_kg(kg)

    def stage_B(qb):
        L = 128 * (qb + 1)
        sc = scs[qb % 2]
        if qb >= 2:
            k.op("dve", lambda e: e.tensor_reduce(out=bs[:, 0:1], in_=sc[:, 0:L], axis=AX.X, op=ALU.max), r=[sc], w=[bs])
            k.op("dve", lambda e: e.tensor_reduce(out=bs[:, 1:2], in_=sc[:, 0:L], axis=AX.X, op=ALU.min), r=[sc, bs], w=[bs])
            k.op("dve", lambda e: e.tensor_scalar(out=bs[:, 1:2], in0=bs[:, 1:2], scalar1=-1.0, scalar2=None, op0=ALU.add), r=[bs], w=[bs])
            k.op("dve", lambda e: e.tensor_tensor(out=bs[:, 2:3], in0=bs[:, 0:1], in1=bs[:, 1:2], op=ALU.subtract), r=[bs], w=[bs])
            k.op("dve", lambda e: e.tensor_tensor(out=sc[:, L - 128:L], in0=sc[:, L - 128:L], in1=CM[:], op=ALU.add), r=[sc, CM], w=[sc])
            for it in range(NIT):
                f = 2.0 ** -(it + 1)
                k.op("dve", lambda e, f=f: e.scalar_tensor_tensor(out=bs[:, 3:4], in0=bs[:, 2:3], scalar=f, op0=ALU.mult, in1=bs[:, 1:2], op1=ALU.add), r=[bs], w=[bs])
                k.op("dve", lambda e: e.tensor_scalar(out=jnk[:, 0:L], in0=sc[:, 0:L], scalar1=bs[:, 3:4], scalar2=None, op0=ALU.is_gt, op1=ALU.add, accum_out=bs[:, 4:5]),
                     r=[sc, bs], w=[jnk, bs])
                k.op("dve", lambda e, f=f: e.tensor_scalar(out=bs[:, 5:6], in0=bs[:, 4:5], scalar1=255.5, scalar2=f, op0=ALU.is_gt, op1=ALU.mult), r=[bs], w=[bs])
                k.op("dve", lambda e: e.scalar_tensor_tensor(out=bs[:, 1:2], in0=bs[:, 5:6], scalar=bs[:, 2:3], op0=ALU.mult, in1=bs[:, 1:2], op1=ALU.add), r=[bs], w=[bs])
            k.op("dve", lambda e: e.tensor_scalar(out=sc[:, 0:L], in0=sc[:, 0:L], scalar1=bs[:, 1:2], scalar2=None, op0=ALU.subtract), r=[sc, bs], w=[sc])
            for k4 in range((qb + 1 + 3) // 4):
                nb = min(4, qb + 1 - k4 * 4)
                bk_ = k.bank()
                for j in range(nb):
                    kb = k4 * 4 + j
                    k.op("pe", lambda e, j=j, kb=kb, bk_=bk_: e.transpose(out=bk_[:, j * 128:(j + 1) * 128], in_=sc[:, kb * 128:(kb + 1) * 128], identity=ident_f[:]),
                         r=[sc, ident_f], w=[bk_])
                k.op("dve", lambda e, k4=k4, nb=nb, bk_=bk_: e.tensor_scalar(out=negT[:, k4 * 4:k4 * 4 + nb, :].rearrange("p a b -> p (a b)"), in0=bk_[:, 0:nb * 128],
                                                                             scalar1=0.0, scalar2=NEG, op0=ALU.is_le, op1=ALU.mult), r=[bk_], w=[negT])
            k.op("dve", lambda e: e.tensor_tensor(out=negT[:, qb, :], in0=negT[:, qb, :], in1=CMT[:], op=ALU.add), r=[negT, CMT], w=[negT])
        else:
            if qb == 1:
                k.op("pool", lambda e: e.memset(negT[:, 0, :], 0.0), w=[negT])
            k.op("pool", lambda e: e.tensor_copy(out=negT[:, qb, :], in_=CMT[:]), r=[CMT, negT], w=[negT])

    def stage_A(qb):
        tsl = slice(qb * 128, (qb + 1) * 128)
        qb_ = qbT[qb % 2]
        qi_ = qiT[qb % 2]
        bo0 = k.bank()
        bo1 = k.bank()
        bdn = k.bank()
        k.reserved = {bo0.name, bo1.name, bdn.name}
        for bz_ in (bo0, bo1, bdn):
            k.op("pe", lambda e, bz_=bz_: e.matmul(bz_[:, :], lhsT=zeros_b[:], rhs=qi_[:].rearrange("p a b -> p (a b)")[:, 0:512], start=True, stop=False), r=[zeros_b, qi_], w=[bz_])
        def kv_pair(kb, kvh):
            bl = k.bank()
            k.op("pe", lambda e, bl=bl: e.matmul(bl[:, :], lhsT=kTb[:, kvh, kb * 128:(kb + 1) * 128], rhs=qb_[:, kvh * 4:(kvh + 1) * 4, :].rearrange("p a b -> p (a b)"),
                                                start=True, stop=True), r=[kTb, qb_], w=[bl])
            E_ = Eb[ecnt[0] % 2]
            P_ = PTs[ecnt[0] % 3]
            ecnt[0] += 1
            k.op("dve", lambda e, bl=bl, E_=E_: e.tensor_tensor(out=E_[:], in0=bl[:, :].rearrange("p (a b) -> p a b", a=4), in1=negT[:, kb, :].unsqueeze(1).to_broadcast([128, 4, 128]),
                                                             op=ALU.add), r=[bl, negT], w=[E_])
            if qb - kb <= 1:
                k.op("pool", lambda e, E_=E_: e.tensor_tensor(out=E_[:], in0=E_[:], in1=Bt4[qb - kb][:, kvh * 4:(kvh + 1) * 4, :], op=ALU.add), r=[E_, Bt4[qb - kb]], w=[E_])
            k.op("act", lambda e, E_=E_, P_=P_: e.activation(out=P_[:], in_=E_[:], func=AF.Exp), r=[E_], w=[P_])
            last = (kb == qb)
            for g_ in range(4):
                h = kvh * 4 + g_
                bo = bo0 if h < 4 else bo1
                k.op("pe", lambda e, g_=g_, h=h, bo=bo, P_=P_: e.matmul(bo[:, (h % 4) * 128:(h % 4 + 1) * 128], lhsT=P_[:, g_, :], rhs=vtok[:, kb, kvh * 128:(kvh + 1) * 128],
                                                                    start=False, stop=last), r=[P_, vtok], w=[bo])
                k.op("pe", lambda e, g_=g_, h=h, P_=P_: e.matmul(bdn[:, h:h + 1], lhsT=P_[:, g_, :], rhs=ones_b[:, 0:1], start=False, stop=last), r=[P_, ones_b], w=[bdn])
        for kb in range(qb + 1):
            for kvh in range(2):
                kv_pair(kb, kvh)
        k.reserved = set()
        k.op("dve", lambda e: e.reciprocal(out=rden[:], in_=bdn[:, 0:8]), r=[bdn], w=[rden])
        for half, bo in ((0, bo0), (1, bo1)):
            k.op("dve", lambda e, half=half, bo=bo: e.tensor_tensor(out=oab[:, half * 4:(half + 1) * 4, :], in0=bo[:, :].rearrange("p (a b) -> p a b", a=4),
                                                                    in1=rden[:, half * 4:(half + 1) * 4].unsqueeze(2).to_broadcast([128, 4, 128]), op=ALU.mult),
                 r=[bo, rden], w=[oab])
        bt_ = k.bank()
        btb = bt_[:].bitcast(BF16)
        for h in range(8):
            k.op("pe", lambda e, h=h: e.transpose(out=btb[:, h * 128:(h + 1) * 128], in_=oab[:, h, :], identity=ident_b[:]), r=[oab, ident_b], w=[bt_])
        k.op("act", lambda e: e.copy(out=mixT[:, 8:16, tsl], in_=btb[:, 0:1024].rearrange("p (a b) -> p a b", a=8)), r=[bt_], w=[(mixT, "b%d" % qb)])

    stage_I(0)
    for qb in range(NT):
        if qb + 1 < NT:
            stage_I(qb + 1)
        stage_B(qb)
        stage_A(qb)
    if stop_after == "P4" and os.environ.get("P4DBG"):
        for nm, b, n in (("sc", sc, T), ("bs", bs, 8), ("negT", negT, NT * 128)):
            dd = dout("dbg_" + nm, [128, n], F32)
            src = b[:] if nm != "negT" else b[:].rearrange("p a b -> p (a b)")
            k.dma("sp", dd.ap(), src, r=[b])
    if stop_after == "P4":
        dbg = dout("dbg_mixT", [128, 16 * TT], BF16)
        k.barrier()
        k.dma("sp", dbg.ap(), mixT[:].rearrange("p a b -> p (a b)"))
        return k.finish()
    k.barrier()
    k.release(m4)
    NPG = NPAGES
    ones4 = k.alloc("ones4", [128, 128], F32)
    zeros4 = k.alloc("zeros4", [128, 128], F32)
    Ltri = k.alloc("Ltri", [128, 128], BF16)
    siota = k.alloc("siota", [128, 128], F32)
    piota = k.alloc("piota", [128, 1], F32)
    jrow = k.alloc("jrow", [128, 256], F32)
    jcol = k.alloc("jcol", [128, 2], F32)
    posc = k.alloc("posc", [128, 128], F32)
    k.op("pool", lambda e: e.memset(ones4[:], 1.0), w=[ones4])
    k.op("pool", lambda e: e.memset(zeros4[:], 0.0), w=[zeros4])
    k.op("pool", lambda e: e.memset(Ltri[:], 1.0), w=[Ltri])
    k.op("pool", lambda e: e.affine_select(out=Ltri[:], in_=Ltri[:], pattern=[[1, 128]], compare_op=ALU.is_ge, fill=0.0, base=-1, channel_multiplier=-1), r=[Ltri], w=[Ltri])
    k.op("pool", lambda e: e.iota(siota[:], pattern=[[1, 128]], base=0, channel_multiplier=0, allow_small_or_imprecise_dtypes=True), w=[siota])
    k.op("pool", lambda e: e.iota(piota[:], pattern=[[0, 1]], base=0, channel_multiplier=1, allow_small_or_imprecise_dtypes=True), w=[piota])
    k.op("pool", lambda e: e.iota(jrow[:], pattern=[[1, 256]], base=0, channel_multiplier=0, allow_small_or_imprecise_dtypes=True), w=[jrow])
    k.op("pool", lambda e: e.iota(jcol[:], pattern=[[128, 2]], base=0, channel_multiplier=1, allow_small_or_imprecise_dtypes=True), w=[jcol])
    k.op("pool", lambda e: e.iota(posc[:], pattern=[[1, 128]], base=0, channel_multiplier=128, allow_small_or_imprecise_dtypes=True), w=[posc])
    thrb = k.alloc("thrb", [128, 31], F32)
    k.dma("sp", thrb[:], bthr.ap().to_broadcast([128, 31]), w=[thrb])
    rbs = k.alloc("rbs", [32, 8], F32)
    rbT = k.alloc("rbT", [8, 32], F32)
    drbT = k.alloc("drbT", [128, 8, 32], F32)
    k.dma("sp", rbs[:], rel_bias.ap(), w=[rbs])
    bq = k.bank()
    k.op("pe", lambda e: e.transpose(out=bq[0:8, 0:32], in_=rbs[:], identity=ident_f[0:32, 0:32]), r=[rbs, ident_f], w=[bq])
    k.op("act", lambda e: e.copy(out=rbT[:], in_=bq[0:8, 0:32]), r=[bq], w=[rbT])
    k.dma("sp", rbTd.ap(), rbT[:], r=[rbT], w=["rbTd"])
    k.dma("sp", drbT[:].rearrange("p a b -> p (a b)"), rbTd.ap().rearrange("a b -> (a b)").unsqueeze(0).to_broadcast([128, 256]), r=["rbTd"], w=[drbT])
    rb0b = k.alloc("rb0b", [128, 8], F32)
    k.op("dve", lambda e: e.tensor_copy(out=rb0b[:], in_=drbT[:, :, 0]), r=[drbT], w=[rb0b])
    dtmp = k.alloc("dtmp", [128, 8, 31], F32)
    k.op("dve", lambda e: e.tensor_tensor(out=dtmp[:], in0=drbT[:, :, 1:32], in1=drbT[:, :, 0:31], op=ALU.subtract), r=[drbT], w=[dtmp])
    pt_i = k.alloc("pt_i", [128, TS], I32)
    pt_f = k.alloc("pt_f", [128, TS], F32)
    k.dma("sp", pt_i[:], page_table.ap().rearrange("s p -> p s"), w=[pt_i], slow=True)
    k.op("dve", lambda e: e.tensor_copy(out=pt_f[:], in_=pt_i[:]), r=[pt_i], w=[pt_f])
    k.op("dve", lambda e: e.tensor_scalar(out=pt_f[:], in0=pt_f[:], scalar1=128.0, scalar2=None, op0=ALU.mult), r=[pt_f], w=[pt_f])
    qiS = k.alloc("qiS", [128, TS, 16], BF16)
    wS = k.alloc("wS", [128, TS, 16], F32)
    kiS = k.alloc("kiS", [128, TS], BF16)
    qbS = k.alloc("qbS", [128, 8, TS], BF16)
    knS = k.alloc("knS", [128, 2, TS], BF16)
    for s_i in range(TS):
        k.dma("sp", qiS[:, s_i, :], iq.ap()[:, :, T + s_i].rearrange("h d -> d h"), r=["plain"], w=[qiS], slow=True)
    k.dma("sp", wS[:].rearrange("p a b -> p (a b)"), iw[T:TT, :].rearrange("a b -> (a b)").unsqueeze(0).to_broadcast([128, TS * 16]), r=["iw"], w=[wS])
    k.dma("sp", kiS[:], ikTd[:, T:TT], r=["ikTd"], w=[kiS])
    k.dma("sp", qbS[:], aq.ap()[:, :, T:TT].rearrange("h d s -> d h s"), r=["plain"], w=[qbS])
    k.dma("sp", knS[:], akT.ap()[:, :, T:TT].rearrange("h d s -> d h s"), r=["plain"], w=[knS])
    scS = k.alloc("scS", [128, TS, 128], F32)
    snew = k.alloc("snew", [128, TS], F32)
    Gp = k.alloc("Gp", [128, NPG * 128], F32)
    kTs = [k.alloc(f"kTs{i}", [128, 4, 128], BF16) for i in range(2)]
    rr = k.alloc("rr", [128, 32, 16], F32)
    knb = k.alloc("knb", [128, 128], BF16)
    ckx2 = cache_kidx.ap()

    def dma_raw(q, fn, r=(), w=()):
        i = k.drr[q]
        k.drr[q] = (i + 1) % len(k.dsem[q])
        sk = ("d", q, i)
        waits = k._collect(q, r, w)
        prev = k.dcnt[q][i]
        kn = k.known[q]
        if prev > 0 and kn.get(sk, 0) < prev:
            kn[sk] = prev
            waits.append((sk, prev))
        k.dcnt[q][i] = prev + 16
        tok = (sk, prev + 16)
        k.ops[q].append((waits, fn, (sk, 16)))
        k._update(tok, r, w)
        return tok

    def scores_seq(s_i):
        dma_raw("pool", lambda e: e.indirect_dma_start(out=Gp[:], out_offset=None, in_=ckx2, in_offset=bass.IndirectOffsetOnAxis(ap=pt_i[:, s_i:s_i + 1], axis=0)),
                r=[pt_i], w=[Gp])
        scb = [k.bank() for _ in range(4)]
        k.reserved = {b_.name for b_ in scb}
        for sb in range(32):
            bt_ = k.bank()
            kt_ = kTs[sb % 2]
            for j in range(4):
                sl_ = 4 * sb + j
                k.op("pe", lambda e, j=j, sl_=sl_, bt_=bt_: e.transpose(out=bt_[:, j * 128:(j + 1) * 128], in_=Gp[:, sl_ * 128:(sl_ + 1) * 128], identity=ident_f[:]),
                     r=[Gp, ident_f], w=[bt_])
            k.op("act", lambda e, bt_=bt_, kt_=kt_: e.copy(out=kt_[:].rearrange("p a b -> p (a b)"), in_=bt_[:, :]), r=[bt_], w=[kt_])
            for j in range(4):
                sl_ = 4 * sb + j
                sbk = scb[sl_ // 32]
                k.op("pe", lambda e, j=j, sl_=sl_, sbk=sbk, kt_=kt_: e.matmul(sbk[:, (sl_ % 32) * 16:(sl_ % 32 + 1) * 16], lhsT=kt_[:, j, :], rhs=qiS[:, s_i, :], start=True, stop=True),
                     r=[kt_, qiS], w=[sbk])
        k.reserved = set()
        for b_i in range(4):
            sbk = scb[b_i]
            k.op("dve", lambda e, sbk=sbk: e.tensor_scalar(out=rr[:].rearrange("p a b -> p (a b)"), in0=sbk[:, :], scalar1=0.0, scalar2=None, op0=ALU.max), r=[sbk], w=[rr])
            k.op("dve", lambda e: e.tensor_tensor(out=rr[:], in0=rr[:], in1=wS[:, s_i, :].unsqueeze(1).to_broadcast([128, 32, 16]), op=ALU.mult), r=[rr, wS], w=[rr])
            k.op("dve", lambda e, b_i=b_i: e.tensor_reduce(out=scS[:, s_i, b_i * 32:(b_i + 1) * 32], in_=rr[:], axis=AX.X, op=ALU.add), r=[rr], w=[scS])
        k.op("dve", lambda e: e.tensor_copy(out=knb[:], in_=kiS[:, s_i:s_i + 1].to_broadcast([128, 128])), r=[kiS], w=[knb])
        bn = k.bank()
        k.op("pe", lambda e: e.matmul(bn[:, 0:16], lhsT=knb[:], rhs=qiS[:, s_i, :], start=True, stop=True), r=[knb, qiS], w=[bn])
        k.op("dve", lambda e: e.tensor_scalar(out=rr[:, 0, :], in0=bn[:, 0:16], scalar1=0.0, scalar2=None, op0=ALU.max), r=[bn], w=[rr])
        k.op("dve", lambda e: e.tensor_tensor(out=rr[:, 0, :], in0=rr[:, 0, :], in1=wS[:, s_i, :], op=ALU.mult), r=[rr, wS], w=[rr])
        k.op("dve", lambda e: e.tensor_reduce(out=snew[:, s_i:s_i + 1], in_=rr[:, 0, :], axis=AX.X, op=ALU.add), r=[rr], w=[snew])

    for s_i in range(0 if skip_p4s else TS):
        scores_seq(s_i)

    pm = k.alloc("pm", [128, 2 * TS], F32)
    gmm = k.alloc("gmm", [TS, 4], F32)
    dgm = k.alloc("dgm", [TS, 2 * TS], F32)
    lo_ = k.alloc("lo_", [128, TS], F32)
    w0_ = k.alloc("w0_", [128, TS], F32)
    bsS = k.alloc("bsS", [128, 6 * TS], F32)
    cmpb = k.alloc("cmpb", [128, TS, 128], F32)
    k.op("dve", lambda e: e.tensor_reduce(out=pm[:, 0:TS], in_=scS[:], axis=AX.X, op=ALU.max), r=[scS], w=[pm])
    k.op("dve", lambda e: e.tensor_reduce(out=pm[:, TS:2 * TS], in_=scS[:], axis=AX.X, op=ALU.min), r=[scS, pm], w=[pm])
    k.op("dve", lambda e: e.tensor_tensor(out=pm[:, 0:TS], in0=pm[:, 0:TS], in1=snew[:], op=ALU.max), r=[pm, snew], w=[pm])
    k.op("dve", lambda e: e.tensor_tensor(out=pm[:, TS:2 * TS], in0=pm[:, TS:2 * TS], in1=snew[:], op=ALU.min), r=[pm, snew], w=[pm])
    bmx = k.bank()
    bmn = k.bank()
    k.op("pe", lambda e: e.transpose(out=bmx[0:TS, 0:128], in_=pm[:, 0:TS], identity=ident_f[:]), r=[pm, ident_f], w=[bmx])
    k.op("pe", lambda e: e.transpose(out=bmn[0:TS, 0:128], in_=pm[:, TS:2 * TS], identity=ident_f[:]), r=[pm, ident_f], w=[bmn])
    k.op("dve", lambda e: e.tensor_reduce(out=gmm[:, 0:1], in_=bmx[0:TS, 0:128], axis=AX.X, op=ALU.max), r=[bmx], w=[gmm])
    k.op("dve", lambda e: e.tensor_reduce(out=gmm[:, 1:2], in_=bmn[0:TS, 0:128], axis=AX.X, op=ALU.min), r=[bmn, gmm], w=[gmm])
    k.op("dve", lambda e: e.tensor_scalar(out=gmm[:, 1:2], in0=gmm[:, 1:2], scalar1=-1.0, scalar2=None, op0=ALU.add), r=[gmm], w=[gmm])
    k.op("dve", lambda e: e.tensor_tensor(out=gmm[:, 2:3], in0=gmm[:, 0:1], in1=gmm[:, 1:2], op=ALU.subtract), r=[gmm], w=[gmm])
    k.op("dve", lambda e: e.tensor_scalar(out=dgm[:, 0:TS], in0=ident_f[0:TS, 0:TS], scalar1=gmm[:, 1:2], scalar2=None, op0=ALU.mult), r=[gmm, ident_f], w=[dgm])
    k.op("dve", lambda e: e.tensor_scalar(out=dgm[:, TS:2 * TS], in0=ident_f[0:TS, 0:TS], scalar1=gmm[:, 2:3], scalar2=None, op0=ALU.mult), r=[gmm, ident_f, dgm], w=[dgm])
    bbc = k.bank()
    k.op("pe", lambda e: e.matmul(bbc[:, 0:2 * TS], lhsT=ones4[0:TS, :], rhs=dgm[:], start=True, stop=True), r=[ones4, dgm], w=[bbc])
    k.op("act", lambda e: e.copy(out=lo_[:], in_=bbc[:, 0:TS]), r=[bbc], w=[lo_])
    k.op("act", lambda e: e.copy(out=w0_[:], in_=bbc[:, TS:2 * TS]), r=[bbc], w=[w0_])
    mid_ = bsS[:, 0:TS]
    cnt_ = bsS[:, TS:2 * TS]
    gn_ = bsS[:, 2 * TS:3 * TS]
    tot_ = bsS[:, 3 * TS:4 * TS]
    ge_ = bsS[:, 4 * TS:5 * TS]

    def bis_iter(it):
        f = 2.0 ** -(it + 1)
        k.op("dve", lambda e: e.scalar_tensor_tensor(out=mid_, in0=w0_[:], scalar=f, op0=ALU.mult, in1=lo_[:], op1=ALU.add), r=[w0_, lo_], w=[bsS])
        k.op("dve", lambda e: e.tensor_tensor(out=cmpb[:], in0=scS[:], in1=mid_.unsqueeze(2).to_broadcast([128, TS, 128]), op=ALU.is_gt), r=[scS, bsS], w=[cmpb])
        k.op("dve", lambda e: e.tensor_reduce(out=cnt_, in_=cmpb[:], axis=AX.X, op=ALU.add), r=[cmpb, bsS], w=[bsS])
        bc_ = k.bank()
        k.op("pe", lambda e: e.matmul(bc_[:, 0:TS], lhsT=ones4[:], rhs=cnt_, start=True, stop=True), r=[ones4, bsS], w=[bc_])
        k.op("dve", lambda e: e.tensor_tensor(out=gn_, in0=snew[:], in1=mid_, op=ALU.is_gt), r=[snew, bsS], w=[bsS])
        k.op("dve", lambda e: e.tensor_tensor(out=tot_, in0=bc_[:, 0:TS], in1=gn_, op=ALU.add), r=[bc_, bsS], w=[bsS])
        k.op("dve", lambda e: e.tensor_scalar(out=ge_, in0=tot_, scalar1=255.5, scalar2=f, op0=ALU.is_gt, op1=ALU.mult), r=[bsS], w=[bsS])
        k.op("dve", lambda e: e.tensor_tensor(out=ge_, in0=ge_, in1=w0_[:], op=ALU.mult), r=[bsS, w0_], w=[bsS])
        k.op("dve", lambda e: e.tensor_tensor(out=lo_[:], in0=lo_[:], in1=ge_, op=ALU.add), r=[lo_, bsS], w=[lo_])

    for it in range(0 if skip_p4s else 20):
        bis_iter(it)

    Msel = k.alloc("Msel", [128, 128], F32)
    Mb = k.alloc("Mb", [128, 128], BF16)
    Bs = k.alloc("Bs", [128, 128], F32)
    cum = k.alloc("cum", [128, 128], F32)
    rank = k.alloc("rank", [128, 128], F32)
    payl = k.alloc("payl", [128, 128, 2], F32)
    Soh = [k.alloc(f"Soh{i}", [128, 256], F32) for i in range(2)]
    idxf = k.alloc("idxf", [128, 4], F32)
    idx_i = k.alloc("idx_i", [128, 2], I32)
    Ksel = k.alloc("Ksel", [128, 2, 256], F32)
    Vsel = k.alloc("Vsel", [128, 2, 256], F32)
    KTs = k.alloc("KTs", [128, 4, 128], F32)
    qf = k.alloc("qf", [128, 8], F32)
    knf = k.alloc("knf", [128, 2], F32)
    sm = k.alloc("sm", [128, 64], F32)
    ind = k.alloc("ind", [128, 2, 31], F32)
    prod = k.alloc("prod", [128, 2, 8, 31], F32)
    Eg = k.alloc("Eg", [128, 2, 8], F32)
    rowp = k.alloc("rowp", [1, 64], F32)
    vnr = k.alloc("vnr", [1, 256], BF16)
    vnf = k.alloc("vnf", [1, 256], F32)
    osb = k.alloc("osb", [4, 2, 130], F32)
    ck2 = cache_k.ap()
    cv2 = cache_v.ap()
    k.op("dve", lambda e: e.tensor_copy(out=payl[:, :, 1], in_=posc[:]), r=[posc], w=[payl])

    def attend_seq(s_i):
        col = T + s_i
        k.op("dve", lambda e: e.tensor_scalar(out=Msel[:], in0=scS[:, s_i, :], scalar1=lo_[:, s_i:s_i + 1], scalar2=None, op0=ALU.is_gt), r=[scS, lo_], w=[Msel])
        k.op("dve", lambda e: e.tensor_copy(out=Mb[:], in_=Msel[:]), r=[Msel], w=[Mb])
        k.op("dve", lambda e: e.tensor_tensor(out=sm[:, 0:1], in0=snew[:, s_i:s_i + 1], in1=lo_[:, s_i:s_i + 1], op=ALU.is_gt), r=[snew, lo_], w=[sm])
        bA_ = k.bank()
        bB_ = k.bank()
        k.op("pe", lambda e: e.matmul(bA_[:, 0:128], lhsT=Ltri[:], rhs=Mb[:], start=True, stop=True), r=[Ltri, Mb], w=[bA_])
        k.op("pe", lambda e: e.matmul(bB_[:, 0:128], lhsT=ones_b[:], rhs=Mb[:], start=True, stop=True), r=[ones_b, Mb], w=[bB_])
        k.op("act", lambda e: e.copy(out=Bs[:], in_=bB_[:, 0:128]), r=[bB_], w=[Bs])
        k.op("dve", lambda e: e.tensor_tensor_scan(out=cum[:], data0=Bs[:], data1=zeros4[:], initial=0.0, op0=ALU.add, op1=ALU.add), r=[Bs, zeros4], w=[cum])
        k.op("dve", lambda e: e.tensor_tensor(out=rank[:], in0=cum[:], in1=Bs[:], op=ALU.subtract), r=[cum, Bs], w=[rank])
        k.op("dve", lambda e: e.tensor_tensor(out=rank[:], in0=rank[:], in1=bA_[:, 0:128], op=ALU.add), r=[rank, bA_], w=[rank])
        k.op("dve", lambda e: e.scalar_tensor_tensor(out=rank[:], in0=rank[:], scalar=1.0, op0=ALU.add, in1=Msel[:], op1=ALU.mult), r=[rank, Msel], w=[rank])
        k.op("dve", lambda e: e.tensor_scalar(out=rank[:], in0=rank[:], scalar1=-1.0, scalar2=None, op0=ALU.add), r=[rank], w=[rank])
        k.op("dve", lambda e: e.tensor_scalar(out=payl[:, :, 0], in0=siota[:], scalar1=pt_f[:, s_i:s_i + 1], scalar2=None, op0=ALU.add), r=[siota, pt_f], w=[payl])
        bacc = k.bank()
        k.reserved = {bacc.name}
        k.op("pe", lambda e: e.matmul(bacc[:, 0:4], lhsT=zeros4[:], rhs=ones4[:, 0:4], start=True, stop=False), r=[zeros4, ones4], w=[bacc])
        for sl_ in range(128):
            so_ = Soh[sl_ % 2]
            k.op("dve", lambda e, sl_=sl_, so_=so_: e.tensor_scalar(out=so_[:], in0=jrow[:], scalar1=rank[:, sl_:sl_ + 1], scalar2=None, op0=ALU.is_equal), r=[jrow, rank], w=[so_])
            for half in range(2):
                k.op("pe", lambda e, sl_=sl_, so_=so_, half=half: e.matmul(bacc[:, half * 2:(half + 1) * 2], lhsT=so_[:, half * 128:(half + 1) * 128], rhs=payl[:, sl_, :],
                                                                      start=False, stop=(sl_ == 127)), r=[so_, payl], w=[bacc])
        k.reserved = set()
        k.op("act", lambda e: e.copy(out=idxf[:], in_=bacc[:, 0:4]), r=[bacc], w=[idxf])
        k.op("dve", lambda e: e.tensor_copy(out=idx_i[:], in_=idxf[:].rearrange("p (a b) -> p a b", b=2)[:, :, 0]), r=[idxf], w=[idx_i])
        for half in range(2):
            dma_raw("pool", lambda e, half=half: e.indirect_dma_start(out=Ksel[:, half, :], out_offset=None, in_=ck2, in_offset=bass.IndirectOffsetOnAxis(ap=idx_i[:, half:half + 1], axis=0)),
                    r=[idx_i], w=[Ksel])
            dma_raw("pool", lambda e, half=half: e.indirect_dma_start(out=Vsel[:, half, :], out_offset=None, in_=cv2, in_offset=bass.IndirectOffsetOnAxis(ap=idx_i[:, half:half + 1], axis=0)),
                    r=[idx_i], w=[Vsel])
        bkt = k.bank()
        for half in range(2):
            for kvh in range(2):
                jj = half * 2 + kvh
                k.op("pe", lambda e, half=half, kvh=kvh, jj=jj: e.transpose(out=bkt[:, jj * 128:(jj + 1) * 128], in_=Ksel[:, half, kvh * 128:(kvh + 1) * 128], identity=ident_f[:]),
                     r=[Ksel, ident_f], w=[bkt])
        k.op("act", lambda e: e.copy(out=KTs[:].rearrange("p a b -> p (a b)"), in_=bkt[:, :]), r=[bkt], w=[KTs])
        k.op("dve", lambda e: e.tensor_copy(out=qf[:], in_=qbS[:, :, s_i]), r=[qbS], w=[qf])
        k.op("dve", lambda e: e.tensor_copy(out=knf[:], in_=knS[:, :, s_i]), r=[knS], w=[knf])
        blg = k.bank()
        for half in range(2):
            for kvh in range(2):
                jj = half * 2 + kvh
                k.op("pe", lambda e, half=half, kvh=kvh, jj=jj: e.matmul(blg[:, half * 8 + kvh * 4:half * 8 + kvh * 4 + 4], lhsT=KTs[:, jj, :], rhs=qf[:, kvh * 4:(kvh + 1) * 4], start=True, stop=True),
                     r=[KTs, qf], w=[blg])
        bln = k.bank()
        for kvh in range(2):
            k.op("pe", lambda e, kvh=kvh: e.matmul(bln[0:1, kvh * 4:(kvh + 1) * 4], lhsT=knf[:, kvh:kvh + 1], rhs=qf[:, kvh * 4:(kvh + 1) * 4], start=True, stop=True), r=[knf, qf], w=[bln])
        k.op("dve", lambda e: e.tensor_scalar(out=sm[:, 2:4], in0=idxf[:].rearrange("p (a b) -> p a b", b=2)[:, :, 1], scalar1=-1.0, scalar2=float(NPG * 128), op0=ALU.mult, op1=ALU.add), r=[idxf], w=[sm])
        k.op("dve", lambda e: e.tensor_tensor(out=ind[:], in0=sm[:, 2:4].unsqueeze(2).to_broadcast([128, 2, 31]), in1=thrb[:].unsqueeze(1).to_broadcast([128, 2, 31]), op=ALU.is_ge), r=[sm, thrb], w=[ind])
        k.op("dve", lambda e: e.tensor_tensor(out=prod[:], in0=ind[:].unsqueeze(2).to_broadcast([128, 2, 8, 31]), in1=dtmp[:].unsqueeze(1).to_broadcast([128, 2, 8, 31]), op=ALU.mult), r=[ind, dtmp], w=[prod])
        k.op("dve", lambda e: e.tensor_reduce(out=Eg[:], in_=prod[:], axis=AX.X, op=ALU.add), r=[prod], w=[Eg])
        k.op("dve", lambda e: e.tensor_tensor(out=Eg[:], in0=Eg[:], in1=rb0b[:].unsqueeze(1).to_broadcast([128, 2, 8]), op=ALU.add), r=[Eg, rb0b], w=[Eg])
        k.op("dve", lambda e: e.tensor_scalar(out=sm[:, 4:6], in0=jcol[:], scalar1=cum[:, 127:128], scalar2=None, op0=ALU.is_lt), r=[jcol, cum, sm], w=[sm])
        k.op("dve", lambda e: e.tensor_scalar(out=sm[:, 4:6], in0=sm[:, 4:6], scalar1=-1.0, scalar2=30000.0, op0=ALU.add, op1=ALU.mult), r=[sm], w=[sm])
        k.op("dve", lambda e: e.tensor_tensor(out=Eg[:], in0=Eg[:], in1=sm[:, 4:6].unsqueeze(2).to_broadcast([128, 2, 8]), op=ALU.add), r=[Eg, sm], w=[Eg])
        k.op("dve", lambda e: e.tensor_tensor(out=Eg[:].rearrange("p a b -> p (a b)"), in0=Eg[:].rearrange("p a b -> p (a b)"), in1=blg[:, 0:16], op=ALU.add), r=[Eg, blg], w=[Eg])
        k.op("act", lambda e: e.activation(out=Eg[:], in_=Eg[:], func=AF.Exp), r=[Eg], w=[Eg])
        k.op("dve", lambda e: e.tensor_scalar(out=rowp[:, 8:9], in0=sm[0:1, 0:1], scalar1=-1.0, scalar2=30000.0, op0=ALU.add, op1=ALU.mult), r=[sm], w=[rowp])
        k.op("dve", lambda e: e.tensor_tensor(out=rowp[:, 0:8], in0=bln[0:1, 0:8], in1=rb0b[0:1, :], op=ALU.add), r=[bln, rb0b, rowp], w=[rowp])
        k.op("dve", lambda e: e.tensor_scalar(out=rowp[:, 0:8], in0=rowp[:, 0:8], scalar1=rowp[:, 8:9], scalar2=None, op0=ALU.add), r=[rowp], w=[rowp])
        k.op("act", lambda e: e.activation(out=rowp[:, 0:8], in_=rowp[:, 0:8], func=AF.Exp), r=[rowp], w=[rowp])
        k.dma("sp", vnr[:], av[col:col + 1, :], r=["av"], w=[vnr])
        k.op("dve", lambda e: e.tensor_copy(out=vnf[:], in_=vnr[:]), r=[vnr], w=[vnf])
        for kvh in range(2):
            bon = k.bank()
            bod = k.bank()
            for half in range(2):
                k.op("pe", lambda e, kvh=kvh, half=half, bon=bon: e.matmul(bon[0:4, 0:128], lhsT=Eg[:, half, kvh * 4:(kvh + 1) * 4], rhs=Vsel[:, half, kvh * 128:(kvh + 1) * 128], start=(half == 0), stop=False),
                     r=[Eg, Vsel], w=[bon])
            k.op("pe", lambda e, kvh=kvh, bon=bon: e.matmul(bon[0:4, 0:128], lhsT=rowp[0:1, kvh * 4:(kvh + 1) * 4], rhs=vnf[0:1, kvh * 128:(kvh + 1) * 128], start=False, stop=True), r=[rowp, vnf], w=[bon])
            for half in range(2):
                k.op("pe", lambda e, kvh=kvh, half=half, bod=bod: e.matmul(bod[0:4, 0:1], lhsT=Eg[:, half, kvh * 4:(kvh + 1) * 4], rhs=ones4[:, 0:1], start=(half == 0), stop=False), r=[Eg, ones4], w=[bod])
            k.op("pe", lambda e, kvh=kvh, bod=bod: e.matmul(bod[0:4, 0:1], lhsT=rowp[0:1, kvh * 4:(kvh + 1) * 4], rhs=ones4[0:1, 0:1], start=False, stop=True), r=[rowp, ones4], w=[bod])
            k.op("dve", lambda e, kvh=kvh, bod=bod: e.reciprocal(out=osb[:, kvh, 128:129], in_=bod[0:4, 0:1]), r=[bod], w=[osb])
            k.op("dve", lambda e, kvh=kvh, bon=bon: e.tensor_scalar(out=osb[:, kvh, 0:128], in0=bon[0:4, 0:128], scalar1=osb[:, kvh, 128:129], scalar2=None, op0=ALU.mult), r=[bon, osb], w=[osb])
        bot = k.bank()
        for kvh in range(2):
            k.op("pe", lambda e, kvh=kvh: e.transpose(out=bot[:, kvh * 4:(kvh + 1) * 4], in_=osb[:, kvh, 0:128], identity=ident_f[0:4, 0:4]), r=[osb, ident_f], w=[bot])
        k.op("act", lambda e: e.copy(out=mixT[:, 8:16, col], in_=bot[:, 0:8]), r=[bot], w=[(mixT, "sb")])

    for s_i in range(0 if skip_p4s else TS):
        attend_seq(s_i)
    k.barrier()
    k.release(m4)
    m5 = k.mark()
    wout = k.alloc("wout", [128, 16, D], BF16)
    for g in range(4):
        k.dma("pool", wout[:, :, g * 512:(g + 1) * 512], w_out[:, g * 512:(g + 1) * 512].rearrange("(k p) n -> p k n", p=128), w=[(wout, g)])
    G1b = k.alloc("G1b", [128, D], BF16)
    A2b = k.alloc("A2b", [128, D], BF16)
    SH2b = k.alloc("SH2b", [128, D], BF16)
    for buf_, idx_ in ((G1b, 2), (SH2b, 3), (A2b, 4)):
        k.dma("pool", buf_[:], modd[0:1, idx_ * D:(idx_ + 1) * D].to_broadcast([128, D]), r=["modd"], w=[buf_])
    xt5 = [k.alloc(f"xt5_{i}", [128, D], F32) for i in range(2)]
    mos = [k.alloc(f"mo{i}", [128, D], F32) for i in range(2)]
    h2b = k.alloc("h2b", [128, D], BF16)
    jnk5 = h2b
    st5 = [k.alloc(f"st5_{i}", [128, 8], F32) for i in range(2)]
    h2st = [k.alloc(f"h2st{i}", [128, 16, 128], BF16) for i in range(2)]

    def p5_tile(ti):
        c0, n = (ti * 128, 128) if ti < NT else (T, TS)
        x_ = xt5[ti % 2]
        mo = mos[ti % 2]
        s_ = st5[ti % 2]
        hs_ = h2st[ti % 2]
        G1_, A2_, SH2_ = (G1b, A2b, SH2b)
        if ti == NT:
            k.dma("pool", G1b[0:TS, :], modd[1:5, 2 * D:3 * D], r=["modd"], w=[G1b])
            k.dma("pool", SH2b[0:TS, :], modd[1:5, 3 * D:4 * D], r=["modd"], w=[SH2b])
            k.dma("pool", A2b[0:TS, :], modd[1:5, 4 * D:5 * D], r=["modd"], w=[A2b])
        if ti == 0:
            k.dma("sp", x_[0:n, :], xp[c0:c0 + n, :], w=[x_])
        if ti + 1 <= NT:
            tn = ti + 1
            cn, nn = (tn * 128, 128) if tn < NT else (T, TS)
            xn_ = xt5[tn % 2]
            k.dma("sp", xn_[0:nn, :], xp[cn:cn + nn, :] if tn < NT else xs.ap(), w=[xn_])
        mkeys = [(mixT, ti), (mixT, "b%d" % ti)] if ti < NT else [(mixT, "s%d" % j) for j in range(TS)] + [(mixT, "sb")]
        for nq in range(4):
            bk_ = k.bank()
            for kk in range(16):
                k.op("pe", lambda e, kk=kk, bk_=bk_, nq=nq: e.matmul(bk_[0:n, :], lhsT=mixT[:, kk, c0:c0 + n], rhs=wout[:, kk, nq * 512:(nq + 1) * 512], start=(kk == 0), stop=(kk == 15)),
                     r=mkeys + [(wout, nq)], w=[bk_])
            k.op("act", lambda e, bk_=bk_, nq=nq: e.copy(out=mo[0:n, nq * 512:(nq + 1) * 512], in_=bk_[0:n, :]), r=[bk_], w=[mo])
        k.op("act", lambda e: e.activation(out=jnk5[0:n, :], in_=mo[0:n, :], func=AF.Square, accum_out=s_[0:n, 0:1]), r=[mo], w=[h2b, s_])
        k.op("act", lambda e: e.activation(out=s_[0:n, 1:2], in_=s_[0:n, 0:1], func=AF.Sqrt, scale=1.0 / D, bias=EPS), r=[s_], w=[s_])
        k.op("dve", lambda e: e.reciprocal(out=s_[0:n, 2:3], in_=s_[0:n, 1:2]), r=[s_], w=[s_])
        k.op("dve", lambda e: e.scalar_tensor_tensor(out=mo[0:n, :], in0=mo[0:n, :], scalar=s_[0:n, 2:3], op0=ALU.mult, in1=G1_[0:n, :], op1=ALU.mult), r=[mo, s_, G1_], w=[mo])
        k.op("pool", lambda e: e.tensor_tensor(out=x_[0:n, :], in0=x_[0:n, :], in1=mo[0:n, :], op=ALU.add), r=[x_, mo], w=[x_])
        k.dma("sp", x1d[c0:c0 + n, :], x_[0:n, :], r=[x_], w=["x1d"])
        k.op("act", lambda e: e.activation(out=jnk5[0:n, :], in_=x_[0:n, :], func=AF.Square, accum_out=s_[0:n, 3:4]), r=[x_, s_], w=[h2b, s_])
        k.op("act", lambda e: e.activation(out=s_[0:n, 4:5], in_=s_[0:n, 3:4], func=AF.Sqrt, scale=1.0 / D, bias=EPS), r=[s_], w=[s_])
        k.op("dve", lambda e: e.reciprocal(out=s_[0:n, 5:6], in_=s_[0:n, 4:5]), r=[s_], w=[s_])
        k.op("dve", lambda e: e.scalar_tensor_tensor(out=mo[0:n, :], in0=x_[0:n, :], scalar=s_[0:n, 5:6], op0=ALU.mult, in1=A2_[0:n, :], op1=ALU.mult), r=[x_, s_, A2_, mo], w=[mo])
        k.op("pool", lambda e: e.tensor_tensor(out=h2b[0:n, :], in0=mo[0:n, :], in1=SH2_[0:n, :], op=ALU.add), r=[mo, SH2_], w=[h2b])
        b0 = k.bank()
        b1 = k.bank()
        for kk in range(16):
            bb = b0 if kk < 8 else b1
            k.op("pe", lambda e, kk=kk, bb=bb: e.transpose(out=bb[:].bitcast(BF16)[:, (kk % 8) * 128:(kk % 8) * 128 + n], in_=h2b[0:n, kk * 128:(kk + 1) * 128], identity=ident_b[0:n, 0:n]),
                 r=[h2b, ident_b], w=[bb])
        for half, bb in ((0, b0), (1, b1)):
            k.op("act", lambda e, half=half, bb=bb: e.copy(out=hs_[:, half * 8:(half + 1) * 8, 0:n], in_=bb[:].bitcast(BF16).rearrange("p (a b) -> p a b", a=8)[:, :, 0:n]),
                 r=[bb], w=[hs_])
        k.dma("sp", h2Td[:, :, c0:c0 + n], hs_[:, :, 0:n], r=[hs_], w=["h2Td"])

    for ti in range(NT + 1):
        p5_tile(ti)
    k.barrier()
    k.release(mP)
    if stop_after == "P5":
        return k.finish()

    alloc_wb()
    TB = 512
    NBLK6 = T // TB
    uT = k.alloc("uT", [128, 64, TB], BF16)
    uTs = k.alloc("uTs", [128, 64, TS], BF16)
    h2Tb = k.alloc("h2Tb", [128, 16, TB], BF16)
    h2Ts = k.alloc("h2Ts", [128, 16, TS], BF16)
    w2b = [k.alloc(f"w2b{i}", [128, 8, 512], BF16) for i in range(2)]
    fbuf = [k.alloc(f"fbuf{i}", [128, D], F32) for i in range(4)]
    x1t = [k.alloc("x1t0", [128, D], F32)] * 2
    G2b = k.alloc("G2b", [128, D], F32)
    load_mod_bcast(G2b, 5)
    rl6 = [k.alloc(f"rl6_{i}", [128, TB], BF16) for i in range(2)]
    st6 = [k.alloc(f"st6_{i}", [128, 4], F32) for i in range(2)]
    w2cnt = [0]
    k.dma("sp", h2Ts[:], h2Td[:, :, T:TT], r=["h2Td"], w=[h2Ts])

    def ffn_block(b):
        with_s = (b == NBLK6 - 1)
        if b == 0:
            k.dma("sp", h2Tb[:], h2Td[:, :, b * TB:(b + 1) * TB], r=["h2Td"], w=[h2Tb])
        def phaseA(g):
            wt = wb[wcnt[0] % 2]
            wcnt[0] += 1
            k.dma("sp", wt[:], w1s[g], r=[("w1s", g)], w=[wt])
            for j in range(4):
                ch = g * 4 + j
                bk_ = k.bank()
                for kk in range(16):
                    k.op("pe", lambda e, kk=kk, bk_=bk_, j=j: e.matmul(bk_[:, :], lhsT=wt[:, kk, j * 128:(j + 1) * 128], rhs=h2Tb[:, kk, :], start=(kk == 0), stop=(kk == 15)),
                         r=[wt, h2Tb], w=[bk_])
                r_ = rl6[ch % 2]
                k.op("act", lambda e, bk_=bk_, r_=r_: e.activation(out=r_[:], in_=bk_[:, :], func=AF.Relu), r=[bk_], w=[r_])
                k.op("dve", lambda e, r_=r_, ch=ch: e.tensor_tensor(out=uT[:, ch, :], in0=r_[:], in1=r_[:], op=ALU.mult), r=[r_], w=[(uT, ch)])
                if with_s:
                    bs_ = k.bank()
                    for kk in range(16):
                        k.op("pe", lambda e, kk=kk, bs_=bs_, j=j: e.matmul(bs_[:, 0:TS], lhsT=wt[:, kk, j * 128:(j + 1) * 128], rhs=h2Ts[:, kk, :], start=(kk == 0), stop=(kk == 15)),
                             r=[wt, h2Ts], w=[bs_])
                    k.op("act", lambda e, bs_=bs_, ch=ch: e.activation(out=uTs[:, ch, :], in_=bs_[:, 0:TS], func=AF.Relu), r=[bs_], w=[(uTs, ch)])
                    k.op("dve", lambda e, ch=ch: e.tensor_tensor(out=uTs[:, ch, :], in0=uTs[:, ch, :], in1=uTs[:, ch, :], op=ALU.mult), r=[(uTs, ch)], w=[(uTs, ch)])
        for g in range(16):
            phaseA(g)
        def phaseB(qc):
            accs = [k.bank() for _ in range(4)]
            accS = k.bank() if with_s else None
            k.reserved = {a_.name for a_ in accs} | ({accS.name} if with_s else set())
            for fgg in range(8):
                w2 = w2b[w2cnt[0] % 2]
                w2cnt[0] += 1
                k.dma("pool", w2[:], w2s[qc, fgg], r=[("w2s", qc, fgg)], w=[w2])
                for c in range(8):
                    ch = fgg * 8 + c
                    first = (fgg == 0 and c == 0)
                    last = (fgg == 7 and c == 7)
                    for tt in range(4):
                        k.op("pe", lambda e, tt=tt, c=c, ch=ch, first=first, last=last, w2=w2: e.matmul(accs[tt][:, :], lhsT=uT[:, ch, tt * 128:(tt + 1) * 128], rhs=w2[:, c, :], start=first, stop=last),
                             r=[(uT, ch), w2], w=[accs[tt]])
                    if with_s:
                        k.op("pe", lambda e, c=c, ch=ch, first=first, last=last, w2=w2: e.matmul(accS[0:TS, :], lhsT=uTs[:, ch, :], rhs=w2[:, c, :], start=first, stop=last),
                             r=[(uTs, ch), w2], w=[accS])
            for tt in range(4):
                k.op("act", lambda e, tt=tt: e.copy(out=fbuf[tt][:, qc * 512:(qc + 1) * 512], in_=accs[tt][:, :]), r=[accs[tt]], w=[(fbuf[tt], qc)])
            if with_s:
                k.op("act", lambda e: e.copy(out=fs[0:TS, qc * 512:(qc + 1) * 512], in_=accS[0:TS, :]), r=[accS], w=[(fs, qc)])
            k.reserved = set()
        for qc in range(4):
            phaseB(qc)
        if b + 1 < NBLK6:
            k.dma("sp", h2Tb[:], h2Td[:, :, (b + 1) * TB:(b + 2) * TB], r=["h2Td"], w=[h2Tb])
        def epi(fb, n, x1src, ydst, G2_, i):
            x_ = x1t[i % 2]
            s_ = st6[i % 2]
            k.dma("pool", x_[0:n, :], x1src, r=["x1d"], w=[x_])
            fk = [(fb, q) for q in range(4)]
            k.op("act", lambda e: e.activation(out=w2b[0][:].rearrange("p a b -> p (a b)")[0:n, 0:D], in_=fb[0:n, :], func=AF.Square, accum_out=s_[0:n, 0:1]), r=fk, w=[w2b[0], s_])
            k.op("act", lambda e: e.activation(out=s_[0:n, 1:2], in_=s_[0:n, 0:1], func=AF.Sqrt, scale=1.0 / D, bias=EPS), r=[s_], w=[s_])
            k.op("dve", lambda e: e.reciprocal(out=s_[0:n, 2:3], in_=s_[0:n, 1:2]), r=[s_], w=[s_])
            k.op("dve", lambda e: e.scalar_tensor_tensor(out=fb[0:n, :], in0=fb[0:n, :], scalar=s_[0:n, 2:3], op0=ALU.mult, in1=G2_[0:n, :], op1=ALU.mult), r=fk + [s_, G2_], w=fk)
            k.op("pool", lambda e: e.tensor_tensor(out=x_[0:n, :], in0=x_[0:n, :], in1=fb[0:n, :], op=ALU.add), r=[x_] + fk, w=[x_])
            k.dma("pool", ydst, x_[0:n, :], r=[x_])
        for tt in range(4):
            r0 = b * TB + tt * 128
            epi(fbuf[tt], 128, x1d[r0:r0 + 128, :], y_p[r0:r0 + 128, :], G2b, tt)
        if with_s:
            k.dma("pool", G2b[0:TS, :], modd[1:5, 5 * D:6 * D], r=["modd"], w=[G2b])
            epi(fs, TS, x1d[T:TT, :], y_s.ap(), G2b, 0)

    fs = k.alloc("fs", [TS, D], F32)
    import os
    for b in range(NBLK6):
        ffn_block(b)
    k.barrier()
    return k.finish()


_CACHE = {}


def _core_inputs(i, a):
    f = np.ascontiguousarray
    return {
        "xp": f(a["x_prompt"][i]),
        "xs": f(a["x_sample"][4 * i:4 * i + 4, 0, :]),
        "c5": f(np.concatenate([a["c_prompt"][i:i + 1], a["c_sample"][4 * i:4 * i + 4]], axis=0)),
        "w_ada": f(a["w_ada"][0]),
        "b_ada": f(a["b_ada"][0][None, :]),
        "gvec": f(np.stack([a["pre1_g"][0], a["post1_g"][0], a["pre2_g"][0], a["post2_g"][0]])),
        "w_in": f(a["w_in"][0]),
        "w_out": f(a["w_out"][0]),
        "w_ff1": f(a["w_ff1"][0]),
        "w_ff2": f(a["w_ff2"][0]),
        "conv_w": f(a["conv_w"][0]),
        "st_conv": f(a["state_conv"][0, 4 * i:4 * i + 4].reshape(12, 3072)),
        "hv": f(np.concatenate([a["a_log"][0], a["dt_bias"][0]])[None, :]),
        "ln_gb": f(np.stack([a["idx_knorm_g"][0], a["idx_knorm_b"][0]])),
        "gdn_g": f(a["gdn_norm_g"][0][None, :]),
        "st_ssm": f(a["state_ssm"][0, 4 * i:4 * i + 4]),
        "rel_bias": f(a["rel_bias"]),
        "boh": _boh(),
        "bthr": _bthr(),
        "page_table": f(a["page_table"][4 * i:4 * i + 4]),
        "cache_kidx": a["cache_kidx"][0].reshape(-1, PAGE * 128),
        "cache_k": a["cache_k"][0].reshape(-1, 256),
        "cache_v": a["cache_v"][0].reshape(-1, 256),
    }


def kernel(**inputs):
    n = 8
    nc = build(n_pool=int(inputs["cache_k"].shape[1]))
    in_maps = [_core_inputs(i, inputs) for i in range(n)]
    res = run_bass_kernel_spmd(nc, in_maps, core_ids=list(range(n)))
    R = res.results
    cat = lambda name: np.stack([r[name] for r in R])
    y_p = cat("y_p")
    y_s = np.concatenate([r["y_s"] for r in R])[:, None, :]
    k_p = cat("k_p").reshape(1, 8, T, 2, 128)
    v_p = cat("v_p").reshape(1, 8, T, 2, 128)
    ki_p = cat("ki_p")[None]
    ssm_p = cat("ssm_p")[None]
    conv_p = cat("conv_p")[None]
    k_s = np.concatenate([r["k_s"] for r in R]).reshape(1, 32, 1, 2, 128)
    v_s = np.concatenate([r["v_s"] for r in R]).reshape(1, 32, 1, 2, 128)
    ki_s = np.concatenate([r["ki_s"] for r in R]).reshape(1, 32, 1, 128)
    ssm_s = np.concatenate([r["ssm_s"] for r in R])[None]
    conv_s = np.concatenate([r["conv_s"] for r in R])[None]
    return (y_p, y_s, k_p, v_p, ki_p, ssm_p, conv_p, k_s, v_s, ki_s, ssm_s, conv_s)
```

```python
import math
import numpy as np
import concourse.bass as bass
import concourse.mybir as mybir
from concourse.bass_utils import run_bass_kernel_spmd

F32 = mybir.dt.float32
BF16 = mybir.dt.bfloat16
I32 = mybir.dt.int32
ALU = mybir.AluOpType
AF = mybir.ActivationFunctionType
AX = mybir.AxisListType

ENGS = ("pe", "dve", "act", "pool", "sp")

D = 2048
T = 2048
TS = 4
TT = T + TS
NT = 16
NPROJ = 7840
DFF = 8192
EPS = 1e-6
NPAGES = 128
PAGE = 128
O_CONV, O_A, O_B, O_Z, O_QB, O_KB, O_VB, O_QI, O_WI, O_KI = 0, 3072, 3080, 3088, 4112, 5136, 5392, 5648, 7696, 7712


def _dsize(dt):
    return {F32: 4, BF16: 2, I32: 4}[dt]


class Buf:
    def __init__(self, name, ap):
        self.name = name
        self.ap = ap

    def __getitem__(self, key):
        return self.ap[key]


class KB:
    def __init__(self, n_dma_sems=(24, 8, 52)):
        self.nc = bass.Bass("TRN2", target_bir_lowering=False)
        nc = self.nc
        self.ops = {e: [] for e in ENGS}
        self._ctx = []
        self.psem = {}
        self.cnt = {e: 0 for e in ENGS}
        for e in ENGS:
            self.psem[e] = self._enter(nc.semaphore("p_" + e))
        self.dsem = {}
        self.dcnt = {}
        self.drr = {}
        for q, n in zip(("sp", "act", "pool"), n_dma_sems):
            self.dsem[q] = [self._enter(nc.semaphore(f"d_{q}{i}")) for i in range(n)]
            self.dcnt[q] = [0] * n
            self.drr[q] = 0
        self.known = {e: {} for e in ENGS}
        self.state = {}
        self.semobj = {}
        for e in ENGS:
            self.semobj[("p", e)] = self.psem[e]
        for q in self.dsem:
            for i, s in enumerate(self.dsem[q]):
                self.semobj[("d", q, i)] = s
        self.n_ops = 0
        self.arena = None
        self.aoff = 0
        self.awords = 0
        self.nbank = 0
        self.reserved = set()

    def _enter(self, cm):
        v = cm.__enter__()
        self._ctx.append(cm)
        return v

    def init_arena(self, words):
        self.arena = self._enter(self.nc.sbuf_tensor("arena", [128, words], F32))
        self.awords = words
        self.aoff = 0

    def alloc(self, name, shape, dt=F32):
        p = shape[0]
        n = int(np.prod(shape[1:]))
        words = (n * _dsize(dt) + 3) // 4
        words = (words + 7) // 8 * 8
        assert self.aoff + words <= self.awords, f"arena overflow at {name}: {self.aoff + words} > {self.awords}"
        ap = self.arena[0:p, self.aoff:self.aoff + words]
        self.aoff += words
        if dt != F32:
            ap = ap.bitcast(dt)
        ap = ap[:, 0:n]
        if len(shape) == 3:
            ap = ap.rearrange("p (a b) -> p a b", a=shape[1])
        elif len(shape) == 4:
            ap = ap.rearrange("p (a b c) -> p a b c", a=shape[1], b=shape[2])
        return Buf(name, ap)

    def mark(self):
        return self.aoff

    def release(self, m):
        self.aoff = m

    def psum_init(self):
        self.pbanks = []
        for i in range(4):
            t = self._enter(self.nc.psum_tensor(f"pp{i}", [128, 1024], F32))
            self.pbanks.append(Buf(f"bank{2 * i}", t[:, 0:512]))
            self.pbanks.append(Buf(f"bank{2 * i + 1}", t[:, 512:1024]))
        self.pdbl = [self._dbl(i) for i in range(4)]

    def _dbl(self, i):
        return None

    def bank(self):
        while True:
            b = self.pbanks[self.nbank % 8]
            self.nbank += 1
            if b.name not in self.reserved:
                return b

    @staticmethod
    def _key(x):
        if isinstance(x, tuple):
            return (KB._key(x[0]),) + tuple(x[1:])
        if isinstance(x, str):
            return x
        return x.name

    def _collect(self, eng, r, w):
        need = {}
        own = ("p", eng)

        def add(tok):
            if tok is None:
                return
            sk, v = tok
            if sk == own and eng == "pe":
                return
            if need.get(sk, 0) < v:
                need[sk] = v

        for x in r:
            st = self.state.get(self._key(x))
            if st:
                add(st[0])
        for x in w:
            st = self.state.get(self._key(x))
            if st:
                add(st[0])
                for t in st[1]:
                    add(t)
        waits = []
        kn = self.known[eng]
        for sk, v in need.items():
            if kn.get(sk, 0) >= v:
                continue
            kn[sk] = v
            waits.append((sk, v))
        return waits

    def _update(self, tok, r, w):
        for x in w:
            self.state[self._key(x)] = [tok, []]
        for x in r:
            kk = self._key(x)
            st = self.state.get(kk)
            if st is None:
                st = self.state[kk] = [None, []]
            st[1].append(tok)
            if len(st[1]) > 24:
                best = {}
                for sk, v in st[1]:
                    if best.get(sk, 0) < v:
                        best[sk] = v
                st[1] = list(best.items())

    def op(self, eng, fn, r=(), w=()):
        waits = self._collect(eng, r, w)
        self.cnt[eng] += 1
        tok = (("p", eng), self.cnt[eng])
        self.ops[eng].append((waits, fn, (("p", eng), 1)))
        self._update(tok, r, w)
        self.n_ops += 1
        return tok

    def dma(self, q, out, in_, r=(), w=(), slow=False):
        if slow:
            fn = lambda e: e.dma_start(out=out, in_=in_, allow_slow_non_contiguous=True)
        else:
            fn = lambda e: e.dma_start(out=out, in_=in_)
        i = self.drr[q]
        self.drr[q] = (i + 1) % len(self.dsem[q])
        sk = ("d", q, i)
        waits = self._collect(q, r, w)
        prev = self.dcnt[q][i]
        kn = self.known[q]
        if prev > 0 and kn.get(sk, 0) < prev:
            kn[sk] = prev
            waits.append((sk, prev))
        self.dcnt[q][i] = prev + 16
        tok = (sk, prev + 16)
        self.ops[q].append((waits, fn, (sk, 16)))
        self._update(tok, r, w)
        self.n_ops += 1
        return tok

    def barrier(self):
        toks = [(("p", e), self.cnt[e]) for e in ENGS if self.cnt[e] > 0]
        for q in self.dsem:
            for i, c in enumerate(self.dcnt[q]):
                if c > 0:
                    toks.append((("d", q, i), c))
        for e in ENGS:
            waits = []
            kn = self.known[e]
            for sk, v in toks:
                if sk == ("p", e):
                    continue
                if kn.get(sk, 0) < v:
                    kn[sk] = v
                    waits.append((sk, v))
            if waits:
                self.ops[e].append((waits, None, None))

    def check_deadlock(self):
        sem = {}
        ptr = {e: 0 for e in ENGS}
        progress = True
        while progress:
            progress = False
            for e in ENGS:
                lst = self.ops[e]
                while ptr[e] < len(lst):
                    waits, fn, inc = lst[ptr[e]]
                    if all(sem.get(sk, 0) >= v for sk, v in waits):
                        if inc is not None:
                            sem[inc[0]] = sem.get(inc[0], 0) + inc[1]
                        ptr[e] += 1
                        progress = True
                    else:
                        break
        stuck = {e: (ptr[e], len(self.ops[e])) for e in ENGS if ptr[e] < len(self.ops[e])}
        if stuck:
            for e in stuck:
                waits, fn, inc = self.ops[e][ptr[e]]
                print("STUCK", e, ptr[e], [(sk, v, sem.get(sk, 0)) for sk, v in waits])
            raise RuntimeError(f"deadlock in sync graph: {stuck}")

    def finish(self):
        self.barrier()
        self.check_deadlock()
        nc = self.nc
        ops = self.ops
        semobj = self.semobj

        def run(e, lst):
            for waits, fn, inc in lst:
                for sk, v in waits:
                    e.wait_ge(semobj[sk], v)
                if fn is not None:
                    ins = fn(e)
                    ins.then_inc(semobj[inc[0]], inc[1])

        with nc.Block() as block:
            @block.tensor
            def _(e):
                run(e, ops["pe"])

            @block.vector
            def _(e):
                run(e, ops["dve"])

            @block.scalar
            def _(e):
                run(e, ops["act"])

            @block.gpsimd
            def _(e):
                run(e, ops["pool"])

            @block.sync
            def _(e):
                run(e, ops["sp"])

        for cm in reversed(self._ctx):
            cm.__exit__(None, None, None)
        self._ctx = []
        return nc


def _t5_bucket_table():
    n = np.arange(0, 256, dtype=np.int32)
    max_exact = 16
    nf = np.maximum(n, 1).astype(np.float32)
    large = max_exact + (np.log(nf / np.float32(max_exact)) / np.float32(math.log(128 / max_exact))
                         * np.float32(32 - max_exact)).astype(np.int32)
    large = np.minimum(large, 31)
    return np.where(n < max_exact, n, large)


def _boh():
    tab = _t5_bucket_table()
    oh = np.zeros((32, 384), np.float32)
    for j in range(384):
        dist = min(max(j - 127, 0), 255)
        oh[tab[dist], j] = 1.0
    return oh


def _bthr():
    tab = _t5_bucket_table()
    thr = np.zeros((1, 31), np.float32)
    for kk in range(1, 32):
        nz = np.nonzero(tab >= kk)[0]
        thr[0, kk - 1] = float(nz[0]) if len(nz) else 1e9
    return thr


def build(stop_after=None, n_pool=5120, skip_p4s=False):
    k = KB()
    nc = k.nc

    def din(name, shape, dt=F32):
        return nc.dram_tensor(name, list(shape), dt, kind="ExternalInput")

    def dout(name, shape, dt=F32):
        return nc.dram_tensor(name, list(shape), dt, kind="ExternalOutput")

    xp = din("xp", [T, D])
    xs = din("xs", [TS, D])
    c5 = din("c5", [5, D])
    w_ada = din("w_ada", [D, 6 * D])
    b_ada = din("b_ada", [1, 6 * D])
    gvec = din("gvec", [4, D])
    w_in = din("w_in", [D, NPROJ])
    w_out = din("w_out", [D, D])
    w_ff1 = din("w_ff1", [D, DFF])
    w_ff2 = din("w_ff2", [DFF, D])
    conv_w = din("conv_w", [4, 3072])
    st_conv = din("st_conv", [TS * 3, 3072])
    hv = din("hv", [1, 16])
    ln_gb = din("ln_gb", [2, 128])
    gdn_g = din("gdn_g", [1, 128])
    st_ssm = din("st_ssm", [TS, 8, 128, 128])
    rel_bias = din("rel_bias", [32, 8])
    boh = din("boh", [32, 384])
    bthr = din("bthr", [1, 31])
    page_table = din("page_table", [TS, NPAGES], I32)
    cache_kidx = din("cache_kidx", [n_pool, PAGE * 128])
    cache_k = din("cache_k", [n_pool * PAGE, 256])
    cache_v = din("cache_v", [n_pool * PAGE, 256])

    y_p = dout("y_p", [T, D])
    y_s = dout("y_s", [TS, D])
    k_p = dout("k_p", [T, 256])
    v_p = dout("v_p", [T, 256])
    ki_p = dout("ki_p", [T, 128])
    ssm_p = dout("ssm_p", [8, 128, 128])
    conv_p = dout("conv_p", [3, 3072])
    k_s = dout("k_s", [TS, 256])
    v_s = dout("v_s", [TS, 256])
    ki_s = dout("ki_s", [TS, 128])
    ssm_s = dout("ssm_s", [TS, 8, 128, 128])
    conv_s = dout("conv_s", [TS, 3, 3072])

    modd = nc.dram_tensor("modd", [5, 6 * D], F32)
    gq = nc.dram_tensor("gq", [8, 128, TT], BF16)
    gk = nc.dram_tensor("gk", [8, 128, TT], BF16)
    gv = nc.dram_tensor("gv", [8, 128, TT], BF16)
    gz = nc.dram_tensor("gz", [TT, 1024], BF16)
    gab = nc.dram_tensor("gab", [TT, 16], F32)
    aq = nc.dram_tensor("aq", [8, 128, TT], BF16)
    akT = nc.dram_tensor("akT", [2, 128, TT], BF16)
    av = nc.dram_tensor("av", [TT, 256], BF16)
    iq = nc.dram_tensor("iq", [16, 128, TT], BF16)
    iw = nc.dram_tensor("iw", [TT, 16], F32)
    ikTd = nc.dram_tensor("ikTd", [128, TT], BF16)
    biasd = nc.dram_tensor("biasd", [8, 384], F32)
    rbTd = nc.dram_tensor("rbTd", [8, 32], F32)
    w1s = nc.dram_tensor("w1s", [16, 128, 16, 512], BF16)
    w2s = nc.dram_tensor("w2s", [4, 8, 128, 8, 512], BF16)
    x1d = nc.dram_tensor("x1d", [TT, D], F32)
    h2Td = nc.dram_tensor("h2Td", [128, 16, TT], BF16)

    k.init_arena(47 * 1024)
    k.psum_init()

    ident_f = k.alloc("ident_f", [128, 128], F32)
    ident_b = k.alloc("ident_b", [128, 128], BF16)
    ones_b = k.alloc("ones_b", [128, 128], BF16)
    k.op("pool", lambda e: e.memset(ident_f[:], 1.0), w=[ident_f])
    k.op("pool", lambda e: e.affine_select(out=ident_f[:], in_=ident_f[:], pattern=[[-1, 128]],
                                           compare_op=ALU.is_equal, fill=0.0, base=0, channel_multiplier=1),
         r=[ident_f], w=[ident_f])
    k.op("pool", lambda e: e.tensor_copy(out=ident_b[:], in_=ident_f[:]), r=[ident_f], w=[ident_b])
    k.op("pool", lambda e: e.memset(ones_b[:], 1.0), w=[ones_b])

    wb = []
    wcnt = [0]

    def alloc_wb():
        wb.clear()
        wb.extend(k.alloc(f"wb{i}", [128, 16, 512], BF16) for i in range(2))

    def load_w(src_dram, c0, ncols):
        b = wb[wcnt[0] % 2]
        wcnt[0] += 1
        k.dma("pool", b[:, :, 0:ncols], src_dram[:, c0:c0 + ncols].rearrange("(k p) n -> p k n", p=128), w=[b])
        return b

    m0 = k.mark()
    alloc_wb()
    c80 = k.alloc("c80", [80, 128], F32)
    cT = k.alloc("cT", [128, 16, 5], BF16)
    mod = k.alloc("mod", [5, 6 * D], F32)
    gv5 = k.alloc("gv5", [5, 4, D], F32)
    k.dma("sp", c80[:], c5.ap().rearrange("r (k p) -> (r k) p", p=128), w=[c80])
    k.dma("sp", mod[:], b_ada.ap().to_broadcast([5, 6 * D]), w=[mod])
    k.dma("sp", gv5[:].rearrange("p a b -> p (a b)"), gvec.ap().rearrange("a b -> (a b)").unsqueeze(0).to_broadcast([5, 4 * D]), w=[gv5])
    bk = k.bank()
    k.op("pe", lambda e: e.transpose(out=bk[:, 0:80], in_=c80[:], identity=ident_f[0:80, 0:80]), r=[c80, ident_f], w=[bk])
    k.op("act", lambda e: e.activation(out=cT[:], in_=bk[:, 0:80].rearrange("p (r k) -> p k r", r=5), func=AF.Silu), r=[bk], w=[cT])
    for n in range(24):
        wt = load_w(w_ada, n * 512, 512)
        bk = k.bank()
        for kk in range(16):
            k.op("pe", lambda e, kk=kk, wt=wt, bk=bk: e.matmul(bk[0:5, :], lhsT=cT[:, kk, :], rhs=wt[:, kk, :], start=(kk == 0), stop=(kk == 15)),
                 r=[cT, wt], w=[bk])
        k.op("dve", lambda e, n=n, bk=bk: e.tensor_tensor(out=mod[:, n * 512:(n + 1) * 512], in0=bk[0:5, :], in1=mod[:, n * 512:(n + 1) * 512], op=ALU.add),
             r=[bk, mod], w=[mod])
    for (sc, gi) in ((1, 0), (4, 2)):
        k.op("dve", lambda e, sc=sc, gi=gi: e.scalar_tensor_tensor(out=mod[:, sc * D:(sc + 1) * D], in0=mod[:, sc * D:(sc + 1) * D], scalar=1.0, op0=ALU.add,
                                                                    in1=gv5[:, gi, :], op1=ALU.mult), r=[mod, gv5], w=[mod])
    for (g, gi) in ((2, 1), (5, 3)):
        k.op("dve", lambda e, g=g, gi=gi: e.tensor_tensor(out=mod[:, g * D:(g + 1) * D], in0=mod[:, g * D:(g + 1) * D], in1=gv5[:, gi, :], op=ALU.mult),
             r=[mod, gv5], w=[mod])
    k.dma("sp", modd.ap(), mod[:], r=[mod], w=["modd"])
    k.barrier()
    k.release(m0)
    if stop_after == "P0":
        return k.finish()

    def load_mod_bcast(buf, idx):
        k.dma("sp", buf[:], modd[0:1, idx * D:(idx + 1) * D].to_broadcast([128, D]), r=["modd"], w=[buf])

    def load_mod_rows(buf, idx):
        k.dma("sp", buf[:], modd[1:5, idx * D:(idx + 1) * D], r=["modd"], w=[buf])

    mP = k.mark()
    hT = k.alloc("hT", [128, 16, TT], BF16)
    ikT = k.alloc("ikT", [128, TT], BF16)
    m1 = k.mark()
    A1 = k.alloc("A1", [128, D], F32)
    SH1 = k.alloc("SH1", [128, D], F32)
    A1s = k.alloc("A1s", [TS, D], F32)
    SH1s = k.alloc("SH1s", [TS, D], F32)
    load_mod_bcast(SH1, 0)
    load_mod_bcast(A1, 1)
    load_mod_rows(SH1s, 0)
    load_mod_rows(A1s, 1)
    xt = [k.alloc(f"xt{i}", [128, D], F32) for i in range(2)]
    hb = [k.alloc(f"hb{i}", [128, D], BF16) for i in range(2)]
    junk = k.alloc("junk", [128, D], BF16)
    st1 = [k.alloc(f"st1_{i}", [128, 4], F32) for i in range(2)]

    def norm_mod(i, np_, src_ap, A, SH, col0, ncol):
        x_ = xt[i % 2]
        h_ = hb[i % 2]
        s_ = st1[i % 2]
        k.dma("sp", x_[0:np_, :], src_ap, w=[x_])
        k.op("act", lambda e: e.activation(out=junk[0:np_, :], in_=x_[0:np_, :], func=AF.Square, accum_out=s_[0:np_, 0:1]), r=[x_], w=[junk, s_])
        k.op("act", lambda e: e.activation(out=s_[0:np_, 1:2], in_=s_[0:np_, 0:1], func=AF.Sqrt, scale=1.0 / D, bias=EPS), r=[s_], w=[s_])
        k.op("dve", lambda e: e.reciprocal(out=s_[0:np_, 2:3], in_=s_[0:np_, 1:2]), r=[s_], w=[s_])
        k.op("dve", lambda e: e.scalar_tensor_tensor(out=x_[0:np_, :], in0=x_[0:np_, :], scalar=s_[0:np_, 2:3], op0=ALU.mult, in1=A[0:np_, :], op1=ALU.mult),
             r=[x_, s_, A], w=[x_])
        k.op("pool", lambda e: e.tensor_tensor(out=h_[0:np_, :], in0=x_[0:np_, :], in1=SH[0:np_, :], op=ALU.add), r=[x_, SH], w=[h_])
        b0 = k.bank()
        b1 = k.bank()
        for kk in range(16):
            bb = b0 if kk < 8 else b1
            k.op("pe", lambda e, kk=kk, bb=bb: e.transpose(out=bb[:].bitcast(BF16)[:, (kk % 8) * 128:(kk % 8) * 128 + np_],
                                                           in_=h_[0:np_, kk * 128:(kk + 1) * 128], identity=ident_b[0:np_, 0:np_]),
                 r=[h_, ident_b], w=[bb])
        for half, bb in ((0, b0), (1, b1)):
            k.op("act", lambda e, half=half, bb=bb: e.copy(out=hT[:, half * 8:(half + 1) * 8, col0:col0 + ncol],
                                                          in_=bb[:].bitcast(BF16).rearrange("p (a b) -> p a b", a=8)[:, :, 0:ncol]),
                 r=[bb], w=[(hT, col0)])

    for i in range(NT):
        norm_mod(i, 128, xp[i * 128:(i + 1) * 128, :], A1, SH1, i * 128, 128)
    norm_mod(NT, TS, xs.ap(), A1s, SH1s, T, TS)
    k.barrier()
    k.release(m1)
    if stop_after == "P1":
        dbg = dout("dbg_hT", [128, 16 * TT], BF16)
        k.dma("sp", dbg.ap(), hT[:].rearrange("p a b -> p (a b)"), r=[hT])
        return k.finish()

    hT_keys = [(hT, i * 128) for i in range(NT)] + [(hT, T)]

    m2 = k.mark()
    alloc_wb()
    cw = k.alloc("cw", [128, 4, 24], F32)
    stc = k.alloc("stc", [128, TS * 3, 24], F32)
    cwl = k.alloc("cwl", [96, 128], F32)
    stl = k.alloc("stl", [96, 3, 128], F32)
    k.dma("sp", cwl[:], conv_w.ap().rearrange("j (ch c) -> (j ch) c", c=128), w=[cwl])
    k.dma("sp", stl[:], st_conv.ap().rearrange("r (ch c) -> (r ch) c", c=128).rearrange("(g p) c -> p g c", p=96), w=[stl])
    bk = k.bank()
    k.op("pe", lambda e, bk=bk: e.transpose(out=bk[:, 0:96], in_=cwl[:], identity=ident_f[0:96, 0:96]), r=[cwl, ident_f], w=[bk])
    k.op("act", lambda e, bk=bk: e.copy(out=cw[:].rearrange("p a b -> p (a b)"), in_=bk[:, 0:96]), r=[bk], w=[cw])
    bk = k.bank()
    for g in range(3):
        k.op("pe", lambda e, g=g, bk=bk: e.transpose(out=bk[:, g * 96:(g + 1) * 96], in_=stl[:, g, :], identity=ident_f[0:96, 0:96]),
             r=[stl, ident_f], w=[bk])
    k.op("act", lambda e, bk=bk: e.copy(out=stc[:].rearrange("p a b -> p (a b)"), in_=bk[:, 0:288]), r=[bk], w=[stc])
    k.dma("sp", conv_s.ap()[:, 0:2, :], st_conv.ap().rearrange("(s j) c -> s j c", j=3)[:, 1:3, :])

    cin = [k.alloc(f"cin{i}", [128, 3 + T], F32) for i in range(2)]
    for cb in cin:
        k.op("pool", lambda e, cb=cb: e.memset(cb[:, 0:3], 0.0), w=[cb])
    acc = [k.alloc(f"acc{i}", [128, TT], F32) for i in range(2)]
    cins = [k.alloc(f"cins{i}", [128, TS, 4], F32) for i in range(2)]
    tmp4 = [k.alloc(f"tmp4{i}", [128, TS, 4], F32) for i in range(2)]
    sqb = [k.alloc(f"sqb{i}", [128, TT], BF16) for i in range(2)]
    rsb = [k.alloc(f"rsb{i}", [128, TT], F32) for i in range(2)]
    ob = [k.alloc(f"ob{i}", [128, TT], BF16) for i in range(2)]
    obc = [0]

    def fm_matmuls(wt, wc0, tg, bk):
        c0, n = (tg * 512, 512) if tg < 4 else (T, TS)
        keys = hT_keys[tg * 4:(tg + 1) * 4] if tg < 4 else [hT_keys[16]]
        for kk in range(16):
            k.op("pe", lambda e, kk=kk: e.matmul(bk[:, 0:n], lhsT=wt[:, kk, wc0:wc0 + 128], rhs=hT[:, kk, c0:c0 + n], start=(kk == 0), stop=(kk == 15)),
                 r=[wt] + keys, w=[bk])

    def next_ob():
        b = ob[obc[0] % 2]
        obc[0] += 1
        return b

    chunk_i = [0]

    def conv_chunk(wt, wc0, ch):
        ci = chunk_i[0]
        chunk_i[0] += 1
        cb = cin[ci % 2]
        ac = acc[ci % 2]
        cs = cins[ci % 2]
        t4 = tmp4[ci % 2]
        for tg in range(4):
            bk = k.bank()
            fm_matmuls(wt, wc0, tg, bk)
            k.op("act", lambda e, bk=bk, tg=tg: e.copy(out=cb[:, 3 + tg * 512:3 + (tg + 1) * 512], in_=bk[:, 0:512]), r=[bk], w=[cb])
        bk = k.bank()
        fm_matmuls(wt, wc0, 4, bk)
        k.op("dve", lambda e: e.tensor_copy(out=cs[:, :, 0:3], in_=stc[:, :, ch].rearrange("p (s j) -> p s j", j=3)), r=[stc], w=[cs])
        k.op("dve", lambda e, bk=bk: e.tensor_copy(out=cs[:, :, 3:4], in_=bk[:, 0:TS].unsqueeze(2)), r=[bk, cs], w=[cs])
        k.dma("sp", conv_p.ap()[:, ch * 128:(ch + 1) * 128].rearrange("r c -> c r"), cb[:, T:T + 3], r=[cb], slow=True)
        k.dma("sp", conv_s.ap()[:, 2, ch * 128:(ch + 1) * 128].rearrange("s c -> c s"), cs[:, :, 3], r=[cs], slow=True)
        k.op("dve", lambda e: e.tensor_scalar(out=ac[:, 0:T], in0=cb[:, 0:T], scalar1=cw[:, 0, ch:ch + 1], scalar2=None, op0=ALU.mult), r=[cb, cw], w=[ac])
        for j in range(1, 4):
            k.op("dve", lambda e, j=j: e.scalar_tensor_tensor(out=ac[:, 0:T], in0=cb[:, j:j + T], scalar=cw[:, j, ch:ch + 1], op0=ALU.mult, in1=ac[:, 0:T], op1=ALU.add),
                 r=[cb, cw, ac], w=[ac])
        k.op("dve", lambda e: e.tensor_tensor(out=t4[:], in0=cs[:], in1=cw[:, :, ch].unsqueeze(1).to_broadcast([128, TS, 4]), op=ALU.mult), r=[cs, cw], w=[t4])
        k.op("dve", lambda e: e.tensor_reduce(out=ac[:, T:TT], in_=t4[:], axis=AX.X, op=ALU.add), r=[t4, ac], w=[ac])
        o = next_ob()
        if ch >= 16:
            k.op("act", lambda e: e.activation(out=o[:], in_=ac[:], func=AF.Silu), r=[ac], w=[o])
            k.dma("sp", gv[ch - 16], o[:], r=[o], w=["gv"])
            return
        sq = sqb[ci % 2]
        rs = rsb[ci % 2]
        k.op("act", lambda e: e.activation(out=ac[:], in_=ac[:], func=AF.Silu), r=[ac], w=[ac])
        k.op("act", lambda e: e.activation(out=sq[:], in_=ac[:], func=AF.Square), r=[ac], w=[sq])
        for tg in range(5):
            c0, n = (tg * 512, 512) if tg < 4 else (T, TS)
            bk = k.bank()
            k.op("pe", lambda e, bk=bk, c0=c0, n=n: e.matmul(bk[:, 0:n], lhsT=ones_b[:], rhs=sq[:, c0:c0 + n], start=True, stop=True), r=[sq, ones_b], w=[bk])
            k.op("act", lambda e, bk=bk, c0=c0, n=n: e.activation(out=rs[:, c0:c0 + n], in_=bk[:, 0:n], func=AF.Sqrt, bias=EPS, scale=1.0), r=[bk], w=[rs])
        k.op("dve", lambda e: e.reciprocal(out=rs[:], in_=rs[:]), r=[rs], w=[rs])
        scl = 128.0 ** -0.5 if ch < 8 else 1.0
        k.op("dve", lambda e: e.scalar_tensor_tensor(out=o[:], in0=ac[:], scalar=scl, op0=ALU.mult, in1=rs[:], op1=ALU.mult), r=[ac, rs], w=[o])
        dst = gq[ch] if ch < 8 else gk[ch - 8]
        k.dma("sp", dst, o[:], r=[o], w=["gqk"])

    def plain_chunk(wt, wc0, dst, scale):
        o = next_ob()
        for tg in range(5):
            c0, n = (tg * 512, 512) if tg < 4 else (T, TS)
            bk = k.bank()
            fm_matmuls(wt, wc0, tg, bk)
            k.op("act", lambda e, bk=bk, c0=c0, n=n: e.activation(out=o[:, c0:c0 + n], in_=bk[:, 0:n], func=AF.Copy, scale=scale), r=[bk], w=[o])
        k.dma("sp", dst, o[:], r=[o], w=["plain"])

    for g in range(6):
        wt = load_w(w_in, O_CONV + g * 512, 512)
        for j in range(4):
            conv_chunk(wt, j * 128, g * 4 + j)
    if stop_after == "P2a":
        k.barrier()
        return k.finish()
    for g in range(2):
        wt = load_w(w_in, O_QB + g * 512, 512)
        for j in range(4):
            plain_chunk(wt, j * 128, aq[g * 4 + j], 128.0 ** -0.5)
    for g in range(4):
        wt = load_w(w_in, O_QI + g * 512, 512)
        for j in range(4):
            plain_chunk(wt, j * 128, iq[g * 4 + j], 1.0)
    wt_kv = load_w(w_in, O_KB, 512)
    for j in range(2):
        plain_chunk(wt_kv, j * 128, akT[j], 1.0)

    if stop_after == "P2b":
        k.barrier()
        return k.finish()
    stg_f = [k.alloc(f"stgf{i}", [128, 512], F32) for i in range(2)]
    stg_b = [k.alloc(f"stgb{i}", [128, 512], BF16) for i in range(2)]
    lnw = [k.alloc(f"lnw{i}", [128, 8], F32) for i in range(2)]
    kib = [k.alloc(f"kib{i}", [128, 128], BF16) for i in range(2)]
    lng = k.alloc("lng", [128, 128], F32)
    lnb = k.alloc("lnb", [128, 128], F32)
    k.dma("sp", lng[:], ln_gb[0:1, :].to_broadcast([128, 128]), w=[lng])
    k.dma("sp", lnb[:], ln_gb[1:2, :].to_broadcast([128, 128]), w=[lnb])
    tcnt = [0]

    def tm_tile(wt, ncols, ti, epilogue):
        c0, n = (ti * 128, 128) if ti < NT else (T, TS)
        bk = k.bank()
        for kk in range(16):
            k.op("pe", lambda e, kk=kk: e.matmul(bk[0:n, 0:ncols], lhsT=hT[:, kk, c0:c0 + n], rhs=wt[:, kk, 0:ncols], start=(kk == 0), stop=(kk == 15)),
                 r=[wt, hT_keys[ti]], w=[bk])
        i = tcnt[0]
        tcnt[0] += 1
        epilogue(bk, c0, n, i)

    def ep_kv(bk, c0, n, i):
        sf = stg_f[i % 2]
        sb_ = stg_b[i % 2]
        import os
        dbg = int(os.environ.get("DBG", "0"))
        k.op("act", lambda e: e.copy(out=sf[0:n, :], in_=bk[0:n, :]), r=[bk], w=[sf])
        if dbg != 3:
            k.op("pool", lambda e: e.tensor_copy(out=sb_[0:n, 0:256], in_=sf[0:n, 256:512]), r=[sf], w=[sb_])
        if dbg == 1:
            pass
        elif c0 < T:
            k.dma("sp", k_p[c0:c0 + n, :], sf[0:n, 0:256], r=[sf])
            k.dma("sp", v_p[c0:c0 + n, :], sf[0:n, 256:512], r=[sf])
        else:
            k.dma("sp", k_s.ap(), sf[0:n, 0:256], r=[sf])
            k.dma("sp", v_s.ap(), sf[0:n, 256:512], r=[sf])
        if dbg not in (2, 3):
            k.dma("sp", av[c0:c0 + n, :], sb_[0:n, 0:256], r=[sb_], w=["av"])

    import os
    _d = int(os.environ.get("DBG", "0"))
    for ti in range(0 if _d == 4 else (NT if _d == 5 else NT + 1)):
        tm_tile(wt_kv, 512, ti, ep_kv)

    if stop_after == "P2c":
        k.barrier()
        return k.finish()

    def ep_z(half):
        def ep(bk, c0, n, i):
            sb_ = stg_b[i % 2]
            k.op("act", lambda e: e.copy(out=sb_[0:n, :], in_=bk[0:n, :]), r=[bk], w=[sb_])
            k.dma("sp", gz[c0:c0 + n, half * 512:(half + 1) * 512], sb_[0:n, :], r=[sb_], w=["gz"])
        return ep

    for half in range(2):
        wt = load_w(w_in, O_Z + half * 512, 512)
        for ti in range(NT + 1):
            tm_tile(wt, 512, ti, ep_z(half))

    if stop_after == "P2d":
        k.barrier()
        return k.finish()

    def ep_ab(bk, c0, n, i):
        sf = stg_f[i % 2]
        k.op("act", lambda e: e.copy(out=sf[0:n, 0:16], in_=bk[0:n, 0:16]), r=[bk], w=[sf])
        k.dma("sp", gab[c0:c0 + n, :], sf[0:n, 0:16], r=[sf], w=["gab"])

    wt = load_w(w_in, O_A, 16)
    for ti in range(NT + 1):
        tm_tile(wt, 16, ti, ep_ab)

    if stop_after == "P2e":
        k.barrier()
        return k.finish()

    def ep_wk(bk, c0, n, i):
        sf = stg_f[i % 2]
        s_ = lnw[i % 2]
        kb_ = kib[i % 2]
        k.op("act", lambda e: e.activation(out=sf[0:n, 0:16], in_=bk[0:n, 0:16], func=AF.Copy, scale=0.25), r=[bk], w=[sf])
        k.dma("sp", iw[c0:c0 + n, :], sf[0:n, 0:16], r=[sf], w=["iw"])
        kf = sf[0:n, 128:256]
        k.op("act", lambda e: e.activation(out=kf, in_=bk[0:n, 16:144], func=AF.Copy, accum_out=s_[0:n, 0:1]), r=[bk, sf], w=[sf, s_])
        k.op("dve", lambda e: e.tensor_scalar(out=s_[0:n, 1:2], in0=s_[0:n, 0:1], scalar1=-1.0 / 128, scalar2=None, op0=ALU.mult), r=[s_], w=[s_])
        k.op("dve", lambda e: e.tensor_scalar(out=kf, in0=kf, scalar1=s_[0:n, 1:2], scalar2=None, op0=ALU.add), r=[sf, s_], w=[sf])
        k.op("act", lambda e: e.activation(out=sf[0:n, 256:384], in_=kf, func=AF.Square, accum_out=s_[0:n, 2:3]), r=[sf, s_], w=[sf, s_])
        k.op("act", lambda e: e.activation(out=s_[0:n, 3:4], in_=s_[0:n, 2:3], func=AF.Sqrt, scale=1.0 / 128, bias=EPS), r=[s_], w=[s_])
        k.op("dve", lambda e: e.reciprocal(out=s_[0:n, 4:5], in_=s_[0:n, 3:4]), r=[s_], w=[s_])
        k.op("dve", lambda e: e.scalar_tensor_tensor(out=kf, in0=kf, scalar=s_[0:n, 4:5], op0=ALU.mult, in1=lng[0:n, :], op1=ALU.mult), r=[sf, s_, lng], w=[sf])
        k.op("dve", lambda e: e.tensor_tensor(out=kf, in0=kf, in1=lnb[0:n, :], op=ALU.add), r=[sf, lnb], w=[sf])
        k.op("dve", lambda e: e.tensor_copy(out=kb_[0:n, :], in_=kf), r=[sf], w=[kb_])
        if c0 < T:
            k.dma("sp", ki_p[c0:c0 + n, :], kf, r=[sf])
        else:
            k.dma("sp", ki_s.ap(), kf, r=[sf])
        b2 = k.bank()
        k.op("pe", lambda e: e.transpose(out=b2[:].bitcast(BF16)[:, 0:n], in_=kb_[0:n, :], identity=ident_b[0:n, 0:n]), r=[kb_, ident_b], w=[b2])
        k.op("act", lambda e: e.copy(out=ikT[:, c0:c0 + n], in_=b2[:].bitcast(BF16)[:, 0:n]), r=[b2], w=[(ikT, c0)])

    wt = load_w(w_in, O_WI, 144)
    for ti in range(NT + 1):
        tm_tile(wt, 144, ti, ep_wk)
    k.dma("sp", ikTd.ap(), ikT[:], r=[(ikT, c) for c in range(0, TT, 128)], w=["ikTd"])
    k.barrier()
    k.release(mP)
    if stop_after == "P2":
        return k.finish()
    conv_jobs = []
    for g in range(16):
        conv_jobs.append((w1s[g], w_ff1[:, g * 512:(g + 1) * 512].rearrange("(k p) c -> p k c", p=128), ("w1s", g)))
    for qc in range(4):
        for fgg in range(8):
            conv_jobs.append((w2s[qc, fgg], w_ff2[fgg * 1024:(fgg + 1) * 1024, qc * 512:(qc + 1) * 512].rearrange("(c p) n -> p c n", p=128), ("w2s", qc, fgg)))

    def issue_conv(nj):
        for _ in range(nj):
            if conv_jobs:
                o_, i_, key_ = conv_jobs.pop(0)
                k.dma("pool", o_, i_, w=[key_])
    mixT = k.alloc("mixT", [128, 16, TT], BF16)
    m3 = k.mark()
    HG = 4
    HW = HG * 128
    ones_f = k.alloc("ones_f", [128, 128], F32)
    TRI = k.alloc("TRI", [128, 128], F32)
    POSM = k.alloc("POSM", [128, HG, 128], F32)
    OFFD = k.alloc("OFFD", [128, HG, 128], F32)
    hvb = k.alloc("hvb", [128, 16], F32)
    gnb = k.alloc("gnb", [128, 128], F32)
    k.op("pool", lambda e: e.memset(ones_f[:], 1.0), w=[ones_f])
    k.op("pool", lambda e: e.memset(TRI[:], 1.0), w=[TRI])
    k.op("pool", lambda e: e.affine_select(out=TRI[:], in_=TRI[:], pattern=[[1, 128]], compare_op=ALU.is_ge, fill=0.0, base=0, channel_multiplier=-1),
         r=[TRI], w=[TRI])
    k.op("pool", lambda e: e.memset(POSM[:], 0.0), w=[POSM])
    k.op("pool", lambda e: e.affine_select(out=POSM[:], in_=POSM[:], pattern=[[0, HG], [-1, 128]], compare_op=ALU.is_ge, fill=30000.0, base=0, channel_multiplier=1),
         r=[POSM], w=[POSM])
    k.op("pool", lambda e: e.memset(OFFD[:], 1.0), w=[OFFD])
    k.op("pool", lambda e: e.affine_select(out=OFFD[:], in_=OFFD[:], pattern=[[0, HG], [-1, 128]], compare_op=ALU.not_equal, fill=0.0, base=0, channel_multiplier=1),
         r=[OFFD], w=[OFFD])
    k.dma("sp", hvb[:], hv.ap().to_broadcast([128, 16]), w=[hvb])
    k.dma("sp", gnb[:], gdn_g.ap().to_broadcast([128, 128]), w=[gnb])

    gabt = k.alloc("gabt", [128, NT, 16], F32)
    k.dma("sp", gabt[:], gab[0:T, :].rearrange("(t p) c -> p t c", p=128), r=["gab"], w=[gabt], slow=True)
    nA = k.alloc("nA", [128, 8], F32)
    G_ = k.alloc("G_", [128, NT, 8], F32)
    Bt = k.alloc("Bt", [128, NT, 8], F32)
    NB = k.alloc("NB", [128, NT, 8], F32)
    GC = k.alloc("GC", [128, NT, 8], F32)
    GL = k.alloc("GL", [128, NT, 8], F32)
    EG = k.alloc("EG", [128, NT, 8], F32)
    EGL = k.alloc("EGL", [128, NT, 8], F32)
    EKD = k.alloc("EKD", [128, NT, 8], F32)
    BEG = k.alloc("BEG", [128, NT, 8], F32)
    k.op("act", lambda e: e.activation(out=nA[:], in_=hvb[:, 0:8], func=AF.Exp), r=[hvb], w=[nA])
    k.op("dve", lambda e: e.tensor_scalar(out=nA[:], in0=nA[:], scalar1=-1.0, scalar2=None, op0=ALU.mult), r=[nA], w=[nA])
    k.op("dve", lambda e: e.tensor_tensor(out=G_[:], in0=gabt[:, :, 0:8], in1=hvb[:, 8:16].unsqueeze(1).to_broadcast([128, NT, 8]), op=ALU.add), r=[gabt, hvb], w=[G_])
    k.op("act", lambda e: e.activation(out=G_[:], in_=G_[:], func=AF.Exp), r=[G_], w=[G_])
    k.op("act", lambda e: e.activation(out=G_[:], in_=G_[:], func=AF.Ln, bias=1.0, scale=1.0), r=[G_], w=[G_])
    k.op("dve", lambda e: e.tensor_tensor(out=G_[:], in0=G_[:], in1=nA[:].unsqueeze(1).to_broadcast([128, NT, 8]), op=ALU.mult), r=[G_, nA], w=[G_])
    k.op("act", lambda e: e.activation(out=Bt[:], in_=gabt[:, :, 8:16], func=AF.Sigmoid), r=[gabt], w=[Bt])
    k.op("dve", lambda e: e.tensor_scalar(out=NB[:], in0=Bt[:], scalar1=-1.0, scalar2=None, op0=ALU.mult), r=[Bt], w=[NB])
    bA = k.bank()
    bB = k.bank()
    for t in range(NT):
        k.op("pe", lambda e, t=t: e.matmul(bA[:, t * 8:(t + 1) * 8], lhsT=TRI[:], rhs=G_[:, t, :], start=True, stop=True), r=[TRI, G_], w=[bA])
        k.op("pe", lambda e, t=t: e.matmul(bB[:, t * 8:(t + 1) * 8], lhsT=ones_f[:], rhs=G_[:, t, :], start=True, stop=True), r=[ones_f, G_], w=[bB])
    k.op("act", lambda e: e.copy(out=GC[:].rearrange("p a b -> p (a b)"), in_=bA[:, 0:NT * 8]), r=[bA], w=[GC])
    k.op("act", lambda e: e.copy(out=GL[:].rearrange("p a b -> p (a b)"), in_=bB[:, 0:NT * 8]), r=[bB], w=[GL])
    k.op("act", lambda e: e.activation(out=EG[:], in_=GC[:], func=AF.Exp), r=[GC], w=[EG])
    k.op("act", lambda e: e.activation(out=EGL[:], in_=GL[:], func=AF.Exp), r=[GL], w=[EGL])
    k.op("dve", lambda e: e.tensor_tensor(out=EKD[:], in0=GL[:], in1=GC[:], op=ALU.subtract), r=[GL, GC], w=[EKD])
    k.op("act", lambda e: e.activation(out=EKD[:], in_=EKD[:], func=AF.Exp), r=[EKD], w=[EKD])
    k.op("dve", lambda e: e.tensor_tensor(out=BEG[:], in0=Bt[:], in1=EG[:], op=ALU.mult), r=[Bt, EG], w=[BEG])

    if stop_after == "P3a":
        k.barrier()
        return k.finish()
    m3g = k.mark()
    qT = k.alloc("qT", [128, HG, TT], BF16)
    kT = k.alloc("kT", [128, HG, TT], BF16)
    vT = k.alloc("vT", [128, HG, TT], BF16)
    NSLOT = 2

    def mk_slot(j):
        d = {}
        for nm, dt in (("Dg", F32), ("egT", BF16), ("decay", F32), ("M0", F32), ("M1", F32), ("MT0", F32), ("MT1", F32),
                       ("PT", F32), ("PTb", BF16), ("vbeta", BF16), ("kbg", BF16), ("kdec", BF16), ("u", F32), ("wT", BF16),
                       ("intra", BF16), ("intraT", BF16), ("qg", BF16)):
            d[nm] = k.alloc(f"{nm}_{j}", [128, HG, 128], dt)
        return d

    slots = [mk_slot(j) for j in range(NSLOT)]
    S_ = k.alloc("S_", [128, HG, 128], F32)
    Sb = k.alloc("Sb", [128, HG, 128], BF16)
    vnew = k.alloc("vnew", [128, HG, 128], BF16)
    o_ = k.alloc("o_", [128, HG, 128], F32)
    sq_ = k.alloc("sq_", [128, HG, 128], F32)
    zt = [k.alloc(f"zt{i}", [128, HG, 128], BF16) for i in range(2)]
    zs = k.alloc("zs", [128, HG, 128], F32)
    oa = k.alloc("oa", [128, HG, 128], BF16)
    sst = k.alloc("sst", [128, 3 * HG], F32)

    def fl(b):
        return b[:].rearrange("p a b -> p (a b)")

    def bc_tok(src_ap):
        return src_ap.unsqueeze(2).to_broadcast([128, HG, 128])

    def stageA(g, i, sl):
        d = slots[sl]
        h0 = g * HG
        tsl = slice(i * 128, (i + 1) * 128)
        Dg, egT, decay, PT, PTb = d["Dg"], d["egT"], d["decay"], d["PT"], d["PTb"]
        Ms = [d["M0"], d["M1"]]
        MTs = [d["MT0"], d["MT1"]]
        k.op("dve", lambda e: e.tensor_tensor(out=Dg[:], in0=ident_f[:].unsqueeze(1).to_broadcast([128, HG, 128]), in1=bc_tok(GC[:, i, h0:h0 + HG]), op=ALU.mult),
             r=[ident_f, GC], w=[Dg])
        b1 = k.bank()
        k.op("pe", lambda e: e.matmul(b1[:, 0:HW], lhsT=ones_f[:], rhs=fl(Dg), start=True, stop=True), r=[ones_f, Dg], w=[b1])
        k.op("act", lambda e: e.activation(out=fl(egT), in_=b1[:, 0:HW], func=AF.Exp), r=[b1], w=[egT])
        b2 = k.bank()
        k.op("pe", lambda e: e.matmul(b2[:, 0:HW], lhsT=ones_f[:], rhs=fl(Dg), start=True, stop=False), r=[ones_f, Dg], w=[b2])
        k.op("pe", lambda e: e.matmul(b2[:, 0:HW], lhsT=ident_f[:], rhs=fl(POSM), start=False, stop=True), r=[ident_f, POSM], w=[b2])
        for h in range(HG):
            k.op("act", lambda e, h=h: e.activation(out=decay[:, h, :], in_=b2[:, h * 128:(h + 1) * 128], func=AF.Exp, scale=-1.0, bias=GC[:, i, h0 + h:h0 + h + 1]),
                 r=[b2, GC], w=[decay])
        k.op("dve", lambda e: e.tensor_tensor(out=d["qg"][:], in0=qT[:, :, tsl], in1=egT[:], op=ALU.mult), r=[qT, egT], w=[d["qg"]])
        b3 = k.bank()
        b4 = k.bank()
        for h in range(HG):
            k.op("pe", lambda e, h=h: e.matmul(b3[:, h * 128:(h + 1) * 128], lhsT=kT[:, h, tsl], rhs=kT[:, h, tsl], start=True, stop=True), r=[kT], w=[b3])
        for h in range(HG):
            k.op("pe", lambda e, h=h: e.matmul(b4[:, h * 128:(h + 1) * 128], lhsT=qT[:, h, tsl], rhs=kT[:, h, tsl], start=True, stop=True), r=[qT, kT], w=[b4])
        k.op("dve", lambda e: e.tensor_tensor(out=fl(d["intra"]), in0=b4[:, 0:HW], in1=fl(decay), op=ALU.mult), r=[b4, decay], w=[d["intra"]])
        k.op("dve", lambda e: e.tensor_tensor(out=Dg[:], in0=decay[:], in1=OFFD[:], op=ALU.mult), r=[decay, OFFD], w=[Dg])
        for h in range(HG):
            k.op("dve", lambda e, h=h: e.scalar_tensor_tensor(out=Ms[0][:, h, :], in0=b3[:, h * 128:(h + 1) * 128], scalar=NB[:, i, h0 + h:h0 + h + 1], op0=ALU.mult,
                                                                in1=Dg[:, h, :], op1=ALU.mult), r=[b3, NB, Dg], w=[Ms[0]])
        yield
        b5 = k.bank()
        b5i = k.bank()
        b5b = b5i[:].bitcast(BF16)
        for h in range(HG):
            k.op("pe", lambda e, h=h: e.transpose(out=b5[:, h * 128:(h + 1) * 128], in_=Ms[0][:, h, :], identity=ident_f[:]), r=[Ms[0], ident_f], w=[b5])
        for h in range(HG):
            k.op("pe", lambda e, h=h: e.transpose(out=b5b[:, h * 128:(h + 1) * 128], in_=d["intra"][:, h, :], identity=ident_b[:]), r=[d["intra"], ident_b], w=[b5i])
        k.op("act", lambda e: e.copy(out=fl(MTs[0]), in_=b5[:, 0:HW]), r=[b5], w=[MTs[0]])
        k.op("act", lambda e: e.copy(out=fl(d["intraT"]), in_=b5b[:, 0:HW]), r=[b5i], w=[d["intraT"]])
        k.op("dve", lambda e: e.tensor_tensor(out=PT[:], in0=MTs[0][:], in1=ident_f[:].unsqueeze(1).to_broadcast([128, HG, 128]), op=ALU.add), r=[MTs[0], ident_f], w=[PT])
        yield
        cur = 0
        for lvl in range(1, 7):
            nx = 1 - cur
            last = (lvl == 6)
            b6 = k.bank()
            for h in range(HG):
                k.op("pe", lambda e, h=h, cur=cur, b6=b6: e.matmul(b6[:, h * 128:(h + 1) * 128], lhsT=MTs[cur][:, h, :], rhs=Ms[cur][:, h, :], start=True, stop=True),
                     r=[MTs[cur], Ms[cur]], w=[b6])
            if not last:
                b7 = k.bank()
                for h in range(HG):
                    k.op("pe", lambda e, h=h, cur=cur, b7=b7: e.matmul(b7[:, h * 128:(h + 1) * 128], lhsT=Ms[cur][:, h, :], rhs=MTs[cur][:, h, :], start=True, stop=True),
                         r=[MTs[cur], Ms[cur]], w=[b7])
            k.op("act", lambda e, nx=nx, b6=b6: e.copy(out=fl(Ms[nx]), in_=b6[:, 0:HW]), r=[b6], w=[Ms[nx]])
            if not last:
                k.op("act", lambda e, nx=nx, b7=b7: e.copy(out=fl(MTs[nx]), in_=b7[:, 0:HW]), r=[b7], w=[MTs[nx]])
            b8 = k.bank()
            for h in range(HG):
                k.op("pe", lambda e, h=h, nx=nx, b8=b8: e.matmul(b8[:, h * 128:(h + 1) * 128], lhsT=Ms[nx][:, h, :], rhs=PT[:, h, :], start=True, stop=True),
                     r=[Ms[nx], PT], w=[b8])
            k.op("dve", lambda e, b8=b8: e.tensor_tensor(out=fl(PT), in0=b8[:, 0:HW], in1=fl(PT), op=ALU.add), r=[b8, PT], w=[PT])
            if last:
                k.op("act", lambda e: e.copy(out=PTb[:], in_=PT[:]), r=[PT], w=[PTb])
            cur = nx
            yield
        b9 = k.bank()
        b9b = b9[:].bitcast(BF16)
        for h in range(HG):
            k.op("pe", lambda e, h=h: e.transpose(out=b9b[:, h * 128:(h + 1) * 128], in_=vT[:, h, tsl], identity=ident_b[:]), r=[vT, ident_b], w=[b9])
        for h in range(HG):
            k.op("pe", lambda e, h=h: e.transpose(out=b9b[:, HW + h * 128:HW + (h + 1) * 128], in_=kT[:, h, tsl], identity=ident_b[:]), r=[kT, ident_b], w=[b9])
        vps = b9b[:, 0:HW].rearrange("p (a b) -> p a b", a=HG)
        kps = b9b[:, HW:2 * HW].rearrange("p (a b) -> p a b", a=HG)
        k.op("dve", lambda e: e.tensor_tensor(out=d["vbeta"][:], in0=vps, in1=bc_tok(Bt[:, i, h0:h0 + HG]), op=ALU.mult), r=[b9, Bt], w=[d["vbeta"]])
        k.op("dve", lambda e: e.tensor_tensor(out=d["kbg"][:], in0=kps, in1=bc_tok(BEG[:, i, h0:h0 + HG]), op=ALU.mult), r=[b9, BEG], w=[d["kbg"]])
        k.op("dve", lambda e: e.tensor_tensor(out=d["kdec"][:], in0=kps, in1=bc_tok(EKD[:, i, h0:h0 + HG]), op=ALU.mult), r=[b9, EKD], w=[d["kdec"]])
        b10 = k.bank()
        b11 = k.bank()
        for h in range(HG):
            k.op("pe", lambda e, h=h: e.matmul(b10[:, h * 128:(h + 1) * 128], lhsT=PTb[:, h, :], rhs=d["vbeta"][:, h, :], start=True, stop=True), r=[PTb, d["vbeta"]], w=[b10])
        for h in range(HG):
            k.op("pe", lambda e, h=h: e.matmul(b11[:, h * 128:(h + 1) * 128], lhsT=d["kbg"][:, h, :], rhs=PTb[:, h, :], start=True, stop=True), r=[PTb, d["kbg"]], w=[b11])
        k.op("act", lambda e: e.copy(out=fl(d["u"]), in_=b10[:, 0:HW]), r=[b10], w=[d["u"]])
        k.op("act", lambda e: e.copy(out=fl(d["wT"]), in_=b11[:, 0:HW]), r=[b11], w=[d["wT"]])
        yield

    def scan_step(g, i, sl):
        d = slots[sl]
        h0 = g * HG
        tsl = slice(i * 128, (i + 1) * 128)
        z_ = zt[i % 2]
        k.dma("sp", fl(z_), gz[i * 128:(i + 1) * 128, h0 * 128:(h0 + HG) * 128], r=["gz"], w=[z_])
        bx = k.bank()
        for h in range(HG):
            k.op("pe", lambda e, h=h: e.matmul(bx[:, h * 128:(h + 1) * 128], lhsT=d["wT"][:, h, :], rhs=Sb[:, h, :], start=True, stop=True), r=[d["wT"], Sb], w=[bx])
        k.op("dve", lambda e: e.tensor_tensor(out=fl(vnew), in0=fl(d["u"]), in1=bx[:, 0:HW], op=ALU.subtract), r=[d["u"], bx], w=[vnew])
        bo = k.bank()
        for h in range(HG):
            k.op("pe", lambda e, h=h: e.matmul(bo[:, h * 128:(h + 1) * 128], lhsT=d["qg"][:, h, :], rhs=Sb[:, h, :], start=True, stop=False), r=[d["qg"], Sb], w=[bo])
            k.op("pe", lambda e, h=h: e.matmul(bo[:, h * 128:(h + 1) * 128], lhsT=d["intraT"][:, h, :], rhs=vnew[:, h, :], start=False, stop=True), r=[d["intraT"], vnew], w=[bo])
        bz = k.bank()
        for h in range(HG):
            k.op("pe", lambda e, h=h: e.matmul(bz[:, h * 128:(h + 1) * 128], lhsT=d["kdec"][:, h, :], rhs=vnew[:, h, :], start=True, stop=True), r=[d["kdec"], vnew], w=[bz])
        k.op("dve", lambda e: e.tensor_tensor(out=S_[:], in0=S_[:], in1=bc_tok(EGL[:, i, h0:h0 + HG]), op=ALU.mult), r=[S_, EGL], w=[S_])
        k.op("dve", lambda e: e.tensor_tensor(out=fl(S_), in0=fl(S_), in1=bz[:, 0:HW], op=ALU.add), r=[S_, bz], w=[S_])
        k.op("act", lambda e: e.copy(out=Sb[:], in_=S_[:]), r=[S_], w=[Sb])
        k.op("act", lambda e: e.copy(out=fl(o_), in_=bo[:, 0:HW]), r=[bo], w=[o_])
        k.op("act", lambda e: e.activation(out=sq_[:], in_=o_[:], func=AF.Square), r=[o_], w=[sq_])
        k.op("dve", lambda e: e.tensor_reduce(out=sst[:, 0:HG], in_=sq_[:], axis=AX.X, op=ALU.add), r=[sq_], w=[sst])
        k.op("act", lambda e: e.activation(out=sst[:, HG:2 * HG], in_=sst[:, 0:HG], func=AF.Sqrt, scale=1.0 / 128, bias=EPS), r=[sst], w=[sst])
        k.op("dve", lambda e: e.reciprocal(out=sst[:, 2 * HG:3 * HG], in_=sst[:, HG:2 * HG]), r=[sst], w=[sst])
        k.op("act", lambda e: e.activation(out=zs[:], in_=z_[:], func=AF.Silu), r=[z_], w=[zs])
        k.op("dve", lambda e: e.tensor_tensor(out=o_[:], in0=o_[:], in1=bc_tok(sst[:, 2 * HG:3 * HG]), op=ALU.mult), r=[o_, sst], w=[o_])
        k.op("dve", lambda e: e.tensor_tensor(out=o_[:], in0=o_[:], in1=gnb[:].unsqueeze(1).to_broadcast([128, HG, 128]), op=ALU.mult), r=[o_, gnb], w=[o_])
        k.op("dve", lambda e: e.tensor_tensor(out=oa[:], in0=o_[:], in1=zs[:], op=ALU.mult), r=[o_, zs], w=[oa])
        bt = k.bank()
        btb = bt[:].bitcast(BF16)
        for h in range(HG):
            k.op("pe", lambda e, h=h: e.transpose(out=btb[:, h * 128:(h + 1) * 128], in_=oa[:, h, :], identity=ident_b[:]), r=[oa, ident_b], w=[bt])
        k.op("act", lambda e: e.copy(out=mixT[:, h0:h0 + HG, tsl], in_=btb[:, 0:HW].rearrange("p (a b) -> p a b", a=HG)), r=[bt], w=[(mixT, i)])

    for g in range(8 // HG):
        h0 = g * HG
        for nm, src, buf in (("q", gq, qT), ("k", gk, kT), ("v", gv, vT)):
            k.dma("sp", buf[:], src[h0:h0 + HG].rearrange("h d t -> d h t"), r=["gqk", "gv"], w=[buf])
        k.op("pool", lambda e: e.memset(S_[:], 0.0), w=[S_])
        k.op("pool", lambda e: e.memset(Sb[:], 0.0), w=[Sb])
        if stop_after == "P3d":
            for _ in stageA(0, 0, 0):
                pass
            for _ in stageA(0, 1, 1):
                pass
            scan_step(0, 0, 0)
            scan_step(0, 1, 1)
            d = slots[0]
            names = ["decay", "M0", "PT", "u", "wT", "intraT", "qg", "kdec", "vbeta", "kbg", "egT"]
            tmpfs = [sq_, zs]
            for ii, nm in enumerate(names):
                dd = dout("dbg_" + nm, [128, HW], F32)
                tmpf = tmpfs[ii % 2]
                k.op("dve", lambda e, nm=nm, tmpf=tmpf: e.tensor_copy(out=fl(tmpf), in_=fl(d[nm])), r=[d[nm]], w=[tmpf])
                k.dma("sp", dd.ap(), fl(tmpf), r=[tmpf])
            for nm, b in (("S", S_), ("o", o_), ("GC", GC), ("G", G_), ("Bt", Bt), ("EKD", EKD), ("EGL", EGL)):
                dd = dout("dbg_" + nm, [128, int(np.prod(b.ap.shape[1:]))], F32)
                k.dma("sp", dd.ap(), b[:].rearrange("p a b -> p (a b)"), r=[b])
            k.barrier()
            return k.finish()
        import os
        _lim = int(os.environ.get("YLIM", "100"))
        for i0 in range(0, NT, NSLOT):
            gens = [stageA(g, i0 + j, j) for j in range(NSLOT)]
            if stop_after == "P3b":
                for _ in range(_lim):
                    for gen in gens:
                        next(gen, None)
                k.barrier()
                return k.finish()
            alive = True
            while alive:
                alive = False
                for gen in gens:
                    try:
                        next(gen)
                        alive = True
                    except StopIteration:
                        pass
            for j in range(NSLOT):
                scan_step(g, i0 + j, j)
            issue_conv(3)
        k.dma("sp", ssm_p.ap()[h0:h0 + HG].rearrange("h a b -> a h b"), S_[:], r=[S_])
    if stop_after == "P3":
        k.barrier()
        return k.finish()
    issue_conv(100)
    k.barrier()
    k.release(m3g)
    S0 = k.alloc("S0", [128, 8, 128], F32)
    qc = k.alloc("qc", [128, 8], BF16)
    kc = k.alloc("kc", [128, 8], BF16)
    vc = k.alloc("vc", [128, 8], BF16)
    qcf = k.alloc("qcf", [128, 8], F32)
    kcf = k.alloc("kcf", [128, 8], F32)
    gabr = k.alloc("gabr", [1, 16], F32)
    zr = k.alloc("zr", [1, 1024], BF16)
    zrs = k.alloc("zrs", [1, 8, 128], F32)
    rw = k.alloc("rw", [1, 64], F32)
    t1 = k.alloc("t1", [1, 8, 128], F32)
    orow = k.alloc("orow", [1, 8, 128], F32)
    sqr = k.alloc("sqr", [1, 8, 128], F32)
    oar = k.alloc("oar", [1, 1024], BF16)
    abs_ = k.alloc("abs_", [128, 8], F32)

    def bc_row(ap8):
        return ap8.unsqueeze(2).to_broadcast([1, 8, 128])

    def sample_gdn(s_i):
        col = T + s_i
        k.dma("sp", S0[:], st_ssm.ap()[s_i].rearrange("h a b -> a h b"), w=[S0])
        k.dma("sp", qc[:], gq.ap()[:, :, col].rearrange("h d -> d h"), r=["gqk"], w=[qc], slow=True)
        k.dma("sp", kc[:], gk.ap()[:, :, col].rearrange("h d -> d h"), r=["gqk"], w=[kc], slow=True)
        k.dma("sp", vc[:], gv.ap()[:, :, col].rearrange("h d -> d h"), r=["gv"], w=[vc], slow=True)
        k.dma("sp", gabr[:], gab[col:col + 1, :], r=["gab"], w=[gabr])
        k.dma("sp", zr[:], gz[col:col + 1, :], r=["gz"], w=[zr])
        k.op("dve", lambda e: e.tensor_copy(out=qcf[:], in_=qc[:]), r=[qc], w=[qcf])
        k.op("dve", lambda e: e.tensor_copy(out=kcf[:], in_=kc[:]), r=[kc], w=[kcf])
        k.op("dve", lambda e: e.tensor_tensor(out=rw[:, 0:8], in0=gabr[:, 0:8], in1=hvb[0:1, 8:16], op=ALU.add), r=[gabr, hvb], w=[rw])
        k.op("act", lambda e: e.activation(out=rw[:, 0:8], in_=rw[:, 0:8], func=AF.Exp), r=[rw], w=[rw])
        k.op("act", lambda e: e.activation(out=rw[:, 0:8], in_=rw[:, 0:8], func=AF.Ln, bias=1.0, scale=1.0), r=[rw], w=[rw])
        k.op("dve", lambda e: e.tensor_tensor(out=rw[:, 0:8], in0=rw[:, 0:8], in1=nA[0:1, :], op=ALU.mult), r=[rw, nA], w=[rw])
        k.op("act", lambda e: e.activation(out=rw[:, 8:16], in_=rw[:, 0:8], func=AF.Exp), r=[rw], w=[rw])
        k.op("act", lambda e: e.activation(out=rw[:, 16:24], in_=gabr[:, 8:16], func=AF.Sigmoid), r=[gabr, rw], w=[rw])
        ba = k.bank()
        bb_ = k.bank()
        for h in range(8):
            bk_ = ba if h < 4 else bb_
            k.op("pe", lambda e, h=h, bk_=bk_: e.matmul(bk_[0:1, (h % 4) * 128:(h % 4 + 1) * 128], lhsT=kcf[:, h:h + 1], rhs=S0[:, h, :], start=True, stop=True),
                 r=[kcf, S0], w=[bk_])
        bv = k.bank()
        bvb = bv[:].bitcast(BF16)
        for h in range(8):
            k.op("pe", lambda e, h=h: e.transpose(out=bvb[0:1, h * 128:(h + 1) * 128], in_=vc[:, h:h + 1], identity=ident_b[:]), r=[vc, ident_b], w=[bv])
        k.op("dve", lambda e: e.tensor_tensor(out=t1[:, 0:4, :], in0=ba[0:1, :].rearrange("p (a b) -> p a b", a=4), in1=bc_row(rw[:, 8:16])[:, 0:4, :], op=ALU.mult),
             r=[ba, rw], w=[t1])
        k.op("dve", lambda e: e.tensor_tensor(out=t1[:, 4:8, :], in0=bb_[0:1, :].rearrange("p (a b) -> p a b", a=4), in1=bc_row(rw[:, 8:16])[:, 4:8, :], op=ALU.mult),
             r=[bb_, rw, t1], w=[t1])
        k.op("dve", lambda e: e.tensor_tensor(out=t1[:], in0=bvb[0:1, 0:1024].rearrange("p (a b) -> p a b", a=8), in1=t1[:], op=ALU.subtract), r=[bv, t1], w=[t1])
        k.op("dve", lambda e: e.tensor_tensor(out=t1[:], in0=t1[:], in1=bc_row(rw[:, 16:24]), op=ALU.mult), r=[t1, rw], w=[t1])
        bd0 = k.bank()
        bd1 = k.bank()
        k.op("pe", lambda e: e.matmul(bd0[:, :], lhsT=ones_f[0:1, :], rhs=t1[:].rearrange("p a b -> p (a b)")[:, 0:512], start=True, stop=True), r=[ones_f, t1], w=[bd0])
        k.op("pe", lambda e: e.matmul(bd1[:, :], lhsT=ones_f[0:1, :], rhs=t1[:].rearrange("p a b -> p (a b)")[:, 512:1024], start=True, stop=True), r=[ones_f, t1], w=[bd1])
        bab = k.bank()
        k.op("pe", lambda e: e.matmul(bab[:, 0:8], lhsT=ones_f[0:1, :], rhs=rw[:, 8:16], start=True, stop=True), r=[ones_f, rw], w=[bab])
        k.op("act", lambda e: e.copy(out=abs_[:], in_=bab[:, 0:8]), r=[bab], w=[abs_])
        k.op("dve", lambda e: e.tensor_tensor(out=S0[:], in0=S0[:], in1=abs_[:].unsqueeze(2).to_broadcast([128, 8, 128]), op=ALU.mult), r=[S0, abs_], w=[S0])
        for h in range(8):
            bd = bd0 if h < 4 else bd1
            k.op("dve", lambda e, h=h, bd=bd: e.scalar_tensor_tensor(out=S0[:, h, :], in0=bd[:, (h % 4) * 128:(h % 4 + 1) * 128], scalar=kcf[:, h:h + 1], op0=ALU.mult,
                                                                      in1=S0[:, h, :], op1=ALU.add), r=[bd, kcf, S0], w=[S0])
        k.dma("sp", ssm_s.ap()[s_i].rearrange("h a b -> a h b"), S0[:], r=[S0])
        bo0 = k.bank()
        bo1 = k.bank()
        for h in range(8):
            bk_ = bo0 if h < 4 else bo1
            k.op("pe", lambda e, h=h, bk_=bk_: e.matmul(bk_[0:1, (h % 4) * 128:(h % 4 + 1) * 128], lhsT=qcf[:, h:h + 1], rhs=S0[:, h, :], start=True, stop=True),
                 r=[qcf, S0], w=[bk_])
        k.op("act", lambda e: e.copy(out=orow[:, 0:4, :], in_=bo0[0:1, :].rearrange("p (a b) -> p a b", a=4)), r=[bo0], w=[orow])
        k.op("act", lambda e: e.copy(out=orow[:, 4:8, :], in_=bo1[0:1, :].rearrange("p (a b) -> p a b", a=4)), r=[bo1, orow], w=[orow])
        k.op("dve", lambda e: e.tensor_tensor(out=sqr[:], in0=orow[:], in1=orow[:], op=ALU.mult), r=[orow], w=[sqr])
        k.op("dve", lambda e: e.tensor_reduce(out=rw[:, 24:32], in_=sqr[:], axis=AX.X, op=ALU.add), r=[sqr, rw], w=[rw])
        k.op("act", lambda e: e.activation(out=rw[:, 32:40], in_=rw[:, 24:32], func=AF.Sqrt, scale=1.0 / 128, bias=EPS), r=[rw], w=[rw])
        k.op("dve", lambda e: e.reciprocal(out=rw[:, 40:48], in_=rw[:, 32:40]), r=[rw], w=[rw])
        k.op("act", lambda e: e.activation(out=zrs[:].rearrange("p a b -> p (a b)"), in_=zr[:], func=AF.Silu), r=[zr], w=[zrs])
        k.op("dve", lambda e: e.tensor_tensor(out=orow[:], in0=orow[:], in1=bc_row(rw[:, 40:48]), op=ALU.mult), r=[orow, rw], w=[orow])
        k.op("dve", lambda e: e.tensor_tensor(out=orow[:], in0=orow[:], in1=gnb[0:1, :].unsqueeze(1).to_broadcast([1, 8, 128]), op=ALU.mult), r=[orow, gnb], w=[orow])
        k.op("dve", lambda e: e.tensor_tensor(out=oar[:].rearrange("p (a b) -> p a b", a=8), in0=orow[:], in1=zrs[:], op=ALU.mult), r=[orow, zrs], w=[oar])
        bt_ = k.bank()
        for h in range(8):
            k.op("pe", lambda e, h=h: e.matmul(bt_[:, h:h + 1], lhsT=oar[0:1, h * 128:(h + 1) * 128], rhs=ones_b[0:1, 0:1], start=True, stop=True), r=[oar, ones_b], w=[bt_])
        k.op("act", lambda e: e.copy(out=mixT[:, 0:8, col], in_=bt_[:, 0:8]), r=[bt_], w=[(mixT, "s%d" % s_i)])
    for s_i in range(TS):
        sample_gdn(s_i)
    k.barrier()
    k.release(m3)
    if stop_after == "P3S":
        return k.finish()
    m4 = k.mark()
    NEG = -30000.0
    NIT = 17
    kTb = k.alloc("kTb", [128, 2, TT], BF16)
    vtok = k.alloc("vtok", [128, NT, 256], BF16)
    ikT4 = k.alloc("ikT4", [128, TT], BF16)
    k.dma("sp", kTb[:], akT.ap().rearrange("h d t -> d h t"), r=["plain"], w=[kTb])
    k.dma("sp", vtok[:], av[0:T, :].rearrange("(t p) c -> p t c", p=128), r=["av"], w=[vtok])
    k.dma("sp", ikT4[:], ikTd.ap(), r=["ikTd"], w=[ikT4])
    ones_f4 = k.alloc("ones_f4", [128, 128], F32)
    zeros_b = k.alloc("zeros_b", [128, 128], BF16)
    Jm = k.alloc("Jm", [128, 128], F32)
    CMT = k.alloc("CMT", [128, 128], F32)
    CM = k.alloc("CM", [128, 128], F32)
    k.op("pool", lambda e: e.memset(ones_f4[:], 1.0), w=[ones_f4])
    k.op("pool", lambda e: e.memset(zeros_b[:], 0.0), w=[zeros_b])
    k.op("pool", lambda e: e.memset(Jm[:], 1.0), w=[Jm])
    k.op("pool", lambda e: e.affine_select(out=Jm[:], in_=Jm[:], pattern=[[1, 128]], compare_op=ALU.is_equal, fill=0.0, base=-127, channel_multiplier=1), r=[Jm], w=[Jm])
    k.op("pool", lambda e: e.memset(CMT[:], 0.0), w=[CMT])
    k.op("pool", lambda e: e.affine_select(out=CMT[:], in_=CMT[:], pattern=[[1, 128]], compare_op=ALU.is_ge, fill=NEG, base=0, channel_multiplier=-1), r=[CMT], w=[CMT])
    k.op("pool", lambda e: e.memset(CM[:], 0.0), w=[CM])
    k.op("pool", lambda e: e.affine_select(out=CM[:], in_=CM[:], pattern=[[-1, 128]], compare_op=ALU.is_ge, fill=NEG, base=0, channel_multiplier=1), r=[CM], w=[CM])
    rb = k.alloc("rb", [32, 8], F32)
    rb31 = k.alloc("rb31", [32, 8], F32)
    bohs = k.alloc("bohs", [32, 384], F32)
    bvec = k.alloc("bvec", [8, 384], F32)
    Tp = k.alloc("Tp", [128, 8, 128], F32)
    Bt4 = [k.alloc(f"Bt4_{i}", [128, 8, 128], F32) for i in range(2)]
    k.dma("sp", rb[:], rel_bias.ap(), w=[rb])
    k.dma("sp", rb31[:], rel_bias[31:32, :].to_broadcast([32, 8]), w=[rb31])
    k.dma("sp", bohs[:], boh.ap(), w=[bohs])
    k.op("dve", lambda e: e.tensor_tensor(out=rb[:], in0=rb[:], in1=rb31[:], op=ALU.subtract), r=[rb, rb31], w=[rb])
    bkb = k.bank()
    k.op("pe", lambda e: e.matmul(bkb[0:8, 0:384], lhsT=rb[:], rhs=bohs[:], start=True, stop=True), r=[rb, bohs], w=[bkb])
    k.op("act", lambda e: e.copy(out=bvec[:], in_=bkb[0:8, 0:384]), r=[bkb], w=[bvec])
    k.dma("sp", biasd.ap(), bvec[:], r=[bvec], w=["biasd"])

    def mk_bias(dl):
        k.dma("sp", Tp[:], bass.AP(tensor=biasd, offset=128 * dl, ap=[[1, 128], [384, 8], [1, 128]]), r=["biasd"], w=[Tp])
        for half in range(2):
            bj = k.bank()
            k.op("pe", lambda e, bj=bj, half=half: e.matmul(bj[:, :], lhsT=Jm[:], rhs=Tp[:].rearrange("p a b -> p (a b)")[:, half * 512:(half + 1) * 512], start=True, stop=True), r=[Jm, Tp], w=[bj])
            k.op("act", lambda e, bj=bj, half=half: e.copy(out=Bt4[dl][:].rearrange("p a b -> p (a b)")[:, half * 512:(half + 1) * 512], in_=bj[:, :]), r=[bj], w=[Bt4[dl]])

    mk_bias(0)
    mk_bias(1)

    qbT = [k.alloc(f"qbT{i}", [128, 8, 128], BF16) for i in range(2)]
    qiT = [k.alloc(f"qiT{i}", [128, 16, 128], BF16) for i in range(2)]
    iwt = [k.alloc(f"iwt{i}", [128, 48], F32) for i in range(2)]
    scs = [k.alloc(f"sc{i}", [128, T], F32) for i in range(2)]
    sc = scs[0]
    Dws = [k.alloc(f"Dw{i}", [128, 16, 128], BF16) for i in range(2)]
    jnk = k.alloc("jnk", [128, T], BF16)
    rl = [k.alloc(f"rl{i}", [128, 512], BF16) for i in range(4)]
    rcnt = [0]
    bs = k.alloc("bs", [128, 8], F32)
    negT = k.alloc("negT", [128, NT, 128], BF16)
    PTs = [k.alloc(f"PTs{i}", [128, 4, 128], BF16) for i in range(4)]
    Bt4b = [k.alloc(f"Bt4b_{i}", [128, 8, 128], BF16) for i in range(2)]
    for i_ in range(2):
        k.op("dve", lambda e, i_=i_: e.tensor_copy(out=Bt4b[i_][:], in_=Bt4[i_][:]), r=[Bt4[i_]], w=[Bt4b[i_]])
    rden = k.alloc("rden", [128, 8], F32)
    oab = k.alloc("oab", [128, 8, 128], BF16)
    ecnt = [0]

    def stage_I(qb):
        L = 128 * (qb + 1)
        tsl = slice(qb * 128, (qb + 1) * 128)
        qb_ = qbT[qb % 2]
        qi_ = qiT[qb % 2]
        iw_ = iwt[qb % 2]
        sc = scs[qb % 2]
        Dw_ = Dws[qb % 2]
        k.dma("sp", qb_[:], aq.ap()[:, :, tsl].rearrange("h d t -> d h t"), r=["plain"], w=[qb_])
        k.dma("sp", qi_[:], iq.ap()[:, :, tsl].rearrange("h d t -> d h t"), r=["plain"], w=[qi_])
        k.dma("sp", iw_[:, 0:16], iw[qb * 128:(qb + 1) * 128, :], r=["iw"], w=[iw_])
        if qb < 2:
            return
        for h in range(16):
            k.op("act", lambda e, h=h: e.activation(out=Dw_[:, h, :], in_=ident_f[:], func=AF.Copy, scale=iw_[:, h:h + 1]), r=[ident_f, iw_], w=[Dw_])

        def idx_kg(kg):
            c0 = kg * 512
            n = min(512, L - c0)
            bacc = k.bank()
            k.reserved = {bacc.name}
            pend = []

            def acc(h, r_):
                k.op("pe", lambda e: e.matmul(bacc[:, 0:n], lhsT=Dw_[:, h, :], rhs=r_[:, 0:n], start=(h == 0), stop=(h == 15)), r=[Dw_, r_], w=[bacc])

            for h in range(16):
                bk_ = k.bank()
                r_ = rl[rcnt[0] % 4]
                rcnt[0] += 1
                k.op("pe", lambda e, h=h, bk_=bk_: e.matmul(bk_[:, 0:n], lhsT=qi_[:, h, :], rhs=ikT4[:, c0:c0 + n], start=True, stop=True), r=[qi_, ikT4], w=[bk_])
                k.op("act", lambda e, bk_=bk_, r_=r_: e.activation(out=r_[:, 0:n], in_=bk_[:, 0:n], func=AF.Relu), r=[bk_], w=[r_])
                pend.append((h, r_))
                if len(pend) > 2:
                    acc(*pend.pop(0))
            while pend:
                acc(*pend.pop(0))
            k.reserved = set()
            k.op("act", lambda e: e.copy(out=sc[:, c0:c0 + n], in_=bacc[:, 0:n]), r=[bacc], w=[sc])

        for kg in range((L + 511) // 512):
            idx_kg(kg)

    def stage_B(qb):
        L = 128 * (qb + 1)
        sc = scs[qb % 2]
        if qb >= 2:
            k.op("dve", lambda e: e.tensor_reduce(out=bs[:, 0:1], in_=sc[:, 0:L], axis=AX.X, op=ALU.max), r=[sc], w=[bs])
            k.op("dve", lambda e: e.tensor_reduce(out=bs[:, 1:2], in_=sc[:, 0:L], axis=AX.X, op=ALU.min), r=[sc, bs], w=[bs])
            k.op("dve", lambda e: e.tensor_scalar(out=bs[:, 1:2], in0=bs[:, 1:2], scalar1=-1.0, scalar2=None, op0=ALU.add), r=[bs], w=[bs])
            k.op("dve", lambda e: e.tensor_tensor(out=bs[:, 2:3], in0=bs[:, 0:1], in1=bs[:, 1:2], op=ALU.subtract), r=[bs], w=[bs])
            k.op("dve", lambda e: e.tensor_tensor(out=sc[:, L - 128:L], in0=sc[:, L - 128:L], in1=CM[:], op=ALU.add), r=[sc, CM], w=[sc])
            for it in range(NIT):
                f = 2.0 ** -(it + 1)
                k.op("dve", lambda e, f=f: e.scalar_tensor_tensor(out=bs[:, 3:4], in0=bs[:, 2:3], scalar=f, op0=ALU.mult, in1=bs[:, 1:2], op1=ALU.add), r=[bs], w=[bs])
                k.op("dve", lambda e: e.tensor_scalar(out=jnk[:, 0:L], in0=sc[:, 0:L], scalar1=bs[:, 3:4], scalar2=None, op0=ALU.is_gt, op1=ALU.add, accum_out=bs[:, 4:5]),
                     r=[sc, bs], w=[jnk, bs])
                k.op("dve", lambda e, f=f: e.tensor_scalar(out=bs[:, 5:6], in0=bs[:, 4:5], scalar1=255.5, scalar2=f, op0=ALU.is_gt, op1=ALU.mult), r=[bs], w=[bs])
                k.op("dve", lambda e: e.scalar_tensor_tensor(out=bs[:, 1:2], in0=bs[:, 5:6], scalar=bs[:, 2:3], op0=ALU.mult, in1=bs[:, 1:2], op1=ALU.add), r=[bs], w=[bs])
            k.op("dve", lambda e: e.tensor_scalar(out=sc[:, 0:L], in0=sc[:, 0:L], scalar1=bs[:, 1:2], scalar2=None, op0=ALU.subtract), r=[sc, bs], w=[sc])
            for k4 in range((qb + 1 + 3) // 4):
                nb = min(4, qb + 1 - k4 * 4)
                bk_ = k.bank()
                for j in range(nb):
                    kb = k4 * 4 + j
                    k.op("pe", lambda e, j=j, kb=kb, bk_=bk_: e.transpose(out=bk_[:, j * 128:(j + 1) * 128], in_=sc[:, kb * 128:(kb + 1) * 128], identity=ident_f[:]),
                         r=[sc, ident_f], w=[bk_])
                k.op("dve", lambda e, k4=k4, nb=nb, bk_=bk_: e.tensor_scalar(out=negT[:, k4 * 4:k4 * 4 + nb, :].rearrange("p a b -> p (a b)"), in0=bk_[:, 0:nb * 128],
                                                                             scalar1=0.0, scalar2=NEG, op0=ALU.is_le, op1=ALU.mult), r=[bk_], w=[negT])
            k.op("dve", lambda e: e.tensor_tensor(out=negT[:, qb, :], in0=negT[:, qb, :], in1=CMT[:], op=ALU.add), r=[negT, CMT], w=[negT])
        else:
            if qb == 1:
                k.op("pool", lambda e: e.memset(negT[:, 0, :], 0.0), w=[negT])
            k.op("pool", lambda e: e.tensor_copy(out=negT[:, qb, :], in_=CMT[:]), r=[CMT, negT], w=[negT])

    def stage_A(qb):
        tsl = slice(qb * 128, (qb + 1) * 128)
        qb_ = qbT[qb % 2]
        qi_ = qiT[qb % 2]
        bo0 = k.bank()
        bo1 = k.bank()
        bdn = k.bank()
        k.reserved = {bo0.name, bo1.name, bdn.name}
        for bz_ in (bo0, bo1, bdn):
            k.op("pe", lambda e, bz_=bz_: e.matmul(bz_[:, :], lhsT=zeros_b[:], rhs=qi_[:].rearrange("p a b -> p (a b)")[:, 0:512], start=True, stop=False), r=[zeros_b, qi_], w=[bz_])
        def kv_logits(kb, kvh):
            bl = k.bank()
            near = (qb - kb <= 1)
            k.op("pe", lambda e, bl=bl: e.matmul(bl[:, :], lhsT=kTb[:, kvh, kb * 128:(kb + 1) * 128], rhs=qb_[:, kvh * 4:(kvh + 1) * 4, :].rearrange("p a b -> p (a b)"),
                                                start=True, stop=False), r=[kTb, qb_], w=[bl])
            k.op("pe", lambda e, bl=bl: e.matmul(bl[:, :].rearrange("p (a b) -> p a b", a=4), lhsT=ident_b[:], rhs=negT[:, kb, :].unsqueeze(1).to_broadcast([128, 4, 128]),
                                                start=False, stop=(not near)), r=[ident_b, negT], w=[bl])
            if near:
                k.op("pe", lambda e, bl=bl: e.matmul(bl[:, :], lhsT=ident_b[:], rhs=Bt4b[qb - kb][:, kvh * 4:(kvh + 1) * 4, :].rearrange("p a b -> p (a b)"),
                                                    start=False, stop=True), r=[ident_b, Bt4b[qb - kb]], w=[bl])
            P_ = PTs[ecnt[0] % 4]
            ecnt[0] += 1
            k.op("act", lambda e, bl=bl, P_=P_: e.activation(out=P_[:].rearrange("p a b -> p (a b)"), in_=bl[:, :], func=AF.Exp), r=[bl], w=[P_])
            return P_

        def kv_pv(kb, kvh, P_):
            last = (kb == qb)
            for g_ in range(4):
                h = kvh * 4 + g_
                bo = bo0 if h < 4 else bo1
                k.op("pe", lambda e, g_=g_, h=h, bo=bo: e.matmul(bo[:, (h % 4) * 128:(h % 4 + 1) * 128], lhsT=P_[:, g_, :], rhs=vtok[:, kb, kvh * 128:(kvh + 1) * 128],
                                                                 start=False, stop=last), r=[P_, vtok], w=[bo])
                k.op("pe", lambda e, g_=g_, h=h: e.matmul(bdn[:, h:h + 1], lhsT=P_[:, g_, :], rhs=ones_b[:, 0:1], start=False, stop=last), r=[P_, ones_b], w=[bdn])

        pend = []
        for kb in range(qb + 1):
            for kvh in range(2):
                pend.append((kb, kvh, kv_logits(kb, kvh)))
                if len(pend) > 2:
                    kv_pv(*pend.pop(0))
        while pend:
            kv_pv(*pend.pop(0))
        k.reserved = set()
        k.op("dve", lambda e: e.reciprocal(out=rden[:], in_=bdn[:, 0:8]), r=[bdn], w=[rden])
        for half, bo in ((0, bo0), (1, bo1)):
            k.op("dve", lambda e, half=half, bo=bo: e.tensor_tensor(out=oab[:, half * 4:(half + 1) * 4, :], in0=bo[:, :].rearrange("p (a b) -> p a b", a=4),
                                                                    in1=rden[:, half * 4:(half + 1) * 4].unsqueeze(2).to_broadcast([128, 4, 128]), op=ALU.mult),
                 r=[bo, rden], w=[oab])
        bt_ = k.bank()
        btb = bt_[:].bitcast(BF16)
        for h in range(8):
            k.op("pe", lambda e, h=h: e.transpose(out=btb[:, h * 128:(h + 1) * 128], in_=oab[:, h, :], identity=ident_b[:]), r=[oab, ident_b], w=[bt_])
        k.op("act", lambda e: e.copy(out=mixT[:, 8:16, tsl], in_=btb[:, 0:1024].rearrange("p (a b) -> p a b", a=8)), r=[bt_], w=[(mixT, "b%d" % qb)])

    stage_I(0)
    for qb in range(NT):
        if qb + 1 < NT:
            stage_I(qb + 1)
        stage_B(qb)
        stage_A(qb)
    if stop_after == "P4" and os.environ.get("P4DBG"):
        for nm, b, n in (("sc", sc, T), ("bs", bs, 8), ("negT", negT, NT * 128)):
            dd = dout("dbg_" + nm, [128, n], F32)
            src = b[:] if nm != "negT" else b[:].rearrange("p a b -> p (a b)")
            k.dma("sp", dd.ap(), src, r=[b])
    if stop_after == "P4":
        dbg = dout("dbg_mixT", [128, 16 * TT], BF16)
        k.barrier()
        k.dma("sp", dbg.ap(), mixT[:].rearrange("p a b -> p (a b)"))
        return k.finish()
    k.barrier()
    k.release(m4)
    NPG = NPAGES
    ones4 = k.alloc("ones4", [128, 128], F32)
    zeros4 = k.alloc("zeros4", [128, 128], F32)
    Ltri = k.alloc("Ltri", [128, 128], BF16)
    siota = k.alloc("siota", [128, 128], F32)
    piota = k.alloc("piota", [128, 1], F32)
    jrow = k.alloc("jrow", [128, 256], F32)
    jcol = k.alloc("jcol", [128, 2], F32)
    posc = k.alloc("posc", [128, 128], F32)
    k.op("pool", lambda e: e.memset(ones4[:], 1.0), w=[ones4])
    k.op("pool", lambda e: e.memset(zeros4[:], 0.0), w=[zeros4])
    k.op("pool", lambda e: e.memset(Ltri[:], 1.0), w=[Ltri])
    k.op("pool", lambda e: e.affine_select(out=Ltri[:], in_=Ltri[:], pattern=[[1, 128]], compare_op=ALU.is_ge, fill=0.0, base=-1, channel_multiplier=-1), r=[Ltri], w=[Ltri])
    k.op("pool", lambda e: e.iota(siota[:], pattern=[[1, 128]], base=0, channel_multiplier=0, allow_small_or_imprecise_dtypes=True), w=[siota])
    k.op("pool", lambda e: e.iota(piota[:], pattern=[[0, 1]], base=0, channel_multiplier=1, allow_small_or_imprecise_dtypes=True), w=[piota])
    k.op("pool", lambda e: e.iota(jrow[:], pattern=[[1, 256]], base=0, channel_multiplier=0, allow_small_or_imprecise_dtypes=True), w=[jrow])
    k.op("pool", lambda e: e.iota(jcol[:], pattern=[[128, 2]], base=0, channel_multiplier=1, allow_small_or_imprecise_dtypes=True), w=[jcol])
    k.op("pool", lambda e: e.iota(posc[:], pattern=[[1, 128]], base=0, channel_multiplier=128, allow_small_or_imprecise_dtypes=True), w=[posc])
    thrb = k.alloc("thrb", [128, 31], F32)
    k.dma("sp", thrb[:], bthr.ap().to_broadcast([128, 31]), w=[thrb])
    rbs = k.alloc("rbs", [32, 8], F32)
    rbT = k.alloc("rbT", [8, 32], F32)
    drbT = k.alloc("drbT", [128, 8, 32], F32)
    k.dma("sp", rbs[:], rel_bias.ap(), w=[rbs])
    bq = k.bank()
    k.op("pe", lambda e: e.transpose(out=bq[0:8, 0:32], in_=rbs[:], identity=ident_f[0:32, 0:32]), r=[rbs, ident_f], w=[bq])
    k.op("act", lambda e: e.copy(out=rbT[:], in_=bq[0:8, 0:32]), r=[bq], w=[rbT])
    k.dma("sp", rbTd.ap(), rbT[:], r=[rbT], w=["rbTd"])
    k.dma("sp", drbT[:].rearrange("p a b -> p (a b)"), rbTd.ap().rearrange("a b -> (a b)").unsqueeze(0).to_broadcast([128, 256]), r=["rbTd"], w=[drbT])
    rb0b = k.alloc("rb0b", [128, 8], F32)
    k.op("dve", lambda e: e.tensor_copy(out=rb0b[:], in_=drbT[:, :, 0]), r=[drbT], w=[rb0b])
    dtmp = k.alloc("dtmp", [128, 8, 31], F32)
    k.op("dve", lambda e: e.tensor_tensor(out=dtmp[:], in0=drbT[:, :, 1:32], in1=drbT[:, :, 0:31], op=ALU.subtract), r=[drbT], w=[dtmp])
    pt_i = k.alloc("pt_i", [128, TS], I32)
    pt_f = k.alloc("pt_f", [128, TS], F32)
    k.dma("sp", pt_i[:], page_table.ap().rearrange("s p -> p s"), w=[pt_i], slow=True)
    k.op("dve", lambda e: e.tensor_copy(out=pt_f[:], in_=pt_i[:]), r=[pt_i], w=[pt_f])
    k.op("dve", lambda e: e.tensor_scalar(out=pt_f[:], in0=pt_f[:], scalar1=128.0, scalar2=None, op0=ALU.mult), r=[pt_f], w=[pt_f])
    qiS = k.alloc("qiS", [128, TS, 16], BF16)
    wS = k.alloc("wS", [128, TS, 16], F32)
    kiS = k.alloc("kiS", [128, TS], BF16)
    qbS = k.alloc("qbS", [128, 8, TS], BF16)
    knS = k.alloc("knS", [128, 2, TS], BF16)
    for s_i in range(TS):
        k.dma("sp", qiS[:, s_i, :], iq.ap()[:, :, T + s_i].rearrange("h d -> d h"), r=["plain"], w=[qiS], slow=True)
    k.dma("sp", wS[:].rearrange("p a b -> p (a b)"), iw[T:TT, :].rearrange("a b -> (a b)").unsqueeze(0).to_broadcast([128, TS * 16]), r=["iw"], w=[wS])
    k.dma("sp", kiS[:], ikTd[:, T:TT], r=["ikTd"], w=[kiS])
    k.dma("sp", qbS[:], aq.ap()[:, :, T:TT].rearrange("h d s -> d h s"), r=["plain"], w=[qbS])
    k.dma("sp", knS[:], akT.ap()[:, :, T:TT].rearrange("h d s -> d h s"), r=["plain"], w=[knS])
    scS = k.alloc("scS", [128, TS, 128], F32)
    snew = k.alloc("snew", [128, TS], F32)
    Gp = k.alloc("Gp", [128, NPG * 128], F32)
    kTs = [k.alloc(f"kTs{i}", [128, 4, 128], BF16) for i in range(2)]
    rr = k.alloc("rr", [128, 32, 16], F32)
    knb = k.alloc("knb", [128, 128], BF16)
    ckx2 = cache_kidx.ap()

    def dma_raw(q, fn, r=(), w=()):
        i = k.drr[q]
        k.drr[q] = (i + 1) % len(k.dsem[q])
        sk = ("d", q, i)
        waits = k._collect(q, r, w)
        prev = k.dcnt[q][i]
        kn = k.known[q]
        if prev > 0 and kn.get(sk, 0) < prev:
            kn[sk] = prev
            waits.append((sk, prev))
        k.dcnt[q][i] = prev + 16
        tok = (sk, prev + 16)
        k.ops[q].append((waits, fn, (sk, 16)))
        k._update(tok, r, w)
        return tok

    def scores_seq(s_i):
        dma_raw("pool", lambda e: e.indirect_dma_start(out=Gp[:], out_offset=None, in_=ckx2, in_offset=bass.IndirectOffsetOnAxis(ap=pt_i[:, s_i:s_i + 1], axis=0)),
                r=[pt_i], w=[Gp])
        scb = [k.bank() for _ in range(4)]
        k.reserved = {b_.name for b_ in scb}
        for sb in range(32):
            bt_ = k.bank()
            kt_ = kTs[sb % 2]
            for j in range(4):
                sl_ = 4 * sb + j
                k.op("pe", lambda e, j=j, sl_=sl_, bt_=bt_: e.transpose(out=bt_[:, j * 128:(j + 1) * 128], in_=Gp[:, sl_ * 128:(sl_ + 1) * 128], identity=ident_f[:]),
                     r=[Gp, ident_f], w=[bt_])
            k.op("act", lambda e, bt_=bt_, kt_=kt_: e.copy(out=kt_[:].rearrange("p a b -> p (a b)"), in_=bt_[:, :]), r=[bt_], w=[kt_])
            for j in range(4):
                sl_ = 4 * sb + j
                sbk = scb[sl_ // 32]
                k.op("pe", lambda e, j=j, sl_=sl_, sbk=sbk, kt_=kt_: e.matmul(sbk[:, (sl_ % 32) * 16:(sl_ % 32 + 1) * 16], lhsT=kt_[:, j, :], rhs=qiS[:, s_i, :], start=True, stop=True),
                     r=[kt_, qiS], w=[sbk])
        k.reserved = set()
        for b_i in range(4):
            sbk = scb[b_i]
            k.op("dve", lambda e, sbk=sbk: e.tensor_scalar(out=rr[:].rearrange("p a b -> p (a b)"), in0=sbk[:, :], scalar1=0.0, scalar2=None, op0=ALU.max), r=[sbk], w=[rr])
            k.op("dve", lambda e: e.tensor_tensor(out=rr[:], in0=rr[:], in1=wS[:, s_i, :].unsqueeze(1).to_broadcast([128, 32, 16]), op=ALU.mult), r=[rr, wS], w=[rr])
            k.op("dve", lambda e, b_i=b_i: e.tensor_reduce(out=scS[:, s_i, b_i * 32:(b_i + 1) * 32], in_=rr[:], axis=AX.X, op=ALU.add), r=[rr], w=[scS])
        k.op("dve", lambda e: e.tensor_copy(out=knb[:], in_=kiS[:, s_i:s_i + 1].to_broadcast([128, 128])), r=[kiS], w=[knb])
        bn = k.bank()
        k.op("pe", lambda e: e.matmul(bn[:, 0:16], lhsT=knb[:], rhs=qiS[:, s_i, :], start=True, stop=True), r=[knb, qiS], w=[bn])
        k.op("dve", lambda e: e.tensor_scalar(out=rr[:, 0, :], in0=bn[:, 0:16], scalar1=0.0, scalar2=None, op0=ALU.max), r=[bn], w=[rr])
        k.op("dve", lambda e: e.tensor_tensor(out=rr[:, 0, :], in0=rr[:, 0, :], in1=wS[:, s_i, :], op=ALU.mult), r=[rr, wS], w=[rr])
        k.op("dve", lambda e: e.tensor_reduce(out=snew[:, s_i:s_i + 1], in_=rr[:, 0, :], axis=AX.X, op=ALU.add), r=[rr], w=[snew])

    for s_i in range(0 if skip_p4s else TS):
        scores_seq(s_i)

    pm = k.alloc("pm", [128, 2 * TS], F32)
    gmm = k.alloc("gmm", [TS, 4], F32)
    dgm = k.alloc("dgm", [TS, 2 * TS], F32)
    lo_ = k.alloc("lo_", [128, TS], F32)
    w0_ = k.alloc("w0_", [128, TS], F32)
    bsS = k.alloc("bsS", [128, 6 * TS], F32)
    cmpb = k.alloc("cmpb", [128, TS, 128], F32)
    k.op("dve", lambda e: e.tensor_reduce(out=pm[:, 0:TS], in_=scS[:], axis=AX.X, op=ALU.max), r=[scS], w=[pm])
    k.op("dve", lambda e: e.tensor_reduce(out=pm[:, TS:2 * TS], in_=scS[:], axis=AX.X, op=ALU.min), r=[scS, pm], w=[pm])
    k.op("dve", lambda e: e.tensor_tensor(out=pm[:, 0:TS], in0=pm[:, 0:TS], in1=snew[:], op=ALU.max), r=[pm, snew], w=[pm])
    k.op("dve", lambda e: e.tensor_tensor(out=pm[:, TS:2 * TS], in0=pm[:, TS:2 * TS], in1=snew[:], op=ALU.min), r=[pm, snew], w=[pm])
    bmx = k.bank()
    bmn = k.bank()
    k.op("pe", lambda e: e.transpose(out=bmx[0:TS, 0:128], in_=pm[:, 0:TS], identity=ident_f[:]), r=[pm, ident_f], w=[bmx])
    k.op("pe", lambda e: e.transpose(out=bmn[0:TS, 0:128], in_=pm[:, TS:2 * TS], identity=ident_f[:]), r=[pm, ident_f], w=[bmn])
    k.op("dve", lambda e: e.tensor_reduce(out=gmm[:, 0:1], in_=bmx[0:TS, 0:128], axis=AX.X, op=ALU.max), r=[bmx], w=[gmm])
    k.op("dve", lambda e: e.tensor_reduce(out=gmm[:, 1:2], in_=bmn[0:TS, 0:128], axis=AX.X, op=ALU.min), r=[bmn, gmm], w=[gmm])
    k.op("dve", lambda e: e.tensor_scalar(out=gmm[:, 1:2], in0=gmm[:, 1:2], scalar1=-1.0, scalar2=None, op0=ALU.add), r=[gmm], w=[gmm])
    k.op("dve", lambda e: e.tensor_tensor(out=gmm[:, 2:3], in0=gmm[:, 0:1], in1=gmm[:, 1:2], op=ALU.subtract), r=[gmm], w=[gmm])
    k.op("dve", lambda e: e.tensor_scalar(out=dgm[:, 0:TS], in0=ident_f[0:TS, 0:TS], scalar1=gmm[:, 1:2], scalar2=None, op0=ALU.mult), r=[gmm, ident_f], w=[dgm])
    k.op("dve", lambda e: e.tensor_scalar(out=dgm[:, TS:2 * TS], in0=ident_f[0:TS, 0:TS], scalar1=gmm[:, 2:3], scalar2=None, op0=ALU.mult), r=[gmm, ident_f, dgm], w=[dgm])
    bbc = k.bank()
    k.op("pe", lambda e: e.matmul(bbc[:, 0:2 * TS], lhsT=ones4[0:TS, :], rhs=dgm[:], start=True, stop=True), r=[ones4, dgm], w=[bbc])
    k.op("act", lambda e: e.copy(out=lo_[:], in_=bbc[:, 0:TS]), r=[bbc], w=[lo_])
    k.op("act", lambda e: e.copy(out=w0_[:], in_=bbc[:, TS:2 * TS]), r=[bbc], w=[w0_])
    mid_ = bsS[:, 0:TS]
    cnt_ = bsS[:, TS:2 * TS]
    gn_ = bsS[:, 2 * TS:3 * TS]
    tot_ = bsS[:, 3 * TS:4 * TS]
    ge_ = bsS[:, 4 * TS:5 * TS]

    def bis_iter(it):
        f = 2.0 ** -(it + 1)
        k.op("dve", lambda e: e.scalar_tensor_tensor(out=mid_, in0=w0_[:], scalar=f, op0=ALU.mult, in1=lo_[:], op1=ALU.add), r=[w0_, lo_], w=[bsS])
        k.op("dve", lambda e: e.tensor_tensor(out=cmpb[:], in0=scS[:], in1=mid_.unsqueeze(2).to_broadcast([128, TS, 128]), op=ALU.is_gt), r=[scS, bsS], w=[cmpb])
        k.op("dve", lambda e: e.tensor_reduce(out=cnt_, in_=cmpb[:], axis=AX.X, op=ALU.add), r=[cmpb, bsS], w=[bsS])
        bc_ = k.bank()
        k.op("pe", lambda e: e.matmul(bc_[:, 0:TS], lhsT=ones4[:], rhs=cnt_, start=True, stop=True), r=[ones4, bsS], w=[bc_])
        k.op("dve", lambda e: e.tensor_tensor(out=gn_, in0=snew[:], in1=mid_, op=ALU.is_gt), r=[snew, bsS], w=[bsS])
        k.op("dve", lambda e: e.tensor_tensor(out=tot_, in0=bc_[:, 0:TS], in1=gn_, op=ALU.add), r=[bc_, bsS], w=[bsS])
        k.op("dve", lambda e: e.tensor_scalar(out=ge_, in0=tot_, scalar1=255.5, scalar2=f, op0=ALU.is_gt, op1=ALU.mult), r=[bsS], w=[bsS])
        k.op("dve", lambda e: e.tensor_tensor(out=ge_, in0=ge_, in1=w0_[:], op=ALU.mult), r=[bsS, w0_], w=[bsS])
        k.op("dve", lambda e: e.tensor_tensor(out=lo_[:], in0=lo_[:], in1=ge_, op=ALU.add), r=[lo_, bsS], w=[lo_])

    for it in range(0 if skip_p4s else 20):
        bis_iter(it)

    Msel = k.alloc("Msel", [128, 128], F32)
    Mb = k.alloc("Mb", [128, 128], BF16)
    Bs = k.alloc("Bs", [128, 128], F32)
    cum = k.alloc("cum", [128, 128], F32)
    rank = k.alloc("rank", [128, 128], F32)
    payl = k.alloc("payl", [128, 128, 2], F32)
    Soh = [k.alloc(f"Soh{i}", [128, 256], F32) for i in range(2)]
    idxf = k.alloc("idxf", [128, 4], F32)
    idx_i = k.alloc("idx_i", [128, 2], I32)
    Ksel = k.alloc("Ksel", [128, 2, 256], F32)
    Vsel = k.alloc("Vsel", [128, 2, 256], F32)
    KTs = k.alloc("KTs", [128, 4, 128], F32)
    qf = k.alloc("qf", [128, 8], F32)
    knf = k.alloc("knf", [128, 2], F32)
    sm = k.alloc("sm", [128, 64], F32)
    ind = k.alloc("ind", [128, 2, 31], F32)
    prod = k.alloc("prod", [128, 2, 8, 31], F32)
    Eg = k.alloc("Eg", [128, 2, 8], F32)
    rowp = k.alloc("rowp", [1, 64], F32)
    vnr = k.alloc("vnr", [1, 256], BF16)
    vnf = k.alloc("vnf", [1, 256], F32)
    osb = k.alloc("osb", [4, 2, 130], F32)
    ck2 = cache_k.ap()
    cv2 = cache_v.ap()
    k.op("dve", lambda e: e.tensor_copy(out=payl[:, :, 1], in_=posc[:]), r=[posc], w=[payl])

    def attend_seq(s_i):
        col = T + s_i
        k.op("dve", lambda e: e.tensor_scalar(out=Msel[:], in0=scS[:, s_i, :], scalar1=lo_[:, s_i:s_i + 1], scalar2=None, op0=ALU.is_gt), r=[scS, lo_], w=[Msel])
        k.op("dve", lambda e: e.tensor_copy(out=Mb[:], in_=Msel[:]), r=[Msel], w=[Mb])
        k.op("dve", lambda e: e.tensor_tensor(out=sm[:, 0:1], in0=snew[:, s_i:s_i + 1], in1=lo_[:, s_i:s_i + 1], op=ALU.is_gt), r=[snew, lo_], w=[sm])
        bA_ = k.bank()
        bB_ = k.bank()
        k.op("pe", lambda e: e.matmul(bA_[:, 0:128], lhsT=Ltri[:], rhs=Mb[:], start=True, stop=True), r=[Ltri, Mb], w=[bA_])
        k.op("pe", lambda e: e.matmul(bB_[:, 0:128], lhsT=ones_b[:], rhs=Mb[:], start=True, stop=True), r=[ones_b, Mb], w=[bB_])
        k.op("act", lambda e: e.copy(out=Bs[:], in_=bB_[:, 0:128]), r=[bB_], w=[Bs])
        k.op("dve", lambda e: e.tensor_tensor_scan(out=cum[:], data0=Bs[:], data1=zeros4[:], initial=0.0, op0=ALU.add, op1=ALU.add), r=[Bs, zeros4], w=[cum])
        k.op("dve", lambda e: e.tensor_tensor(out=rank[:], in0=cum[:], in1=Bs[:], op=ALU.subtract), r=[cum, Bs], w=[rank])
        k.op("dve", lambda e: e.tensor_tensor(out=rank[:], in0=rank[:], in1=bA_[:, 0:128], op=ALU.add), r=[rank, bA_], w=[rank])
        k.op("dve", lambda e: e.scalar_tensor_tensor(out=rank[:], in0=rank[:], scalar=1.0, op0=ALU.add, in1=Msel[:], op1=ALU.mult), r=[rank, Msel], w=[rank])
        k.op("dve", lambda e: e.tensor_scalar(out=rank[:], in0=rank[:], scalar1=-1.0, scalar2=None, op0=ALU.add), r=[rank], w=[rank])
        k.op("dve", lambda e: e.tensor_scalar(out=payl[:, :, 0], in0=siota[:], scalar1=pt_f[:, s_i:s_i + 1], scalar2=None, op0=ALU.add), r=[siota, pt_f], w=[payl])
        bacc = k.bank()
        k.reserved = {bacc.name}
        k.op("pe", lambda e: e.matmul(bacc[:, 0:4], lhsT=zeros4[:], rhs=ones4[:, 0:4], start=True, stop=False), r=[zeros4, ones4], w=[bacc])
        for sl_ in range(128):
            so_ = Soh[sl_ % 2]
            k.op("dve", lambda e, sl_=sl_, so_=so_: e.tensor_scalar(out=so_[:], in0=jrow[:], scalar1=rank[:, sl_:sl_ + 1], scalar2=None, op0=ALU.is_equal), r=[jrow, rank], w=[so_])
            for half in range(2):
                k.op("pe", lambda e, sl_=sl_, so_=so_, half=half: e.matmul(bacc[:, half * 2:(half + 1) * 2], lhsT=so_[:, half * 128:(half + 1) * 128], rhs=payl[:, sl_, :],
                                                                      start=False, stop=(sl_ == 127)), r=[so_, payl], w=[bacc])
        k.reserved = set()
        k.op("act", lambda e: e.copy(out=idxf[:], in_=bacc[:, 0:4]), r=[bacc], w=[idxf])
        k.op("dve", lambda e: e.tensor_copy(out=idx_i[:], in_=idxf[:].rearrange("p (a b) -> p a b", b=2)[:, :, 0]), r=[idxf], w=[idx_i])
        for half in range(2):
            dma_raw("pool", lambda e, half=half: e.indirect_dma_start(out=Ksel[:, half, :], out_offset=None, in_=ck2, in_offset=bass.IndirectOffsetOnAxis(ap=idx_i[:, half:half + 1], axis=0)),
                    r=[idx_i], w=[Ksel])
            dma_raw("pool", lambda e, half=half: e.indirect_dma_start(out=Vsel[:, half, :], out_offset=None, in_=cv2, in_offset=bass.IndirectOffsetOnAxis(ap=idx_i[:, half:half + 1], axis=0)),
                    r=[idx_i], w=[Vsel])
        bkt = k.bank()
        for half in range(2):
            for kvh in range(2):
                jj = half * 2 + kvh
                k.op("pe", lambda e, half=half, kvh=kvh, jj=jj: e.transpose(out=bkt[:, jj * 128:(jj + 1) * 128], in_=Ksel[:, half, kvh * 128:(kvh + 1) * 128], identity=ident_f[:]),
                     r=[Ksel, ident_f], w=[bkt])
        k.op("act", lambda e: e.copy(out=KTs[:].rearrange("p a b -> p (a b)"), in_=bkt[:, :]), r=[bkt], w=[KTs])
        k.op("dve", lambda e: e.tensor_copy(out=qf[:], in_=qbS[:, :, s_i]), r=[qbS], w=[qf])
        k.op("dve", lambda e: e.tensor_copy(out=knf[:], in_=knS[:, :, s_i]), r=[knS], w=[knf])
        blg = k.bank()
        for half in range(2):
            for kvh in range(2):
                jj = half * 2 + kvh
                k.op("pe", lambda e, half=half, kvh=kvh, jj=jj: e.matmul(blg[:, half * 8 + kvh * 4:half * 8 + kvh * 4 + 4], lhsT=KTs[:, jj, :], rhs=qf[:, kvh * 4:(kvh + 1) * 4], start=True, stop=True),
                     r=[KTs, qf], w=[blg])
        bln = k.bank()
        for kvh in range(2):
            k.op("pe", lambda e, kvh=kvh: e.matmul(bln[0:1, kvh * 4:(kvh + 1) * 4], lhsT=knf[:, kvh:kvh + 1], rhs=qf[:, kvh * 4:(kvh + 1) * 4], start=True, stop=True), r=[knf, qf], w=[bln])
        k.op("dve", lambda e: e.tensor_scalar(out=sm[:, 2:4], in0=idxf[:].rearrange("p (a b) -> p a b", b=2)[:, :, 1], scalar1=-1.0, scalar2=float(NPG * 128), op0=ALU.mult, op1=ALU.add), r=[idxf], w=[sm])
        k.op("dve", lambda e: e.tensor_tensor(out=ind[:], in0=sm[:, 2:4].unsqueeze(2).to_broadcast([128, 2, 31]), in1=thrb[:].unsqueeze(1).to_broadcast([128, 2, 31]), op=ALU.is_ge), r=[sm, thrb], w=[ind])
        k.op("dve", lambda e: e.tensor_tensor(out=prod[:], in0=ind[:].unsqueeze(2).to_broadcast([128, 2, 8, 31]), in1=dtmp[:].unsqueeze(1).to_broadcast([128, 2, 8, 31]), op=ALU.mult), r=[ind, dtmp], w=[prod])
        k.op("dve", lambda e: e.tensor_reduce(out=Eg[:], in_=prod[:], axis=AX.X, op=ALU.add), r=[prod], w=[Eg])
        k.op("dve", lambda e: e.tensor_tensor(out=Eg[:], in0=Eg[:], in1=rb0b[:].unsqueeze(1).to_broadcast([128, 2, 8]), op=ALU.add), r=[Eg, rb0b], w=[Eg])
        k.op("dve", lambda e: e.tensor_scalar(out=sm[:, 4:6], in0=jcol[:], scalar1=cum[:, 127:128], scalar2=None, op0=ALU.is_lt), r=[jcol, cum, sm], w=[sm])
        k.op("dve", lambda e: e.tensor_scalar(out=sm[:, 4:6], in0=sm[:, 4:6], scalar1=-1.0, scalar2=30000.0, op0=ALU.add, op1=ALU.mult), r=[sm], w=[sm])
        k.op("dve", lambda e: e.tensor_tensor(out=Eg[:], in0=Eg[:], in1=sm[:, 4:6].unsqueeze(2).to_broadcast([128, 2, 8]), op=ALU.add), r=[Eg, sm], w=[Eg])
        k.op("dve", lambda e: e.tensor_tensor(out=Eg[:].rearrange("p a b -> p (a b)"), in0=Eg[:].rearrange("p a b -> p (a b)"), in1=blg[:, 0:16], op=ALU.add), r=[Eg, blg], w=[Eg])
        k.op("act", lambda e: e.activation(out=Eg[:], in_=Eg[:], func=AF.Exp), r=[Eg], w=[Eg])
        k.op("dve", lambda e: e.tensor_scalar(out=rowp[:, 8:9], in0=sm[0:1, 0:1], scalar1=-1.0, scalar2=30000.0, op0=ALU.add, op1=ALU.mult), r=[sm], w=[rowp])
        k.op("dve", lambda e: e.tensor_tensor(out=rowp[:, 0:8], in0=bln[0:1, 0:8], in1=rb0b[0:1, :], op=ALU.add), r=[bln, rb0b, rowp], w=[rowp])
        k.op("dve", lambda e: e.tensor_scalar(out=rowp[:, 0:8], in0=rowp[:, 0:8], scalar1=rowp[:, 8:9], scalar2=None, op0=ALU.add), r=[rowp], w=[rowp])
        k.op("act", lambda e: e.activation(out=rowp[:, 0:8], in_=rowp[:, 0:8], func=AF.Exp), r=[rowp], w=[rowp])
        k.dma("sp", vnr[:], av[col:col + 1, :], r=["av"], w=[vnr])
        k.op("dve", lambda e: e.tensor_copy(out=vnf[:], in_=vnr[:]), r=[vnr], w=[vnf])
        for kvh in range(2):
            bon = k.bank()
            bod = k.bank()
            for half in range(2):
                k.op("pe", lambda e, kvh=kvh, half=half, bon=bon: e.matmul(bon[0:4, 0:128], lhsT=Eg[:, half, kvh * 4:(kvh + 1) * 4], rhs=Vsel[:, half, kvh * 128:(kvh + 1) * 128], start=(half == 0), stop=False),
                     r=[Eg, Vsel], w=[bon])
            k.op("pe", lambda e, kvh=kvh, bon=bon: e.matmul(bon[0:4, 0:128], lhsT=rowp[0:1, kvh * 4:(kvh + 1) * 4], rhs=vnf[0:1, kvh * 128:(kvh + 1) * 128], start=False, stop=True), r=[rowp, vnf], w=[bon])
            for half in range(2):
                k.op("pe", lambda e, kvh=kvh, half=half, bod=bod: e.matmul(bod[0:4, 0:1], lhsT=Eg[:, half, kvh * 4:(kvh + 1) * 4], rhs=ones4[:, 0:1], start=(half == 0), stop=False), r=[Eg, ones4], w=[bod])
            k.op("pe", lambda e, kvh=kvh, bod=bod: e.matmul(bod[0:4, 0:1], lhsT=rowp[0:1, kvh * 4:(kvh + 1) * 4], rhs=ones4[0:1, 0:1], start=False, stop=True), r=[rowp, ones4], w=[bod])
            k.op("dve", lambda e, kvh=kvh, bod=bod: e.reciprocal(out=osb[:, kvh, 128:129], in_=bod[0:4, 0:1]), r=[bod], w=[osb])
            k.op("dve", lambda e, kvh=kvh, bon=bon: e.tensor_scalar(out=osb[:, kvh, 0:128], in0=bon[0:4, 0:128], scalar1=osb[:, kvh, 128:129], scalar2=None, op0=ALU.mult), r=[bon, osb], w=[osb])
        bot = k.bank()
        for kvh in range(2):
            k.op("pe", lambda e, kvh=kvh: e.transpose(out=bot[:, kvh * 4:(kvh + 1) * 4], in_=osb[:, kvh, 0:128], identity=ident_f[0:4, 0:4]), r=[osb, ident_f], w=[bot])
        k.op("act", lambda e: e.copy(out=mixT[:, 8:16, col], in_=bot[:, 0:8]), r=[bot], w=[(mixT, "sb")])

    for s_i in range(0 if skip_p4s else TS):
        attend_seq(s_i)
    k.barrier()
    k.release(m4)
    m5 = k.mark()
    wout = k.alloc("wout", [128, 16, D], BF16)
    for g in range(4):
        k.dma("pool", wout[:, :, g * 512:(g + 1) * 512], w_out[:, g * 512:(g + 1) * 512].rearrange("(k p) n -> p k n", p=128), w=[(wout, g)])
    G1b = k.alloc("G1b", [128, D], BF16)
    A2b = k.alloc("A2b", [128, D], BF16)
    SH2b = k.alloc("SH2b", [128, D], BF16)
    for buf_, idx_ in ((G1b, 2), (SH2b, 3), (A2b, 4)):
        k.dma("pool", buf_[:], modd[0:1, idx_ * D:(idx_ + 1) * D].to_broadcast([128, D]), r=["modd"], w=[buf_])
    xt5 = [k.alloc(f"xt5_{i}", [128, D], F32) for i in range(2)]
    mos = [k.alloc(f"mo{i}", [128, D], F32) for i in range(2)]
    h2b = k.alloc("h2b", [128, D], BF16)
    jnk5 = k.alloc("jnk5", [128, D], BF16)
    st5 = [k.alloc(f"st5_{i}", [128, 8], F32) for i in range(2)]
    h2st = [k.alloc("h2st0", [128, 16, 128], BF16)] * 2

    def p5_tile(ti):
        c0, n = (ti * 128, 128) if ti < NT else (T, TS)
        x_ = xt5[ti % 2]
        mo = mos[ti % 2]
        s_ = st5[ti % 2]
        hs_ = h2st[ti % 2]
        G1_, A2_, SH2_ = (G1b, A2b, SH2b)
        if ti == NT:
            k.dma("pool", G1b[0:TS, :], modd[1:5, 2 * D:3 * D], r=["modd"], w=[G1b])
            k.dma("pool", SH2b[0:TS, :], modd[1:5, 3 * D:4 * D], r=["modd"], w=[SH2b])
            k.dma("pool", A2b[0:TS, :], modd[1:5, 4 * D:5 * D], r=["modd"], w=[A2b])
        if ti == 0:
            k.dma("sp", x_[0:n, :], xp[c0:c0 + n, :], w=[x_])
        if ti + 1 <= NT:
            tn = ti + 1
            cn, nn = (tn * 128, 128) if tn < NT else (T, TS)
            xn_ = xt5[tn % 2]
            k.dma("sp", xn_[0:nn, :], xp[cn:cn + nn, :] if tn < NT else xs.ap(), w=[xn_])
        mkeys = [(mixT, ti), (mixT, "b%d" % ti)] if ti < NT else [(mixT, "s%d" % j) for j in range(TS)] + [(mixT, "sb")]
        for nq in range(4):
            bk_ = k.bank()
            for kk in range(16):
                k.op("pe", lambda e, kk=kk, bk_=bk_, nq=nq: e.matmul(bk_[0:n, :], lhsT=mixT[:, kk, c0:c0 + n], rhs=wout[:, kk, nq * 512:(nq + 1) * 512], start=(kk == 0), stop=(kk == 15)),
                     r=mkeys + [(wout, nq)], w=[bk_])
            k.op("act", lambda e, bk_=bk_, nq=nq: e.copy(out=mo[0:n, nq * 512:(nq + 1) * 512], in_=bk_[0:n, :]), r=[bk_], w=[mo])
        k.op("act", lambda e: e.activation(out=jnk5[0:n, :], in_=mo[0:n, :], func=AF.Square, accum_out=s_[0:n, 0:1]), r=[mo], w=[jnk5, s_])
        k.op("act", lambda e: e.activation(out=s_[0:n, 1:2], in_=s_[0:n, 0:1], func=AF.Sqrt, scale=1.0 / D, bias=EPS), r=[s_], w=[s_])
        k.op("dve", lambda e: e.reciprocal(out=s_[0:n, 2:3], in_=s_[0:n, 1:2]), r=[s_], w=[s_])
        k.op("dve", lambda e: e.scalar_tensor_tensor(out=mo[0:n, :], in0=mo[0:n, :], scalar=s_[0:n, 2:3], op0=ALU.mult, in1=G1_[0:n, :], op1=ALU.mult), r=[mo, s_, G1_], w=[mo])
        k.op("dve", lambda e: e.tensor_tensor(out=x_[0:n, :], in0=x_[0:n, :], in1=mo[0:n, :], op=ALU.add), r=[x_, mo], w=[x_])
        k.dma("sp", x1d[c0:c0 + n, :], x_[0:n, :], r=[x_], w=["x1d"])
        k.op("act", lambda e: e.activation(out=jnk5[0:n, :], in_=x_[0:n, :], func=AF.Square, accum_out=s_[0:n, 3:4]), r=[x_, s_], w=[jnk5, s_])
        k.op("act", lambda e: e.activation(out=s_[0:n, 4:5], in_=s_[0:n, 3:4], func=AF.Sqrt, scale=1.0 / D, bias=EPS), r=[s_], w=[s_])
        k.op("dve", lambda e: e.reciprocal(out=s_[0:n, 5:6], in_=s_[0:n, 4:5]), r=[s_], w=[s_])
        k.op("dve", lambda e: e.scalar_tensor_tensor(out=mo[0:n, :], in0=x_[0:n, :], scalar=s_[0:n, 5:6], op0=ALU.mult, in1=A2_[0:n, :], op1=ALU.mult), r=[x_, s_, A2_, mo], w=[mo])
        k.op("dve", lambda e: e.tensor_tensor(out=h2b[0:n, :], in0=mo[0:n, :], in1=SH2_[0:n, :], op=ALU.add), r=[mo, SH2_], w=[h2b])
        b0 = k.bank()
        b1 = k.bank()
        for kk in range(16):
            bb = b0 if kk < 8 else b1
            k.op("pe", lambda e, kk=kk, bb=bb: e.transpose(out=bb[:].bitcast(BF16)[:, (kk % 8) * 128:(kk % 8) * 128 + n], in_=h2b[0:n, kk * 128:(kk + 1) * 128], identity=ident_b[0:n, 0:n]),
                 r=[h2b, ident_b], w=[bb])
        for half, bb in ((0, b0), (1, b1)):
            k.op("act", lambda e, half=half, bb=bb: e.copy(out=hs_[:, half * 8:(half + 1) * 8, 0:n], in_=bb[:].bitcast(BF16).rearrange("p (a b) -> p a b", a=8)[:, :, 0:n]),
                 r=[bb], w=[hs_])
        k.dma("sp", h2Td[:, :, c0:c0 + n], hs_[:, :, 0:n], r=[hs_], w=["h2Td"])

    for ti in range(NT + 1):
        p5_tile(ti)
    k.barrier()
    k.release(mP)
    if stop_after == "P5":
        return k.finish()

    alloc_wb()
    TB = 512
    NBLK6 = T // TB
    uT = k.alloc("uT", [128, 64, TB], BF16)
    uTs = k.alloc("uTs", [128, 64, TS], BF16)
    h2Tb = k.alloc("h2Tb", [128, 16, TB], BF16)
    h2Ts = k.alloc("h2Ts", [128, 16, TS], BF16)
    w2b = [k.alloc(f"w2b{i}", [128, 8, 512], BF16) for i in range(2)]
    fbuf = [k.alloc(f"fbuf{i}", [128, D], F32) for i in range(4)]
    x1t = [k.alloc("x1t0", [128, D], F32)] * 2
    G2b = k.alloc("G2b", [128, D], F32)
    load_mod_bcast(G2b, 5)
    rl6 = [k.alloc(f"rl6_{i}", [128, TB], BF16) for i in range(2)]
    st6 = [k.alloc(f"st6_{i}", [128, 4], F32) for i in range(2)]
    w2cnt = [0]
    k.dma("sp", h2Ts[:], h2Td[:, :, T:TT], r=["h2Td"], w=[h2Ts])

    def ffn_block(b):
        with_s = (b == NBLK6 - 1)
        if b == 0:
            k.dma("sp", h2Tb[:], h2Td[:, :, b * TB:(b + 1) * TB], r=["h2Td"], w=[h2Tb])
        def phaseA(g):
            wt = wb[wcnt[0] % 2]
            wcnt[0] += 1
            k.dma("sp", wt[:], w1s[g], r=[("w1s", g)], w=[wt])
            for j in range(4):
                ch = g * 4 + j
                bk_ = k.bank()
                for kk in range(16):
                    k.op("pe", lambda e, kk=kk, bk_=bk_, j=j: e.matmul(bk_[:, :], lhsT=wt[:, kk, j * 128:(j + 1) * 128], rhs=h2Tb[:, kk, :], start=(kk == 0), stop=(kk == 15)),
                         r=[wt, h2Tb], w=[bk_])
                r_ = rl6[ch % 2]
                k.op("act", lambda e, bk_=bk_, r_=r_: e.activation(out=r_[:], in_=bk_[:, :], func=AF.Relu), r=[bk_], w=[r_])
                k.op("dve", lambda e, r_=r_, ch=ch: e.tensor_tensor(out=uT[:, ch, :], in0=r_[:], in1=r_[:], op=ALU.mult), r=[r_], w=[(uT, ch)])
                if with_s:
                    bs_ = k.bank()
                    for kk in range(16):
                        k.op("pe", lambda e, kk=kk, bs_=bs_, j=j: e.matmul(bs_[:, 0:TS], lhsT=wt[:, kk, j * 128:(j + 1) * 128], rhs=h2Ts[:, kk, :], start=(kk == 0), stop=(kk == 15)),
                             r=[wt, h2Ts], w=[bs_])
                    k.op("act", lambda e, bs_=bs_, ch=ch: e.activation(out=uTs[:, ch, :], in_=bs_[:, 0:TS], func=AF.Relu), r=[bs_], w=[(uTs, ch)])
                    k.op("dve", lambda e, ch=ch: e.tensor_tensor(out=uTs[:, ch, :], in0=uTs[:, ch, :], in1=uTs[:, ch, :], op=ALU.mult), r=[(uTs, ch)], w=[(uTs, ch)])
        for g in range(16):
            phaseA(g)
        def phaseB(qc):
            accs = [k.bank() for _ in range(4)]
            accS = k.bank() if with_s else None
            k.reserved = {a_.name for a_ in accs} | ({accS.name} if with_s else set())
            for fgg in range(8):
                w2 = w2b[w2cnt[0] % 2]
                w2cnt[0] += 1
                k.dma("pool", w2[:], w2s[qc, fgg], r=[("w2s", qc, fgg)], w=[w2])
                for c in range(8):
                    ch = fgg * 8 + c
                    first = (fgg == 0 and c == 0)
                    last = (fgg == 7 and c == 7)
                    for tt in range(4):
                        k.op("pe", lambda e, tt=tt, c=c, ch=ch, first=first, last=last, w2=w2: e.matmul(accs[tt][:, :], lhsT=uT[:, ch, tt * 128:(tt + 1) * 128], rhs=w2[:, c, :], start=first, stop=last),
                             r=[(uT, ch), w2], w=[accs[tt]])
                    if with_s:
                        k.op("pe", lambda e, c=c, ch=ch, first=first, last=last, w2=w2: e.matmul(accS[0:TS, :], lhsT=uTs[:, ch, :], rhs=w2[:, c, :], start=first, stop=last),
                             r=[(uTs, ch), w2], w=[accS])
            for tt in range(4):
                k.op("act", lambda e, tt=tt: e.copy(out=fbuf[tt][:, qc * 512:(qc + 1) * 512], in_=accs[tt][:, :]), r=[accs[tt]], w=[(fbuf[tt], qc)])
            if with_s:
                k.op("act", lambda e: e.copy(out=fs[0:TS, qc * 512:(qc + 1) * 512], in_=accS[0:TS, :]), r=[accS], w=[(fs, qc)])
            k.reserved = set()
        for qc in range(4):
            phaseB(qc)
        if b + 1 < NBLK6:
            k.dma("sp", h2Tb[:], h2Td[:, :, (b + 1) * TB:(b + 2) * TB], r=["h2Td"], w=[h2Tb])
        def epi(fb, n, x1src, ydst, G2_, i):
            x_ = x1t[i % 2]
            s_ = st6[i % 2]
            k.dma("pool", x_[0:n, :], x1src, r=["x1d"], w=[x_])
            fk = [(fb, q) for q in range(4)]
            k.op("act", lambda e: e.activation(out=w2b[0][:].rearrange("p a b -> p (a b)")[0:n, 0:D], in_=fb[0:n, :], func=AF.Square, accum_out=s_[0:n, 0:1]), r=fk, w=[w2b[0], s_])
            k.op("act", lambda e: e.activation(out=s_[0:n, 1:2], in_=s_[0:n, 0:1], func=AF.Sqrt, scale=1.0 / D, bias=EPS), r=[s_], w=[s_])
            k.op("dve", lambda e: e.reciprocal(out=s_[0:n, 2:3], in_=s_[0:n, 1:2]), r=[s_], w=[s_])
            k.op("dve", lambda e: e.scalar_tensor_tensor(out=fb[0:n, :], in0=fb[0:n, :], scalar=s_[0:n, 2:3], op0=ALU.mult, in1=G2_[0:n, :], op1=ALU.mult), r=fk + [s_, G2_], w=fk)
            k.op("dve", lambda e: e.tensor_tensor(out=x_[0:n, :], in0=x_[0:n, :], in1=fb[0:n, :], op=ALU.add), r=[x_] + fk, w=[x_])
            k.dma("pool", ydst, x_[0:n, :], r=[x_])
        for tt in range(4):
            r0 = b * TB + tt * 128
            epi(fbuf[tt], 128, x1d[r0:r0 + 128, :], y_p[r0:r0 + 128, :], G2b, tt)
        if with_s:
            k.dma("pool", G2b[0:TS, :], modd[1:5, 5 * D:6 * D], r=["modd"], w=[G2b])
            epi(fs, TS, x1d[T:TT, :], y_s.ap(), G2b, 0)

    fs = k.alloc("fs", [TS, D], F32)
    import os
    for b in range(NBLK6):
        ffn_block(b)
    k.barrier()
    return k.finish()


_CACHE = {}


def _core_inputs(i, a):
    f = np.ascontiguousarray
    return {
        "xp": f(a["x_prompt"][i]),
        "xs": f(a["x_sample"][4 * i:4 * i + 4, 0, :]),
        "c5": f(np.concatenate([a["c_prompt"][i:i + 1], a["c_sample"][4 * i:4 * i + 4]], axis=0)),
        "w_ada": f(a["w_ada"][0]),
        "b_ada": f(a["b_ada"][0][None, :]),
        "gvec": f(np.stack([a["pre1_g"][0], a["post1_g"][0], a["pre2_g"][0], a["post2_g"][0]])),
        "w_in": f(a["w_in"][0]),
        "w_out": f(a["w_out"][0]),
        "w_ff1": f(a["w_ff1"][0]),
        "w_ff2": f(a["w_ff2"][0]),
        "conv_w": f(a["conv_w"][0]),
        "st_conv": f(a["state_conv"][0, 4 * i:4 * i + 4].reshape(12, 3072)),
        "hv": f(np.concatenate([a["a_log"][0], a["dt_bias"][0]])[None, :]),
        "ln_gb": f(np.stack([a["idx_knorm_g"][0], a["idx_knorm_b"][0]])),
        "gdn_g": f(a["gdn_norm_g"][0][None, :]),
        "st_ssm": f(a["state_ssm"][0, 4 * i:4 * i + 4]),
        "rel_bias": f(a["rel_bias"]),
        "boh": _boh(),
        "bthr": _bthr(),
        "page_table": f(a["page_table"][4 * i:4 * i + 4]),
        "cache_kidx": a["cache_kidx"][0].reshape(-1, PAGE * 128),
        "cache_k": a["cache_k"][0].reshape(-1, 256),
        "cache_v": a["cache_v"][0].reshape(-1, 256),
    }


def kernel(**inputs):
    n = 8
    nc = build(n_pool=int(inputs["cache_k"].shape[1]))
    in_maps = [_core_inputs(i, inputs) for i in range(n)]
    res = run_bass_kernel_spmd(nc, in_maps, core_ids=list(range(n)))
    R = res.results
    cat = lambda name: np.stack([r[name] for r in R])
    y_p = cat("y_p")
    y_s = np.concatenate([r["y_s"] for r in R])[:, None, :]
    k_p = cat("k_p").reshape(1, 8, T, 2, 128)
    v_p = cat("v_p").reshape(1, 8, T, 2, 128)
    ki_p = cat("ki_p")[None]
    ssm_p = cat("ssm_p")[None]
    conv_p = cat("conv_p")[None]
    k_s = np.concatenate([r["k_s"] for r in R]).reshape(1, 32, 1, 2, 128)
    v_s = np.concatenate([r["v_s"] for r in R]).reshape(1, 32, 1, 2, 128)
    ki_s = np.concatenate([r["ki_s"] for r in R]).reshape(1, 32, 1, 128)
    ssm_s = np.concatenate([r["ssm_s"] for r in R])[None]
    conv_s = np.concatenate([r["conv_s"] for r in R])[None]
    return (y_p, y_s, k_p, v_p, ki_p, ssm_p, conv_p, k_s, v_s, ki_s, ssm_s, conv_s)
```

```python
import math
import numpy as np
import concourse.bass as bass
import concourse.mybir as mybir
from concourse.bass_utils import run_bass_kernel_spmd

F32 = mybir.dt.float32
BF16 = mybir.dt.bfloat16
I32 = mybir.dt.int32
ALU = mybir.AluOpType
AF = mybir.ActivationFunctionType
AX = mybir.AxisListType

ENGS = ("pe", "dve", "act", "pool", "sp")

D = 2048
T = 2048
TS = 4
TT = T + TS
NT = 16
NPROJ = 7840
DFF = 8192
EPS = 1e-6
NPAGES = 128
PAGE = 128
O_CONV, O_A, O_B, O_Z, O_QB, O_KB, O_VB, O_QI, O_WI, O_KI = 0, 3072, 3080, 3088, 4112, 5136, 5392, 5648, 7696, 7712


def _dsize(dt):
    return {F32: 4, BF16: 2, I32: 4}[dt]


class Buf:
    def __init__(self, name, ap):
        self.name = name
        self.ap = ap

    def __getitem__(self, key):
        return self.ap[key]


class KB:
    def __init__(self, n_dma_sems=(24, 8, 52)):
        self.nc = bass.Bass("TRN2", target_bir_lowering=False)
        nc = self.nc
        self.ops = {e: [] for e in ENGS}
        self._ctx = []
        self.psem = {}
        self.cnt = {e: 0 for e in ENGS}
        for e in ENGS:
            self.psem[e] = self._enter(nc.semaphore("p_" + e))
        self.dsem = {}
        self.dcnt = {}
        self.drr = {}
        for q, n in zip(("sp", "act", "pool"), n_dma_sems):
            self.dsem[q] = [self._enter(nc.semaphore(f"d_{q}{i}")) for i in range(n)]
            self.dcnt[q] = [0] * n
            self.drr[q] = 0
        self.known = {e: {} for e in ENGS}
        self.state = {}
        self.semobj = {}
        for e in ENGS:
            self.semobj[("p", e)] = self.psem[e]
        for q in self.dsem:
            for i, s in enumerate(self.dsem[q]):
                self.semobj[("d", q, i)] = s
        self.n_ops = 0
        self.arena = None
        self.aoff = 0
        self.awords = 0
        self.nbank = 0
        self.reserved = set()

    def _enter(self, cm):
        v = cm.__enter__()
        self._ctx.append(cm)
        return v

    def init_arena(self, words):
        self.arena = self._enter(self.nc.sbuf_tensor("arena", [128, words], F32))
        self.awords = words
        self.aoff = 0

    def alloc(self, name, shape, dt=F32):
        p = shape[0]
        n = int(np.prod(shape[1:]))
        words = (n * _dsize(dt) + 3) // 4
        words = (words + 7) // 8 * 8
        assert self.aoff + words <= self.awords, f"arena overflow at {name}: {self.aoff + words} > {self.awords}"
        ap = self.arena[0:p, self.aoff:self.aoff + words]
        self.aoff += words
        if dt != F32:
            ap = ap.bitcast(dt)
        ap = ap[:, 0:n]
        if len(shape) == 3:
            ap = ap.rearrange("p (a b) -> p a b", a=shape[1])
        elif len(shape) == 4:
            ap = ap.rearrange("p (a b c) -> p a b c", a=shape[1], b=shape[2])
        return Buf(name, ap)

    def mark(self):
        return self.aoff

    def release(self, m):
        self.aoff = m

    def psum_init(self):
        self.pbanks = []
        for i in range(4):
            t = self._enter(self.nc.psum_tensor(f"pp{i}", [128, 1024], F32))
            self.pbanks.append(Buf(f"bank{2 * i}", t[:, 0:512]))
            self.pbanks.append(Buf(f"bank{2 * i + 1}", t[:, 512:1024]))
        self.pdbl = [self._dbl(i) for i in range(4)]

    def _dbl(self, i):
        return None

    def bank(self):
        while True:
            b = self.pbanks[self.nbank % 8]
            self.nbank += 1
            if b.name not in self.reserved:
                return b

    @staticmethod
    def _key(x):
        if isinstance(x, tuple):
            return (KB._key(x[0]),) + tuple(x[1:])
        if isinstance(x, str):
            return x
        return x.name

    def _collect(self, eng, r, w):
        need = {}
        own = ("p", eng)

        def add(tok):
            if tok is None:
                return
            sk, v = tok
            if sk == own and eng == "pe":
                return
            if need.get(sk, 0) < v:
                need[sk] = v

        for x in r:
            st = self.state.get(self._key(x))
            if st:
                add(st[0])
        for x in w:
            st = self.state.get(self._key(x))
            if st:
                add(st[0])
                for t in st[1]:
                    add(t)
        waits = []
        kn = self.known[eng]
        for sk, v in need.items():
            if kn.get(sk, 0) >= v:
                continue
            kn[sk] = v
            waits.append((sk, v))
        return waits

    def _update(self, tok, r, w):
        for x in w:
            self.state[self._key(x)] = [tok, []]
        for x in r:
            kk = self._key(x)
            st = self.state.get(kk)
            if st is None:
                st = self.state[kk] = [None, []]
            st[1].append(tok)
            if len(st[1]) > 24:
                best = {}
                for sk, v in st[1]:
                    if best.get(sk, 0) < v:
                        best[sk] = v
                st[1] = list(best.items())

    def op(self, eng, fn, r=(), w=()):
        waits = self._collect(eng, r, w)
        self.cnt[eng] += 1
        tok = (("p", eng), self.cnt[eng])
        self.ops[eng].append((waits, fn, (("p", eng), 1)))
        self._update(tok, r, w)
        self.n_ops += 1
        return tok

    def dma(self, q, out, in_, r=(), w=(), slow=False):
        if slow:
            fn = lambda e: e.dma_start(out=out, in_=in_, allow_slow_non_contiguous=True)
        else:
            fn = lambda e: e.dma_start(out=out, in_=in_)
        i = self.drr[q]
        self.drr[q] = (i + 1) % len(self.dsem[q])
        sk = ("d", q, i)
        waits = self._collect(q, r, w)
        prev = self.dcnt[q][i]
        kn = self.known[q]
        if prev > 0 and kn.get(sk, 0) < prev:
            kn[sk] = prev
            waits.append((sk, prev))
        self.dcnt[q][i] = prev + 16
        tok = (sk, prev + 16)
        self.ops[q].append((waits, fn, (sk, 16)))
        self._update(tok, r, w)
        self.n_ops += 1
        return tok

    def barrier(self):
        toks = [(("p", e), self.cnt[e]) for e in ENGS if self.cnt[e] > 0]
        for q in self.dsem:
            for i, c in enumerate(self.dcnt[q]):
                if c > 0:
                    toks.append((("d", q, i), c))
        for e in ENGS:
            waits = []
            kn = self.known[e]
            for sk, v in toks:
                if sk == ("p", e):
                    continue
                if kn.get(sk, 0) < v:
                    kn[sk] = v
                    waits.append((sk, v))
            if waits:
                self.ops[e].append((waits, None, None))

    def check_deadlock(self):
        sem = {}
        ptr = {e: 0 for e in ENGS}
        progress = True
        while progress:
            progress = False
            for e in ENGS:
                lst = self.ops[e]
                while ptr[e] < len(lst):
                    waits, fn, inc = lst[ptr[e]]
                    if all(sem.get(sk, 0) >= v for sk, v in waits):
                        if inc is not None:
                            sem[inc[0]] = sem.get(inc[0], 0) + inc[1]
                        ptr[e] += 1
                        progress = True
                    else:
                        break
        stuck = {e: (ptr[e], len(self.ops[e])) for e in ENGS if ptr[e] < len(self.ops[e])}
        if stuck:
            for e in stuck:
                waits, fn, inc = self.ops[e][ptr[e]]
                print("STUCK", e, ptr[e], [(sk, v, sem.get(sk, 0)) for sk, v in waits])
            raise RuntimeError(f"deadlock in sync graph: {stuck}")

    def finish(self):
        self.barrier()
        self.check_deadlock()
        nc = self.nc
        ops = self.ops
        semobj = self.semobj

        def run(e, lst):
            for waits, fn, inc in lst:
                for sk, v in waits:
                    e.wait_ge(semobj[sk], v)
                if fn is not None:
                    ins = fn(e)
                    ins.then_inc(semobj[inc[0]], inc[1])

        with nc.Block() as block:
            @block.tensor
            def _(e):
                run(e, ops["pe"])

            @block.vector
            def _(e):
                run(e, ops["dve"])

            @block.scalar
            def _(e):
                run(e, ops["act"])

            @block.gpsimd
            def _(e):
                run(e, ops["pool"])

            @block.sync
            def _(e):
                run(e, ops["sp"])

        for cm in reversed(self._ctx):
            cm.__exit__(None, None, None)
        self._ctx = []
        return nc


def _t5_bucket_table():
    n = np.arange(0, 256, dtype=np.int32)
    max_exact = 16
    nf = np.maximum(n, 1).astype(np.float32)
    large = max_exact + (np.log(nf / np.float32(max_exact)) / np.float32(math.log(128 / max_exact))
                         * np.float32(32 - max_exact)).astype(np.int32)
    large = np.minimum(large, 31)
    return np.where(n < max_exact, n, large)


def _boh():
    tab = _t5_bucket_table()
    oh = np.zeros((32, 384), np.float32)
    for j in range(384):
        dist = min(max(j - 127, 0), 255)
        oh[tab[dist], j] = 1.0
    return oh


def _bthr():
    tab = _t5_bucket_table()
    thr = np.zeros((1, 31), np.float32)
    for kk in range(1, 32):
        nz = np.nonzero(tab >= kk)[0]
        thr[0, kk - 1] = float(nz[0]) if len(nz) else 1e9
    return thr


def build(stop_after=None, n_pool=5120, skip_p4s=False):
    k = KB()
    nc = k.nc

    def din(name, shape, dt=F32):
        return nc.dram_tensor(name, list(shape), dt, kind="ExternalInput")

    def dout(name, shape, dt=F32):
        return nc.dram_tensor(name, list(shape), dt, kind="ExternalOutput")

    xp = din("xp", [T, D])
    xs = din("xs", [TS, D])
    c5 = din("c5", [5, D])
    w_ada = din("w_ada", [D, 6 * D])
    b_ada = din("b_ada", [1, 6 * D])
    gvec = din("gvec", [4, D])
    w_in = din("w_in", [D, NPROJ])
    w_out = din("w_out", [D, D])
    w_ff1 = din("w_ff1", [D, DFF])
    w_ff2 = din("w_ff2", [DFF, D])
    conv_w = din("conv_w", [4, 3072])
    st_conv = din("st_conv", [TS * 3, 3072])
    hv = din("hv", [1, 16])
    ln_gb = din("ln_gb", [2, 128])
    gdn_g = din("gdn_g", [1, 128])
    st_ssm = din("st_ssm", [TS, 8, 128, 128])
    rel_bias = din("rel_bias", [32, 8])
    boh = din("boh", [32, 384])
    bthr = din("bthr", [1, 31])
    page_table = din("page_table", [TS, NPAGES], I32)
    cache_kidx = din("cache_kidx", [n_pool, PAGE * 128])
    cache_k = din("cache_k", [n_pool * PAGE, 256])
    cache_v = din("cache_v", [n_pool * PAGE, 256])

    y_p = dout("y_p", [T, D])
    y_s = dout("y_s", [TS, D])
    k_p = dout("k_p", [T, 256])
    v_p = dout("v_p", [T, 256])
    ki_p = dout("ki_p", [T, 128])
    ssm_p = dout("ssm_p", [8, 128, 128])
    conv_p = dout("conv_p", [3, 3072])
    k_s = dout("k_s", [TS, 256])
    v_s = dout("v_s", [TS, 256])
    ki_s = dout("ki_s", [TS, 128])
    ssm_s = dout("ssm_s", [TS, 8, 128, 128])
    conv_s = dout("conv_s", [TS, 3, 3072])

    modd = nc.dram_tensor("modd", [5, 6 * D], F32)
    gq = nc.dram_tensor("gq", [8, 128, TT], BF16)
    gk = nc.dram_tensor("gk", [8, 128, TT], BF16)
    gv = nc.dram_tensor("gv", [8, 128, TT], BF16)
    gz = nc.dram_tensor("gz", [TT, 1024], BF16)
    gab = nc.dram_tensor("gab", [TT, 16], F32)
    aq = nc.dram_tensor("aq", [8, 128, TT], BF16)
    akT = nc.dram_tensor("akT", [2, 128, TT], BF16)
    av = nc.dram_tensor("av", [TT, 256], BF16)
    iq = nc.dram_tensor("iq", [16, 128, TT], BF16)
    iw = nc.dram_tensor("iw", [TT, 16], F32)
    ikTd = nc.dram_tensor("ikTd", [128, TT], BF16)
    biasd = nc.dram_tensor("biasd", [8, 384], F32)
    rbTd = nc.dram_tensor("rbTd", [8, 32], F32)
    w1s = nc.dram_tensor("w1s", [16, 128, 16, 512], BF16)
    w2s = nc.dram_tensor("w2s", [4, 8, 128, 8, 512], BF16)
    x1d = nc.dram_tensor("x1d", [TT, D], F32)
    h2Td = nc.dram_tensor("h2Td", [128, 16, TT], BF16)

    k.init_arena(47 * 1024)
    k.psum_init()

    ident_f = k.alloc("ident_f", [128, 128], F32)
    ident_b = k.alloc("ident_b", [128, 128], BF16)
    ones_b = k.alloc("ones_b", [128, 128], BF16)
    k.op("pool", lambda e: e.memset(ident_f[:], 1.0), w=[ident_f])
    k.op("pool", lambda e: e.affine_select(out=ident_f[:], in_=ident_f[:], pattern=[[-1, 128]],
                                           compare_op=ALU.is_equal, fill=0.0, base=0, channel_multiplier=1),
         r=[ident_f], w=[ident_f])
    k.op("pool", lambda e: e.tensor_copy(out=ident_b[:], in_=ident_f[:]), r=[ident_f], w=[ident_b])
    k.op("pool", lambda e: e.memset(ones_b[:], 1.0), w=[ones_b])

    wb = []
    wcnt = [0]

    def alloc_wb():
        wb.clear()
        wb.extend(k.alloc(f"wb{i}", [128, 16, 512], BF16) for i in range(2))

    def load_w(src_dram, c0, ncols):
        b = wb[wcnt[0] % 2]
        wcnt[0] += 1
        k.dma("pool", b[:, :, 0:ncols], src_dram[:, c0:c0 + ncols].rearrange("(k p) n -> p k n", p=128), w=[b])
        return b

    m0 = k.mark()
    alloc_wb()
    c80 = k.alloc("c80", [80, 128], F32)
    cT = k.alloc("cT", [128, 16, 5], BF16)
    mod = k.alloc("mod", [5, 6 * D], F32)
    gv5 = k.alloc("gv5", [5, 4, D], F32)
    k.dma("sp", c80[:], c5.ap().rearrange("r (k p) -> (r k) p", p=128), w=[c80])
    k.dma("sp", mod[:], b_ada.ap().to_broadcast([5, 6 * D]), w=[mod])
    k.dma("sp", gv5[:].rearrange("p a b -> p (a b)"), gvec.ap().rearrange("a b -> (a b)").unsqueeze(0).to_broadcast([5, 4 * D]), w=[gv5])
    bk = k.bank()
    k.op("pe", lambda e: e.transpose(out=bk[:, 0:80], in_=c80[:], identity=ident_f[0:80, 0:80]), r=[c80, ident_f], w=[bk])
    k.op("act", lambda e: e.activation(out=cT[:], in_=bk[:, 0:80].rearrange("p (r k) -> p k r", r=5), func=AF.Silu), r=[bk], w=[cT])
    for n in range(24):
        wt = load_w(w_ada, n * 512, 512)
        bk = k.bank()
        for kk in range(16):
            k.op("pe", lambda e, kk=kk, wt=wt, bk=bk: e.matmul(bk[0:5, :], lhsT=cT[:, kk, :], rhs=wt[:, kk, :], start=(kk == 0), stop=(kk == 15)),
                 r=[cT, wt], w=[bk])
        k.op("dve", lambda e, n=n, bk=bk: e.tensor_tensor(out=mod[:, n * 512:(n + 1) * 512], in0=bk[0:5, :], in1=mod[:, n * 512:(n + 1) * 512], op=ALU.add),
             r=[bk, mod], w=[mod])
    for (sc, gi) in ((1, 0), (4, 2)):
        k.op("dve", lambda e, sc=sc, gi=gi: e.scalar_tensor_tensor(out=mod[:, sc * D:(sc + 1) * D], in0=mod[:, sc * D:(sc + 1) * D], scalar=1.0, op0=ALU.add,
                                                                    in1=gv5[:, gi, :], op1=ALU.mult), r=[mod, gv5], w=[mod])
    for (g, gi) in ((2, 1), (5, 3)):
        k.op("dve", lambda e, g=g, gi=gi: e.tensor_tensor(out=mod[:, g * D:(g + 1) * D], in0=mod[:, g * D:(g + 1) * D], in1=gv5[:, gi, :], op=ALU.mult),
             r=[mod, gv5], w=[mod])
    k.dma("sp", modd.ap(), mod[:], r=[mod], w=["modd"])
    k.barrier()
    k.release(m0)
    if stop_after == "P0":
        return k.finish()

    def load_mod_bcast(buf, idx):
        k.dma("sp", buf[:], modd[0:1, idx * D:(idx + 1) * D].to_broadcast([128, D]), r=["modd"], w=[buf])

    def load_mod_rows(buf, idx):
        k.dma("sp", buf[:], modd[1:5, idx * D:(idx + 1) * D], r=["modd"], w=[buf])

    mP = k.mark()
    hT = k.alloc("hT", [128, 16, TT], BF16)
    ikT = k.alloc("ikT", [128, TT], BF16)
    m1 = k.mark()
    A1 = k.alloc("A1", [128, D], F32)
    SH1 = k.alloc("SH1", [128, D], F32)
    A1s = k.alloc("A1s", [TS, D], F32)
    SH1s = k.alloc("SH1s", [TS, D], F32)
    load_mod_bcast(SH1, 0)
    load_mod_bcast(A1, 1)
    load_mod_rows(SH1s, 0)
    load_mod_rows(A1s, 1)
    xt = [k.alloc(f"xt{i}", [128, D], F32) for i in range(2)]
    hb = [k.alloc(f"hb{i}", [128, D], BF16) for i in range(2)]
    junk = k.alloc("junk", [128, D], BF16)
    st1 = [k.alloc(f"st1_{i}", [128, 4], F32) for i in range(2)]

    def norm_mod(i, np_, src_ap, A, SH, col0, ncol):
        x_ = xt[i % 2]
        h_ = hb[i % 2]
        s_ = st1[i % 2]
        k.dma("sp", x_[0:np_, :], src_ap, w=[x_])
        k.op("act", lambda e: e.activation(out=junk[0:np_, :], in_=x_[0:np_, :], func=AF.Square, accum_out=s_[0:np_, 0:1]), r=[x_], w=[junk, s_])
        k.op("act", lambda e: e.activation(out=s_[0:np_, 1:2], in_=s_[0:np_, 0:1], func=AF.Sqrt, scale=1.0 / D, bias=EPS), r=[s_], w=[s_])
        k.op("dve", lambda e: e.reciprocal(out=s_[0:np_, 2:3], in_=s_[0:np_, 1:2]), r=[s_], w=[s_])
        k.op("dve", lambda e: e.scalar_tensor_tensor(out=x_[0:np_, :], in0=x_[0:np_, :], scalar=s_[0:np_, 2:3], op0=ALU.mult, in1=A[0:np_, :], op1=ALU.mult),
             r=[x_, s_, A], w=[x_])
        k.op("pool", lambda e: e.tensor_tensor(out=h_[0:np_, :], in0=x_[0:np_, :], in1=SH[0:np_, :], op=ALU.add), r=[x_, SH], w=[h_])
        b0 = k.bank()
        b1 = k.bank()
        for kk in range(16):
            bb = b0 if kk < 8 else b1
            k.op("pe", lambda e, kk=kk, bb=bb: e.transpose(out=bb[:].bitcast(BF16)[:, (kk % 8) * 128:(kk % 8) * 128 + np_],
                                                           in_=h_[0:np_, kk * 128:(kk + 1) * 128], identity=ident_b[0:np_, 0:np_]),
                 r=[h_, ident_b], w=[bb])
        for half, bb in ((0, b0), (1, b1)):
            k.op("act", lambda e, half=half, bb=bb: e.copy(out=hT[:, half * 8:(half + 1) * 8, col0:col0 + ncol],
                                                          in_=bb[:].bitcast(BF16).rearrange("p (a b) -> p a b", a=8)[:, :, 0:ncol]),
                 r=[bb], w=[(hT, col0)])

    for i in range(NT):
        norm_mod(i, 128, xp[i * 128:(i + 1) * 128, :], A1, SH1, i * 128, 128)
    norm_mod(NT, TS, xs.ap(), A1s, SH1s, T, TS)
    k.barrier()
    k.release(m1)
    if stop_after == "P1":
        dbg = dout("dbg_hT", [128, 16 * TT], BF16)
        k.dma("sp", dbg.ap(), hT[:].rearrange("p a b -> p (a b)"), r=[hT])
        return k.finish()

    hT_keys = [(hT, i * 128) for i in range(NT)] + [(hT, T)]

    m2 = k.mark()
    alloc_wb()
    cw = k.alloc("cw", [128, 4, 24], F32)
    stc = k.alloc("stc", [128, TS * 3, 24], F32)
    cwl = k.alloc("cwl", [96, 128], F32)
    stl = k.alloc("stl", [96, 3, 128], F32)
    k.dma("sp", cwl[:], conv_w.ap().rearrange("j (ch c) -> (j ch) c", c=128), w=[cwl])
    k.dma("sp", stl[:], st_conv.ap().rearrange("r (ch c) -> (r ch) c", c=128).rearrange("(g p) c -> p g c", p=96), w=[stl])
    bk = k.bank()
    k.op("pe", lambda e, bk=bk: e.transpose(out=bk[:, 0:96], in_=cwl[:], identity=ident_f[0:96, 0:96]), r=[cwl, ident_f], w=[bk])
    k.op("act", lambda e, bk=bk: e.copy(out=cw[:].rearrange("p a b -> p (a b)"), in_=bk[:, 0:96]), r=[bk], w=[cw])
    bk = k.bank()
    for g in range(3):
        k.op("pe", lambda e, g=g, bk=bk: e.transpose(out=bk[:, g * 96:(g + 1) * 96], in_=stl[:, g, :], identity=ident_f[0:96, 0:96]),
             r=[stl, ident_f], w=[bk])
    k.op("act", lambda e, bk=bk: e.copy(out=stc[:].rearrange("p a b -> p (a b)"), in_=bk[:, 0:288]), r=[bk], w=[stc])
    k.dma("sp", conv_s.ap()[:, 0:2, :], st_conv.ap().rearrange("(s j) c -> s j c", j=3)[:, 1:3, :])

    cin = [k.alloc(f"cin{i}", [128, 3 + T], F32) for i in range(2)]
    for cb in cin:
        k.op("pool", lambda e, cb=cb: e.memset(cb[:, 0:3], 0.0), w=[cb])
    acc = [k.alloc(f"acc{i}", [128, TT], F32) for i in range(2)]
    cins = [k.alloc(f"cins{i}", [128, TS, 4], F32) for i in range(2)]
    tmp4 = [k.alloc(f"tmp4{i}", [128, TS, 4], F32) for i in range(2)]
    sqb = [k.alloc(f"sqb{i}", [128, TT], BF16) for i in range(2)]
    rsb = [k.alloc(f"rsb{i}", [128, TT], F32) for i in range(2)]
    ob = [k.alloc(f"ob{i}", [128, TT], BF16) for i in range(2)]
    obc = [0]

    def fm_matmuls(wt, wc0, tg, bk):
        c0, n = (tg * 512, 512) if tg < 4 else (T, TS)
        keys = hT_keys[tg * 4:(tg + 1) * 4] if tg < 4 else [hT_keys[16]]
        for kk in range(16):
            k.op("pe", lambda e, kk=kk: e.matmul(bk[:, 0:n], lhsT=wt[:, kk, wc0:wc0 + 128], rhs=hT[:, kk, c0:c0 + n], start=(kk == 0), stop=(kk == 15)),
                 r=[wt] + keys, w=[bk])

    def next_ob():
        b = ob[obc[0] % 2]
        obc[0] += 1
        return b

    chunk_i = [0]

    def conv_chunk(wt, wc0, ch):
        ci = chunk_i[0]
        chunk_i[0] += 1
        cb = cin[ci % 2]
        ac = acc[ci % 2]
        cs = cins[ci % 2]
        t4 = tmp4[ci % 2]
        for tg in range(4):
            bk = k.bank()
            fm_matmuls(wt, wc0, tg, bk)
            k.op("act", lambda e, bk=bk, tg=tg: e.copy(out=cb[:, 3 + tg * 512:3 + (tg + 1) * 512], in_=bk[:, 0:512]), r=[bk], w=[cb])
        bk = k.bank()
        fm_matmuls(wt, wc0, 4, bk)
        k.op("dve", lambda e: e.tensor_copy(out=cs[:, :, 0:3], in_=stc[:, :, ch].rearrange("p (s j) -> p s j", j=3)), r=[stc], w=[cs])
        k.op("dve", lambda e, bk=bk: e.tensor_copy(out=cs[:, :, 3:4], in_=bk[:, 0:TS].unsqueeze(2)), r=[bk, cs], w=[cs])
        k.dma("sp", conv_p.ap()[:, ch * 128:(ch + 1) * 128].rearrange("r c -> c r"), cb[:, T:T + 3], r=[cb], slow=True)
        k.dma("sp", conv_s.ap()[:, 2, ch * 128:(ch + 1) * 128].rearrange("s c -> c s"), cs[:, :, 3], r=[cs], slow=True)
        k.op("dve", lambda e: e.tensor_scalar(out=ac[:, 0:T], in0=cb[:, 0:T], scalar1=cw[:, 0, ch:ch + 1], scalar2=None, op0=ALU.mult), r=[cb, cw], w=[ac])
        for j in range(1, 4):
            k.op("dve", lambda e, j=j: e.scalar_tensor_tensor(out=ac[:, 0:T], in0=cb[:, j:j + T], scalar=cw[:, j, ch:ch + 1], op0=ALU.mult, in1=ac[:, 0:T], op1=ALU.add),
                 r=[cb, cw, ac], w=[ac])
        k.op("dve", lambda e: e.tensor_tensor(out=t4[:], in0=cs[:], in1=cw[:, :, ch].unsqueeze(1).to_broadcast([128, TS, 4]), op=ALU.mult), r=[cs, cw], w=[t4])
        k.op("dve", lambda e: e.tensor_reduce(out=ac[:, T:TT], in_=t4[:], axis=AX.X, op=ALU.add), r=[t4, ac], w=[ac])
        o = next_ob()
        if ch >= 16:
            k.op("act", lambda e: e.activation(out=o[:], in_=ac[:], func=AF.Silu), r=[ac], w=[o])
            k.dma("sp", gv[ch - 16], o[:], r=[o], w=["gv"])
            return
        sq = sqb[ci % 2]
        rs = rsb[ci % 2]
        k.op("act", lambda e: e.activation(out=ac[:], in_=ac[:], func=AF.Silu), r=[ac], w=[ac])
        k.op("act", lambda e: e.activation(out=sq[:], in_=ac[:], func=AF.Square), r=[ac], w=[sq])
        for tg in range(5):
            c0, n = (tg * 512, 512) if tg < 4 else (T, TS)
            bk = k.bank()
            k.op("pe", lambda e, bk=bk, c0=c0, n=n: e.matmul(bk[:, 0:n], lhsT=ones_b[:], rhs=sq[:, c0:c0 + n], start=True, stop=True), r=[sq, ones_b], w=[bk])
            k.op("act", lambda e, bk=bk, c0=c0, n=n: e.activation(out=rs[:, c0:c0 + n], in_=bk[:, 0:n], func=AF.Sqrt, bias=EPS, scale=1.0), r=[bk], w=[rs])
        k.op("dve", lambda e: e.reciprocal(out=rs[:], in_=rs[:]), r=[rs], w=[rs])
        scl = 128.0 ** -0.5 if ch < 8 else 1.0
        k.op("dve", lambda e: e.scalar_tensor_tensor(out=o[:], in0=ac[:], scalar=scl, op0=ALU.mult, in1=rs[:], op1=ALU.mult), r=[ac, rs], w=[o])
        dst = gq[ch] if ch < 8 else gk[ch - 8]
        k.dma("sp", dst, o[:], r=[o], w=["gqk"])

    def plain_chunk(wt, wc0, dst, scale):
        o = next_ob()
        for tg in range(5):
            c0, n = (tg * 512, 512) if tg < 4 else (T, TS)
            bk = k.bank()
            fm_matmuls(wt, wc0, tg, bk)
            k.op("act", lambda e, bk=bk, c0=c0, n=n: e.activation(out=o[:, c0:c0 + n], in_=bk[:, 0:n], func=AF.Copy, scale=scale), r=[bk], w=[o])
        k.dma("sp", dst, o[:], r=[o], w=["plain"])

    for g in range(6):
        wt = load_w(w_in, O_CONV + g * 512, 512)
        for j in range(4):
            conv_chunk(wt, j * 128, g * 4 + j)
    if stop_after == "P2a":
        k.barrier()
        return k.finish()
    for g in range(2):
        wt = load_w(w_in, O_QB + g * 512, 512)
        for j in range(4):
            plain_chunk(wt, j * 128, aq[g * 4 + j], 128.0 ** -0.5)
    for g in range(4):
        wt = load_w(w_in, O_QI + g * 512, 512)
        for j in range(4):
            plain_chunk(wt, j * 128, iq[g * 4 + j], 1.0)
    wt_kv = load_w(w_in, O_KB, 512)
    for j in range(2):
        plain_chunk(wt_kv, j * 128, akT[j], 1.0)

    if stop_after == "P2b":
        k.barrier()
        return k.finish()
    stg_f = [k.alloc(f"stgf{i}", [128, 512], F32) for i in range(2)]
    stg_b = [k.alloc(f"stgb{i}", [128, 512], BF16) for i in range(2)]
    lnw = [k.alloc(f"lnw{i}", [128, 8], F32) for i in range(2)]
    kib = [k.alloc(f"kib{i}", [128, 128], BF16) for i in range(2)]
    lng = k.alloc("lng", [128, 128], F32)
    lnb = k.alloc("lnb", [128, 128], F32)
    k.dma("sp", lng[:], ln_gb[0:1, :].to_broadcast([128, 128]), w=[lng])
    k.dma("sp", lnb[:], ln_gb[1:2, :].to_broadcast([128, 128]), w=[lnb])
    tcnt = [0]

    def tm_tile(wt, ncols, ti, epilogue):
        c0, n = (ti * 128, 128) if ti < NT else (T, TS)
        bk = k.bank()
        for kk in range(16):
            k.op("pe", lambda e, kk=kk: e.matmul(bk[0:n, 0:ncols], lhsT=hT[:, kk, c0:c0 + n], rhs=wt[:, kk, 0:ncols], start=(kk == 0), stop=(kk == 15)),
                 r=[wt, hT_keys[ti]], w=[bk])
        i = tcnt[0]
        tcnt[0] += 1
        epilogue(bk, c0, n, i)

    def ep_kv(bk, c0, n, i):
        sf = stg_f[i % 2]
        sb_ = stg_b[i % 2]
        import os
        dbg = int(os.environ.get("DBG", "0"))
        k.op("act", lambda e: e.copy(out=sf[0:n, :], in_=bk[0:n, :]), r=[bk], w=[sf])
        if dbg != 3:
            k.op("pool", lambda e: e.tensor_copy(out=sb_[0:n, 0:256], in_=sf[0:n, 256:512]), r=[sf], w=[sb_])
        if dbg == 1:
            pass
        elif c0 < T:
            k.dma("sp", k_p[c0:c0 + n, :], sf[0:n, 0:256], r=[sf])
            k.dma("sp", v_p[c0:c0 + n, :], sf[0:n, 256:512], r=[sf])
        else:
            k.dma("sp", k_s.ap(), sf[0:n, 0:256], r=[sf])
            k.dma("sp", v_s.ap(), sf[0:n, 256:512], r=[sf])
        if dbg not in (2, 3):
            k.dma("sp", av[c0:c0 + n, :], sb_[0:n, 0:256], r=[sb_], w=["av"])

    import os
    _d = int(os.environ.get("DBG", "0"))
    for ti in range(0 if _d == 4 else (NT if _d == 5 else NT + 1)):
        tm_tile(wt_kv, 512, ti, ep_kv)

    if stop_after == "P2c":
        k.barrier()
        return k.finish()

    def ep_z(half):
        def ep(bk, c0, n, i):
            sb_ = stg_b[i % 2]
            k.op("act", lambda e: e.copy(out=sb_[0:n, :], in_=bk[0:n, :]), r=[bk], w=[sb_])
            k.dma("sp", gz[c0:c0 + n, half * 512:(half + 1) * 512], sb_[0:n, :], r=[sb_], w=["gz"])
        return ep

    for half in range(2):
        wt = load_w(w_in, O_Z + half * 512, 512)
        for ti in range(NT + 1):
            tm_tile(wt, 512, ti, ep_z(half))

    if stop_after == "P2d":
        k.barrier()
        return k.finish()

    def ep_ab(bk, c0, n, i):
        sf = stg_f[i % 2]
        k.op("act", lambda e: e.copy(out=sf[0:n, 0:16], in_=bk[0:n, 0:16]), r=[bk], w=[sf])
        k.dma("sp", gab[c0:c0 + n, :], sf[0:n, 0:16], r=[sf], w=["gab"])

    wt = load_w(w_in, O_A, 16)
    for ti in range(NT + 1):
        tm_tile(wt, 16, ti, ep_ab)

    if stop_after == "P2e":
        k.barrier()
        return k.finish()

    def ep_wk(bk, c0, n, i):
        sf = stg_f[i % 2]
        s_ = lnw[i % 2]
        kb_ = kib[i % 2]
        k.op("act", lambda e: e.activation(out=sf[0:n, 0:16], in_=bk[0:n, 0:16], func=AF.Copy, scale=0.25), r=[bk], w=[sf])
        k.dma("sp", iw[c0:c0 + n, :], sf[0:n, 0:16], r=[sf], w=["iw"])
        kf = sf[0:n, 128:256]
        k.op("act", lambda e: e.activation(out=kf, in_=bk[0:n, 16:144], func=AF.Copy, accum_out=s_[0:n, 0:1]), r=[bk, sf], w=[sf, s_])
        k.op("dve", lambda e: e.tensor_scalar(out=s_[0:n, 1:2], in0=s_[0:n, 0:1], scalar1=-1.0 / 128, scalar2=None, op0=ALU.mult), r=[s_], w=[s_])
        k.op("dve", lambda e: e.tensor_scalar(out=kf, in0=kf, scalar1=s_[0:n, 1:2], scalar2=None, op0=ALU.add), r=[sf, s_], w=[sf])
        k.op("act", lambda e: e.activation(out=sf[0:n, 256:384], in_=kf, func=AF.Square, accum_out=s_[0:n, 2:3]), r=[sf, s_], w=[sf, s_])
        k.op("act", lambda e: e.activation(out=s_[0:n, 3:4], in_=s_[0:n, 2:3], func=AF.Sqrt, scale=1.0 / 128, bias=EPS), r=[s_], w=[s_])
        k.op("dve", lambda e: e.reciprocal(out=s_[0:n, 4:5], in_=s_[0:n, 3:4]), r=[s_], w=[s_])
        k.op("dve", lambda e: e.scalar_tensor_tensor(out=kf, in0=kf, scalar=s_[0:n, 4:5], op0=ALU.mult, in1=lng[0:n, :], op1=ALU.mult), r=[sf, s_, lng], w=[sf])
        k.op("dve", lambda e: e.tensor_tensor(out=kf, in0=kf, in1=lnb[0:n, :], op=ALU.add), r=[sf, lnb], w=[sf])
        k.op("dve", lambda e: e.tensor_copy(out=kb_[0:n, :], in_=kf), r=[sf], w=[kb_])
        if c0 < T:
            k.dma("sp", ki_p[c0:c0 + n, :], kf, r=[sf])
        else:
            k.dma("sp", ki_s.ap(), kf, r=[sf])
        b2 = k.bank()
        k.op("pe", lambda e: e.transpose(out=b2[:].bitcast(BF16)[:, 0:n], in_=kb_[0:n, :], identity=ident_b[0:n, 0:n]), r=[kb_, ident_b], w=[b2])
        k.op("act", lambda e: e.copy(out=ikT[:, c0:c0 + n], in_=b2[:].bitcast(BF16)[:, 0:n]), r=[b2], w=[(ikT, c0)])

    wt = load_w(w_in, O_WI, 144)
    for ti in range(NT + 1):
        tm_tile(wt, 144, ti, ep_wk)
    k.dma("sp", ikTd.ap(), ikT[:], r=[(ikT, c) for c in range(0, TT, 128)], w=["ikTd"])
    k.barrier()
    k.release(mP)
    if stop_after == "P2":
        return k.finish()
    conv_jobs = []
    for g in range(16):
        conv_jobs.append((w1s[g], w_ff1[:, g * 512:(g + 1) * 512].rearrange("(k p) c -> p k c", p=128), ("w1s", g)))
    for qc in range(4):
        for fgg in range(8):
            conv_jobs.append((w2s[qc, fgg], w_ff2[fgg * 1024:(fgg + 1) * 1024, qc * 512:(qc + 1) * 512].rearrange("(c p) n -> p c n", p=128), ("w2s", qc, fgg)))

    def issue_conv(nj):
        for _ in range(nj):
            if conv_jobs:
                o_, i_, key_ = conv_jobs.pop(0)
                k.dma("pool", o_, i_, w=[key_])
    mixT = k.alloc("mixT", [128, 16, TT], BF16)
    m3 = k.mark()
    HG = 4
    HW = HG * 128
    ones_f = k.alloc("ones_f", [128, 128], F32)
    TRI = k.alloc("TRI", [128, 128], F32)
    POSM = k.alloc("POSM", [128, HG, 128], F32)
    OFFD = k.alloc("OFFD", [128, HG, 128], F32)
    hvb = k.alloc("hvb", [128, 16], F32)
    gnb = k.alloc("gnb", [128, 128], F32)
    k.op("pool", lambda e: e.memset(ones_f[:], 1.0), w=[ones_f])
    k.op("pool", lambda e: e.memset(TRI[:], 1.0), w=[TRI])
    k.op("pool", lambda e: e.affine_select(out=TRI[:], in_=TRI[:], pattern=[[1, 128]], compare_op=ALU.is_ge, fill=0.0, base=0, channel_multiplier=-1),
         r=[TRI], w=[TRI])
    k.op("pool", lambda e: e.memset(POSM[:], 0.0), w=[POSM])
    k.op("pool", lambda e: e.affine_select(out=POSM[:], in_=POSM[:], pattern=[[0, HG], [-1, 128]], compare_op=ALU.is_ge, fill=30000.0, base=0, channel_multiplier=1),
         r=[POSM], w=[POSM])
    k.op("pool", lambda e: e.memset(OFFD[:], 1.0), w=[OFFD])
    k.op("pool", lambda e: e.affine_select(out=OFFD[:], in_=OFFD[:], pattern=[[0, HG], [-1, 128]], compare_op=ALU.not_equal, fill=0.0, base=0, channel_multiplier=1),
         r=[OFFD], w=[OFFD])
    k.dma("sp", hvb[:], hv.ap().to_broadcast([128, 16]), w=[hvb])
    k.dma("sp", gnb[:], gdn_g.ap().to_broadcast([128, 128]), w=[gnb])

    gabt = k.alloc("gabt", [128, NT, 16], F32)
    k.dma("sp", gabt[:], gab[0:T, :].rearrange("(t p) c -> p t c", p=128), r=["gab"], w=[gabt], slow=True)
    nA = k.alloc("nA", [128, 8], F32)
    G_ = k.alloc("G_", [128, NT, 8], F32)
    Bt = k.alloc("Bt", [128, NT, 8], F32)
    NB = k.alloc("NB", [128, NT, 8], F32)
    GC = k.alloc("GC", [128, NT, 8], F32)
    GL = k.alloc("GL", [128, NT, 8], F32)
    EG = k.alloc("EG", [128, NT, 8], F32)
    EGL = k.alloc("EGL", [128, NT, 8], F32)
    EKD = k.alloc("EKD", [128, NT, 8], F32)
    BEG = k.alloc("BEG", [128, NT, 8], F32)
    k.op("act", lambda e: e.activation(out=nA[:], in_=hvb[:, 0:8], func=AF.Exp), r=[hvb], w=[nA])
    k.op("dve", lambda e: e.tensor_scalar(out=nA[:], in0=nA[:], scalar1=-1.0, scalar2=None, op0=ALU.mult), r=[nA], w=[nA])
    k.op("dve", lambda e: e.tensor_tensor(out=G_[:], in0=gabt[:, :, 0:8], in1=hvb[:, 8:16].unsqueeze(1).to_broadcast([128, NT, 8]), op=ALU.add), r=[gabt, hvb], w=[G_])
    k.op("act", lambda e: e.activation(out=G_[:], in_=G_[:], func=AF.Exp), r=[G_], w=[G_])
    k.op("act", lambda e: e.activation(out=G_[:], in_=G_[:], func=AF.Ln, bias=1.0, scale=1.0), r=[G_], w=[G_])
    k.op("dve", lambda e: e.tensor_tensor(out=G_[:], in0=G_[:], in1=nA[:].unsqueeze(1).to_broadcast([128, NT, 8]), op=ALU.mult), r=[G_, nA], w=[G_])
    k.op("act", lambda e: e.activation(out=Bt[:], in_=gabt[:, :, 8:16], func=AF.Sigmoid), r=[gabt], w=[Bt])
    k.op("dve", lambda e: e.tensor_scalar(out=NB[:], in0=Bt[:], scalar1=-1.0, scalar2=None, op0=ALU.mult), r=[Bt], w=[NB])
    bA = k.bank()
    bB = k.bank()
    for t in range(NT):
        k.op("pe", lambda e, t=t: e.matmul(bA[:, t * 8:(t + 1) * 8], lhsT=TRI[:], rhs=G_[:, t, :], start=True, stop=True), r=[TRI, G_], w=[bA])
        k.op("pe", lambda e, t=t: e.matmul(bB[:, t * 8:(t + 1) * 8], lhsT=ones_f[:], rhs=G_[:, t, :], start=True, stop=True), r=[ones_f, G_], w=[bB])
    k.op("act", lambda e: e.copy(out=GC[:].rearrange("p a b -> p (a b)"), in_=bA[:, 0:NT * 8]), r=[bA], w=[GC])
    k.op("act", lambda e: e.copy(out=GL[:].rearrange("p a b -> p (a b)"), in_=bB[:, 0:NT * 8]), r=[bB], w=[GL])
    k.op("act", lambda e: e.activation(out=EG[:], in_=GC[:], func=AF.Exp), r=[GC], w=[EG])
    k.op("act", lambda e: e.activation(out=EGL[:], in_=GL[:], func=AF.Exp), r=[GL], w=[EGL])
    k.op("dve", lambda e: e.tensor_tensor(out=EKD[:], in0=GL[:], in1=GC[:], op=ALU.subtract), r=[GL, GC], w=[EKD])
    k.op("act", lambda e: e.activation(out=EKD[:], in_=EKD[:], func=AF.Exp), r=[EKD], w=[EKD])
    k.op("dve", lambda e: e.tensor_tensor(out=BEG[:], in0=Bt[:], in1=EG[:], op=ALU.mult), r=[Bt, EG], w=[BEG])

    if stop_after == "P3a":
        k.barrier()
        return k.finish()
    m3g = k.mark()
    qT = k.alloc("qT", [128, HG, TT], BF16)
    kT = k.alloc("kT", [128, HG, TT], BF16)
    vT = k.alloc("vT", [128, HG, TT], BF16)
    NSLOT = 2

    def mk_slot(j):
        d = {}
        for nm, dt in (("Dg", F32), ("egT", BF16), ("decay", F32), ("M0", F32), ("M1", F32), ("MT0", F32), ("MT1", F32),
                       ("PT", F32), ("PTb", BF16), ("vbeta", BF16), ("kbg", BF16), ("kdec", BF16), ("u", F32), ("wT", BF16),
                       ("intra", BF16), ("intraT", BF16), ("qg", BF16)):
            d[nm] = k.alloc(f"{nm}_{j}", [128, HG, 128], dt)
        return d

    slots = [mk_slot(j) for j in range(NSLOT)]
    S_ = k.alloc("S_", [128, HG, 128], F32)
    Sb = k.alloc("Sb", [128, HG, 128], BF16)
    vnew = k.alloc("vnew", [128, HG, 128], BF16)
    o_ = k.alloc("o_", [128, HG, 128], F32)
    sq_ = k.alloc("sq_", [128, HG, 128], F32)
    zt = [k.alloc(f"zt{i}", [128, HG, 128], BF16) for i in range(2)]
    zs = k.alloc("zs", [128, HG, 128], F32)
    oa = k.alloc("oa", [128, HG, 128], BF16)
    sst = k.alloc("sst", [128, 3 * HG], F32)

    def fl(b):
        return b[:].rearrange("p a b -> p (a b)")

    def bc_tok(src_ap):
        return src_ap.unsqueeze(2).to_broadcast([128, HG, 128])

    def stageA(g, i, sl):
        d = slots[sl]
        h0 = g * HG
        tsl = slice(i * 128, (i + 1) * 128)
        Dg, egT, decay, PT, PTb = d["Dg"], d["egT"], d["decay"], d["PT"], d["PTb"]
        Ms = [d["M0"], d["M1"]]
        MTs = [d["MT0"], d["MT1"]]
        k.op("dve", lambda e: e.tensor_tensor(out=Dg[:], in0=ident_f[:].unsqueeze(1).to_broadcast([128, HG, 128]), in1=bc_tok(GC[:, i, h0:h0 + HG]), op=ALU.mult),
             r=[ident_f, GC], w=[Dg])
        b1 = k.bank()
        k.op("pe", lambda e: e.matmul(b1[:, 0:HW], lhsT=ones_f[:], rhs=fl(Dg), start=True, stop=True), r=[ones_f, Dg], w=[b1])
        k.op("act", lambda e: e.activation(out=fl(egT), in_=b1[:, 0:HW], func=AF.Exp), r=[b1], w=[egT])
        b2 = k.bank()
        k.op("pe", lambda e: e.matmul(b2[:, 0:HW], lhsT=ones_f[:], rhs=fl(Dg), start=True, stop=False), r=[ones_f, Dg], w=[b2])
        k.op("pe", lambda e: e.matmul(b2[:, 0:HW], lhsT=ident_f[:], rhs=fl(POSM), start=False, stop=True), r=[ident_f, POSM], w=[b2])
        for h in range(HG):
            k.op("act", lambda e, h=h: e.activation(out=decay[:, h, :], in_=b2[:, h * 128:(h + 1) * 128], func=AF.Exp, scale=-1.0, bias=GC[:, i, h0 + h:h0 + h + 1]),
                 r=[b2, GC], w=[decay])
        k.op("dve", lambda e: e.tensor_tensor(out=d["qg"][:], in0=qT[:, :, tsl], in1=egT[:], op=ALU.mult), r=[qT, egT], w=[d["qg"]])
        b3 = k.bank()
        b4 = k.bank()
        for h in range(HG):
            k.op("pe", lambda e, h=h: e.matmul(b3[:, h * 128:(h + 1) * 128], lhsT=kT[:, h, tsl], rhs=kT[:, h, tsl], start=True, stop=True), r=[kT], w=[b3])
        for h in range(HG):
            k.op("pe", lambda e, h=h: e.matmul(b4[:, h * 128:(h + 1) * 128], lhsT=qT[:, h, tsl], rhs=kT[:, h, tsl], start=True, stop=True), r=[qT, kT], w=[b4])
        k.op("dve", lambda e: e.tensor_tensor(out=fl(d["intra"]), in0=b4[:, 0:HW], in1=fl(decay), op=ALU.mult), r=[b4, decay], w=[d["intra"]])
        k.op("dve", lambda e: e.tensor_tensor(out=Dg[:], in0=decay[:], in1=OFFD[:], op=ALU.mult), r=[decay, OFFD], w=[Dg])
        for h in range(HG):
            k.op("dve", lambda e, h=h: e.scalar_tensor_tensor(out=Ms[0][:, h, :], in0=b3[:, h * 128:(h + 1) * 128], scalar=NB[:, i, h0 + h:h0 + h + 1], op0=ALU.mult,
                                                                in1=Dg[:, h, :], op1=ALU.mult), r=[b3, NB, Dg], w=[Ms[0]])
        yield
        b5 = k.bank()
        b5i = k.bank()
        b5b = b5i[:].bitcast(BF16)
        for h in range(HG):
            k.op("pe", lambda e, h=h: e.transpose(out=b5[:, h * 128:(h + 1) * 128], in_=Ms[0][:, h, :], identity=ident_f[:]), r=[Ms[0], ident_f], w=[b5])
        for h in range(HG):
            k.op("pe", lambda e, h=h: e.transpose(out=b5b[:, h * 128:(h + 1) * 128], in_=d["intra"][:, h, :], identity=ident_b[:]), r=[d["intra"], ident_b], w=[b5i])
        k.op("act", lambda e: e.copy(out=fl(MTs[0]), in_=b5[:, 0:HW]), r=[b5], w=[MTs[0]])
        k.op("act", lambda e: e.copy(out=fl(d["intraT"]), in_=b5b[:, 0:HW]), r=[b5i], w=[d["intraT"]])
        k.op("dve", lambda e: e.tensor_tensor(out=PT[:], in0=MTs[0][:], in1=ident_f[:].unsqueeze(1).to_broadcast([128, HG, 128]), op=ALU.add), r=[MTs[0], ident_f], w=[PT])
        yield
        cur = 0
        for lvl in range(1, 7):
            nx = 1 - cur
            last = (lvl == 6)
            b6 = k.bank()
            for h in range(HG):
                k.op("pe", lambda e, h=h, cur=cur, b6=b6: e.matmul(b6[:, h * 128:(h + 1) * 128], lhsT=MTs[cur][:, h, :], rhs=Ms[cur][:, h, :], start=True, stop=True),
                     r=[MTs[cur], Ms[cur]], w=[b6])
            if not last:
                b7 = k.bank()
                for h in range(HG):
                    k.op("pe", lambda e, h=h, cur=cur, b7=b7: e.matmul(b7[:, h * 128:(h + 1) * 128], lhsT=Ms[cur][:, h, :], rhs=MTs[cur][:, h, :], start=True, stop=True),
                         r=[MTs[cur], Ms[cur]], w=[b7])
            k.op("act", lambda e, nx=nx, b6=b6: e.copy(out=fl(Ms[nx]), in_=b6[:, 0:HW]), r=[b6], w=[Ms[nx]])
            if not last:
                k.op("act", lambda e, nx=nx, b7=b7: e.copy(out=fl(MTs[nx]), in_=b7[:, 0:HW]), r=[b7], w=[MTs[nx]])
            b8 = k.bank()
            for h in range(HG):
                k.op("pe", lambda e, h=h, nx=nx, b8=b8: e.matmul(b8[:, h * 128:(h + 1) * 128], lhsT=Ms[nx][:, h, :], rhs=PT[:, h, :], start=True, stop=True),
                     r=[Ms[nx], PT], w=[b8])
            k.op("dve", lambda e, b8=b8: e.tensor_tensor(out=fl(PT), in0=b8[:, 0:HW], in1=fl(PT), op=ALU.add), r=[b8, PT], w=[PT])
            if last:
                k.op("act", lambda e: e.copy(out=PTb[:], in_=PT[:]), r=[PT], w=[PTb])
            cur = nx
            yield
        b9 = k.bank()
        b9b = b9[:].bitcast(BF16)
        for h in range(HG):
            k.op("pe", lambda e, h=h: e.transpose(out=b9b[:, h * 128:(h + 1) * 128], in_=vT[:, h, tsl], identity=ident_b[:]), r=[vT, ident_b], w=[b9])
        for h in range(HG):
            k.op("pe", lambda e, h=h: e.transpose(out=b9b[:, HW + h * 128:HW + (h + 1) * 128], in_=kT[:, h, tsl], identity=ident_b[:]), r=[kT, ident_b], w=[b9])
        vps = b9b[:, 0:HW].rearrange("p (a b) -> p a b", a=HG)
        kps = b9b[:, HW:2 * HW].rearrange("p (a b) -> p a b", a=HG)
        k.op("dve", lambda e: e.tensor_tensor(out=d["vbeta"][:], in0=vps, in1=bc_tok(Bt[:, i, h0:h0 + HG]), op=ALU.mult), r=[b9, Bt], w=[d["vbeta"]])
        k.op("dve", lambda e: e.tensor_tensor(out=d["kbg"][:], in0=kps, in1=bc_tok(BEG[:, i, h0:h0 + HG]), op=ALU.mult), r=[b9, BEG], w=[d["kbg"]])
        k.op("dve", lambda e: e.tensor_tensor(out=d["kdec"][:], in0=kps, in1=bc_tok(EKD[:, i, h0:h0 + HG]), op=ALU.mult), r=[b9, EKD], w=[d["kdec"]])
        b10 = k.bank()
        b11 = k.bank()
        for h in range(HG):
            k.op("pe", lambda e, h=h: e.matmul(b10[:, h * 128:(h + 1) * 128], lhsT=PTb[:, h, :], rhs=d["vbeta"][:, h, :], start=True, stop=True), r=[PTb, d["vbeta"]], w=[b10])
        for h in range(HG):
            k.op("pe", lambda e, h=h: e.matmul(b11[:, h * 128:(h + 1) * 128], lhsT=d["kbg"][:, h, :], rhs=PTb[:, h, :], start=True, stop=True), r=[PTb, d["kbg"]], w=[b11])
        k.op("act", lambda e: e.copy(out=fl(d["u"]), in_=b10[:, 0:HW]), r=[b10], w=[d["u"]])
        k.op("act", lambda e: e.copy(out=fl(d["wT"]), in_=b11[:, 0:HW]), r=[b11], w=[d["wT"]])
        yield

    def scan_step(g, i, sl):
        d = slots[sl]
        h0 = g * HG
        tsl = slice(i * 128, (i + 1) * 128)
        z_ = zt[i % 2]
        k.dma("sp", fl(z_), gz[i * 128:(i + 1) * 128, h0 * 128:(h0 + HG) * 128], r=["gz"], w=[z_])
        bx = k.bank()
        for h in range(HG):
            k.op("pe", lambda e, h=h: e.matmul(bx[:, h * 128:(h + 1) * 128], lhsT=d["wT"][:, h, :], rhs=Sb[:, h, :], start=True, stop=True), r=[d["wT"], Sb], w=[bx])
        k.op("dve", lambda e: e.tensor_tensor(out=fl(vnew), in0=fl(d["u"]), in1=bx[:, 0:HW], op=ALU.subtract), r=[d["u"], bx], w=[vnew])
        bo = k.bank()
        for h in range(HG):
            k.op("pe", lambda e, h=h: e.matmul(bo[:, h * 128:(h + 1) * 128], lhsT=d["qg"][:, h, :], rhs=Sb[:, h, :], start=True, stop=False), r=[d["qg"], Sb], w=[bo])
            k.op("pe", lambda e, h=h: e.matmul(bo[:, h * 128:(h + 1) * 128], lhsT=d["intraT"][:, h, :], rhs=vnew[:, h, :], start=False, stop=True), r=[d["intraT"], vnew], w=[bo])
        bz = k.bank()
        for h in range(HG):
            k.op("pe", lambda e, h=h: e.matmul(bz[:, h * 128:(h + 1) * 128], lhsT=d["kdec"][:, h, :], rhs=vnew[:, h, :], start=True, stop=True), r=[d["kdec"], vnew], w=[bz])
        k.op("dve", lambda e: e.tensor_tensor(out=S_[:], in0=S_[:], in1=bc_tok(EGL[:, i, h0:h0 + HG]), op=ALU.mult), r=[S_, EGL], w=[S_])
        k.op("dve", lambda e: e.tensor_tensor(out=fl(S_), in0=fl(S_), in1=bz[:, 0:HW], op=ALU.add), r=[S_, bz], w=[S_])
        k.op("act", lambda e: e.copy(out=Sb[:], in_=S_[:]), r=[S_], w=[Sb])
        k.op("act", lambda e: e.copy(out=fl(o_), in_=bo[:, 0:HW]), r=[bo], w=[o_])
        k.op("act", lambda e: e.activation(out=sq_[:], in_=o_[:], func=AF.Square), r=[o_], w=[sq_])
        k.op("dve", lambda e: e.tensor_reduce(out=sst[:, 0:HG], in_=sq_[:], axis=AX.X, op=ALU.add), r=[sq_], w=[sst])
        k.op("act", lambda e: e.activation(out=sst[:, HG:2 * HG], in_=sst[:, 0:HG], func=AF.Sqrt, scale=1.0 / 128, bias=EPS), r=[sst], w=[sst])
        k.op("dve", lambda e: e.reciprocal(out=sst[:, 2 * HG:3 * HG], in_=sst[:, HG:2 * HG]), r=[sst], w=[sst])
        k.op("act", lambda e: e.activation(out=zs[:], in_=z_[:], func=AF.Silu), r=[z_], w=[zs])
        k.op("dve", lambda e: e.tensor_tensor(out=o_[:], in0=o_[:], in1=bc_tok(sst[:, 2 * HG:3 * HG]), op=ALU.mult), r=[o_, sst], w=[o_])
        k.op("dve", lambda e: e.tensor_tensor(out=o_[:], in0=o_[:], in1=gnb[:].unsqueeze(1).to_broadcast([128, HG, 128]), op=ALU.mult), r=[o_, gnb], w=[o_])
        k.op("dve", lambda e: e.tensor_tensor(out=oa[:], in0=o_[:], in1=zs[:], op=ALU.mult), r=[o_, zs], w=[oa])
        bt = k.bank()
        btb = bt[:].bitcast(BF16)
        for h in range(HG):
            k.op("pe", lambda e, h=h: e.transpose(out=btb[:, h * 128:(h + 1) * 128], in_=oa[:, h, :], identity=ident_b[:]), r=[oa, ident_b], w=[bt])
        k.op("act", lambda e: e.copy(out=mixT[:, h0:h0 + HG, tsl], in_=btb[:, 0:HW].rearrange("p (a b) -> p a b", a=HG)), r=[bt], w=[(mixT, i)])

    for g in range(8 // HG):
        h0 = g * HG
        for nm, src, buf in (("q", gq, qT), ("k", gk, kT), ("v", gv, vT)):
            k.dma("sp", buf[:], src[h0:h0 + HG].rearrange("h d t -> d h t"), r=["gqk", "gv"], w=[buf])
        k.op("pool", lambda e: e.memset(S_[:], 0.0), w=[S_])
        k.op("pool", lambda e: e.memset(Sb[:], 0.0), w=[Sb])
        if stop_after == "P3d":
            for _ in stageA(0, 0, 0):
                pass
            for _ in stageA(0, 1, 1):
                pass
            scan_step(0, 0, 0)
            scan_step(0, 1, 1)
            d = slots[0]
            names = ["decay", "M0", "PT", "u", "wT", "intraT", "qg", "kdec", "vbeta", "kbg", "egT"]
            tmpfs = [sq_, zs]
            for ii, nm in enumerate(names):
                dd = dout("dbg_" + nm, [128, HW], F32)
                tmpf = tmpfs[ii % 2]
                k.op("dve", lambda e, nm=nm, tmpf=tmpf: e.tensor_copy(out=fl(tmpf), in_=fl(d[nm])), r=[d[nm]], w=[tmpf])
                k.dma("sp", dd.ap(), fl(tmpf), r=[tmpf])
            for nm, b in (("S", S_), ("o", o_), ("GC", GC), ("G", G_), ("Bt", Bt), ("EKD", EKD), ("EGL", EGL)):
                dd = dout("dbg_" + nm, [128, int(np.prod(b.ap.shape[1:]))], F32)
                k.dma("sp", dd.ap(), b[:].rearrange("p a b -> p (a b)"), r=[b])
            k.barrier()
            return k.finish()
        import os
        _lim = int(os.environ.get("YLIM", "100"))
        for i0 in range(0, NT, NSLOT):
            gens = [stageA(g, i0 + j, j) for j in range(NSLOT)]
            if stop_after == "P3b":
                for _ in range(_lim):
                    for gen in gens:
                        next(gen, None)
                k.barrier()
                return k.finish()
            alive = True
            while alive:
                alive = False
                for gen in gens:
                    try:
                        next(gen)
                        alive = True
                    except StopIteration:
                        pass
            for j in range(NSLOT):
                scan_step(g, i0 + j, j)
            issue_conv(3)
        k.dma("sp", ssm_p.ap()[h0:h0 + HG].rearrange("h a b -> a h b"), S_[:], r=[S_])
    if stop_after == "P3":
        k.barrier()
        return k.finish()
    issue_conv(100)
    k.barrier()
    k.release(m3g)
    S0 = k.alloc("S0", [128, 8, 128], F32)
    qc = k.alloc("qc", [128, 8], BF16)
    kc = k.alloc("kc", [128, 8], BF16)
    vc = k.alloc("vc", [128, 8], BF16)
    qcf = k.alloc("qcf", [128, 8], F32)
    kcf = k.alloc("kcf", [128, 8], F32)
    gabr = k.alloc("gabr", [1, 16], F32)
    zr = k.alloc("zr", [1, 1024], BF16)
    zrs = k.alloc("zrs", [1, 8, 128], F32)
    rw = k.alloc("rw", [1, 64], F32)
    t1 = k.alloc("t1", [1, 8, 128], F32)
    orow = k.alloc("orow", [1, 8, 128], F32)
    sqr = k.alloc("sqr", [1, 8, 128], F32)
    oar = k.alloc("oar", [1, 1024], BF16)
    abs_ = k.alloc("abs_", [128, 8], F32)

    def bc_row(ap8):
        return ap8.unsqueeze(2).to_broadcast([1, 8, 128])

    def sample_gdn(s_i):
        col = T + s_i
        k.dma("sp", S0[:], st_ssm.ap()[s_i].rearrange("h a b -> a h b"), w=[S0])
        k.dma("sp", qc[:], gq.ap()[:, :, col].rearrange("h d -> d h"), r=["gqk"], w=[qc], slow=True)
        k.dma("sp", kc[:], gk.ap()[:, :, col].rearrange("h d -> d h"), r=["gqk"], w=[kc], slow=True)
        k.dma("sp", vc[:], gv.ap()[:, :, col].rearrange("h d -> d h"), r=["gv"], w=[vc], slow=True)
        k.dma("sp", gabr[:], gab[col:col + 1, :], r=["gab"], w=[gabr])
        k.dma("sp", zr[:], gz[col:col + 1, :], r=["gz"], w=[zr])
        k.op("dve", lambda e: e.tensor_copy(out=qcf[:], in_=qc[:]), r=[qc], w=[qcf])
        k.op("dve", lambda e: e.tensor_copy(out=kcf[:], in_=kc[:]), r=[kc], w=[kcf])
        k.op("dve", lambda e: e.tensor_tensor(out=rw[:, 0:8], in0=gabr[:, 0:8], in1=hvb[0:1, 8:16], op=ALU.add), r=[gabr, hvb], w=[rw])
        k.op("act", lambda e: e.activation(out=rw[:, 0:8], in_=rw[:, 0:8], func=AF.Exp), r=[rw], w=[rw])
        k.op("act", lambda e: e.activation(out=rw[:, 0:8], in_=rw[:, 0:8], func=AF.Ln, bias=1.0, scale=1.0), r=[rw], w=[rw])
        k.op("dve", lambda e: e.tensor_tensor(out=rw[:, 0:8], in0=rw[:, 0:8], in1=nA[0:1, :], op=ALU.mult), r=[rw, nA], w=[rw])
        k.op("act", lambda e: e.activation(out=rw[:, 8:16], in_=rw[:, 0:8], func=AF.Exp), r=[rw], w=[rw])
        k.op("act", lambda e: e.activation(out=rw[:, 16:24], in_=gabr[:, 8:16], func=AF.Sigmoid), r=[gabr, rw], w=[rw])
        ba = k.bank()
        bb_ = k.bank()
        for h in range(8):
            bk_ = ba if h < 4 else bb_
            k.op("pe", lambda e, h=h, bk_=bk_: e.matmul(bk_[0:1, (h % 4) * 128:(h % 4 + 1) * 128], lhsT=kcf[:, h:h + 1], rhs=S0[:, h, :], start=True, stop=True),
                 r=[kcf, S0], w=[bk_])
        bv = k.bank()
        bvb = bv[:].bitcast(BF16)
        for h in range(8):
            k.op("pe", lambda e, h=h: e.transpose(out=bvb[0:1, h * 128:(h + 1) * 128], in_=vc[:, h:h + 1], identity=ident_b[:]), r=[vc, ident_b], w=[bv])
        k.op("dve", lambda e: e.tensor_tensor(out=t1[:, 0:4, :], in0=ba[0:1, :].rearrange("p (a b) -> p a b", a=4), in1=bc_row(rw[:, 8:16])[:, 0:4, :], op=ALU.mult),
             r=[ba, rw], w=[t1])
        k.op("dve", lambda e: e.tensor_tensor(out=t1[:, 4:8, :], in0=bb_[0:1, :].rearrange("p (a b) -> p a b", a=4), in1=bc_row(rw[:, 8:16])[:, 4:8, :], op=ALU.mult),
             r=[bb_, rw, t1], w=[t1])
        k.op("dve", lambda e: e.tensor_tensor(out=t1[:], in0=bvb[0:1, 0:1024].rearrange("p (a b) -> p a b", a=8), in1=t1[:], op=ALU.subtract), r=[bv, t1], w=[t1])
        k.op("dve", lambda e: e.tensor_tensor(out=t1[:], in0=t1[:], in1=bc_row(rw[:, 16:24]), op=ALU.mult), r=[t1, rw], w=[t1])
        bd0 = k.bank()
        bd1 = k.bank()
        k.op("pe", lambda e: e.matmul(bd0[:, :], lhsT=ones_f[0:1, :], rhs=t1[:].rearrange("p a b -> p (a b)")[:, 0:512], start=True, stop=True), r=[ones_f, t1], w=[bd0])
        k.op("pe", lambda e: e.matmul(bd1[:, :], lhsT=ones_f[0:1, :], rhs=t1[:].rearrange("p a b -> p (a b)")[:, 512:1024], start=True, stop=True), r=[ones_f, t1], w=[bd1])
        bab = k.bank()
        k.op("pe", lambda e: e.matmul(bab[:, 0:8], lhsT=ones_f[0:1, :], rhs=rw[:, 8:16], start=True, stop=True), r=[ones_f, rw], w=[bab])
        k.op("act", lambda e: e.copy(out=abs_[:], in_=bab[:, 0:8]), r=[bab], w=[abs_])
        k.op("dve", lambda e: e.tensor_tensor(out=S0[:], in0=S0[:], in1=abs_[:].unsqueeze(2).to_broadcast([128, 8, 128]), op=ALU.mult), r=[S0, abs_], w=[S0])
        for h in range(8):
            bd = bd0 if h < 4 else bd1
            k.op("dve", lambda e, h=h, bd=bd: e.scalar_tensor_tensor(out=S0[:, h, :], in0=bd[:, (h % 4) * 128:(h % 4 + 1) * 128], scalar=kcf[:, h:h + 1], op0=ALU.mult,
                                                                      in1=S0[:, h, :], op1=ALU.add), r=[bd, kcf, S0], w=[S0])
        k.dma("sp", ssm_s.ap()[s_i].rearrange("h a b -> a h b"), S0[:], r=[S0])
        bo0 = k.bank()
        bo1 = k.bank()
        for h in range(8):
            bk_ = bo0 if h < 4 else bo1
            k.op("pe", lambda e, h=h, bk_=bk_: e.matmul(bk_[0:1, (h % 4) * 128:(h % 4 + 1) * 128], lhsT=qcf[:, h:h + 1], rhs=S0[:, h, :], start=True, stop=True),
                 r=[qcf, S0], w=[bk_])
        k.op("act", lambda e: e.copy(out=orow[:, 0:4, :], in_=bo0[0:1, :].rearrange("p (a b) -> p a b", a=4)), r=[bo0], w=[orow])
        k.op("act", lambda e: e.copy(out=orow[:, 4:8, :], in_=bo1[0:1, :].rearrange("p (a b) -> p a b", a=4)), r=[bo1, orow], w=[orow])
        k.op("dve", lambda e: e.tensor_tensor(out=sqr[:], in0=orow[:], in1=orow[:], op=ALU.mult), r=[orow], w=[sqr])
        k.op("dve", lambda e: e.tensor_reduce(out=rw[:, 24:32], in_=sqr[:], axis=AX.X, op=ALU.add), r=[sqr, rw], w=[rw])
        k.op("act", lambda e: e.activation(out=rw[:, 32:40], in_=rw[:, 24:32], func=AF.Sqrt, scale=1.0 / 128, bias=EPS), r=[rw], w=[rw])
        k.op("dve", lambda e: e.reciprocal(out=rw[:, 40:48], in_=rw[:, 32:40]), r=[rw], w=[rw])
        k.op("act", lambda e: e.activation(out=zrs[:].rearrange("p a b -> p (a b)"), in_=zr[:], func=AF.Silu), r=[zr], w=[zrs])
        k.op("dve", lambda e: e.tensor_tensor(out=orow[:], in0=orow[:], in1=bc_row(rw[:, 40:48]), op=ALU.mult), r=[orow, rw], w=[orow])
        k.op("dve", lambda e: e.tensor_tensor(out=orow[:], in0=orow[:], in1=gnb[0:1, :].unsqueeze(1).to_broadcast([1, 8, 128]), op=ALU.mult), r=[orow, gnb], w=[orow])
        k.op("dve", lambda e: e.tensor_tensor(out=oar[:].rearrange("p (a b) -> p a b", a=8), in0=orow[:], in1=zrs[:], op=ALU.mult), r=[orow, zrs], w=[oar])
        bt_ = k.bank()
        for h in range(8):
            k.op("pe", lambda e, h=h: e.matmul(bt_[:, h:h + 1], lhsT=oar[0:1, h * 128:(h + 1) * 128], rhs=ones_b[0:1, 0:1], start=True, stop=True), r=[oar, ones_b], w=[bt_])
        k.op("act", lambda e: e.copy(out=mixT[:, 0:8, col], in_=bt_[:, 0:8]), r=[bt_], w=[(mixT, "s%d" % s_i)])
    for s_i in range(TS):
        sample_gdn(s_i)
    k.barrier()
    k.release(m3)
    if stop_after == "P3S":
        return k.finish()
    m4 = k.mark()
    NEG = -30000.0
    NIT = 17
    kTb = k.alloc("kTb", [128, 2, TT], BF16)
    vtok = k.alloc("vtok", [128, NT, 256], BF16)
    ikT4 = k.alloc("ikT4", [128, TT], BF16)
    k.dma("sp", kTb[:], akT.ap().rearrange("h d t -> d h t"), r=["plain"], w=[kTb])
    k.dma("sp", vtok[:], av[0:T, :].rearrange("(t p) c -> p t c", p=128), r=["av"], w=[vtok])
    k.dma("sp", ikT4[:], ikTd.ap(), r=["ikTd"], w=[ikT4])
    ones_f4 = k.alloc("ones_f4", [128, 128], F32)
    zeros_b = k.alloc("zeros_b", [128, 128], BF16)
    Jm = k.alloc("Jm", [128, 128], F32)
    CMT = k.alloc("CMT", [128, 128], F32)
    CM = k.alloc("CM", [128, 128], F32)
    k.op("pool", lambda e: e.memset(ones_f4[:], 1.0), w=[ones_f4])
    k.op("pool", lambda e: e.memset(zeros_b[:], 0.0), w=[zeros_b])
    k.op("pool", lambda e: e.memset(Jm[:], 1.0), w=[Jm])
    k.op("pool", lambda e: e.affine_select(out=Jm[:], in_=Jm[:], pattern=[[1, 128]], compare_op=ALU.is_equal, fill=0.0, base=-127, channel_multiplier=1), r=[Jm], w=[Jm])
    k.op("pool", lambda e: e.memset(CMT[:], 0.0), w=[CMT])
    k.op("pool", lambda e: e.affine_select(out=CMT[:], in_=CMT[:], pattern=[[1, 128]], compare_op=ALU.is_ge, fill=NEG, base=0, channel_multiplier=-1), r=[CMT], w=[CMT])
    k.op("pool", lambda e: e.memset(CM[:], 0.0), w=[CM])
    k.op("pool", lambda e: e.affine_select(out=CM[:], in_=CM[:], pattern=[[-1, 128]], compare_op=ALU.is_ge, fill=NEG, base=0, channel_multiplier=1), r=[CM], w=[CM])
    rb = k.alloc("rb", [32, 8], F32)
    rb31 = k.alloc("rb31", [32, 8], F32)
    bohs = k.alloc("bohs", [32, 384], F32)
    bvec = k.alloc("bvec", [8, 384], F32)
    Tp = k.alloc("Tp", [128, 8, 128], F32)
    Bt4 = [k.alloc(f"Bt4_{i}", [128, 8, 128], F32) for i in range(2)]
    k.dma("sp", rb[:], rel_bias.ap(), w=[rb])
    k.dma("sp", rb31[:], rel_bias[31:32, :].to_broadcast([32, 8]), w=[rb31])
    k.dma("sp", bohs[:], boh.ap(), w=[bohs])
    k.op("dve", lambda e: e.tensor_tensor(out=rb[:], in0=rb[:], in1=rb31[:], op=ALU.subtract), r=[rb, rb31], w=[rb])
    bkb = k.bank()
    k.op("pe", lambda e: e.matmul(bkb[0:8, 0:384], lhsT=rb[:], rhs=bohs[:], start=True, stop=True), r=[rb, bohs], w=[bkb])
    k.op("act", lambda e: e.copy(out=bvec[:], in_=bkb[0:8, 0:384]), r=[bkb], w=[bvec])
    k.dma("sp", biasd.ap(), bvec[:], r=[bvec], w=["biasd"])

    def mk_bias(dl):
        k.dma("sp", Tp[:], bass.AP(tensor=biasd, offset=128 * dl, ap=[[1, 128], [384, 8], [1, 128]]), r=["biasd"], w=[Tp])
        for half in range(2):
            bj = k.bank()
            k.op("pe", lambda e, bj=bj, half=half: e.matmul(bj[:, :], lhsT=Jm[:], rhs=Tp[:].rearrange("p a b -> p (a b)")[:, half * 512:(half + 1) * 512], start=True, stop=True), r=[Jm, Tp], w=[bj])
            k.op("act", lambda e, bj=bj, half=half: e.copy(out=Bt4[dl][:].rearrange("p a b -> p (a b)")[:, half * 512:(half + 1) * 512], in_=bj[:, :]), r=[bj], w=[Bt4[dl]])

    mk_bias(0)
    mk_bias(1)

    qbT = [k.alloc(f"qbT{i}", [128, 8, 128], BF16) for i in range(2)]
    qiT = [k.alloc(f"qiT{i}", [128, 16, 128], BF16) for i in range(2)]
    iwt = [k.alloc(f"iwt{i}", [128, 48], F32) for i in range(2)]
    scs = [k.alloc(f"sc{i}", [128, T], F32) for i in range(2)]
    sc = scs[0]
    Dws = [k.alloc(f"Dw{i}", [128, 16, 128], BF16) for i in range(2)]
    jnk = k.alloc("jnk", [128, T], BF16)
    rl = [k.alloc(f"rl{i}", [128, 512], BF16) for i in range(4)]
    rcnt = [0]
    bs = k.alloc("bs", [128, 8], F32)
    negT = k.alloc("negT", [128, NT, 128], BF16)
    PTs = [k.alloc(f"PTs{i}", [128, 4, 128], BF16) for i in range(4)]
    Bt4b = [k.alloc(f"Bt4b_{i}", [128, 8, 128], BF16) for i in range(2)]
    for i_ in range(2):
        k.op("dve", lambda e, i_=i_: e.tensor_copy(out=Bt4b[i_][:], in_=Bt4[i_][:]), r=[Bt4[i_]], w=[Bt4b[i_]])
    rden = k.alloc("rden", [128, 8], F32)
    oab = k.alloc("oab", [128, 8, 128], BF16)
    ecnt = [0]

    def stage_I(qb):
        L = 128 * (qb + 1)
        tsl = slice(qb * 128, (qb + 1) * 128)
        qb_ = qbT[qb % 2]
        qi_ = qiT[qb % 2]
        iw_ = iwt[qb % 2]
        sc = scs[qb % 2]
        Dw_ = Dws[qb % 2]
        k.dma("sp", qb_[:], aq.ap()[:, :, tsl].rearrange("h d t -> d h t"), r=["plain"], w=[qb_])
        k.dma("sp", qi_[:], iq.ap()[:, :, tsl].rearrange("h d t -> d h t"), r=["plain"], w=[qi_])
        k.dma("sp", iw_[:, 0:16], iw[qb * 128:(qb + 1) * 128, :], r=["iw"], w=[iw_])
        if qb < 2:
            return
        for h in range(16):
            k.op("act", lambda e, h=h: e.activation(out=Dw_[:, h, :], in_=ident_f[:], func=AF.Copy, scale=iw_[:, h:h + 1]), r=[ident_f, iw_], w=[Dw_])

        def idx_kg(kg):
            c0 = kg * 512
            n = min(512, L - c0)
            bacc = k.bank()
            k.reserved = {bacc.name}
            pend = []

            def acc(h, r_):
                k.op("pe", lambda e: e.matmul(bacc[:, 0:n], lhsT=Dw_[:, h, :], rhs=r_[:, 0:n], start=(h == 0), stop=(h == 15)), r=[Dw_, r_], w=[bacc])

            for h in range(16):
                bk_ = k.bank()
                r_ = rl[rcnt[0] % 4]
                rcnt[0] += 1
                k.op("pe", lambda e, h=h, bk_=bk_: e.matmul(bk_[:, 0:n], lhsT=qi_[:, h, :], rhs=ikT4[:, c0:c0 + n], start=True, stop=True), r=[qi_, ikT4], w=[bk_])
                k.op("act", lambda e, bk_=bk_, r_=r_: e.activation(out=r_[:, 0:n], in_=bk_[:, 0:n], func=AF.Relu), r=[bk_], w=[r_])
                pend.append((h, r_))
                if len(pend) > 2:
                    acc(*pend.pop(0))
            while pend:
                acc(*pend.pop(0))
            k.reserved = set()
            k.op("act", lambda e: e.copy(out=sc[:, c0:c0 + n], in_=bacc[:, 0:n]), r=[bacc], w=[sc])

        for kg in range((L + 511) // 512):
            idx_kg(kg)

    def stage_B(qb):
        L = 128 * (qb + 1)
        sc = scs[qb % 2]
        if qb >= 2:
            k.op("dve", lambda e: e.tensor_reduce(out=bs[:, 0:1], in_=sc[:, 0:L], axis=AX.X, op=ALU.max), r=[sc], w=[bs])
            k.op("dve", lambda e: e.tensor_reduce(out=bs[:, 1:2], in_=sc[:, 0:L], axis=AX.X, op=ALU.min), r=[sc, bs], w=[bs])
            k.op("dve", lambda e: e.tensor_scalar(out=bs[:, 1:2], in0=bs[:, 1:2], scalar1=-1.0, scalar2=None, op0=ALU.add), r=[bs], w=[bs])
            k.op("dve", lambda e: e.tensor_tensor(out=bs[:, 2:3], in0=bs[:, 0:1], in1=bs[:, 1:2], op=ALU.subtract), r=[bs], w=[bs])
            k.op("dve", lambda e: e.tensor_tensor(out=sc[:, L - 128:L], in0=sc[:, L - 128:L], in1=CM[:], op=ALU.add), r=[sc, CM], w=[sc])
            for it in range(NIT):
                f = 2.0 ** -(it + 1)
                k.op("dve", lambda e, f=f: e.scalar_tensor_tensor(out=bs[:, 3:4], in0=bs[:, 2:3], scalar=f, op0=ALU.mult, in1=bs[:, 1:2], op1=ALU.add), r=[bs], w=[bs])
                k.op("dve", lambda e: e.tensor_scalar(out=jnk[:, 0:L], in0=sc[:, 0:L], scalar1=bs[:, 3:4], scalar2=None, op0=ALU.is_gt, op1=ALU.add, accum_out=bs[:, 4:5]),
                     r=[sc, bs], w=[jnk, bs])
                k.op("dve", lambda e, f=f: e.tensor_scalar(out=bs[:, 5:6], in0=bs[:, 4:5], scalar1=255.5, scalar2=f, op0=ALU.is_gt, op1=ALU.mult), r=[bs], w=[bs])
                k.op("dve", lambda e: e.scalar_tensor_tensor(out=bs[:, 1:2], in0=bs[:, 5:6], scalar=bs[:, 2:3], op0=ALU.mult, in1=bs[:, 1:2], op1=ALU.add), r=[bs], w=[bs])
            k.op("dve", lambda e: e.tensor_scalar(out=sc[:, 0:L], in0=sc[:, 0:L], scalar1=bs[:, 1:2], scalar2=None, op0=ALU.subtract), r=[sc, bs], w=[sc])
            for k4 in range((qb + 1 + 3) // 4):
                nb = min(4, qb + 1 - k4 * 4)
                bk_ = k.bank()
                for j in range(nb):
                    kb = k4 * 4 + j
                    k.op("pe", lambda e, j=j, kb=kb, bk_=bk_: e.transpose(out=bk_[:, j * 128:(j + 1) * 128], in_=sc[:, kb * 128:(kb + 1) * 128], identity=ident_f[:]),
                         r=[sc, ident_f], w=[bk_])
                k.op("dve", lambda e, k4=k4, nb=nb, bk_=bk_: e.tensor_scalar(out=negT[:, k4 * 4:k4 * 4 + nb, :].rearrange("p a b -> p (a b)"), in0=bk_[:, 0:nb * 128],
                                                                             scalar1=0.0, scalar2=NEG, op0=ALU.is_le, op1=ALU.mult), r=[bk_], w=[negT])
            k.op("dve", lambda e: e.tensor_tensor(out=negT[:, qb, :], in0=negT[:, qb, :], in1=CMT[:], op=ALU.add), r=[negT, CMT], w=[negT])
        else:
            if qb == 1:
                k.op("pool", lambda e: e.memset(negT[:, 0, :], 0.0), w=[negT])
            k.op("pool", lambda e: e.tensor_copy(out=negT[:, qb, :], in_=CMT[:]), r=[CMT, negT], w=[negT])

    def stage_A(qb):
        tsl = slice(qb * 128, (qb + 1) * 128)
        qb_ = qbT[qb % 2]
        qi_ = qiT[qb % 2]
        bo0 = k.bank()
        bo1 = k.bank()
        bdn = k.bank()
        k.reserved = {bo0.name, bo1.name, bdn.name}
        for bz_ in (bo0, bo1, bdn):
            k.op("pe", lambda e, bz_=bz_: e.matmul(bz_[:, :], lhsT=zeros_b[:], rhs=qi_[:].rearrange("p a b -> p (a b)")[:, 0:512], start=True, stop=False), r=[zeros_b, qi_], w=[bz_])
        def kv_logits(kb, kvh):
            bl = k.bank()
            near = (qb - kb <= 1)
            k.op("pe", lambda e, bl=bl: e.matmul(bl[:, :], lhsT=kTb[:, kvh, kb * 128:(kb + 1) * 128], rhs=qb_[:, kvh * 4:(kvh + 1) * 4, :].rearrange("p a b -> p (a b)"),
                                                start=True, stop=False), r=[kTb, qb_], w=[bl])
            k.op("pe", lambda e, bl=bl: e.matmul(bl[:, :].rearrange("p (a b) -> p a b", a=4), lhsT=ident_b[:], rhs=negT[:, kb, :].unsqueeze(1).to_broadcast([128, 4, 128]),
                                                start=False, stop=(not near)), r=[ident_b, negT], w=[bl])
            if near:
                k.op("pe", lambda e, bl=bl: e.matmul(bl[:, :], lhsT=ident_b[:], rhs=Bt4b[qb - kb][:, kvh * 4:(kvh + 1) * 4, :].rearrange("p a b -> p (a b)"),
                                                    start=False, stop=True), r=[ident_b, Bt4b[qb - kb]], w=[bl])
            P_ = PTs[ecnt[0] % 4]
            ecnt[0] += 1
            k.op("act", lambda e, bl=bl, P_=P_: e.activation(out=P_[:].rearrange("p a b -> p (a b)"), in_=bl[:, :], func=AF.Exp), r=[bl], w=[P_])
            return P_

        def kv_pv(kb, kvh, P_):
            last = (kb == qb)
            for g_ in range(4):
                h = kvh * 4 + g_
                bo = bo0 if h < 4 else bo1
                k.op("pe", lambda e, g_=g_, h=h, bo=bo: e.matmul(bo[:, (h % 4) * 128:(h % 4 + 1) * 128], lhsT=P_[:, g_, :], rhs=vtok[:, kb, kvh * 128:(kvh + 1) * 128],
                                                                 start=False, stop=last), r=[P_, vtok], w=[bo])
                k.op("pe", lambda e, g_=g_, h=h: e.matmul(bdn[:, h:h + 1], lhsT=P_[:, g_, :], rhs=ones_b[:, 0:1], start=False, stop=last), r=[P_, ones_b], w=[bdn])

        pend = []
        for kb in range(qb + 1):
            for kvh in range(2):
                pend.append((kb, kvh, kv_logits(kb, kvh)))
                if len(pend) > 2:
                    kv_pv(*pend.pop(0))
        while pend:
            kv_pv(*pend.pop(0))
        k.reserved = set()
        k.op("dve", lambda e: e.reciprocal(out=rden[:], in_=bdn[:, 0:8]), r=[bdn], w=[rden])
        for half, bo in ((0, bo0), (1, bo1)):
            k.op("dve", lambda e, half=half, bo=bo: e.tensor_tensor(out=oab[:, half * 4:(half + 1) * 4, :], in0=bo[:, :].rearrange("p (a b) -> p a b", a=4),
                                                                    in1=rden[:, half * 4:(half + 1) * 4].unsqueeze(2).to_broadcast([128, 4, 128]), op=ALU.mult),
                 r=[bo, rden], w=[oab])
        bt_ = k.bank()
        btb = bt_[:].bitcast(BF16)
        for h in range(8):
            k.op("pe", lambda e, h=h: e.transpose(out=btb[:, h * 128:(h + 1) * 128], in_=oab[:, h, :], identity=ident_b[:]), r=[oab, ident_b], w=[bt_])
        k.op("act", lambda e: e.copy(out=mixT[:, 8:16, tsl], in_=btb[:, 0:1024].rearrange("p (a b) -> p a b", a=8)), r=[bt_], w=[(mixT, "b%d" % qb)])

    stage_I(0)
    for qb in range(NT):
        if qb + 1 < NT:
            stage_I(qb + 1)
        stage_B(qb)
        stage_A(qb)
    if stop_after == "P4" and os.environ.get("P4DBG"):
        for nm, b, n in (("sc", sc, T), ("bs", bs, 8), ("negT", negT, NT * 128)):
            dd = dout("dbg_" + nm, [128, n], F32)
            src = b[:] if nm != "negT" else b[:].rearrange("p a b -> p (a b)")
            k.dma("sp", dd.ap(), src, r=[b])
    if stop_after == "P4":
        dbg = dout("dbg_mixT", [128, 16 * TT], BF16)
        k.barrier()
        k.dma("sp", dbg.ap(), mixT[:].rearrange("p a b -> p (a b)"))
        return k.finish()
    k.barrier()
    k.release(m4)
    NPG = NPAGES
    ones4 = k.alloc("ones4", [128, 128], F32)
    zeros4 = k.alloc("zeros4", [128, 128], F32)
    Ltri = k.alloc("Ltri", [128, 128], BF16)
    siota = k.alloc("siota", [128, 128], F32)
    piota = k.alloc("piota", [128, 1], F32)
    jrow = k.alloc("jrow", [128, 256], F32)
    jcol = k.alloc("jcol", [128, 2], F32)
    posc = k.alloc("posc", [128, 128], F32)
    k.op("pool", lambda e: e.memset(ones4[:], 1.0), w=[ones4])
    k.op("pool", lambda e: e.memset(zeros4[:], 0.0), w=[zeros4])
    k.op("pool", lambda e: e.memset(Ltri[:], 1.0), w=[Ltri])
    k.op("pool", lambda e: e.affine_select(out=Ltri[:], in_=Ltri[:], pattern=[[1, 128]], compare_op=ALU.is_ge, fill=0.0, base=-1, channel_multiplier=-1), r=[Ltri], w=[Ltri])
    k.op("pool", lambda e: e.iota(siota[:], pattern=[[1, 128]], base=0, channel_multiplier=0, allow_small_or_imprecise_dtypes=True), w=[siota])
    k.op("pool", lambda e: e.iota(piota[:], pattern=[[0, 1]], base=0, channel_multiplier=1, allow_small_or_imprecise_dtypes=True), w=[piota])
    k.op("pool", lambda e: e.iota(jrow[:], pattern=[[1, 256]], base=0, channel_multiplier=0, allow_small_or_imprecise_dtypes=True), w=[jrow])
    k.op("pool", lambda e: e.iota(jcol[:], pattern=[[128, 2]], base=0, channel_multiplier=1, allow_small_or_imprecise_dtypes=True), w=[jcol])
    k.op("pool", lambda e: e.iota(posc[:], pattern=[[1, 128]], base=0, channel_multiplier=128, allow_small_or_imprecise_dtypes=True), w=[posc])
    thrb = k.alloc("thrb", [128, 31], F32)
    k.dma("sp", thrb[:], bthr.ap().to_broadcast([128, 31]), w=[thrb])
    rbs = k.alloc("rbs", [32, 8], F32)
    rbT = k.alloc("rbT", [8, 32], F32)
    drbT = k.alloc("drbT", [128, 8, 32], F32)
    k.dma("sp", rbs[:], rel_bias.ap(), w=[rbs])
    bq = k.bank()
    k.op("pe", lambda e: e.transpose(out=bq[0:8, 0:32], in_=rbs[:], identity=ident_f[0:32, 0:32]), r=[rbs, ident_f], w=[bq])
    k.op("act", lambda e: e.copy(out=rbT[:], in_=bq[0:8, 0:32]), r=[bq], w=[rbT])
    k.dma("sp", rbTd.ap(), rbT[:], r=[rbT], w=["rbTd"])
    k.dma("sp", drbT[:].rearrange("p a b -> p (a b)"), rbTd.ap().rearrange("a b -> (a b)").unsqueeze(0).to_broadcast([128, 256]), r=["rbTd"], w=[drbT])
    rb0b = k.alloc("rb0b", [128, 8], F32)
    k.op("dve", lambda e: e.tensor_copy(out=rb0b[:], in_=drbT[:, :, 0]), r=[drbT], w=[rb0b])
    dtmp = k.alloc("dtmp", [128, 8, 31], F32)
    k.op("dve", lambda e: e.tensor_tensor(out=dtmp[:], in0=drbT[:, :, 1:32], in1=drbT[:, :, 0:31], op=ALU.subtract), r=[drbT], w=[dtmp])
    pt_i = k.alloc("pt_i", [128, TS], I32)
    pt_f = k.alloc("pt_f", [128, TS], F32)
    k.dma("sp", pt_i[:], page_table.ap().rearrange("s p -> p s"), w=[pt_i], slow=True)
    k.op("dve", lambda e: e.tensor_copy(out=pt_f[:], in_=pt_i[:]), r=[pt_i], w=[pt_f])
    pt2f = k.alloc("pt2f", [128, TS, 2], F32)
    pt2i = k.alloc("pt2i", [128, TS, 2], I32)
    for hf_ in range(2):
        k.op("dve", lambda e, hf_=hf_: e.tensor_scalar(out=pt2f[:, :, hf_], in0=pt_f[:], scalar1=2.0, scalar2=float(hf_), op0=ALU.mult, op1=ALU.add), r=[pt_f], w=[pt2f])
    k.op("dve", lambda e: e.tensor_copy(out=pt2i[:], in_=pt2f[:]), r=[pt2f], w=[pt2i])
    k.op("dve", lambda e: e.tensor_scalar(out=pt_f[:], in0=pt_f[:], scalar1=128.0, scalar2=None, op0=ALU.mult), r=[pt_f, pt2f], w=[pt_f])
    qiS = k.alloc("qiS", [128, TS, 16], BF16)
    wS = k.alloc("wS", [128, TS, 16], F32)
    kiS = k.alloc("kiS", [128, TS], BF16)
    qbS = k.alloc("qbS", [128, 8, TS], BF16)
    knS = k.alloc("knS", [128, 2, TS], BF16)
    for s_i in range(TS):
        k.dma("sp", qiS[:, s_i, :], iq.ap()[:, :, T + s_i].rearrange("h d -> d h"), r=["plain"], w=[qiS], slow=True)
    k.dma("sp", wS[:].rearrange("p a b -> p (a b)"), iw[T:TT, :].rearrange("a b -> (a b)").unsqueeze(0).to_broadcast([128, TS * 16]), r=["iw"], w=[wS])
    k.dma("sp", kiS[:], ikTd[:, T:TT], r=["ikTd"], w=[kiS])
    k.dma("sp", qbS[:], aq.ap()[:, :, T:TT].rearrange("h d s -> d h s"), r=["plain"], w=[qbS])
    k.dma("sp", knS[:], akT.ap()[:, :, T:TT].rearrange("h d s -> d h s"), r=["plain"], w=[knS])
    scS = k.alloc("scS", [128, TS, 128], F32)
    snew = k.alloc("snew", [128, TS], F32)
    Gh = [k.alloc(f"Gh{i}", [128, 64 * 128], F32) for i in range(2)]
    ghc = [0]
    kTs = [k.alloc(f"kTs{i}", [128, 4, 128], BF16) for i in range(2)]
    rr = k.alloc("rr", [128, 32, 16], F32)
    knb = k.alloc("knb", [128, 128], BF16)
    ckx2 = cache_kidx.ap().rearrange("n (h e) -> (n h) e", h=2)

    def dma_raw(q, fn, r=(), w=()):
        i = k.drr[q]
        k.drr[q] = (i + 1) % len(k.dsem[q])
        sk = ("d", q, i)
        waits = k._collect(q, r, w)
        prev = k.dcnt[q][i]
        kn = k.known[q]
        if prev > 0 and kn.get(sk, 0) < prev:
            kn[sk] = prev
            waits.append((sk, prev))
        k.dcnt[q][i] = prev + 16
        tok = (sk, prev + 16)
        k.ops[q].append((waits, fn, (sk, 16)))
        k._update(tok, r, w)
        return tok

    def scores_seq(s_i):
        scb = [k.bank() for _ in range(4)]
        for hf_ in range(2):
            Gp = Gh[ghc[0] % 2]
            ghc[0] += 1
            dma_raw("pool", lambda e, Gp=Gp, hf_=hf_: e.indirect_dma_start(out=Gp[:], out_offset=None, in_=ckx2, in_offset=bass.IndirectOffsetOnAxis(ap=pt2i[:, s_i, hf_:hf_ + 1], axis=0)),
                    r=[pt2i], w=[Gp])
            k.reserved = {b_.name for b_ in scb}
            for sb in range(16):
                bt_ = k.bank()
                kt_ = kTs[sb % 2]
                for j in range(4):
                    sl_ = 4 * sb + j
                    k.op("pe", lambda e, j=j, sl_=sl_, bt_=bt_, Gp=Gp: e.transpose(out=bt_[:, j * 128:(j + 1) * 128], in_=Gp[:, sl_ * 128:(sl_ + 1) * 128], identity=ident_f[:]),
                         r=[Gp, ident_f], w=[bt_])
                k.op("act", lambda e, bt_=bt_, kt_=kt_: e.copy(out=kt_[:].rearrange("p a b -> p (a b)"), in_=bt_[:, :]), r=[bt_], w=[kt_])
                for j in range(4):
                    sl_ = hf_ * 64 + 4 * sb + j
                    sbk = scb[sl_ // 32]
                    k.op("pe", lambda e, j=j, sl_=sl_, sbk=sbk, kt_=kt_: e.matmul(sbk[:, (sl_ % 32) * 16:(sl_ % 32 + 1) * 16], lhsT=kt_[:, j, :], rhs=qiS[:, s_i, :], start=True, stop=True),
                         r=[kt_, qiS], w=[sbk])
        k.reserved = set()
        for b_i in range(4):
            sbk = scb[b_i]
            k.op("dve", lambda e, sbk=sbk: e.tensor_scalar(out=rr[:].rearrange("p a b -> p (a b)"), in0=sbk[:, :], scalar1=0.0, scalar2=None, op0=ALU.max), r=[sbk], w=[rr])
            k.op("dve", lambda e: e.tensor_tensor(out=rr[:], in0=rr[:], in1=wS[:, s_i, :].unsqueeze(1).to_broadcast([128, 32, 16]), op=ALU.mult), r=[rr, wS], w=[rr])
            k.op("dve", lambda e, b_i=b_i: e.tensor_reduce(out=scS[:, s_i, b_i * 32:(b_i + 1) * 32], in_=rr[:], axis=AX.X, op=ALU.add), r=[rr], w=[scS])
        k.op("dve", lambda e: e.tensor_copy(out=knb[:], in_=kiS[:, s_i:s_i + 1].to_broadcast([128, 128])), r=[kiS], w=[knb])
        bn = k.bank()
        k.op("pe", lambda e: e.matmul(bn[:, 0:16], lhsT=knb[:], rhs=qiS[:, s_i, :], start=True, stop=True), r=[knb, qiS], w=[bn])
        k.op("dve", lambda e: e.tensor_scalar(out=rr[:, 0, :], in0=bn[:, 0:16], scalar1=0.0, scalar2=None, op0=ALU.max), r=[bn], w=[rr])
        k.op("dve", lambda e: e.tensor_tensor(out=rr[:, 0, :], in0=rr[:, 0, :], in1=wS[:, s_i, :], op=ALU.mult), r=[rr, wS], w=[rr])
        k.op("dve", lambda e: e.tensor_reduce(out=snew[:, s_i:s_i + 1], in_=rr[:, 0, :], axis=AX.X, op=ALU.add), r=[rr], w=[snew])

    for s_i in range(0 if skip_p4s else TS):
        scores_seq(s_i)

    pm = k.alloc("pm", [128, 2 * TS], F32)
    gmm = k.alloc("gmm", [TS, 4], F32)
    dgm = k.alloc("dgm", [TS, 2 * TS], F32)
    lo_ = k.alloc("lo_", [128, TS], F32)
    w0_ = k.alloc("w0_", [128, TS], F32)
    bsS = k.alloc("bsS", [128, 6 * TS], F32)
    cmpb = k.alloc("cmpb", [128, TS, 128], F32)
    k.op("dve", lambda e: e.tensor_reduce(out=pm[:, 0:TS], in_=scS[:], axis=AX.X, op=ALU.max), r=[scS], w=[pm])
    k.op("dve", lambda e: e.tensor_reduce(out=pm[:, TS:2 * TS], in_=scS[:], axis=AX.X, op=ALU.min), r=[scS, pm], w=[pm])
    k.op("dve", lambda e: e.tensor_tensor(out=pm[:, 0:TS], in0=pm[:, 0:TS], in1=snew[:], op=ALU.max), r=[pm, snew], w=[pm])
    k.op("dve", lambda e: e.tensor_tensor(out=pm[:, TS:2 * TS], in0=pm[:, TS:2 * TS], in1=snew[:], op=ALU.min), r=[pm, snew], w=[pm])
    bmx = k.bank()
    bmn = k.bank()
    k.op("pe", lambda e: e.transpose(out=bmx[0:TS, 0:128], in_=pm[:, 0:TS], identity=ident_f[:]), r=[pm, ident_f], w=[bmx])
    k.op("pe", lambda e: e.transpose(out=bmn[0:TS, 0:128], in_=pm[:, TS:2 * TS], identity=ident_f[:]), r=[pm, ident_f], w=[bmn])
    k.op("dve", lambda e: e.tensor_reduce(out=gmm[:, 0:1], in_=bmx[0:TS, 0:128], axis=AX.X, op=ALU.max), r=[bmx], w=[gmm])
    k.op("dve", lambda e: e.tensor_reduce(out=gmm[:, 1:2], in_=bmn[0:TS, 0:128], axis=AX.X, op=ALU.min), r=[bmn, gmm], w=[gmm])
    k.op("dve", lambda e: e.tensor_scalar(out=gmm[:, 1:2], in0=gmm[:, 1:2], scalar1=-1.0, scalar2=None, op0=ALU.add), r=[gmm], w=[gmm])
    k.op("dve", lambda e: e.tensor_tensor(out=gmm[:, 2:3], in0=gmm[:, 0:1], in1=gmm[:, 1:2], op=ALU.subtract), r=[gmm], w=[gmm])
    k.op("dve", lambda e: e.tensor_scalar(out=dgm[:, 0:TS], in0=ident_f[0:TS, 0:TS], scalar1=gmm[:, 1:2], scalar2=None, op0=ALU.mult), r=[gmm, ident_f], w=[dgm])
    k.op("dve", lambda e: e.tensor_scalar(out=dgm[:, TS:2 * TS], in0=ident_f[0:TS, 0:TS], scalar1=gmm[:, 2:3], scalar2=None, op0=ALU.mult), r=[gmm, ident_f, dgm], w=[dgm])
    bbc = k.bank()
    k.op("pe", lambda e: e.matmul(bbc[:, 0:2 * TS], lhsT=ones4[0:TS, :], rhs=dgm[:], start=True, stop=True), r=[ones4, dgm], w=[bbc])
    k.op("act", lambda e: e.copy(out=lo_[:], in_=bbc[:, 0:TS]), r=[bbc], w=[lo_])
    k.op("act", lambda e: e.copy(out=w0_[:], in_=bbc[:, TS:2 * TS]), r=[bbc], w=[w0_])
    mid_ = bsS[:, 0:TS]
    cnt_ = bsS[:, TS:2 * TS]
    gn_ = bsS[:, 2 * TS:3 * TS]
    tot_ = bsS[:, 3 * TS:4 * TS]
    ge_ = bsS[:, 4 * TS:5 * TS]

    def bis_iter(it):
        f = 2.0 ** -(it + 1)
        k.op("dve", lambda e: e.scalar_tensor_tensor(out=mid_, in0=w0_[:], scalar=f, op0=ALU.mult, in1=lo_[:], op1=ALU.add), r=[w0_, lo_], w=[bsS])
        k.op("dve", lambda e: e.tensor_tensor(out=cmpb[:], in0=scS[:], in1=mid_.unsqueeze(2).to_broadcast([128, TS, 128]), op=ALU.is_gt), r=[scS, bsS], w=[cmpb])
        k.op("dve", lambda e: e.tensor_reduce(out=cnt_, in_=cmpb[:], axis=AX.X, op=ALU.add), r=[cmpb, bsS], w=[bsS])
        bc_ = k.bank()
        k.op("pe", lambda e: e.matmul(bc_[:, 0:TS], lhsT=ones4[:], rhs=cnt_, start=True, stop=True), r=[ones4, bsS], w=[bc_])
        k.op("dve", lambda e: e.tensor_tensor(out=gn_, in0=snew[:], in1=mid_, op=ALU.is_gt), r=[snew, bsS], w=[bsS])
        k.op("dve", lambda e: e.tensor_tensor(out=tot_, in0=bc_[:, 0:TS], in1=gn_, op=ALU.add), r=[bc_, bsS], w=[bsS])
        k.op("dve", lambda e: e.tensor_scalar(out=ge_, in0=tot_, scalar1=255.5, scalar2=f, op0=ALU.is_gt, op1=ALU.mult), r=[bsS], w=[bsS])
        k.op("dve", lambda e: e.tensor_tensor(out=ge_, in0=ge_, in1=w0_[:], op=ALU.mult), r=[bsS, w0_], w=[bsS])
        k.op("dve", lambda e: e.tensor_tensor(out=lo_[:], in0=lo_[:], in1=ge_, op=ALU.add), r=[lo_, bsS], w=[lo_])

    for it in range(0 if skip_p4s else 20):
        bis_iter(it)

    Msel = k.alloc("Msel", [128, 128], F32)
    Mb = k.alloc("Mb", [128, 128], BF16)
    Bs = k.alloc("Bs", [128, 128], F32)
    cum = k.alloc("cum", [128, 128], F32)
    rank = k.alloc("rank", [128, 128], F32)
    payl = k.alloc("payl", [128, 128, 2], F32)
    Soh = [k.alloc(f"Soh{i}", [128, 256], F32) for i in range(2)]
    idxf = k.alloc("idxf", [128, 4], F32)
    idx_i = k.alloc("idx_i", [128, 2], I32)
    Ksel = k.alloc("Ksel", [128, 2, 256], F32)
    Vsel = k.alloc("Vsel", [128, 2, 256], F32)
    KTs = k.alloc("KTs", [128, 4, 128], F32)
    qf = k.alloc("qf", [128, 8], F32)
    knf = k.alloc("knf", [128, 2], F32)
    sm = k.alloc("sm", [128, 64], F32)
    ind = k.alloc("ind", [128, 2, 31], F32)
    prod = k.alloc("prod", [128, 2, 8, 31], F32)
    Eg = k.alloc("Eg", [128, 2, 8], F32)
    rowp = k.alloc("rowp", [1, 64], F32)
    vnr = k.alloc("vnr", [1, 256], BF16)
    vnf = k.alloc("vnf", [1, 256], F32)
    osb = k.alloc("osb", [4, 2, 130], F32)
    ck2 = cache_k.ap()
    cv2 = cache_v.ap()
    k.op("dve", lambda e: e.tensor_copy(out=payl[:, :, 1], in_=posc[:]), r=[posc], w=[payl])

    def attend_seq(s_i):
        col = T + s_i
        k.op("dve", lambda e: e.tensor_scalar(out=Msel[:], in0=scS[:, s_i, :], scalar1=lo_[:, s_i:s_i + 1], scalar2=None, op0=ALU.is_gt), r=[scS, lo_], w=[Msel])
        k.op("dve", lambda e: e.tensor_copy(out=Mb[:], in_=Msel[:]), r=[Msel], w=[Mb])
        k.op("dve", lambda e: e.tensor_tensor(out=sm[:, 0:1], in0=snew[:, s_i:s_i + 1], in1=lo_[:, s_i:s_i + 1], op=ALU.is_gt), r=[snew, lo_], w=[sm])
        bA_ = k.bank()
        bB_ = k.bank()
        k.op("pe", lambda e: e.matmul(bA_[:, 0:128], lhsT=Ltri[:], rhs=Mb[:], start=True, stop=True), r=[Ltri, Mb], w=[bA_])
        k.op("pe", lambda e: e.matmul(bB_[:, 0:128], lhsT=ones_b[:], rhs=Mb[:], start=True, stop=True), r=[ones_b, Mb], w=[bB_])
        k.op("act", lambda e: e.copy(out=Bs[:], in_=bB_[:, 0:128]), r=[bB_], w=[Bs])
        k.op("dve", lambda e: e.tensor_tensor_scan(out=cum[:], data0=Bs[:], data1=zeros4[:], initial=0.0, op0=ALU.add, op1=ALU.add), r=[Bs, zeros4], w=[cum])
        k.op("dve", lambda e: e.tensor_tensor(out=rank[:], in0=cum[:], in1=Bs[:], op=ALU.subtract), r=[cum, Bs], w=[rank])
        k.op("dve", lambda e: e.tensor_tensor(out=rank[:], in0=rank[:], in1=bA_[:, 0:128], op=ALU.add), r=[rank, bA_], w=[rank])
        k.op("dve", lambda e: e.scalar_tensor_tensor(out=rank[:], in0=rank[:], scalar=1.0, op0=ALU.add, in1=Msel[:], op1=ALU.mult), r=[rank, Msel], w=[rank])
        k.op("dve", lambda e: e.tensor_scalar(out=rank[:], in0=rank[:], scalar1=-1.0, scalar2=None, op0=ALU.add), r=[rank], w=[rank])
        k.op("dve", lambda e: e.tensor_scalar(out=payl[:, :, 0], in0=siota[:], scalar1=pt_f[:, s_i:s_i + 1], scalar2=None, op0=ALU.add), r=[siota, pt_f], w=[payl])
        bacc = k.bank()
        k.reserved = {bacc.name}
        k.op("pe", lambda e: e.matmul(bacc[:, 0:4], lhsT=zeros4[:], rhs=ones4[:, 0:4], start=True, stop=False), r=[zeros4, ones4], w=[bacc])
        for sl_ in range(128):
            so_ = Soh[sl_ % 2]
            k.op("dve", lambda e, sl_=sl_, so_=so_: e.tensor_scalar(out=so_[:], in0=jrow[:], scalar1=rank[:, sl_:sl_ + 1], scalar2=None, op0=ALU.is_equal), r=[jrow, rank], w=[so_])
            for half in range(2):
                k.op("pe", lambda e, sl_=sl_, so_=so_, half=half: e.matmul(bacc[:, half * 2:(half + 1) * 2], lhsT=so_[:, half * 128:(half + 1) * 128], rhs=payl[:, sl_, :],
                                                                      start=False, stop=(sl_ == 127)), r=[so_, payl], w=[bacc])
        k.reserved = set()
        k.op("act", lambda e: e.copy(out=idxf[:], in_=bacc[:, 0:4]), r=[bacc], w=[idxf])
        k.op("dve", lambda e: e.tensor_copy(out=idx_i[:], in_=idxf[:].rearrange("p (a b) -> p a b", b=2)[:, :, 0]), r=[idxf], w=[idx_i])
        for half in range(2):
            dma_raw("pool", lambda e, half=half: e.indirect_dma_start(out=Ksel[:, half, :], out_offset=None, in_=ck2, in_offset=bass.IndirectOffsetOnAxis(ap=idx_i[:, half:half + 1], axis=0)),
                    r=[idx_i], w=[Ksel])
            dma_raw("pool", lambda e, half=half: e.indirect_dma_start(out=Vsel[:, half, :], out_offset=None, in_=cv2, in_offset=bass.IndirectOffsetOnAxis(ap=idx_i[:, half:half + 1], axis=0)),
                    r=[idx_i], w=[Vsel])
        bkt = k.bank()
        for half in range(2):
            for kvh in range(2):
                jj = half * 2 + kvh
                k.op("pe", lambda e, half=half, kvh=kvh, jj=jj: e.transpose(out=bkt[:, jj * 128:(jj + 1) * 128], in_=Ksel[:, half, kvh * 128:(kvh + 1) * 128], identity=ident_f[:]),
                     r=[Ksel, ident_f], w=[bkt])
        k.op("act", lambda e: e.copy(out=KTs[:].rearrange("p a b -> p (a b)"), in_=bkt[:, :]), r=[bkt], w=[KTs])
        k.op("dve", lambda e: e.tensor_copy(out=qf[:], in_=qbS[:, :, s_i]), r=[qbS], w=[qf])
        k.op("dve", lambda e: e.tensor_copy(out=knf[:], in_=knS[:, :, s_i]), r=[knS], w=[knf])
        blg = k.bank()
        for half in range(2):
            for kvh in range(2):
                jj = half * 2 + kvh
                k.op("pe", lambda e, half=half, kvh=kvh, jj=jj: e.matmul(blg[:, half * 8 + kvh * 4:half * 8 + kvh * 4 + 4], lhsT=KTs[:, jj, :], rhs=qf[:, kvh * 4:(kvh + 1) * 4], start=True, stop=True),
                     r=[KTs, qf], w=[blg])
        bln = k.bank()
        for kvh in range(2):
            k.op("pe", lambda e, kvh=kvh: e.matmul(bln[0:1, kvh * 4:(kvh + 1) * 4], lhsT=knf[:, kvh:kvh + 1], rhs=qf[:, kvh * 4:(kvh + 1) * 4], start=True, stop=True), r=[knf, qf], w=[bln])
        k.op("dve", lambda e: e.tensor_scalar(out=sm[:, 2:4], in0=idxf[:].rearrange("p (a b) -> p a b", b=2)[:, :, 1], scalar1=-1.0, scalar2=float(NPG * 128), op0=ALU.mult, op1=ALU.add), r=[idxf], w=[sm])
        k.op("dve", lambda e: e.tensor_tensor(out=ind[:], in0=sm[:, 2:4].unsqueeze(2).to_broadcast([128, 2, 31]), in1=thrb[:].unsqueeze(1).to_broadcast([128, 2, 31]), op=ALU.is_ge), r=[sm, thrb], w=[ind])
        k.op("dve", lambda e: e.tensor_tensor(out=prod[:], in0=ind[:].unsqueeze(2).to_broadcast([128, 2, 8, 31]), in1=dtmp[:].unsqueeze(1).to_broadcast([128, 2, 8, 31]), op=ALU.mult), r=[ind, dtmp], w=[prod])
        k.op("dve", lambda e: e.tensor_reduce(out=Eg[:], in_=prod[:], axis=AX.X, op=ALU.add), r=[prod], w=[Eg])
        k.op("dve", lambda e: e.tensor_tensor(out=Eg[:], in0=Eg[:], in1=rb0b[:].unsqueeze(1).to_broadcast([128, 2, 8]), op=ALU.add), r=[Eg, rb0b], w=[Eg])
        k.op("dve", lambda e: e.tensor_scalar(out=sm[:, 4:6], in0=jcol[:], scalar1=cum[:, 127:128], scalar2=None, op0=ALU.is_lt), r=[jcol, cum, sm], w=[sm])
        k.op("dve", lambda e: e.tensor_scalar(out=sm[:, 4:6], in0=sm[:, 4:6], scalar1=-1.0, scalar2=30000.0, op0=ALU.add, op1=ALU.mult), r=[sm], w=[sm])
        k.op("dve", lambda e: e.tensor_tensor(out=Eg[:], in0=Eg[:], in1=sm[:, 4:6].unsqueeze(2).to_broadcast([128, 2, 8]), op=ALU.add), r=[Eg, sm], w=[Eg])
        k.op("dve", lambda e: e.tensor_tensor(out=Eg[:].rearrange("p a b -> p (a b)"), in0=Eg[:].rearrange("p a b -> p (a b)"), in1=blg[:, 0:16], op=ALU.add), r=[Eg, blg], w=[Eg])
        k.op("act", lambda e: e.activation(out=Eg[:], in_=Eg[:], func=AF.Exp), r=[Eg], w=[Eg])
        k.op("dve", lambda e: e.tensor_scalar(out=rowp[:, 8:9], in0=sm[0:1, 0:1], scalar1=-1.0, scalar2=30000.0, op0=ALU.add, op1=ALU.mult), r=[sm], w=[rowp])
        k.op("dve", lambda e: e.tensor_tensor(out=rowp[:, 0:8], in0=bln[0:1, 0:8], in1=rb0b[0:1, :], op=ALU.add), r=[bln, rb0b, rowp], w=[rowp])
        k.op("dve", lambda e: e.tensor_scalar(out=rowp[:, 0:8], in0=rowp[:, 0:8], scalar1=rowp[:, 8:9], scalar2=None, op0=ALU.add), r=[rowp], w=[rowp])
        k.op("act", lambda e: e.activation(out=rowp[:, 0:8], in_=rowp[:, 0:8], func=AF.Exp), r=[rowp], w=[rowp])
        k.dma("sp", vnr[:], av[col:col + 1, :], r=["av"], w=[vnr])
        k.op("dve", lambda e: e.tensor_copy(out=vnf[:], in_=vnr[:]), r=[vnr], w=[vnf])
        for kvh in range(2):
            bon = k.bank()
            bod = k.bank()
            for half in range(2):
                k.op("pe", lambda e, kvh=kvh, half=half, bon=bon: e.matmul(bon[0:4, 0:128], lhsT=Eg[:, half, kvh * 4:(kvh + 1) * 4], rhs=Vsel[:, half, kvh * 128:(kvh + 1) * 128], start=(half == 0), stop=False),
                     r=[Eg, Vsel], w=[bon])
            k.op("pe", lambda e, kvh=kvh, bon=bon: e.matmul(bon[0:4, 0:128], lhsT=rowp[0:1, kvh * 4:(kvh + 1) * 4], rhs=vnf[0:1, kvh * 128:(kvh + 1) * 128], start=False, stop=True), r=[rowp, vnf], w=[bon])
            for half in range(2):
                k.op("pe", lambda e, kvh=kvh, half=half, bod=bod: e.matmul(bod[0:4, 0:1], lhsT=Eg[:, half, kvh * 4:(kvh + 1) * 4], rhs=ones4[:, 0:1], start=(half == 0), stop=False), r=[Eg, ones4], w=[bod])
            k.op("pe", lambda e, kvh=kvh, bod=bod: e.matmul(bod[0:4, 0:1], lhsT=rowp[0:1, kvh * 4:(kvh + 1) * 4], rhs=ones4[0:1, 0:1], start=False, stop=True), r=[rowp, ones4], w=[bod])
            k.op("dve", lambda e, kvh=kvh, bod=bod: e.reciprocal(out=osb[:, kvh, 128:129], in_=bod[0:4, 0:1]), r=[bod], w=[osb])
            k.op("dve", lambda e, kvh=kvh, bon=bon: e.tensor_scalar(out=osb[:, kvh, 0:128], in0=bon[0:4, 0:128], scalar1=osb[:, kvh, 128:129], scalar2=None, op0=ALU.mult), r=[bon, osb], w=[osb])
        bot = k.bank()
        for kvh in range(2):
            k.op("pe", lambda e, kvh=kvh: e.transpose(out=bot[:, kvh * 4:(kvh + 1) * 4], in_=osb[:, kvh, 0:128], identity=ident_f[0:4, 0:4]), r=[osb, ident_f], w=[bot])
        k.op("act", lambda e: e.copy(out=mixT[:, 8:16, col], in_=bot[:, 0:8]), r=[bot], w=[(mixT, "sb")])

    for s_i in range(0 if skip_p4s else TS):
        attend_seq(s_i)
    k.barrier()
    k.release(m4)
    m5 = k.mark()
    wout = k.alloc("wout", [128, 16, D], BF16)
    for g in range(4):
        k.dma("pool", wout[:, :, g * 512:(g + 1) * 512], w_out[:, g * 512:(g + 1) * 512].rearrange("(k p) n -> p k n", p=128), w=[(wout, g)])
    G1b = k.alloc("G1b", [128, D], BF16)
    A2b = k.alloc("A2b", [128, D], BF16)
    SH2b = k.alloc("SH2b", [128, D], BF16)
    for buf_, idx_ in ((G1b, 2), (SH2b, 3), (A2b, 4)):
        k.dma("pool", buf_[:], modd[0:1, idx_ * D:(idx_ + 1) * D].to_broadcast([128, D]), r=["modd"], w=[buf_])
    xt5 = [k.alloc(f"xt5_{i}", [128, D], F32) for i in range(2)]
    mos = [k.alloc(f"mo{i}", [128, D], F32) for i in range(2)]
    h2b = k.alloc("h2b", [128, D], BF16)
    jnk5 = k.alloc("jnk5", [128, D], BF16)
    st5 = [k.alloc(f"st5_{i}", [128, 8], F32) for i in range(2)]
    h2st = [k.alloc("h2st0", [128, 16, 128], BF16)] * 2

    def p5_tile(ti):
        c0, n = (ti * 128, 128) if ti < NT else (T, TS)
        x_ = xt5[ti % 2]
        mo = mos[ti % 2]
        s_ = st5[ti % 2]
        hs_ = h2st[ti % 2]
        G1_, A2_, SH2_ = (G1b, A2b, SH2b)
        if ti == NT:
            k.dma("pool", G1b[0:TS, :], modd[1:5, 2 * D:3 * D], r=["modd"], w=[G1b])
            k.dma("pool", SH2b[0:TS, :], modd[1:5, 3 * D:4 * D], r=["modd"], w=[SH2b])
            k.dma("pool", A2b[0:TS, :], modd[1:5, 4 * D:5 * D], r=["modd"], w=[A2b])
        if ti == 0:
            k.dma("sp", x_[0:n, :], xp[c0:c0 + n, :], w=[x_])
        if ti + 1 <= NT:
            tn = ti + 1
            cn, nn = (tn * 128, 128) if tn < NT else (T, TS)
            xn_ = xt5[tn % 2]
            k.dma("sp", xn_[0:nn, :], xp[cn:cn + nn, :] if tn < NT else xs.ap(), w=[xn_])
        mkeys = [(mixT, ti), (mixT, "b%d" % ti)] if ti < NT else [(mixT, "s%d" % j) for j in range(TS)] + [(mixT, "sb")]
        for nq in range(4):
            bk_ = k.bank()
            for kk in range(16):
                k.op("pe", lambda e, kk=kk, bk_=bk_, nq=nq: e.matmul(bk_[0:n, :], lhsT=mixT[:, kk, c0:c0 + n], rhs=wout[:, kk, nq * 512:(nq + 1) * 512], start=(kk == 0), stop=(kk == 15)),
                     r=mkeys + [(wout, nq)], w=[bk_])
            k.op("act", lambda e, bk_=bk_, nq=nq: e.copy(out=mo[0:n, nq * 512:(nq + 1) * 512], in_=bk_[0:n, :]), r=[bk_], w=[mo])
        k.op("act", lambda e: e.activation(out=jnk5[0:n, :], in_=mo[0:n, :], func=AF.Square, accum_out=s_[0:n, 0:1]), r=[mo], w=[jnk5, s_])
        k.op("act", lambda e: e.activation(out=s_[0:n, 1:2], in_=s_[0:n, 0:1], func=AF.Sqrt, scale=1.0 / D, bias=EPS), r=[s_], w=[s_])
        k.op("dve", lambda e: e.reciprocal(out=s_[0:n, 2:3], in_=s_[0:n, 1:2]), r=[s_], w=[s_])
        k.op("dve", lambda e: e.scalar_tensor_tensor(out=mo[0:n, :], in0=mo[0:n, :], scalar=s_[0:n, 2:3], op0=ALU.mult, in1=G1_[0:n, :], op1=ALU.mult), r=[mo, s_, G1_], w=[mo])
        k.op("dve", lambda e: e.tensor_tensor(out=x_[0:n, :], in0=x_[0:n, :], in1=mo[0:n, :], op=ALU.add), r=[x_, mo], w=[x_])
        k.dma("sp", x1d[c0:c0 + n, :], x_[0:n, :], r=[x_], w=["x1d"])
        k.op("act", lambda e: e.activation(out=jnk5[0:n, :], in_=x_[0:n, :], func=AF.Square, accum_out=s_[0:n, 3:4]), r=[x_, s_], w=[jnk5, s_])
        k.op("act", lambda e: e.activation(out=s_[0:n, 4:5], in_=s_[0:n, 3:4], func=AF.Sqrt, scale=1.0 / D, bias=EPS), r=[s_], w=[s_])
        k.op("dve", lambda e: e.reciprocal(out=s_[0:n, 5:6], in_=s_[0:n, 4:5]), r=[s_], w=[s_])
        k.op("dve", lambda e: e.scalar_tensor_tensor(out=mo[0:n, :], in0=x_[0:n, :], scalar=s_[0:n, 5:6], op0=ALU.mult, in1=A2_[0:n, :], op1=ALU.mult), r=[x_, s_, A2_, mo], w=[mo])
        k.op("dve", lambda e: e.tensor_tensor(out=h2b[0:n, :], in0=mo[0:n, :], in1=SH2_[0:n, :], op=ALU.add), r=[mo, SH2_], w=[h2b])
        b0 = k.bank()
        b1 = k.bank()
        for kk in range(16):
            bb = b0 if kk < 8 else b1
            k.op("pe", lambda e, kk=kk, bb=bb: e.transpose(out=bb[:].bitcast(BF16)[:, (kk % 8) * 128:(kk % 8) * 128 + n], in_=h2b[0:n, kk * 128:(kk + 1) * 128], identity=ident_b[0:n, 0:n]),
                 r=[h2b, ident_b], w=[bb])
        for half, bb in ((0, b0), (1, b1)):
            k.op("act", lambda e, half=half, bb=bb: e.copy(out=hs_[:, half * 8:(half + 1) * 8, 0:n], in_=bb[:].bitcast(BF16).rearrange("p (a b) -> p a b", a=8)[:, :, 0:n]),
                 r=[bb], w=[hs_])
        k.dma("sp", h2Td[:, :, c0:c0 + n], hs_[:, :, 0:n], r=[hs_], w=["h2Td"])

    for ti in range(NT + 1):
        p5_tile(ti)
    k.barrier()
    k.release(mP)
    if stop_after == "P5":
        return k.finish()

    alloc_wb()
    TB = 512
    NBLK6 = T // TB
    uT = k.alloc("uT", [128, 64, TB], BF16)
    uTs = k.alloc("uTs", [128, 64, TS], BF16)
    h2Tb = k.alloc("h2Tb", [128, 16, TB], BF16)
    h2Ts = k.alloc("h2Ts", [128, 16, TS], BF16)
    w2b = [k.alloc(f"w2b{i}", [128, 8, 512], BF16) for i in range(2)]
    fbuf = [k.alloc(f"fbuf{i}", [128, D], F32) for i in range(4)]
    x1t = [k.alloc("x1t0", [128, D], F32)] * 2
    G2b = k.alloc("G2b", [128, D], F32)
    load_mod_bcast(G2b, 5)
    rl6 = [k.alloc(f"rl6_{i}", [128, TB], BF16) for i in range(2)]
    st6 = [k.alloc(f"st6_{i}", [128, 4], F32) for i in range(2)]
    w2cnt = [0]
    k.dma("sp", h2Ts[:], h2Td[:, :, T:TT], r=["h2Td"], w=[h2Ts])

    def ffn_block(b):
        with_s = (b == NBLK6 - 1)
        if b == 0:
            k.dma("sp", h2Tb[:], h2Td[:, :, b * TB:(b + 1) * TB], r=["h2Td"], w=[h2Tb])
        def phaseA(g):
            wt = wb[wcnt[0] % 2]
            wcnt[0] += 1
            k.dma("sp", wt[:], w1s[g], r=[("w1s", g)], w=[wt])
            for j in range(4):
                ch = g * 4 + j
                bk_ = k.bank()
                for kk in range(16):
                    k.op("pe", lambda e, kk=kk, bk_=bk_, j=j: e.matmul(bk_[:, :], lhsT=wt[:, kk, j * 128:(j + 1) * 128], rhs=h2Tb[:, kk, :], start=(kk == 0), stop=(kk == 15)),
                         r=[wt, h2Tb], w=[bk_])
                r_ = rl6[ch % 2]
                k.op("act", lambda e, bk_=bk_, r_=r_: e.activation(out=r_[:], in_=bk_[:, :], func=AF.Relu), r=[bk_], w=[r_])
                k.op("dve", lambda e, r_=r_, ch=ch: e.tensor_tensor(out=uT[:, ch, :], in0=r_[:], in1=r_[:], op=ALU.mult), r=[r_], w=[(uT, ch)])
                if with_s:
                    bs_ = k.bank()
                    for kk in range(16):
                        k.op("pe", lambda e, kk=kk, bs_=bs_, j=j: e.matmul(bs_[:, 0:TS], lhsT=wt[:, kk, j * 128:(j + 1) * 128], rhs=h2Ts[:, kk, :], start=(kk == 0), stop=(kk == 15)),
                             r=[wt, h2Ts], w=[bs_])
                    k.op("act", lambda e, bs_=bs_, ch=ch: e.activation(out=uTs[:, ch, :], in_=bs_[:, 0:TS], func=AF.Relu), r=[bs_], w=[(uTs, ch)])
                    k.op("dve", lambda e, ch=ch: e.tensor_tensor(out=uTs[:, ch, :], in0=uTs[:, ch, :], in1=uTs[:, ch, :], op=ALU.mult), r=[(uTs, ch)], w=[(uTs, ch)])
        for g in range(16):
            phaseA(g)
        def phaseB(qc):
            accs = [k.bank() for _ in range(4)]
            accS = k.bank() if with_s else None
            k.reserved = {a_.name for a_ in accs} | ({accS.name} if with_s else set())
            for fgg in range(8):
                w2 = w2b[w2cnt[0] % 2]
                w2cnt[0] += 1
                k.dma("pool", w2[:], w2s[qc, fgg], r=[("w2s", qc, fgg)], w=[w2])
                for c in range(8):
                    ch = fgg * 8 + c
                    first = (fgg == 0 and c == 0)
                    last = (fgg == 7 and c == 7)
                    for tt in range(4):
                        k.op("pe", lambda e, tt=tt, c=c, ch=ch, first=first, last=last, w2=w2: e.matmul(accs[tt][:, :], lhsT=uT[:, ch, tt * 128:(tt + 1) * 128], rhs=w2[:, c, :], start=first, stop=last),
                             r=[(uT, ch), w2], w=[accs[tt]])
                    if with_s:
                        k.op("pe", lambda e, c=c, ch=ch, first=first, last=last, w2=w2: e.matmul(accS[0:TS, :], lhsT=uTs[:, ch, :], rhs=w2[:, c, :], start=first, stop=last),
                             r=[(uTs, ch), w2], w=[accS])
            for tt in range(4):
                k.op("act", lambda e, tt=tt: e.copy(out=fbuf[tt][:, qc * 512:(qc + 1) * 512], in_=accs[tt][:, :]), r=[accs[tt]], w=[(fbuf[tt], qc)])
            if with_s:
                k.op("act", lambda e: e.copy(out=fs[0:TS, qc * 512:(qc + 1) * 512], in_=accS[0:TS, :]), r=[accS], w=[(fs, qc)])
            k.reserved = set()
        for qc in range(4):
            phaseB(qc)
        if b + 1 < NBLK6:
            k.dma("sp", h2Tb[:], h2Td[:, :, (b + 1) * TB:(b + 2) * TB], r=["h2Td"], w=[h2Tb])
        def epi(fb, n, x1src, ydst, G2_, i):
            x_ = x1t[i % 2]
            s_ = st6[i % 2]
            k.dma("pool", x_[0:n, :], x1src, r=["x1d"], w=[x_])
            fk = [(fb, q) for q in range(4)]
            k.op("act", lambda e: e.activation(out=w2b[0][:].rearrange("p a b -> p (a b)")[0:n, 0:D], in_=fb[0:n, :], func=AF.Square, accum_out=s_[0:n, 0:1]), r=fk, w=[w2b[0], s_])
            k.op("act", lambda e: e.activation(out=s_[0:n, 1:2], in_=s_[0:n, 0:1], func=AF.Sqrt, scale=1.0 / D, bias=EPS), r=[s_], w=[s_])
            k.op("dve", lambda e: e.reciprocal(out=s_[0:n, 2:3], in_=s_[0:n, 1:2]), r=[s_], w=[s_])
            k.op("dve", lambda e: e.scalar_tensor_tensor(out=fb[0:n, :], in0=fb[0:n, :], scalar=s_[0:n, 2:3], op0=ALU.mult, in1=G2_[0:n, :], op1=ALU.mult), r=fk + [s_, G2_], w=fk)
            k.op("dve", lambda e: e.tensor_tensor(out=x_[0:n, :], in0=x_[0:n, :], in1=fb[0:n, :], op=ALU.add), r=[x_] + fk, w=[x_])
            k.dma("pool", ydst, x_[0:n, :], r=[x_])
        for tt in range(4):
            r0 = b * TB + tt * 128
            epi(fbuf[tt], 128, x1d[r0:r0 + 128, :], y_p[r0:r0 + 128, :], G2b, tt)
        if with_s:
            k.dma("pool", G2b[0:TS, :], modd[1:5, 5 * D:6 * D], r=["modd"], w=[G2b])
            epi(fs, TS, x1d[T:TT, :], y_s.ap(), G2b, 0)

    fs = k.alloc("fs", [TS, D], F32)
    import os
    for b in range(NBLK6):
        ffn_block(b)
    k.barrier()
    return k.finish()


_CACHE = {}


def _core_inputs(i, a):
    f = np.ascontiguousarray
    return {
        "xp": f(a["x_prompt"][i]),
        "xs": f(a["x_sample"][4 * i:4 * i + 4, 0, :]),
        "c5": f(np.concatenate([a["c_prompt"][i:i + 1], a["c_sample"][4 * i:4 * i + 4]], axis=0)),
        "w_ada": f(a["w_ada"][0]),
        "b_ada": f(a["b_ada"][0][None, :]),
        "gvec": f(np.stack([a["pre1_g"][0], a["post1_g"][0], a["pre2_g"][0], a["post2_g"][0]])),
        "w_in": f(a["w_in"][0]),
        "w_out": f(a["w_out"][0]),
        "w_ff1": f(a["w_ff1"][0]),
        "w_ff2": f(a["w_ff2"][0]),
        "conv_w": f(a["conv_w"][0]),
        "st_conv": f(a["state_conv"][0, 4 * i:4 * i + 4].reshape(12, 3072)),
        "hv": f(np.concatenate([a["a_log"][0], a["dt_bias"][0]])[None, :]),
        "ln_gb": f(np.stack([a["idx_knorm_g"][0], a["idx_knorm_b"][0]])),
        "gdn_g": f(a["gdn_norm_g"][0][None, :]),
        "st_ssm": f(a["state_ssm"][0, 4 * i:4 * i + 4]),
        "rel_bias": f(a["rel_bias"]),
        "boh": _boh(),
        "bthr": _bthr(),
        "page_table": f(a["page_table"][4 * i:4 * i + 4]),
        "cache_kidx": a["cache_kidx"][0].reshape(-1, PAGE * 128),
        "cache_k": a["cache_k"][0].reshape(-1, 256),
        "cache_v": a["cache_v"][0].reshape(-1, 256),
    }


def kernel(**inputs):
    n = 8
    nc = build(n_pool=int(inputs["cache_k"].shape[1]))
    in_maps = [_core_inputs(i, inputs) for i in range(n)]
    res = run_bass_kernel_spmd(nc, in_maps, core_ids=list(range(n)))
    R = res.results
    cat = lambda name: np.stack([r[name] for r in R])
    y_p = cat("y_p")
    y_s = np.concatenate([r["y_s"] for r in R])[:, None, :]
    k_p = cat("k_p").reshape(1, 8, T, 2, 128)
    v_p = cat("v_p").reshape(1, 8, T, 2, 128)
    ki_p = cat("ki_p")[None]
    ssm_p = cat("ssm_p")[None]
    conv_p = cat("conv_p")[None]
    k_s = np.concatenate([r["k_s"] for r in R]).reshape(1, 32, 1, 2, 128)
    v_s = np.concatenate([r["v_s"] for r in R]).reshape(1, 32, 1, 2, 128)
    ki_s = np.concatenate([r["ki_s"] for r in R]).reshape(1, 32, 1, 128)
    ssm_s = np.concatenate([r["ssm_s"] for r in R])[None]
    conv_s = np.concatenate([r["conv_s"] for r in R])[None]
    return (y_p, y_s, k_p, v_p, ki_p, ssm_p, conv_p, k_s, v_s, ki_s, ssm_s, conv_s)
```

```python
import math
import numpy as np
import concourse.bass as bass
import concourse.mybir as mybir
from concourse.bass_utils import run_bass_kernel_spmd

F32 = mybir.dt.float32
BF16 = mybir.dt.bfloat16
I32 = mybir.dt.int32
ALU = mybir.AluOpType
AF = mybir.ActivationFunctionType
AX = mybir.AxisListType

ENGS = ("pe", "dve", "act", "pool", "sp")

D = 2048
T = 2048
TS = 4
TT = T + TS
NT = 16
NPROJ = 7840
DFF = 8192
EPS = 1e-6
NPAGES = 128
PAGE = 128
O_CONV, O_A, O_B, O_Z, O_QB, O_KB, O_VB, O_QI, O_WI, O_KI = 0, 3072, 3080, 3088, 4112, 5136, 5392, 5648, 7696, 7712


def _dsize(dt):
    return {F32: 4, BF16: 2, I32: 4}[dt]


class Buf:
    def __init__(self, name, ap):
        self.name = name
        self.ap = ap

    def __getitem__(self, key):
        return self.ap[key]


class KB:
    def __init__(self, n_dma_sems=(24, 8, 52)):
        self.nc = bass.Bass("TRN2", target_bir_lowering=False)
        nc = self.nc
        self.ops = {e: [] for e in ENGS}
        self._ctx = []
        self.psem = {}
        self.cnt = {e: 0 for e in ENGS}
        for e in ENGS:
            self.psem[e] = self._enter(nc.semaphore("p_" + e))
        self.dsem = {}
        self.dcnt = {}
        self.drr = {}
        for q, n in zip(("sp", "act", "pool"), n_dma_sems):
            self.dsem[q] = [self._enter(nc.semaphore(f"d_{q}{i}")) for i in range(n)]
            self.dcnt[q] = [0] * n
            self.drr[q] = 0
        self.known = {e: {} for e in ENGS}
        self.state = {}
        self.semobj = {}
        for e in ENGS:
            self.semobj[("p", e)] = self.psem[e]
        for q in self.dsem:
            for i, s in enumerate(self.dsem[q]):
                self.semobj[("d", q, i)] = s
        self.n_ops = 0
        self.arena = None
        self.aoff = 0
        self.awords = 0
        self.nbank = 0
        self.reserved = set()

    def _enter(self, cm):
        v = cm.__enter__()
        self._ctx.append(cm)
        return v

    def init_arena(self, words):
        self.arena = self._enter(self.nc.sbuf_tensor("arena", [128, words], F32))
        self.awords = words
        self.aoff = 0

    def alloc(self, name, shape, dt=F32):
        p = shape[0]
        n = int(np.prod(shape[1:]))
        words = (n * _dsize(dt) + 3) // 4
        words = (words + 7) // 8 * 8
        assert self.aoff + words <= self.awords, f"arena overflow at {name}: {self.aoff + words} > {self.awords}"
        ap = self.arena[0:p, self.aoff:self.aoff + words]
        self.aoff += words
        if dt != F32:
            ap = ap.bitcast(dt)
        ap = ap[:, 0:n]
        if len(shape) == 3:
            ap = ap.rearrange("p (a b) -> p a b", a=shape[1])
        elif len(shape) == 4:
            ap = ap.rearrange("p (a b c) -> p a b c", a=shape[1], b=shape[2])
        return Buf(name, ap)

    def mark(self):
        return self.aoff

    def release(self, m):
        self.aoff = m

    def psum_init(self):
        self.pbanks = []
        for i in range(4):
            t = self._enter(self.nc.psum_tensor(f"pp{i}", [128, 1024], F32))
            self.pbanks.append(Buf(f"bank{2 * i}", t[:, 0:512]))
            self.pbanks.append(Buf(f"bank{2 * i + 1}", t[:, 512:1024]))
        self.pdbl = [self._dbl(i) for i in range(4)]

    def _dbl(self, i):
        return None

    def bank(self):
        while True:
            b = self.pbanks[self.nbank % 8]
            self.nbank += 1
            if b.name not in self.reserved:
                return b

    @staticmethod
    def _key(x):
        if isinstance(x, tuple):
            return (KB._key(x[0]),) + tuple(x[1:])
        if isinstance(x, str):
            return x
        return x.name

    def _collect(self, eng, r, w):
        need = {}
        own = ("p", eng)

        def add(tok):
            if tok is None:
                return
            sk, v = tok
            if sk == own and eng == "pe":
                return
            if need.get(sk, 0) < v:
                need[sk] = v

        for x in r:
            st = self.state.get(self._key(x))
            if st:
                add(st[0])
        for x in w:
            st = self.state.get(self._key(x))
            if st:
                add(st[0])
                for t in st[1]:
                    add(t)
        waits = []
        kn = self.known[eng]
        for sk, v in need.items():
            if kn.get(sk, 0) >= v:
                continue
            kn[sk] = v
            waits.append((sk, v))
        return waits

    def _update(self, tok, r, w):
        for x in w:
            self.state[self._key(x)] = [tok, []]
        for x in r:
            kk = self._key(x)
            st = self.state.get(kk)
            if st is None:
                st = self.state[kk] = [None, []]
            st[1].append(tok)
            if len(st[1]) > 24:
                best = {}
                for sk, v in st[1]:
                    if best.get(sk, 0) < v:
                        best[sk] = v
                st[1] = list(best.items())

    def op(self, eng, fn, r=(), w=()):
        waits = self._collect(eng, r, w)
        self.cnt[eng] += 1
        tok = (("p", eng), self.cnt[eng])
        self.ops[eng].append((waits, fn, (("p", eng), 1)))
        self._update(tok, r, w)
        self.n_ops += 1
        return tok

    def dma(self, q, out, in_, r=(), w=(), slow=False):
        if slow:
            fn = lambda e: e.dma_start(out=out, in_=in_, allow_slow_non_contiguous=True)
        else:
            fn = lambda e: e.dma_start(out=out, in_=in_)
        i = self.drr[q]
        self.drr[q] = (i + 1) % len(self.dsem[q])
        sk = ("d", q, i)
        waits = self._collect(q, r, w)
        prev = self.dcnt[q][i]
        kn = self.known[q]
        if prev > 0 and kn.get(sk, 0) < prev:
            kn[sk] = prev
            waits.append((sk, prev))
        self.dcnt[q][i] = prev + 16
        tok = (sk, prev + 16)
        self.ops[q].append((waits, fn, (sk, 16)))
        self._update(tok, r, w)
        self.n_ops += 1
        return tok

    def barrier(self):
        toks = [(("p", e), self.cnt[e]) for e in ENGS if self.cnt[e] > 0]
        for q in self.dsem:
            for i, c in enumerate(self.dcnt[q]):
                if c > 0:
                    toks.append((("d", q, i), c))
        for e in ENGS:
            waits = []
            kn = self.known[e]
            for sk, v in toks:
                if sk == ("p", e):
                    continue
                if kn.get(sk, 0) < v:
                    kn[sk] = v
                    waits.append((sk, v))
            if waits:
                self.ops[e].append((waits, None, None))

    def check_deadlock(self):
        sem = {}
        ptr = {e: 0 for e in ENGS}
        progress = True
        while progress:
            progress = False
            for e in ENGS:
                lst = self.ops[e]
                while ptr[e] < len(lst):
                    waits, fn, inc = lst[ptr[e]]
                    if all(sem.get(sk, 0) >= v for sk, v in waits):
                        if inc is not None:
                            sem[inc[0]] = sem.get(inc[0], 0) + inc[1]
                        ptr[e] += 1
                        progress = True
                    else:
                        break
        stuck = {e: (ptr[e], len(self.ops[e])) for e in ENGS if ptr[e] < len(self.ops[e])}
        if stuck:
            for e in stuck:
                waits, fn, inc = self.ops[e][ptr[e]]
                print("STUCK", e, ptr[e], [(sk, v, sem.get(sk, 0)) for sk, v in waits])
            raise RuntimeError(f"deadlock in sync graph: {stuck}")

    def finish(self):
        self.barrier()
        self.check_deadlock()
        nc = self.nc
        ops = self.ops
        semobj = self.semobj

        def run(e, lst):
            for waits, fn, inc in lst:
                for sk, v in waits:
                    e.wait_ge(semobj[sk], v)
                if fn is not None:
                    ins = fn(e)
                    ins.then_inc(semobj[inc[0]], inc[1])

        with nc.Block() as block:
            @block.tensor
            def _(e):
                run(e, ops["pe"])

            @block.vector
            def _(e):
                run(e, ops["dve"])

            @block.scalar
            def _(e):
                run(e, ops["act"])

            @block.gpsimd
            def _(e):
                run(e, ops["pool"])

            @block.sync
            def _(e):
                run(e, ops["sp"])

        for cm in reversed(self._ctx):
            cm.__exit__(None, None, None)
        self._ctx = []
        return nc


def _t5_bucket_table():
    n = np.arange(0, 256, dtype=np.int32)
    max_exact = 16
    nf = np.maximum(n, 1).astype(np.float32)
    large = max_exact + (np.log(nf / np.float32(max_exact)) / np.float32(math.log(128 / max_exact))
                         * np.float32(32 - max_exact)).astype(np.int32)
    large = np.minimum(large, 31)
    return np.where(n < max_exact, n, large)


def _boh():
    tab = _t5_bucket_table()
    oh = np.zeros((32, 384), np.float32)
    for j in range(384):
        dist = min(max(j - 127, 0), 255)
        oh[tab[dist], j] = 1.0
    return oh


def _bthr():
    tab = _t5_bucket_table()
    thr = np.zeros((1, 31), np.float32)
    for kk in range(1, 32):
        nz = np.nonzero(tab >= kk)[0]
        thr[0, kk - 1] = float(nz[0]) if len(nz) else 1e9
    return thr


def build(stop_after=None, n_pool=5120, skip_p4s=False):
    k = KB()
    nc = k.nc

    def din(name, shape, dt=F32):
        return nc.dram_tensor(name, list(shape), dt, kind="ExternalInput")

    def dout(name, shape, dt=F32):
        return nc.dram_tensor(name, list(shape), dt, kind="ExternalOutput")

    xp = din("xp", [T, D])
    xs = din("xs", [TS, D])
    c5 = din("c5", [5, D])
    w_ada = din("w_ada", [D, 6 * D])
    b_ada = din("b_ada", [1, 6 * D])
    gvec = din("gvec", [4, D])
    w_in = din("w_in", [D, NPROJ])
    w_out = din("w_out", [D, D])
    w_ff1 = din("w_ff1", [D, DFF])
    w_ff2 = din("w_ff2", [DFF, D])
    conv_w = din("conv_w", [4, 3072])
    st_conv = din("st_conv", [TS * 3, 3072])
    hv = din("hv", [1, 16])
    ln_gb = din("ln_gb", [2, 128])
    gdn_g = din("gdn_g", [1, 128])
    st_ssm = din("st_ssm", [TS, 8, 128, 128])
    rel_bias = din("rel_bias", [32, 8])
    boh = din("boh", [32, 384])
    bthr = din("bthr", [1, 31])
    page_table = din("page_table", [TS, NPAGES], I32)
    cache_kidx = din("cache_kidx", [n_pool, PAGE * 128])
    cache_k = din("cache_k", [n_pool * PAGE, 256])
    cache_v = din("cache_v", [n_pool * PAGE, 256])

    y_p = dout("y_p", [T, D])
    y_s = dout("y_s", [TS, D])
    k_p = dout("k_p", [T, 256])
    v_p = dout("v_p", [T, 256])
    ki_p = dout("ki_p", [T, 128])
    ssm_p = dout("ssm_p", [8, 128, 128])
    conv_p = dout("conv_p", [3, 3072])
    k_s = dout("k_s", [TS, 256])
    v_s = dout("v_s", [TS, 256])
    ki_s = dout("ki_s", [TS, 128])
    ssm_s = dout("ssm_s", [TS, 8, 128, 128])
    conv_s = dout("conv_s", [TS, 3, 3072])

    modd = nc.dram_tensor("modd", [5, 6 * D], F32)
    gq = nc.dram_tensor("gq", [8, 128, TT], BF16)
    gk = nc.dram_tensor("gk", [8, 128, TT], BF16)
    gv = nc.dram_tensor("gv", [8, 128, TT], BF16)
    gz = nc.dram_tensor("gz", [TT, 1024], BF16)
    gab = nc.dram_tensor("gab", [TT, 16], F32)
    aq = nc.dram_tensor("aq", [8, 128, TT], BF16)
    akT = nc.dram_tensor("akT", [2, 128, TT], BF16)
    av = nc.dram_tensor("av", [TT, 256], BF16)
    iq = nc.dram_tensor("iq", [16, 128, TT], BF16)
    iw = nc.dram_tensor("iw", [TT, 16], F32)
    ikTd = nc.dram_tensor("ikTd", [128, TT], BF16)
    biasd = nc.dram_tensor("biasd", [8, 384], F32)
    rbTd = nc.dram_tensor("rbTd", [8, 32], F32)
    w1s = nc.dram_tensor("w1s", [16, 128, 16, 512], BF16)
    w2s = nc.dram_tensor("w2s", [4, 8, 128, 8, 512], BF16)
    x1d = nc.dram_tensor("x1d", [TT, D], F32)
    h2Td = nc.dram_tensor("h2Td", [128, 16, TT], BF16)

    k.init_arena(47 * 1024)
    k.psum_init()

    ident_f = k.alloc("ident_f", [128, 128], F32)
    ident_b = k.alloc("ident_b", [128, 128], BF16)
    ones_b = k.alloc("ones_b", [128, 128], BF16)
    k.op("pool", lambda e: e.memset(ident_f[:], 1.0), w=[ident_f])
    k.op("pool", lambda e: e.affine_select(out=ident_f[:], in_=ident_f[:], pattern=[[-1, 128]],
                                           compare_op=ALU.is_equal, fill=0.0, base=0, channel_multiplier=1),
         r=[ident_f], w=[ident_f])
    k.op("pool", lambda e: e.tensor_copy(out=ident_b[:], in_=ident_f[:]), r=[ident_f], w=[ident_b])
    k.op("pool", lambda e: e.memset(ones_b[:], 1.0), w=[ones_b])

    wb = []
    wcnt = [0]

    def alloc_wb():
        wb.clear()
        wb.extend(k.alloc(f"wb{i}", [128, 16, 512], BF16) for i in range(2))

    def load_w(src_dram, c0, ncols):
        b = wb[wcnt[0] % 2]
        wcnt[0] += 1
        k.dma("pool", b[:, :, 0:ncols], src_dram[:, c0:c0 + ncols].rearrange("(k p) n -> p k n", p=128), w=[b])
        return b

    m0 = k.mark()
    alloc_wb()
    c80 = k.alloc("c80", [80, 128], F32)
    cT = k.alloc("cT", [128, 16, 5], BF16)
    mod = k.alloc("mod", [5, 6 * D], F32)
    gv5 = k.alloc("gv5", [5, 4, D], F32)
    k.dma("sp", c80[:], c5.ap().rearrange("r (k p) -> (r k) p", p=128), w=[c80])
    k.dma("sp", mod[:], b_ada.ap().to_broadcast([5, 6 * D]), w=[mod])
    k.dma("sp", gv5[:].rearrange("p a b -> p (a b)"), gvec.ap().rearrange("a b -> (a b)").unsqueeze(0).to_broadcast([5, 4 * D]), w=[gv5])
    bk = k.bank()
    k.op("pe", lambda e: e.transpose(out=bk[:, 0:80], in_=c80[:], identity=ident_f[0:80, 0:80]), r=[c80, ident_f], w=[bk])
    k.op("act", lambda e: e.activation(out=cT[:], in_=bk[:, 0:80].rearrange("p (r k) -> p k r", r=5), func=AF.Silu), r=[bk], w=[cT])
    for n in range(24):
        wt = load_w(w_ada, n * 512, 512)
        bk = k.bank()
        for kk in range(16):
            k.op("pe", lambda e, kk=kk, wt=wt, bk=bk: e.matmul(bk[0:5, :], lhsT=cT[:, kk, :], rhs=wt[:, kk, :], start=(kk == 0), stop=(kk == 15)),
                 r=[cT, wt], w=[bk])
        k.op("dve", lambda e, n=n, bk=bk: e.tensor_tensor(out=mod[:, n * 512:(n + 1) * 512], in0=bk[0:5, :], in1=mod[:, n * 512:(n + 1) * 512], op=ALU.add),
             r=[bk, mod], w=[mod])
    for (sc, gi) in ((1, 0), (4, 2)):
        k.op("dve", lambda e, sc=sc, gi=gi: e.scalar_tensor_tensor(out=mod[:, sc * D:(sc + 1) * D], in0=mod[:, sc * D:(sc + 1) * D], scalar=1.0, op0=ALU.add,
                                                                    in1=gv5[:, gi, :], op1=ALU.mult), r=[mod, gv5], w=[mod])
    for (g, gi) in ((2, 1), (5, 3)):
        k.op("dve", lambda e, g=g, gi=gi: e.tensor_tensor(out=mod[:, g * D:(g + 1) * D], in0=mod[:, g * D:(g + 1) * D], in1=gv5[:, gi, :], op=ALU.mult),
             r=[mod, gv5], w=[mod])
    k.dma("sp", modd.ap(), mod[:], r=[mod], w=["modd"])
    k.barrier()
    k.release(m0)
    if stop_after == "P0":
        return k.finish()

    def load_mod_bcast(buf, idx):
        k.dma("sp", buf[:], modd[0:1, idx * D:(idx + 1) * D].to_broadcast([128, D]), r=["modd"], w=[buf])

    def load_mod_rows(buf, idx):
        k.dma("sp", buf[:], modd[1:5, idx * D:(idx + 1) * D], r=["modd"], w=[buf])

    mP = k.mark()
    hT = k.alloc("hT", [128, 16, TT], BF16)
    ikT = k.alloc("ikT", [128, TT], BF16)
    m1 = k.mark()
    A1 = k.alloc("A1", [128, D], F32)
    SH1 = k.alloc("SH1", [128, D], F32)
    A1s = k.alloc("A1s", [TS, D], F32)
    SH1s = k.alloc("SH1s", [TS, D], F32)
    load_mod_bcast(SH1, 0)
    load_mod_bcast(A1, 1)
    load_mod_rows(SH1s, 0)
    load_mod_rows(A1s, 1)
    xt = [k.alloc(f"xt{i}", [128, D], F32) for i in range(2)]
    hb = [k.alloc(f"hb{i}", [128, D], BF16) for i in range(2)]
    junk = k.alloc("junk", [128, D], BF16)
    st1 = [k.alloc(f"st1_{i}", [128, 4], F32) for i in range(2)]

    def norm_mod(i, np_, src_ap, A, SH, col0, ncol):
        x_ = xt[i % 2]
        h_ = hb[i % 2]
        s_ = st1[i % 2]
        k.dma("sp", x_[0:np_, :], src_ap, w=[x_])
        k.op("act", lambda e: e.activation(out=junk[0:np_, :], in_=x_[0:np_, :], func=AF.Square, accum_out=s_[0:np_, 0:1]), r=[x_], w=[junk, s_])
        k.op("act", lambda e: e.activation(out=s_[0:np_, 1:2], in_=s_[0:np_, 0:1], func=AF.Sqrt, scale=1.0 / D, bias=EPS), r=[s_], w=[s_])
        k.op("dve", lambda e: e.reciprocal(out=s_[0:np_, 2:3], in_=s_[0:np_, 1:2]), r=[s_], w=[s_])
        k.op("dve", lambda e: e.scalar_tensor_tensor(out=x_[0:np_, :], in0=x_[0:np_, :], scalar=s_[0:np_, 2:3], op0=ALU.mult, in1=A[0:np_, :], op1=ALU.mult),
             r=[x_, s_, A], w=[x_])
        k.op("pool", lambda e: e.tensor_tensor(out=h_[0:np_, :], in0=x_[0:np_, :], in1=SH[0:np_, :], op=ALU.add), r=[x_, SH], w=[h_])
        b0 = k.bank()
        b1 = k.bank()
        for kk in range(16):
            bb = b0 if kk < 8 else b1
            k.op("pe", lambda e, kk=kk, bb=bb: e.transpose(out=bb[:].bitcast(BF16)[:, (kk % 8) * 128:(kk % 8) * 128 + np_],
                                                           in_=h_[0:np_, kk * 128:(kk + 1) * 128], identity=ident_b[0:np_, 0:np_]),
                 r=[h_, ident_b], w=[bb])
        for half, bb in ((0, b0), (1, b1)):
            k.op("act", lambda e, half=half, bb=bb: e.copy(out=hT[:, half * 8:(half + 1) * 8, col0:col0 + ncol],
                                                          in_=bb[:].bitcast(BF16).rearrange("p (a b) -> p a b", a=8)[:, :, 0:ncol]),
                 r=[bb], w=[(hT, col0)])

    for i in range(NT):
        norm_mod(i, 128, xp[i * 128:(i + 1) * 128, :], A1, SH1, i * 128, 128)
    norm_mod(NT, TS, xs.ap(), A1s, SH1s, T, TS)
    k.barrier()
    k.release(m1)
    if stop_after == "P1":
        dbg = dout("dbg_hT", [128, 16 * TT], BF16)
        k.dma("sp", dbg.ap(), hT[:].rearrange("p a b -> p (a b)"), r=[hT])
        return k.finish()

    hT_keys = [(hT, i * 128) for i in range(NT)] + [(hT, T)]

    m2 = k.mark()
    alloc_wb()
    cw = k.alloc("cw", [128, 4, 24], F32)
    stc = k.alloc("stc", [128, TS * 3, 24], F32)
    cwl = k.alloc("cwl", [96, 128], F32)
    stl = k.alloc("stl", [96, 3, 128], F32)
    k.dma("sp", cwl[:], conv_w.ap().rearrange("j (ch c) -> (j ch) c", c=128), w=[cwl])
    k.dma("sp", stl[:], st_conv.ap().rearrange("r (ch c) -> (r ch) c", c=128).rearrange("(g p) c -> p g c", p=96), w=[stl])
    bk = k.bank()
    k.op("pe", lambda e, bk=bk: e.transpose(out=bk[:, 0:96], in_=cwl[:], identity=ident_f[0:96, 0:96]), r=[cwl, ident_f], w=[bk])
    k.op("act", lambda e, bk=bk: e.copy(out=cw[:].rearrange("p a b -> p (a b)"), in_=bk[:, 0:96]), r=[bk], w=[cw])
    bk = k.bank()
    for g in range(3):
        k.op("pe", lambda e, g=g, bk=bk: e.transpose(out=bk[:, g * 96:(g + 1) * 96], in_=stl[:, g, :], identity=ident_f[0:96, 0:96]),
             r=[stl, ident_f], w=[bk])
    k.op("act", lambda e, bk=bk: e.copy(out=stc[:].rearrange("p a b -> p (a b)"), in_=bk[:, 0:288]), r=[bk], w=[stc])
    k.dma("sp", conv_s.ap()[:, 0:2, :], st_conv.ap().rearrange("(s j) c -> s j c", j=3)[:, 1:3, :])

    cin = [k.alloc(f"cin{i}", [128, 3 + T], F32) for i in range(2)]
    for cb in cin:
        k.op("pool", lambda e, cb=cb: e.memset(cb[:, 0:3], 0.0), w=[cb])
    acc = [k.alloc(f"acc{i}", [128, TT], F32) for i in range(2)]
    cins = [k.alloc(f"cins{i}", [128, TS, 4], F32) for i in range(2)]
    tmp4 = [k.alloc(f"tmp4{i}", [128, TS, 4], F32) for i in range(2)]
    sqb = [k.alloc(f"sqb{i}", [128, TT], BF16) for i in range(2)]
    rsb = [k.alloc(f"rsb{i}", [128, TT], F32) for i in range(2)]
    ob = [k.alloc(f"ob{i}", [128, TT], BF16) for i in range(2)]
    obc = [0]

    def fm_matmuls(wt, wc0, tg, bk):
        c0, n = (tg * 512, 512) if tg < 4 else (T, TS)
        keys = hT_keys[tg * 4:(tg + 1) * 4] if tg < 4 else [hT_keys[16]]
        for kk in range(16):
            k.op("pe", lambda e, kk=kk: e.matmul(bk[:, 0:n], lhsT=wt[:, kk, wc0:wc0 + 128], rhs=hT[:, kk, c0:c0 + n], start=(kk == 0), stop=(kk == 15)),
                 r=[wt] + keys, w=[bk])

    def next_ob():
        b = ob[obc[0] % 2]
        obc[0] += 1
        return b

    chunk_i = [0]

    def conv_chunk(wt, wc0, ch):
        ci = chunk_i[0]
        chunk_i[0] += 1
        cb = cin[ci % 2]
        ac = acc[ci % 2]
        cs = cins[ci % 2]
        t4 = tmp4[ci % 2]
        for tg in range(4):
            bk = k.bank()
            fm_matmuls(wt, wc0, tg, bk)
            k.op("act", lambda e, bk=bk, tg=tg: e.copy(out=cb[:, 3 + tg * 512:3 + (tg + 1) * 512], in_=bk[:, 0:512]), r=[bk], w=[cb])
        bk = k.bank()
        fm_matmuls(wt, wc0, 4, bk)
        k.op("dve", lambda e: e.tensor_copy(out=cs[:, :, 0:3], in_=stc[:, :, ch].rearrange("p (s j) -> p s j", j=3)), r=[stc], w=[cs])
        k.op("dve", lambda e, bk=bk: e.tensor_copy(out=cs[:, :, 3:4], in_=bk[:, 0:TS].unsqueeze(2)), r=[bk, cs], w=[cs])
        k.dma("sp", conv_p.ap()[:, ch * 128:(ch + 1) * 128].rearrange("r c -> c r"), cb[:, T:T + 3], r=[cb], slow=True)
        k.dma("sp", conv_s.ap()[:, 2, ch * 128:(ch + 1) * 128].rearrange("s c -> c s"), cs[:, :, 3], r=[cs], slow=True)
        k.op("dve", lambda e: e.tensor_scalar(out=ac[:, 0:T], in0=cb[:, 0:T], scalar1=cw[:, 0, ch:ch + 1], scalar2=None, op0=ALU.mult), r=[cb, cw], w=[ac])
        for j in range(1, 4):
            k.op("dve", lambda e, j=j: e.scalar_tensor_tensor(out=ac[:, 0:T], in0=cb[:, j:j + T], scalar=cw[:, j, ch:ch + 1], op0=ALU.mult, in1=ac[:, 0:T], op1=ALU.add),
                 r=[cb, cw, ac], w=[ac])
        k.op("dve", lambda e: e.tensor_tensor(out=t4[:], in0=cs[:], in1=cw[:, :, ch].unsqueeze(1).to_broadcast([128, TS, 4]), op=ALU.mult), r=[cs, cw], w=[t4])
        k.op("dve", lambda e: e.tensor_reduce(out=ac[:, T:TT], in_=t4[:], axis=AX.X, op=ALU.add), r=[t4, ac], w=[ac])
        o = next_ob()
        if ch >= 16:
            k.op("act", lambda e: e.activation(out=o[:], in_=ac[:], func=AF.Silu), r=[ac], w=[o])
            k.dma("sp", gv[ch - 16], o[:], r=[o], w=["gv"])
            return
        sq = sqb[ci % 2]
        rs = rsb[ci % 2]
        k.op("act", lambda e: e.activation(out=ac[:], in_=ac[:], func=AF.Silu), r=[ac], w=[ac])
        k.op("act", lambda e: e.activation(out=sq[:], in_=ac[:], func=AF.Square), r=[ac], w=[sq])
        for tg in range(5):
            c0, n = (tg * 512, 512) if tg < 4 else (T, TS)
            bk = k.bank()
            k.op("pe", lambda e, bk=bk, c0=c0, n=n: e.matmul(bk[:, 0:n], lhsT=ones_b[:], rhs=sq[:, c0:c0 + n], start=True, stop=True), r=[sq, ones_b], w=[bk])
            k.op("act", lambda e, bk=bk, c0=c0, n=n: e.activation(out=rs[:, c0:c0 + n], in_=bk[:, 0:n], func=AF.Sqrt, bias=EPS, scale=1.0), r=[bk], w=[rs])
        k.op("dve", lambda e: e.reciprocal(out=rs[:], in_=rs[:]), r=[rs], w=[rs])
        scl = 128.0 ** -0.5 if ch < 8 else 1.0
        k.op("dve", lambda e: e.scalar_tensor_tensor(out=o[:], in0=ac[:], scalar=scl, op0=ALU.mult, in1=rs[:], op1=ALU.mult), r=[ac, rs], w=[o])
        dst = gq[ch] if ch < 8 else gk[ch - 8]
        k.dma("sp", dst, o[:], r=[o], w=["gqk"])

    def plain_chunk(wt, wc0, dst, scale):
        o = next_ob()
        for tg in range(5):
            c0, n = (tg * 512, 512) if tg < 4 else (T, TS)
            bk = k.bank()
            fm_matmuls(wt, wc0, tg, bk)
            k.op("act", lambda e, bk=bk, c0=c0, n=n: e.activation(out=o[:, c0:c0 + n], in_=bk[:, 0:n], func=AF.Copy, scale=scale), r=[bk], w=[o])
        k.dma("sp", dst, o[:], r=[o], w=["plain"])

    for g in range(6):
        wt = load_w(w_in, O_CONV + g * 512, 512)
        for j in range(4):
            conv_chunk(wt, j * 128, g * 4 + j)
    if stop_after == "P2a":
        k.barrier()
        return k.finish()
    for g in range(2):
        wt = load_w(w_in, O_QB + g * 512, 512)
        for j in range(4):
            plain_chunk(wt, j * 128, aq[g * 4 + j], 128.0 ** -0.5)
    for g in range(4):
        wt = load_w(w_in, O_QI + g * 512, 512)
        for j in range(4):
            plain_chunk(wt, j * 128, iq[g * 4 + j], 1.0)
    wt_kv = load_w(w_in, O_KB, 512)
    for j in range(2):
        plain_chunk(wt_kv, j * 128, akT[j], 1.0)

    if stop_after == "P2b":
        k.barrier()
        return k.finish()
    stg_f = [k.alloc(f"stgf{i}", [128, 512], F32) for i in range(2)]
    stg_b = [k.alloc(f"stgb{i}", [128, 512], BF16) for i in range(2)]
    lnw = [k.alloc(f"lnw{i}", [128, 8], F32) for i in range(2)]
    kib = [k.alloc(f"kib{i}", [128, 128], BF16) for i in range(2)]
    lng = k.alloc("lng", [128, 128], F32)
    lnb = k.alloc("lnb", [128, 128], F32)
    k.dma("sp", lng[:], ln_gb[0:1, :].to_broadcast([128, 128]), w=[lng])
    k.dma("sp", lnb[:], ln_gb[1:2, :].to_broadcast([128, 128]), w=[lnb])
    tcnt = [0]

    def tm_tile(wt, ncols, ti, epilogue):
        c0, n = (ti * 128, 128) if ti < NT else (T, TS)
        bk = k.bank()
        for kk in range(16):
            k.op("pe", lambda e, kk=kk: e.matmul(bk[0:n, 0:ncols], lhsT=hT[:, kk, c0:c0 + n], rhs=wt[:, kk, 0:ncols], start=(kk == 0), stop=(kk == 15)),
                 r=[wt, hT_keys[ti]], w=[bk])
        i = tcnt[0]
        tcnt[0] += 1
        epilogue(bk, c0, n, i)

    def ep_kv(bk, c0, n, i):
        sf = stg_f[i % 2]
        sb_ = stg_b[i % 2]
        import os
        dbg = int(os.environ.get("DBG", "0"))
        k.op("act", lambda e: e.copy(out=sf[0:n, :], in_=bk[0:n, :]), r=[bk], w=[sf])
        if dbg != 3:
            k.op("pool", lambda e: e.tensor_copy(out=sb_[0:n, 0:256], in_=sf[0:n, 256:512]), r=[sf], w=[sb_])
        if dbg == 1:
            pass
        elif c0 < T:
            k.dma("sp", k_p[c0:c0 + n, :], sf[0:n, 0:256], r=[sf])
            k.dma("sp", v_p[c0:c0 + n, :], sf[0:n, 256:512], r=[sf])
        else:
            k.dma("sp", k_s.ap(), sf[0:n, 0:256], r=[sf])
            k.dma("sp", v_s.ap(), sf[0:n, 256:512], r=[sf])
        if dbg not in (2, 3):
            k.dma("sp", av[c0:c0 + n, :], sb_[0:n, 0:256], r=[sb_], w=["av"])

    import os
    _d = int(os.environ.get("DBG", "0"))
    for ti in range(0 if _d == 4 else (NT if _d == 5 else NT + 1)):
        tm_tile(wt_kv, 512, ti, ep_kv)

    if stop_after == "P2c":
        k.barrier()
        return k.finish()

    def ep_z(half):
        def ep(bk, c0, n, i):
            sb_ = stg_b[i % 2]
            k.op("act", lambda e: e.copy(out=sb_[0:n, :], in_=bk[0:n, :]), r=[bk], w=[sb_])
            k.dma("sp", gz[c0:c0 + n, half * 512:(half + 1) * 512], sb_[0:n, :], r=[sb_], w=["gz"])
        return ep

    for half in range(2):
        wt = load_w(w_in, O_Z + half * 512, 512)
        for ti in range(NT + 1):
            tm_tile(wt, 512, ti, ep_z(half))

    if stop_after == "P2d":
        k.barrier()
        return k.finish()

    def ep_ab(bk, c0, n, i):
        sf = stg_f[i % 2]
        k.op("act", lambda e: e.copy(out=sf[0:n, 0:16], in_=bk[0:n, 0:16]), r=[bk], w=[sf])
        k.dma("sp", gab[c0:c0 + n, :], sf[0:n, 0:16], r=[sf], w=["gab"])

    wt = load_w(w_in, O_A, 16)
    for ti in range(NT + 1):
        tm_tile(wt, 16, ti, ep_ab)

    if stop_after == "P2e":
        k.barrier()
        return k.finish()

    def ep_wk(bk, c0, n, i):
        sf = stg_f[i % 2]
        s_ = lnw[i % 2]
        kb_ = kib[i % 2]
        k.op("act", lambda e: e.activation(out=sf[0:n, 0:16], in_=bk[0:n, 0:16], func=AF.Copy, scale=0.25), r=[bk], w=[sf])
        k.dma("sp", iw[c0:c0 + n, :], sf[0:n, 0:16], r=[sf], w=["iw"])
        kf = sf[0:n, 128:256]
        k.op("act", lambda e: e.activation(out=kf, in_=bk[0:n, 16:144], func=AF.Copy, accum_out=s_[0:n, 0:1]), r=[bk, sf], w=[sf, s_])
        k.op("dve", lambda e: e.tensor_scalar(out=s_[0:n, 1:2], in0=s_[0:n, 0:1], scalar1=-1.0 / 128, scalar2=None, op0=ALU.mult), r=[s_], w=[s_])
        k.op("dve", lambda e: e.tensor_scalar(out=kf, in0=kf, scalar1=s_[0:n, 1:2], scalar2=None, op0=ALU.add), r=[sf, s_], w=[sf])
        k.op("act", lambda e: e.activation(out=sf[0:n, 256:384], in_=kf, func=AF.Square, accum_out=s_[0:n, 2:3]), r=[sf, s_], w=[sf, s_])
        k.op("act", lambda e: e.activation(out=s_[0:n, 3:4], in_=s_[0:n, 2:3], func=AF.Sqrt, scale=1.0 / 128, bias=EPS), r=[s_], w=[s_])
        k.op("dve", lambda e: e.reciprocal(out=s_[0:n, 4:5], in_=s_[0:n, 3:4]), r=[s_], w=[s_])
        k.op("dve", lambda e: e.scalar_tensor_tensor(out=kf, in0=kf, scalar=s_[0:n, 4:5], op0=ALU.mult, in1=lng[0:n, :], op1=ALU.mult), r=[sf, s_, lng], w=[sf])
        k.op("dve", lambda e: e.tensor_tensor(out=kf, in0=kf, in1=lnb[0:n, :], op=ALU.add), r=[sf, lnb], w=[sf])
        k.op("dve", lambda e: e.tensor_copy(out=kb_[0:n, :], in_=kf), r=[sf], w=[kb_])
        if c0 < T:
            k.dma("sp", ki_p[c0:c0 + n, :], kf, r=[sf])
        else:
            k.dma("sp", ki_s.ap(), kf, r=[sf])
        b2 = k.bank()
        k.op("pe", lambda e: e.transpose(out=b2[:].bitcast(BF16)[:, 0:n], in_=kb_[0:n, :], identity=ident_b[0:n, 0:n]), r=[kb_, ident_b], w=[b2])
        k.op("act", lambda e: e.copy(out=ikT[:, c0:c0 + n], in_=b2[:].bitcast(BF16)[:, 0:n]), r=[b2], w=[(ikT, c0)])

    wt = load_w(w_in, O_WI, 144)
    for ti in range(NT + 1):
        tm_tile(wt, 144, ti, ep_wk)
    k.dma("sp", ikTd.ap(), ikT[:], r=[(ikT, c) for c in range(0, TT, 128)], w=["ikTd"])
    k.barrier()
    k.release(mP)
    if stop_after == "P2":
        return k.finish()
    conv_jobs = []
    for g in range(16):
        conv_jobs.append((w1s[g], w_ff1[:, g * 512:(g + 1) * 512].rearrange("(k p) c -> p k c", p=128), ("w1s", g)))
    for qc in range(4):
        for fgg in range(8):
            conv_jobs.append((w2s[qc, fgg], w_ff2[fgg * 1024:(fgg + 1) * 1024, qc * 512:(qc + 1) * 512].rearrange("(c p) n -> p c n", p=128), ("w2s", qc, fgg)))

    def issue_conv(nj):
        for _ in range(nj):
            if conv_jobs:
                o_, i_, key_ = conv_jobs.pop(0)
                k.dma("pool", o_, i_, w=[key_])
    mixT = k.alloc("mixT", [128, 16, TT], BF16)
    m3 = k.mark()
    HG = 4
    HW = HG * 128
    ones_f = k.alloc("ones_f", [128, 128], F32)
    TRI = k.alloc("TRI", [128, 128], F32)
    POSM = k.alloc("POSM", [128, HG, 128], F32)
    OFFD = k.alloc("OFFD", [128, HG, 128], F32)
    hvb = k.alloc("hvb", [128, 16], F32)
    gnb = k.alloc("gnb", [128, 128], F32)
    k.op("pool", lambda e: e.memset(ones_f[:], 1.0), w=[ones_f])
    k.op("pool", lambda e: e.memset(TRI[:], 1.0), w=[TRI])
    k.op("pool", lambda e: e.affine_select(out=TRI[:], in_=TRI[:], pattern=[[1, 128]], compare_op=ALU.is_ge, fill=0.0, base=0, channel_multiplier=-1),
         r=[TRI], w=[TRI])
    k.op("pool", lambda e: e.memset(POSM[:], 0.0), w=[POSM])
    k.op("pool", lambda e: e.affine_select(out=POSM[:], in_=POSM[:], pattern=[[0, HG], [-1, 128]], compare_op=ALU.is_ge, fill=30000.0, base=0, channel_multiplier=1),
         r=[POSM], w=[POSM])
    k.op("pool", lambda e: e.memset(OFFD[:], 1.0), w=[OFFD])
    k.op("pool", lambda e: e.affine_select(out=OFFD[:], in_=OFFD[:], pattern=[[0, HG], [-1, 128]], compare_op=ALU.not_equal, fill=0.0, base=0, channel_multiplier=1),
         r=[OFFD], w=[OFFD])
    k.dma("sp", hvb[:], hv.ap().to_broadcast([128, 16]), w=[hvb])
    k.dma("sp", gnb[:], gdn_g.ap().to_broadcast([128, 128]), w=[gnb])

    gabt = k.alloc("gabt", [128, NT, 16], F32)
    k.dma("sp", gabt[:], gab[0:T, :].rearrange("(t p) c -> p t c", p=128), r=["gab"], w=[gabt], slow=True)
    nA = k.alloc("nA", [128, 8], F32)
    G_ = k.alloc("G_", [128, NT, 8], F32)
    Bt = k.alloc("Bt", [128, NT, 8], F32)
    NB = k.alloc("NB", [128, NT, 8], F32)
    GC = k.alloc("GC", [128, NT, 8], F32)
    GL = k.alloc("GL", [128, NT, 8], F32)
    EG = k.alloc("EG", [128, NT, 8], F32)
    EGL = k.alloc("EGL", [128, NT, 8], F32)
    EKD = k.alloc("EKD", [128, NT, 8], F32)
    BEG = k.alloc("BEG", [128, NT, 8], F32)
    k.op("act", lambda e: e.activation(out=nA[:], in_=hvb[:, 0:8], func=AF.Exp), r=[hvb], w=[nA])
    k.op("dve", lambda e: e.tensor_scalar(out=nA[:], in0=nA[:], scalar1=-1.0, scalar2=None, op0=ALU.mult), r=[nA], w=[nA])
    k.op("dve", lambda e: e.tensor_tensor(out=G_[:], in0=gabt[:, :, 0:8], in1=hvb[:, 8:16].unsqueeze(1).to_broadcast([128, NT, 8]), op=ALU.add), r=[gabt, hvb], w=[G_])
    k.op("act", lambda e: e.activation(out=G_[:], in_=G_[:], func=AF.Exp), r=[G_], w=[G_])
    k.op("act", lambda e: e.activation(out=G_[:], in_=G_[:], func=AF.Ln, bias=1.0, scale=1.0), r=[G_], w=[G_])
    k.op("dve", lambda e: e.tensor_tensor(out=G_[:], in0=G_[:], in1=nA[:].unsqueeze(1).to_broadcast([128, NT, 8]), op=ALU.mult), r=[G_, nA], w=[G_])
    k.op("act", lambda e: e.activation(out=Bt[:], in_=gabt[:, :, 8:16], func=AF.Sigmoid), r=[gabt], w=[Bt])
    k.op("dve", lambda e: e.tensor_scalar(out=NB[:], in0=Bt[:], scalar1=-1.0, scalar2=None, op0=ALU.mult), r=[Bt], w=[NB])
    bA = k.bank()
    bB = k.bank()
    for t in range(NT):
        k.op("pe", lambda e, t=t: e.matmul(bA[:, t * 8:(t + 1) * 8], lhsT=TRI[:], rhs=G_[:, t, :], start=True, stop=True), r=[TRI, G_], w=[bA])
        k.op("pe", lambda e, t=t: e.matmul(bB[:, t * 8:(t + 1) * 8], lhsT=ones_f[:], rhs=G_[:, t, :], start=True, stop=True), r=[ones_f, G_], w=[bB])
    k.op("act", lambda e: e.copy(out=GC[:].rearrange("p a b -> p (a b)"), in_=bA[:, 0:NT * 8]), r=[bA], w=[GC])
    k.op("act", lambda e: e.copy(out=GL[:].rearrange("p a b -> p (a b)"), in_=bB[:, 0:NT * 8]), r=[bB], w=[GL])
    k.op("act", lambda e: e.activation(out=EG[:], in_=GC[:], func=AF.Exp), r=[GC], w=[EG])
    k.op("act", lambda e: e.activation(out=EGL[:], in_=GL[:], func=AF.Exp), r=[GL], w=[EGL])
    k.op("dve", lambda e: e.tensor_tensor(out=EKD[:], in0=GL[:], in1=GC[:], op=ALU.subtract), r=[GL, GC], w=[EKD])
    k.op("act", lambda e: e.activation(out=EKD[:], in_=EKD[:], func=AF.Exp), r=[EKD], w=[EKD])
    k.op("dve", lambda e: e.tensor_tensor(out=BEG[:], in0=Bt[:], in1=EG[:], op=ALU.mult), r=[Bt, EG], w=[BEG])

    if stop_after == "P3a":
        k.barrier()
        return k.finish()
    m3g = k.mark()
    qT = k.alloc("qT", [128, HG, TT], BF16)
    kT = k.alloc("kT", [128, HG, TT], BF16)
    vT = k.alloc("vT", [128, HG, TT], BF16)
    NSLOT = 2

    def mk_slot(j):
        d = {}
        for nm, dt in (("Dg", F32), ("egT", BF16), ("decay", F32), ("M0", F32), ("M1", F32), ("MT0", F32), ("MT1", F32),
                       ("PT", F32), ("PTb", BF16), ("vbeta", BF16), ("kbg", BF16), ("kdec", BF16), ("u", F32), ("wT", BF16),
                       ("intra", BF16), ("intraT", BF16), ("qg", BF16)):
            d[nm] = k.alloc(f"{nm}_{j}", [128, HG, 128], dt)
        return d

    slots = [mk_slot(j) for j in range(NSLOT)]
    S_ = k.alloc("S_", [128, HG, 128], F32)
    Sb = k.alloc("Sb", [128, HG, 128], BF16)
    vnew = k.alloc("vnew", [128, HG, 128], BF16)
    o_ = k.alloc("o_", [128, HG, 128], F32)
    sq_ = k.alloc("sq_", [128, HG, 128], F32)
    zt = [k.alloc(f"zt{i}", [128, HG, 128], BF16) for i in range(2)]
    zs = k.alloc("zs", [128, HG, 128], F32)
    oa = k.alloc("oa", [128, HG, 128], BF16)
    sst = k.alloc("sst", [128, 3 * HG], F32)

    def fl(b):
        return b[:].rearrange("p a b -> p (a b)")

    def bc_tok(src_ap):
        return src_ap.unsqueeze(2).to_broadcast([128, HG, 128])

    def stageA(g, i, sl):
        d = slots[sl]
        h0 = g * HG
        tsl = slice(i * 128, (i + 1) * 128)
        Dg, egT, decay, PT, PTb = d["Dg"], d["egT"], d["decay"], d["PT"], d["PTb"]
        Ms = [d["M0"], d["M1"]]
        MTs = [d["MT0"], d["MT1"]]
        k.op("dve", lambda e: e.tensor_tensor(out=Dg[:], in0=ident_f[:].unsqueeze(1).to_broadcast([128, HG, 128]), in1=bc_tok(GC[:, i, h0:h0 + HG]), op=ALU.mult),
             r=[ident_f, GC], w=[Dg])
        b1 = k.bank()
        k.op("pe", lambda e: e.matmul(b1[:, 0:HW], lhsT=ones_f[:], rhs=fl(Dg), start=True, stop=True), r=[ones_f, Dg], w=[b1])
        k.op("act", lambda e: e.activation(out=fl(egT), in_=b1[:, 0:HW], func=AF.Exp), r=[b1], w=[egT])
        b2 = k.bank()
        k.op("pe", lambda e: e.matmul(b2[:, 0:HW], lhsT=ones_f[:], rhs=fl(Dg), start=True, stop=False), r=[ones_f, Dg], w=[b2])
        k.op("pe", lambda e: e.matmul(b2[:, 0:HW], lhsT=ident_f[:], rhs=fl(POSM), start=False, stop=True), r=[ident_f, POSM], w=[b2])
        for h in range(HG):
            k.op("act", lambda e, h=h: e.activation(out=decay[:, h, :], in_=b2[:, h * 128:(h + 1) * 128], func=AF.Exp, scale=-1.0, bias=GC[:, i, h0 + h:h0 + h + 1]),
                 r=[b2, GC], w=[decay])
        k.op("dve", lambda e: e.tensor_tensor(out=d["qg"][:], in0=qT[:, :, tsl], in1=egT[:], op=ALU.mult), r=[qT, egT], w=[d["qg"]])
        b3 = k.bank()
        b4 = k.bank()
        for h in range(HG):
            k.op("pe", lambda e, h=h: e.matmul(b3[:, h * 128:(h + 1) * 128], lhsT=kT[:, h, tsl], rhs=kT[:, h, tsl], start=True, stop=True), r=[kT], w=[b3])
        for h in range(HG):
            k.op("pe", lambda e, h=h: e.matmul(b4[:, h * 128:(h + 1) * 128], lhsT=qT[:, h, tsl], rhs=kT[:, h, tsl], start=True, stop=True), r=[qT, kT], w=[b4])
        k.op("dve", lambda e: e.tensor_tensor(out=fl(d["intra"]), in0=b4[:, 0:HW], in1=fl(decay), op=ALU.mult), r=[b4, decay], w=[d["intra"]])
        k.op("dve", lambda e: e.tensor_tensor(out=Dg[:], in0=decay[:], in1=OFFD[:], op=ALU.mult), r=[decay, OFFD], w=[Dg])
        for h in range(HG):
            k.op("dve", lambda e, h=h: e.scalar_tensor_tensor(out=Ms[0][:, h, :], in0=b3[:, h * 128:(h + 1) * 128], scalar=NB[:, i, h0 + h:h0 + h + 1], op0=ALU.mult,
                                                                in1=Dg[:, h, :], op1=ALU.mult), r=[b3, NB, Dg], w=[Ms[0]])
        yield
        b5 = k.bank()
        b5i = k.bank()
        b5b = b5i[:].bitcast(BF16)
        for h in range(HG):
            k.op("pe", lambda e, h=h: e.transpose(out=b5[:, h * 128:(h + 1) * 128], in_=Ms[0][:, h, :], identity=ident_f[:]), r=[Ms[0], ident_f], w=[b5])
        for h in range(HG):
            k.op("pe", lambda e, h=h: e.transpose(out=b5b[:, h * 128:(h + 1) * 128], in_=d["intra"][:, h, :], identity=ident_b[:]), r=[d["intra"], ident_b], w=[b5i])
        k.op("act", lambda e: e.copy(out=fl(MTs[0]), in_=b5[:, 0:HW]), r=[b5], w=[MTs[0]])
        k.op("act", lambda e: e.copy(out=fl(d["intraT"]), in_=b5b[:, 0:HW]), r=[b5i], w=[d["intraT"]])
        k.op("dve", lambda e: e.tensor_tensor(out=PT[:], in0=MTs[0][:], in1=ident_f[:].unsqueeze(1).to_broadcast([128, HG, 128]), op=ALU.add), r=[MTs[0], ident_f], w=[PT])
        yield
        cur = 0
        for lvl in range(1, 7):
            nx = 1 - cur
            last = (lvl == 6)
            b6 = k.bank()
            for h in range(HG):
                k.op("pe", lambda e, h=h, cur=cur, b6=b6: e.matmul(b6[:, h * 128:(h + 1) * 128], lhsT=MTs[cur][:, h, :], rhs=Ms[cur][:, h, :], start=True, stop=True),
                     r=[MTs[cur], Ms[cur]], w=[b6])
            if not last:
                b7 = k.bank()
                for h in range(HG):
                    k.op("pe", lambda e, h=h, cur=cur, b7=b7: e.matmul(b7[:, h * 128:(h + 1) * 128], lhsT=Ms[cur][:, h, :], rhs=MTs[cur][:, h, :], start=True, stop=True),
                         r=[MTs[cur], Ms[cur]], w=[b7])
            k.op("act", lambda e, nx=nx, b6=b6: e.copy(out=fl(Ms[nx]), in_=b6[:, 0:HW]), r=[b6], w=[Ms[nx]])
            if not last:
                k.op("act", lambda e, nx=nx, b7=b7: e.copy(out=fl(MTs[nx]), in_=b7[:, 0:HW]), r=[b7], w=[MTs[nx]])
            b8 = k.bank()
            for h in range(HG):
                k.op("pe", lambda e, h=h, nx=nx, b8=b8: e.matmul(b8[:, h * 128:(h + 1) * 128], lhsT=Ms[nx][:, h, :], rhs=PT[:, h, :], start=True, stop=True),
                     r=[Ms[nx], PT], w=[b8])
            k.op("dve", lambda e, b8=b8: e.tensor_tensor(out=fl(PT), in0=b8[:, 0:HW], in1=fl(PT), op=ALU.add), r=[b8, PT], w=[PT])
            if last:
                k.op("act", lambda e: e.copy(out=PTb[:], in_=PT[:]), r=[PT], w=[PTb])
            cur = nx
            yield
        b9 = k.bank()
        b9b = b9[:].bitcast(BF16)
        for h in range(HG):
            k.op("pe", lambda e, h=h: e.transpose(out=b9b[:, h * 128:(h + 1) * 128], in_=vT[:, h, tsl], identity=ident_b[:]), r=[vT, ident_b], w=[b9])
        for h in range(HG):
            k.op("pe", lambda e, h=h: e.transpose(out=b9b[:, HW + h * 128:HW + (h + 1) * 128], in_=kT[:, h, tsl], identity=ident_b[:]), r=[kT, ident_b], w=[b9])
        vps = b9b[:, 0:HW].rearrange("p (a b) -> p a b", a=HG)
        kps = b9b[:, HW:2 * HW].rearrange("p (a b) -> p a b", a=HG)
        k.op("dve", lambda e: e.tensor_tensor(out=d["vbeta"][:], in0=vps, in1=bc_tok(Bt[:, i, h0:h0 + HG]), op=ALU.mult), r=[b9, Bt], w=[d["vbeta"]])
        k.op("dve", lambda e: e.tensor_tensor(out=d["kbg"][:], in0=kps, in1=bc_tok(BEG[:, i, h0:h0 + HG]), op=ALU.mult), r=[b9, BEG], w=[d["kbg"]])
        k.op("dve", lambda e: e.tensor_tensor(out=d["kdec"][:], in0=kps, in1=bc_tok(EKD[:, i, h0:h0 + HG]), op=ALU.mult), r=[b9, EKD], w=[d["kdec"]])
        b10 = k.bank()
        b11 = k.bank()
        for h in range(HG):
            k.op("pe", lambda e, h=h: e.matmul(b10[:, h * 128:(h + 1) * 128], lhsT=PTb[:, h, :], rhs=d["vbeta"][:, h, :], start=True, stop=True), r=[PTb, d["vbeta"]], w=[b10])
        for h in range(HG):
            k.op("pe", lambda e, h=h: e.matmul(b11[:, h * 128:(h + 1) * 128], lhsT=d["kbg"][:, h, :], rhs=PTb[:, h, :], start=True, stop=True), r=[PTb, d["kbg"]], w=[b11])
        k.op("act", lambda e: e.copy(out=fl(d["u"]), in_=b10[:, 0:HW]), r=[b10], w=[d["u"]])
        k.op("act", lambda e: e.copy(out=fl(d["wT"]), in_=b11[:, 0:HW]), r=[b11], w=[d["wT"]])
        yield

    def scan_step(g, i, sl):
        d = slots[sl]
        h0 = g * HG
        tsl = slice(i * 128, (i + 1) * 128)
        z_ = zt[i % 2]
        k.dma("sp", fl(z_), gz[i * 128:(i + 1) * 128, h0 * 128:(h0 + HG) * 128], r=["gz"], w=[z_])
        bx = k.bank()
        for h in range(HG):
            k.op("pe", lambda e, h=h: e.matmul(bx[:, h * 128:(h + 1) * 128], lhsT=d["wT"][:, h, :], rhs=Sb[:, h, :], start=True, stop=True), r=[d["wT"], Sb], w=[bx])
        k.op("dve", lambda e: e.tensor_tensor(out=fl(vnew), in0=fl(d["u"]), in1=bx[:, 0:HW], op=ALU.subtract), r=[d["u"], bx], w=[vnew])
        bo = k.bank()
        for h in range(HG):
            k.op("pe", lambda e, h=h: e.matmul(bo[:, h * 128:(h + 1) * 128], lhsT=d["qg"][:, h, :], rhs=Sb[:, h, :], start=True, stop=False), r=[d["qg"], Sb], w=[bo])
            k.op("pe", lambda e, h=h: e.matmul(bo[:, h * 128:(h + 1) * 128], lhsT=d["intraT"][:, h, :], rhs=vnew[:, h, :], start=False, stop=True), r=[d["intraT"], vnew], w=[bo])
        bz = k.bank()
        for h in range(HG):
            k.op("pe", lambda e, h=h: e.matmul(bz[:, h * 128:(h + 1) * 128], lhsT=d["kdec"][:, h, :], rhs=vnew[:, h, :], start=True, stop=True), r=[d["kdec"], vnew], w=[bz])
        k.op("dve", lambda e: e.tensor_tensor(out=S_[:], in0=S_[:], in1=bc_tok(EGL[:, i, h0:h0 + HG]), op=ALU.mult), r=[S_, EGL], w=[S_])
        k.op("dve", lambda e: e.tensor_tensor(out=fl(S_), in0=fl(S_), in1=bz[:, 0:HW], op=ALU.add), r=[S_, bz], w=[S_])
        k.op("act", lambda e: e.copy(out=Sb[:], in_=S_[:]), r=[S_], w=[Sb])
        k.op("act", lambda e: e.copy(out=fl(o_), in_=bo[:, 0:HW]), r=[bo], w=[o_])
        k.op("act", lambda e: e.activation(out=sq_[:], in_=o_[:], func=AF.Square), r=[o_], w=[sq_])
        k.op("dve", lambda e: e.tensor_reduce(out=sst[:, 0:HG], in_=sq_[:], axis=AX.X, op=ALU.add), r=[sq_], w=[sst])
        k.op("act", lambda e: e.activation(out=sst[:, HG:2 * HG], in_=sst[:, 0:HG], func=AF.Sqrt, scale=1.0 / 128, bias=EPS), r=[sst], w=[sst])
        k.op("dve", lambda e: e.reciprocal(out=sst[:, 2 * HG:3 * HG], in_=sst[:, HG:2 * HG]), r=[sst], w=[sst])
        k.op("act", lambda e: e.activation(out=zs[:], in_=z_[:], func=AF.Silu), r=[z_], w=[zs])
        k.op("dve", lambda e: e.tensor_tensor(out=o_[:], in0=o_[:], in1=bc_tok(sst[:, 2 * HG:3 * HG]), op=ALU.mult), r=[o_, sst], w=[o_])
        k.op("dve", lambda e: e.tensor_tensor(out=o_[:], in0=o_[:], in1=gnb[:].unsqueeze(1).to_broadcast([128, HG, 128]), op=ALU.mult), r=[o_, gnb], w=[o_])
        k.op("dve", lambda e: e.tensor_tensor(out=oa[:], in0=o_[:], in1=zs[:], op=ALU.mult), r=[o_, zs], w=[oa])
        bt = k.bank()
        btb = bt[:].bitcast(BF16)
        for h in range(HG):
            k.op("pe", lambda e, h=h: e.transpose(out=btb[:, h * 128:(h + 1) * 128], in_=oa[:, h, :], identity=ident_b[:]), r=[oa, ident_b], w=[bt])
        k.op("act", lambda e: e.copy(out=mixT[:, h0:h0 + HG, tsl], in_=btb[:, 0:HW].rearrange("p (a b) -> p a b", a=HG)), r=[bt], w=[(mixT, i)])

    for g in range(8 // HG):
        h0 = g * HG
        for nm, src, buf in (("q", gq, qT), ("k", gk, kT), ("v", gv, vT)):
            k.dma("sp", buf[:], src[h0:h0 + HG].rearrange("h d t -> d h t"), r=["gqk", "gv"], w=[buf])
        k.op("pool", lambda e: e.memset(S_[:], 0.0), w=[S_])
        k.op("pool", lambda e: e.memset(Sb[:], 0.0), w=[Sb])
        if stop_after == "P3d":
            for _ in stageA(0, 0, 0):
                pass
            for _ in stageA(0, 1, 1):
                pass
            scan_step(0, 0, 0)
            scan_step(0, 1, 1)
            d = slots[0]
            names = ["decay", "M0", "PT", "u", "wT", "intraT", "qg", "kdec", "vbeta", "kbg", "egT"]
            tmpfs = [sq_, zs]
            for ii, nm in enumerate(names):
                dd = dout("dbg_" + nm, [128, HW], F32)
                tmpf = tmpfs[ii % 2]
                k.op("dve", lambda e, nm=nm, tmpf=tmpf: e.tensor_copy(out=fl(tmpf), in_=fl(d[nm])), r=[d[nm]], w=[tmpf])
                k.dma("sp", dd.ap(), fl(tmpf), r=[tmpf])
            for nm, b in (("S", S_), ("o", o_), ("GC", GC), ("G", G_), ("Bt", Bt), ("EKD", EKD), ("EGL", EGL)):
                dd = dout("dbg_" + nm, [128, int(np.prod(b.ap.shape[1:]))], F32)
                k.dma("sp", dd.ap(), b[:].rearrange("p a b -> p (a b)"), r=[b])
            k.barrier()
            return k.finish()
        import os
        _lim = int(os.environ.get("YLIM", "100"))
        for i0 in range(0, NT, NSLOT):
            gens = [stageA(g, i0 + j, j) for j in range(NSLOT)]
            if stop_after == "P3b":
                for _ in range(_lim):
                    for gen in gens:
                        next(gen, None)
                k.barrier()
                return k.finish()
            alive = True
            while alive:
                alive = False
                for gen in gens:
                    try:
                        next(gen)
                        alive = True
                    except StopIteration:
                        pass
            for j in range(NSLOT):
                scan_step(g, i0 + j, j)
            issue_conv(3)
        k.dma("sp", ssm_p.ap()[h0:h0 + HG].rearrange("h a b -> a h b"), S_[:], r=[S_])
    if stop_after == "P3":
        k.barrier()
        return k.finish()
    issue_conv(100)
    k.barrier()
    k.release(m3g)
    S0 = k.alloc("S0", [128, 8, 128], F32)
    qc = k.alloc("qc", [128, 8], BF16)
    kc = k.alloc("kc", [128, 8], BF16)
    vc = k.alloc("vc", [128, 8], BF16)
    qcf = k.alloc("qcf", [128, 8], F32)
    kcf = k.alloc("kcf", [128, 8], F32)
    gabr = k.alloc("gabr", [1, 16], F32)
    zr = k.alloc("zr", [1, 1024], BF16)
    zrs = k.alloc("zrs", [1, 8, 128], F32)
    rw = k.alloc("rw", [1, 64], F32)
    t1 = k.alloc("t1", [1, 8, 128], F32)
    orow = k.alloc("orow", [1, 8, 128], F32)
    sqr = k.alloc("sqr", [1, 8, 128], F32)
    oar = k.alloc("oar", [1, 1024], BF16)
    abs_ = k.alloc("abs_", [128, 8], F32)

    def bc_row(ap8):
        return ap8.unsqueeze(2).to_broadcast([1, 8, 128])

    def sample_gdn(s_i):
        col = T + s_i
        k.dma("sp", S0[:], st_ssm.ap()[s_i].rearrange("h a b -> a h b"), w=[S0])
        k.dma("sp", qc[:], gq.ap()[:, :, col].rearrange("h d -> d h"), r=["gqk"], w=[qc], slow=True)
        k.dma("sp", kc[:], gk.ap()[:, :, col].rearrange("h d -> d h"), r=["gqk"], w=[kc], slow=True)
        k.dma("sp", vc[:], gv.ap()[:, :, col].rearrange("h d -> d h"), r=["gv"], w=[vc], slow=True)
        k.dma("sp", gabr[:], gab[col:col + 1, :], r=["gab"], w=[gabr])
        k.dma("sp", zr[:], gz[col:col + 1, :], r=["gz"], w=[zr])
        k.op("dve", lambda e: e.tensor_copy(out=qcf[:], in_=qc[:]), r=[qc], w=[qcf])
        k.op("dve", lambda e: e.tensor_copy(out=kcf[:], in_=kc[:]), r=[kc], w=[kcf])
        k.op("dve", lambda e: e.tensor_tensor(out=rw[:, 0:8], in0=gabr[:, 0:8], in1=hvb[0:1, 8:16], op=ALU.add), r=[gabr, hvb], w=[rw])
        k.op("act", lambda e: e.activation(out=rw[:, 0:8], in_=rw[:, 0:8], func=AF.Exp), r=[rw], w=[rw])
        k.op("act", lambda e: e.activation(out=rw[:, 0:8], in_=rw[:, 0:8], func=AF.Ln, bias=1.0, scale=1.0), r=[rw], w=[rw])
        k.op("dve", lambda e: e.tensor_tensor(out=rw[:, 0:8], in0=rw[:, 0:8], in1=nA[0:1, :], op=ALU.mult), r=[rw, nA], w=[rw])
        k.op("act", lambda e: e.activation(out=rw[:, 8:16], in_=rw[:, 0:8], func=AF.Exp), r=[rw], w=[rw])
        k.op("act", lambda e: e.activation(out=rw[:, 16:24], in_=gabr[:, 8:16], func=AF.Sigmoid), r=[gabr, rw], w=[rw])
        ba = k.bank()
        bb_ = k.bank()
        for h in range(8):
            bk_ = ba if h < 4 else bb_
            k.op("pe", lambda e, h=h, bk_=bk_: e.matmul(bk_[0:1, (h % 4) * 128:(h % 4 + 1) * 128], lhsT=kcf[:, h:h + 1], rhs=S0[:, h, :], start=True, stop=True),
                 r=[kcf, S0], w=[bk_])
        bv = k.bank()
        bvb = bv[:].bitcast(BF16)
        for h in range(8):
            k.op("pe", lambda e, h=h: e.transpose(out=bvb[0:1, h * 128:(h + 1) * 128], in_=vc[:, h:h + 1], identity=ident_b[:]), r=[vc, ident_b], w=[bv])
        k.op("dve", lambda e: e.tensor_tensor(out=t1[:, 0:4, :], in0=ba[0:1, :].rearrange("p (a b) -> p a b", a=4), in1=bc_row(rw[:, 8:16])[:, 0:4, :], op=ALU.mult),
             r=[ba, rw], w=[t1])
        k.op("dve", lambda e: e.tensor_tensor(out=t1[:, 4:8, :], in0=bb_[0:1, :].rearrange("p (a b) -> p a b", a=4), in1=bc_row(rw[:, 8:16])[:, 4:8, :], op=ALU.mult),
             r=[bb_, rw, t1], w=[t1])
        k.op("dve", lambda e: e.tensor_tensor(out=t1[:], in0=bvb[0:1, 0:1024].rearrange("p (a b) -> p a b", a=8), in1=t1[:], op=ALU.subtract), r=[bv, t1], w=[t1])
        k.op("dve", lambda e: e.tensor_tensor(out=t1[:], in0=t1[:], in1=bc_row(rw[:, 16:24]), op=ALU.mult), r=[t1, rw], w=[t1])
        bd0 = k.bank()
        bd1 = k.bank()
        k.op("pe", lambda e: e.matmul(bd0[:, :], lhsT=ones_f[0:1, :], rhs=t1[:].rearrange("p a b -> p (a b)")[:, 0:512], start=True, stop=True), r=[ones_f, t1], w=[bd0])
        k.op("pe", lambda e: e.matmul(bd1[:, :], lhsT=ones_f[0:1, :], rhs=t1[:].rearrange("p a b -> p (a b)")[:, 512:1024], start=True, stop=True), r=[ones_f, t1], w=[bd1])
        bab = k.bank()
        k.op("pe", lambda e: e.matmul(bab[:, 0:8], lhsT=ones_f[0:1, :], rhs=rw[:, 8:16], start=True, stop=True), r=[ones_f, rw], w=[bab])
        k.op("act", lambda e: e.copy(out=abs_[:], in_=bab[:, 0:8]), r=[bab], w=[abs_])
        k.op("dve", lambda e: e.tensor_tensor(out=S0[:], in0=S0[:], in1=abs_[:].unsqueeze(2).to_broadcast([128, 8, 128]), op=ALU.mult), r=[S0, abs_], w=[S0])
        for h in range(8):
            bd = bd0 if h < 4 else bd1
            k.op("dve", lambda e, h=h, bd=bd: e.scalar_tensor_tensor(out=S0[:, h, :], in0=bd[:, (h % 4) * 128:(h % 4 + 1) * 128], scalar=kcf[:, h:h + 1], op0=ALU.mult,
                                                                      in1=S0[:, h, :], op1=ALU.add), r=[bd, kcf, S0], w=[S0])
        k.dma("sp", ssm_s.ap()[s_i].rearrange("h a b -> a h b"), S0[:], r=[S0])
        bo0 = k.bank()
        bo1 = k.bank()
        for h in range(8):
            bk_ = bo0 if h < 4 else bo1
            k.op("pe", lambda e, h=h, bk_=bk_: e.matmul(bk_[0:1, (h % 4) * 128:(h % 4 + 1) * 128], lhsT=qcf[:, h:h + 1], rhs=S0[:, h, :], start=True, stop=True),
                 r=[qcf, S0], w=[bk_])
        k.op("act", lambda e: e.copy(out=orow[:, 0:4, :], in_=bo0[0:1, :].rearrange("p (a b) -> p a b", a=4)), r=[bo0], w=[orow])
        k.op("act", lambda e: e.copy(out=orow[:, 4:8, :], in_=bo1[0:1, :].rearrange("p (a b) -> p a b", a=4)), r=[bo1, orow], w=[orow])
        k.op("dve", lambda e: e.tensor_tensor(out=sqr[:], in0=orow[:], in1=orow[:], op=ALU.mult), r=[orow], w=[sqr])
        k.op("dve", lambda e: e.tensor_reduce(out=rw[:, 24:32], in_=sqr[:], axis=AX.X, op=ALU.add), r=[sqr, rw], w=[rw])
        k.op("act", lambda e: e.activation(out=rw[:, 32:40], in_=rw[:, 24:32], func=AF.Sqrt, scale=1.0 / 128, bias=EPS), r=[rw], w=[rw])
        k.op("dve", lambda e: e.reciprocal(out=rw[:, 40:48], in_=rw[:, 32:40]), r=[rw], w=[rw])
        k.op("act", lambda e: e.activation(out=zrs[:].rearrange("p a b -> p (a b)"), in_=zr[:], func=AF.Silu), r=[zr], w=[zrs])
        k.op("dve", lambda e: e.tensor_tensor(out=orow[:], in0=orow[:], in1=bc_row(rw[:, 40:48]), op=ALU.mult), r=[orow, rw], w=[orow])
        k.op("dve", lambda e: e.tensor_tensor(out=orow[:], in0=orow[:], in1=gnb[0:1, :].unsqueeze(1).to_broadcast([1, 8, 128]), op=ALU.mult), r=[orow, gnb], w=[orow])
        k.op("dve", lambda e: e.tensor_tensor(out=oar[:].rearrange("p (a b) -> p a b", a=8), in0=orow[:], in1=zrs[:], op=ALU.mult), r=[orow, zrs], w=[oar])
        bt_ = k.bank()
        for h in range(8):
            k.op("pe", lambda e, h=h: e.matmul(bt_[:, h:h + 1], lhsT=oar[0:1, h * 128:(h + 1) * 128], rhs=ones_b[0:1, 0:1], start=True, stop=True), r=[oar, ones_b], w=[bt_])
        k.op("act", lambda e: e.copy(out=mixT[:, 0:8, col], in_=bt_[:, 0:8]), r=[bt_], w=[(mixT, "s%d" % s_i)])
    for s_i in range(TS):
        sample_gdn(s_i)
    k.barrier()
    k.release(m3)
    if stop_after == "P3S":
        return k.finish()
    m4 = k.mark()
    NEG = -30000.0
    NIT = 17
    kTb = k.alloc("kTb", [128, 2, TT], BF16)
    vtok = k.alloc("vtok", [128, NT, 256], BF16)
    ikT4 = k.alloc("ikT4", [128, TT], BF16)
    k.dma("sp", kTb[:], akT.ap().rearrange("h d t -> d h t"), r=["plain"], w=[kTb])
    k.dma("sp", vtok[:], av[0:T, :].rearrange("(t p) c -> p t c", p=128), r=["av"], w=[vtok])
    k.dma("sp", ikT4[:], ikTd.ap(), r=["ikTd"], w=[ikT4])
    ones_f4 = k.alloc("ones_f4", [128, 128], F32)
    zeros_b = k.alloc("zeros_b", [128, 128], BF16)
    Jm = k.alloc("Jm", [128, 128], F32)
    CMT = k.alloc("CMT", [128, 128], F32)
    CM = k.alloc("CM", [128, 128], F32)
    k.op("pool", lambda e: e.memset(ones_f4[:], 1.0), w=[ones_f4])
    k.op("pool", lambda e: e.memset(zeros_b[:], 0.0), w=[zeros_b])
    k.op("pool", lambda e: e.memset(Jm[:], 1.0), w=[Jm])
    k.op("pool", lambda e: e.affine_select(out=Jm[:], in_=Jm[:], pattern=[[1, 128]], compare_op=ALU.is_equal, fill=0.0, base=-127, channel_multiplier=1), r=[Jm], w=[Jm])
    k.op("pool", lambda e: e.memset(CMT[:], 0.0), w=[CMT])
    k.op("pool", lambda e: e.affine_select(out=CMT[:], in_=CMT[:], pattern=[[1, 128]], compare_op=ALU.is_ge, fill=NEG, base=0, channel_multiplier=-1), r=[CMT], w=[CMT])
    k.op("pool", lambda e: e.memset(CM[:], 0.0), w=[CM])
    k.op("pool", lambda e: e.affine_select(out=CM[:], in_=CM[:], pattern=[[-1, 128]], compare_op=ALU.is_ge, fill=NEG, base=0, channel_multiplier=1), r=[CM], w=[CM])
    rb = k.alloc("rb", [32, 8], F32)
    rb31 = k.alloc("rb31", [32, 8], F32)
    bohs = k.alloc("bohs", [32, 384], F32)
    bvec = k.alloc("bvec", [8, 384], F32)
    Tp = k.alloc("Tp", [128, 8, 128], F32)
    Bt4 = [k.alloc(f"Bt4_{i}", [128, 8, 128], F32) for i in range(2)]
    k.dma("sp", rb[:], rel_bias.ap(), w=[rb])
    k.dma("sp", rb31[:], rel_bias[31:32, :].to_broadcast([32, 8]), w=[rb31])
    k.dma("sp", bohs[:], boh.ap(), w=[bohs])
    k.op("dve", lambda e: e.tensor_tensor(out=rb[:], in0=rb[:], in1=rb31[:], op=ALU.subtract), r=[rb, rb31], w=[rb])
    bkb = k.bank()
    k.op("pe", lambda e: e.matmul(bkb[0:8, 0:384], lhsT=rb[:], rhs=bohs[:], start=True, stop=True), r=[rb, bohs], w=[bkb])
    k.op("act", lambda e: e.copy(out=bvec[:], in_=bkb[0:8, 0:384]), r=[bkb], w=[bvec])
    k.dma("sp", biasd.ap(), bvec[:], r=[bvec], w=["biasd"])

    def mk_bias(dl):
        k.dma("sp", Tp[:], bass.AP(tensor=biasd, offset=128 * dl, ap=[[1, 128], [384, 8], [1, 128]]), r=["biasd"], w=[Tp])
        for half in range(2):
            bj = k.bank()
            k.op("pe", lambda e, bj=bj, half=half: e.matmul(bj[:, :], lhsT=Jm[:], rhs=Tp[:].rearrange("p a b -> p (a b)")[:, half * 512:(half + 1) * 512], start=True, stop=True), r=[Jm, Tp], w=[bj])
            k.op("act", lambda e, bj=bj, half=half: e.copy(out=Bt4[dl][:].rearrange("p a b -> p (a b)")[:, half * 512:(half + 1) * 512], in_=bj[:, :]), r=[bj], w=[Bt4[dl]])

    mk_bias(0)
    mk_bias(1)

    qbT = [k.alloc(f"qbT{i}", [128, 8, 128], BF16) for i in range(2)]
    qiT = [k.alloc(f"qiT{i}", [128, 16, 128], BF16) for i in range(2)]
    iwt = [k.alloc(f"iwt{i}", [128, 48], F32) for i in range(2)]
    scs = [k.alloc(f"sc{i}", [128, T], F32) for i in range(2)]
    sc = scs[0]
    Dws = [k.alloc(f"Dw{i}", [128, 16, 128], BF16) for i in range(2)]
    jnk = k.alloc("jnk", [128, T], BF16)
    rl = [k.alloc(f"rl{i}", [128, 512], BF16) for i in range(4)]
    rcnt = [0]
    bs = k.alloc("bs", [128, 8], F32)
    negT = k.alloc("negT", [128, NT, 128], BF16)
    PTs = [k.alloc(f"PTs{i}", [128, 4, 128], BF16) for i in range(6)]
    Bt4b = [k.alloc(f"Bt4b_{i}", [128, 8, 128], BF16) for i in range(2)]
    for i_ in range(2):
        k.op("dve", lambda e, i_=i_: e.tensor_copy(out=Bt4b[i_][:], in_=Bt4[i_][:]), r=[Bt4[i_]], w=[Bt4b[i_]])
    rden = k.alloc("rden", [128, 8], F32)
    oab = k.alloc("oab", [128, 8, 128], BF16)
    ecnt = [0]

    def stage_I(qb):
        L = 128 * (qb + 1)
        tsl = slice(qb * 128, (qb + 1) * 128)
        qb_ = qbT[qb % 2]
        qi_ = qiT[qb % 2]
        iw_ = iwt[qb % 2]
        sc = scs[qb % 2]
        Dw_ = Dws[qb % 2]
        k.dma("sp", qb_[:], aq.ap()[:, :, tsl].rearrange("h d t -> d h t"), r=["plain"], w=[qb_])
        k.dma("sp", qi_[:], iq.ap()[:, :, tsl].rearrange("h d t -> d h t"), r=["plain"], w=[qi_])
        k.dma("sp", iw_[:, 0:16], iw[qb * 128:(qb + 1) * 128, :], r=["iw"], w=[iw_])
        if qb < 2:
            return
        for h in range(16):
            k.op("act", lambda e, h=h: e.activation(out=Dw_[:, h, :], in_=ident_f[:], func=AF.Copy, scale=iw_[:, h:h + 1]), r=[ident_f, iw_], w=[Dw_])

        def idx_kg(kg):
            c0 = kg * 512
            n = min(512, L - c0)
            bacc = k.bank()
            k.reserved = {bacc.name}
            pend = []

            def acc(h, r_):
                k.op("pe", lambda e: e.matmul(bacc[:, 0:n], lhsT=Dw_[:, h, :], rhs=r_[:, 0:n], start=(h == 0), stop=(h == 15)), r=[Dw_, r_], w=[bacc])

            for h in range(16):
                bk_ = k.bank()
                r_ = rl[rcnt[0] % 4]
                rcnt[0] += 1
                k.op("pe", lambda e, h=h, bk_=bk_: e.matmul(bk_[:, 0:n], lhsT=qi_[:, h, :], rhs=ikT4[:, c0:c0 + n], start=True, stop=True), r=[qi_, ikT4], w=[bk_])
                k.op("act", lambda e, bk_=bk_, r_=r_: e.activation(out=r_[:, 0:n], in_=bk_[:, 0:n], func=AF.Relu), r=[bk_], w=[r_])
                pend.append((h, r_))
                if len(pend) > 2:
                    acc(*pend.pop(0))
            while pend:
                acc(*pend.pop(0))
            k.reserved = set()
            k.op("act", lambda e: e.copy(out=sc[:, c0:c0 + n], in_=bacc[:, 0:n]), r=[bacc], w=[sc])

        for kg in range((L + 511) // 512):
            idx_kg(kg)

    def stage_B(qb):
        L = 128 * (qb + 1)
        sc = scs[qb % 2]
        if qb >= 2:
            k.op("dve", lambda e: e.tensor_reduce(out=bs[:, 0:1], in_=sc[:, 0:L], axis=AX.X, op=ALU.max), r=[sc], w=[bs])
            k.op("dve", lambda e: e.tensor_reduce(out=bs[:, 1:2], in_=sc[:, 0:L], axis=AX.X, op=ALU.min), r=[sc, bs], w=[bs])
            k.op("dve", lambda e: e.tensor_scalar(out=bs[:, 1:2], in0=bs[:, 1:2], scalar1=-1.0, scalar2=None, op0=ALU.add), r=[bs], w=[bs])
            k.op("dve", lambda e: e.tensor_tensor(out=bs[:, 2:3], in0=bs[:, 0:1], in1=bs[:, 1:2], op=ALU.subtract), r=[bs], w=[bs])
            k.op("dve", lambda e: e.tensor_tensor(out=sc[:, L - 128:L], in0=sc[:, L - 128:L], in1=CM[:], op=ALU.add), r=[sc, CM], w=[sc])
            for it in range(NIT):
                f = 2.0 ** -(it + 1)
                k.op("dve", lambda e, f=f: e.scalar_tensor_tensor(out=bs[:, 3:4], in0=bs[:, 2:3], scalar=f, op0=ALU.mult, in1=bs[:, 1:2], op1=ALU.add), r=[bs], w=[bs])
                k.op("dve", lambda e: e.tensor_scalar(out=jnk[:, 0:L], in0=sc[:, 0:L], scalar1=bs[:, 3:4], scalar2=None, op0=ALU.is_gt, op1=ALU.add, accum_out=bs[:, 4:5]),
                     r=[sc, bs], w=[jnk, bs])
                k.op("dve", lambda e, f=f: e.tensor_scalar(out=bs[:, 5:6], in0=bs[:, 4:5], scalar1=255.5, scalar2=f, op0=ALU.is_gt, op1=ALU.mult), r=[bs], w=[bs])
                k.op("dve", lambda e: e.scalar_tensor_tensor(out=bs[:, 1:2], in0=bs[:, 5:6], scalar=bs[:, 2:3], op0=ALU.mult, in1=bs[:, 1:2], op1=ALU.add), r=[bs], w=[bs])
            k.op("dve", lambda e: e.tensor_scalar(out=sc[:, 0:L], in0=sc[:, 0:L], scalar1=bs[:, 1:2], scalar2=None, op0=ALU.subtract), r=[sc, bs], w=[sc])
            for k4 in range((qb + 1 + 3) // 4):
                nb = min(4, qb + 1 - k4 * 4)
                bk_ = k.bank()
                for j in range(nb):
                    kb = k4 * 4 + j
                    k.op("pe", lambda e, j=j, kb=kb, bk_=bk_: e.transpose(out=bk_[:, j * 128:(j + 1) * 128], in_=sc[:, kb * 128:(kb + 1) * 128], identity=ident_f[:]),
                         r=[sc, ident_f], w=[bk_])
                k.op("dve", lambda e, k4=k4, nb=nb, bk_=bk_: e.tensor_scalar(out=negT[:, k4 * 4:k4 * 4 + nb, :].rearrange("p a b -> p (a b)"), in0=bk_[:, 0:nb * 128],
                                                                             scalar1=0.0, scalar2=NEG, op0=ALU.is_le, op1=ALU.mult), r=[bk_], w=[negT])
            k.op("dve", lambda e: e.tensor_tensor(out=negT[:, qb, :], in0=negT[:, qb, :], in1=CMT[:], op=ALU.add), r=[negT, CMT], w=[negT])
        else:
            if qb == 1:
                k.op("pool", lambda e: e.memset(negT[:, 0, :], 0.0), w=[negT])
            k.op("pool", lambda e: e.tensor_copy(out=negT[:, qb, :], in_=CMT[:]), r=[CMT, negT], w=[negT])

    def stage_A(qb):
        tsl = slice(qb * 128, (qb + 1) * 128)
        qb_ = qbT[qb % 2]
        qi_ = qiT[qb % 2]
        bo0 = k.bank()
        bo1 = k.bank()
        bdn = k.bank()
        k.reserved = {bo0.name, bo1.name, bdn.name}
        for bz_ in (bo0, bo1, bdn):
            k.op("pe", lambda e, bz_=bz_: e.matmul(bz_[:, :], lhsT=zeros_b[:], rhs=qi_[:].rearrange("p a b -> p (a b)")[:, 0:512], start=True, stop=False), r=[zeros_b, qi_], w=[bz_])
        def kv_logits(kb, kvh):
            bl = k.bank()
            near = (qb - kb <= 1)
            k.op("pe", lambda e, bl=bl: e.matmul(bl[:, :], lhsT=kTb[:, kvh, kb * 128:(kb + 1) * 128], rhs=qb_[:, kvh * 4:(kvh + 1) * 4, :].rearrange("p a b -> p (a b)"),
                                                start=True, stop=False), r=[kTb, qb_], w=[bl])
            k.op("pe", lambda e, bl=bl: e.matmul(bl[:, :].rearrange("p (a b) -> p a b", a=4), lhsT=ident_b[:], rhs=negT[:, kb, :].unsqueeze(1).to_broadcast([128, 4, 128]),
                                                start=False, stop=(not near)), r=[ident_b, negT], w=[bl])
            if near:
                k.op("pe", lambda e, bl=bl: e.matmul(bl[:, :], lhsT=ident_b[:], rhs=Bt4b[qb - kb][:, kvh * 4:(kvh + 1) * 4, :].rearrange("p a b -> p (a b)"),
                                                    start=False, stop=True), r=[ident_b, Bt4b[qb - kb]], w=[bl])
            P_ = PTs[ecnt[0] % 6]
            ecnt[0] += 1
            k.op("act", lambda e, bl=bl, P_=P_: e.activation(out=P_[:].rearrange("p a b -> p (a b)"), in_=bl[:, :], func=AF.Exp), r=[bl], w=[P_])
            return P_

        def kv_pv(kb, kvh, P_):
            last = (kb == qb)
            for g_ in range(4):
                h = kvh * 4 + g_
                bo = bo0 if h < 4 else bo1
                k.op("pe", lambda e, g_=g_, h=h, bo=bo: e.matmul(bo[:, (h % 4) * 128:(h % 4 + 1) * 128], lhsT=P_[:, g_, :], rhs=vtok[:, kb, kvh * 128:(kvh + 1) * 128],
                                                                 start=False, stop=last), r=[P_, vtok], w=[bo])
                k.op("pe", lambda e, g_=g_, h=h: e.matmul(bdn[:, h:h + 1], lhsT=P_[:, g_, :], rhs=ones_b[:, 0:1], start=False, stop=last), r=[P_, ones_b], w=[bdn])

        pend = []
        for kb in range(qb + 1):
            for kvh in range(2):
                pend.append((kb, kvh, kv_logits(kb, kvh)))
                if len(pend) > 3:
                    kv_pv(*pend.pop(0))
        while pend:
            kv_pv(*pend.pop(0))
        k.reserved = set()
        k.op("dve", lambda e: e.reciprocal(out=rden[:], in_=bdn[:, 0:8]), r=[bdn], w=[rden])
        for half, bo in ((0, bo0), (1, bo1)):
            k.op("dve", lambda e, half=half, bo=bo: e.tensor_tensor(out=oab[:, half * 4:(half + 1) * 4, :], in0=bo[:, :].rearrange("p (a b) -> p a b", a=4),
                                                                    in1=rden[:, half * 4:(half + 1) * 4].unsqueeze(2).to_broadcast([128, 4, 128]), op=ALU.mult),
                 r=[bo, rden], w=[oab])
        bt_ = k.bank()
        btb = bt_[:].bitcast(BF16)
        for h in range(8):
            k.op("pe", lambda e, h=h: e.transpose(out=btb[:, h * 128:(h + 1) * 128], in_=oab[:, h, :], identity=ident_b[:]), r=[oab, ident_b], w=[bt_])
        k.op("act", lambda e: e.copy(out=mixT[:, 8:16, tsl], in_=btb[:, 0:1024].rearrange("p (a b) -> p a b", a=8)), r=[bt_], w=[(mixT, "b%d" % qb)])

    stage_I(0)
    for qb in range(NT):
        if qb + 1 < NT:
            stage_I(qb + 1)
        stage_B(qb)
        stage_A(qb)
    if stop_after == "P4" and os.environ.get("P4DBG"):
        for nm, b, n in (("sc", sc, T), ("bs", bs, 8), ("negT", negT, NT * 128)):
            dd = dout("dbg_" + nm, [128, n], F32)
            src = b[:] if nm != "negT" else b[:].rearrange("p a b -> p (a b)")
            k.dma("sp", dd.ap(), src, r=[b])
    if stop_after == "P4":
        dbg = dout("dbg_mixT", [128, 16 * TT], BF16)
        k.barrier()
        k.dma("sp", dbg.ap(), mixT[:].rearrange("p a b -> p (a b)"))
        return k.finish()
    k.barrier()
    k.release(m4)
    NPG = NPAGES
    ones4 = k.alloc("ones4", [128, 128], F32)
    zeros4 = k.alloc("zeros4", [128, 128], F32)
    Ltri = k.alloc("Ltri", [128, 128], BF16)
    siota = k.alloc("siota", [128, 128], F32)
    piota = k.alloc("piota", [128, 1], F32)
    jrow = k.alloc("jrow", [128, 256], F32)
    jcol = k.alloc("jcol", [128, 2], F32)
    posc = k.alloc("posc", [128, 128], F32)
    k.op("pool", lambda e: e.memset(ones4[:], 1.0), w=[ones4])
    k.op("pool", lambda e: e.memset(zeros4[:], 0.0), w=[zeros4])
    k.op("pool", lambda e: e.memset(Ltri[:], 1.0), w=[Ltri])
    k.op("pool", lambda e: e.affine_select(out=Ltri[:], in_=Ltri[:], pattern=[[1, 128]], compare_op=ALU.is_ge, fill=0.0, base=-1, channel_multiplier=-1), r=[Ltri], w=[Ltri])
    k.op("pool", lambda e: e.iota(siota[:], pattern=[[1, 128]], base=0, channel_multiplier=0, allow_small_or_imprecise_dtypes=True), w=[siota])
    k.op("pool", lambda e: e.iota(piota[:], pattern=[[0, 1]], base=0, channel_multiplier=1, allow_small_or_imprecise_dtypes=True), w=[piota])
    k.op("pool", lambda e: e.iota(jrow[:], pattern=[[1, 256]], base=0, channel_multiplier=0, allow_small_or_imprecise_dtypes=True), w=[jrow])
    k.op("pool", lambda e: e.iota(jcol[:], pattern=[[128, 2]], base=0, channel_multiplier=1, allow_small_or_imprecise_dtypes=True), w=[jcol])
    k.op("pool", lambda e: e.iota(posc[:], pattern=[[1, 128]], base=0, channel_multiplier=128, allow_small_or_imprecise_dtypes=True), w=[posc])
    thrb = k.alloc("thrb", [128, 31], F32)
    k.dma("sp", thrb[:], bthr.ap().to_broadcast([128, 31]), w=[thrb])
    rbs = k.alloc("rbs", [32, 8], F32)
    rbT = k.alloc("rbT", [8, 32], F32)
    drbT = k.alloc("drbT", [128, 8, 32], F32)
    k.dma("sp", rbs[:], rel_bias.ap(), w=[rbs])
    bq = k.bank()
    k.op("pe", lambda e: e.transpose(out=bq[0:8, 0:32], in_=rbs[:], identity=ident_f[0:32, 0:32]), r=[rbs, ident_f], w=[bq])
    k.op("act", lambda e: e.copy(out=rbT[:], in_=bq[0:8, 0:32]), r=[bq], w=[rbT])
    k.dma("sp", rbTd.ap(), rbT[:], r=[rbT], w=["rbTd"])
    k.dma("sp", drbT[:].rearrange("p a b -> p (a b)"), rbTd.ap().rearrange("a b -> (a b)").unsqueeze(0).to_broadcast([128, 256]), r=["rbTd"], w=[drbT])
    rb0b = k.alloc("rb0b", [128, 8], F32)
    k.op("dve", lambda e: e.tensor_copy(out=rb0b[:], in_=drbT[:, :, 0]), r=[drbT], w=[rb0b])
    dtmp = k.alloc("dtmp", [128, 8, 31], F32)
    k.op("dve", lambda e: e.tensor_tensor(out=dtmp[:], in0=drbT[:, :, 1:32], in1=drbT[:, :, 0:31], op=ALU.subtract), r=[drbT], w=[dtmp])
    pt_i = k.alloc("pt_i", [128, TS], I32)
    pt_f = k.alloc("pt_f", [128, TS], F32)
    k.dma("sp", pt_i[:], page_table.ap().rearrange("s p -> p s"), w=[pt_i], slow=True)
    k.op("dve", lambda e: e.tensor_copy(out=pt_f[:], in_=pt_i[:]), r=[pt_i], w=[pt_f])
    pt2f = k.alloc("pt2f", [128, TS, 2], F32)
    pt2i = k.alloc("pt2i", [128, TS, 2], I32)
    for hf_ in range(2):
        k.op("dve", lambda e, hf_=hf_: e.tensor_scalar(out=pt2f[:, :, hf_], in0=pt_f[:], scalar1=2.0, scalar2=float(hf_), op0=ALU.mult, op1=ALU.add), r=[pt_f], w=[pt2f])
    k.op("dve", lambda e: e.tensor_copy(out=pt2i[:], in_=pt2f[:]), r=[pt2f], w=[pt2i])
    k.op("dve", lambda e: e.tensor_scalar(out=pt_f[:], in0=pt_f[:], scalar1=128.0, scalar2=None, op0=ALU.mult), r=[pt_f, pt2f], w=[pt_f])
    qiS = k.alloc("qiS", [128, TS, 16], BF16)
    wS = k.alloc("wS", [128, TS, 16], F32)
    kiS = k.alloc("kiS", [128, TS], BF16)
    qbS = k.alloc("qbS", [128, 8, TS], BF16)
    knS = k.alloc("knS", [128, 2, TS], BF16)
    for s_i in range(TS):
        k.dma("sp", qiS[:, s_i, :], iq.ap()[:, :, T + s_i].rearrange("h d -> d h"), r=["plain"], w=[qiS], slow=True)
    k.dma("sp", wS[:].rearrange("p a b -> p (a b)"), iw[T:TT, :].rearrange("a b -> (a b)").unsqueeze(0).to_broadcast([128, TS * 16]), r=["iw"], w=[wS])
    k.dma("sp", kiS[:], ikTd[:, T:TT], r=["ikTd"], w=[kiS])
    k.dma("sp", qbS[:], aq.ap()[:, :, T:TT].rearrange("h d s -> d h s"), r=["plain"], w=[qbS])
    k.dma("sp", knS[:], akT.ap()[:, :, T:TT].rearrange("h d s -> d h s"), r=["plain"], w=[knS])
    scS = k.alloc("scS", [128, TS, 128], F32)
    snew = k.alloc("snew", [128, TS], F32)
    Gh = [k.alloc(f"Gh{i}", [128, 64 * 128], F32) for i in range(2)]
    ghc = [0]
    kTs = [k.alloc(f"kTs{i}", [128, 4, 128], BF16) for i in range(2)]
    rr = k.alloc("rr", [128, 32, 16], F32)
    knb = k.alloc("knb", [128, 128], BF16)
    ckx2 = cache_kidx.ap().rearrange("n (h e) -> (n h) e", h=2)

    def dma_raw(q, fn, r=(), w=()):
        i = k.drr[q]
        k.drr[q] = (i + 1) % len(k.dsem[q])
        sk = ("d", q, i)
        waits = k._collect(q, r, w)
        prev = k.dcnt[q][i]
        kn = k.known[q]
        if prev > 0 and kn.get(sk, 0) < prev:
            kn[sk] = prev
            waits.append((sk, prev))
        k.dcnt[q][i] = prev + 16
        tok = (sk, prev + 16)
        k.ops[q].append((waits, fn, (sk, 16)))
        k._update(tok, r, w)
        return tok

    def scores_seq(s_i):
        scb = [k.bank() for _ in range(4)]
        for hf_ in range(2):
            Gp = Gh[ghc[0] % 2]
            ghc[0] += 1
            dma_raw("pool", lambda e, Gp=Gp, hf_=hf_: e.indirect_dma_start(out=Gp[:], out_offset=None, in_=ckx2, in_offset=bass.IndirectOffsetOnAxis(ap=pt2i[:, s_i, hf_:hf_ + 1], axis=0)),
                    r=[pt2i], w=[Gp])
            k.reserved = {b_.name for b_ in scb}
            for sb in range(16):
                bt_ = k.bank()
                kt_ = kTs[sb % 2]
                for j in range(4):
                    sl_ = 4 * sb + j
                    k.op("pe", lambda e, j=j, sl_=sl_, bt_=bt_, Gp=Gp: e.transpose(out=bt_[:, j * 128:(j + 1) * 128], in_=Gp[:, sl_ * 128:(sl_ + 1) * 128], identity=ident_f[:]),
                         r=[Gp, ident_f], w=[bt_])
                k.op("act", lambda e, bt_=bt_, kt_=kt_: e.copy(out=kt_[:].rearrange("p a b -> p (a b)"), in_=bt_[:, :]), r=[bt_], w=[kt_])
                for j in range(4):
                    sl_ = hf_ * 64 + 4 * sb + j
                    sbk = scb[sl_ // 32]
                    k.op("pe", lambda e, j=j, sl_=sl_, sbk=sbk, kt_=kt_: e.matmul(sbk[:, (sl_ % 32) * 16:(sl_ % 32 + 1) * 16], lhsT=kt_[:, j, :], rhs=qiS[:, s_i, :], start=True, stop=True),
                         r=[kt_, qiS], w=[sbk])
        k.reserved = set()
        for b_i in range(4):
            sbk = scb[b_i]
            k.op("dve", lambda e, sbk=sbk: e.tensor_scalar(out=rr[:].rearrange("p a b -> p (a b)"), in0=sbk[:, :], scalar1=0.0, scalar2=None, op0=ALU.max), r=[sbk], w=[rr])
            k.op("dve", lambda e: e.tensor_tensor(out=rr[:], in0=rr[:], in1=wS[:, s_i, :].unsqueeze(1).to_broadcast([128, 32, 16]), op=ALU.mult), r=[rr, wS], w=[rr])
            k.op("dve", lambda e, b_i=b_i: e.tensor_reduce(out=scS[:, s_i, b_i * 32:(b_i + 1) * 32], in_=rr[:], axis=AX.X, op=ALU.add), r=[rr], w=[scS])
        k.op("dve", lambda e: e.tensor_copy(out=knb[:], in_=kiS[:, s_i:s_i + 1].to_broadcast([128, 128])), r=[kiS], w=[knb])
        bn = k.bank()
        k.op("pe", lambda e: e.matmul(bn[:, 0:16], lhsT=knb[:], rhs=qiS[:, s_i, :], start=True, stop=True), r=[knb, qiS], w=[bn])
        k.op("dve", lambda e: e.tensor_scalar(out=rr[:, 0, :], in0=bn[:, 0:16], scalar1=0.0, scalar2=None, op0=ALU.max), r=[bn], w=[rr])
        k.op("dve", lambda e: e.tensor_tensor(out=rr[:, 0, :], in0=rr[:, 0, :], in1=wS[:, s_i, :], op=ALU.mult), r=[rr, wS], w=[rr])
        k.op("dve", lambda e: e.tensor_reduce(out=snew[:, s_i:s_i + 1], in_=rr[:, 0, :], axis=AX.X, op=ALU.add), r=[rr], w=[snew])

    for s_i in range(0 if skip_p4s else TS):
        scores_seq(s_i)

    pm = k.alloc("pm", [128, 2 * TS], F32)
    gmm = k.alloc("gmm", [TS, 4], F32)
    dgm = k.alloc("dgm", [TS, 2 * TS], F32)
    lo_ = k.alloc("lo_", [128, TS], F32)
    w0_ = k.alloc("w0_", [128, TS], F32)
    bsS = k.alloc("bsS", [128, 6 * TS], F32)
    cmpb = k.alloc("cmpb", [128, TS, 128], F32)
    k.op("dve", lambda e: e.tensor_reduce(out=pm[:, 0:TS], in_=scS[:], axis=AX.X, op=ALU.max), r=[scS], w=[pm])
    k.op("dve", lambda e: e.tensor_reduce(out=pm[:, TS:2 * TS], in_=scS[:], axis=AX.X, op=ALU.min), r=[scS, pm], w=[pm])
    k.op("dve", lambda e: e.tensor_tensor(out=pm[:, 0:TS], in0=pm[:, 0:TS], in1=snew[:], op=ALU.max), r=[pm, snew], w=[pm])
    k.op("dve", lambda e: e.tensor_tensor(out=pm[:, TS:2 * TS], in0=pm[:, TS:2 * TS], in1=snew[:], op=ALU.min), r=[pm, snew], w=[pm])
    bmx = k.bank()
    bmn = k.bank()
    k.op("pe", lambda e: e.transpose(out=bmx[0:TS, 0:128], in_=pm[:, 0:TS], identity=ident_f[:]), r=[pm, ident_f], w=[bmx])
    k.op("pe", lambda e: e.transpose(out=bmn[0:TS, 0:128], in_=pm[:, TS:2 * TS], identity=ident_f[:]), r=[pm, ident_f], w=[bmn])
    k.op("dve", lambda e: e.tensor_reduce(out=gmm[:, 0:1], in_=bmx[0:TS, 0:128], axis=AX.X, op=ALU.max), r=[bmx], w=[gmm])
    k.op("dve", lambda e: e.tensor_reduce(out=gmm[:, 1:2], in_=bmn[0:TS, 0:128], axis=AX.X, op=ALU.min), r=[bmn, gmm], w=[gmm])
    k.op("dve", lambda e: e.tensor_scalar(out=gmm[:, 1:2], in0=gmm[:, 1:2], scalar1=-1.0, scalar2=None, op0=ALU.add), r=[gmm], w=[gmm])
    k.op("dve", lambda e: e.tensor_tensor(out=gmm[:, 2:3], in0=gmm[:, 0:1], in1=gmm[:, 1:2], op=ALU.subtract), r=[gmm], w=[gmm])
    k.op("dve", lambda e: e.tensor_scalar(out=dgm[:, 0:TS], in0=ident_f[0:TS, 0:TS], scalar1=gmm[:, 1:2], scalar2=None, op0=ALU.mult), r=[gmm, ident_f], w=[dgm])
    k.op("dve", lambda e: e.tensor_scalar(out=dgm[:, TS:2 * TS], in0=ident_f[0:TS, 0:TS], scalar1=gmm[:, 2:3], scalar2=None, op0=ALU.mult), r=[gmm, ident_f, dgm], w=[dgm])
    bbc = k.bank()
    k.op("pe", lambda e: e.matmul(bbc[:, 0:2 * TS], lhsT=ones4[0:TS, :], rhs=dgm[:], start=True, stop=True), r=[ones4, dgm], w=[bbc])
    k.op("act", lambda e: e.copy(out=lo_[:], in_=bbc[:, 0:TS]), r=[bbc], w=[lo_])
    k.op("act", lambda e: e.copy(out=w0_[:], in_=bbc[:, TS:2 * TS]), r=[bbc], w=[w0_])
    mid_ = bsS[:, 0:TS]
    cnt_ = bsS[:, TS:2 * TS]
    gn_ = bsS[:, 2 * TS:3 * TS]
    tot_ = bsS[:, 3 * TS:4 * TS]
    ge_ = bsS[:, 4 * TS:5 * TS]

    def bis_iter(it):
        f = 2.0 ** -(it + 1)
        k.op("dve", lambda e: e.scalar_tensor_tensor(out=mid_, in0=w0_[:], scalar=f, op0=ALU.mult, in1=lo_[:], op1=ALU.add), r=[w0_, lo_], w=[bsS])
        k.op("dve", lambda e: e.tensor_tensor(out=cmpb[:], in0=scS[:], in1=mid_.unsqueeze(2).to_broadcast([128, TS, 128]), op=ALU.is_gt), r=[scS, bsS], w=[cmpb])
        k.op("dve", lambda e: e.tensor_reduce(out=cnt_, in_=cmpb[:], axis=AX.X, op=ALU.add), r=[cmpb, bsS], w=[bsS])
        bc_ = k.bank()
        k.op("pe", lambda e: e.matmul(bc_[:, 0:TS], lhsT=ones4[:], rhs=cnt_, start=True, stop=True), r=[ones4, bsS], w=[bc_])
        k.op("dve", lambda e: e.tensor_tensor(out=gn_, in0=snew[:], in1=mid_, op=ALU.is_gt), r=[snew, bsS], w=[bsS])
        k.op("dve", lambda e: e.tensor_tensor(out=tot_, in0=bc_[:, 0:TS], in1=gn_, op=ALU.add), r=[bc_, bsS], w=[bsS])
        k.op("dve", lambda e: e.tensor_scalar(out=ge_, in0=tot_, scalar1=255.5, scalar2=f, op0=ALU.is_gt, op1=ALU.mult), r=[bsS], w=[bsS])
        k.op("dve", lambda e: e.tensor_tensor(out=ge_, in0=ge_, in1=w0_[:], op=ALU.mult), r=[bsS, w0_], w=[bsS])
        k.op("dve", lambda e: e.tensor_tensor(out=lo_[:], in0=lo_[:], in1=ge_, op=ALU.add), r=[lo_, bsS], w=[lo_])

    for it in range(0 if skip_p4s else 20):
        bis_iter(it)

    Msel = k.alloc("Msel", [128, 128], F32)
    Mb = k.alloc("Mb", [128, 128], BF16)
    Bs = k.alloc("Bs", [128, 128], F32)
    cum = k.alloc("cum", [128, 128], F32)
    rank = k.alloc("rank", [128, 128], F32)
    payl = k.alloc("payl", [128, 128, 2], F32)
    Soh = [k.alloc(f"Soh{i}", [128, 256], F32) for i in range(2)]
    idxf = k.alloc("idxf", [128, 4], F32)
    idx_i = k.alloc("idx_i", [128, 2], I32)
    Ksel = k.alloc("Ksel", [128, 2, 256], F32)
    Vsel = k.alloc("Vsel", [128, 2, 256], F32)
    KTs = k.alloc("KTs", [128, 4, 128], F32)
    qf = k.alloc("qf", [128, 8], F32)
    knf = k.alloc("knf", [128, 2], F32)
    sm = k.alloc("sm", [128, 64], F32)
    ind = k.alloc("ind", [128, 2, 31], F32)
    prod = k.alloc("prod", [128, 2, 8, 31], F32)
    Eg = k.alloc("Eg", [128, 2, 8], F32)
    rowp = k.alloc("rowp", [1, 64], F32)
    vnr = k.alloc("vnr", [1, 256], BF16)
    vnf = k.alloc("vnf", [1, 256], F32)
    osb = k.alloc("osb", [4, 2, 130], F32)
    ck2 = cache_k.ap()
    cv2 = cache_v.ap()
    k.op("dve", lambda e: e.tensor_copy(out=payl[:, :, 1], in_=posc[:]), r=[posc], w=[payl])

    def attend_seq(s_i):
        col = T + s_i
        k.op("dve", lambda e: e.tensor_scalar(out=Msel[:], in0=scS[:, s_i, :], scalar1=lo_[:, s_i:s_i + 1], scalar2=None, op0=ALU.is_gt), r=[scS, lo_], w=[Msel])
        k.op("dve", lambda e: e.tensor_copy(out=Mb[:], in_=Msel[:]), r=[Msel], w=[Mb])
        k.op("dve", lambda e: e.tensor_tensor(out=sm[:, 0:1], in0=snew[:, s_i:s_i + 1], in1=lo_[:, s_i:s_i + 1], op=ALU.is_gt), r=[snew, lo_], w=[sm])
        bA_ = k.bank()
        bB_ = k.bank()
        k.op("pe", lambda e: e.matmul(bA_[:, 0:128], lhsT=Ltri[:], rhs=Mb[:], start=True, stop=True), r=[Ltri, Mb], w=[bA_])
        k.op("pe", lambda e: e.matmul(bB_[:, 0:128], lhsT=ones_b[:], rhs=Mb[:], start=True, stop=True), r=[ones_b, Mb], w=[bB_])
        k.op("act", lambda e: e.copy(out=Bs[:], in_=bB_[:, 0:128]), r=[bB_], w=[Bs])
        k.op("dve", lambda e: e.tensor_tensor_scan(out=cum[:], data0=Bs[:], data1=zeros4[:], initial=0.0, op0=ALU.add, op1=ALU.add), r=[Bs, zeros4], w=[cum])
        k.op("dve", lambda e: e.tensor_tensor(out=rank[:], in0=cum[:], in1=Bs[:], op=ALU.subtract), r=[cum, Bs], w=[rank])
        k.op("dve", lambda e: e.tensor_tensor(out=rank[:], in0=rank[:], in1=bA_[:, 0:128], op=ALU.add), r=[rank, bA_], w=[rank])
        k.op("dve", lambda e: e.scalar_tensor_tensor(out=rank[:], in0=rank[:], scalar=1.0, op0=ALU.add, in1=Msel[:], op1=ALU.mult), r=[rank, Msel], w=[rank])
        k.op("dve", lambda e: e.tensor_scalar(out=rank[:], in0=rank[:], scalar1=-1.0, scalar2=None, op0=ALU.add), r=[rank], w=[rank])
        k.op("dve", lambda e: e.tensor_scalar(out=payl[:, :, 0], in0=siota[:], scalar1=pt_f[:, s_i:s_i + 1], scalar2=None, op0=ALU.add), r=[siota, pt_f], w=[payl])
        bacc = k.bank()
        k.reserved = {bacc.name}
        k.op("pe", lambda e: e.matmul(bacc[:, 0:4], lhsT=zeros4[:], rhs=ones4[:, 0:4], start=True, stop=False), r=[zeros4, ones4], w=[bacc])
        for sl_ in range(128):
            so_ = Soh[sl_ % 2]
            k.op("dve", lambda e, sl_=sl_, so_=so_: e.tensor_scalar(out=so_[:], in0=jrow[:], scalar1=rank[:, sl_:sl_ + 1], scalar2=None, op0=ALU.is_equal), r=[jrow, rank], w=[so_])
            for half in range(2):
                k.op("pe", lambda e, sl_=sl_, so_=so_, half=half: e.matmul(bacc[:, half * 2:(half + 1) * 2], lhsT=so_[:, half * 128:(half + 1) * 128], rhs=payl[:, sl_, :],
                                                                      start=False, stop=(sl_ == 127)), r=[so_, payl], w=[bacc])
        k.reserved = set()
        k.op("act", lambda e: e.copy(out=idxf[:], in_=bacc[:, 0:4]), r=[bacc], w=[idxf])
        k.op("dve", lambda e: e.tensor_copy(out=idx_i[:], in_=idxf[:].rearrange("p (a b) -> p a b", b=2)[:, :, 0]), r=[idxf], w=[idx_i])
        for half in range(2):
            dma_raw("pool", lambda e, half=half: e.indirect_dma_start(out=Ksel[:, half, :], out_offset=None, in_=ck2, in_offset=bass.IndirectOffsetOnAxis(ap=idx_i[:, half:half + 1], axis=0)),
                    r=[idx_i], w=[Ksel])
            dma_raw("pool", lambda e, half=half: e.indirect_dma_start(out=Vsel[:, half, :], out_offset=None, in_=cv2, in_offset=bass.IndirectOffsetOnAxis(ap=idx_i[:, half:half + 1], axis=0)),
                    r=[idx_i], w=[Vsel])
        bkt = k.bank()
        for half in range(2):
            for kvh in range(2):
                jj = half * 2 + kvh
                k.op("pe", lambda e, half=half, kvh=kvh, jj=jj: e.transpose(out=bkt[:, jj * 128:(jj + 1) * 128], in_=Ksel[:, half, kvh * 128:(kvh + 1) * 128], identity=ident_f[:]),
                     r=[Ksel, ident_f], w=[bkt])
        k.op("act", lambda e: e.copy(out=KTs[:].rearrange("p a b -> p (a b)"), in_=bkt[:, :]), r=[bkt], w=[KTs])
        k.op("dve", lambda e: e.tensor_copy(out=qf[:], in_=qbS[:, :, s_i]), r=[qbS], w=[qf])
        k.op("dve", lambda e: e.tensor_copy(out=knf[:], in_=knS[:, :, s_i]), r=[knS], w=[knf])
        blg = k.bank()
        for half in range(2):
            for kvh in range(2):
                jj = half * 2 + kvh
                k.op("pe", lambda e, half=half, kvh=kvh, jj=jj: e.matmul(blg[:, half * 8 + kvh * 4:half * 8 + kvh * 4 + 4], lhsT=KTs[:, jj, :], rhs=qf[:, kvh * 4:(kvh + 1) * 4], start=True, stop=True),
                     r=[KTs, qf], w=[blg])
        bln = k.bank()
        for kvh in range(2):
            k.op("pe", lambda e, kvh=kvh: e.matmul(bln[0:1, kvh * 4:(kvh + 1) * 4], lhsT=knf[:, kvh:kvh + 1], rhs=qf[:, kvh * 4:(kvh + 1) * 4], start=True, stop=True), r=[knf, qf], w=[bln])
        k.op("dve", lambda e: e.tensor_scalar(out=sm[:, 2:4], in0=idxf[:].rearrange("p (a b) -> p a b", b=2)[:, :, 1], scalar1=-1.0, scalar2=float(NPG * 128), op0=ALU.mult, op1=ALU.add), r=[idxf], w=[sm])
        k.op("dve", lambda e: e.tensor_tensor(out=ind[:], in0=sm[:, 2:4].unsqueeze(2).to_broadcast([128, 2, 31]), in1=thrb[:].unsqueeze(1).to_broadcast([128, 2, 31]), op=ALU.is_ge), r=[sm, thrb], w=[ind])
        k.op("dve", lambda e: e.tensor_tensor(out=prod[:], in0=ind[:].unsqueeze(2).to_broadcast([128, 2, 8, 31]), in1=dtmp[:].unsqueeze(1).to_broadcast([128, 2, 8, 31]), op=ALU.mult), r=[ind, dtmp], w=[prod])
        k.op("dve", lambda e: e.tensor_reduce(out=Eg[:], in_=prod[:], axis=AX.X, op=ALU.add), r=[prod], w=[Eg])
        k.op("dve", lambda e: e.tensor_tensor(out=Eg[:], in0=Eg[:], in1=rb0b[:].unsqueeze(1).to_broadcast([128, 2, 8]), op=ALU.add), r=[Eg, rb0b], w=[Eg])
        k.op("dve", lambda e: e.tensor_scalar(out=sm[:, 4:6], in0=jcol[:], scalar1=cum[:, 127:128], scalar2=None, op0=ALU.is_lt), r=[jcol, cum, sm], w=[sm])
        k.op("dve", lambda e: e.tensor_scalar(out=sm[:, 4:6], in0=sm[:, 4:6], scalar1=-1.0, scalar2=30000.0, op0=ALU.add, op1=ALU.mult), r=[sm], w=[sm])
        k.op("dve", lambda e: e.tensor_tensor(out=Eg[:], in0=Eg[:], in1=sm[:, 4:6].unsqueeze(2).to_broadcast([128, 2, 8]), op=ALU.add), r=[Eg, sm], w=[Eg])
        k.op("dve", lambda e: e.tensor_tensor(out=Eg[:].rearrange("p a b -> p (a b)"), in0=Eg[:].rearrange("p a b -> p (a b)"), in1=blg[:, 0:16], op=ALU.add), r=[Eg, blg], w=[Eg])
        k.op("act", lambda e: e.activation(out=Eg[:], in_=Eg[:], func=AF.Exp), r=[Eg], w=[Eg])
        k.op("dve", lambda e: e.tensor_scalar(out=rowp[:, 8:9], in0=sm[0:1, 0:1], scalar1=-1.0, scalar2=30000.0, op0=ALU.add, op1=ALU.mult), r=[sm], w=[rowp])
        k.op("dve", lambda e: e.tensor_tensor(out=rowp[:, 0:8], in0=bln[0:1, 0:8], in1=rb0b[0:1, :], op=ALU.add), r=[bln, rb0b, rowp], w=[rowp])
        k.op("dve", lambda e: e.tensor_scalar(out=rowp[:, 0:8], in0=rowp[:, 0:8], scalar1=rowp[:, 8:9], scalar2=None, op0=ALU.add), r=[rowp], w=[rowp])
        k.op("act", lambda e: e.activation(out=rowp[:, 0:8], in_=rowp[:, 0:8], func=AF.Exp), r=[rowp], w=[rowp])
        k.dma("sp", vnr[:], av[col:col + 1, :], r=["av"], w=[vnr])
        k.op("dve", lambda e: e.tensor_copy(out=vnf[:], in_=vnr[:]), r=[vnr], w=[vnf])
        for kvh in range(2):
            bon = k.bank()
            bod = k.bank()
            for half in range(2):
                k.op("pe", lambda e, kvh=kvh, half=half, bon=bon: e.matmul(bon[0:4, 0:128], lhsT=Eg[:, half, kvh * 4:(kvh + 1) * 4], rhs=Vsel[:, half, kvh * 128:(kvh + 1) * 128], start=(half == 0), stop=False),
                     r=[Eg, Vsel], w=[bon])
            k.op("pe", lambda e, kvh=kvh, bon=bon: e.matmul(bon[0:4, 0:128], lhsT=rowp[0:1, kvh * 4:(kvh + 1) * 4], rhs=vnf[0:1, kvh * 128:(kvh + 1) * 128], start=False, stop=True), r=[rowp, vnf], w=[bon])
            for half in range(2):
                k.op("pe", lambda e, kvh=kvh, half=half, bod=bod: e.matmul(bod[0:4, 0:1], lhsT=Eg[:, half, kvh * 4:(kvh + 1) * 4], rhs=ones4[:, 0:1], start=(half == 0), stop=False), r=[Eg, ones4], w=[bod])
            k.op("pe", lambda e, kvh=kvh, bod=bod: e.matmul(bod[0:4, 0:1], lhsT=rowp[0:1, kvh * 4:(kvh + 1) * 4], rhs=ones4[0:1, 0:1], start=False, stop=True), r=[rowp, ones4], w=[bod])
            k.op("dve", lambda e, kvh=kvh, bod=bod: e.reciprocal(out=osb[:, kvh, 128:129], in_=bod[0:4, 0:1]), r=[bod], w=[osb])
            k.op("dve", lambda e, kvh=kvh, bon=bon: e.tensor_scalar(out=osb[:, kvh, 0:128], in0=bon[0:4, 0:128], scalar1=osb[:, kvh, 128:129], scalar2=None, op0=ALU.mult), r=[bon, osb], w=[osb])
        bot = k.bank()
        for kvh in range(2):
            k.op("pe", lambda e, kvh=kvh: e.transpose(out=bot[:, kvh * 4:(kvh + 1) * 4], in_=osb[:, kvh, 0:128], identity=ident_f[0:4, 0:4]), r=[osb, ident_f], w=[bot])
        k.op("act", lambda e: e.copy(out=mixT[:, 8:16, col], in_=bot[:, 0:8]), r=[bot], w=[(mixT, "sb")])

    for s_i in range(0 if skip_p4s else TS):
        attend_seq(s_i)
    k.barrier()
    k.release(m4)
    m5 = k.mark()
    wout = k.alloc("wout", [128, 16, D], BF16)
    for g in range(4):
        k.dma("pool", wout[:, :, g * 512:(g + 1) * 512], w_out[:, g * 512:(g + 1) * 512].rearrange("(k p) n -> p k n", p=128), w=[(wout, g)])
    G1b = k.alloc("G1b", [128, D], BF16)
    A2b = k.alloc("A2b", [128, D], BF16)
    SH2b = k.alloc("SH2b", [128, D], BF16)
    for buf_, idx_ in ((G1b, 2), (SH2b, 3), (A2b, 4)):
        k.dma("pool", buf_[:], modd[0:1, idx_ * D:(idx_ + 1) * D].to_broadcast([128, D]), r=["modd"], w=[buf_])
    xt5 = [k.alloc(f"xt5_{i}", [128, D], F32) for i in range(2)]
    mos = [k.alloc(f"mo{i}", [128, D], F32) for i in range(2)]
    h2b = k.alloc("h2b", [128, D], BF16)
    jnk5 = k.alloc("jnk5", [128, D], BF16)
    st5 = [k.alloc(f"st5_{i}", [128, 8], F32) for i in range(2)]
    h2st = [k.alloc("h2st0", [128, 16, 128], BF16)] * 2

    def p5_tile(ti):
        c0, n = (ti * 128, 128) if ti < NT else (T, TS)
        x_ = xt5[ti % 2]
        mo = mos[ti % 2]
        s_ = st5[ti % 2]
        hs_ = h2st[ti % 2]
        G1_, A2_, SH2_ = (G1b, A2b, SH2b)
        if ti == NT:
            k.dma("pool", G1b[0:TS, :], modd[1:5, 2 * D:3 * D], r=["modd"], w=[G1b])
            k.dma("pool", SH2b[0:TS, :], modd[1:5, 3 * D:4 * D], r=["modd"], w=[SH2b])
            k.dma("pool", A2b[0:TS, :], modd[1:5, 4 * D:5 * D], r=["modd"], w=[A2b])
        if ti == 0:
            k.dma("sp", x_[0:n, :], xp[c0:c0 + n, :], w=[x_])
        if ti + 1 <= NT:
            tn = ti + 1
            cn, nn = (tn * 128, 128) if tn < NT else (T, TS)
            xn_ = xt5[tn % 2]
            k.dma("sp", xn_[0:nn, :], xp[cn:cn + nn, :] if tn < NT else xs.ap(), w=[xn_])
        mkeys = [(mixT, ti), (mixT, "b%d" % ti)] if ti < NT else [(mixT, "s%d" % j) for j in range(TS)] + [(mixT, "sb")]
        for nq in range(4):
            bk_ = k.bank()
            for kk in range(16):
                k.op("pe", lambda e, kk=kk, bk_=bk_, nq=nq: e.matmul(bk_[0:n, :], lhsT=mixT[:, kk, c0:c0 + n], rhs=wout[:, kk, nq * 512:(nq + 1) * 512], start=(kk == 0), stop=(kk == 15)),
                     r=mkeys + [(wout, nq)], w=[bk_])
            k.op("act", lambda e, bk_=bk_, nq=nq: e.copy(out=mo[0:n, nq * 512:(nq + 1) * 512], in_=bk_[0:n, :]), r=[bk_], w=[mo])
        k.op("act", lambda e: e.activation(out=jnk5[0:n, :], in_=mo[0:n, :], func=AF.Square, accum_out=s_[0:n, 0:1]), r=[mo], w=[jnk5, s_])
        k.op("act", lambda e: e.activation(out=s_[0:n, 1:2], in_=s_[0:n, 0:1], func=AF.Sqrt, scale=1.0 / D, bias=EPS), r=[s_], w=[s_])
        k.op("dve", lambda e: e.reciprocal(out=s_[0:n, 2:3], in_=s_[0:n, 1:2]), r=[s_], w=[s_])
        k.op("dve", lambda e: e.scalar_tensor_tensor(out=mo[0:n, :], in0=mo[0:n, :], scalar=s_[0:n, 2:3], op0=ALU.mult, in1=G1_[0:n, :], op1=ALU.mult), r=[mo, s_, G1_], w=[mo])
        k.op("dve", lambda e: e.tensor_tensor(out=x_[0:n, :], in0=x_[0:n, :], in1=mo[0:n, :], op=ALU.add), r=[x_, mo], w=[x_])
        k.dma("sp", x1d[c0:c0 + n, :], x_[0:n, :], r=[x_], w=["x1d"])
        k.op("act", lambda e: e.activation(out=jnk5[0:n, :], in_=x_[0:n, :], func=AF.Square, accum_out=s_[0:n, 3:4]), r=[x_, s_], w=[jnk5, s_])
        k.op("act", lambda e: e.activation(out=s_[0:n, 4:5], in_=s_[0:n, 3:4], func=AF.Sqrt, scale=1.0 / D, bias=EPS), r=[s_], w=[s_])
        k.op("dve", lambda e: e.reciprocal(out=s_[0:n, 5:6], in_=s_[0:n, 4:5]), r=[s_], w=[s_])
        k.op("dve", lambda e: e.scalar_tensor_tensor(out=mo[0:n, :], in0=x_[0:n, :], scalar=s_[0:n, 5:6], op0=ALU.mult, in1=A2_[0:n, :], op1=ALU.mult), r=[x_, s_, A2_, mo], w=[mo])
        k.op("dve", lambda e: e.tensor_tensor(out=h2b[0:n, :], in0=mo[0:n, :], in1=SH2_[0:n, :], op=ALU.add), r=[mo, SH2_], w=[h2b])
        b0 = k.bank()
        b1 = k.bank()
        for kk in range(16):
            bb = b0 if kk < 8 else b1
            k.op("pe", lambda e, kk=kk, bb=bb: e.transpose(out=bb[:].bitcast(BF16)[:, (kk % 8) * 128:(kk % 8) * 128 + n], in_=h2b[0:n, kk * 128:(kk + 1) * 128], identity=ident_b[0:n, 0:n]),
                 r=[h2b, ident_b], w=[bb])
        for half, bb in ((0, b0), (1, b1)):
            k.op("act", lambda e, half=half, bb=bb: e.copy(out=hs_[:, half * 8:(half + 1) * 8, 0:n], in_=bb[:].bitcast(BF16).rearrange("p (a b) -> p a b", a=8)[:, :, 0:n]),
                 r=[bb], w=[hs_])
        k.dma("sp", h2Td[:, :, c0:c0 + n], hs_[:, :, 0:n], r=[hs_], w=["h2Td"])

    for ti in range(NT + 1):
        p5_tile(ti)
    k.barrier()
    k.release(mP)
    if stop_after == "P5":
        return k.finish()

    alloc_wb()
    TB = 512
    NBLK6 = T // TB
    uT = k.alloc("uT", [128, 64, TB], BF16)
    uTs = k.alloc("uTs", [128, 64, TS], BF16)
    h2Tb = k.alloc("h2Tb", [128, 16, TB], BF16)
    h2Ts = k.alloc("h2Ts", [128, 16, TS], BF16)
    w2b = [k.alloc(f"w2b{i}", [128, 8, 512], BF16) for i in range(2)]
    fbuf = [k.alloc(f"fbuf{i}", [128, D], F32) for i in range(4)]
    x1t = [k.alloc("x1t0", [128, D], F32)] * 2
    G2b = k.alloc("G2b", [128, D], F32)
    load_mod_bcast(G2b, 5)
    rl6 = [k.alloc(f"rl6_{i}", [128, TB], BF16) for i in range(2)]
    st6 = [k.alloc(f"st6_{i}", [128, 4], F32) for i in range(2)]
    w2cnt = [0]
    k.dma("sp", h2Ts[:], h2Td[:, :, T:TT], r=["h2Td"], w=[h2Ts])

    def ffn_block(b):
        with_s = (b == NBLK6 - 1)
        if b == 0:
            k.dma("sp", h2Tb[:], h2Td[:, :, b * TB:(b + 1) * TB], r=["h2Td"], w=[h2Tb])
        def phaseA(g):
            wt = wb[wcnt[0] % 2]
            wcnt[0] += 1
            k.dma("sp", wt[:], w1s[g], r=[("w1s", g)], w=[wt])
            for j in range(4):
                ch = g * 4 + j
                bk_ = k.bank()
                for kk in range(16):
                    k.op("pe", lambda e, kk=kk, bk_=bk_, j=j: e.matmul(bk_[:, :], lhsT=wt[:, kk, j * 128:(j + 1) * 128], rhs=h2Tb[:, kk, :], start=(kk == 0), stop=(kk == 15)),
                         r=[wt, h2Tb], w=[bk_])
                r_ = rl6[ch % 2]
                k.op("act", lambda e, bk_=bk_, r_=r_: e.activation(out=r_[:], in_=bk_[:, :], func=AF.Relu), r=[bk_], w=[r_])
                k.op("dve", lambda e, r_=r_, ch=ch: e.tensor_tensor(out=uT[:, ch, :], in0=r_[:], in1=r_[:], op=ALU.mult), r=[r_], w=[(uT, ch)])
                if with_s:
                    bs_ = k.bank()
                    for kk in range(16):
                        k.op("pe", lambda e, kk=kk, bs_=bs_, j=j: e.matmul(bs_[:, 0:TS], lhsT=wt[:, kk, j * 128:(j + 1) * 128], rhs=h2Ts[:, kk, :], start=(kk == 0), stop=(kk == 15)),
                             r=[wt, h2Ts], w=[bs_])
                    k.op("act", lambda e, bs_=bs_, ch=ch: e.activation(out=uTs[:, ch, :], in_=bs_[:, 0:TS], func=AF.Relu), r=[bs_], w=[(uTs, ch)])
                    k.op("dve", lambda e, ch=ch: e.tensor_tensor(out=uTs[:, ch, :], in0=uTs[:, ch, :], in1=uTs[:, ch, :], op=ALU.mult), r=[(uTs, ch)], w=[(uTs, ch)])
        for g in range(16):
            phaseA(g)
        def phaseB(qc):
            accs = [k.bank() for _ in range(4)]
            accS = k.bank() if with_s else None
            k.reserved = {a_.name for a_ in accs} | ({accS.name} if with_s else set())
            for fgg in range(8):
                w2 = w2b[w2cnt[0] % 2]
                w2cnt[0] += 1
                k.dma("pool", w2[:], w2s[qc, fgg], r=[("w2s", qc, fgg)], w=[w2])
                for c in range(8):
                    ch = fgg * 8 + c
                    first = (fgg == 0 and c == 0)
                    last = (fgg == 7 and c == 7)
                    for tt in range(4):
                        k.op("pe", lambda e, tt=tt, c=c, ch=ch, first=first, last=last, w2=w2: e.matmul(accs[tt][:, :], lhsT=uT[:, ch, tt * 128:(tt + 1) * 128], rhs=w2[:, c, :], start=first, stop=last),
                             r=[(uT, ch), w2], w=[accs[tt]])
                    if with_s:
                        k.op("pe", lambda e, c=c, ch=ch, first=first, last=last, w2=w2: e.matmul(accS[0:TS, :], lhsT=uTs[:, ch, :], rhs=w2[:, c, :], start=first, stop=last),
                             r=[(uTs, ch), w2], w=[accS])
            for tt in range(4):
                k.op("act", lambda e, tt=tt: e.copy(out=fbuf[tt][:, qc * 512:(qc + 1) * 512], in_=accs[tt][:, :]), r=[accs[tt]], w=[(fbuf[tt], qc)])
            if with_s:
                k.op("act", lambda e: e.copy(out=fs[0:TS, qc * 512:(qc + 1) * 512], in_=accS[0:TS, :]), r=[accS], w=[(fs, qc)])
            k.reserved = set()
        for qc in range(4):
            phaseB(qc)
        if b + 1 < NBLK6:
            k.dma("sp", h2Tb[:], h2Td[:, :, (b + 1) * TB:(b + 2) * TB], r=["h2Td"], w=[h2Tb])
        def epi(fb, n, x1src, ydst, G2_, i):
            x_ = x1t[i % 2]
            s_ = st6[i % 2]
            k.dma("pool", x_[0:n, :], x1src, r=["x1d"], w=[x_])
            fk = [(fb, q) for q in range(4)]
            k.op("act", lambda e: e.activation(out=w2b[0][:].rearrange("p a b -> p (a b)")[0:n, 0:D], in_=fb[0:n, :], func=AF.Square, accum_out=s_[0:n, 0:1]), r=fk, w=[w2b[0], s_])
            k.op("act", lambda e: e.activation(out=s_[0:n, 1:2], in_=s_[0:n, 0:1], func=AF.Sqrt, scale=1.0 / D, bias=EPS), r=[s_], w=[s_])
            k.op("dve", lambda e: e.reciprocal(out=s_[0:n, 2:3], in_=s_[0:n, 1:2]), r=[s_], w=[s_])
            k.op("dve", lambda e: e.scalar_tensor_tensor(out=fb[0:n, :], in0=fb[0:n, :], scalar=s_[0:n, 2:3], op0=ALU.mult, in1=G2_[0:n, :], op1=ALU.mult), r=fk + [s_, G2_], w=fk)
            k.op("dve", lambda e: e.tensor_tensor(out=x_[0:n, :], in0=x_[0:n, :], in1=fb[0:n, :], op=ALU.add), r=[x_] + fk, w=[x_])
            k.dma("pool", ydst, x_[0:n, :], r=[x_])
        for tt in range(4):
            r0 = b * TB + tt * 128
            epi(fbuf[tt], 128, x1d[r0:r0 + 128, :], y_p[r0:r0 + 128, :], G2b, tt)
        if with_s:
            k.dma("pool", G2b[0:TS, :], modd[1:5, 5 * D:6 * D], r=["modd"], w=[G2b])
            epi(fs, TS, x1d[T:TT, :], y_s.ap(), G2b, 0)

    fs = k.alloc("fs", [TS, D], F32)
    import os
    for b in range(NBLK6):
        ffn_block(b)
    k.barrier()
    return k.finish()


_CACHE = {}


def _core_inputs(i, a):
    f = np.ascontiguousarray
    return {
        "xp": f(a["x_prompt"][i]),
        "xs": f(a["x_sample"][4 * i:4 * i + 4, 0, :]),
        "c5": f(np.concatenate([a["c_prompt"][i:i + 1], a["c_sample"][4 * i:4 * i + 4]], axis=0)),
        "w_ada": f(a["w_ada"][0]),
        "b_ada": f(a["b_ada"][0][None, :]),
        "gvec": f(np.stack([a["pre1_g"][0], a["post1_g"][0], a["pre2_g"][0], a["post2_g"][0]])),
        "w_in": f(a["w_in"][0]),
        "w_out": f(a["w_out"][0]),
        "w_ff1": f(a["w_ff1"][0]),
        "w_ff2": f(a["w_ff2"][0]),
        "conv_w": f(a["conv_w"][0]),
        "st_conv": f(a["state_conv"][0, 4 * i:4 * i + 4].reshape(12, 3072)),
        "hv": f(np.concatenate([a["a_log"][0], a["dt_bias"][0]])[None, :]),
        "ln_gb": f(np.stack([a["idx_knorm_g"][0], a["idx_knorm_b"][0]])),
        "gdn_g": f(a["gdn_norm_g"][0][None, :]),
        "st_ssm": f(a["state_ssm"][0, 4 * i:4 * i + 4]),
        "rel_bias": f(a["rel_bias"]),
        "boh": _boh(),
        "bthr": _bthr(),
        "page_table": f(a["page_table"][4 * i:4 * i + 4]),
        "cache_kidx": a["cache_kidx"][0].reshape(-1, PAGE * 128),
        "cache_k": a["cache_k"][0].reshape(-1, 256),
        "cache_v": a["cache_v"][0].reshape(-1, 256),
    }


def kernel(**inputs):
    n = 8
    nc = build(n_pool=int(inputs["cache_k"].shape[1]))
    in_maps = [_core_inputs(i, inputs) for i in range(n)]
    res = run_bass_kernel_spmd(nc, in_maps, core_ids=list(range(n)))
    R = res.results
    cat = lambda name: np.stack([r[name] for r in R])
    y_p = cat("y_p")
    y_s = np.concatenate([r["y_s"] for r in R])[:, None, :]
    k_p = cat("k_p").reshape(1, 8, T, 2, 128)
    v_p = cat("v_p").reshape(1, 8, T, 2, 128)
    ki_p = cat("ki_p")[None]
    ssm_p = cat("ssm_p")[None]
    conv_p = cat("conv_p")[None]
    k_s = np.concatenate([r["k_s"] for r in R]).reshape(1, 32, 1, 2, 128)
    v_s = np.concatenate([r["v_s"] for r in R]).reshape(1, 32, 1, 2, 128)
    ki_s = np.concatenate([r["ki_s"] for r in R]).reshape(1, 32, 1, 128)
    ssm_s = np.concatenate([r["ssm_s"] for r in R])[None]
    conv_s = np.concatenate([r["conv_s"] for r in R])[None]
    return (y_p, y_s, k_p, v_p, ki_p, ssm_p, conv_p, k_s, v_s, ki_s, ssm_s, conv_s)
```
